# Optimizing a Trainium2 kernel written in Bass

```python
import math
import jax, jax.numpy as jnp
from jax import lax
import numpy as np

D_MODEL = 1024
BATCH = 8
SEQ = 4096
DEPTH = 1

M_HEADS = 4
M_HEAD_DIM = 128
M_WIDTH = M_HEADS * M_HEAD_DIM
CONV_WIDTH = 4
CHUNK = 64
D_HEADS = 4
D_HEAD_DIM = 64
D_V_DIM = 2 * D_HEAD_DIM
D_WIDTH = D_HEADS * D_V_DIM
ROPE_THETA = 500000.0
ROPE_DIM = D_HEAD_DIM // 4
Q_BLOCK = 128
N_GROUPS = 4
EXPERTS_PER_GROUP = 8
N_EXPERTS = N_GROUPS * EXPERTS_PER_GROUP
TOP_K = 2
D_EXPERT = D_MODEL // 4
EPS = 1e-6
IN_SIZES = (M_WIDTH, M_WIDTH, M_WIDTH, M_WIDTH, M_HEADS, M_HEADS,
            D_WIDTH, D_WIDTH, D_WIDTH, D_MODEL, D_MODEL)
IN_COLS = 4 * M_WIDTH + 2 * M_HEADS + 3 * D_WIDTH + 2 * D_MODEL

kernel_name = "hybrid_mlstm_diffattn_hmoe_block"


def rmsnorm(x, w):
    x32 = x.astype(jnp.float32)
    r = x32 * lax.rsqrt(jnp.mean(x32 * x32, axis=-1, keepdims=True) + EPS)
    return (r * w.astype(jnp.float32)).astype(x.dtype)


def to_heads(t, h):
    b, s, _ = t.shape
    return t.reshape(b, s, h, -1).transpose(0, 2, 1, 3)


def causal_conv(x, w, b):
    s = x.shape[1]
    xp = jnp.pad(x, ((0, 0), (CONV_WIDTH - 1, 0), (0, 0)))
    out = b
    for j in range(CONV_WIDTH):
        out = out + w[j] * xp[:, j:j + s]
    return out


def partial_rope(t, positions):
    inv_freq = ROPE_THETA ** (-jnp.arange(0, ROPE_DIM, 2, dtype=jnp.float32) / ROPE_DIM)
    ang = positions.astype(jnp.float32)[..., None] * inv_freq
    cos = jnp.cos(ang)[:, None, None].astype(t.dtype)
    sin = jnp.sin(ang)[:, None, None].astype(t.dtype)
    rot, rest = t[..., :ROPE_DIM], t[..., ROPE_DIM:]
    r1, r2 = jnp.split(rot, 2, axis=-1)
    rot = jnp.concatenate([r1 * cos - r2 * sin, r2 * cos + r1 * sin], axis=-1)
    return jnp.concatenate([rot, rest], axis=-1)


def mlstm_chunkwise(q, k, v, log_i, log_f):
    b_, h_, s_, d_ = q.shape
    nc = s_ // CHUNK
    chunks = lambda t: t.reshape(b_, h_, nc, CHUNK, *t.shape[3:]).transpose(2, 0, 1, 3, *range(4, t.ndim + 1))
    qc, kc, vc = chunks(q), chunks(k), chunks(v)
    ic, fc = chunks(log_i), chunks(log_f)
    causal = jnp.tril(jnp.ones((CHUNK, CHUNK), dtype=bool))

    def step(carry, inp):
        C, n, m = carry
        qt, kt, vt, it, ft = inp
        bcum = jnp.cumsum(ft, axis=-1)
        dmat = jnp.where(causal, bcum[..., :, None] - bcum[..., None, :] + it[..., None, :], -jnp.inf)
        m_inter = bcum + m[..., None]
        m_t = jnp.maximum(m_inter, jnp.max(dmat, axis=-1))
        s = jnp.einsum('bhtd,bhsd->bhts', qt, kt) * jnp.exp(dmat - m_t[..., None])
        inter = jnp.exp(m_inter - m_t)
        num = jnp.einsum('bhts,bhsd->bhtd', s, vt) + inter[..., None] * jnp.einsum('bhtk,bhkv->bhtv', qt, C)
        den = jnp.sum(s, axis=-1) + inter * jnp.einsum('bhtk,bhk->bht', qt, n)
        den = jnp.maximum(jnp.abs(den), jnp.exp(-m_t))
        h = num / den[..., None]
        b_last = bcum[..., -1]
        w = b_last[..., None] - bcum + it
        m_new = jnp.maximum(b_last + m, jnp.max(w, axis=-1))
        wexp = jnp.exp(w - m_new[..., None])
        decay = jnp.exp(b_last + m - m_new)
        C_new = decay[..., None, None] * C + jnp.einsum('bhs,bhsk,bhsv->bhkv', wexp, kt, vt)
        n_new = decay[..., None] * n + jnp.einsum('bhs,bhsk->bhk', wexp, kt)
        return (C_new, n_new, m_new), h

    init = (jnp.zeros((b_, h_, d_, d_), jnp.float32), jnp.zeros((b_, h_, d_), jnp.float32),
            jnp.zeros((b_, h_), jnp.float32))
    _, hs = lax.scan(step, init, (qc, kc, vc, ic, fc))
    return hs.transpose(1, 2, 0, 3, 4).reshape(b_, h_, s_, d_)


def diff_attention(q, k, v, lam):
    b_, h_, _, s_, d_ = q.shape
    nb = s_ // Q_BLOCK
    qb = q.reshape(b_, h_, 2, nb, Q_BLOCK, d_).transpose(3, 0, 1, 2, 4, 5)
    scale = d_ ** -0.5
    kpos = jnp.arange(s_)

    def block(args):
        qi, i = args
        sc = jnp.einsum('bhmqd,bhmkd->bhmqk', qi, k).astype(jnp.float32) * scale
        qpos = i * Q_BLOCK + jnp.arange(Q_BLOCK)
        sc = jnp.where(kpos[None, :] <= qpos[:, None], sc, -jnp.inf)
        p = jax.nn.softmax(sc, axis=-1)
        a = p[:, :, 0] - lam * p[:, :, 1]
        return jnp.einsum('bhqk,bhkd->bhqd', a.astype(v.dtype), v)

    out = lax.map(block, (qb, jnp.arange(nb)))
    return out.transpose(1, 2, 0, 3, 4).reshape(b_, h_, s_, v.shape[-1])


def hierarchical_moe(h, w_rg, b_rg, w_re, b_re, w_gate, w_up, w_down):
    b_, s_, d_ = h.shape
    hf = h.reshape(-1, d_)
    g_logits = (hf @ w_rg + b_rg).astype(jnp.float32)
    pg = jax.nn.softmax(g_logits, axis=-1)
    pg_top, g_idx = lax.top_k(pg, 1)
    e_logits = (hf @ w_re + b_re).astype(jnp.float32).reshape(-1, N_GROUPS, EXPERTS_PER_GROUP)
    e_sel = jnp.take_along_axis(e_logits, g_idx[:, :, None], axis=1)[:, 0]
    pe = jax.nn.softmax(e_sel, axis=-1)
    pe_top, j_idx = lax.top_k(pe, TOP_K)
    weights = pg_top * pe_top / jnp.sum(pe_top, axis=-1, keepdims=True)
    e_idx = g_idx * EXPERTS_PER_GROUP + j_idx
    combine = jnp.sum(jax.nn.one_hot(e_idx, N_EXPERTS, dtype=jnp.float32) * weights[..., None], axis=1)
    combine = combine.astype(h.dtype)
    y = jnp.zeros_like(hf)
    for e in range(N_EXPERTS):
        act = jax.nn.silu(hf @ w_gate[e]) * (hf @ w_up[e])
        y = y + combine[:, e:e + 1] * (act @ w_down[e])
    return y.reshape(b_, s_, d_)


def setup_inputs(seed: int = 0) -> dict:
    key = jax.random.key(seed)
    ks = iter(jax.random.split(key, 40))
    nrm = lambda shape, s: jax.random.normal(next(ks), shape, jnp.float32) * s
    L = DEPTH
    x = nrm((BATCH, SEQ, D_MODEL), 1.0)
    c = nrm((BATCH, D_MODEL), 1.0)
    positions = (jnp.arange(SEQ, dtype=jnp.int32)[None, :]
                 + jax.random.randint(next(ks), (BATCH, 1), 0, 1024, dtype=jnp.int32))
    f_bias = jnp.broadcast_to(jnp.linspace(3.0, 6.0, M_HEADS, dtype=jnp.float32), (L, M_HEADS))
    return {
        "x": x, "c": c, "positions": positions,
        "w_ada": nrm((L, D_MODEL, 6 * D_MODEL), 0.5 * D_MODEL ** -0.5),
        "b_ada": nrm((L, 6 * D_MODEL), 0.02),
        "norm1_w": 1.0 + nrm((L, D_MODEL), 0.02),
        "w_in": nrm((L, D_MODEL, IN_COLS), D_MODEL ** -0.5),
        "b_igate": nrm((L, M_HEADS), 0.1),
        "b_fgate": f_bias + nrm((L, M_HEADS), 0.1),
        "conv_w": nrm((L, CONV_WIDTH, 2 * M_WIDTH), CONV_WIDTH ** -0.5),
        "conv_b": nrm((L, 2 * M_WIDTH), 0.02),
        "mlstm_norm_w": 1.0 + nrm((L, M_HEADS, M_HEAD_DIM), 0.02),
        "q_norm_w": 1.0 + nrm((L, D_HEAD_DIM), 0.02),
        "k_norm_w": 1.0 + nrm((L, D_HEAD_DIM), 0.02),
        "lam_q1": nrm((L, D_HEAD_DIM), 0.1),
        "lam_k1": nrm((L, D_HEAD_DIM), 0.1),
        "lam_q2": nrm((L, D_HEAD_DIM), 0.1),
        "lam_k2": nrm((L, D_HEAD_DIM), 0.1),
        "subln_w": 1.0 + nrm((L, D_V_DIM), 0.02),
        "w_br_m": nrm((L, M_WIDTH, D_MODEL), M_WIDTH ** -0.5),
        "w_br_d": nrm((L, D_WIDTH, D_MODEL), D_WIDTH ** -0.5),
        "w_out": nrm((L, D_MODEL, D_MODEL), D_MODEL ** -0.5),
        "norm2_w": 1.0 + nrm((L, D_MODEL), 0.02),
        "w_rg": nrm((L, D_MODEL, N_GROUPS), D_MODEL ** -0.5),
        "b_rg": nrm((L, N_GROUPS), 0.01),
        "w_re": nrm((L, D_MODEL, N_EXPERTS), D_MODEL ** -0.5),
        "b_re": nrm((L, N_EXPERTS), 0.01),
        "w_gate": nrm((L, N_EXPERTS, D_MODEL, D_EXPERT), D_MODEL ** -0.5),
        "w_up": nrm((L, N_EXPERTS, D_MODEL, D_EXPERT), D_MODEL ** -0.5),
        "w_down": nrm((L, N_EXPERTS, D_EXPERT, D_MODEL), D_EXPERT ** -0.5),
    }


def reference(x, c, positions, w_ada, b_ada, norm1_w, w_in, b_igate, b_fgate, conv_w, conv_b,
              mlstm_norm_w, q_norm_w, k_norm_w, lam_q1, lam_k1, lam_q2, lam_k2, subln_w,
              w_br_m, w_br_d, w_out, norm2_w, w_rg, b_rg, w_re, b_re, w_gate, w_up, w_down):
    split_idx = [int(v) for v in np.cumsum(IN_SIZES)[:-1]]
    for l in range(DEPTH):
        mod = (c @ w_ada[l] + b_ada[l])[:, None, :]
        shift1, scale1, gate1, shift2, scale2, gate2 = jnp.split(mod, 6, axis=-1)

        h = rmsnorm(x, norm1_w[l]) * (1.0 + scale1) + shift1
        proj = h @ w_in[l]
        (q_m, k_m, v_m, o_m, i_pre, f_pre, q_d, k_d, v_d, g_m, g_d) = jnp.split(proj, split_idx, axis=-1)

        qk = jax.nn.silu(causal_conv(jnp.concatenate([q_m, k_m], axis=-1), conv_w[l], conv_b[l]))
        q_m, k_m = jnp.split(qk, 2, axis=-1)
        qh = to_heads(q_m, M_HEADS).astype(jnp.float32)
        kh = to_heads(k_m, M_HEADS).astype(jnp.float32) * (M_HEAD_DIM ** -0.5)
        vh = to_heads(v_m, M_HEADS).astype(jnp.float32)
        log_i = (i_pre + b_igate[l]).astype(jnp.float32).transpose(0, 2, 1)
        log_f = jax.nn.log_sigmoid((f_pre + b_fgate[l]).astype(jnp.float32)).transpose(0, 2, 1)
        hm = mlstm_chunkwise(qh, kh, vh, log_i, log_f).astype(x.dtype)
        hm = rmsnorm(hm.transpose(0, 2, 1, 3), mlstm_norm_w[l])
        hm = hm.reshape(x.shape[0], x.shape[1], M_WIDTH) * jax.nn.sigmoid(o_m)

        b_, s_ = x.shape[0], x.shape[1]
        qd = q_d.reshape(b_, s_, D_HEADS, 2, D_HEAD_DIM).transpose(0, 2, 3, 1, 4)
        kd = k_d.reshape(b_, s_, D_HEADS, 2, D_HEAD_DIM).transpose(0, 2, 3, 1, 4)
        qd = partial_rope(rmsnorm(qd, q_norm_w[l]), positions)
        kd = partial_rope(rmsnorm(kd, k_norm_w[l]), positions)
        vd = to_heads(v_d, D_HEADS)
        lam_init = 0.8 - 0.6 * math.exp(-0.3 * l)
        lam = (jnp.exp(jnp.sum(lam_q1[l] * lam_k1[l])) - jnp.exp(jnp.sum(lam_q2[l] * lam_k2[l]))
               + lam_init).astype(x.dtype)
        hd = diff_attention(qd, kd, vd, lam)
        hd = rmsnorm(hd.transpose(0, 2, 1, 3), subln_w[l]) * (1.0 - lam_init)
        hd = hd.reshape(b_, s_, D_WIDTH)

        merged = jax.nn.sigmoid(g_m) * (hm @ w_br_m[l]) + jax.nn.sigmoid(g_d) * (hd @ w_br_d[l])
        x = x + gate1 * (merged @ w_out[l])

        h2 = rmsnorm(x, norm2_w[l]) * (1.0 + scale2) + shift2
        x = x + gate2 * hierarchical_moe(h2, w_rg[l], b_rg[l], w_re[l], b_re[l],
                                         w_gate[l], w_up[l], w_down[l])
    return x
```

```python
import math
import os
from contextlib import ExitStack

import numpy as np
import concourse.bass as bass
import concourse.mybir as mybir
from concourse.bass_utils import run_bass_kernel_spmd

F32 = mybir.dt.float32
BF16 = mybir.dt.bfloat16
I32 = mybir.dt.int32
AF = mybir.ActivationFunctionType
ALU = mybir.AluOpType
AX = mybir.AxisListType

S = 4096
D = 1024
NT = 32
NB = 8
EPS = 1e-6
NCST = 5 * 128 + 2


class _StopBuild(Exception):
    pass


class Trk:
    LIMIT = 50000
    NDSEM = 12

    def __init__(self, nc):
        self.nc = nc
        self.eng = {"pe": nc.tensor, "act": nc.scalar, "dve": nc.vector, "pool": nc.gpsimd, "sp": nc.sync}
        self.cur = {}
        self.seq = {}
        self.gen = {}
        for e in self.eng:
            self.cur[e] = [nc.alloc_semaphore(f"s_{e}_0"), 0]
            self.seq[e] = 0
            self.gen[e] = 0
        self.allsems = [(e, self.cur[e]) for e in self.eng]
        self.waited = {e: {} for e in self.eng}
        self.lastw = {}
        self.readers = {}
        self.children = {}
        self.dring = {}
        for q in ("sp", "pool", "act", "wc"):
            self.dring[q] = [[nc.alloc_semaphore(f"d_{q}_{i}"), 0] for i in range(self.NDSEM)]
        self.dpos = {q: 0 for q in self.dring}
        self.all_dma = []

    def _related(self, k):
        ks = [k[:i] for i in range(1, len(k) + 1)]
        stack = [k]
        while stack:
            p = stack.pop()
            for c in self.children.get(p, ()):
                ks.append(c)
                stack.append(c)
        return ks

    def _register(self, k):
        for i in range(1, len(k)):
            self.children.setdefault(k[:i], set()).add(k[:i + 1])

    def _wait(self, e, ev):
        sem, val, src, sq = ev
        if src == e:
            if e == "pe":
                return
            if e != "pool" and self.seq[e] - sq >= 6:
                return
        w = self.waited[e]
        if w.get(sem.name, 0) >= val:
            return
        self.eng[e].wait_ge(sem, val)
        w[sem.name] = val

    def _deps(self, e, r, w):
        evs = []
        for k in r:
            for kk in self._related(k):
                if kk in self.lastw:
                    evs.append(self.lastw[kk])
        for k in w:
            for kk in self._related(k):
                if kk in self.lastw:
                    evs.append(self.lastw[kk])
                evs.extend(self.readers.get(kk, {}).values())
        for ev in evs:
            self._wait(e, ev)

    def _record(self, ev, r, w):
        for k in r:
            self._register(k)
            self.readers.setdefault(k, {})[ev[2] if ev[2] is not None else ev[0].name] = ev
        for k in w:
            self._register(k)
            self.lastw[k] = ev
            self.readers[k] = {}
            for kk in self._related(k):
                if kk != k and len(kk) > len(k):
                    self.readers[kk] = {}
                    self.lastw.pop(kk, None)

    @staticmethod
    def _norm(ks):
        out = []
        for k in ks:
            k = tuple(k) if isinstance(k, (tuple, list)) else (k,)
            if k[0] == "ps":
                k = k[:2]
            out.append(k)
        return out

    def op(self, e, fn, r=(), w=()):
        r = self._norm(r)
        w = self._norm(w)
        w = w + [k for k in r if k[0] == "ps" and k not in w]
        self._deps(e, r, w)
        ins = fn()
        c = self.cur[e]
        c[1] += 1
        self.seq[e] += 1
        ins.then_inc(c[0], 1)
        ev = (c[0], c[1], e, self.seq[e])
        self._record(ev, r, w)
        if c[1] >= self.LIMIT:
            self.gen[e] += 1
            self.cur[e] = [self.nc.alloc_semaphore(f"s_{e}_{self.gen[e]}"), 0]
            self.allsems.append((e, self.cur[e]))
        return ins

    def dma(self, q, out, in_, r=(), w=(), ring=None, **kw):
        r = self._norm(r)
        w = self._norm(w)
        rq = ring or q
        ring = self.dring[rq]
        slot = ring[self.dpos[rq] % self.NDSEM]
        self.dpos[rq] += 1
        if slot[1] > 0:
            self._wait(q, (slot[0], slot[1], None, 0))
        self._deps(q, r, w)
        ins = self.eng[q].dma_start(out=out, in_=in_, **kw)
        slot[1] += 16
        ins.then_inc(slot[0], 16)
        ev = (slot[0], slot[1], None, 0)
        self._record(ev, r, w)
        return ins

    def dmai(self, out, in_, out_idx=None, in_idx=None, r=(), w=()):
        q = "pool"
        r = self._norm(r)
        w = self._norm(w)
        ring = self.dring[q]
        slot = ring[self.dpos[q] % self.NDSEM]
        self.dpos[q] += 1
        if slot[1] > 0:
            self._wait(q, (slot[0], slot[1], None, 0))
        self._deps(q, r, w)
        ins = self.nc.gpsimd.indirect_dma_start(
            out=out, out_offset=(bass.IndirectOffsetOnAxis(ap=out_idx, axis=0) if out_idx is not None else None),
            in_=in_, in_offset=(bass.IndirectOffsetOnAxis(ap=in_idx, axis=0) if in_idx is not None else None))
        slot[1] += 16
        ins.then_inc(slot[0], 16)
        ev = (slot[0], slot[1], None, 0)
        self._record(ev, r, w)
        return ins

    def barrier(self, full=True):
        evs = []
        for (se, c) in self.allsems:
            if c[1] > 0:
                evs.append((c[0], c[1], "__" + se, 0))
        for q, ring in self.dring.items():
            if q == "wc" and not full:
                continue
            for slot in ring:
                if slot[1] > 0:
                    evs.append((slot[0], slot[1], None, 0))
        for e in self.eng:
            for ev in evs:
                if ev[2] == "__" + e:
                    continue
                self._wait(e, ev)
        self.lastw.clear()
        self.readers.clear()
        self.children.clear()


def build(stage=99, dbg=None):
    nc = bass.Bass("TRN2", target_bir_lowering=False)
    T = Trk(nc)
    _scope = [None]

    def scope_mark(nm):
        if os.environ.get("KSCOPE") != "1":
            return
        if _scope[0] is not None:
            _scope[0].__exit__(None, None, None)
        _scope[0] = nc.named_scope(nm)
        _scope[0].__enter__()

    PE, ACT, DVE, POOL, SP = "pe", "act", "dve", "pool", "sp"

    def din(name, shape, dt=F32):
        return nc.dram_tensor(name, list(shape), dt, kind="ExternalInput").ap()

    def dscr(name, shape, dt):
        return nc.dram_tensor(name, list(shape), dt, kind="Internal").ap()

    x_in = din("x", [S, D])
    cT_in = din("cT", [128, 8])
    pos_in = din("pos", [1, S], I32)
    w_ada = din("w_ada", [D, 6 * D])
    b_ada_col = din("b_ada_col", [128, 48])
    b_ada_row = din("b_ada_row", [1, 6 * D])
    n1w_in = din("n1w", [128, 8])
    n2w_in = din("n2w", [128, 8])
    n2row_in = din("n2row", [1, D])
    w_in = din("w_in", [D, 5640])
    bgate_in = din("bgate", [4, 2])
    convw_in = din("convw", [128, 8, 4])
    convb_in = din("convb", [128, 8])
    mnw_in = din("mnw", [1, 512])
    qnw_in = din("qnw", [128, 1])
    knw_in = din("knw", [128, 1])
    qkrow_in = din("qkrow", [1, 128])
    lamv_in = din("lamv", [1, 256])
    subw_in = din("subw", [128, 1])
    w_br_m = din("w_br_m", [512, D])
    w_br_d = din("w_br_d", [512, D])
    w_out = din("w_out", [D, D])
    wr_in = din("wr", [D, 36])
    br_in = din("br", [1, 36])
    w_gate = din("w_gate", [32, D, 256])
    w_up = din("w_up", [32, D, 256])
    w_down = din("w_down", [32, 256, D])
    consts_in = din("consts", [128, NCST])
    out_d = nc.dram_tensor("out", [S, D], F32, kind="ExternalOutput").ap()

    xnT_d = dscr("xnT_d", [D, S], BF16)
    hmT_d = dscr("hmT_d", [512, S], BF16)
    hdT_d = dscr("hdT_d", [512, S], BF16)
    x1_d = dscr("x1_d", [S, D], F32)
    xn2T_d = dscr("xn2T_d", [D, S], BF16)
    combT_d = dscr("combT_d", [32, S], BF16)
    dec_d = dscr("dec_d", [4, 32], F32)
    gi_d = dscr("gi_d", [4, S], F32)
    NSLOT = 96
    X2_d = dscr("X2_d", [S, D], BF16)
    Xs_d = dscr("Xs_d", [NSLOT * 128, D], BF16)
    Ys_d = dscr("Ys_d", [NSLOT * 128, D], BF16)
    Wcat_d = dscr("Wcat_d", [32 * 128, 6144], BF16)
    gf_d = dscr("gf_d", [4, S], F32)

    dbg_out = {}
    if dbg:
        for name, (shape, dt) in dbg.items():
            dbg_out[name] = nc.dram_tensor("dbg_" + name, list(shape), dt, kind="ExternalOutput").ap()

    PS = [nc.alloc_psum_tensor(f"ps{i}", [128, 512], F32) for i in range(8)]

    def psb(i):
        return PS[i][:].bitcast(BF16)

    xnT_v = xnT_d.rearrange("(k p) t -> p k t", p=128)
    xn2T_v = xn2T_d.rearrange("(k p) t -> p k t", p=128)
    hmT_v = hmT_d.rearrange("(k p) t -> p k t", p=128)
    hdT_v = hdT_d.rearrange("(k p) t -> p k t", p=128)
    w_in_v = w_in.rearrange("(k p) n -> p k n", p=128)
    wg_v = w_gate.rearrange("e (k p) f -> e p k f", p=128)
    wu_v = w_up.rearrange("e (k p) f -> e p k f", p=128)
    wdn_v = w_down.rearrange("e (k p) n -> e p k n", p=128)

    glob = ExitStack()

    def sbg(name, shape, dt):
        return glob.enter_context(nc.sbuf_tensor("s_" + name, list(shape), dt))

    cst = sbg("cst", [128, NCST], F32)
    cstb = sbg("cstb", [128, 5 * 128], BF16)
    modc = sbg("modc", [128, 48], F32)
    a1 = sbg("a1", [128, 8], F32)
    a2 = sbg("a2", [128, 8], F32)
    g1b = sbg("g1b", [128, D], F32)
    g2b = sbg("g2b", [128, D], F32)
    a2b = sbg("a2b", [128, D], F32)
    s2b = sbg("s2b", [128, D], F32)
    rank_all = sbg("rank_all", [128, NT, 32], F32)
    m1_all = sbg("m1_all", [128, NT, 32], F32)
    m2_all = sbg("m2_all", [128, NT, 32], F32)
    w12 = sbg("w12", [128, NT, 2], F32)
    tot = sbg("tot", [128, 32], F32)
    smallc = sbg("smallc", [128, 16], F32)
    ident_f = cst[:, 0:128]
    tri_f = cst[:, 128:256]
    invf = cst[:, 640:641]
    ident_b = cstb[:, 0:128]
    tri_b = cstb[:, 128:256]
    bo64_b = cstb[:, 256:384]
    RT_b = cstb[:, 384:512]
    ones_b = cstb[:, 512:640]
    ones_f = cst[:, 512:640]

    T.dma(SP, cst[:], consts_in[:, :], w=["cst"])
    T.op(DVE, lambda: nc.vector.tensor_copy(out=cstb[:], in_=cst[:, 0:640]), r=["cst"], w=["cstb"])

    early = ExitStack()
    wst = [early.enter_context(nc.sbuf_tensor(f"s_wst{i}", [128, 6144], BF16)) for i in range(2)]
    for e in range(32):
        b = e % 2
        gu = wst[b][:, 0:4096].rearrange("p (k f) -> p k f", k=8)
        T.dma(POOL, gu[:, :, 0:256], wg_v[e], w=[("wst", b, 0)], ring="wc")
        T.dma(POOL, gu[:, :, 256:512], wu_v[e], w=[("wst", b, 1)], ring="wc")
        T.dma(POOL, wst[b][:, 4096:6144].rearrange("p (k n) -> p k n", k=2), wdn_v[e], w=[("wst", b, 2)], ring="wc")
        T.dma(POOL, Wcat_d[e * 128:(e + 1) * 128, :], wst[b][:], r=[("wst", b)], w=[("Wcat", e)], ring="wc")
    early_open = [True]
    scope_mark("ph0")
    with ExitStack() as ph:
        def sb(name, shape, dt):
            return ph.enter_context(nc.sbuf_tensor("s_" + name, list(shape), dt))
        cT = sb("cT", [128, 8], F32)
        cT2 = sb("cT2", [128, 8, 2], F32)
        cbc = sb("cbc", [128, 8, 128], F32)
        wa = [sb(f"wa{i}", [128, 8, 512], F32) for i in range(2)]
        bcol = sb("bcol", [128, 48], F32)
        brow = sb("brow", [128, 2048], F32)
        nw = sb("nw", [128, 16], F32)
        tmp8 = sb("tmp8", [128, 8], F32)
        lamb = sb("lamb", [128, 256], F32)
        qkrow = sb("qkrow", [128, 128], F32)
        lt = sb("lt", [128, 128], F32)
        l2 = sb("l2", [128, 4], F32)

        T.dma(SP, cT[:], cT_in[:, :], w=["cT"])
        T.dma(SP, bcol[:], b_ada_col[:, :], w=["bcol"])
        T.dma(SP, brow[:, 0:1024], b_ada_row[0:1, 2048:3072].partition_broadcast(128), w=["brow0"])
        T.dma(SP, brow[:, 1024:2048], b_ada_row[0:1, 5120:6144].partition_broadcast(128), w=["brow1"])
        T.dma(SP, nw[:, 0:8], n1w_in[:, :], w=["nw0"])
        T.dma(SP, nw[:, 8:16], n2w_in[:, :], w=["nw1"])
        T.dma(SP, smallc[:, 0:1], qnw_in[:, :], w=["sc0"])
        T.dma(SP, smallc[:, 1:2], knw_in[:, :], w=["sc1"])
        T.dma(SP, smallc[:, 2:3], subw_in[:, :], w=["sc2"])
        T.dma(SP, lamb[:], lamv_in[0:1, :].partition_broadcast(128), w=["lamb"])
        T.dma(SP, qkrow[:], qkrow_in[0:1, :].partition_broadcast(128), w=["qkrow"])
        for k in range(2):
            T.op(DVE, lambda k=k: nc.vector.tensor_copy(out=cT2[:, :, k], in_=cT[:]), r=["cT"], w=[("cT2", k)])
        for kc in range(8):
            T.op(DVE, lambda kc=kc: nc.vector.tensor_copy(out=cbc[:, kc, :], in_=cT[:, kc:kc + 1].to_broadcast([128, 128])),
                 r=["cT"], w=[("cbc", kc)])
        w_ada_v = w_ada.rearrange("(k p) n -> p k n", p=128)
        gate_bank = {4: 1, 5: 2, 6: 5, 7: 6, 8: 1, 9: 2, 10: 3, 11: 4}
        n2rb = sb("n2rb", [128, D], F32)
        T.dma(SP, n2rb[:], n2row_in[0:1, :].partition_broadcast(128), w=["n2rb"])
        T.dma(SP, s2b[:], b_ada_row[0:1, 3072:4096].partition_broadcast(128), w=["s2b_bias"])
        T.dma(SP, a2b[:], b_ada_row[0:1, 4096:5120].partition_broadcast(128), w=["a2b_bias"])
        for blk in range(12):
            b = blk % 2
            for hh in range(2):
                T.dma(SP, wa[b][:, 4 * hh:4 * hh + 4, :], w_ada_v[:, 4 * hh:4 * hh + 4, blk * 512:(blk + 1) * 512], w=[("wa", b)])
            for m in range(4):
                j = blk * 4 + m
                for kc in range(8):
                    T.op(PE, lambda j=j, m=m, kc=kc, b=b: nc.tensor.matmul(
                        PS[0][:, 2 * j:2 * j + 2], lhsT=wa[b][:, kc, m * 128:(m + 1) * 128], rhs=cT2[:, kc, :],
                        start=(kc == 0), stop=(kc == 7)), r=[("wa", b), "cT2"], w=[("ps", 0)])
            if blk in gate_bank:
                gb = gate_bank[blk]
                for kc in range(8):
                    T.op(PE, lambda kc=kc, b=b, gb=gb: nc.tensor.matmul(
                        PS[gb][:, :], lhsT=cbc[:, kc, :], rhs=wa[b][:, kc, :], start=(kc == 0), stop=(kc == 7)),
                        r=[("wa", b), "cbc"], w=[("ps", gb)])
                if blk == 5:
                    for hh in range(2):
                        T.op(DVE, lambda hh=hh: nc.vector.tensor_tensor(out=g1b[:, hh * 512:(hh + 1) * 512], in0=PS[1 + hh][:, :],
                                                                      in1=brow[:, hh * 512:(hh + 1) * 512], op=ALU.add),
                             r=[("ps", 1 + hh), "brow0"], w=[("g1b", hh)])
                if blk == 7:
                    for hh in range(2):
                        T.op(DVE, lambda hh=hh: nc.vector.tensor_tensor(out=s2b[:, hh * 512:(hh + 1) * 512], in0=PS[5 + hh][:, :],
                                                                      in1=s2b[:, hh * 512:(hh + 1) * 512], op=ALU.add),
                             r=[("ps", 5 + hh), "s2b_bias"], w=[("s2b", hh)])
                if blk == 9:
                    for hh in range(2):
                        sl = slice(hh * 512, (hh + 1) * 512)
                        T.op(DVE, lambda hh=hh, sl=sl: nc.vector.tensor_tensor(out=a2b[:, sl], in0=PS[1 + hh][:, :], in1=a2b[:, sl], op=ALU.add),
                             r=[("ps", 1 + hh), "a2b_bias"], w=[("a2b", hh)])
                        T.op(DVE, lambda sl=sl: nc.vector.scalar_tensor_tensor(out=a2b[:, sl], in0=a2b[:, sl], scalar=1.0, in1=n2rb[:, sl], op0=ALU.add, op1=ALU.mult),
                             r=[("a2b", hh), "n2rb"], w=[("a2b", hh)])
        T.op(DVE, lambda: nc.vector.tensor_tensor(
            out=modc[:], in0=PS[0][:, 0:96].rearrange("p (m two) -> p m two", two=2)[:, :, 0], in1=bcol[:], op=ALU.add),
            r=[("ps", 0), "bcol"], w=["modc"])
        for gi, (gt, b0) in enumerate(((g1b, 1), (g2b, 3))):
            if gi == 0:
                continue
            for hh in range(2):
                T.op(DVE, lambda gt=gt, b0=b0, hh=hh, gi=gi: nc.vector.tensor_tensor(
                    out=gt[:, hh * 512:(hh + 1) * 512], in0=PS[b0 + hh][:, :],
                    in1=brow[:, gi * 1024 + hh * 512: gi * 1024 + (hh + 1) * 512], op=ALU.add),
                    r=[("ps", b0 + hh), f"brow{gi}"], w=[(f"g{gi + 1}b", hh)])
        for (av, sc0, nwo, akey) in ((a1, 8, 0, "a1"), (a2, 32, 8, "a2")):
            T.op(DVE, lambda sc0=sc0: nc.vector.tensor_scalar(out=tmp8[:], in0=modc[:, sc0:sc0 + 8], scalar1=1.0, scalar2=None, op0=ALU.add),
                 r=["modc"], w=["tmp8"])
            T.op(DVE, lambda av=av, nwo=nwo: nc.vector.tensor_tensor(out=av[:], in0=tmp8[:], in1=nw[:, nwo:nwo + 8], op=ALU.mult),
                 r=["tmp8", "nw0", "nw1"], w=[akey])
        T.op(DVE, lambda: nc.vector.tensor_scalar(out=smallc[:, 2:3], in0=smallc[:, 2:3], scalar1=0.8, scalar2=None, op0=ALU.mult),
             r=["sc2"], w=["sc2"])
        T.op(DVE, lambda: nc.vector.tensor_reduce(out=l2[:, 0:2], in_=qkrow[:].rearrange("p (a b) -> p a b", a=2), axis=AX.X, op=ALU.max,
                                                  apply_absolute_value=True), r=["qkrow"], w=["l2a"])
        T.op(DVE, lambda: nc.vector.tensor_tensor(out=l2[:, 2:3], in0=l2[:, 0:1], in1=l2[:, 1:2], op=ALU.mult), r=["l2a"], w=["l2b"])
        T.op(DVE, lambda: nc.vector.tensor_scalar(out=smallc[:, 3:4], in0=l2[:, 2:3], scalar1=-8.0, scalar2=None, op0=ALU.mult),
             r=["l2b"], w=["sc3"])
        lv = lamb[:].rearrange("p (a b) -> p a b", a=4)
        T.op(DVE, lambda: nc.vector.tensor_tensor(out=lt[:].rearrange("p (a b) -> p a b", a=2), in0=lv[:, 0:4:2, :], in1=lv[:, 1:4:2, :], op=ALU.mult),
             r=["lamb"], w=["lt"])
        T.op(DVE, lambda: nc.vector.tensor_reduce(out=l2[:, 0:2], in_=lt[:].rearrange("p (a b) -> p a b", a=2), axis=AX.X, op=ALU.add),
             r=["lt", "l2a", "l2b"], w=["l2a"])
        T.op(ACT, lambda: nc.scalar.activation(out=l2[:, 2:4], in_=l2[:, 0:2], func=AF.Exp), r=["l2a"], w=["l2b"])
        T.op(DVE, lambda: nc.vector.tensor_tensor(out=l2[:, 0:1], in0=l2[:, 3:4], in1=l2[:, 2:3], op=ALU.subtract), r=["l2b"], w=["l2a"])
        T.op(DVE, lambda: nc.vector.tensor_scalar(out=smallc[:, 4:5], in0=l2[:, 0:1], scalar1=-0.2, scalar2=None, op0=ALU.add),
             r=["l2a"], w=["sc4"])
        T.barrier(full=False)
    if dbg and "modc" in dbg_out:
        T.dma(SP, dbg_out["modc"][:, :], modc[:], r=["modc"])
        T.dma(SP, dbg_out["g1b"][:, :], g1b[:], r=["g1b"])
        T.dma(SP, dbg_out["smallc"][:, :], smallc[:], r=["sc0"])

    def dump_dram(dst, src, rows, cols, dt):
        with nc.sbuf_tensor("s_dump_" + dst.name.replace(".", "_"), [128, cols], dt) as tmp:
            for r0 in range(0, rows, 128):
                n = min(128, rows - r0)
                T.dma(SP, tmp[0:n, :], src[r0:r0 + n, :], w=["dumptmp"])
                T.dma(SP, dst[r0:r0 + n, :], tmp[0:n, :], r=["dumptmp"], w=[("dumpdst", r0)])
            T.barrier()

    def finish():
        if _scope[0] is not None:
            _scope[0].__exit__(None, None, None)
            _scope[0] = None
        T.barrier()
        if early_open[0]:
            early.close()
            early_open[0] = False
        glob.close()
        return nc

    if stage <= 0:
        return finish()

    s1 = modc[:, 0:8]
    s2 = modc[:, 24:32]

    scope_mark("ph1")
    def norm_tile(ph_bufs, src_ap, rkeys, tag):
        ss, sd, junk = ph_bufs["ss"], ph_bufs["sd"], ph_bufs["junk"]
        T.op(ACT, lambda: nc.scalar.activation(out=junk[:], in_=src_ap, func=AF.Square, accum_out=ss[:]), r=rkeys, w=["junk", "ss"])
        T.op(ACT, lambda: nc.scalar.activation(out=sd[:], in_=ss[:], func=AF.Ln, bias=ph_bufs["epsc"][:], scale=1.0 / D), r=["ss"], w=["sd"])
        T.op(ACT, lambda: nc.scalar.activation(out=ph_bufs["rs"][:], in_=sd[:], func=AF.Exp, scale=-0.5), r=["sd"], w=["rs"])

    with ExitStack() as ph:
        def sb(name, shape, dt):
            return ph.enter_context(nc.sbuf_tensor("s_" + name, list(shape), dt))
        xt = [sb(f"xt{i}", [128, D], F32) for i in range(2)]
        xh = [sb(f"xh{i}", [128, D], BF16) for i in range(2)]
        hTb = [sb(f"hTb{i}", [128, 8, 512], BF16) for i in range(2)]
        bufs = dict(ss=sb("ss", [128, 1], F32), sd=sb("sd", [128, 1], F32), rs=sb("rs", [128, 1], F32),
                    junk=sb("junk", [128, D], BF16), epsc=sb("epsc", [128, 1], F32))
        T.op(DVE, lambda: nc.vector.memset(bufs["epsc"][:], EPS), w=["epsc"])
        pass
        for t in range(int(os.environ.get('K1_TILES', NT))):
            b = t % 2
            blk, tl = t // 4, t % 4
            hb = blk % 2
            T.dma(SP, xt[b][:], x_in[t * 128:(t + 1) * 128, :], w=[("xt", b)])
            KO = int(os.environ.get('K1_OPS', 9))
            if KO < 1:
                continue
            norm_tile(bufs, xt[b][:], [("xt", b), "epsc"], "n1")
            if KO < 2:
                continue
            T.op(DVE, lambda b=b: nc.vector.tensor_scalar(out=xh[b][:], in0=xt[b][:], scalar1=bufs["rs"][:], scalar2=None, op0=ALU.mult),
                 r=[("xt", b), "rs"], w=[("xh", b)])
            if KO < 3:
                continue
            pb = t % 2
            pv = psb(pb).rearrange("p (k t) -> p k t", k=8)
            for kc in range(8):
                T.op(PE, lambda kc=kc, b=b, pv=pv: nc.tensor.transpose(pv[:, kc, :], xh[b][:, kc * 128:(kc + 1) * 128], ident_b),
                     r=[("xh", b), "cstb"], w=[("ps", pb)])
            if KO < 4:
                continue
            for kc in range(8):
                T.op(ACT, lambda kc=kc, pv=pv, hb=hb, tl=tl: nc.scalar.activation(
                    out=hTb[hb][:, kc, tl * 128:(tl + 1) * 128], in_=pv[:, kc, :], func=AF.Identity,
                    **({} if os.environ.get('K1_NOAP') == '1' else ({'scale': a1[:, kc:kc + 1]} if os.environ.get('K1_NOAP') == '2' else
                        ({'bias': s1[:, kc:kc + 1]} if os.environ.get('K1_NOAP') == '3' else dict(scale=a1[:, kc:kc + 1], bias=s1[:, kc:kc + 1]))))),
                    r=[("ps", pb), "a1", "modc"], w=[("hTb", hb, tl)])
            if tl == 3:
                T.dma(SP, xnT_v[:, :, blk * 512:(blk + 1) * 512], hTb[hb][:], r=[("hTb", hb)], w=[("xnT", blk)])
        T.barrier()
    early.close()
    early_open[0] = False
    if dbg and "xnT" in dbg_out:
        dump_dram(dbg_out["xnT"], xnT_d, 1024, S, BF16)
    if stage <= 1:
        return finish()

    scope_mark("phA")
    def sin_reduce(ph_sb, dst_bf, ang, n, tagk):
        ki = ph_sb["ki"]
        kf = ph_sb["kf"]
        msk = ph_sb["msk"]
        C1 = 6.28125
        C2 = 2.0 * math.pi - 6.28125
        T.op(DVE, lambda: nc.vector.tensor_scalar(out=ki[:, 0:n], in0=ang, scalar1=1.0 / (2.0 * math.pi), scalar2=None, op0=ALU.mult),
             r=[tagk], w=["ki"])
        T.op(DVE, lambda: nc.vector.tensor_copy(out=kf[:, 0:n], in_=ki[:, 0:n]), r=["ki"], w=["kf"])
        T.op(DVE, lambda: nc.vector.scalar_tensor_tensor(out=ang, in0=kf[:, 0:n], scalar=-C1, in1=ang, op0=ALU.mult, op1=ALU.add),
             r=["kf", tagk], w=[tagk])
        T.op(DVE, lambda: nc.vector.scalar_tensor_tensor(out=ang, in0=kf[:, 0:n], scalar=-C2, in1=ang, op0=ALU.mult, op1=ALU.add),
             r=["kf", tagk], w=[tagk])
        T.op(DVE, lambda: nc.vector.tensor_scalar(out=msk[:, 0:n], in0=ang, scalar1=math.pi, scalar2=-2.0 * math.pi, op0=ALU.is_gt, op1=ALU.mult),
             r=[tagk], w=["msk"])
        T.op(DVE, lambda: nc.vector.tensor_tensor(out=ang, in0=ang, in1=msk[:, 0:n], op=ALU.add), r=[tagk, "msk"], w=[tagk])
        T.op(DVE, lambda: nc.vector.tensor_scalar(out=msk[:, 0:n], in0=ang, scalar1=-math.pi, scalar2=2.0 * math.pi, op0=ALU.is_lt, op1=ALU.mult),
             r=[tagk], w=["msk"])
        T.op(DVE, lambda: nc.vector.tensor_tensor(out=ang, in0=ang, in1=msk[:, 0:n], op=ALU.add), r=[tagk, "msk"], w=[tagk])
        T.op(DVE, lambda: nc.vector.tensor_scalar(out=ang, in0=ang, scalar1=math.pi, scalar2=-math.pi, op0=ALU.min, op1=ALU.max),
             r=[tagk], w=[tagk])
        T.op(ACT, lambda: nc.scalar.activation(out=dst_bf, in_=ang, func=AF.Sin), r=[tagk], w=[tagk + "_o"])

    persist = ExitStack()

    def sbp(name, shape, dt):
        return persist.enter_context(nc.sbuf_tensor("s_" + name, list(shape), dt))

    with ExitStack() as ph:
        def sb(name, shape, dt):
            return ph.enter_context(nc.sbuf_tensor("s_" + name, list(shape), dt))
        CosB = sb("CosB", [128, S], BF16)
        SinB = sb("SinB", [128, S], BF16)
        with ExitStack() as ph2:
            posi = ph2.enter_context(nc.sbuf_tensor("s_posi", [128, 1024], I32))
            posf = ph2.enter_context(nc.sbuf_tensor("s_posf", [128, 1024], F32))
            ang = ph2.enter_context(nc.sbuf_tensor("s_ang", [128, 1024], F32))
            tb = dict(ki=ph2.enter_context(nc.sbuf_tensor("s_ki", [128, 1024], I32)),
                      kf=ph2.enter_context(nc.sbuf_tensor("s_kf", [128, 1024], F32)),
                      msk=ph2.enter_context(nc.sbuf_tensor("s_msk", [128, 1024], F32)))
            for c4 in range(4):
                cs = slice(c4 * 1024, (c4 + 1) * 1024)
                T.dma(SP, posi[:], pos_in[0:1, cs].partition_broadcast(128), w=["posi"])
                T.op(DVE, lambda: nc.vector.tensor_copy(out=posf[:], in_=posi[:]), r=["posi"], w=["posf"])
                T.op(DVE, lambda: nc.vector.tensor_scalar(out=ang[:], in0=posf[:], scalar1=invf, scalar2=None, op0=ALU.mult),
                     r=["posf", "cst"], w=["ang"])
                sin_reduce(tb, SinB[:, cs], ang[:], 1024, "ang")
                T.op(DVE, lambda: nc.vector.tensor_scalar(out=ang[:], in0=posf[:], scalar1=invf, scalar2=math.pi / 2, op0=ALU.mult, op1=ALU.add),
                     r=["posf", "cst", "ang_o"], w=["ang"])
                sin_reduce(tb, CosB[:, cs], ang[:], 1024, "ang")
            T.barrier()

        wqkv = sb("wqkv", [128, 8, 1536], BF16)
        wgt = sb("wgt", [128, 8, 8], BF16)
        bg = sb("bg", [4, 2], F32)
        nbf = sb("nbf", [4, 1], F32)
        epsc = sb("epscA", [128, 1], F32)
        gst = [[sb(f"gst{g}_{i}", [4, 512], F32) for i in range(2)] for g in range(2)]
        kT = sb("kT", [128, 4, S], BF16)
        v1 = sb("v1", [128, NT, 4, 128], BF16)
        xb = [sb(f"xbA{i}", [128, 8, 512], BF16) for i in range(2)]
        qT = sb("qT", [128, 4, 512], BF16)
        hdTb = sb("hdTb", [128, 4, 512], BF16)
        Pb = [[sb(f"P{m}_{i}", [128, 512], BF16) for i in range(2)] for m in range(2)]
        sqb = [sb(f"sqb{i}", [128, 512], BF16) for i in range(2)]
        qsf = [sb(f"qsf{i}", [128, 512], F32) for i in range(2)]
        sdf = [sb(f"sdf{i}", [128, 512], F32) for i in range(2)]
        qnb = [sb(f"qnb{i}", [128, 512], BF16) for i in range(2)]
        t1f = [sb(f"t1f{i}", [128, 512], F32) for i in range(2)]
        t2f = [sb(f"t2f{i}", [128, 512], F32) for i in range(2)]
        fo = [sb(f"fo{i}", [128, 512], F32) for i in range(4)]

        T.op(DVE, lambda: nc.vector.memset(epsc[:], EPS), w=["epscA"])
        T.dma(POOL, wqkv[:, 0:4, :], w_in_v[:, 0:4, 2056:3592], w=[("wqkv", 0)])
        T.dma(POOL, wqkv[:, 4:8, :], w_in_v[:, 4:8, 2056:3592], w=[("wqkv", 1)])
        T.dma(POOL, wgt[:], w_in_v[:, :, 2048:2056], w=["wgt"])
        T.dma(SP, bg[:], bgate_in[:, :], w=["bg"])
        T.op(DVE, lambda: nc.vector.tensor_scalar(out=nbf[:], in0=bg[:, 1:2], scalar1=-1.0, scalar2=None, op0=ALU.mult), r=["bg"], w=["nbf"])

        negc = smallc[:, 3:4]
        neglam = smallc[:, 4:5]
        subw8 = smallc[:, 2:3]

        for blk in range(NB):
            xbb = blk % 2
            bs = slice(blk * 512, (blk + 1) * 512)
            T.dma(SP, xb[xbb][:], xnT_v[:, :, bs], w=[("xbA", xbb)])
            for c in range(8):
                st = c % 2
                isq = c < 4
                hh = c % 4
                col0 = (0 if isq else 512) + hh * 128
                for kc in range(8):
                    T.op(PE, lambda kc=kc, col0=col0, st=st: nc.tensor.matmul(
                        PS[st][:, :], lhsT=wqkv[:, kc, col0:col0 + 128], rhs=xb[xbb][:, kc, :], start=(kc == 0), stop=(kc == 7)),
                        r=[("wqkv",), ("xbA", xbb)], w=[("ps", st)])
                wcol = smallc[:, 0:1] if isq else smallc[:, 1:2]
                T.op(ACT, lambda st=st: nc.scalar.activation(out=sqb[st][:], in_=PS[st][:, :], func=AF.Square), r=[("ps", st)], w=[("sqb", st)])
                T.op(ACT, lambda st=st, wcol=wcol: nc.scalar.activation(out=qsf[st][:], in_=PS[st][:, :], func=AF.Identity, scale=wcol),
                     r=[("ps", st), "sc0", "sc1"], w=[("qsf", st)])
                T.op(PE, lambda st=st: nc.tensor.matmul(PS[2 + st][:, :], lhsT=bo64_b, rhs=sqb[st][:], start=True, stop=True),
                     r=[("sqb", st), "cstb"], w=[("ps", 2 + st)])
                T.op(ACT, lambda st=st: nc.scalar.activation(out=sdf[st][:], in_=PS[2 + st][:, :], func=AF.Ln, bias=epsc[:], scale=1.0 / 64),
                     r=[("ps", 2 + st), "epscA"], w=[("sdf", st)])
                T.op(ACT, lambda st=st: nc.scalar.activation(out=sdf[st][:], in_=sdf[st][:], func=AF.Exp, scale=-0.5),
                     r=[("sdf", st)], w=[("sdf", st)])
                T.op(DVE, lambda st=st: nc.vector.tensor_tensor(out=qnb[st][:], in0=qsf[st][:], in1=sdf[st][:], op=ALU.mult),
                     r=[("qsf", st), ("sdf", st)], w=[("qnb", st)])
                T.op(PE, lambda st=st: nc.tensor.matmul(PS[2 + st][:, :], lhsT=RT_b, rhs=qnb[st][:], start=True, stop=True),
                     r=[("qnb", st), "cstb"], w=[("ps", 2 + st)])
                T.op(POOL, lambda st=st: nc.gpsimd.tensor_tensor(out=t1f[st][:], in0=qnb[st][:], in1=CosB[:, bs], op=ALU.mult),
                     r=[("qnb", st), "CosB"], w=[("t1f", st)])
                T.op(DVE, lambda st=st: nc.vector.tensor_tensor(out=t2f[st][:], in0=PS[2 + st][:, :], in1=SinB[:, bs], op=ALU.mult),
                     r=[("ps", 2 + st), "SinB"], w=[("t2f", st)])
                if isq:
                    dst, dk = qT[:, hh, :], ("qT", hh)
                else:
                    dst, dk = kT[:, hh, bs], ("kT", hh, blk)
                T.op(POOL, lambda st=st, dst=dst: nc.gpsimd.tensor_tensor(out=dst, in0=t1f[st][:], in1=t2f[st][:], op=ALU.add),
                     r=[("t1f", st), ("t2f", st)], w=[dk])
            for tl in range(4):
                t = blk * 4 + tl
                for kc in range(8):
                    T.op(PE, lambda kc=kc, tl=tl: nc.tensor.matmul(
                        PS[tl % 2][:, :], lhsT=xb[xbb][:, kc, tl * 128:(tl + 1) * 128], rhs=wqkv[:, kc, 1024:1536], start=(kc == 0), stop=(kc == 7)),
                        r=[("wqkv",), ("xbA", xbb)], w=[("ps", tl % 2)])
                T.op(ACT, lambda t=t, tl=tl: nc.scalar.copy(out=v1[:, t, :, :].rearrange("p h d -> p (h d)"), in_=PS[tl % 2][:, :]),
                     r=[("ps", tl % 2)], w=[("v1", t)])
            for gi in range(2):
                for kc in range(8):
                    T.op(PE, lambda kc=kc, gi=gi: nc.tensor.matmul(
                        PS[1][0:4, :], lhsT=wgt[:, kc, 4 * gi:4 * gi + 4], rhs=xb[xbb][:, kc, :], start=(kc == 0), stop=(kc == 7)),
                        r=["wgt", ("xbA", xbb)], w=[("ps", 1)])
                if gi == 0:
                    T.op(ACT, lambda: nc.scalar.activation(out=gst[0][xbb][:], in_=PS[1][0:4, :], func=AF.Identity, bias=bg[:, 0:1]),
                         r=[("ps", 1), "bg"], w=[("gst", 0, xbb)])
                    T.dma(SP, gi_d[:, bs], gst[0][xbb][:], r=[("gst", 0, xbb)], w=[("gi_d", blk)])
                else:
                    T.op(ACT, lambda: nc.scalar.activation(out=gst[1][xbb][:], in_=PS[1][0:4, :], func=AF.Exp, bias=nbf[:], scale=-1.0),
                         r=[("ps", 1), "nbf"], w=[("gst", 1, xbb)])
                    T.dma(SP, gf_d[:, bs], gst[1][xbb][:], r=[("gst", 1, xbb)], w=[("gf_d", blk)])
            for hh in range(4):
                nkt = blk * 4 + 4
                prev = None

                def pv_step(kt, c0, pbuf):
                    first = (kt == 0)
                    last = (kt == nkt - 1)
                    for m in range(2):
                        T.op(PE, lambda m=m: nc.tensor.matmul(
                            PS[4 + 2 * m][:, c0:512], lhsT=v1[:, kt, hh, :], rhs=Pb[m][pbuf][:, c0:512], start=first, stop=last,
                            skip_group_check=True), r=[("v1", kt), ("P", m, pbuf)], w=[("ps", 4 + 2 * m)])
                        T.op(PE, lambda m=m: nc.tensor.matmul(
                            PS[5 + 2 * m][:, c0:512], lhsT=ones_b, rhs=Pb[m][pbuf][:, c0:512], start=first, stop=last,
                            skip_group_check=True), r=["cstb", ("P", m, pbuf)], w=[("ps", 5 + 2 * m)])

                for kt in range(nkt):
                    ktl = kt - blk * 4
                    c0 = ktl * 128 if ktl > 0 else 0
                    pbuf = kt % 2
                    sbk = 2 * (kt % 2)
                    for m in range(2):
                        T.op(PE, lambda m=m, kt=kt, c0=c0, sbk=sbk: nc.tensor.matmul(
                            PS[sbk + m][:, c0:512], lhsT=kT[64 * m:64 * m + 64, hh, kt * 128:(kt + 1) * 128],
                            rhs=qT[64 * m:64 * m + 64, hh, c0:512], start=True, stop=True),
                            r=[("kT", hh, kt // 4), ("qT", hh)], w=[("ps", sbk + m)])
                    if prev is not None:
                        pv_step(*prev)
                    for m in range(2):
                        T.op(ACT, lambda m=m, c0=c0, pbuf=pbuf, sbk=sbk: nc.scalar.activation(
                            out=Pb[m][pbuf][:, c0:512], in_=PS[sbk + m][:, c0:512], func=AF.Exp, bias=negc, scale=0.125),
                            r=[("ps", sbk + m), "sc3"], w=[("P", m, pbuf)])
                        if ktl >= 0:
                            T.op(POOL, lambda m=m, c0=c0, pbuf=pbuf: nc.gpsimd.affine_select(
                                out=Pb[m][pbuf][:, c0:c0 + 128], in_=Pb[m][pbuf][:, c0:c0 + 128], pattern=[[1, 128]],
                                compare_op=ALU.is_ge, fill=0.0, base=0, channel_multiplier=-1),
                                r=[("P", m, pbuf)], w=[("P", m, pbuf)])
                    prev = (kt, c0, pbuf)
                pv_step(*prev)
                T.op(ACT, lambda: nc.scalar.activation(out=fo[0][:], in_=PS[5][:, :], func=AF.Ln), r=[("ps", 5)], w=[("fo", 0)])
                T.op(ACT, lambda: nc.scalar.activation(out=fo[0][:], in_=fo[0][:], func=AF.Exp, scale=-1.0), r=[("fo", 0)], w=[("fo", 0)])
                T.op(DVE, lambda: nc.vector.tensor_tensor(out=fo[1][:], in0=PS[4][:, :], in1=fo[0][:], op=ALU.mult),
                     r=[("ps", 4), ("fo", 0)], w=[("fo", 1)])
                T.op(ACT, lambda: nc.scalar.activation(out=fo[2][:], in_=PS[7][:, :], func=AF.Ln), r=[("ps", 7)], w=[("fo", 2)])
                T.op(ACT, lambda: nc.scalar.activation(out=fo[0][:], in_=fo[2][:], func=AF.Exp, scale=-1.0), r=[("fo", 2), ("fo", 0)], w=[("fo", 0)])
                T.op(DVE, lambda: nc.vector.tensor_tensor(out=fo[2][:], in0=PS[6][:, :], in1=fo[0][:], op=ALU.mult),
                     r=[("ps", 6), ("fo", 0)], w=[("fo", 2)])
                T.op(DVE, lambda: nc.vector.scalar_tensor_tensor(out=fo[3][:], in0=fo[2][:], scalar=neglam, in1=fo[1][:], op0=ALU.mult, op1=ALU.add),
                     r=[("fo", 1), ("fo", 2), "sc4"], w=[("fo", 3)])
                T.op(ACT, lambda: nc.scalar.activation(out=sqb[0][:], in_=fo[3][:], func=AF.Square), r=[("fo", 3)], w=[("sqb", 0)])
                T.op(PE, lambda: nc.tensor.matmul(PS[0][:, :], lhsT=ones_b, rhs=sqb[0][:], start=True, stop=True),
                     r=[("sqb", 0), "cstb"], w=[("ps", 0)])
                T.op(ACT, lambda: nc.scalar.activation(out=fo[0][:], in_=PS[0][:, :], func=AF.Ln, bias=epsc[:], scale=1.0 / 128),
                     r=[("ps", 0), "epscA"], w=[("fo", 0)])
                T.op(ACT, lambda: nc.scalar.activation(out=fo[0][:], in_=fo[0][:], func=AF.Exp, scale=-0.5), r=[("fo", 0)], w=[("fo", 0)])
                T.op(DVE, lambda: nc.vector.scalar_tensor_tensor(out=hdTb[:, hh, :], in0=fo[3][:], scalar=subw8, in1=fo[0][:], op0=ALU.mult, op1=ALU.mult),
                     r=[("fo", 3), ("fo", 0), "sc2"], w=[("hdTb", hh)])
            T.dma(SP, hdT_v[:, :, bs], hdTb[:], r=[("hdTb",)], w=[("hdT", blk)])
        T.barrier()
    if dbg and "hdT" in dbg_out:
        dump_dram(dbg_out["hdT"], hdT_d, 512, S, BF16)
        dump_dram(dbg_out["irow"], gi_d, 4, S, F32)
        dump_dram(dbg_out["frow"], gf_d, 4, S, F32)
    if stage <= 2:
        persist.close()
        return finish()

    scope_mark("phB")
    wcol = sbp("wcol", [128, NT, 8], F32)
    decb = sbp("decb", [128, 128], F32)
    with ExitStack() as ph:
        def sb(name, shape, dt):
            return ph.enter_context(nc.sbuf_tensor("s_" + name, list(shape), dt))
        irow = sb("irow", [4, S], F32)
        frow = sb("frow", [4, S], F32)
        T.dma(SP, irow[:], gi_d[:, :], w=[("irow",)])
        T.dma(SP, frow[:], gf_d[:, :], w=[("frow",)])
        onesr = sb("onesr", [4, S], F32)
        csr = sb("csr", [4, S], F32)
        nbr = sb("nbr", [4, S], F32)
        gr = sb("gr", [4, S], F32)
        pe_ = sb("pe_", [4, 33], F32)
        G = sb("G", [4, 32], F32)
        Ms = sb("Ms", [4, 32], F32)
        mp = sb("mp", [4, 33], F32)
        dec = sb("dec", [4, 32], F32)
        T.op(ACT, lambda: nc.scalar.activation(out=frow[:], in_=frow[:], func=AF.Ln, bias=1.0), r=[("frow",)], w=[("frow",)])
        T.op(DVE, lambda: nc.vector.memset(onesr[:], 1.0), w=["onesr"])
        T.op(DVE, lambda: nc.vector.tensor_tensor_scan(out=csr[:], data0=onesr[:], data1=frow[:], initial=0.0, op0=ALU.mult, op1=ALU.add),
             r=["onesr", ("frow",)], w=["csr"])
        T.op(DVE, lambda: nc.vector.memset(pe_[:, 0:1], 0.0), w=[("pe_", 0)])
        T.op(DVE, lambda: nc.vector.tensor_copy(out=pe_[:, 1:33], in_=csr[:].rearrange("p (j s) -> p j s", s=128)[:, :, 127]),
             r=["csr"], w=[("pe_", 1)])
        T.op(DVE, lambda: nc.vector.tensor_tensor(out=nbr[:].rearrange("p (j s) -> p j s", s=128), in0=csr[:].rearrange("p (j s) -> p j s", s=128),
                                                  in1=pe_[:, 0:32].unsqueeze(2).to_broadcast([4, 32, 128]), op=ALU.subtract),
             r=["csr", ("pe_",)], w=["nbr"])
        T.op(DVE, lambda: nc.vector.tensor_tensor(out=gr[:], in0=irow[:], in1=nbr[:], op=ALU.add), r=[("irow",), "nbr"], w=["gr"])
        T.op(DVE, lambda: nc.vector.tensor_reduce(out=G[:], in_=gr[:].rearrange("p (j s) -> p j s", s=128), axis=AX.X, op=ALU.max),
             r=["gr"], w=["G"])
        T.op(DVE, lambda: nc.vector.memset(mp[:, 0:1], 0.0), w=[("mp", 0)])
        nbl = nbr[:].rearrange("p (j s) -> p j s", s=128)[:, :, 127]
        for j in range(NT):
            T.op(DVE, lambda j=j: nc.vector.tensor_tensor(out=Ms[:, j:j + 1], in0=mp[:, j:j + 1], in1=G[:, j:j + 1], op=ALU.max),
                 r=[("mp", j), "G"], w=[("Ms", j)])
            T.op(DVE, lambda j=j: nc.vector.tensor_tensor(out=mp[:, j + 1:j + 2], in0=Ms[:, j:j + 1], in1=nbl[:, j:j + 1], op=ALU.subtract),
                 r=[("Ms", j), "nbr"], w=[("mp", j + 1)])
        Msb = Ms[:].unsqueeze(2).to_broadcast([4, 32, 128])
        T.op(DVE, lambda: nc.vector.tensor_tensor(out=gr[:].rearrange("p (j s) -> p j s", s=128), in0=gr[:].rearrange("p (j s) -> p j s", s=128),
                                                  in1=Msb, op=ALU.subtract), r=["gr", ("Ms",)], w=["gr"])
        T.op(DVE, lambda: nc.vector.tensor_tensor(out=nbr[:].rearrange("p (j s) -> p j s", s=128), in0=nbr[:].rearrange("p (j s) -> p j s", s=128),
                                                  in1=Msb, op=ALU.subtract), r=["nbr", ("Ms",)], w=["nbr"])
        T.op(ACT, lambda: nc.scalar.activation(out=gr[:], in_=gr[:], func=AF.Exp), r=["gr"], w=["gr"])
        T.op(ACT, lambda: nc.scalar.activation(out=nbr[:], in_=nbr[:], func=AF.Exp), r=["nbr"], w=["nbr"])
        T.op(DVE, lambda: nc.vector.tensor_scalar(out=gr[:], in0=gr[:], scalar1=128.0 ** -0.5, scalar2=None, op0=ALU.mult), r=["gr"], w=["gr"])
        T.op(DVE, lambda: nc.vector.tensor_tensor(out=dec[:], in0=mp[:, 0:32], in1=Ms[:], op=ALU.subtract), r=[("mp",), ("Ms",)], w=["dec"])
        T.op(ACT, lambda: nc.scalar.activation(out=dec[:], in_=dec[:], func=AF.Exp), r=["dec"], w=["dec"])
        T.dma(SP, dec_d[:, :], dec[:], r=["dec"], w=["dec_d"])
        T.dma(SP, decb[:], dec_d.rearrange("h j -> (h j)").unsqueeze(0).partition_broadcast(128), r=["dec_d"], w=["decb"])
        wv = PS[0][:, 0:256].rearrange("p (t e) -> p t e", e=8)
        for t in range(NT):
            T.op(PE, lambda t=t: nc.tensor.transpose(wv[:, t, 0:4], gr[:, t * 128:(t + 1) * 128], ident_f[0:4, 0:4]),
                 r=["gr", "cst"], w=[("ps", 0, t, 0)])
            T.op(PE, lambda t=t: nc.tensor.transpose(wv[:, t, 4:8], nbr[:, t * 128:(t + 1) * 128], ident_f[0:4, 0:4]),
                 r=["nbr", "cst"], w=[("ps", 0, t, 1)])
        T.op(DVE, lambda: nc.vector.tensor_copy(out=wcol[:], in_=wv), r=[("ps", 0)], w=["wcol"])
        T.barrier()
    if dbg and "wcol" in dbg_out:
        T.dma(SP, dbg_out["wcol"][:, :], wcol[:].rearrange("p t e -> p (t e)"), r=["wcol"])
        T.dma(SP, dbg_out["decb"][:, :], decb[:], r=["decb"])
    if stage <= 3:
        persist.close()
        return finish()

    scope_mark("phC")
    with ExitStack() as ph:
        def sb(name, shape, dt):
            return ph.enter_context(nc.sbuf_tensor("s_" + name, list(shape), dt))
        wm = sb("wm", [128, 8, 2048], BF16)
        cw = sb("cw", [128, 8, 4], F32)
        cbias = sb("cbias", [128, 8], F32)
        dg = sb("dg", [128, 8, 4, 128], BF16)
        mnwb = sb("mnwb", [128, 512], F32)
        epsc = sb("epscC", [128, 1], F32)
        xb = [sb(f"xbC{i}", [128, 8, 512], BF16) for i in range(2)]
        pre = sb("pre", [128, 8, 516], BF16)
        qkc = sb("qkc", [128, 8, 512], BF16)
        vm1 = [sb(f"vm1_{i}", [128, 4, 130], BF16) for i in range(2)]
        so = [sb(f"so{i}", [128, 512], F32) for i in range(2)]
        ST = [sb(f"ST{i}", [128, 128], BF16) for i in range(4)]
        kw = [sb(f"kw{i}", [128, 128], BF16) for i in range(4)]
        Cst = sb("Cst", [128, 4, 130], F32)
        Cd = sb("Cd", [128, 4, 130], BF16)
        hbuf = sb("hbuf", [128, 512], F32)
        sqh = sb("sqh", [128, 512], F32)
        hmb = sb("hmb", [128, 512], BF16)
        hmTb = [sb(f"hmTb{i}", [128, 4, 512], BF16) for i in range(2)]
        dn = sb("dn", [128, 8], F32)
        ssh = sb("ssh", [128, 8], F32)

        T.op(DVE, lambda: nc.vector.memset(epsc[:], EPS), w=["epscC"])
        T.dma(POOL, wm[:, 0:4, :], w_in_v[:, 0:4, 0:2048], w=[("wm", 0)], max_dma_last_dim=8192)
        T.dma(POOL, wm[:, 4:8, :], w_in_v[:, 4:8, 0:2048], w=[("wm", 1)], max_dma_last_dim=8192)
        T.dma(SP, cw[:], convw_in[:, :, :], w=["cw"])
        T.dma(SP, cbias[:], convb_in[:, :], w=["cbias"])
        T.dma(SP, mnwb[:], mnw_in[0:1, :].partition_broadcast(128), w=["mnwb"])
        for c in range(8):
            for j in range(4):
                T.op(DVE, lambda c=c, j=j: nc.vector.tensor_scalar(out=dg[:, c, j, :], in0=ident_f, scalar1=cw[:, c, j:j + 1], scalar2=None, op0=ALU.mult),
                     r=["cw", "cst"], w=[("dg", c, j)])
        T.op(DVE, lambda: nc.vector.memset(pre[:, :, 0:4], 0.0), w=[("pre", "halo")])
        T.op(DVE, lambda: nc.vector.memset(Cst[:], 0.0), w=["Cst"])
        T.op(DVE, lambda: nc.vector.memset(Cd[:], 0.0), w=["Cd"])
        for i in range(2):
            T.op(DVE, lambda i=i: nc.vector.memset(vm1[i][:, :, 128:130], 1.0), w=[("vm1", i)])

        for blk in range(NB):
            xbb = blk % 2
            bs = slice(blk * 512, (blk + 1) * 512)
            T.dma(SP, xb[xbb][:], xnT_v[:, :, bs], w=[("xbC", xbb)])
            for c in range(8):
                pb = c % 2
                for kc in range(8):
                    T.op(PE, lambda kc=kc, c=c, pb=pb: nc.tensor.matmul(
                        PS[pb][:, :], lhsT=wm[:, kc, c * 128:(c + 1) * 128], rhs=xb[xbb][:, kc, :], start=(kc == 0), stop=(kc == 7)),
                        r=[("wm",), ("xbC", xbb)], w=[("ps", pb)])
                if blk > 0:
                    T.op(DVE, lambda c=c: nc.vector.tensor_copy(out=pre[:, c, 1:4], in_=pre[:, c, 513:516]),
                         r=[("pre", c)], w=[("pre", "halo", c)])
                T.op(ACT, lambda c=c, pb=pb: nc.scalar.copy(out=pre[:, c, 4:516], in_=PS[pb][:, :]),
                     r=[("ps", pb), ("pre", "halo", c)], w=[("pre", c)])
                cb2 = 2 + c % 2
                for j in range(4):
                    T.op(PE, lambda c=c, j=j, cb2=cb2: nc.tensor.matmul(
                        PS[cb2][:, :], lhsT=dg[:, c, j, :], rhs=pre[:, c, 1 + j:513 + j], start=(j == 0), stop=(j == 3)),
                        r=[("dg", c), ("pre", c), ("pre", "halo", c)], w=[("ps", cb2)])
                T.op(ACT, lambda c=c, cb2=cb2: nc.scalar.activation(out=qkc[:, c, :], in_=PS[cb2][:, :], func=AF.Silu, bias=cbias[:, c:c + 1]),
                     r=[("ps", cb2), "cbias"], w=[("qkc", c)])
            for tl in range(4):
                t = blk * 4 + tl
                vb = t % 2
                ts_ = slice(tl * 128, (tl + 1) * 128)
                for half in range(2):
                    for kc in range(8):
                        T.op(PE, lambda kc=kc, half=half: nc.tensor.matmul(
                            PS[4 + half][:, :], lhsT=xb[xbb][:, kc, ts_], rhs=wm[:, kc, 1024 + half * 512:1536 + half * 512],
                            start=(kc == 0), stop=(kc == 7)), r=[("wm",), ("xbC", xbb)], w=[("ps", 4 + half)])
                T.op(ACT, lambda vb=vb: nc.scalar.copy(out=vm1[vb][:, :, 0:128], in_=PS[4][:, :].rearrange("p (h d) -> p h d", h=4)),
                     r=[("ps", 4)], w=[("vm1", vb)])
                T.op(ACT, lambda vb=vb: nc.scalar.activation(out=so[vb][:], in_=PS[5][:, :], func=AF.Sigmoid), r=[("ps", 5)], w=[("so", vb)])
                def head_ops(hh):
                    wc = wcol[:, t, hh:hh + 1]
                    cc = wcol[:, t, 4 + hh:5 + hh]
                    dcol = decb[:, hh * 32 + t:hh * 32 + t + 1]
                    if hh % 2 == 0:
                        bA, bK, bU, bN = 6, 1, 7, 0
                    else:
                        bA, bK, bU, bN = 2, 3, 4, 5
                    kps = psb(bK)[:, 0:128]
                    ops = []
                    ops.append(lambda: T.op(PE, lambda: nc.tensor.matmul(PS[bA][:, 0:128], lhsT=qkc[:, 4 + hh, ts_], rhs=qkc[:, hh, ts_], start=True, stop=True),
                                            r=[("qkc", 4 + hh), ("qkc", hh)], w=[("ps", bA)]))
                    ops.append(lambda: T.op(DVE, lambda: nc.vector.scalar_tensor_tensor(
                        out=ST[hh][:], in0=PS[bA][:, 0:128], scalar=wc, in1=tri_f, op0=ALU.mult, op1=ALU.mult),
                        r=[("ps", bA), "wcol", "cst"], w=[("ST", hh)]))
                    ops.append(lambda: T.op(PE, lambda: nc.tensor.transpose(kps, qkc[:, 4 + hh, ts_], ident_b),
                                            r=[("qkc", 4 + hh), "cstb"], w=[("ps", bK)]))
                    ops.append(lambda: T.op(DVE, lambda: nc.vector.tensor_scalar(out=kw[hh][:], in0=kps, scalar1=wc, scalar2=None, op0=ALU.mult),
                                            r=[("ps", bK), "wcol"], w=[("kw", hh)]))
                    ops.append(lambda: T.op(ACT, lambda: nc.scalar.activation(out=Cd[:, hh, :], in_=Cst[:, hh, :], func=AF.Identity, scale=dcol),
                                            r=[("Cst", hh), "decb"], w=[("Cd", hh)]))
                    ops.append(lambda: T.op(PE, lambda: nc.tensor.matmul(PS[bU][:, 0:129], lhsT=kw[hh][:], rhs=vm1[vb][:, hh, 0:129], start=True, stop=True),
                                            r=[("kw", hh), ("vm1", vb)], w=[("ps", bU)]))
                    ops.append(lambda: T.op(PE, lambda: nc.tensor.matmul(PS[bN][:, 0:129], lhsT=ST[hh][:], rhs=vm1[vb][:, hh, 0:129], start=True, stop=False),
                                            r=[("ST", hh), ("vm1", vb)], w=[("ps", bN)]))
                    ops.append(lambda: T.op(PE, lambda: nc.tensor.matmul(PS[bN][:, 0:129], lhsT=qkc[:, hh, ts_], rhs=Cd[:, hh, 0:129], start=False, stop=True),
                                            r=[("qkc", hh), ("Cd", hh)], w=[("ps", bN)]))
                    ops.append(lambda: T.op(DVE, lambda: nc.vector.scalar_tensor_tensor(
                        out=Cst[:, hh, 0:129], in0=Cst[:, hh, 0:129], scalar=dcol, in1=PS[bU][:, 0:129], op0=ALU.mult, op1=ALU.add),
                        r=[("Cst", hh), "decb", ("ps", bU)], w=[("Cst", hh)]))
                    ops.append(lambda: T.op(DVE, lambda: nc.vector.tensor_reduce(
                        out=dn[:, hh:hh + 1], in_=PS[bN][:, 128:129], axis=AX.X, op=ALU.max, apply_absolute_value=True),
                        r=[("ps", bN)], w=[("dn", hh)]))
                    ops.append(lambda: T.op(DVE, lambda: nc.vector.tensor_scalar(
                        out=dn[:, hh:hh + 1], in0=dn[:, hh:hh + 1], scalar1=cc, scalar2=None, op0=ALU.max),
                        r=[("dn", hh), "wcol"], w=[("dn", hh)]))
                    ops.append(lambda: T.op(DVE, lambda: nc.vector.reciprocal(out=dn[:, 4 + hh:5 + hh], in_=dn[:, hh:hh + 1]), r=[("dn", hh)], w=[("dn", 4 + hh)]))
                    ops.append(lambda: T.op(ACT, lambda: nc.scalar.activation(out=hbuf[:, hh * 128:(hh + 1) * 128], in_=PS[bN][:, 0:128], func=AF.Copy,
                                                                              scale=dn[:, 4 + hh:5 + hh]),
                                            r=[("ps", bN), ("dn", 4 + hh)], w=[("hbuf", hh)]))
                    return ops

                for pair in ((0, 1), (2, 3)):
                    pops = [head_ops(hh) for hh in pair]
                    for i in range(len(pops[0])):
                        for po in pops:
                            po[i]()
                T.op(DVE, lambda: nc.vector.tensor_tensor(out=sqh[:], in0=hbuf[:], in1=hbuf[:], op=ALU.mult), r=[("hbuf",)], w=["sqh"])
                T.op(DVE, lambda: nc.vector.tensor_reduce(out=ssh[:, 0:4], in_=sqh[:].rearrange("p (h d) -> p h d", h=4), axis=AX.X, op=ALU.add),
                     r=["sqh"], w=[("ssh", 0)])
                T.op(ACT, lambda: nc.scalar.activation(out=ssh[:, 4:8], in_=ssh[:, 0:4], func=AF.Sqrt, bias=epsc[:], scale=1.0 / 128),
                     r=[("ssh", 0), "epscC"], w=[("ssh", 1)])
                T.op(DVE, lambda: nc.vector.reciprocal(out=ssh[:, 4:8], in_=ssh[:, 4:8]), r=[("ssh", 1)], w=[("ssh", 1)])
                T.op(DVE, lambda: nc.vector.tensor_tensor(out=sqh[:].rearrange("p (h d) -> p h d", h=4), in0=hbuf[:].rearrange("p (h d) -> p h d", h=4),
                                                          in1=ssh[:, 4:8].unsqueeze(2).to_broadcast([128, 4, 128]), op=ALU.mult),
                     r=[("hbuf",), ("ssh", 1)], w=["sqh"])
                T.op(DVE, lambda: nc.vector.tensor_tensor(out=sqh[:], in0=sqh[:], in1=mnwb[:], op=ALU.mult), r=["sqh", "mnwb"], w=["sqh"])
                T.op(DVE, lambda vb=vb: nc.vector.tensor_tensor(out=hmb[:], in0=sqh[:], in1=so[vb][:], op=ALU.mult), r=["sqh", ("so", vb)], w=["hmb"])
                hb = blk % 2
                tp = psb(3).rearrange("p (k t) -> p k t", k=8)
                for hh in range(4):
                    T.op(PE, lambda hh=hh: nc.tensor.transpose(tp[:, hh, :], hmb[:, hh * 128:(hh + 1) * 128], ident_b),
                         r=["hmb", "cstb"], w=[("ps", 3)])
                T.op(ACT, lambda hb=hb: nc.scalar.copy(out=hmTb[hb][:, :, ts_], in_=tp[:, 0:4, :]), r=[("ps", 3)], w=[("hmTb", hb, tl)])
            T.dma(SP, hmT_v[:, :, bs], hmTb[blk % 2][:], r=[("hmTb", blk % 2)], w=[("hmT", blk)])
        T.barrier()
    persist.close()
    if dbg and "hmT" in dbg_out:
        dump_dram(dbg_out["hmT"], hmT_d, 512, S, BF16)
    if stage <= 4:
        return finish()

    scope_mark("phD")
    with ExitStack() as ph:
        def sb(name, shape, dt):
            return ph.enter_context(nc.sbuf_tensor("s_" + name, list(shape), dt))
        wg = sb("wg", [128, 8, 2048], BF16)
        wbm = sb("wbm", [128, 4, D], BF16)
        wbd = sb("wbd", [128, 4, D], BF16)
        wo = sb("wo", [128, 8, D], BF16)
        wstg = [sb(f"wstg{i}", [128, D], F32) for i in range(2)]
        wr = sb("wrt", [128, 8, 36], F32)
        brb = sb("brb", [128, 36], F32)
        xb = [sb(f"xbD{i}", [128, 8, 512], BF16) for i in range(2)]
        hmb_ = [sb(f"hmD{i}", [128, 4, 512], BF16) for i in range(2)]
        hdb_ = [sb(f"hdD{i}", [128, 4, 512], BF16) for i in range(2)]
        sg = [sb(f"sg{i}", [128, 512], F32) for i in range(4)]
        tt = [sb(f"tt{i}", [128, 512], F32) for i in range(4)]
        mT = sb("mT", [128, 8, 512], BF16)
        xt = [sb(f"xtD{i}", [128, D], F32) for i in range(2)]
        x1t = [sb(f"x1t{i}", [128, D], F32) for i in range(2)]
        xhf = sb("xhf", [128, D], F32)
        h2f = sb("h2f", [128, 8, 128], F32)
        x2t = [sb(f"x2t{i}", [128, D], BF16) for i in range(2)]
        ohb = sb("ohb", [128, 32], BF16)
        bufs = dict(ss=sb("ssD", [128, 1], F32), sd=sb("sdD", [128, 1], F32), rs=sb("rsD", [128, 1], F32),
                    junk=sb("junkD", [128, D], BF16), epsc=sb("epscD", [128, 1], F32))
        lg = sb("lg", [128, 36], F32)
        r8 = sb("r8", [128, 80], F32)

        T.op(DVE, lambda: nc.vector.memset(bufs["epsc"][:], EPS), w=["epsc"])
        T.op(DVE, lambda: nc.vector.memset(tot[:], 0.0), w=["tot"])
        T.dma(POOL, wg[:, 0:4, :], w_in_v[:, 0:4, 3592:5640], w=[("wg", 0)], max_dma_last_dim=8192)
        T.dma(POOL, wg[:, 4:8, :], w_in_v[:, 4:8, 3592:5640], w=[("wg", 1)], max_dma_last_dim=8192)
        T.dma(POOL, wbm[:], w_br_m.rearrange("(k p) n -> p k n", p=128), w=["wbm"])
        T.dma(POOL, wbd[:], w_br_d.rearrange("(k p) n -> p k n", p=128), w=["wbd"])
        T.dma(SP, wr[:], wr_in.rearrange("(k p) n -> p k n", p=128), w=["wrt"])
        T.dma(SP, brb[:], br_in[0:1, :].partition_broadcast(128), w=["brb"])
        w_out_v = w_out.rearrange("(k p) n -> p k n", p=128)
        for kc in range(8):
            b = kc % 2
            T.dma(SP, wstg[b][:], w_out_v[:, kc, :], w=[("wstg", b)])
            T.op(DVE, lambda kc=kc, b=b: nc.vector.tensor_tensor(out=wo[:, kc, :], in0=wstg[b][:], in1=g1b[:], op=ALU.mult),
                 r=[("wstg", b), "g1b"], w=[("wo", kc)])

        for blk in range(NB):
            xbb = blk % 2
            bs = slice(blk * 512, (blk + 1) * 512)
            def load_blk(bk):
                bb_ = bk % 2
                sl_ = slice(bk * 512, (bk + 1) * 512)
                T.dma(SP, xb[bb_][:], xnT_v[:, :, sl_], w=[("xbD", bb_)])
                T.dma(SP, hmb_[bb_][:], hmT_v[:, :, sl_], w=[("hmD", bb_)])
                T.dma(SP, hdb_[bb_][:], hdT_v[:, :, sl_], w=[("hdD", bb_)])

            if blk == 0:
                load_blk(0)
                T.dma(SP, xt[0][:], x_in[0:128, :], w=[("xtD", 0)])
            for oc in range(8):
                st = oc % 2
                for which, (colbase, wbr, hsrc, hkey, wkey) in enumerate(((0, wbm, hmb_, "hmD", "wbm"), (1024, wbd, hdb_, "hdD", "wbd"))):
                    gb = 0 + which
                    pbk = 2 + which
                    for kc in range(8):
                        T.op(PE, lambda kc=kc, colbase=colbase, gb=gb: nc.tensor.matmul(
                            PS[gb][:, :], lhsT=wg[:, kc, colbase + oc * 128:colbase + (oc + 1) * 128], rhs=xb[xbb][:, kc, :],
                            start=(kc == 0), stop=(kc == 7)), r=[("wg",), ("xbD", xbb)], w=[("ps", gb)])
                    T.op(ACT, lambda gb=gb, which=which: nc.scalar.activation(out=sg[2 * st + which][:], in_=PS[gb][:, :], func=AF.Sigmoid),
                         r=[("ps", gb)], w=[("sg", 2 * st + which)])
                    for kc in range(4):
                        T.op(PE, lambda kc=kc, wbr=wbr, hsrc=hsrc, pbk=pbk: nc.tensor.matmul(
                            PS[pbk][:, :], lhsT=wbr[:, kc, oc * 128:(oc + 1) * 128], rhs=hsrc[xbb][:, kc, :],
                            start=(kc == 0), stop=(kc == 3)), r=[wkey, (hkey, xbb)], w=[("ps", pbk)])
                    T.op(DVE, lambda which=which, pbk=pbk: nc.vector.tensor_tensor(out=tt[2 * st + which][:], in0=PS[pbk][:, :], in1=sg[2 * st + which][:], op=ALU.mult),
                         r=[("ps", pbk), ("sg", 2 * st + which)], w=[("tt", 2 * st + which)])
                T.op(POOL, lambda st=st: nc.gpsimd.tensor_tensor(out=mT[:, oc, :], in0=tt[2 * st][:], in1=tt[2 * st + 1][:], op=ALU.add),
                     r=[("tt", 2 * st), ("tt", 2 * st + 1)], w=[("mT", oc)])
            for tl in range(4):
                t = blk * 4 + tl
                b = t % 2
                ts_ = slice(tl * 128, (tl + 1) * 128)
                if t + 1 < NT:
                    T.dma(SP, xt[(t + 1) % 2][:], x_in[(t + 1) * 128:(t + 2) * 128, :], w=[("xtD", (t + 1) % 2)])
                if tl == 3 and blk + 1 < NB:
                    load_blk(blk + 1)
                for half in range(2):
                    for oc in range(8):
                        T.op(PE, lambda oc=oc, half=half: nc.tensor.matmul(
                            PS[4 + half][:, :], lhsT=mT[:, oc, ts_], rhs=wo[:, oc, half * 512:(half + 1) * 512],
                            start=(oc == 0), stop=(oc == 7)), r=[("mT", oc), ("wo",)], w=[("ps", 4 + half)])
                    T.op(DVE, lambda half=half, b=b: nc.vector.tensor_tensor(
                        out=x1t[b][:, half * 512:(half + 1) * 512], in0=PS[4 + half][:, :], in1=xt[b][:, half * 512:(half + 1) * 512], op=ALU.add),
                        r=[("ps", 4 + half), ("xtD", b)], w=[("x1t", b, half)])
                T.dma(SP, x1_d[t * 128:(t + 1) * 128, :], x1t[b][:], r=[("x1t", b)], w=[("x1d", t)])
                norm_tile(bufs, x1t[b][:], [("x1t", b), "epsc"], "n2")
                T.op(DVE, lambda b=b: nc.vector.tensor_scalar(out=xhf[:], in0=x1t[b][:], scalar1=bufs["rs"][:], scalar2=None, op0=ALU.mult),
                     r=[("x1t", b), "rs"], w=["xhf"])
                for kc in range(8):
                    T.op(PE, lambda kc=kc: nc.tensor.transpose(PS[6 + kc // 4][:, (kc % 4) * 128:(kc % 4 + 1) * 128], xhf[:, kc * 128:(kc + 1) * 128], ident_f),
                         r=["xhf", "cst"], w=[("ps", 6 + kc // 4, kc % 4)])
                for kc in range(8):
                    T.op(ACT, lambda kc=kc: nc.scalar.activation(
                        out=h2f[:, kc, :], in_=PS[6 + kc // 4][:, (kc % 4) * 128:(kc % 4 + 1) * 128], func=AF.Identity,
                        scale=a2[:, kc:kc + 1], bias=s2[:, kc:kc + 1]), r=[("ps", 6 + kc // 4, kc % 4), "a2", "modc"], w=[("h2f", kc)])
                T.op(DVE, lambda: nc.vector.tensor_tensor(out=xhf[:], in0=xhf[:], in1=a2b[:], op=ALU.mult), r=["xhf", ("a2b",)], w=["xhf"])
                T.op(POOL, lambda b=b: nc.gpsimd.tensor_tensor(out=x2t[b][:], in0=xhf[:], in1=s2b[:], op=ALU.add), r=["xhf", ("s2b",)], w=[("x2t", b)])
                T.dma(SP, X2_d[t * 128:(t + 1) * 128, :], x2t[b][:], r=[("x2t", b)], w=[("X2d", t)])
                for kc in range(8):
                    T.op(PE, lambda kc=kc: nc.tensor.matmul(PS[0][:, 0:36], lhsT=h2f[:, kc, :], rhs=wr[:, kc, :], start=(kc == 0), stop=(kc == 7)),
                         r=[("h2f", kc), "wrt"], w=[("ps", 0)])
                T.op(DVE, lambda: nc.vector.tensor_tensor(out=lg[:], in0=PS[0][:, 0:36], in1=brb[:], op=ALU.add), r=[("ps", 0), "brb"], w=["lg"])
                gl = lg[:, 0:4]
                el = lg[:, 4:36].rearrange("p (g j) -> p g j", g=4)
                R = lambda a_, b_: r8[:, a_:b_]
                T.op(DVE, lambda: nc.vector.tensor_reduce(out=R(0, 1), in_=gl, axis=AX.X, op=ALU.max), r=["lg"], w=[("r8", 0)])
                T.op(DVE, lambda: nc.vector.tensor_scalar(out=R(4, 8), in0=gl, scalar1=R(0, 1), scalar2=None, op0=ALU.is_ge), r=["lg", ("r8", 0)], w=[("r8", 1)])
                T.op(DVE, lambda: nc.vector.tensor_scalar(out=R(1, 2), in0=R(0, 1), scalar1=-1.0, scalar2=None, op0=ALU.mult), r=[("r8", 0)], w=[("r8", 2)])
                T.op(ACT, lambda: nc.scalar.activation(out=R(8, 12), in_=gl, func=AF.Exp, bias=R(1, 2), accum_out=R(2, 3)), r=["lg", ("r8", 2)], w=[("r8", 3)])
                T.op(DVE, lambda: nc.vector.reciprocal(out=R(3, 4), in_=R(2, 3)), r=[("r8", 3)], w=[("r8", 4)])
                T.op(DVE, lambda: nc.vector.tensor_tensor(out=R(16, 48).rearrange("p (g j) -> p g j", g=4), in0=el,
                                                          in1=R(4, 8).unsqueeze(2).to_broadcast([128, 4, 8]), op=ALU.mult),
                     r=["lg", ("r8", 1)], w=[("r8", 5)])
                T.op(DVE, lambda: nc.vector.tensor_reduce(out=R(48, 56), in_=R(16, 48).rearrange("p (g j) -> p j g", g=4), axis=AX.X, op=ALU.add),
                     r=[("r8", 5)], w=[("r8", 6)])
                T.op(DVE, lambda: nc.vector.max(out=R(56, 64), in_=R(48, 56)), r=[("r8", 6)], w=[("r8", 7)])
                T.op(DVE, lambda: nc.vector.tensor_scalar(out=R(64, 72), in0=R(48, 56), scalar1=R(56, 57), scalar2=None, op0=ALU.is_equal),
                     r=[("r8", 6), ("r8", 7)], w=[("r8", 9)])
                T.op(DVE, lambda: nc.vector.tensor_scalar(out=R(72, 80), in0=R(48, 56), scalar1=R(57, 58), scalar2=None, op0=ALU.is_equal),
                     r=[("r8", 6), ("r8", 7)], w=[("r8", 10)])
                T.op(DVE, lambda: nc.vector.tensor_tensor(out=R(1, 2), in0=R(57, 58), in1=R(56, 57), op=ALU.subtract), r=[("r8", 7), ("r8", 3)], w=[("r8", 2)])
                T.op(ACT, lambda: nc.scalar.activation(out=R(2, 3), in_=R(1, 2), func=AF.Exp), r=[("r8", 2), ("r8", 4)], w=[("r8", 3)])
                T.op(DVE, lambda: nc.vector.tensor_scalar(out=R(1, 2), in0=R(2, 3), scalar1=1.0, scalar2=None, op0=ALU.add), r=[("r8", 3)], w=[("r8", 2)])
                T.op(DVE, lambda: nc.vector.reciprocal(out=R(1, 2), in_=R(1, 2)), r=[("r8", 2)], w=[("r8", 2)])
                T.op(DVE, lambda t=t: nc.vector.tensor_tensor(out=w12[:, t, 0:1], in0=R(1, 2), in1=R(3, 4), op=ALU.mult), r=[("r8", 2), ("r8", 4)], w=[("w12", t, 0)])
                T.op(DVE, lambda t=t: nc.vector.tensor_tensor(out=w12[:, t, 1:2], in0=w12[:, t, 0:1], in1=R(2, 3), op=ALU.mult), r=[("w12", t, 0), ("r8", 3)], w=[("w12", t, 1)])
                for g in range(4):
                    T.op(DVE, lambda g=g, t=t: nc.vector.tensor_scalar(out=m1_all[:, t, g * 8:(g + 1) * 8], in0=R(64, 72), scalar1=R(4 + g, 5 + g), scalar2=None, op0=ALU.mult),
                         r=[("r8", 9), ("r8", 1)], w=[("m1", t, g)])
                    T.op(DVE, lambda g=g, t=t: nc.vector.tensor_scalar(out=m2_all[:, t, g * 8:(g + 1) * 8], in0=R(72, 80), scalar1=R(4 + g, 5 + g), scalar2=None, op0=ALU.mult),
                         r=[("r8", 10), ("r8", 1)], w=[("m2", t, g)])
                T.op(DVE, lambda t=t: nc.vector.tensor_tensor(out=ohb[:], in0=m1_all[:, t, :], in1=m2_all[:, t, :], op=ALU.add),
                     r=[("m1", t), ("m2", t)], w=["ohb"])
                T.op(PE, lambda: nc.tensor.matmul(PS[1][:, 0:32], lhsT=tri_b, rhs=ohb[:], start=True, stop=True), r=["ohb", "cstb"], w=[("ps", 1)])
                T.op(DVE, lambda t=t: nc.vector.tensor_tensor(out=rank_all[:, t, :], in0=PS[1][:, 0:32], in1=tot[:], op=ALU.add),
                     r=[("ps", 1), "tot"], w=[("rank", t)])
                T.op(DVE, lambda t=t: nc.vector.tensor_tensor(out=rank_all[:, t, :], in0=rank_all[:, t, :], in1=ohb[:], op=ALU.subtract),
                     r=[("rank", t), "ohb"], w=[("rank", t)])
                T.op(PE, lambda: nc.tensor.matmul(PS[1][:, 0:32], lhsT=ones_b, rhs=ohb[:], start=True, stop=True), r=["ohb", "cstb"], w=[("ps", 1)])
                T.op(DVE, lambda: nc.vector.tensor_tensor(out=tot[:], in0=PS[1][:, 0:32], in1=tot[:], op=ALU.add), r=[("ps", 1), "tot"], w=["tot"])
        T.barrier()
    if dbg and "x1" in dbg_out:
        dump_dram(dbg_out["x1"], x1_d, S, D, F32)
        T.dma(SP, dbg_out["tot"][:, :], tot[:], r=["tot"])
        T.dma(SP, dbg_out["rank"][:, :], rank_all[:].rearrange("p t e -> p (t e)"), r=[("rank",)])
        T.dma(SP, dbg_out["m1"][:, :], m1_all[:].rearrange("p t e -> p (t e)"), r=[("m1",)])
        T.dma(SP, dbg_out["m2"][:, :], m2_all[:].rearrange("p t e -> p (t e)"), r=[("m2",)])
        T.dma(SP, dbg_out["w12"][:, :], w12[:].rearrange("p t e -> p (t e)"), r=[("w12",)])
    if stage <= 5:
        return finish()

    scope_mark("phE")
    with ExitStack() as ph:
        def sb(name, shape, dt):
            return ph.enter_context(nc.sbuf_tensor(name, list(shape), dt))
        P12 = sb("P12", [128, 2, NT], I32)
        NSPL = int(os.environ.get("KSPLIT", 1))
        widx = sb("widx", [128, NSPL, NSLOT], I32)
        with ExitStack() as ph2:
            def sb2(name, shape, dt):
                return ph2.enter_context(nc.sbuf_tensor("s_" + name, list(shape), dt))
            ni = sb2("ni", [128, 32], I32)
            ntf = sb2("ntf", [128, 32], F32)
            onesr = sb2("ones32", [128, 32], F32)
            tend = sb2("tend", [128, 32], F32)
            off = sb2("off", [128, 32], F32)
            big = sb2("big", [128, NT, 32], F32)
            pf = sb2("pf", [128, 2, NT], F32)
            cmp3 = sb2("cmp3", [128, NSLOT, 32], F32)
            sidx = sb2("sidx", [128, NSLOT], F32)
            esl = sb2("esl", [128, NSLOT], F32)
            pio = sb2("pio", [128, 1], F32)
            pioi = sb2("pioi", [128, 1], I32)
            T.op(DVE, lambda: nc.vector.tensor_scalar(out=ntf[:], in0=tot[:], scalar1=127.0, scalar2=None, op0=ALU.add), r=["tot"], w=["ntf"])
            T.op(DVE, lambda: nc.vector.tensor_copy(out=ni[:], in_=ntf[:]), r=["ntf"], w=["ni"])
            T.op(DVE, lambda: nc.vector.tensor_scalar(out=ni[:], in0=ni[:], scalar1=7, scalar2=None, op0=ALU.arith_shift_right),
                 r=["ni"], w=["ni"])
            T.op(DVE, lambda: nc.vector.tensor_copy(out=ntf[:], in_=ni[:]), r=["ni"], w=["ntf"])
            T.op(DVE, lambda: nc.vector.memset(onesr[:], 1.0), w=["ones32"])
            T.op(DVE, lambda: nc.vector.tensor_tensor_scan(out=tend[:], data0=onesr[:], data1=ntf[:], initial=0.0, op0=ALU.mult, op1=ALU.add),
                 r=["ones32", "ntf"], w=["tend"])
            T.op(DVE, lambda: nc.vector.tensor_tensor(out=off[:], in0=tend[:], in1=ntf[:], op=ALU.subtract), r=["tend", "ntf"], w=["off"])
            T.op(DVE, lambda: nc.vector.tensor_scalar(out=off[:], in0=off[:], scalar1=128.0, scalar2=None, op0=ALU.mult), r=["off"], w=["off"])
            for k, mk in enumerate((m1_all, m2_all)):
                T.op(DVE, lambda: nc.vector.tensor_tensor(out=big[:], in0=rank_all[:], in1=off[:].unsqueeze(1).to_broadcast([128, NT, 32]), op=ALU.add),
                     r=[("rank",), "off"], w=["big"])
                T.op(DVE, lambda mk=mk: nc.vector.tensor_tensor(out=big[:], in0=big[:], in1=mk[:], op=ALU.mult), r=["big", ("m1",), ("m2",)], w=["big"])
                T.op(DVE, lambda k=k: nc.vector.tensor_reduce(out=pf[:, k, :], in_=big[:], axis=AX.X, op=ALU.add), r=["big"], w=[("pf", k)])
            T.op(DVE, lambda: nc.vector.tensor_scalar(out=pf[:], in0=pf[:], scalar1=0.0, scalar2=float(NSLOT * 128 - 1), op0=ALU.max, op1=ALU.min),
                 r=[("pf",)], w=[("pf",)])
            T.op(DVE, lambda: nc.vector.tensor_copy(out=P12[:], in_=pf[:]), r=[("pf",)], w=["P12"])
            T.op(POOL, lambda: nc.gpsimd.iota(out=sidx[:], pattern=[[1, NSLOT]], base=0, channel_multiplier=0, allow_small_or_imprecise_dtypes=True), w=["sidx"])
            T.op(POOL, lambda: nc.gpsimd.iota(out=pio[:], pattern=[[1, 1]], base=0, channel_multiplier=1, allow_small_or_imprecise_dtypes=True), w=["pio"])
            T.op(DVE, lambda: nc.vector.tensor_tensor(out=cmp3[:], in0=tend[:].unsqueeze(1).to_broadcast([128, NSLOT, 32]),
                                                      in1=sidx[:].unsqueeze(2).to_broadcast([128, NSLOT, 32]), op=ALU.is_le),
                 r=["tend", "sidx"], w=["cmp3"])
            T.op(DVE, lambda: nc.vector.tensor_reduce(out=esl[:], in_=cmp3[:], axis=AX.X, op=ALU.add), r=["cmp3"], w=["esl"])
            T.op(DVE, lambda: nc.vector.tensor_scalar(out=esl[:], in0=esl[:], scalar1=31.0, scalar2=128.0, op0=ALU.min, op1=ALU.mult), r=["esl"], w=["esl"])
            T.op(DVE, lambda: nc.vector.tensor_scalar(out=esl[:], in0=esl[:], scalar1=pio[:], scalar2=None, op0=ALU.add), r=["esl", "pio"], w=["esl"])
            for ci in range(NSPL):
                T.op(DVE, lambda ci=ci: nc.vector.tensor_scalar(out=sidx[:], in0=esl[:], scalar1=float(NSPL), scalar2=float(ci), op0=ALU.mult, op1=ALU.add),
                     r=["esl", "cmp3"], w=["sidx"])
                T.op(DVE, lambda ci=ci: nc.vector.tensor_copy(out=widx[:, ci, :], in_=sidx[:]), r=["sidx"], w=[("widx", ci)])
            T.barrier()
        if dbg and "P12" in dbg_out:
            T.dma(SP, dbg_out["P12"][:, :], P12[:].rearrange("p k t -> p (k t)"), r=["P12"])
            T.dma(SP, dbg_out["widx"][:, :], widx[:, 0, :], r=["widx"])

        KE = int(os.environ.get("KE", 9))
        if KE >= 2:
            x2l = [sb(f"x2l{i}", [128, D], BF16) for i in range(3)]
            for t in range(NT):
                b = t % 3
                T.dma(SP, x2l[b][:], X2_d[t * 128:(t + 1) * 128, :], w=[("x2l", b)])
                for k in range(2):
                    T.dmai(Xs_d[:, :], x2l[b][:], out_idx=P12[:, k, t:t + 1], r=[("x2l", b), "P12"], w=[("Xs", t, k)])
            T.barrier()

        if KE >= 3:
            xs = [sb(f"xs{i}", [128, D], BF16) for i in range(3)]
            wsl = [sb(f"wsl{i}", [128, 6144], BF16) for i in range(3)]
            xT = [sb(f"xTs{i}", [128, 8, 128], BF16) for i in range(2)]
            sS = [sb(f"sS{i}", [128, 256], F32) for i in range(2)]
            ac = [sb(f"ac{i}", [128, 256], BF16) for i in range(2)]
            aT = [sb(f"aTs{i}", [128, 2, 128], BF16) for i in range(2)]
            yo = [sb(f"yo{i}", [128, D], BF16) for i in range(2)]
            NSL = int(os.environ.get("KSLOTS", NSLOT))

            def load_slot(sl):
                b3 = sl % 3
                T.dma(SP, xs[b3][:], Xs_d[sl * 128:(sl + 1) * 128, :], r=[("Xs",)], w=[("xs", b3)])
                cw_ = 6144 // NSPL
                for ci in range(NSPL):
                    T.dmai(wsl[b3][:, ci * cw_:(ci + 1) * cw_], Wcat_d.rearrange("r (c w) -> (r c) w", c=NSPL), in_idx=widx[:, ci, sl:sl + 1],
                           r=[("Wcat",), "widx"], w=[("wsl", b3, ci)])

            for sl in range(min(2, NSL)):
                load_slot(sl)
            for sl in range(NSL):
                b2 = sl % 2
                b3 = sl % 3
                base = 4 * b2
                if sl + 2 < NSL:
                    load_slot(sl + 2)
                xTp = psb(base).rearrange("p (k t) -> p k t", k=8)
                for kc in range(8):
                    T.op(PE, lambda kc=kc: nc.tensor.transpose(xTp[:, kc, :], xs[b3][:, kc * 128:(kc + 1) * 128], ident_b),
                         r=[("xs", b3), "cstb"], w=[("ps", base)])
                T.op(ACT, lambda: nc.scalar.copy(out=xT[b2][:, 0:4, :], in_=xTp[:, 0:4, :]), r=[("ps", base)], w=[("xTs", b2, 0)])
                T.op(DVE, lambda: nc.vector.tensor_copy(out=xT[b2][:, 4:8, :], in_=xTp[:, 4:8, :]), r=[("ps", base), ("xTs", b2, 0)], w=[("xTs", b2, 1)])
                for kc in range(8):
                    T.op(PE, lambda kc=kc: nc.tensor.matmul(PS[base + 1][:, :], lhsT=xT[b2][:, kc, :], rhs=wsl[b3][:, kc * 512:(kc + 1) * 512],
                                                            start=(kc == 0), stop=(kc == 7)), r=[("xTs", b2), ("wsl", b3)], w=[("ps", base + 1)])
                T.op(ACT, lambda: nc.scalar.activation(out=sS[b2][:], in_=PS[base + 1][:, 0:256], func=AF.Silu), r=[("ps", base + 1)], w=[("sS", b2)])
                T.op(DVE, lambda: nc.vector.tensor_tensor(out=ac[b2][:], in0=PS[base + 1][:, 256:512], in1=sS[b2][:], op=ALU.mult),
                     r=[("ps", base + 1), ("sS", b2)], w=[("ac", b2)])
                aTp = psb(base)[:, 0:256].rearrange("p (k t) -> p k t", k=2)
                for fc in range(2):
                    T.op(PE, lambda fc=fc: nc.tensor.transpose(aTp[:, fc, :], ac[b2][:, fc * 128:(fc + 1) * 128], ident_b),
                         r=[("ac", b2), "cstb"], w=[("ps", base)])
                T.op(ACT, lambda: nc.scalar.copy(out=aT[b2][:], in_=aTp), r=[("ps", base)], w=[("aTs", b2)])
                for cb_ in range(2):
                    for fc in range(2):
                        T.op(PE, lambda fc=fc, cb_=cb_: nc.tensor.matmul(
                            PS[base + 2 + cb_][:, :], lhsT=aT[b2][:, fc, :], rhs=wsl[b3][:, 4096 + fc * 1024 + cb_ * 512: 4096 + fc * 1024 + (cb_ + 1) * 512],
                            start=(fc == 0), stop=(fc == 1)), r=[("aTs", b2), ("wsl", b3)], w=[("ps", base + 2 + cb_)])
                T.op(ACT, lambda: nc.scalar.copy(out=yo[b2][:, 0:512], in_=PS[base + 2][:, :]), r=[("ps", base + 2)], w=[("yo", b2, 0)])
                T.op(DVE, lambda: nc.vector.tensor_copy(out=yo[b2][:, 512:1024], in_=PS[base + 3][:, :]), r=[("ps", base + 3)], w=[("yo", b2, 1)])
                T.dma(SP, Ys_d[sl * 128:(sl + 1) * 128, :], yo[b2][:], r=[("yo", b2)], w=[("Ys", sl)])
            T.barrier()

        if KE >= 4:
            y1 = [sb(f"y1_{i}", [128, D], BF16) for i in range(3)]
            ya = [sb(f"ya_{i}", [128, D], F32) for i in range(3)]
            y2 = [sb(f"y2_{i}", [128, D], BF16) for i in range(3)]
            x1l = [sb(f"x1l{i}", [128, D], F32) for i in range(3)]
            def load_tok(t):
                b = t % 3
                T.dma(SP, x1l[b][:], x1_d[t * 128:(t + 1) * 128, :], w=[("x1l", b)])
                T.dmai(y1[b][:], Ys_d[:, :], in_idx=P12[:, 0, t:t + 1], r=[("Ys",), "P12"], w=[("y1", b)])
                T.dmai(y2[b][:], Ys_d[:, :], in_idx=P12[:, 1, t:t + 1], r=[("Ys",), "P12"], w=[("y2", b)])

            load_tok(0)
            load_tok(1)
            for t in range(NT):
                b = t % 3
                if t + 2 < NT:
                    load_tok(t + 2)
                T.op(DVE, lambda b=b, t=t: nc.vector.tensor_scalar(out=ya[b][:], in0=y1[b][:], scalar1=w12[:, t, 0:1], scalar2=None, op0=ALU.mult),
                     r=[("y1", b), ("w12",)], w=[("ya", b)])
                T.op(DVE, lambda b=b, t=t: nc.vector.scalar_tensor_tensor(out=ya[b][:], in0=y2[b][:], scalar=w12[:, t, 1:2], in1=ya[b][:], op0=ALU.mult, op1=ALU.add),
                     r=[("ya", b), ("y2", b), ("w12",)], w=[("ya", b)])
                T.op(DVE, lambda b=b: nc.vector.tensor_tensor(out=ya[b][:], in0=ya[b][:], in1=g2b[:], op=ALU.mult), r=[("ya", b), ("g2b",)], w=[("ya", b)])
                T.op(DVE, lambda b=b: nc.vector.tensor_tensor(out=x1l[b][:], in0=x1l[b][:], in1=ya[b][:], op=ALU.add), r=[("ya", b), ("x1l", b)], w=[("x1l", b)])
                T.dma(SP, out_d[t * 128:(t + 1) * 128, :], x1l[b][:], r=[("x1l", b)], w=[("out", t)])
            T.barrier()
    return finish()


def _consts():
    c = np.zeros((128, NCST), np.float32)
    c[:, 0:128] = np.eye(128, dtype=np.float32)
    s = np.arange(128)[:, None]
    t = np.arange(128)[None, :]
    c[:, 128:256] = (s <= t).astype(np.float32)
    c[:, 256:384] = ((s // 64) == (t // 64)).astype(np.float32)
    R = np.zeros((128, 128), np.float32)
    for base in (0, 64):
        for d in range(8):
            R[base + d, base + d + 8] = -1.0
            R[base + d + 8, base + d] = 1.0
    c[:, 384:512] = R.T
    c[:, 512:640] = 1.0
    inv = 500000.0 ** (-np.arange(0, 16, 2, dtype=np.float32) / 16.0)
    for p in range(128):
        d = p % 64
        c[p, 640] = inv[d % 8] if d < 16 else 0.0
    return c


_NC_CACHE = {}


def make_in_maps(inputs):
    f = lambda k: np.ascontiguousarray(np.asarray(inputs[k], dtype=np.float32))
    x = f("x")
    c = f("c")
    pos = np.ascontiguousarray(np.asarray(inputs["positions"], dtype=np.int32))
    b_ada = f("b_ada")[0]
    shared = {
        "w_ada": f("w_ada")[0],
        "b_ada_col": np.ascontiguousarray(b_ada.reshape(48, 128).T),
        "b_ada_row": np.ascontiguousarray(b_ada.reshape(1, -1)),
        "n1w": np.ascontiguousarray(f("norm1_w")[0].reshape(8, 128).T),
        "n2w": np.ascontiguousarray(f("norm2_w")[0].reshape(8, 128).T),
        "n2row": np.ascontiguousarray(f("norm2_w")[0].reshape(1, D)),
        "w_in": f("w_in")[0],
        "bgate": np.ascontiguousarray(np.stack([f("b_igate")[0], f("b_fgate")[0]], axis=1)),
        "convw": np.ascontiguousarray(f("conv_w")[0].T.reshape(8, 128, 4).transpose(1, 0, 2)),
        "convb": np.ascontiguousarray(f("conv_b")[0].reshape(8, 128).T),
        "mnw": np.ascontiguousarray(f("mlstm_norm_w")[0].reshape(1, 512)),
        "qnw": np.ascontiguousarray(np.tile(f("q_norm_w")[0], 2).reshape(128, 1)),
        "knw": np.ascontiguousarray(np.tile(f("k_norm_w")[0], 2).reshape(128, 1)),
        "qkrow": np.ascontiguousarray(np.concatenate([f("q_norm_w")[0], f("k_norm_w")[0]]).reshape(1, 128)),
        "lamv": np.ascontiguousarray(np.concatenate([f("lam_q1")[0], f("lam_k1")[0], f("lam_q2")[0], f("lam_k2")[0]]).reshape(1, 256)),
        "subw": np.ascontiguousarray(f("subln_w")[0].reshape(128, 1)),
        "w_br_m": f("w_br_m")[0],
        "w_br_d": f("w_br_d")[0],
        "w_out": f("w_out")[0],
        "wr": np.ascontiguousarray(np.concatenate([f("w_rg")[0], f("w_re")[0]], axis=1)),
        "br": np.ascontiguousarray(np.concatenate([f("b_rg")[0], f("b_re")[0]]).reshape(1, 36)),
        "w_gate": f("w_gate")[0],
        "w_up": f("w_up")[0],
        "w_down": f("w_down")[0],
        "consts": _consts(),
    }
    maps = []
    for b in range(8):
        m = dict(shared)
        m["x"] = x[b]
        m["cT"] = np.ascontiguousarray(c[b].reshape(8, 128).T)
        m["pos"] = np.ascontiguousarray(pos[b].reshape(1, S))
        maps.append(m)
    return maps


def kernel(**inputs):
    if "nc" not in _NC_CACHE:
        _NC_CACHE["nc"] = build()
    nc = _NC_CACHE["nc"]
    maps = make_in_maps(inputs)
    res = run_bass_kernel_spmd(nc, maps, core_ids=list(range(8)))
    out = np.stack([np.asarray(r["out"], dtype=np.float32) for r in res.results], axis=0)
    return out
```

```python
import math
import os
from contextlib import ExitStack

import numpy as np
import concourse.bass as bass
import concourse.mybir as mybir
from concourse.bass_utils import run_bass_kernel_spmd

F32 = mybir.dt.float32
BF16 = mybir.dt.bfloat16
I32 = mybir.dt.int32
AF = mybir.ActivationFunctionType
ALU = mybir.AluOpType
AX = mybir.AxisListType

S = 4096
D = 1024
NT = 32
NB = 8
EPS = 1e-6
NCST = 5 * 128 + 2


class _StopBuild(Exception):
    pass


class Trk:
    LIMIT = 50000
    NDSEM = 12

    def __init__(self, nc):
        self.nc = nc
        self.eng = {"pe": nc.tensor, "act": nc.scalar, "dve": nc.vector, "pool": nc.gpsimd, "sp": nc.sync}
        self.cur = {}
        self.seq = {}
        self.gen = {}
        for e in self.eng:
            self.cur[e] = [nc.alloc_semaphore(f"s_{e}_0"), 0]
            self.seq[e] = 0
            self.gen[e] = 0
        self.allsems = [(e, self.cur[e]) for e in self.eng]
        self.waited = {e: {} for e in self.eng}
        self.lastw = {}
        self.readers = {}
        self.children = {}
        self.dring = {}
        for q in ("sp", "pool", "act", "wc"):
            self.dring[q] = [[nc.alloc_semaphore(f"d_{q}_{i}"), 0] for i in range(self.NDSEM)]
        self.dpos = {q: 0 for q in self.dring}
        self.all_dma = []

    def _related(self, k):
        ks = [k[:i] for i in range(1, len(k) + 1)]
        stack = [k]
        while stack:
            p = stack.pop()
            for c in self.children.get(p, ()):
                ks.append(c)
                stack.append(c)
        return ks

    def _register(self, k):
        for i in range(1, len(k)):
            self.children.setdefault(k[:i], set()).add(k[:i + 1])

    def _wait(self, e, ev):
        sem, val, src, sq = ev
        if src == e:
            if e == "pe":
                return
            if e != "pool" and self.seq[e] - sq >= 6:
                return
        w = self.waited[e]
        if w.get(sem.name, 0) >= val:
            return
        self.eng[e].wait_ge(sem, val)
        w[sem.name] = val

    def _deps(self, e, r, w):
        evs = []
        for k in r:
            for kk in self._related(k):
                if kk in self.lastw:
                    evs.append(self.lastw[kk])
        for k in w:
            for kk in self._related(k):
                if kk in self.lastw:
                    evs.append(self.lastw[kk])
                evs.extend(self.readers.get(kk, {}).values())
        for ev in evs:
            self._wait(e, ev)

    def _record(self, ev, r, w):
        for k in r:
            self._register(k)
            self.readers.setdefault(k, {})[ev[2] if ev[2] is not None else ev[0].name] = ev
        for k in w:
            self._register(k)
            self.lastw[k] = ev
            self.readers[k] = {}
            for kk in self._related(k):
                if kk != k and len(kk) > len(k):
                    self.readers[kk] = {}
                    self.lastw.pop(kk, None)

    @staticmethod
    def _norm(ks):
        out = []
        for k in ks:
            k = tuple(k) if isinstance(k, (tuple, list)) else (k,)
            if k[0] == "ps":
                k = k[:2]
            out.append(k)
        return out

    def op(self, e, fn, r=(), w=()):
        r = self._norm(r)
        w = self._norm(w)
        w = w + [k for k in r if k[0] == "ps" and k not in w]
        self._deps(e, r, w)
        ins = fn()
        c = self.cur[e]
        c[1] += 1
        self.seq[e] += 1
        ins.then_inc(c[0], 1)
        ev = (c[0], c[1], e, self.seq[e])
        self._record(ev, r, w)
        if c[1] >= self.LIMIT:
            self.gen[e] += 1
            self.cur[e] = [self.nc.alloc_semaphore(f"s_{e}_{self.gen[e]}"), 0]
            self.allsems.append((e, self.cur[e]))
        return ins

    def dma(self, q, out, in_, r=(), w=(), ring=None, **kw):
        r = self._norm(r)
        w = self._norm(w)
        rq = ring or q
        ring = self.dring[rq]
        slot = ring[self.dpos[rq] % self.NDSEM]
        self.dpos[rq] += 1
        if slot[1] > 0:
            self._wait(q, (slot[0], slot[1], None, 0))
        self._deps(q, r, w)
        ins = self.eng[q].dma_start(out=out, in_=in_, **kw)
        slot[1] += 16
        ins.then_inc(slot[0], 16)
        ev = (slot[0], slot[1], None, 0)
        self._record(ev, r, w)
        return ins

    def dmai(self, out, in_, out_idx=None, in_idx=None, r=(), w=()):
        q = "pool"
        r = self._norm(r)
        w = self._norm(w)
        ring = self.dring[q]
        slot = ring[self.dpos[q] % self.NDSEM]
        self.dpos[q] += 1
        if slot[1] > 0:
            self._wait(q, (slot[0], slot[1], None, 0))
        self._deps(q, r, w)
        ins = self.nc.gpsimd.indirect_dma_start(
            out=out, out_offset=(bass.IndirectOffsetOnAxis(ap=out_idx, axis=0) if out_idx is not None else None),
            in_=in_, in_offset=(bass.IndirectOffsetOnAxis(ap=in_idx, axis=0) if in_idx is not None else None))
        slot[1] += 16
        ins.then_inc(slot[0], 16)
        ev = (slot[0], slot[1], None, 0)
        self._record(ev, r, w)
        return ins

    def barrier(self, full=True):
        evs = []
        for (se, c) in self.allsems:
            if c[1] > 0:
                evs.append((c[0], c[1], "__" + se, 0))
        for q, ring in self.dring.items():
            if q == "wc" and not full:
                continue
            for slot in ring:
                if slot[1] > 0:
                    evs.append((slot[0], slot[1], None, 0))
        for e in self.eng:
            for ev in evs:
                if ev[2] == "__" + e:
                    continue
                self._wait(e, ev)
        self.lastw.clear()
        self.readers.clear()
        self.children.clear()


def build(stage=99, dbg=None):
    nc = bass.Bass("TRN2", target_bir_lowering=False)
    T = Trk(nc)
    _scope = [None]

    def scope_mark(nm):
        if os.environ.get("KSCOPE") != "1":
            return
        if _scope[0] is not None:
            _scope[0].__exit__(None, None, None)
        _scope[0] = nc.named_scope(nm)
        _scope[0].__enter__()

    PE, ACT, DVE, POOL, SP = "pe", "act", "dve", "pool", "sp"

    def din(name, shape, dt=F32):
        return nc.dram_tensor(name, list(shape), dt, kind="ExternalInput").ap()

    def dscr(name, shape, dt):
        return nc.dram_tensor(name, list(shape), dt, kind="Internal").ap()

    x_in = din("x", [S, D])
    cT_in = din("cT", [128, 8])
    pos_in = din("pos", [1, S], I32)
    w_ada = din("w_ada", [D, 6 * D])
    b_ada_col = din("b_ada_col", [128, 48])
    b_ada_row = din("b_ada_row", [1, 6 * D])
    n1w_in = din("n1w", [128, 8])
    n2w_in = din("n2w", [128, 8])
    n2row_in = din("n2row", [1, D])
    w_in = din("w_in", [D, 5640])
    bgate_in = din("bgate", [4, 2])
    convw_in = din("convw", [128, 8, 4])
    convb_in = din("convb", [128, 8])
    mnw_in = din("mnw", [1, 512])
    qnw_in = din("qnw", [128, 1])
    knw_in = din("knw", [128, 1])
    qkrow_in = din("qkrow", [1, 128])
    lamv_in = din("lamv", [1, 256])
    subw_in = din("subw", [128, 1])
    w_br_m = din("w_br_m", [512, D])
    w_br_d = din("w_br_d", [512, D])
    w_out = din("w_out", [D, D])
    wr_in = din("wr", [D, 36])
    br_in = din("br", [1, 36])
    w_gate = din("w_gate", [32, D, 256])
    w_up = din("w_up", [32, D, 256])
    w_down = din("w_down", [32, 256, D])
    consts_in = din("consts", [128, NCST])
    out_d = nc.dram_tensor("out", [S, D], F32, kind="ExternalOutput").ap()

    xnT_d = dscr("xnT_d", [D, S], BF16)
    hmT_d = dscr("hmT_d", [512, S], BF16)
    hdT_d = dscr("hdT_d", [512, S], BF16)
    x1_d = dscr("x1_d", [S, D], F32)
    xn2T_d = dscr("xn2T_d", [D, S], BF16)
    combT_d = dscr("combT_d", [32, S], BF16)
    dec_d = dscr("dec_d", [4, 32], F32)
    gi_d = dscr("gi_d", [4, S], F32)
    NSLOT = 96
    X2_d = dscr("X2_d", [S, D], BF16)
    Xs_d = dscr("Xs_d", [NSLOT * 128, D], BF16)
    Ys_d = dscr("Ys_d", [NSLOT * 128, D], BF16)
    Wcat_d = dscr("Wcat_d", [32 * 128, 6144], BF16)
    gf_d = dscr("gf_d", [4, S], F32)

    dbg_out = {}
    if dbg:
        for name, (shape, dt) in dbg.items():
            dbg_out[name] = nc.dram_tensor("dbg_" + name, list(shape), dt, kind="ExternalOutput").ap()

    PS = [nc.alloc_psum_tensor(f"ps{i}", [128, 512], F32) for i in range(8)]

    def psb(i):
        return PS[i][:].bitcast(BF16)

    xnT_v = xnT_d.rearrange("(k p) t -> p k t", p=128)
    xn2T_v = xn2T_d.rearrange("(k p) t -> p k t", p=128)
    hmT_v = hmT_d.rearrange("(k p) t -> p k t", p=128)
    hdT_v = hdT_d.rearrange("(k p) t -> p k t", p=128)
    w_in_v = w_in.rearrange("(k p) n -> p k n", p=128)
    wg_v = w_gate.rearrange("e (k p) f -> e p k f", p=128)
    wu_v = w_up.rearrange("e (k p) f -> e p k f", p=128)
    wdn_v = w_down.rearrange("e (k p) n -> e p k n", p=128)

    glob = ExitStack()

    def sbg(name, shape, dt):
        return glob.enter_context(nc.sbuf_tensor("s_" + name, list(shape), dt))

    cst = sbg("cst", [128, NCST], F32)
    cstb = sbg("cstb", [128, 5 * 128], BF16)
    modc = sbg("modc", [128, 48], F32)
    a1 = sbg("a1", [128, 8], F32)
    a2 = sbg("a2", [128, 8], F32)
    g1b = sbg("g1b", [128, D], F32)
    g2b = sbg("g2b", [128, D], F32)
    a2b = sbg("a2b", [128, D], F32)
    s2b = sbg("s2b", [128, D], F32)
    rank_all = sbg("rank_all", [128, NT, 32], F32)
    m1_all = sbg("m1_all", [128, NT, 32], F32)
    m2_all = sbg("m2_all", [128, NT, 32], F32)
    w12 = sbg("w12", [128, NT, 2], F32)
    tot = sbg("tot", [128, 32], F32)
    smallc = sbg("smallc", [128, 16], F32)
    ident_f = cst[:, 0:128]
    tri_f = cst[:, 128:256]
    invf = cst[:, 640:641]
    ident_b = cstb[:, 0:128]
    tri_b = cstb[:, 128:256]
    bo64_b = cstb[:, 256:384]
    RT_b = cstb[:, 384:512]
    ones_b = cstb[:, 512:640]
    ones_f = cst[:, 512:640]

    T.dma(SP, cst[:], consts_in[:, :], w=["cst"])
    T.op(DVE, lambda: nc.vector.tensor_copy(out=cstb[:], in_=cst[:, 0:640]), r=["cst"], w=["cstb"])

    early = ExitStack()
    wst = [early.enter_context(nc.sbuf_tensor(f"s_wst{i}", [128, 6144], BF16)) for i in range(2)]
    for e in range(32):
        b = e % 2
        gu = wst[b][:, 0:4096].rearrange("p (k f) -> p k f", k=8)
        T.dma(POOL, gu[:, :, 0:256], wg_v[e], w=[("wst", b, 0)], ring="wc")
        T.dma(POOL, gu[:, :, 256:512], wu_v[e], w=[("wst", b, 1)], ring="wc")
        T.dma(POOL, wst[b][:, 4096:6144].rearrange("p (k n) -> p k n", k=2), wdn_v[e], w=[("wst", b, 2)], ring="wc")
        T.dma(POOL, Wcat_d[e * 128:(e + 1) * 128, :], wst[b][:], r=[("wst", b)], w=[("Wcat", e)], ring="wc")
    early_open = [True]
    scope_mark("ph0")
    with ExitStack() as ph:
        def sb(name, shape, dt):
            return ph.enter_context(nc.sbuf_tensor("s_" + name, list(shape), dt))
        cT = sb("cT", [128, 8], F32)
        cT2 = sb("cT2", [128, 8, 2], F32)
        cbc = sb("cbc", [128, 8, 128], F32)
        wa = [sb(f"wa{i}", [128, 8, 512], F32) for i in range(2)]
        bcol = sb("bcol", [128, 48], F32)
        brow = sb("brow", [128, 2048], F32)
        nw = sb("nw", [128, 16], F32)
        tmp8 = sb("tmp8", [128, 8], F32)
        lamb = sb("lamb", [128, 256], F32)
        qkrow = sb("qkrow", [128, 128], F32)
        lt = sb("lt", [128, 128], F32)
        l2 = sb("l2", [128, 4], F32)

        T.dma(SP, cT[:], cT_in[:, :], w=["cT"])
        T.dma(SP, bcol[:], b_ada_col[:, :], w=["bcol"])
        T.dma(SP, brow[:, 0:1024], b_ada_row[0:1, 2048:3072].partition_broadcast(128), w=["brow0"])
        T.dma(SP, brow[:, 1024:2048], b_ada_row[0:1, 5120:6144].partition_broadcast(128), w=["brow1"])
        T.dma(SP, nw[:, 0:8], n1w_in[:, :], w=["nw0"])
        T.dma(SP, nw[:, 8:16], n2w_in[:, :], w=["nw1"])
        T.dma(SP, smallc[:, 0:1], qnw_in[:, :], w=["sc0"])
        T.dma(SP, smallc[:, 1:2], knw_in[:, :], w=["sc1"])
        T.dma(SP, smallc[:, 2:3], subw_in[:, :], w=["sc2"])
        T.dma(SP, lamb[:], lamv_in[0:1, :].partition_broadcast(128), w=["lamb"])
        T.dma(SP, qkrow[:], qkrow_in[0:1, :].partition_broadcast(128), w=["qkrow"])
        for k in range(2):
            T.op(DVE, lambda k=k: nc.vector.tensor_copy(out=cT2[:, :, k], in_=cT[:]), r=["cT"], w=[("cT2", k)])
        for kc in range(8):
            T.op(DVE, lambda kc=kc: nc.vector.tensor_copy(out=cbc[:, kc, :], in_=cT[:, kc:kc + 1].to_broadcast([128, 128])),
                 r=["cT"], w=[("cbc", kc)])
        w_ada_v = w_ada.rearrange("(k p) n -> p k n", p=128)
        gate_bank = {4: 1, 5: 2, 6: 5, 7: 6, 8: 1, 9: 2, 10: 3, 11: 4}
        n2rb = sb("n2rb", [128, D], F32)
        T.dma(SP, n2rb[:], n2row_in[0:1, :].partition_broadcast(128), w=["n2rb"])
        T.dma(SP, s2b[:], b_ada_row[0:1, 3072:4096].partition_broadcast(128), w=["s2b_bias"])
        T.dma(SP, a2b[:], b_ada_row[0:1, 4096:5120].partition_broadcast(128), w=["a2b_bias"])
        for blk in range(12):
            b = blk % 2
            for hh in range(2):
                T.dma(SP, wa[b][:, 4 * hh:4 * hh + 4, :], w_ada_v[:, 4 * hh:4 * hh + 4, blk * 512:(blk + 1) * 512], w=[("wa", b)])
            for m in range(4):
                j = blk * 4 + m
                for kc in range(8):
                    T.op(PE, lambda j=j, m=m, kc=kc, b=b: nc.tensor.matmul(
                        PS[0][:, 2 * j:2 * j + 2], lhsT=wa[b][:, kc, m * 128:(m + 1) * 128], rhs=cT2[:, kc, :],
                        start=(kc == 0), stop=(kc == 7)), r=[("wa", b), "cT2"], w=[("ps", 0)])
            if blk in gate_bank:
                gb = gate_bank[blk]
                for kc in range(8):
                    T.op(PE, lambda kc=kc, b=b, gb=gb: nc.tensor.matmul(
                        PS[gb][:, :], lhsT=cbc[:, kc, :], rhs=wa[b][:, kc, :], start=(kc == 0), stop=(kc == 7)),
                        r=[("wa", b), "cbc"], w=[("ps", gb)])
                if blk == 5:
                    for hh in range(2):
                        T.op(DVE, lambda hh=hh: nc.vector.tensor_tensor(out=g1b[:, hh * 512:(hh + 1) * 512], in0=PS[1 + hh][:, :],
                                                                      in1=brow[:, hh * 512:(hh + 1) * 512], op=ALU.add),
                             r=[("ps", 1 + hh), "brow0"], w=[("g1b", hh)])
                if blk == 7:
                    for hh in range(2):
                        T.op(DVE, lambda hh=hh: nc.vector.tensor_tensor(out=s2b[:, hh * 512:(hh + 1) * 512], in0=PS[5 + hh][:, :],
                                                                      in1=s2b[:, hh * 512:(hh + 1) * 512], op=ALU.add),
                             r=[("ps", 5 + hh), "s2b_bias"], w=[("s2b", hh)])
                if blk == 9:
                    for hh in range(2):
                        sl = slice(hh * 512, (hh + 1) * 512)
                        T.op(DVE, lambda hh=hh, sl=sl: nc.vector.tensor_tensor(out=a2b[:, sl], in0=PS[1 + hh][:, :], in1=a2b[:, sl], op=ALU.add),
                             r=[("ps", 1 + hh), "a2b_bias"], w=[("a2b", hh)])
                        T.op(DVE, lambda sl=sl: nc.vector.scalar_tensor_tensor(out=a2b[:, sl], in0=a2b[:, sl], scalar=1.0, in1=n2rb[:, sl], op0=ALU.add, op1=ALU.mult),
                             r=[("a2b", hh), "n2rb"], w=[("a2b", hh)])
        T.op(DVE, lambda: nc.vector.tensor_tensor(
            out=modc[:], in0=PS[0][:, 0:96].rearrange("p (m two) -> p m two", two=2)[:, :, 0], in1=bcol[:], op=ALU.add),
            r=[("ps", 0), "bcol"], w=["modc"])
        for gi, (gt, b0) in enumerate(((g1b, 1), (g2b, 3))):
            if gi == 0:
                continue
            for hh in range(2):
                T.op(DVE, lambda gt=gt, b0=b0, hh=hh, gi=gi: nc.vector.tensor_tensor(
                    out=gt[:, hh * 512:(hh + 1) * 512], in0=PS[b0 + hh][:, :],
                    in1=brow[:, gi * 1024 + hh * 512: gi * 1024 + (hh + 1) * 512], op=ALU.add),
                    r=[("ps", b0 + hh), f"brow{gi}"], w=[(f"g{gi + 1}b", hh)])
        for (av, sc0, nwo, akey) in ((a1, 8, 0, "a1"), (a2, 32, 8, "a2")):
            T.op(DVE, lambda sc0=sc0: nc.vector.tensor_scalar(out=tmp8[:], in0=modc[:, sc0:sc0 + 8], scalar1=1.0, scalar2=None, op0=ALU.add),
                 r=["modc"], w=["tmp8"])
            T.op(DVE, lambda av=av, nwo=nwo: nc.vector.tensor_tensor(out=av[:], in0=tmp8[:], in1=nw[:, nwo:nwo + 8], op=ALU.mult),
                 r=["tmp8", "nw0", "nw1"], w=[akey])
        T.op(DVE, lambda: nc.vector.tensor_scalar(out=smallc[:, 2:3], in0=smallc[:, 2:3], scalar1=0.8, scalar2=None, op0=ALU.mult),
             r=["sc2"], w=["sc2"])
        T.op(DVE, lambda: nc.vector.tensor_reduce(out=l2[:, 0:2], in_=qkrow[:].rearrange("p (a b) -> p a b", a=2), axis=AX.X, op=ALU.max,
                                                  apply_absolute_value=True), r=["qkrow"], w=["l2a"])
        T.op(DVE, lambda: nc.vector.tensor_tensor(out=l2[:, 2:3], in0=l2[:, 0:1], in1=l2[:, 1:2], op=ALU.mult), r=["l2a"], w=["l2b"])
        T.op(DVE, lambda: nc.vector.tensor_scalar(out=smallc[:, 3:4], in0=l2[:, 2:3], scalar1=-8.0, scalar2=None, op0=ALU.mult),
             r=["l2b"], w=["sc3"])
        lv = lamb[:].rearrange("p (a b) -> p a b", a=4)
        T.op(DVE, lambda: nc.vector.tensor_tensor(out=lt[:].rearrange("p (a b) -> p a b", a=2), in0=lv[:, 0:4:2, :], in1=lv[:, 1:4:2, :], op=ALU.mult),
             r=["lamb"], w=["lt"])
        T.op(DVE, lambda: nc.vector.tensor_reduce(out=l2[:, 0:2], in_=lt[:].rearrange("p (a b) -> p a b", a=2), axis=AX.X, op=ALU.add),
             r=["lt", "l2a", "l2b"], w=["l2a"])
        T.op(ACT, lambda: nc.scalar.activation(out=l2[:, 2:4], in_=l2[:, 0:2], func=AF.Exp), r=["l2a"], w=["l2b"])
        T.op(DVE, lambda: nc.vector.tensor_tensor(out=l2[:, 0:1], in0=l2[:, 3:4], in1=l2[:, 2:3], op=ALU.subtract), r=["l2b"], w=["l2a"])
        T.op(DVE, lambda: nc.vector.tensor_scalar(out=smallc[:, 4:5], in0=l2[:, 0:1], scalar1=-0.2, scalar2=None, op0=ALU.add),
             r=["l2a"], w=["sc4"])
        T.barrier(full=False)
    if dbg and "modc" in dbg_out:
        T.dma(SP, dbg_out["modc"][:, :], modc[:], r=["modc"])
        T.dma(SP, dbg_out["g1b"][:, :], g1b[:], r=["g1b"])
        T.dma(SP, dbg_out["smallc"][:, :], smallc[:], r=["sc0"])

    def dump_dram(dst, src, rows, cols, dt):
        with nc.sbuf_tensor("s_dump_" + dst.name.replace(".", "_"), [128, cols], dt) as tmp:
            for r0 in range(0, rows, 128):
                n = min(128, rows - r0)
                T.dma(SP, tmp[0:n, :], src[r0:r0 + n, :], w=["dumptmp"])
                T.dma(SP, dst[r0:r0 + n, :], tmp[0:n, :], r=["dumptmp"], w=[("dumpdst", r0)])
            T.barrier()

    def finish():
        if _scope[0] is not None:
            _scope[0].__exit__(None, None, None)
            _scope[0] = None
        T.barrier()
        if early_open[0]:
            early.close()
            early_open[0] = False
        glob.close()
        return nc

    if stage <= 0:
        return finish()

    s1 = modc[:, 0:8]
    s2 = modc[:, 24:32]

    scope_mark("ph1")
    def norm_tile(ph_bufs, src_ap, rkeys, tag):
        ss, sd, junk = ph_bufs["ss"], ph_bufs["sd"], ph_bufs["junk"]
        T.op(ACT, lambda: nc.scalar.activation(out=junk[:], in_=src_ap, func=AF.Square, accum_out=ss[:]), r=rkeys, w=["junk", "ss"])
        T.op(ACT, lambda: nc.scalar.activation(out=sd[:], in_=ss[:], func=AF.Ln, bias=ph_bufs["epsc"][:], scale=1.0 / D), r=["ss"], w=["sd"])
        T.op(ACT, lambda: nc.scalar.activation(out=ph_bufs["rs"][:], in_=sd[:], func=AF.Exp, scale=-0.5), r=["sd"], w=["rs"])

    with ExitStack() as ph:
        def sb(name, shape, dt):
            return ph.enter_context(nc.sbuf_tensor("s_" + name, list(shape), dt))
        xt = [sb(f"xt{i}", [128, D], F32) for i in range(2)]
        xh = [sb(f"xh{i}", [128, D], BF16) for i in range(2)]
        hTb = [sb(f"hTb{i}", [128, 8, 512], BF16) for i in range(2)]
        bufs = dict(ss=sb("ss", [128, 1], F32), sd=sb("sd", [128, 1], F32), rs=sb("rs", [128, 1], F32),
                    junk=sb("junk", [128, D], BF16), epsc=sb("epsc", [128, 1], F32))
        T.op(DVE, lambda: nc.vector.memset(bufs["epsc"][:], EPS), w=["epsc"])
        pass
        for t in range(int(os.environ.get('K1_TILES', NT))):
            b = t % 2
            blk, tl = t // 4, t % 4
            hb = blk % 2
            T.dma(SP, xt[b][:], x_in[t * 128:(t + 1) * 128, :], w=[("xt", b)])
            KO = int(os.environ.get('K1_OPS', 9))
            if KO < 1:
                continue
            norm_tile(bufs, xt[b][:], [("xt", b), "epsc"], "n1")
            if KO < 2:
                continue
            T.op(DVE, lambda b=b: nc.vector.tensor_scalar(out=xh[b][:], in0=xt[b][:], scalar1=bufs["rs"][:], scalar2=None, op0=ALU.mult),
                 r=[("xt", b), "rs"], w=[("xh", b)])
            if KO < 3:
                continue
            pb = t % 2
            pv = psb(pb).rearrange("p (k t) -> p k t", k=8)
            for kc in range(8):
                T.op(PE, lambda kc=kc, b=b, pv=pv: nc.tensor.transpose(pv[:, kc, :], xh[b][:, kc * 128:(kc + 1) * 128], ident_b),
                     r=[("xh", b), "cstb"], w=[("ps", pb)])
            if KO < 4:
                continue
            for kc in range(8):
                T.op(ACT, lambda kc=kc, pv=pv, hb=hb, tl=tl: nc.scalar.activation(
                    out=hTb[hb][:, kc, tl * 128:(tl + 1) * 128], in_=pv[:, kc, :], func=AF.Identity,
                    **({} if os.environ.get('K1_NOAP') == '1' else ({'scale': a1[:, kc:kc + 1]} if os.environ.get('K1_NOAP') == '2' else
                        ({'bias': s1[:, kc:kc + 1]} if os.environ.get('K1_NOAP') == '3' else dict(scale=a1[:, kc:kc + 1], bias=s1[:, kc:kc + 1]))))),
                    r=[("ps", pb), "a1", "modc"], w=[("hTb", hb, tl)])
            if tl == 3:
                T.dma(SP, xnT_v[:, :, blk * 512:(blk + 1) * 512], hTb[hb][:], r=[("hTb", hb)], w=[("xnT", blk)])
        T.barrier()
    early.close()
    early_open[0] = False
    if dbg and "xnT" in dbg_out:
        dump_dram(dbg_out["xnT"], xnT_d, 1024, S, BF16)
    if stage <= 1:
        return finish()

    scope_mark("phA")
    def sin_reduce(ph_sb, dst_bf, ang, n, tagk):
        ki = ph_sb["ki"]
        kf = ph_sb["kf"]
        msk = ph_sb["msk"]
        C1 = 6.28125
        C2 = 2.0 * math.pi - 6.28125
        T.op(DVE, lambda: nc.vector.tensor_scalar(out=ki[:, 0:n], in0=ang, scalar1=1.0 / (2.0 * math.pi), scalar2=None, op0=ALU.mult),
             r=[tagk], w=["ki"])
        T.op(DVE, lambda: nc.vector.tensor_copy(out=kf[:, 0:n], in_=ki[:, 0:n]), r=["ki"], w=["kf"])
        T.op(DVE, lambda: nc.vector.scalar_tensor_tensor(out=ang, in0=kf[:, 0:n], scalar=-C1, in1=ang, op0=ALU.mult, op1=ALU.add),
             r=["kf", tagk], w=[tagk])
        T.op(DVE, lambda: nc.vector.scalar_tensor_tensor(out=ang, in0=kf[:, 0:n], scalar=-C2, in1=ang, op0=ALU.mult, op1=ALU.add),
             r=["kf", tagk], w=[tagk])
        T.op(DVE, lambda: nc.vector.tensor_scalar(out=msk[:, 0:n], in0=ang, scalar1=math.pi, scalar2=-2.0 * math.pi, op0=ALU.is_gt, op1=ALU.mult),
             r=[tagk], w=["msk"])
        T.op(DVE, lambda: nc.vector.tensor_tensor(out=ang, in0=ang, in1=msk[:, 0:n], op=ALU.add), r=[tagk, "msk"], w=[tagk])
        T.op(DVE, lambda: nc.vector.tensor_scalar(out=msk[:, 0:n], in0=ang, scalar1=-math.pi, scalar2=2.0 * math.pi, op0=ALU.is_lt, op1=ALU.mult),
             r=[tagk], w=["msk"])
        T.op(DVE, lambda: nc.vector.tensor_tensor(out=ang, in0=ang, in1=msk[:, 0:n], op=ALU.add), r=[tagk, "msk"], w=[tagk])
        T.op(DVE, lambda: nc.vector.tensor_scalar(out=ang, in0=ang, scalar1=math.pi, scalar2=-math.pi, op0=ALU.min, op1=ALU.max),
             r=[tagk], w=[tagk])
        T.op(ACT, lambda: nc.scalar.activation(out=dst_bf, in_=ang, func=AF.Sin), r=[tagk], w=[tagk + "_o"])

    persist = ExitStack()

    def sbp(name, shape, dt):
        return persist.enter_context(nc.sbuf_tensor("s_" + name, list(shape), dt))

    with ExitStack() as ph:
        def sb(name, shape, dt):
            return ph.enter_context(nc.sbuf_tensor("s_" + name, list(shape), dt))
        CosB = sb("CosB", [128, S], BF16)
        SinB = sb("SinB", [128, S], BF16)
        with ExitStack() as ph2:
            posi = ph2.enter_context(nc.sbuf_tensor("s_posi", [128, 1024], I32))
            posf = ph2.enter_context(nc.sbuf_tensor("s_posf", [128, 1024], F32))
            ang = ph2.enter_context(nc.sbuf_tensor("s_ang", [128, 1024], F32))
            tb = dict(ki=ph2.enter_context(nc.sbuf_tensor("s_ki", [128, 1024], I32)),
                      kf=ph2.enter_context(nc.sbuf_tensor("s_kf", [128, 1024], F32)),
                      msk=ph2.enter_context(nc.sbuf_tensor("s_msk", [128, 1024], F32)))
            for c4 in range(4):
                cs = slice(c4 * 1024, (c4 + 1) * 1024)
                T.dma(SP, posi[:], pos_in[0:1, cs].partition_broadcast(128), w=["posi"])
                T.op(DVE, lambda: nc.vector.tensor_copy(out=posf[:], in_=posi[:]), r=["posi"], w=["posf"])
                T.op(DVE, lambda: nc.vector.tensor_scalar(out=ang[:], in0=posf[:], scalar1=invf, scalar2=None, op0=ALU.mult),
                     r=["posf", "cst"], w=["ang"])
                sin_reduce(tb, SinB[:, cs], ang[:], 1024, "ang")
                T.op(DVE, lambda: nc.vector.tensor_scalar(out=ang[:], in0=posf[:], scalar1=invf, scalar2=math.pi / 2, op0=ALU.mult, op1=ALU.add),
                     r=["posf", "cst", "ang_o"], w=["ang"])
                sin_reduce(tb, CosB[:, cs], ang[:], 1024, "ang")
            T.barrier()

        wqkv = sb("wqkv", [128, 8, 1536], BF16)
        wgt = sb("wgt", [128, 8, 8], BF16)
        bg = sb("bg", [4, 2], F32)
        nbf = sb("nbf", [4, 1], F32)
        epsc = sb("epscA", [128, 1], F32)
        gst = [[sb(f"gst{g}_{i}", [4, 512], F32) for i in range(2)] for g in range(2)]
        kT = sb("kT", [128, 4, S], BF16)
        v1 = sb("v1", [128, NT, 4, 128], BF16)
        xb = [sb(f"xbA{i}", [128, 8, 512], BF16) for i in range(2)]
        qT = sb("qT", [128, 4, 512], BF16)
        hdTb = sb("hdTb", [128, 4, 512], BF16)
        Pb = [[sb(f"P{m}_{i}", [128, 512], BF16) for i in range(2)] for m in range(2)]
        sqb = [sb(f"sqb{i}", [128, 512], BF16) for i in range(2)]
        qsf = [sb(f"qsf{i}", [128, 512], F32) for i in range(2)]
        sdf = [sb(f"sdf{i}", [128, 512], F32) for i in range(2)]
        qnb = [sb(f"qnb{i}", [128, 512], BF16) for i in range(2)]
        t1f = [sb(f"t1f{i}", [128, 512], F32) for i in range(2)]
        t2f = [sb(f"t2f{i}", [128, 512], F32) for i in range(2)]
        fo = [sb(f"fo{i}", [128, 512], F32) for i in range(4)]

        T.op(DVE, lambda: nc.vector.memset(epsc[:], EPS), w=["epscA"])
        T.dma(POOL, wqkv[:, 0:4, :], w_in_v[:, 0:4, 2056:3592], w=[("wqkv", 0)])
        T.dma(POOL, wqkv[:, 4:8, :], w_in_v[:, 4:8, 2056:3592], w=[("wqkv", 1)])
        T.dma(POOL, wgt[:], w_in_v[:, :, 2048:2056], w=["wgt"])
        T.dma(SP, bg[:], bgate_in[:, :], w=["bg"])
        T.op(DVE, lambda: nc.vector.tensor_scalar(out=nbf[:], in0=bg[:, 1:2], scalar1=-1.0, scalar2=None, op0=ALU.mult), r=["bg"], w=["nbf"])

        negc = smallc[:, 3:4]
        neglam = smallc[:, 4:5]
        subw8 = smallc[:, 2:3]

        for blk in range(NB):
            xbb = blk % 2
            bs = slice(blk * 512, (blk + 1) * 512)
            T.dma(SP, xb[xbb][:], xnT_v[:, :, bs], w=[("xbA", xbb)])
            for c in range(8):
                st = c % 2
                isq = c < 4
                hh = c % 4
                col0 = (0 if isq else 512) + hh * 128
                for kc in range(8):
                    T.op(PE, lambda kc=kc, col0=col0, st=st: nc.tensor.matmul(
                        PS[st][:, :], lhsT=wqkv[:, kc, col0:col0 + 128], rhs=xb[xbb][:, kc, :], start=(kc == 0), stop=(kc == 7)),
                        r=[("wqkv",), ("xbA", xbb)], w=[("ps", st)])
                wcol = smallc[:, 0:1] if isq else smallc[:, 1:2]
                T.op(ACT, lambda st=st: nc.scalar.activation(out=sqb[st][:], in_=PS[st][:, :], func=AF.Square), r=[("ps", st)], w=[("sqb", st)])
                T.op(ACT, lambda st=st, wcol=wcol: nc.scalar.activation(out=qsf[st][:], in_=PS[st][:, :], func=AF.Identity, scale=wcol),
                     r=[("ps", st), "sc0", "sc1"], w=[("qsf", st)])
                T.op(PE, lambda st=st: nc.tensor.matmul(PS[2 + st][:, :], lhsT=bo64_b, rhs=sqb[st][:], start=True, stop=True),
                     r=[("sqb", st), "cstb"], w=[("ps", 2 + st)])
                T.op(ACT, lambda st=st: nc.scalar.activation(out=sdf[st][:], in_=PS[2 + st][:, :], func=AF.Ln, bias=epsc[:], scale=1.0 / 64),
                     r=[("ps", 2 + st), "epscA"], w=[("sdf", st)])
                T.op(ACT, lambda st=st: nc.scalar.activation(out=sdf[st][:], in_=sdf[st][:], func=AF.Exp, scale=-0.5),
                     r=[("sdf", st)], w=[("sdf", st)])
                T.op(DVE, lambda st=st: nc.vector.tensor_tensor(out=qnb[st][:], in0=qsf[st][:], in1=sdf[st][:], op=ALU.mult),
                     r=[("qsf", st), ("sdf", st)], w=[("qnb", st)])
                T.op(PE, lambda st=st: nc.tensor.matmul(PS[2 + st][:, :], lhsT=RT_b, rhs=qnb[st][:], start=True, stop=True),
                     r=[("qnb", st), "cstb"], w=[("ps", 2 + st)])
                T.op(POOL, lambda st=st: nc.gpsimd.tensor_tensor(out=t1f[st][:], in0=qnb[st][:], in1=CosB[:, bs], op=ALU.mult),
                     r=[("qnb", st), "CosB"], w=[("t1f", st)])
                T.op(DVE, lambda st=st: nc.vector.tensor_tensor(out=t2f[st][:], in0=PS[2 + st][:, :], in1=SinB[:, bs], op=ALU.mult),
                     r=[("ps", 2 + st), "SinB"], w=[("t2f", st)])
                if isq:
                    dst, dk = qT[:, hh, :], ("qT", hh)
                else:
                    dst, dk = kT[:, hh, bs], ("kT", hh, blk)
                T.op(POOL, lambda st=st, dst=dst: nc.gpsimd.tensor_tensor(out=dst, in0=t1f[st][:], in1=t2f[st][:], op=ALU.add),
                     r=[("t1f", st), ("t2f", st)], w=[dk])
            for tl in range(4):
                t = blk * 4 + tl
                for kc in range(8):
                    T.op(PE, lambda kc=kc, tl=tl: nc.tensor.matmul(
                        PS[tl % 2][:, :], lhsT=xb[xbb][:, kc, tl * 128:(tl + 1) * 128], rhs=wqkv[:, kc, 1024:1536], start=(kc == 0), stop=(kc == 7)),
                        r=[("wqkv",), ("xbA", xbb)], w=[("ps", tl % 2)])
                T.op(ACT, lambda t=t, tl=tl: nc.scalar.copy(out=v1[:, t, :, :].rearrange("p h d -> p (h d)"), in_=PS[tl % 2][:, :]),
                     r=[("ps", tl % 2)], w=[("v1", t)])
            for gi in range(2):
                for kc in range(8):
                    T.op(PE, lambda kc=kc, gi=gi: nc.tensor.matmul(
                        PS[1][0:4, :], lhsT=wgt[:, kc, 4 * gi:4 * gi + 4], rhs=xb[xbb][:, kc, :], start=(kc == 0), stop=(kc == 7)),
                        r=["wgt", ("xbA", xbb)], w=[("ps", 1)])
                if gi == 0:
                    T.op(ACT, lambda: nc.scalar.activation(out=gst[0][xbb][:], in_=PS[1][0:4, :], func=AF.Identity, bias=bg[:, 0:1]),
                         r=[("ps", 1), "bg"], w=[("gst", 0, xbb)])
                    T.dma(SP, gi_d[:, bs], gst[0][xbb][:], r=[("gst", 0, xbb)], w=[("gi_d", blk)])
                else:
                    T.op(ACT, lambda: nc.scalar.activation(out=gst[1][xbb][:], in_=PS[1][0:4, :], func=AF.Exp, bias=nbf[:], scale=-1.0),
                         r=[("ps", 1), "nbf"], w=[("gst", 1, xbb)])
                    T.dma(SP, gf_d[:, bs], gst[1][xbb][:], r=[("gst", 1, xbb)], w=[("gf_d", blk)])
            for hh in range(4):
                nkt = blk * 4 + 4
                prev = None

                def pv_step(kt, c0, pbuf):
                    first = (kt == 0)
                    last = (kt == nkt - 1)
                    for m in range(2):
                        T.op(PE, lambda m=m: nc.tensor.matmul(
                            PS[4 + 2 * m][:, c0:512], lhsT=v1[:, kt, hh, :], rhs=Pb[m][pbuf][:, c0:512], start=first, stop=last,
                            skip_group_check=True), r=[("v1", kt), ("P", m, pbuf)], w=[("ps", 4 + 2 * m)])
                        T.op(PE, lambda m=m: nc.tensor.matmul(
                            PS[5 + 2 * m][:, c0:512], lhsT=ones_b, rhs=Pb[m][pbuf][:, c0:512], start=first, stop=last,
                            skip_group_check=True), r=["cstb", ("P", m, pbuf)], w=[("ps", 5 + 2 * m)])

                for kt in range(nkt):
                    ktl = kt - blk * 4
                    c0 = ktl * 128 if ktl > 0 else 0
                    pbuf = kt % 2
                    sbk = 2 * (kt % 2)
                    for m in range(2):
                        T.op(PE, lambda m=m, kt=kt, c0=c0, sbk=sbk: nc.tensor.matmul(
                            PS[sbk + m][:, c0:512], lhsT=kT[64 * m:64 * m + 64, hh, kt * 128:(kt + 1) * 128],
                            rhs=qT[64 * m:64 * m + 64, hh, c0:512], start=True, stop=True),
                            r=[("kT", hh, kt // 4), ("qT", hh)], w=[("ps", sbk + m)])
                    if prev is not None:
                        pv_step(*prev)
                    for m in range(2):
                        T.op(ACT, lambda m=m, c0=c0, pbuf=pbuf, sbk=sbk: nc.scalar.activation(
                            out=Pb[m][pbuf][:, c0:512], in_=PS[sbk + m][:, c0:512], func=AF.Exp, bias=negc, scale=0.125),
                            r=[("ps", sbk + m), "sc3"], w=[("P", m, pbuf)])
                        if ktl >= 0:
                            T.op(POOL, lambda m=m, c0=c0, pbuf=pbuf: nc.gpsimd.affine_select(
                                out=Pb[m][pbuf][:, c0:c0 + 128], in_=Pb[m][pbuf][:, c0:c0 + 128], pattern=[[1, 128]],
                                compare_op=ALU.is_ge, fill=0.0, base=0, channel_multiplier=-1),
                                r=[("P", m, pbuf)], w=[("P", m, pbuf)])
                    prev = (kt, c0, pbuf)
                pv_step(*prev)
                T.op(ACT, lambda: nc.scalar.activation(out=fo[0][:], in_=PS[5][:, :], func=AF.Ln), r=[("ps", 5)], w=[("fo", 0)])
                T.op(ACT, lambda: nc.scalar.activation(out=fo[0][:], in_=fo[0][:], func=AF.Exp, scale=-1.0), r=[("fo", 0)], w=[("fo", 0)])
                T.op(DVE, lambda: nc.vector.tensor_tensor(out=fo[1][:], in0=PS[4][:, :], in1=fo[0][:], op=ALU.mult),
                     r=[("ps", 4), ("fo", 0)], w=[("fo", 1)])
                T.op(ACT, lambda: nc.scalar.activation(out=fo[2][:], in_=PS[7][:, :], func=AF.Ln), r=[("ps", 7)], w=[("fo", 2)])
                T.op(ACT, lambda: nc.scalar.activation(out=fo[0][:], in_=fo[2][:], func=AF.Exp, scale=-1.0), r=[("fo", 2), ("fo", 0)], w=[("fo", 0)])
                T.op(DVE, lambda: nc.vector.tensor_tensor(out=fo[2][:], in0=PS[6][:, :], in1=fo[0][:], op=ALU.mult),
                     r=[("ps", 6), ("fo", 0)], w=[("fo", 2)])
                T.op(DVE, lambda: nc.vector.scalar_tensor_tensor(out=fo[3][:], in0=fo[2][:], scalar=neglam, in1=fo[1][:], op0=ALU.mult, op1=ALU.add),
                     r=[("fo", 1), ("fo", 2), "sc4"], w=[("fo", 3)])
                T.op(ACT, lambda: nc.scalar.activation(out=sqb[0][:], in_=fo[3][:], func=AF.Square), r=[("fo", 3)], w=[("sqb", 0)])
                T.op(PE, lambda: nc.tensor.matmul(PS[0][:, :], lhsT=ones_b, rhs=sqb[0][:], start=True, stop=True),
                     r=[("sqb", 0), "cstb"], w=[("ps", 0)])
                T.op(ACT, lambda: nc.scalar.activation(out=fo[0][:], in_=PS[0][:, :], func=AF.Ln, bias=epsc[:], scale=1.0 / 128),
                     r=[("ps", 0), "epscA"], w=[("fo", 0)])
                T.op(ACT, lambda: nc.scalar.activation(out=fo[0][:], in_=fo[0][:], func=AF.Exp, scale=-0.5), r=[("fo", 0)], w=[("fo", 0)])
                T.op(DVE, lambda: nc.vector.scalar_tensor_tensor(out=hdTb[:, hh, :], in0=fo[3][:], scalar=subw8, in1=fo[0][:], op0=ALU.mult, op1=ALU.mult),
                     r=[("fo", 3), ("fo", 0), "sc2"], w=[("hdTb", hh)])
            T.dma(SP, hdT_v[:, :, bs], hdTb[:], r=[("hdTb",)], w=[("hdT", blk)])
        T.barrier()
    if dbg and "hdT" in dbg_out:
        dump_dram(dbg_out["hdT"], hdT_d, 512, S, BF16)
        dump_dram(dbg_out["irow"], gi_d, 4, S, F32)
        dump_dram(dbg_out["frow"], gf_d, 4, S, F32)
    if stage <= 2:
        persist.close()
        return finish()

    scope_mark("phB")
    wcol = sbp("wcol", [128, NT, 8], F32)
    decb = sbp("decb", [128, 128], F32)
    with ExitStack() as ph:
        def sb(name, shape, dt):
            return ph.enter_context(nc.sbuf_tensor("s_" + name, list(shape), dt))
        irow = sb("irow", [4, S], F32)
        frow = sb("frow", [4, S], F32)
        T.dma(SP, irow[:], gi_d[:, :], w=[("irow",)])
        T.dma(SP, frow[:], gf_d[:, :], w=[("frow",)])
        onesr = sb("onesr", [4, S], F32)
        csr = sb("csr", [4, S], F32)
        nbr = sb("nbr", [4, S], F32)
        gr = sb("gr", [4, S], F32)
        pe_ = sb("pe_", [4, 33], F32)
        G = sb("G", [4, 32], F32)
        Ms = sb("Ms", [4, 32], F32)
        mp = sb("mp", [4, 33], F32)
        dec = sb("dec", [4, 32], F32)
        T.op(ACT, lambda: nc.scalar.activation(out=frow[:], in_=frow[:], func=AF.Ln, bias=1.0), r=[("frow",)], w=[("frow",)])
        T.op(DVE, lambda: nc.vector.memset(onesr[:], 1.0), w=["onesr"])
        T.op(DVE, lambda: nc.vector.tensor_tensor_scan(out=csr[:], data0=onesr[:], data1=frow[:], initial=0.0, op0=ALU.mult, op1=ALU.add),
             r=["onesr", ("frow",)], w=["csr"])
        T.op(DVE, lambda: nc.vector.memset(pe_[:, 0:1], 0.0), w=[("pe_", 0)])
        T.op(DVE, lambda: nc.vector.tensor_copy(out=pe_[:, 1:33], in_=csr[:].rearrange("p (j s) -> p j s", s=128)[:, :, 127]),
             r=["csr"], w=[("pe_", 1)])
        T.op(DVE, lambda: nc.vector.tensor_tensor(out=nbr[:].rearrange("p (j s) -> p j s", s=128), in0=csr[:].rearrange("p (j s) -> p j s", s=128),
                                                  in1=pe_[:, 0:32].unsqueeze(2).to_broadcast([4, 32, 128]), op=ALU.subtract),
             r=["csr", ("pe_",)], w=["nbr"])
        T.op(DVE, lambda: nc.vector.tensor_tensor(out=gr[:], in0=irow[:], in1=nbr[:], op=ALU.add), r=[("irow",), "nbr"], w=["gr"])
        T.op(DVE, lambda: nc.vector.tensor_reduce(out=G[:], in_=gr[:].rearrange("p (j s) -> p j s", s=128), axis=AX.X, op=ALU.max),
             r=["gr"], w=["G"])
        T.op(DVE, lambda: nc.vector.memset(mp[:, 0:1], 0.0), w=[("mp", 0)])
        nbl = nbr[:].rearrange("p (j s) -> p j s", s=128)[:, :, 127]
        for j in range(NT):
            T.op(DVE, lambda j=j: nc.vector.tensor_tensor(out=Ms[:, j:j + 1], in0=mp[:, j:j + 1], in1=G[:, j:j + 1], op=ALU.max),
                 r=[("mp", j), "G"], w=[("Ms", j)])
            T.op(DVE, lambda j=j: nc.vector.tensor_tensor(out=mp[:, j + 1:j + 2], in0=Ms[:, j:j + 1], in1=nbl[:, j:j + 1], op=ALU.subtract),
                 r=[("Ms", j), "nbr"], w=[("mp", j + 1)])
        Msb = Ms[:].unsqueeze(2).to_broadcast([4, 32, 128])
        T.op(DVE, lambda: nc.vector.tensor_tensor(out=gr[:].rearrange("p (j s) -> p j s", s=128), in0=gr[:].rearrange("p (j s) -> p j s", s=128),
                                                  in1=Msb, op=ALU.subtract), r=["gr", ("Ms",)], w=["gr"])
        T.op(DVE, lambda: nc.vector.tensor_tensor(out=nbr[:].rearrange("p (j s) -> p j s", s=128), in0=nbr[:].rearrange("p (j s) -> p j s", s=128),
                                                  in1=Msb, op=ALU.subtract), r=["nbr", ("Ms",)], w=["nbr"])
        T.op(ACT, lambda: nc.scalar.activation(out=gr[:], in_=gr[:], func=AF.Exp), r=["gr"], w=["gr"])
        T.op(ACT, lambda: nc.scalar.activation(out=nbr[:], in_=nbr[:], func=AF.Exp), r=["nbr"], w=["nbr"])
        T.op(DVE, lambda: nc.vector.tensor_scalar(out=gr[:], in0=gr[:], scalar1=128.0 ** -0.5, scalar2=None, op0=ALU.mult), r=["gr"], w=["gr"])
        T.op(DVE, lambda: nc.vector.tensor_tensor(out=dec[:], in0=mp[:, 0:32], in1=Ms[:], op=ALU.subtract), r=[("mp",), ("Ms",)], w=["dec"])
        T.op(ACT, lambda: nc.scalar.activation(out=dec[:], in_=dec[:], func=AF.Exp), r=["dec"], w=["dec"])
        T.dma(SP, dec_d[:, :], dec[:], r=["dec"], w=["dec_d"])
        T.dma(SP, decb[:], dec_d.rearrange("h j -> (h j)").unsqueeze(0).partition_broadcast(128), r=["dec_d"], w=["decb"])
        wv = PS[0][:, 0:256].rearrange("p (t e) -> p t e", e=8)
        for t in range(NT):
            T.op(PE, lambda t=t: nc.tensor.transpose(wv[:, t, 0:4], gr[:, t * 128:(t + 1) * 128], ident_f[0:4, 0:4]),
                 r=["gr", "cst"], w=[("ps", 0, t, 0)])
            T.op(PE, lambda t=t: nc.tensor.transpose(wv[:, t, 4:8], nbr[:, t * 128:(t + 1) * 128], ident_f[0:4, 0:4]),
                 r=["nbr", "cst"], w=[("ps", 0, t, 1)])
        T.op(DVE, lambda: nc.vector.tensor_copy(out=wcol[:], in_=wv), r=[("ps", 0)], w=["wcol"])
        T.barrier()
    if dbg and "wcol" in dbg_out:
        T.dma(SP, dbg_out["wcol"][:, :], wcol[:].rearrange("p t e -> p (t e)"), r=["wcol"])
        T.dma(SP, dbg_out["decb"][:, :], decb[:], r=["decb"])
    if stage <= 3:
        persist.close()
        return finish()

    scope_mark("phC")
    with ExitStack() as ph:
        def sb(name, shape, dt):
            return ph.enter_context(nc.sbuf_tensor("s_" + name, list(shape), dt))
        wm = sb("wm", [128, 8, 2048], BF16)
        cw = sb("cw", [128, 8, 4], F32)
        cbias = sb("cbias", [128, 8], F32)
        dg = sb("dg", [128, 8, 4, 128], BF16)
        mnwb = sb("mnwb", [128, 512], F32)
        epsc = sb("epscC", [128, 1], F32)
        xb = [sb(f"xbC{i}", [128, 8, 512], BF16) for i in range(2)]
        pre = sb("pre", [128, 8, 516], BF16)
        qkc = sb("qkc", [128, 8, 512], BF16)
        vm1 = [sb(f"vm1_{i}", [128, 4, 130], BF16) for i in range(2)]
        so = [sb(f"so{i}", [128, 512], F32) for i in range(2)]
        ST = [sb(f"ST{i}", [128, 128], BF16) for i in range(4)]
        kw = [sb(f"kw{i}", [128, 128], BF16) for i in range(4)]
        Cst = sb("Cst", [128, 4, 130], F32)
        Cd = sb("Cd", [128, 4, 130], BF16)
        hbuf = sb("hbuf", [128, 512], F32)
        sqh = sb("sqh", [128, 512], F32)
        hmb = sb("hmb", [128, 512], BF16)
        hmTb = [sb(f"hmTb{i}", [128, 4, 512], BF16) for i in range(2)]
        dn = sb("dn", [128, 8], F32)
        ssh = sb("ssh", [128, 8], F32)

        T.op(DVE, lambda: nc.vector.memset(epsc[:], EPS), w=["epscC"])
        T.dma(POOL, wm[:, 0:4, :], w_in_v[:, 0:4, 0:2048], w=[("wm", 0)], max_dma_last_dim=8192)
        T.dma(POOL, wm[:, 4:8, :], w_in_v[:, 4:8, 0:2048], w=[("wm", 1)], max_dma_last_dim=8192)
        T.dma(SP, cw[:], convw_in[:, :, :], w=["cw"])
        T.dma(SP, cbias[:], convb_in[:, :], w=["cbias"])
        T.dma(SP, mnwb[:], mnw_in[0:1, :].partition_broadcast(128), w=["mnwb"])
        for c in range(8):
            for j in range(4):
                T.op(DVE, lambda c=c, j=j: nc.vector.tensor_scalar(out=dg[:, c, j, :], in0=ident_f, scalar1=cw[:, c, j:j + 1], scalar2=None, op0=ALU.mult),
                     r=["cw", "cst"], w=[("dg", c, j)])
        T.op(DVE, lambda: nc.vector.memset(pre[:, :, 0:4], 0.0), w=[("pre", "halo")])
        T.op(DVE, lambda: nc.vector.memset(Cst[:], 0.0), w=["Cst"])
        T.op(DVE, lambda: nc.vector.memset(Cd[:], 0.0), w=["Cd"])
        for i in range(2):
            T.op(DVE, lambda i=i: nc.vector.memset(vm1[i][:, :, 128:130], 1.0), w=[("vm1", i)])

        for blk in range(NB):
            xbb = blk % 2
            bs = slice(blk * 512, (blk + 1) * 512)
            T.dma(SP, xb[xbb][:], xnT_v[:, :, bs], w=[("xbC", xbb)])
            for c in range(8):
                pb = c % 2
                for kc in range(8):
                    T.op(PE, lambda kc=kc, c=c, pb=pb: nc.tensor.matmul(
                        PS[pb][:, :], lhsT=wm[:, kc, c * 128:(c + 1) * 128], rhs=xb[xbb][:, kc, :], start=(kc == 0), stop=(kc == 7)),
                        r=[("wm",), ("xbC", xbb)], w=[("ps", pb)])
                if blk > 0:
                    T.op(DVE, lambda c=c: nc.vector.tensor_copy(out=pre[:, c, 1:4], in_=pre[:, c, 513:516]),
                         r=[("pre", c)], w=[("pre", "halo", c)])
                T.op(ACT, lambda c=c, pb=pb: nc.scalar.copy(out=pre[:, c, 4:516], in_=PS[pb][:, :]),
                     r=[("ps", pb), ("pre", "halo", c)], w=[("pre", c)])
                cb2 = 2 + c % 2
                for j in range(4):
                    T.op(PE, lambda c=c, j=j, cb2=cb2: nc.tensor.matmul(
                        PS[cb2][:, :], lhsT=dg[:, c, j, :], rhs=pre[:, c, 1 + j:513 + j], start=(j == 0), stop=(j == 3)),
                        r=[("dg", c), ("pre", c), ("pre", "halo", c)], w=[("ps", cb2)])
                T.op(ACT, lambda c=c, cb2=cb2: nc.scalar.activation(out=qkc[:, c, :], in_=PS[cb2][:, :], func=AF.Silu, bias=cbias[:, c:c + 1]),
                     r=[("ps", cb2), "cbias"], w=[("qkc", c)])
            for tl in range(4):
                t = blk * 4 + tl
                vb = t % 2
                ts_ = slice(tl * 128, (tl + 1) * 128)
                for half in range(2):
                    for kc in range(8):
                        T.op(PE, lambda kc=kc, half=half: nc.tensor.matmul(
                            PS[4 + half][:, :], lhsT=xb[xbb][:, kc, ts_], rhs=wm[:, kc, 1024 + half * 512:1536 + half * 512],
                            start=(kc == 0), stop=(kc == 7)), r=[("wm",), ("xbC", xbb)], w=[("ps", 4 + half)])
                T.op(ACT, lambda vb=vb: nc.scalar.copy(out=vm1[vb][:, :, 0:128], in_=PS[4][:, :].rearrange("p (h d) -> p h d", h=4)),
                     r=[("ps", 4)], w=[("vm1", vb)])
                T.op(ACT, lambda vb=vb: nc.scalar.activation(out=so[vb][:], in_=PS[5][:, :], func=AF.Sigmoid), r=[("ps", 5)], w=[("so", vb)])
                def head_ops(hh):
                    wc = wcol[:, t, hh:hh + 1]
                    cc = wcol[:, t, 4 + hh:5 + hh]
                    dcol = decb[:, hh * 32 + t:hh * 32 + t + 1]
                    if hh % 2 == 0:
                        bA, bK, bU, bN = 6, 1, 7, 0
                    else:
                        bA, bK, bU, bN = 2, 3, 4, 5
                    kps = psb(bK)[:, 0:128]
                    ops = []
                    ops.append(lambda: T.op(PE, lambda: nc.tensor.matmul(PS[bA][:, 0:128], lhsT=qkc[:, 4 + hh, ts_], rhs=qkc[:, hh, ts_], start=True, stop=True),
                                            r=[("qkc", 4 + hh), ("qkc", hh)], w=[("ps", bA)]))
                    ops.append(lambda: T.op(DVE, lambda: nc.vector.scalar_tensor_tensor(
                        out=ST[hh][:], in0=PS[bA][:, 0:128], scalar=wc, in1=tri_f, op0=ALU.mult, op1=ALU.mult),
                        r=[("ps", bA), "wcol", "cst"], w=[("ST", hh)]))
                    ops.append(lambda: T.op(PE, lambda: nc.tensor.transpose(kps, qkc[:, 4 + hh, ts_], ident_b),
                                            r=[("qkc", 4 + hh), "cstb"], w=[("ps", bK)]))
                    ops.append(lambda: T.op(DVE, lambda: nc.vector.tensor_scalar(out=kw[hh][:], in0=kps, scalar1=wc, scalar2=None, op0=ALU.mult),
                                            r=[("ps", bK), "wcol"], w=[("kw", hh)]))
                    ops.append(lambda: T.op(ACT, lambda: nc.scalar.activation(out=Cd[:, hh, :], in_=Cst[:, hh, :], func=AF.Identity, scale=dcol),
                                            r=[("Cst", hh), "decb"], w=[("Cd", hh)]))
                    ops.append(lambda: T.op(PE, lambda: nc.tensor.matmul(PS[bU][:, 0:129], lhsT=kw[hh][:], rhs=vm1[vb][:, hh, 0:129], start=True, stop=True),
                                            r=[("kw", hh), ("vm1", vb)], w=[("ps", bU)]))
                    ops.append(lambda: T.op(PE, lambda: nc.tensor.matmul(PS[bN][:, 0:129], lhsT=ST[hh][:], rhs=vm1[vb][:, hh, 0:129], start=True, stop=False),
                                            r=[("ST", hh), ("vm1", vb)], w=[("ps", bN)]))
                    ops.append(lambda: T.op(PE, lambda: nc.tensor.matmul(PS[bN][:, 0:129], lhsT=qkc[:, hh, ts_], rhs=Cd[:, hh, 0:129], start=False, stop=True),
                                            r=[("qkc", hh), ("Cd", hh)], w=[("ps", bN)]))
                    ops.append(lambda: T.op(DVE, lambda: nc.vector.scalar_tensor_tensor(
                        out=Cst[:, hh, 0:129], in0=Cst[:, hh, 0:129], scalar=dcol, in1=PS[bU][:, 0:129], op0=ALU.mult, op1=ALU.add),
                        r=[("Cst", hh), "decb", ("ps", bU)], w=[("Cst", hh)]))
                    ops.append(lambda: T.op(DVE, lambda: nc.vector.tensor_reduce(
                        out=dn[:, hh:hh + 1], in_=PS[bN][:, 128:129], axis=AX.X, op=ALU.max, apply_absolute_value=True),
                        r=[("ps", bN)], w=[("dn", hh)]))
                    ops.append(lambda: T.op(DVE, lambda: nc.vector.tensor_scalar(
                        out=dn[:, hh:hh + 1], in0=dn[:, hh:hh + 1], scalar1=cc, scalar2=None, op0=ALU.max),
                        r=[("dn", hh), "wcol"], w=[("dn", hh)]))
                    ops.append(lambda: T.op(DVE, lambda: nc.vector.reciprocal(out=dn[:, 4 + hh:5 + hh], in_=dn[:, hh:hh + 1]), r=[("dn", hh)], w=[("dn", 4 + hh)]))
                    ops.append(lambda: T.op(ACT, lambda: nc.scalar.activation(out=hbuf[:, hh * 128:(hh + 1) * 128], in_=PS[bN][:, 0:128], func=AF.Copy,
                                                                              scale=dn[:, 4 + hh:5 + hh]),
                                            r=[("ps", bN), ("dn", 4 + hh)], w=[("hbuf", hh)]))
                    return ops

                for pair in ((0, 1), (2, 3)):
                    pops = [head_ops(hh) for hh in pair]
                    for i in range(len(pops[0])):
                        for po in pops:
                            po[i]()
                T.op(DVE, lambda: nc.vector.tensor_tensor(out=sqh[:], in0=hbuf[:], in1=hbuf[:], op=ALU.mult), r=[("hbuf",)], w=["sqh"])
                T.op(DVE, lambda: nc.vector.tensor_reduce(out=ssh[:, 0:4], in_=sqh[:].rearrange("p (h d) -> p h d", h=4), axis=AX.X, op=ALU.add),
                     r=["sqh"], w=[("ssh", 0)])
                T.op(ACT, lambda: nc.scalar.activation(out=ssh[:, 4:8], in_=ssh[:, 0:4], func=AF.Sqrt, bias=epsc[:], scale=1.0 / 128),
                     r=[("ssh", 0), "epscC"], w=[("ssh", 1)])
                T.op(DVE, lambda: nc.vector.reciprocal(out=ssh[:, 4:8], in_=ssh[:, 4:8]), r=[("ssh", 1)], w=[("ssh", 1)])
                T.op(DVE, lambda: nc.vector.tensor_tensor(out=sqh[:].rearrange("p (h d) -> p h d", h=4), in0=hbuf[:].rearrange("p (h d) -> p h d", h=4),
                                                          in1=ssh[:, 4:8].unsqueeze(2).to_broadcast([128, 4, 128]), op=ALU.mult),
                     r=[("hbuf",), ("ssh", 1)], w=["sqh"])
                T.op(DVE, lambda: nc.vector.tensor_tensor(out=sqh[:], in0=sqh[:], in1=mnwb[:], op=ALU.mult), r=["sqh", "mnwb"], w=["sqh"])
                T.op(DVE, lambda vb=vb: nc.vector.tensor_tensor(out=hmb[:], in0=sqh[:], in1=so[vb][:], op=ALU.mult), r=["sqh", ("so", vb)], w=["hmb"])
                hb = blk % 2
                tp = psb(3).rearrange("p (k t) -> p k t", k=8)
                for hh in range(4):
                    T.op(PE, lambda hh=hh: nc.tensor.transpose(tp[:, hh, :], hmb[:, hh * 128:(hh + 1) * 128], ident_b),
                         r=["hmb", "cstb"], w=[("ps", 3)])
                T.op(ACT, lambda hb=hb: nc.scalar.copy(out=hmTb[hb][:, :, ts_], in_=tp[:, 0:4, :]), r=[("ps", 3)], w=[("hmTb", hb, tl)])
            T.dma(SP, hmT_v[:, :, bs], hmTb[blk % 2][:], r=[("hmTb", blk % 2)], w=[("hmT", blk)])
        T.barrier()
    persist.close()
    if dbg and "hmT" in dbg_out:
        dump_dram(dbg_out["hmT"], hmT_d, 512, S, BF16)
    if stage <= 4:
        return finish()

    scope_mark("phD")
    with ExitStack() as ph:
        def sb(name, shape, dt):
            return ph.enter_context(nc.sbuf_tensor("s_" + name, list(shape), dt))
        wg = sb("wg", [128, 8, 2048], BF16)
        wbm = sb("wbm", [128, 4, D], BF16)
        wbd = sb("wbd", [128, 4, D], BF16)
        wo = sb("wo", [128, 8, D], BF16)
        wstg = [sb(f"wstg{i}", [128, D], F32) for i in range(2)]
        wr = sb("wrt", [128, 8, 36], F32)
        brb = sb("brb", [128, 36], F32)
        xb = [sb(f"xbD{i}", [128, 8, 512], BF16) for i in range(2)]
        hmb_ = [sb(f"hmD{i}", [128, 4, 512], BF16) for i in range(2)]
        hdb_ = [sb(f"hdD{i}", [128, 4, 512], BF16) for i in range(2)]
        sg = [sb(f"sg{i}", [128, 512], F32) for i in range(4)]
        tt = [sb(f"tt{i}", [128, 512], F32) for i in range(4)]
        mT = sb("mT", [128, 8, 512], BF16)
        xt = [sb(f"xtD{i}", [128, D], F32) for i in range(2)]
        x1t = [sb(f"x1t{i}", [128, D], F32) for i in range(2)]
        xhf = sb("xhf", [128, D], F32)
        h2f = sb("h2f", [128, 8, 128], F32)
        x2t = [sb(f"x2t{i}", [128, D], BF16) for i in range(2)]
        ohb = sb("ohb", [128, 32], BF16)
        bufs = dict(ss=sb("ssD", [128, 1], F32), sd=sb("sdD", [128, 1], F32), rs=sb("rsD", [128, 1], F32),
                    junk=sb("junkD", [128, D], BF16), epsc=sb("epscD", [128, 1], F32))
        lg = sb("lg", [128, 36], F32)
        r8 = sb("r8", [128, 80], F32)

        T.op(DVE, lambda: nc.vector.memset(bufs["epsc"][:], EPS), w=["epsc"])
        T.op(DVE, lambda: nc.vector.memset(tot[:], 0.0), w=["tot"])
        T.dma(POOL, wg[:, 0:4, :], w_in_v[:, 0:4, 3592:5640], w=[("wg", 0)], max_dma_last_dim=8192)
        T.dma(POOL, wg[:, 4:8, :], w_in_v[:, 4:8, 3592:5640], w=[("wg", 1)], max_dma_last_dim=8192)
        T.dma(POOL, wbm[:], w_br_m.rearrange("(k p) n -> p k n", p=128), w=["wbm"])
        T.dma(POOL, wbd[:], w_br_d.rearrange("(k p) n -> p k n", p=128), w=["wbd"])
        T.dma(SP, wr[:], wr_in.rearrange("(k p) n -> p k n", p=128), w=["wrt"])
        T.dma(SP, brb[:], br_in[0:1, :].partition_broadcast(128), w=["brb"])
        w_out_v = w_out.rearrange("(k p) n -> p k n", p=128)
        for kc in range(8):
            b = kc % 2
            T.dma(SP, wstg[b][:], w_out_v[:, kc, :], w=[("wstg", b)])
            T.op(DVE, lambda kc=kc, b=b: nc.vector.tensor_tensor(out=wo[:, kc, :], in0=wstg[b][:], in1=g1b[:], op=ALU.mult),
                 r=[("wstg", b), "g1b"], w=[("wo", kc)])

        for blk in range(NB):
            xbb = blk % 2
            bs = slice(blk * 512, (blk + 1) * 512)
            def load_blk(bk):
                bb_ = bk % 2
                sl_ = slice(bk * 512, (bk + 1) * 512)
                T.dma(SP, xb[bb_][:], xnT_v[:, :, sl_], w=[("xbD", bb_)])
                T.dma(SP, hmb_[bb_][:], hmT_v[:, :, sl_], w=[("hmD", bb_)])
                T.dma(SP, hdb_[bb_][:], hdT_v[:, :, sl_], w=[("hdD", bb_)])

            if blk == 0:
                load_blk(0)
                T.dma(SP, xt[0][:], x_in[0:128, :], w=[("xtD", 0)])
            for oc in range(8):
                st = oc % 2
                for which, (colbase, wbr, hsrc, hkey, wkey) in enumerate(((0, wbm, hmb_, "hmD", "wbm"), (1024, wbd, hdb_, "hdD", "wbd"))):
                    gb = 0 + which
                    pbk = 2 + which
                    for kc in range(8):
                        T.op(PE, lambda kc=kc, colbase=colbase, gb=gb: nc.tensor.matmul(
                            PS[gb][:, :], lhsT=wg[:, kc, colbase + oc * 128:colbase + (oc + 1) * 128], rhs=xb[xbb][:, kc, :],
                            start=(kc == 0), stop=(kc == 7)), r=[("wg",), ("xbD", xbb)], w=[("ps", gb)])
                    T.op(ACT, lambda gb=gb, which=which: nc.scalar.activation(out=sg[2 * st + which][:], in_=PS[gb][:, :], func=AF.Sigmoid),
                         r=[("ps", gb)], w=[("sg", 2 * st + which)])
                    for kc in range(4):
                        T.op(PE, lambda kc=kc, wbr=wbr, hsrc=hsrc, pbk=pbk: nc.tensor.matmul(
                            PS[pbk][:, :], lhsT=wbr[:, kc, oc * 128:(oc + 1) * 128], rhs=hsrc[xbb][:, kc, :],
                            start=(kc == 0), stop=(kc == 3)), r=[wkey, (hkey, xbb)], w=[("ps", pbk)])
                    T.op(DVE, lambda which=which, pbk=pbk: nc.vector.tensor_tensor(out=tt[2 * st + which][:], in0=PS[pbk][:, :], in1=sg[2 * st + which][:], op=ALU.mult),
                         r=[("ps", pbk), ("sg", 2 * st + which)], w=[("tt", 2 * st + which)])
                T.op(POOL, lambda st=st: nc.gpsimd.tensor_tensor(out=mT[:, oc, :], in0=tt[2 * st][:], in1=tt[2 * st + 1][:], op=ALU.add),
                     r=[("tt", 2 * st), ("tt", 2 * st + 1)], w=[("mT", oc)])
            for tl in range(4):
                t = blk * 4 + tl
                b = t % 2
                ts_ = slice(tl * 128, (tl + 1) * 128)
                if t + 1 < NT:
                    T.dma(SP, xt[(t + 1) % 2][:], x_in[(t + 1) * 128:(t + 2) * 128, :], w=[("xtD", (t + 1) % 2)])
                if tl == 3 and blk + 1 < NB:
                    load_blk(blk + 1)
                for half in range(2):
                    for oc in range(8):
                        T.op(PE, lambda oc=oc, half=half: nc.tensor.matmul(
                            PS[4 + half][:, :], lhsT=mT[:, oc, ts_], rhs=wo[:, oc, half * 512:(half + 1) * 512],
                            start=(oc == 0), stop=(oc == 7)), r=[("mT", oc), ("wo",)], w=[("ps", 4 + half)])
                    T.op(DVE, lambda half=half, b=b: nc.vector.tensor_tensor(
                        out=x1t[b][:, half * 512:(half + 1) * 512], in0=PS[4 + half][:, :], in1=xt[b][:, half * 512:(half + 1) * 512], op=ALU.add),
                        r=[("ps", 4 + half), ("xtD", b)], w=[("x1t", b, half)])
                T.dma(SP, x1_d[t * 128:(t + 1) * 128, :], x1t[b][:], r=[("x1t", b)], w=[("x1d", t)])
                norm_tile(bufs, x1t[b][:], [("x1t", b), "epsc"], "n2")
                T.op(DVE, lambda b=b: nc.vector.tensor_scalar(out=xhf[:], in0=x1t[b][:], scalar1=bufs["rs"][:], scalar2=None, op0=ALU.mult),
                     r=[("x1t", b), "rs"], w=["xhf"])
                for kc in range(8):
                    T.op(PE, lambda kc=kc: nc.tensor.transpose(PS[6 + kc // 4][:, (kc % 4) * 128:(kc % 4 + 1) * 128], xhf[:, kc * 128:(kc + 1) * 128], ident_f),
                         r=["xhf", "cst"], w=[("ps", 6 + kc // 4, kc % 4)])
                for kc in range(8):
                    T.op(ACT, lambda kc=kc: nc.scalar.activation(
                        out=h2f[:, kc, :], in_=PS[6 + kc // 4][:, (kc % 4) * 128:(kc % 4 + 1) * 128], func=AF.Identity,
                        scale=a2[:, kc:kc + 1], bias=s2[:, kc:kc + 1]), r=[("ps", 6 + kc // 4, kc % 4), "a2", "modc"], w=[("h2f", kc)])
                T.op(DVE, lambda: nc.vector.tensor_tensor(out=xhf[:], in0=xhf[:], in1=a2b[:], op=ALU.mult), r=["xhf", ("a2b",)], w=["xhf"])
                T.op(POOL, lambda b=b: nc.gpsimd.tensor_tensor(out=x2t[b][:], in0=xhf[:], in1=s2b[:], op=ALU.add), r=["xhf", ("s2b",)], w=[("x2t", b)])
                T.dma(SP, X2_d[t * 128:(t + 1) * 128, :], x2t[b][:], r=[("x2t", b)], w=[("X2d", t)])
                for kc in range(8):
                    T.op(PE, lambda kc=kc: nc.tensor.matmul(PS[0][:, 0:36], lhsT=h2f[:, kc, :], rhs=wr[:, kc, :], start=(kc == 0), stop=(kc == 7)),
                         r=[("h2f", kc), "wrt"], w=[("ps", 0)])
                T.op(DVE, lambda: nc.vector.tensor_tensor(out=lg[:], in0=PS[0][:, 0:36], in1=brb[:], op=ALU.add), r=[("ps", 0), "brb"], w=["lg"])
                gl = lg[:, 0:4]
                el = lg[:, 4:36].rearrange("p (g j) -> p g j", g=4)
                R = lambda a_, b_: r8[:, a_:b_]
                T.op(DVE, lambda: nc.vector.tensor_reduce(out=R(0, 1), in_=gl, axis=AX.X, op=ALU.max), r=["lg"], w=[("r8", 0)])
                T.op(DVE, lambda: nc.vector.tensor_scalar(out=R(4, 8), in0=gl, scalar1=R(0, 1), scalar2=None, op0=ALU.is_ge), r=["lg", ("r8", 0)], w=[("r8", 1)])
                T.op(DVE, lambda: nc.vector.tensor_scalar(out=R(1, 2), in0=R(0, 1), scalar1=-1.0, scalar2=None, op0=ALU.mult), r=[("r8", 0)], w=[("r8", 2)])
                T.op(ACT, lambda: nc.scalar.activation(out=R(8, 12), in_=gl, func=AF.Exp, bias=R(1, 2), accum_out=R(2, 3)), r=["lg", ("r8", 2)], w=[("r8", 3)])
                T.op(DVE, lambda: nc.vector.reciprocal(out=R(3, 4), in_=R(2, 3)), r=[("r8", 3)], w=[("r8", 4)])
                T.op(DVE, lambda: nc.vector.tensor_tensor(out=R(16, 48).rearrange("p (g j) -> p g j", g=4), in0=el,
                                                          in1=R(4, 8).unsqueeze(2).to_broadcast([128, 4, 8]), op=ALU.mult),
                     r=["lg", ("r8", 1)], w=[("r8", 5)])
                T.op(DVE, lambda: nc.vector.tensor_reduce(out=R(48, 56), in_=R(16, 48).rearrange("p (g j) -> p j g", g=4), axis=AX.X, op=ALU.add),
                     r=[("r8", 5)], w=[("r8", 6)])
                T.op(DVE, lambda: nc.vector.max(out=R(56, 64), in_=R(48, 56)), r=[("r8", 6)], w=[("r8", 7)])
                T.op(DVE, lambda: nc.vector.tensor_scalar(out=R(64, 72), in0=R(48, 56), scalar1=R(56, 57), scalar2=None, op0=ALU.is_equal),
                     r=[("r8", 6), ("r8", 7)], w=[("r8", 9)])
                T.op(DVE, lambda: nc.vector.tensor_scalar(out=R(72, 80), in0=R(48, 56), scalar1=R(57, 58), scalar2=None, op0=ALU.is_equal),
                     r=[("r8", 6), ("r8", 7)], w=[("r8", 10)])
                T.op(DVE, lambda: nc.vector.tensor_tensor(out=R(1, 2), in0=R(57, 58), in1=R(56, 57), op=ALU.subtract), r=[("r8", 7), ("r8", 3)], w=[("r8", 2)])
                T.op(ACT, lambda: nc.scalar.activation(out=R(2, 3), in_=R(1, 2), func=AF.Exp), r=[("r8", 2), ("r8", 4)], w=[("r8", 3)])
                T.op(DVE, lambda: nc.vector.tensor_scalar(out=R(1, 2), in0=R(2, 3), scalar1=1.0, scalar2=None, op0=ALU.add), r=[("r8", 3)], w=[("r8", 2)])
                T.op(DVE, lambda: nc.vector.reciprocal(out=R(1, 2), in_=R(1, 2)), r=[("r8", 2)], w=[("r8", 2)])
                T.op(DVE, lambda t=t: nc.vector.tensor_tensor(out=w12[:, t, 0:1], in0=R(1, 2), in1=R(3, 4), op=ALU.mult), r=[("r8", 2), ("r8", 4)], w=[("w12", t, 0)])
                T.op(DVE, lambda t=t: nc.vector.tensor_tensor(out=w12[:, t, 1:2], in0=w12[:, t, 0:1], in1=R(2, 3), op=ALU.mult), r=[("w12", t, 0), ("r8", 3)], w=[("w12", t, 1)])
                for g in range(4):
                    T.op(DVE, lambda g=g, t=t: nc.vector.tensor_scalar(out=m1_all[:, t, g * 8:(g + 1) * 8], in0=R(64, 72), scalar1=R(4 + g, 5 + g), scalar2=None, op0=ALU.mult),
                         r=[("r8", 9), ("r8", 1)], w=[("m1", t, g)])
                    T.op(DVE, lambda g=g, t=t: nc.vector.tensor_scalar(out=m2_all[:, t, g * 8:(g + 1) * 8], in0=R(72, 80), scalar1=R(4 + g, 5 + g), scalar2=None, op0=ALU.mult),
                         r=[("r8", 10), ("r8", 1)], w=[("m2", t, g)])
                T.op(DVE, lambda t=t: nc.vector.tensor_tensor(out=ohb[:], in0=m1_all[:, t, :], in1=m2_all[:, t, :], op=ALU.add),
                     r=[("m1", t), ("m2", t)], w=["ohb"])
                T.op(PE, lambda: nc.tensor.matmul(PS[1][:, 0:32], lhsT=tri_b, rhs=ohb[:], start=True, stop=True), r=["ohb", "cstb"], w=[("ps", 1)])
                T.op(DVE, lambda t=t: nc.vector.tensor_tensor(out=rank_all[:, t, :], in0=PS[1][:, 0:32], in1=tot[:], op=ALU.add),
                     r=[("ps", 1), "tot"], w=[("rank", t)])
                T.op(DVE, lambda t=t: nc.vector.tensor_tensor(out=rank_all[:, t, :], in0=rank_all[:, t, :], in1=ohb[:], op=ALU.subtract),
                     r=[("rank", t), "ohb"], w=[("rank", t)])
                T.op(PE, lambda: nc.tensor.matmul(PS[1][:, 0:32], lhsT=ones_b, rhs=ohb[:], start=True, stop=True), r=["ohb", "cstb"], w=[("ps", 1)])
                T.op(DVE, lambda: nc.vector.tensor_tensor(out=tot[:], in0=PS[1][:, 0:32], in1=tot[:], op=ALU.add), r=[("ps", 1), "tot"], w=["tot"])
        T.barrier()
    if dbg and "x1" in dbg_out:
        dump_dram(dbg_out["x1"], x1_d, S, D, F32)
        T.dma(SP, dbg_out["tot"][:, :], tot[:], r=["tot"])
        T.dma(SP, dbg_out["rank"][:, :], rank_all[:].rearrange("p t e -> p (t e)"), r=[("rank",)])
        T.dma(SP, dbg_out["m1"][:, :], m1_all[:].rearrange("p t e -> p (t e)"), r=[("m1",)])
        T.dma(SP, dbg_out["m2"][:, :], m2_all[:].rearrange("p t e -> p (t e)"), r=[("m2",)])
        T.dma(SP, dbg_out["w12"][:, :], w12[:].rearrange("p t e -> p (t e)"), r=[("w12",)])
    if stage <= 5:
        return finish()

    scope_mark("phE")
    with ExitStack() as ph:
        def sb(name, shape, dt):
            return ph.enter_context(nc.sbuf_tensor(name, list(shape), dt))
        P12 = sb("P12", [128, 2, NT], I32)
        NSPL = int(os.environ.get("KSPLIT", 1))
        widx = sb("widx", [128, NSPL, NSLOT], I32)
        with ExitStack() as ph2:
            def sb2(name, shape, dt):
                return ph2.enter_context(nc.sbuf_tensor("s_" + name, list(shape), dt))
            ni = sb2("ni", [128, 32], I32)
            ntf = sb2("ntf", [128, 32], F32)
            onesr = sb2("ones32", [128, 32], F32)
            tend = sb2("tend", [128, 32], F32)
            off = sb2("off", [128, 32], F32)
            big = sb2("big", [128, NT, 32], F32)
            pf = sb2("pf", [128, 2, NT], F32)
            cmp3 = sb2("cmp3", [128, NSLOT, 32], F32)
            sidx = sb2("sidx", [128, NSLOT], F32)
            esl = sb2("esl", [128, NSLOT], F32)
            pio = sb2("pio", [128, 1], F32)
            pioi = sb2("pioi", [128, 1], I32)
            T.op(DVE, lambda: nc.vector.tensor_scalar(out=ntf[:], in0=tot[:], scalar1=127.0, scalar2=None, op0=ALU.add), r=["tot"], w=["ntf"])
            T.op(DVE, lambda: nc.vector.tensor_copy(out=ni[:], in_=ntf[:]), r=["ntf"], w=["ni"])
            T.op(DVE, lambda: nc.vector.tensor_scalar(out=ni[:], in0=ni[:], scalar1=7, scalar2=None, op0=ALU.arith_shift_right),
                 r=["ni"], w=["ni"])
            T.op(DVE, lambda: nc.vector.tensor_copy(out=ntf[:], in_=ni[:]), r=["ni"], w=["ntf"])
            T.op(DVE, lambda: nc.vector.memset(onesr[:], 1.0), w=["ones32"])
            T.op(DVE, lambda: nc.vector.tensor_tensor_scan(out=tend[:], data0=onesr[:], data1=ntf[:], initial=0.0, op0=ALU.mult, op1=ALU.add),
                 r=["ones32", "ntf"], w=["tend"])
            T.op(DVE, lambda: nc.vector.tensor_tensor(out=off[:], in0=tend[:], in1=ntf[:], op=ALU.subtract), r=["tend", "ntf"], w=["off"])
            T.op(DVE, lambda: nc.vector.tensor_scalar(out=off[:], in0=off[:], scalar1=128.0, scalar2=None, op0=ALU.mult), r=["off"], w=["off"])
            for k, mk in enumerate((m1_all, m2_all)):
                T.op(DVE, lambda: nc.vector.tensor_tensor(out=big[:], in0=rank_all[:], in1=off[:].unsqueeze(1).to_broadcast([128, NT, 32]), op=ALU.add),
                     r=[("rank",), "off"], w=["big"])
                T.op(DVE, lambda mk=mk: nc.vector.tensor_tensor(out=big[:], in0=big[:], in1=mk[:], op=ALU.mult), r=["big", ("m1",), ("m2",)], w=["big"])
                T.op(DVE, lambda k=k: nc.vector.tensor_reduce(out=pf[:, k, :], in_=big[:], axis=AX.X, op=ALU.add), r=["big"], w=[("pf", k)])
            T.op(DVE, lambda: nc.vector.tensor_scalar(out=pf[:], in0=pf[:], scalar1=0.0, scalar2=float(NSLOT * 128 - 1), op0=ALU.max, op1=ALU.min),
                 r=[("pf",)], w=[("pf",)])
            T.op(DVE, lambda: nc.vector.tensor_copy(out=P12[:], in_=pf[:]), r=[("pf",)], w=["P12"])
            T.op(POOL, lambda: nc.gpsimd.iota(out=sidx[:], pattern=[[1, NSLOT]], base=0, channel_multiplier=0, allow_small_or_imprecise_dtypes=True), w=["sidx"])
            T.op(POOL, lambda: nc.gpsimd.iota(out=pio[:], pattern=[[1, 1]], base=0, channel_multiplier=1, allow_small_or_imprecise_dtypes=True), w=["pio"])
            T.op(DVE, lambda: nc.vector.tensor_tensor(out=cmp3[:], in0=tend[:].unsqueeze(1).to_broadcast([128, NSLOT, 32]),
                                                      in1=sidx[:].unsqueeze(2).to_broadcast([128, NSLOT, 32]), op=ALU.is_le),
                 r=["tend", "sidx"], w=["cmp3"])
            T.op(DVE, lambda: nc.vector.tensor_reduce(out=esl[:], in_=cmp3[:], axis=AX.X, op=ALU.add), r=["cmp3"], w=["esl"])
            T.op(DVE, lambda: nc.vector.tensor_scalar(out=esl[:], in0=esl[:], scalar1=31.0, scalar2=128.0, op0=ALU.min, op1=ALU.mult), r=["esl"], w=["esl"])
            T.op(DVE, lambda: nc.vector.tensor_scalar(out=esl[:], in0=esl[:], scalar1=pio[:], scalar2=None, op0=ALU.add), r=["esl", "pio"], w=["esl"])
            for ci in range(NSPL):
                T.op(DVE, lambda ci=ci: nc.vector.tensor_scalar(out=sidx[:], in0=esl[:], scalar1=float(NSPL), scalar2=float(ci), op0=ALU.mult, op1=ALU.add),
                     r=["esl", "cmp3"], w=["sidx"])
                T.op(DVE, lambda ci=ci: nc.vector.tensor_copy(out=widx[:, ci, :], in_=sidx[:]), r=["sidx"], w=[("widx", ci)])
            T.barrier()
        if dbg and "P12" in dbg_out:
            T.dma(SP, dbg_out["P12"][:, :], P12[:].rearrange("p k t -> p (k t)"), r=["P12"])
            T.dma(SP, dbg_out["widx"][:, :], widx[:, 0, :], r=["widx"])

        KE = int(os.environ.get("KE", 9))
        if KE >= 2:
            x2l = [sb(f"x2l{i}", [128, D], BF16) for i in range(3)]
            for t in range(NT):
                b = t % 3
                T.dma(SP, x2l[b][:], X2_d[t * 128:(t + 1) * 128, :], w=[("x2l", b)])
                for k in range(2):
                    T.dmai(Xs_d[:, :], x2l[b][:], out_idx=P12[:, k, t:t + 1], r=[("x2l", b), "P12"], w=[("Xs", t, k)])
            T.barrier()

        if KE >= 3:
            xs = [sb(f"xs{i}", [128, D], BF16) for i in range(3)]
            wsl = [sb(f"wsl{i}", [128, 6144], BF16) for i in range(3)]
            xT = [sb(f"xTs{i}", [128, 8, 128], BF16) for i in range(2)]
            sS = [sb(f"sS{i}", [128, 256], F32) for i in range(2)]
            ac = [sb(f"ac{i}", [128, 256], BF16) for i in range(2)]
            aT = [sb(f"aTs{i}", [128, 2, 128], BF16) for i in range(2)]
            yo = [sb(f"yo{i}", [128, D], BF16) for i in range(2)]
            NSL = int(os.environ.get("KSLOTS", NSLOT))

            def load_slot(sl):
                b3 = sl % 3
                T.dma(SP, xs[b3][:], Xs_d[sl * 128:(sl + 1) * 128, :], r=[("Xs",)], w=[("xs", b3)])
                cw_ = 6144 // NSPL
                for ci in range(NSPL):
                    T.dmai(wsl[b3][:, ci * cw_:(ci + 1) * cw_], Wcat_d.rearrange("r (c w) -> (r c) w", c=NSPL), in_idx=widx[:, ci, sl:sl + 1],
                           r=[("Wcat",), "widx"], w=[("wsl", b3, ci)])

            def slot_s1(sl):
                b2, b3 = sl % 2, sl % 3
                base = 4 * b2
                xTp = psb(base).rearrange("p (k t) -> p k t", k=8)
                for kc in range(8):
                    T.op(PE, lambda kc=kc: nc.tensor.transpose(xTp[:, kc, :], xs[b3][:, kc * 128:(kc + 1) * 128], ident_b),
                         r=[("xs", b3), "cstb"], w=[("ps", base)])
                T.op(ACT, lambda: nc.scalar.copy(out=xT[b2][:, 0:4, :], in_=xTp[:, 0:4, :]), r=[("ps", base)], w=[("xTs", b2, 0)])
                T.op(DVE, lambda: nc.vector.tensor_copy(out=xT[b2][:, 4:8, :], in_=xTp[:, 4:8, :]), r=[("ps", base), ("xTs", b2, 0)], w=[("xTs", b2, 1)])
                for kc in range(8):
                    T.op(PE, lambda kc=kc: nc.tensor.matmul(PS[base + 1][:, :], lhsT=xT[b2][:, kc, :], rhs=wsl[b3][:, kc * 512:(kc + 1) * 512],
                                                            start=(kc == 0), stop=(kc == 7)), r=[("xTs", b2), ("wsl", b3)], w=[("ps", base + 1)])

            def slot_s2(sl):
                b2, b3 = sl % 2, sl % 3
                base = 4 * b2
                T.op(ACT, lambda: nc.scalar.activation(out=sS[b2][:], in_=PS[base + 1][:, 0:256], func=AF.Silu), r=[("ps", base + 1)], w=[("sS", b2)])
                T.op(DVE, lambda: nc.vector.tensor_tensor(out=ac[b2][:], in0=PS[base + 1][:, 256:512], in1=sS[b2][:], op=ALU.mult),
                     r=[("ps", base + 1), ("sS", b2)], w=[("ac", b2)])
                aTp = psb(base)[:, 0:256].rearrange("p (k t) -> p k t", k=2)
                for fc in range(2):
                    T.op(PE, lambda fc=fc: nc.tensor.transpose(aTp[:, fc, :], ac[b2][:, fc * 128:(fc + 1) * 128], ident_b),
                         r=[("ac", b2), "cstb"], w=[("ps", base)])
                T.op(ACT, lambda: nc.scalar.copy(out=aT[b2][:], in_=aTp), r=[("ps", base)], w=[("aTs", b2)])
                for cb_ in range(2):
                    for fc in range(2):
                        T.op(PE, lambda fc=fc, cb_=cb_: nc.tensor.matmul(
                            PS[base + 2 + cb_][:, :], lhsT=aT[b2][:, fc, :], rhs=wsl[b3][:, 4096 + fc * 1024 + cb_ * 512: 4096 + fc * 1024 + (cb_ + 1) * 512],
                            start=(fc == 0), stop=(fc == 1)), r=[("aTs", b2), ("wsl", b3)], w=[("ps", base + 2 + cb_)])
                T.op(ACT, lambda: nc.scalar.copy(out=yo[b2][:, 0:512], in_=PS[base + 2][:, :]), r=[("ps", base + 2)], w=[("yo", b2, 0)])
                T.op(DVE, lambda: nc.vector.tensor_copy(out=yo[b2][:, 512:1024], in_=PS[base + 3][:, :]), r=[("ps", base + 3)], w=[("yo", b2, 1)])
                T.dma(SP, Ys_d[sl * 128:(sl + 1) * 128, :], yo[b2][:], r=[("yo", b2)], w=[("Ys", sl)])

            for sl in range(min(2, NSL)):
                load_slot(sl)
            slot_s1(0)
            for sl in range(NSL):
                if sl + 2 < NSL:
                    load_slot(sl + 2)
                if sl + 1 < NSL:
                    slot_s1(sl + 1)
                slot_s2(sl)
            T.barrier()

        if KE >= 4:
            y1 = [sb(f"y1_{i}", [128, D], BF16) for i in range(3)]
            ya = [sb(f"ya_{i}", [128, D], F32) for i in range(3)]
            y2 = [sb(f"y2_{i}", [128, D], BF16) for i in range(3)]
            x1l = [sb(f"x1l{i}", [128, D], F32) for i in range(3)]
            def load_tok(t):
                b = t % 3
                T.dma(SP, x1l[b][:], x1_d[t * 128:(t + 1) * 128, :], w=[("x1l", b)])
                T.dmai(y1[b][:], Ys_d[:, :], in_idx=P12[:, 0, t:t + 1], r=[("Ys",), "P12"], w=[("y1", b)])
                T.dmai(y2[b][:], Ys_d[:, :], in_idx=P12[:, 1, t:t + 1], r=[("Ys",), "P12"], w=[("y2", b)])

            load_tok(0)
            load_tok(1)
            for t in range(NT):
                b = t % 3
                if t + 2 < NT:
                    load_tok(t + 2)
                T.op(DVE, lambda b=b, t=t: nc.vector.tensor_scalar(out=ya[b][:], in0=y1[b][:], scalar1=w12[:, t, 0:1], scalar2=None, op0=ALU.mult),
                     r=[("y1", b), ("w12",)], w=[("ya", b)])
                T.op(DVE, lambda b=b, t=t: nc.vector.scalar_tensor_tensor(out=ya[b][:], in0=y2[b][:], scalar=w12[:, t, 1:2], in1=ya[b][:], op0=ALU.mult, op1=ALU.add),
                     r=[("ya", b), ("y2", b), ("w12",)], w=[("ya", b)])
                T.op(DVE, lambda b=b: nc.vector.tensor_tensor(out=ya[b][:], in0=ya[b][:], in1=g2b[:], op=ALU.mult), r=[("ya", b), ("g2b",)], w=[("ya", b)])
                T.op(DVE, lambda b=b: nc.vector.tensor_tensor(out=x1l[b][:], in0=x1l[b][:], in1=ya[b][:], op=ALU.add), r=[("ya", b), ("x1l", b)], w=[("x1l", b)])
                T.dma(SP, out_d[t * 128:(t + 1) * 128, :], x1l[b][:], r=[("x1l", b)], w=[("out", t)])
            T.barrier()
    return finish()


def _consts():
    c = np.zeros((128, NCST), np.float32)
    c[:, 0:128] = np.eye(128, dtype=np.float32)
    s = np.arange(128)[:, None]
    t = np.arange(128)[None, :]
    c[:, 128:256] = (s <= t).astype(np.float32)
    c[:, 256:384] = ((s // 64) == (t // 64)).astype(np.float32)
    R = np.zeros((128, 128), np.float32)
    for base in (0, 64):
        for d in range(8):
            R[base + d, base + d + 8] = -1.0
            R[base + d + 8, base + d] = 1.0
    c[:, 384:512] = R.T
    c[:, 512:640] = 1.0
    inv = 500000.0 ** (-np.arange(0, 16, 2, dtype=np.float32) / 16.0)
    for p in range(128):
        d = p % 64
        c[p, 640] = inv[d % 8] if d < 16 else 0.0
    return c


_NC_CACHE = {}


def make_in_maps(inputs):
    f = lambda k: np.ascontiguousarray(np.asarray(inputs[k], dtype=np.float32))
    x = f("x")
    c = f("c")
    pos = np.ascontiguousarray(np.asarray(inputs["positions"], dtype=np.int32))
    b_ada = f("b_ada")[0]
    shared = {
        "w_ada": f("w_ada")[0],
        "b_ada_col": np.ascontiguousarray(b_ada.reshape(48, 128).T),
        "b_ada_row": np.ascontiguousarray(b_ada.reshape(1, -1)),
        "n1w": np.ascontiguousarray(f("norm1_w")[0].reshape(8, 128).T),
        "n2w": np.ascontiguousarray(f("norm2_w")[0].reshape(8, 128).T),
        "n2row": np.ascontiguousarray(f("norm2_w")[0].reshape(1, D)),
        "w_in": f("w_in")[0],
        "bgate": np.ascontiguousarray(np.stack([f("b_igate")[0], f("b_fgate")[0]], axis=1)),
        "convw": np.ascontiguousarray(f("conv_w")[0].T.reshape(8, 128, 4).transpose(1, 0, 2)),
        "convb": np.ascontiguousarray(f("conv_b")[0].reshape(8, 128).T),
        "mnw": np.ascontiguousarray(f("mlstm_norm_w")[0].reshape(1, 512)),
        "qnw": np.ascontiguousarray(np.tile(f("q_norm_w")[0], 2).reshape(128, 1)),
        "knw": np.ascontiguousarray(np.tile(f("k_norm_w")[0], 2).reshape(128, 1)),
        "qkrow": np.ascontiguousarray(np.concatenate([f("q_norm_w")[0], f("k_norm_w")[0]]).reshape(1, 128)),
        "lamv": np.ascontiguousarray(np.concatenate([f("lam_q1")[0], f("lam_k1")[0], f("lam_q2")[0], f("lam_k2")[0]]).reshape(1, 256)),
        "subw": np.ascontiguousarray(f("subln_w")[0].reshape(128, 1)),
        "w_br_m": f("w_br_m")[0],
        "w_br_d": f("w_br_d")[0],
        "w_out": f("w_out")[0],
        "wr": np.ascontiguousarray(np.concatenate([f("w_rg")[0], f("w_re")[0]], axis=1)),
        "br": np.ascontiguousarray(np.concatenate([f("b_rg")[0], f("b_re")[0]]).reshape(1, 36)),
        "w_gate": f("w_gate")[0],
        "w_up": f("w_up")[0],
        "w_down": f("w_down")[0],
        "consts": _consts(),
    }
    maps = []
    for b in range(8):
        m = dict(shared)
        m["x"] = x[b]
        m["cT"] = np.ascontiguousarray(c[b].reshape(8, 128).T)
        m["pos"] = np.ascontiguousarray(pos[b].reshape(1, S))
        maps.append(m)
    return maps


def kernel(**inputs):
    if "nc" not in _NC_CACHE:
        _NC_CACHE["nc"] = build()
    nc = _NC_CACHE["nc"]
    maps = make_in_maps(inputs)
    res = run_bass_kernel_spmd(nc, maps, core_ids=list(range(8)))
    out = np.stack([np.asarray(r["out"], dtype=np.float32) for r in res.results], axis=0)
    return out
```

```python
import math
import os
from contextlib import ExitStack

import numpy as np
import concourse.bass as bass
import concourse.mybir as mybir
from concourse.bass_utils import run_bass_kernel_spmd

F32 = mybir.dt.float32
BF16 = mybir.dt.bfloat16
I32 = mybir.dt.int32
AF = mybir.ActivationFunctionType
ALU = mybir.AluOpType
AX = mybir.AxisListType

S = 4096
D = 1024
NT = 32
NB = 8
EPS = 1e-6
NCST = 5 * 128 + 2


class _StopBuild(Exception):
    pass


class Trk:
    LIMIT = 50000
    NDSEM = 12

    def __init__(self, nc):
        self.nc = nc
        self.eng = {"pe": nc.tensor, "act": nc.scalar, "dve": nc.vector, "pool": nc.gpsimd, "sp": nc.sync}
        self.cur = {}
        self.seq = {}
        self.gen = {}
        for e in self.eng:
            self.cur[e] = [nc.alloc_semaphore(f"s_{e}_0"), 0]
            self.seq[e] = 0
            self.gen[e] = 0
        self.allsems = [(e, self.cur[e]) for e in self.eng]
        self.waited = {e: {} for e in self.eng}
        self.lastw = {}
        self.readers = {}
        self.children = {}
        self.dring = {}
        for q in ("sp", "pool", "act", "wc"):
            self.dring[q] = [[nc.alloc_semaphore(f"d_{q}_{i}"), 0] for i in range(self.NDSEM)]
        self.dpos = {q: 0 for q in self.dring}
        self.all_dma = []

    def _related(self, k):
        ks = [k[:i] for i in range(1, len(k) + 1)]
        stack = [k]
        while stack:
            p = stack.pop()
            for c in self.children.get(p, ()):
                ks.append(c)
                stack.append(c)
        return ks

    def _register(self, k):
        for i in range(1, len(k)):
            self.children.setdefault(k[:i], set()).add(k[:i + 1])

    def _wait(self, e, ev):
        sem, val, src, sq = ev
        if src == e:
            if e == "pe":
                return
            if e != "pool" and self.seq[e] - sq >= 6:
                return
        w = self.waited[e]
        if w.get(sem.name, 0) >= val:
            return
        self.eng[e].wait_ge(sem, val)
        w[sem.name] = val

    def _deps(self, e, r, w):
        evs = []
        for k in r:
            for kk in self._related(k):
                if kk in self.lastw:
                    evs.append(self.lastw[kk])
        for k in w:
            for kk in self._related(k):
                if kk in self.lastw:
                    evs.append(self.lastw[kk])
                evs.extend(self.readers.get(kk, {}).values())
        for ev in evs:
            self._wait(e, ev)

    def _record(self, ev, r, w):
        for k in r:
            self._register(k)
            self.readers.setdefault(k, {})[ev[2] if ev[2] is not None else ev[0].name] = ev
        for k in w:
            self._register(k)
            self.lastw[k] = ev
            self.readers[k] = {}
            for kk in self._related(k):
                if kk != k and len(kk) > len(k):
                    self.readers[kk] = {}
                    self.lastw.pop(kk, None)

    @staticmethod
    def _norm(ks):
        out = []
        for k in ks:
            k = tuple(k) if isinstance(k, (tuple, list)) else (k,)
            if k[0] == "ps":
                k = k[:2]
            out.append(k)
        return out

    def op(self, e, fn, r=(), w=()):
        r = self._norm(r)
        w = self._norm(w)
        w = w + [k for k in r if k[0] == "ps" and k not in w]
        self._deps(e, r, w)
        ins = fn()
        c = self.cur[e]
        c[1] += 1
        self.seq[e] += 1
        ins.then_inc(c[0], 1)
        ev = (c[0], c[1], e, self.seq[e])
        self._record(ev, r, w)
        if c[1] >= self.LIMIT:
            self.gen[e] += 1
            self.cur[e] = [self.nc.alloc_semaphore(f"s_{e}_{self.gen[e]}"), 0]
            self.allsems.append((e, self.cur[e]))
        return ins

    def dma(self, q, out, in_, r=(), w=(), ring=None, **kw):
        r = self._norm(r)
        w = self._norm(w)
        rq = ring or q
        ring = self.dring[rq]
        slot = ring[self.dpos[rq] % self.NDSEM]
        self.dpos[rq] += 1
        if slot[1] > 0:
            self._wait(q, (slot[0], slot[1], None, 0))
        self._deps(q, r, w)
        ins = self.eng[q].dma_start(out=out, in_=in_, **kw)
        slot[1] += 16
        ins.then_inc(slot[0], 16)
        ev = (slot[0], slot[1], None, 0)
        self._record(ev, r, w)
        return ins

    def dmai(self, out, in_, out_idx=None, in_idx=None, r=(), w=()):
        q = "pool"
        r = self._norm(r)
        w = self._norm(w)
        ring = self.dring[q]
        slot = ring[self.dpos[q] % self.NDSEM]
        self.dpos[q] += 1
        if slot[1] > 0:
            self._wait(q, (slot[0], slot[1], None, 0))
        self._deps(q, r, w)
        ins = self.nc.gpsimd.indirect_dma_start(
            out=out, out_offset=(bass.IndirectOffsetOnAxis(ap=out_idx, axis=0) if out_idx is not None else None),
            in_=in_, in_offset=(bass.IndirectOffsetOnAxis(ap=in_idx, axis=0) if in_idx is not None else None))
        slot[1] += 16
        ins.then_inc(slot[0], 16)
        ev = (slot[0], slot[1], None, 0)
        self._record(ev, r, w)
        return ins

    def barrier(self, full=True):
        evs = []
        for (se, c) in self.allsems:
            if c[1] > 0:
                evs.append((c[0], c[1], "__" + se, 0))
        for q, ring in self.dring.items():
            if q == "wc" and not full:
                continue
            for slot in ring:
                if slot[1] > 0:
                    evs.append((slot[0], slot[1], None, 0))
        for e in self.eng:
            for ev in evs:
                if ev[2] == "__" + e:
                    continue
                self._wait(e, ev)
        self.lastw.clear()
        self.readers.clear()
        self.children.clear()


def build(stage=99, dbg=None):
    nc = bass.Bass("TRN2", target_bir_lowering=False)
    T = Trk(nc)
    _scope = [None]

    def scope_mark(nm):
        if os.environ.get("KSCOPE") != "1":
            return
        if _scope[0] is not None:
            _scope[0].__exit__(None, None, None)
        _scope[0] = nc.named_scope(nm)
        _scope[0].__enter__()

    PE, ACT, DVE, POOL, SP = "pe", "act", "dve", "pool", "sp"

    def din(name, shape, dt=F32):
        return nc.dram_tensor(name, list(shape), dt, kind="ExternalInput").ap()

    def dscr(name, shape, dt):
        return nc.dram_tensor(name, list(shape), dt, kind="Internal").ap()

    x_in = din("x", [S, D])
    cT_in = din("cT", [128, 8])
    pos_in = din("pos", [1, S], I32)
    w_ada = din("w_ada", [D, 6 * D])
    b_ada_col = din("b_ada_col", [128, 48])
    b_ada_row = din("b_ada_row", [1, 6 * D])
    n1w_in = din("n1w", [128, 8])
    n2w_in = din("n2w", [128, 8])
    n2row_in = din("n2row", [1, D])
    w_in = din("w_in", [D, 5640])
    bgate_in = din("bgate", [4, 2])
    convw_in = din("convw", [128, 8, 4])
    convb_in = din("convb", [128, 8])
    mnw_in = din("mnw", [1, 512])
    qnw_in = din("qnw", [128, 1])
    knw_in = din("knw", [128, 1])
    qkrow_in = din("qkrow", [1, 128])
    lamv_in = din("lamv", [1, 256])
    subw_in = din("subw", [128, 1])
    w_br_m = din("w_br_m", [512, D])
    w_br_d = din("w_br_d", [512, D])
    w_out = din("w_out", [D, D])
    wr_in = din("wr", [D, 36])
    br_in = din("br", [1, 36])
    w_gate = din("w_gate", [32, D, 256])
    w_up = din("w_up", [32, D, 256])
    w_down = din("w_down", [32, 256, D])
    consts_in = din("consts", [128, NCST])
    out_d = nc.dram_tensor("out", [S, D], F32, kind="ExternalOutput").ap()

    xnT_d = dscr("xnT_d", [D, S], BF16)
    hmT_d = dscr("hmT_d", [512, S], BF16)
    hdT_d = dscr("hdT_d", [512, S], BF16)
    x1_d = dscr("x1_d", [S, D], F32)
    xn2T_d = dscr("xn2T_d", [D, S], BF16)
    combT_d = dscr("combT_d", [32, S], BF16)
    dec_d = dscr("dec_d", [4, 32], F32)
    gi_d = dscr("gi_d", [4, S], F32)
    NSLOT = 96
    X2_d = dscr("X2_d", [S, D], BF16)
    Xs_d = dscr("Xs_d", [NSLOT * 128, D], BF16)
    Ys_d = dscr("Ys_d", [NSLOT * 128, D], BF16)
    Wcat_d = dscr("Wcat_d", [32 * 128, 6144], BF16)
    gf_d = dscr("gf_d", [4, S], F32)

    dbg_out = {}
    if dbg:
        for name, (shape, dt) in dbg.items():
            dbg_out[name] = nc.dram_tensor("dbg_" + name, list(shape), dt, kind="ExternalOutput").ap()

    PS = [nc.alloc_psum_tensor(f"ps{i}", [128, 512], F32) for i in range(8)]

    def psb(i):
        return PS[i][:].bitcast(BF16)

    xnT_v = xnT_d.rearrange("(k p) t -> p k t", p=128)
    xn2T_v = xn2T_d.rearrange("(k p) t -> p k t", p=128)
    hmT_v = hmT_d.rearrange("(k p) t -> p k t", p=128)
    hdT_v = hdT_d.rearrange("(k p) t -> p k t", p=128)
    w_in_v = w_in.rearrange("(k p) n -> p k n", p=128)
    wg_v = w_gate.rearrange("e (k p) f -> e p k f", p=128)
    wu_v = w_up.rearrange("e (k p) f -> e p k f", p=128)
    wdn_v = w_down.rearrange("e (k p) n -> e p k n", p=128)

    glob = ExitStack()

    def sbg(name, shape, dt):
        return glob.enter_context(nc.sbuf_tensor("s_" + name, list(shape), dt))

    cst = sbg("cst", [128, NCST], F32)
    cstb = sbg("cstb", [128, 5 * 128], BF16)
    modc = sbg("modc", [128, 48], F32)
    a1 = sbg("a1", [128, 8], F32)
    a2 = sbg("a2", [128, 8], F32)
    g1b = sbg("g1b", [128, D], F32)
    g2b = sbg("g2b", [128, D], F32)
    a2b = sbg("a2b", [128, D], F32)
    s2b = sbg("s2b", [128, D], F32)
    rank_all = sbg("rank_all", [128, NT, 32], F32)
    m1_all = sbg("m1_all", [128, NT, 32], F32)
    m2_all = sbg("m2_all", [128, NT, 32], F32)
    w12 = sbg("w12", [128, NT, 2], F32)
    tot = sbg("tot", [128, 32], F32)
    smallc = sbg("smallc", [128, 16], F32)
    ident_f = cst[:, 0:128]
    tri_f = cst[:, 128:256]
    invf = cst[:, 640:641]
    ident_b = cstb[:, 0:128]
    tri_b = cstb[:, 128:256]
    bo64_b = cstb[:, 256:384]
    RT_b = cstb[:, 384:512]
    ones_b = cstb[:, 512:640]
    ones_f = cst[:, 512:640]

    T.dma(SP, cst[:], consts_in[:, :], w=["cst"])
    T.op(DVE, lambda: nc.vector.tensor_copy(out=cstb[:], in_=cst[:, 0:640]), r=["cst"], w=["cstb"])

    early = ExitStack()
    wst = [early.enter_context(nc.sbuf_tensor(f"s_wst{i}", [128, 6144], BF16)) for i in range(2)]
    for e in range(32):
        b = e % 2
        gu = wst[b][:, 0:4096].rearrange("p (k f) -> p k f", k=8)
        T.dma(POOL, gu[:, :, 0:256], wg_v[e], w=[("wst", b, 0)], ring="wc")
        T.dma(POOL, gu[:, :, 256:512], wu_v[e], w=[("wst", b, 1)], ring="wc")
        T.dma(POOL, wst[b][:, 4096:6144].rearrange("p (k n) -> p k n", k=2), wdn_v[e], w=[("wst", b, 2)], ring="wc")
        T.dma(POOL, Wcat_d[e * 128:(e + 1) * 128, :], wst[b][:], r=[("wst", b)], w=[("Wcat", e)], ring="wc")
    early_open = [True]
    scope_mark("ph0")
    with ExitStack() as ph:
        def sb(name, shape, dt):
            return ph.enter_context(nc.sbuf_tensor("s_" + name, list(shape), dt))
        cT = sb("cT", [128, 8], F32)
        cT2 = sb("cT2", [128, 8, 2], F32)
        cbc = sb("cbc", [128, 8, 128], F32)
        wa = [sb(f"wa{i}", [128, 8, 512], F32) for i in range(2)]
        bcol = sb("bcol", [128, 48], F32)
        brow = sb("brow", [128, 2048], F32)
        nw = sb("nw", [128, 16], F32)
        tmp8 = sb("tmp8", [128, 8], F32)
        lamb = sb("lamb", [128, 256], F32)
        qkrow = sb("qkrow", [128, 128], F32)
        lt = sb("lt", [128, 128], F32)
        l2 = sb("l2", [128, 4], F32)

        T.dma(SP, cT[:], cT_in[:, :], w=["cT"])
        T.dma(SP, bcol[:], b_ada_col[:, :], w=["bcol"])
        T.dma(SP, brow[:, 0:1024], b_ada_row[0:1, 2048:3072].partition_broadcast(128), w=["brow0"])
        T.dma(SP, brow[:, 1024:2048], b_ada_row[0:1, 5120:6144].partition_broadcast(128), w=["brow1"])
        T.dma(SP, nw[:, 0:8], n1w_in[:, :], w=["nw0"])
        T.dma(SP, nw[:, 8:16], n2w_in[:, :], w=["nw1"])
        T.dma(SP, smallc[:, 0:1], qnw_in[:, :], w=["sc0"])
        T.dma(SP, smallc[:, 1:2], knw_in[:, :], w=["sc1"])
        T.dma(SP, smallc[:, 2:3], subw_in[:, :], w=["sc2"])
        T.dma(SP, lamb[:], lamv_in[0:1, :].partition_broadcast(128), w=["lamb"])
        T.dma(SP, qkrow[:], qkrow_in[0:1, :].partition_broadcast(128), w=["qkrow"])
        for k in range(2):
            T.op(DVE, lambda k=k: nc.vector.tensor_copy(out=cT2[:, :, k], in_=cT[:]), r=["cT"], w=[("cT2", k)])
        for kc in range(8):
            T.op(DVE, lambda kc=kc: nc.vector.tensor_copy(out=cbc[:, kc, :], in_=cT[:, kc:kc + 1].to_broadcast([128, 128])),
                 r=["cT"], w=[("cbc", kc)])
        w_ada_v = w_ada.rearrange("(k p) n -> p k n", p=128)
        gate_bank = {4: 1, 5: 2, 6: 5, 7: 6, 8: 1, 9: 2, 10: 3, 11: 4}
        n2rb = sb("n2rb", [128, D], F32)
        T.dma(SP, n2rb[:], n2row_in[0:1, :].partition_broadcast(128), w=["n2rb"])
        T.dma(SP, s2b[:], b_ada_row[0:1, 3072:4096].partition_broadcast(128), w=["s2b_bias"])
        T.dma(SP, a2b[:], b_ada_row[0:1, 4096:5120].partition_broadcast(128), w=["a2b_bias"])
        for blk in range(12):
            b = blk % 2
            for hh in range(2):
                T.dma(SP, wa[b][:, 4 * hh:4 * hh + 4, :], w_ada_v[:, 4 * hh:4 * hh + 4, blk * 512:(blk + 1) * 512], w=[("wa", b)])
            for m in range(4):
                j = blk * 4 + m
                for kc in range(8):
                    T.op(PE, lambda j=j, m=m, kc=kc, b=b: nc.tensor.matmul(
                        PS[0][:, 2 * j:2 * j + 2], lhsT=wa[b][:, kc, m * 128:(m + 1) * 128], rhs=cT2[:, kc, :],
                        start=(kc == 0), stop=(kc == 7)), r=[("wa", b), "cT2"], w=[("ps", 0)])
            if blk in gate_bank:
                gb = gate_bank[blk]
                for kc in range(8):
                    T.op(PE, lambda kc=kc, b=b, gb=gb: nc.tensor.matmul(
                        PS[gb][:, :], lhsT=cbc[:, kc, :], rhs=wa[b][:, kc, :], start=(kc == 0), stop=(kc == 7)),
                        r=[("wa", b), "cbc"], w=[("ps", gb)])
                if blk == 5:
                    for hh in range(2):
                        T.op(DVE, lambda hh=hh: nc.vector.tensor_tensor(out=g1b[:, hh * 512:(hh + 1) * 512], in0=PS[1 + hh][:, :],
                                                                      in1=brow[:, hh * 512:(hh + 1) * 512], op=ALU.add),
                             r=[("ps", 1 + hh), "brow0"], w=[("g1b", hh)])
                if blk == 7:
                    for hh in range(2):
                        T.op(DVE, lambda hh=hh: nc.vector.tensor_tensor(out=s2b[:, hh * 512:(hh + 1) * 512], in0=PS[5 + hh][:, :],
                                                                      in1=s2b[:, hh * 512:(hh + 1) * 512], op=ALU.add),
                             r=[("ps", 5 + hh), "s2b_bias"], w=[("s2b", hh)])
                if blk == 9:
                    for hh in range(2):
                        sl = slice(hh * 512, (hh + 1) * 512)
                        T.op(DVE, lambda hh=hh, sl=sl: nc.vector.tensor_tensor(out=a2b[:, sl], in0=PS[1 + hh][:, :], in1=a2b[:, sl], op=ALU.add),
                             r=[("ps", 1 + hh), "a2b_bias"], w=[("a2b", hh)])
                        T.op(DVE, lambda sl=sl: nc.vector.scalar_tensor_tensor(out=a2b[:, sl], in0=a2b[:, sl], scalar=1.0, in1=n2rb[:, sl], op0=ALU.add, op1=ALU.mult),
                             r=[("a2b", hh), "n2rb"], w=[("a2b", hh)])
        T.op(DVE, lambda: nc.vector.tensor_tensor(
            out=modc[:], in0=PS[0][:, 0:96].rearrange("p (m two) -> p m two", two=2)[:, :, 0], in1=bcol[:], op=ALU.add),
            r=[("ps", 0), "bcol"], w=["modc"])
        for gi, (gt, b0) in enumerate(((g1b, 1), (g2b, 3))):
            if gi == 0:
                continue
            for hh in range(2):
                T.op(DVE, lambda gt=gt, b0=b0, hh=hh, gi=gi: nc.vector.tensor_tensor(
                    out=gt[:, hh * 512:(hh + 1) * 512], in0=PS[b0 + hh][:, :],
                    in1=brow[:, gi * 1024 + hh * 512: gi * 1024 + (hh + 1) * 512], op=ALU.add),
                    r=[("ps", b0 + hh), f"brow{gi}"], w=[(f"g{gi + 1}b", hh)])
        for (av, sc0, nwo, akey) in ((a1, 8, 0, "a1"), (a2, 32, 8, "a2")):
            T.op(DVE, lambda sc0=sc0: nc.vector.tensor_scalar(out=tmp8[:], in0=modc[:, sc0:sc0 + 8], scalar1=1.0, scalar2=None, op0=ALU.add),
                 r=["modc"], w=["tmp8"])
            T.op(DVE, lambda av=av, nwo=nwo: nc.vector.tensor_tensor(out=av[:], in0=tmp8[:], in1=nw[:, nwo:nwo + 8], op=ALU.mult),
                 r=["tmp8", "nw0", "nw1"], w=[akey])
        T.op(DVE, lambda: nc.vector.tensor_scalar(out=smallc[:, 2:3], in0=smallc[:, 2:3], scalar1=0.8, scalar2=None, op0=ALU.mult),
             r=["sc2"], w=["sc2"])
        T.op(DVE, lambda: nc.vector.tensor_reduce(out=l2[:, 0:2], in_=qkrow[:].rearrange("p (a b) -> p a b", a=2), axis=AX.X, op=ALU.max,
                                                  apply_absolute_value=True), r=["qkrow"], w=["l2a"])
        T.op(DVE, lambda: nc.vector.tensor_tensor(out=l2[:, 2:3], in0=l2[:, 0:1], in1=l2[:, 1:2], op=ALU.mult), r=["l2a"], w=["l2b"])
        T.op(DVE, lambda: nc.vector.tensor_scalar(out=smallc[:, 3:4], in0=l2[:, 2:3], scalar1=-8.0, scalar2=None, op0=ALU.mult),
             r=["l2b"], w=["sc3"])
        lv = lamb[:].rearrange("p (a b) -> p a b", a=4)
        T.op(DVE, lambda: nc.vector.tensor_tensor(out=lt[:].rearrange("p (a b) -> p a b", a=2), in0=lv[:, 0:4:2, :], in1=lv[:, 1:4:2, :], op=ALU.mult),
             r=["lamb"], w=["lt"])
        T.op(DVE, lambda: nc.vector.tensor_reduce(out=l2[:, 0:2], in_=lt[:].rearrange("p (a b) -> p a b", a=2), axis=AX.X, op=ALU.add),
             r=["lt", "l2a", "l2b"], w=["l2a"])
        T.op(ACT, lambda: nc.scalar.activation(out=l2[:, 2:4], in_=l2[:, 0:2], func=AF.Exp), r=["l2a"], w=["l2b"])
        T.op(DVE, lambda: nc.vector.tensor_tensor(out=l2[:, 0:1], in0=l2[:, 3:4], in1=l2[:, 2:3], op=ALU.subtract), r=["l2b"], w=["l2a"])
        T.op(DVE, lambda: nc.vector.tensor_scalar(out=smallc[:, 4:5], in0=l2[:, 0:1], scalar1=-0.2, scalar2=None, op0=ALU.add),
             r=["l2a"], w=["sc4"])
        T.barrier(full=False)
    if dbg and "modc" in dbg_out:
        T.dma(SP, dbg_out["modc"][:, :], modc[:], r=["modc"])
        T.dma(SP, dbg_out["g1b"][:, :], g1b[:], r=["g1b"])
        T.dma(SP, dbg_out["smallc"][:, :], smallc[:], r=["sc0"])

    def dump_dram(dst, src, rows, cols, dt):
        with nc.sbuf_tensor("s_dump_" + dst.name.replace(".", "_"), [128, cols], dt) as tmp:
            for r0 in range(0, rows, 128):
                n = min(128, rows - r0)
                T.dma(SP, tmp[0:n, :], src[r0:r0 + n, :], w=["dumptmp"])
                T.dma(SP, dst[r0:r0 + n, :], tmp[0:n, :], r=["dumptmp"], w=[("dumpdst", r0)])
            T.barrier()

    def finish():
        if _scope[0] is not None:
            _scope[0].__exit__(None, None, None)
            _scope[0] = None
        T.barrier()
        if early_open[0]:
            early.close()
            early_open[0] = False
        glob.close()
        return nc

    if stage <= 0:
        return finish()

    s1 = modc[:, 0:8]
    s2 = modc[:, 24:32]

    scope_mark("ph1")
    def norm_tile(ph_bufs, src_ap, rkeys, tag):
        ss, sd, junk = ph_bufs["ss"], ph_bufs["sd"], ph_bufs["junk"]
        T.op(ACT, lambda: nc.scalar.activation(out=junk[:], in_=src_ap, func=AF.Square, accum_out=ss[:]), r=rkeys, w=["junk", "ss"])
        T.op(ACT, lambda: nc.scalar.activation(out=sd[:], in_=ss[:], func=AF.Ln, bias=ph_bufs["epsc"][:], scale=1.0 / D), r=["ss"], w=["sd"])
        T.op(ACT, lambda: nc.scalar.activation(out=ph_bufs["rs"][:], in_=sd[:], func=AF.Exp, scale=-0.5), r=["sd"], w=["rs"])

    with ExitStack() as ph:
        def sb(name, shape, dt):
            return ph.enter_context(nc.sbuf_tensor("s_" + name, list(shape), dt))
        xt = [sb(f"xt{i}", [128, D], F32) for i in range(2)]
        xh = [sb(f"xh{i}", [128, D], BF16) for i in range(2)]
        hTb = [sb(f"hTb{i}", [128, 8, 512], BF16) for i in range(2)]
        bufs = dict(ss=sb("ss", [128, 1], F32), sd=sb("sd", [128, 1], F32), rs=sb("rs", [128, 1], F32),
                    junk=sb("junk", [128, D], BF16), epsc=sb("epsc", [128, 1], F32))
        T.op(DVE, lambda: nc.vector.memset(bufs["epsc"][:], EPS), w=["epsc"])
        pass
        for t in range(int(os.environ.get('K1_TILES', NT))):
            b = t % 2
            blk, tl = t // 4, t % 4
            hb = blk % 2
            T.dma(SP, xt[b][:], x_in[t * 128:(t + 1) * 128, :], w=[("xt", b)])
            KO = int(os.environ.get('K1_OPS', 9))
            if KO < 1:
                continue
            norm_tile(bufs, xt[b][:], [("xt", b), "epsc"], "n1")
            if KO < 2:
                continue
            T.op(DVE, lambda b=b: nc.vector.tensor_scalar(out=xh[b][:], in0=xt[b][:], scalar1=bufs["rs"][:], scalar2=None, op0=ALU.mult),
                 r=[("xt", b), "rs"], w=[("xh", b)])
            if KO < 3:
                continue
            pb = t % 2
            pv = psb(pb).rearrange("p (k t) -> p k t", k=8)
            for kc in range(8):
                T.op(PE, lambda kc=kc, b=b, pv=pv: nc.tensor.transpose(pv[:, kc, :], xh[b][:, kc * 128:(kc + 1) * 128], ident_b),
                     r=[("xh", b), "cstb"], w=[("ps", pb)])
            if KO < 4:
                continue
            for kc in range(8):
                T.op(ACT, lambda kc=kc, pv=pv, hb=hb, tl=tl: nc.scalar.activation(
                    out=hTb[hb][:, kc, tl * 128:(tl + 1) * 128], in_=pv[:, kc, :], func=AF.Identity,
                    **({} if os.environ.get('K1_NOAP') == '1' else ({'scale': a1[:, kc:kc + 1]} if os.environ.get('K1_NOAP') == '2' else
                        ({'bias': s1[:, kc:kc + 1]} if os.environ.get('K1_NOAP') == '3' else dict(scale=a1[:, kc:kc + 1], bias=s1[:, kc:kc + 1]))))),
                    r=[("ps", pb), "a1", "modc"], w=[("hTb", hb, tl)])
            if tl == 3:
                T.dma(SP, xnT_v[:, :, blk * 512:(blk + 1) * 512], hTb[hb][:], r=[("hTb", hb)], w=[("xnT", blk)])
        T.barrier()
    early.close()
    early_open[0] = False
    if dbg and "xnT" in dbg_out:
        dump_dram(dbg_out["xnT"], xnT_d, 1024, S, BF16)
    if stage <= 1:
        return finish()

    scope_mark("phA")
    def sin_reduce(ph_sb, dst_bf, ang, n, tagk):
        ki = ph_sb["ki"]
        kf = ph_sb["kf"]
        msk = ph_sb["msk"]
        C1 = 6.28125
        C2 = 2.0 * math.pi - 6.28125
        T.op(DVE, lambda: nc.vector.tensor_scalar(out=ki[:, 0:n], in0=ang, scalar1=1.0 / (2.0 * math.pi), scalar2=None, op0=ALU.mult),
             r=[tagk], w=["ki"])
        T.op(DVE, lambda: nc.vector.tensor_copy(out=kf[:, 0:n], in_=ki[:, 0:n]), r=["ki"], w=["kf"])
        T.op(DVE, lambda: nc.vector.scalar_tensor_tensor(out=ang, in0=kf[:, 0:n], scalar=-C1, in1=ang, op0=ALU.mult, op1=ALU.add),
             r=["kf", tagk], w=[tagk])
        T.op(DVE, lambda: nc.vector.scalar_tensor_tensor(out=ang, in0=kf[:, 0:n], scalar=-C2, in1=ang, op0=ALU.mult, op1=ALU.add),
             r=["kf", tagk], w=[tagk])
        T.op(DVE, lambda: nc.vector.tensor_scalar(out=msk[:, 0:n], in0=ang, scalar1=math.pi, scalar2=-2.0 * math.pi, op0=ALU.is_gt, op1=ALU.mult),
             r=[tagk], w=["msk"])
        T.op(DVE, lambda: nc.vector.tensor_tensor(out=ang, in0=ang, in1=msk[:, 0:n], op=ALU.add), r=[tagk, "msk"], w=[tagk])
        T.op(DVE, lambda: nc.vector.tensor_scalar(out=msk[:, 0:n], in0=ang, scalar1=-math.pi, scalar2=2.0 * math.pi, op0=ALU.is_lt, op1=ALU.mult),
             r=[tagk], w=["msk"])
        T.op(DVE, lambda: nc.vector.tensor_tensor(out=ang, in0=ang, in1=msk[:, 0:n], op=ALU.add), r=[tagk, "msk"], w=[tagk])
        T.op(DVE, lambda: nc.vector.tensor_scalar(out=ang, in0=ang, scalar1=math.pi, scalar2=-math.pi, op0=ALU.min, op1=ALU.max),
             r=[tagk], w=[tagk])
        T.op(ACT, lambda: nc.scalar.activation(out=dst_bf, in_=ang, func=AF.Sin), r=[tagk], w=[tagk + "_o"])

    persist = ExitStack()

    def sbp(name, shape, dt):
        return persist.enter_context(nc.sbuf_tensor("s_" + name, list(shape), dt))

    with ExitStack() as ph:
        def sb(name, shape, dt):
            return ph.enter_context(nc.sbuf_tensor("s_" + name, list(shape), dt))
        CosB = sb("CosB", [128, S], BF16)
        SinB = sb("SinB", [128, S], BF16)
        with ExitStack() as ph2:
            posi = ph2.enter_context(nc.sbuf_tensor("s_posi", [128, 1024], I32))
            posf = ph2.enter_context(nc.sbuf_tensor("s_posf", [128, 1024], F32))
            ang = ph2.enter_context(nc.sbuf_tensor("s_ang", [128, 1024], F32))
            tb = dict(ki=ph2.enter_context(nc.sbuf_tensor("s_ki", [128, 1024], I32)),
                      kf=ph2.enter_context(nc.sbuf_tensor("s_kf", [128, 1024], F32)),
                      msk=ph2.enter_context(nc.sbuf_tensor("s_msk", [128, 1024], F32)))
            for c4 in range(4):
                cs = slice(c4 * 1024, (c4 + 1) * 1024)
                T.dma(SP, posi[:], pos_in[0:1, cs].partition_broadcast(128), w=["posi"])
                T.op(DVE, lambda: nc.vector.tensor_copy(out=posf[:], in_=posi[:]), r=["posi"], w=["posf"])
                T.op(DVE, lambda: nc.vector.tensor_scalar(out=ang[:], in0=posf[:], scalar1=invf, scalar2=None, op0=ALU.mult),
                     r=["posf", "cst"], w=["ang"])
                sin_reduce(tb, SinB[:, cs], ang[:], 1024, "ang")
                T.op(DVE, lambda: nc.vector.tensor_scalar(out=ang[:], in0=posf[:], scalar1=invf, scalar2=math.pi / 2, op0=ALU.mult, op1=ALU.add),
                     r=["posf", "cst", "ang_o"], w=["ang"])
                sin_reduce(tb, CosB[:, cs], ang[:], 1024, "ang")
            T.barrier()

        wqkv = sb("wqkv", [128, 8, 1536], BF16)
        wgt = sb("wgt", [128, 8, 8], BF16)
        bg = sb("bg", [4, 2], F32)
        nbf = sb("nbf", [4, 1], F32)
        epsc = sb("epscA", [128, 1], F32)
        gst = [[sb(f"gst{g}_{i}", [4, 512], F32) for i in range(2)] for g in range(2)]
        kT = sb("kT", [128, 4, S], BF16)
        v1 = sb("v1", [128, NT, 4, 128], BF16)
        xb = [sb(f"xbA{i}", [128, 8, 512], BF16) for i in range(2)]
        qT = sb("qT", [128, 4, 512], BF16)
        hdTb = sb("hdTb", [128, 4, 512], BF16)
        Pb = [[sb(f"P{m}_{i}", [128, 512], BF16) for i in range(2)] for m in range(2)]
        sqb = [sb(f"sqb{i}", [128, 512], BF16) for i in range(2)]
        qsf = [sb(f"qsf{i}", [128, 512], F32) for i in range(2)]
        sdf = [sb(f"sdf{i}", [128, 512], F32) for i in range(2)]
        qnb = [sb(f"qnb{i}", [128, 512], BF16) for i in range(2)]
        t1f = [sb(f"t1f{i}", [128, 512], F32) for i in range(2)]
        t2f = [sb(f"t2f{i}", [128, 512], F32) for i in range(2)]
        fo = [sb(f"fo{i}", [128, 512], F32) for i in range(4)]

        T.op(DVE, lambda: nc.vector.memset(epsc[:], EPS), w=["epscA"])
        T.dma(POOL, wqkv[:, 0:4, :], w_in_v[:, 0:4, 2056:3592], w=[("wqkv", 0)])
        T.dma(POOL, wqkv[:, 4:8, :], w_in_v[:, 4:8, 2056:3592], w=[("wqkv", 1)])
        T.dma(POOL, wgt[:], w_in_v[:, :, 2048:2056], w=["wgt"])
        T.dma(SP, bg[:], bgate_in[:, :], w=["bg"])
        T.op(DVE, lambda: nc.vector.tensor_scalar(out=nbf[:], in0=bg[:, 1:2], scalar1=-1.0, scalar2=None, op0=ALU.mult), r=["bg"], w=["nbf"])

        negc = smallc[:, 3:4]
        neglam = smallc[:, 4:5]
        subw8 = smallc[:, 2:3]

        for blk in range(NB):
            xbb = blk % 2
            bs = slice(blk * 512, (blk + 1) * 512)
            T.dma(SP, xb[xbb][:], xnT_v[:, :, bs], w=[("xbA", xbb)])
            for c in range(8):
                st = c % 2
                isq = c < 4
                hh = c % 4
                col0 = (0 if isq else 512) + hh * 128
                for kc in range(8):
                    T.op(PE, lambda kc=kc, col0=col0, st=st: nc.tensor.matmul(
                        PS[st][:, :], lhsT=wqkv[:, kc, col0:col0 + 128], rhs=xb[xbb][:, kc, :], start=(kc == 0), stop=(kc == 7)),
                        r=[("wqkv",), ("xbA", xbb)], w=[("ps", st)])
                wcol = smallc[:, 0:1] if isq else smallc[:, 1:2]
                T.op(ACT, lambda st=st: nc.scalar.activation(out=sqb[st][:], in_=PS[st][:, :], func=AF.Square), r=[("ps", st)], w=[("sqb", st)])
                T.op(ACT, lambda st=st, wcol=wcol: nc.scalar.activation(out=qsf[st][:], in_=PS[st][:, :], func=AF.Identity, scale=wcol),
                     r=[("ps", st), "sc0", "sc1"], w=[("qsf", st)])
                T.op(PE, lambda st=st: nc.tensor.matmul(PS[2 + st][:, :], lhsT=bo64_b, rhs=sqb[st][:], start=True, stop=True),
                     r=[("sqb", st), "cstb"], w=[("ps", 2 + st)])
                T.op(ACT, lambda st=st: nc.scalar.activation(out=sdf[st][:], in_=PS[2 + st][:, :], func=AF.Ln, bias=epsc[:], scale=1.0 / 64),
                     r=[("ps", 2 + st), "epscA"], w=[("sdf", st)])
                T.op(ACT, lambda st=st: nc.scalar.activation(out=sdf[st][:], in_=sdf[st][:], func=AF.Exp, scale=-0.5),
                     r=[("sdf", st)], w=[("sdf", st)])
                T.op(DVE, lambda st=st: nc.vector.tensor_tensor(out=qnb[st][:], in0=qsf[st][:], in1=sdf[st][:], op=ALU.mult),
                     r=[("qsf", st), ("sdf", st)], w=[("qnb", st)])
                T.op(PE, lambda st=st: nc.tensor.matmul(PS[2 + st][:, :], lhsT=RT_b, rhs=qnb[st][:], start=True, stop=True),
                     r=[("qnb", st), "cstb"], w=[("ps", 2 + st)])
                T.op(POOL, lambda st=st: nc.gpsimd.tensor_tensor(out=t1f[st][:], in0=qnb[st][:], in1=CosB[:, bs], op=ALU.mult),
                     r=[("qnb", st), "CosB"], w=[("t1f", st)])
                T.op(DVE, lambda st=st: nc.vector.tensor_tensor(out=t2f[st][:], in0=PS[2 + st][:, :], in1=SinB[:, bs], op=ALU.mult),
                     r=[("ps", 2 + st), "SinB"], w=[("t2f", st)])
                if isq:
                    dst, dk = qT[:, hh, :], ("qT", hh)
                else:
                    dst, dk = kT[:, hh, bs], ("kT", hh, blk)
                T.op(POOL, lambda st=st, dst=dst: nc.gpsimd.tensor_tensor(out=dst, in0=t1f[st][:], in1=t2f[st][:], op=ALU.add),
                     r=[("t1f", st), ("t2f", st)], w=[dk])
            for tl in range(4):
                t = blk * 4 + tl
                for kc in range(8):
                    T.op(PE, lambda kc=kc, tl=tl: nc.tensor.matmul(
                        PS[tl % 2][:, :], lhsT=xb[xbb][:, kc, tl * 128:(tl + 1) * 128], rhs=wqkv[:, kc, 1024:1536], start=(kc == 0), stop=(kc == 7)),
                        r=[("wqkv",), ("xbA", xbb)], w=[("ps", tl % 2)])
                T.op(ACT, lambda t=t, tl=tl: nc.scalar.copy(out=v1[:, t, :, :].rearrange("p h d -> p (h d)"), in_=PS[tl % 2][:, :]),
                     r=[("ps", tl % 2)], w=[("v1", t)])
            for gi in range(2):
                for kc in range(8):
                    T.op(PE, lambda kc=kc, gi=gi: nc.tensor.matmul(
                        PS[1][0:4, :], lhsT=wgt[:, kc, 4 * gi:4 * gi + 4], rhs=xb[xbb][:, kc, :], start=(kc == 0), stop=(kc == 7)),
                        r=["wgt", ("xbA", xbb)], w=[("ps", 1)])
                if gi == 0:
                    T.op(ACT, lambda: nc.scalar.activation(out=gst[0][xbb][:], in_=PS[1][0:4, :], func=AF.Identity, bias=bg[:, 0:1]),
                         r=[("ps", 1), "bg"], w=[("gst", 0, xbb)])
                    T.dma(SP, gi_d[:, bs], gst[0][xbb][:], r=[("gst", 0, xbb)], w=[("gi_d", blk)])
                else:
                    T.op(ACT, lambda: nc.scalar.activation(out=gst[1][xbb][:], in_=PS[1][0:4, :], func=AF.Exp, bias=nbf[:], scale=-1.0),
                         r=[("ps", 1), "nbf"], w=[("gst", 1, xbb)])
                    T.dma(SP, gf_d[:, bs], gst[1][xbb][:], r=[("gst", 1, xbb)], w=[("gf_d", blk)])
            for hh in range(4):
                nkt = blk * 4 + 4
                prev = None

                def pv_step(kt, c0, pbuf):
                    first = (kt == 0)
                    last = (kt == nkt - 1)
                    for m in range(2):
                        T.op(PE, lambda m=m: nc.tensor.matmul(
                            PS[4 + 2 * m][:, c0:512], lhsT=v1[:, kt, hh, :], rhs=Pb[m][pbuf][:, c0:512], start=first, stop=last,
                            skip_group_check=True), r=[("v1", kt), ("P", m, pbuf)], w=[("ps", 4 + 2 * m)])
                        T.op(PE, lambda m=m: nc.tensor.matmul(
                            PS[5 + 2 * m][:, c0:512], lhsT=ones_b, rhs=Pb[m][pbuf][:, c0:512], start=first, stop=last,
                            skip_group_check=True), r=["cstb", ("P", m, pbuf)], w=[("ps", 5 + 2 * m)])

                for kt in range(nkt):
                    ktl = kt - blk * 4
                    c0 = ktl * 128 if ktl > 0 else 0
                    pbuf = kt % 2
                    sbk = 2 * (kt % 2)
                    for m in range(2):
                        T.op(PE, lambda m=m, kt=kt, c0=c0, sbk=sbk: nc.tensor.matmul(
                            PS[sbk + m][:, c0:512], lhsT=kT[64 * m:64 * m + 64, hh, kt * 128:(kt + 1) * 128],
                            rhs=qT[64 * m:64 * m + 64, hh, c0:512], start=True, stop=True),
                            r=[("kT", hh, kt // 4), ("qT", hh)], w=[("ps", sbk + m)])
                    if prev is not None:
                        pv_step(*prev)
                    for m in range(2):
                        T.op(ACT, lambda m=m, c0=c0, pbuf=pbuf, sbk=sbk: nc.scalar.activation(
                            out=Pb[m][pbuf][:, c0:512], in_=PS[sbk + m][:, c0:512], func=AF.Exp, bias=negc, scale=0.125),
                            r=[("ps", sbk + m), "sc3"], w=[("P", m, pbuf)])
                        if ktl >= 0:
                            T.op(POOL, lambda m=m, c0=c0, pbuf=pbuf: nc.gpsimd.affine_select(
                                out=Pb[m][pbuf][:, c0:c0 + 128], in_=Pb[m][pbuf][:, c0:c0 + 128], pattern=[[1, 128]],
                                compare_op=ALU.is_ge, fill=0.0, base=0, channel_multiplier=-1),
                                r=[("P", m, pbuf)], w=[("P", m, pbuf)])
                    prev = (kt, c0, pbuf)
                pv_step(*prev)
                T.op(ACT, lambda: nc.scalar.activation(out=fo[0][:], in_=PS[5][:, :], func=AF.Ln), r=[("ps", 5)], w=[("fo", 0)])
                T.op(ACT, lambda: nc.scalar.activation(out=fo[0][:], in_=fo[0][:], func=AF.Exp, scale=-1.0), r=[("fo", 0)], w=[("fo", 0)])
                T.op(DVE, lambda: nc.vector.tensor_tensor(out=fo[1][:], in0=PS[4][:, :], in1=fo[0][:], op=ALU.mult),
                     r=[("ps", 4), ("fo", 0)], w=[("fo", 1)])
                T.op(ACT, lambda: nc.scalar.activation(out=fo[2][:], in_=PS[7][:, :], func=AF.Ln), r=[("ps", 7)], w=[("fo", 2)])
                T.op(ACT, lambda: nc.scalar.activation(out=fo[0][:], in_=fo[2][:], func=AF.Exp, scale=-1.0), r=[("fo", 2), ("fo", 0)], w=[("fo", 0)])
                T.op(DVE, lambda: nc.vector.tensor_tensor(out=fo[2][:], in0=PS[6][:, :], in1=fo[0][:], op=ALU.mult),
                     r=[("ps", 6), ("fo", 0)], w=[("fo", 2)])
                T.op(DVE, lambda: nc.vector.scalar_tensor_tensor(out=fo[3][:], in0=fo[2][:], scalar=neglam, in1=fo[1][:], op0=ALU.mult, op1=ALU.add),
                     r=[("fo", 1), ("fo", 2), "sc4"], w=[("fo", 3)])
                T.op(ACT, lambda: nc.scalar.activation(out=sqb[0][:], in_=fo[3][:], func=AF.Square), r=[("fo", 3)], w=[("sqb", 0)])
                T.op(PE, lambda: nc.tensor.matmul(PS[0][:, :], lhsT=ones_b, rhs=sqb[0][:], start=True, stop=True),
                     r=[("sqb", 0), "cstb"], w=[("ps", 0)])
                T.op(ACT, lambda: nc.scalar.activation(out=fo[0][:], in_=PS[0][:, :], func=AF.Ln, bias=epsc[:], scale=1.0 / 128),
                     r=[("ps", 0), "epscA"], w=[("fo", 0)])
                T.op(ACT, lambda: nc.scalar.activation(out=fo[0][:], in_=fo[0][:], func=AF.Exp, scale=-0.5), r=[("fo", 0)], w=[("fo", 0)])
                T.op(DVE, lambda: nc.vector.scalar_tensor_tensor(out=hdTb[:, hh, :], in0=fo[3][:], scalar=subw8, in1=fo[0][:], op0=ALU.mult, op1=ALU.mult),
                     r=[("fo", 3), ("fo", 0), "sc2"], w=[("hdTb", hh)])
            T.dma(SP, hdT_v[:, :, bs], hdTb[:], r=[("hdTb",)], w=[("hdT", blk)])
        T.barrier()
    if dbg and "hdT" in dbg_out:
        dump_dram(dbg_out["hdT"], hdT_d, 512, S, BF16)
        dump_dram(dbg_out["irow"], gi_d, 4, S, F32)
        dump_dram(dbg_out["frow"], gf_d, 4, S, F32)
    if stage <= 2:
        persist.close()
        return finish()

    scope_mark("phB")
    wcol = sbp("wcol", [128, NT, 8], F32)
    decb = sbp("decb", [128, 128], F32)
    with ExitStack() as ph:
        def sb(name, shape, dt):
            return ph.enter_context(nc.sbuf_tensor("s_" + name, list(shape), dt))
        irow = sb("irow", [4, S], F32)
        frow = sb("frow", [4, S], F32)
        T.dma(SP, irow[:], gi_d[:, :], w=[("irow",)])
        T.dma(SP, frow[:], gf_d[:, :], w=[("frow",)])
        onesr = sb("onesr", [4, S], F32)
        csr = sb("csr", [4, S], F32)
        nbr = sb("nbr", [4, S], F32)
        gr = sb("gr", [4, S], F32)
        pe_ = sb("pe_", [4, 33], F32)
        G = sb("G", [4, 32], F32)
        Ms = sb("Ms", [4, 32], F32)
        mp = sb("mp", [4, 33], F32)
        dec = sb("dec", [4, 32], F32)
        T.op(ACT, lambda: nc.scalar.activation(out=frow[:], in_=frow[:], func=AF.Ln, bias=1.0), r=[("frow",)], w=[("frow",)])
        T.op(DVE, lambda: nc.vector.memset(onesr[:], 1.0), w=["onesr"])
        T.op(DVE, lambda: nc.vector.tensor_tensor_scan(out=csr[:], data0=onesr[:], data1=frow[:], initial=0.0, op0=ALU.mult, op1=ALU.add),
             r=["onesr", ("frow",)], w=["csr"])
        T.op(DVE, lambda: nc.vector.memset(pe_[:, 0:1], 0.0), w=[("pe_", 0)])
        T.op(DVE, lambda: nc.vector.tensor_copy(out=pe_[:, 1:33], in_=csr[:].rearrange("p (j s) -> p j s", s=128)[:, :, 127]),
             r=["csr"], w=[("pe_", 1)])
        T.op(DVE, lambda: nc.vector.tensor_tensor(out=nbr[:].rearrange("p (j s) -> p j s", s=128), in0=csr[:].rearrange("p (j s) -> p j s", s=128),
                                                  in1=pe_[:, 0:32].unsqueeze(2).to_broadcast([4, 32, 128]), op=ALU.subtract),
             r=["csr", ("pe_",)], w=["nbr"])
        T.op(DVE, lambda: nc.vector.tensor_tensor(out=gr[:], in0=irow[:], in1=nbr[:], op=ALU.add), r=[("irow",), "nbr"], w=["gr"])
        T.op(DVE, lambda: nc.vector.tensor_reduce(out=G[:], in_=gr[:].rearrange("p (j s) -> p j s", s=128), axis=AX.X, op=ALU.max),
             r=["gr"], w=["G"])
        T.op(DVE, lambda: nc.vector.memset(mp[:, 0:1], 0.0), w=[("mp", 0)])
        nbl = nbr[:].rearrange("p (j s) -> p j s", s=128)[:, :, 127]
        for j in range(NT):
            T.op(DVE, lambda j=j: nc.vector.tensor_tensor(out=Ms[:, j:j + 1], in0=mp[:, j:j + 1], in1=G[:, j:j + 1], op=ALU.max),
                 r=[("mp", j), "G"], w=[("Ms", j)])
            T.op(DVE, lambda j=j: nc.vector.tensor_tensor(out=mp[:, j + 1:j + 2], in0=Ms[:, j:j + 1], in1=nbl[:, j:j + 1], op=ALU.subtract),
                 r=[("Ms", j), "nbr"], w=[("mp", j + 1)])
        Msb = Ms[:].unsqueeze(2).to_broadcast([4, 32, 128])
        T.op(DVE, lambda: nc.vector.tensor_tensor(out=gr[:].rearrange("p (j s) -> p j s", s=128), in0=gr[:].rearrange("p (j s) -> p j s", s=128),
                                                  in1=Msb, op=ALU.subtract), r=["gr", ("Ms",)], w=["gr"])
        T.op(DVE, lambda: nc.vector.tensor_tensor(out=nbr[:].rearrange("p (j s) -> p j s", s=128), in0=nbr[:].rearrange("p (j s) -> p j s", s=128),
                                                  in1=Msb, op=ALU.subtract), r=["nbr", ("Ms",)], w=["nbr"])
        T.op(ACT, lambda: nc.scalar.activation(out=gr[:], in_=gr[:], func=AF.Exp), r=["gr"], w=["gr"])
        T.op(ACT, lambda: nc.scalar.activation(out=nbr[:], in_=nbr[:], func=AF.Exp), r=["nbr"], w=["nbr"])
        T.op(DVE, lambda: nc.vector.tensor_scalar(out=gr[:], in0=gr[:], scalar1=128.0 ** -0.5, scalar2=None, op0=ALU.mult), r=["gr"], w=["gr"])
        T.op(DVE, lambda: nc.vector.tensor_tensor(out=dec[:], in0=mp[:, 0:32], in1=Ms[:], op=ALU.subtract), r=[("mp",), ("Ms",)], w=["dec"])
        T.op(ACT, lambda: nc.scalar.activation(out=dec[:], in_=dec[:], func=AF.Exp), r=["dec"], w=["dec"])
        T.dma(SP, dec_d[:, :], dec[:], r=["dec"], w=["dec_d"])
        T.dma(SP, decb[:], dec_d.rearrange("h j -> (h j)").unsqueeze(0).partition_broadcast(128), r=["dec_d"], w=["decb"])
        wv = PS[0][:, 0:256].rearrange("p (t e) -> p t e", e=8)
        for t in range(NT):
            T.op(PE, lambda t=t: nc.tensor.transpose(wv[:, t, 0:4], gr[:, t * 128:(t + 1) * 128], ident_f[0:4, 0:4]),
                 r=["gr", "cst"], w=[("ps", 0, t, 0)])
            T.op(PE, lambda t=t: nc.tensor.transpose(wv[:, t, 4:8], nbr[:, t * 128:(t + 1) * 128], ident_f[0:4, 0:4]),
                 r=["nbr", "cst"], w=[("ps", 0, t, 1)])
        T.op(DVE, lambda: nc.vector.tensor_copy(out=wcol[:], in_=wv), r=[("ps", 0)], w=["wcol"])
        T.barrier()
    if dbg and "wcol" in dbg_out:
        T.dma(SP, dbg_out["wcol"][:, :], wcol[:].rearrange("p t e -> p (t e)"), r=["wcol"])
        T.dma(SP, dbg_out["decb"][:, :], decb[:], r=["decb"])
    if stage <= 3:
        persist.close()
        return finish()

    scope_mark("phC")
    with ExitStack() as ph:
        def sb(name, shape, dt):
            return ph.enter_context(nc.sbuf_tensor("s_" + name, list(shape), dt))
        wm = sb("wm", [128, 8, 2048], BF16)
        cw = sb("cw", [128, 8, 4], F32)
        cbias = sb("cbias", [128, 8], F32)
        dg = sb("dg", [128, 8, 4, 128], BF16)
        mnwb = sb("mnwb", [128, 512], F32)
        epsc = sb("epscC", [128, 1], F32)
        xb = [sb(f"xbC{i}", [128, 8, 512], BF16) for i in range(2)]
        pre = sb("pre", [128, 8, 516], BF16)
        qkc = sb("qkc", [128, 8, 512], BF16)
        vm1 = [sb(f"vm1_{i}", [128, 4, 130], BF16) for i in range(2)]
        so = [sb(f"so{i}", [128, 512], F32) for i in range(2)]
        ST = [sb(f"ST{i}", [128, 128], BF16) for i in range(4)]
        kw = [sb(f"kw{i}", [128, 128], BF16) for i in range(4)]
        Cst = sb("Cst", [128, 4, 130], F32)
        Cd = sb("Cd", [128, 4, 130], BF16)
        hbuf = sb("hbuf", [128, 512], F32)
        sqh = sb("sqh", [128, 512], F32)
        hmb = sb("hmb", [128, 512], BF16)
        hmTb = [sb(f"hmTb{i}", [128, 4, 512], BF16) for i in range(2)]
        dn = sb("dn", [128, 8], F32)
        ssh = sb("ssh", [128, 8], F32)

        T.op(DVE, lambda: nc.vector.memset(epsc[:], EPS), w=["epscC"])
        T.dma(POOL, wm[:, 0:4, :], w_in_v[:, 0:4, 0:2048], w=[("wm", 0)], max_dma_last_dim=8192)
        T.dma(POOL, wm[:, 4:8, :], w_in_v[:, 4:8, 0:2048], w=[("wm", 1)], max_dma_last_dim=8192)
        T.dma(SP, cw[:], convw_in[:, :, :], w=["cw"])
        T.dma(SP, cbias[:], convb_in[:, :], w=["cbias"])
        T.dma(SP, mnwb[:], mnw_in[0:1, :].partition_broadcast(128), w=["mnwb"])
        for c in range(8):
            for j in range(4):
                T.op(DVE, lambda c=c, j=j: nc.vector.tensor_scalar(out=dg[:, c, j, :], in0=ident_f, scalar1=cw[:, c, j:j + 1], scalar2=None, op0=ALU.mult),
                     r=["cw", "cst"], w=[("dg", c, j)])
        T.op(DVE, lambda: nc.vector.memset(pre[:, :, 0:4], 0.0), w=[("pre", "halo")])
        T.op(DVE, lambda: nc.vector.memset(Cst[:], 0.0), w=["Cst"])
        T.op(DVE, lambda: nc.vector.memset(Cd[:], 0.0), w=["Cd"])
        for i in range(2):
            T.op(DVE, lambda i=i: nc.vector.memset(vm1[i][:, :, 128:130], 1.0), w=[("vm1", i)])

        for blk in range(NB):
            xbb = blk % 2
            bs = slice(blk * 512, (blk + 1) * 512)
            T.dma(SP, xb[xbb][:], xnT_v[:, :, bs], w=[("xbC", xbb)])
            for c in range(8):
                pb = c % 2
                for kc in range(8):
                    T.op(PE, lambda kc=kc, c=c, pb=pb: nc.tensor.matmul(
                        PS[pb][:, :], lhsT=wm[:, kc, c * 128:(c + 1) * 128], rhs=xb[xbb][:, kc, :], start=(kc == 0), stop=(kc == 7)),
                        r=[("wm",), ("xbC", xbb)], w=[("ps", pb)])
                if blk > 0:
                    T.op(DVE, lambda c=c: nc.vector.tensor_copy(out=pre[:, c, 1:4], in_=pre[:, c, 513:516]),
                         r=[("pre", c)], w=[("pre", "halo", c)])
                T.op(ACT, lambda c=c, pb=pb: nc.scalar.copy(out=pre[:, c, 4:516], in_=PS[pb][:, :]),
                     r=[("ps", pb), ("pre", "halo", c)], w=[("pre", c)])
                cb2 = 2 + c % 2
                for j in range(4):
                    T.op(PE, lambda c=c, j=j, cb2=cb2: nc.tensor.matmul(
                        PS[cb2][:, :], lhsT=dg[:, c, j, :], rhs=pre[:, c, 1 + j:513 + j], start=(j == 0), stop=(j == 3)),
                        r=[("dg", c), ("pre", c), ("pre", "halo", c)], w=[("ps", cb2)])
                T.op(ACT, lambda c=c, cb2=cb2: nc.scalar.activation(out=qkc[:, c, :], in_=PS[cb2][:, :], func=AF.Silu, bias=cbias[:, c:c + 1]),
                     r=[("ps", cb2), "cbias"], w=[("qkc", c)])
            for tl in range(4):
                t = blk * 4 + tl
                vb = t % 2
                ts_ = slice(tl * 128, (tl + 1) * 128)
                def emit_vo(tl_):
                    t_ = blk * 4 + tl_
                    vb_ = t_ % 2
                    sl_ = slice(tl_ * 128, (tl_ + 1) * 128)
                    for half in range(2):
                        for kc in range(8):
                            T.op(PE, lambda kc=kc, half=half: nc.tensor.matmul(
                                PS[4 + half][:, :], lhsT=xb[xbb][:, kc, sl_], rhs=wm[:, kc, 1024 + half * 512:1536 + half * 512],
                                start=(kc == 0), stop=(kc == 7)), r=[("wm",), ("xbC", xbb)], w=[("ps", 4 + half)])
                    T.op(ACT, lambda: nc.scalar.copy(out=vm1[vb_][:, :, 0:128], in_=PS[4][:, :].rearrange("p (h d) -> p h d", h=4)),
                         r=[("ps", 4)], w=[("vm1", vb_)])
                    T.op(ACT, lambda: nc.scalar.activation(out=so[vb_][:], in_=PS[5][:, :], func=AF.Sigmoid), r=[("ps", 5)], w=[("so", vb_)])

                if tl == 0:
                    emit_vo(0)
                if tl < 3:
                    emit_vo(tl + 1)
                def head_ops(hh):
                    wc = wcol[:, t, hh:hh + 1]
                    cc = wcol[:, t, 4 + hh:5 + hh]
                    dcol = decb[:, hh * 32 + t:hh * 32 + t + 1]
                    if hh % 2 == 0:
                        bA, bK, bU, bN = 6, 1, 7, 0
                    else:
                        bA, bK, bU, bN = 2, 3, 4, 5
                    kps = psb(bK)[:, 0:128]
                    ops = []
                    ops.append(lambda: T.op(PE, lambda: nc.tensor.matmul(PS[bA][:, 0:128], lhsT=qkc[:, 4 + hh, ts_], rhs=qkc[:, hh, ts_], start=True, stop=True),
                                            r=[("qkc", 4 + hh), ("qkc", hh)], w=[("ps", bA)]))
                    ops.append(lambda: T.op(DVE, lambda: nc.vector.scalar_tensor_tensor(
                        out=ST[hh][:], in0=PS[bA][:, 0:128], scalar=wc, in1=tri_f, op0=ALU.mult, op1=ALU.mult),
                        r=[("ps", bA), "wcol", "cst"], w=[("ST", hh)]))
                    ops.append(lambda: T.op(PE, lambda: nc.tensor.transpose(kps, qkc[:, 4 + hh, ts_], ident_b),
                                            r=[("qkc", 4 + hh), "cstb"], w=[("ps", bK)]))
                    ops.append(lambda: T.op(DVE, lambda: nc.vector.tensor_scalar(out=kw[hh][:], in0=kps, scalar1=wc, scalar2=None, op0=ALU.mult),
                                            r=[("ps", bK), "wcol"], w=[("kw", hh)]))
                    ops.append(lambda: T.op(ACT, lambda: nc.scalar.activation(out=Cd[:, hh, :], in_=Cst[:, hh, :], func=AF.Identity, scale=dcol),
                                            r=[("Cst", hh), "decb"], w=[("Cd", hh)]))
                    ops.append(lambda: T.op(PE, lambda: nc.tensor.matmul(PS[bU][:, 0:129], lhsT=kw[hh][:], rhs=vm1[vb][:, hh, 0:129], start=True, stop=True),
                                            r=[("kw", hh), ("vm1", vb)], w=[("ps", bU)]))
                    ops.append(lambda: T.op(PE, lambda: nc.tensor.matmul(PS[bN][:, 0:129], lhsT=ST[hh][:], rhs=vm1[vb][:, hh, 0:129], start=True, stop=False),
                                            r=[("ST", hh), ("vm1", vb)], w=[("ps", bN)]))
                    ops.append(lambda: T.op(PE, lambda: nc.tensor.matmul(PS[bN][:, 0:129], lhsT=qkc[:, hh, ts_], rhs=Cd[:, hh, 0:129], start=False, stop=True),
                                            r=[("qkc", hh), ("Cd", hh)], w=[("ps", bN)]))
                    ops.append(lambda: T.op(DVE, lambda: nc.vector.scalar_tensor_tensor(
                        out=Cst[:, hh, 0:129], in0=Cst[:, hh, 0:129], scalar=dcol, in1=PS[bU][:, 0:129], op0=ALU.mult, op1=ALU.add),
                        r=[("Cst", hh), "decb", ("ps", bU)], w=[("Cst", hh)]))
                    ops.append(lambda: T.op(DVE, lambda: nc.vector.tensor_reduce(
                        out=dn[:, hh:hh + 1], in_=PS[bN][:, 128:129], axis=AX.X, op=ALU.max, apply_absolute_value=True),
                        r=[("ps", bN)], w=[("dn", hh)]))
                    ops.append(lambda: T.op(DVE, lambda: nc.vector.tensor_scalar(
                        out=dn[:, hh:hh + 1], in0=dn[:, hh:hh + 1], scalar1=cc, scalar2=None, op0=ALU.max),
                        r=[("dn", hh), "wcol"], w=[("dn", hh)]))
                    ops.append(lambda: T.op(DVE, lambda: nc.vector.reciprocal(out=dn[:, 4 + hh:5 + hh], in_=dn[:, hh:hh + 1]), r=[("dn", hh)], w=[("dn", 4 + hh)]))
                    ops.append(lambda: T.op(ACT, lambda: nc.scalar.activation(out=hbuf[:, hh * 128:(hh + 1) * 128], in_=PS[bN][:, 0:128], func=AF.Copy,
                                                                              scale=dn[:, 4 + hh:5 + hh]),
                                            r=[("ps", bN), ("dn", 4 + hh)], w=[("hbuf", hh)]))
                    return ops

                for pair in ((0, 1), (2, 3)):
                    pops = [head_ops(hh) for hh in pair]
                    for i in range(len(pops[0])):
                        for po in pops:
                            po[i]()
                T.op(DVE, lambda: nc.vector.tensor_tensor(out=sqh[:], in0=hbuf[:], in1=hbuf[:], op=ALU.mult), r=[("hbuf",)], w=["sqh"])
                T.op(DVE, lambda: nc.vector.tensor_reduce(out=ssh[:, 0:4], in_=sqh[:].rearrange("p (h d) -> p h d", h=4), axis=AX.X, op=ALU.add),
                     r=["sqh"], w=[("ssh", 0)])
                T.op(ACT, lambda: nc.scalar.activation(out=ssh[:, 4:8], in_=ssh[:, 0:4], func=AF.Sqrt, bias=epsc[:], scale=1.0 / 128),
                     r=[("ssh", 0), "epscC"], w=[("ssh", 1)])
                T.op(DVE, lambda: nc.vector.reciprocal(out=ssh[:, 4:8], in_=ssh[:, 4:8]), r=[("ssh", 1)], w=[("ssh", 1)])
                T.op(DVE, lambda: nc.vector.tensor_tensor(out=sqh[:].rearrange("p (h d) -> p h d", h=4), in0=hbuf[:].rearrange("p (h d) -> p h d", h=4),
                                                          in1=ssh[:, 4:8].unsqueeze(2).to_broadcast([128, 4, 128]), op=ALU.mult),
                     r=[("hbuf",), ("ssh", 1)], w=["sqh"])
                T.op(DVE, lambda: nc.vector.tensor_tensor(out=sqh[:], in0=sqh[:], in1=mnwb[:], op=ALU.mult), r=["sqh", "mnwb"], w=["sqh"])
                T.op(DVE, lambda vb=vb: nc.vector.tensor_tensor(out=hmb[:], in0=sqh[:], in1=so[vb][:], op=ALU.mult), r=["sqh", ("so", vb)], w=["hmb"])
                hb = blk % 2
                tp = psb(3).rearrange("p (k t) -> p k t", k=8)
                for hh in range(4):
                    T.op(PE, lambda hh=hh: nc.tensor.transpose(tp[:, hh, :], hmb[:, hh * 128:(hh + 1) * 128], ident_b),
                         r=["hmb", "cstb"], w=[("ps", 3)])
                T.op(ACT, lambda hb=hb: nc.scalar.copy(out=hmTb[hb][:, :, ts_], in_=tp[:, 0:4, :]), r=[("ps", 3)], w=[("hmTb", hb, tl)])
            T.dma(SP, hmT_v[:, :, bs], hmTb[blk % 2][:], r=[("hmTb", blk % 2)], w=[("hmT", blk)])
        T.barrier()
    persist.close()
    if dbg and "hmT" in dbg_out:
        dump_dram(dbg_out["hmT"], hmT_d, 512, S, BF16)
    if stage <= 4:
        return finish()

    scope_mark("phD")
    with ExitStack() as ph:
        def sb(name, shape, dt):
            return ph.enter_context(nc.sbuf_tensor("s_" + name, list(shape), dt))
        wg = sb("wg", [128, 8, 2048], BF16)
        wbm = sb("wbm", [128, 4, D], BF16)
        wbd = sb("wbd", [128, 4, D], BF16)
        wo = sb("wo", [128, 8, D], BF16)
        wstg = [sb(f"wstg{i}", [128, D], F32) for i in range(2)]
        wr = sb("wrt", [128, 8, 36], F32)
        brb = sb("brb", [128, 36], F32)
        xb = [sb(f"xbD{i}", [128, 8, 512], BF16) for i in range(2)]
        hmb_ = [sb(f"hmD{i}", [128, 4, 512], BF16) for i in range(2)]
        hdb_ = [sb(f"hdD{i}", [128, 4, 512], BF16) for i in range(2)]
        sg = [sb(f"sg{i}", [128, 512], F32) for i in range(4)]
        tt = [sb(f"tt{i}", [128, 512], F32) for i in range(4)]
        mT = sb("mT", [128, 8, 512], BF16)
        xt = [sb(f"xtD{i}", [128, D], F32) for i in range(2)]
        x1t = [sb(f"x1t{i}", [128, D], F32) for i in range(2)]
        xhf = sb("xhf", [128, D], F32)
        h2f = sb("h2f", [128, 8, 128], F32)
        x2t = [sb(f"x2t{i}", [128, D], BF16) for i in range(2)]
        ohb = sb("ohb", [128, 32], BF16)
        bufs = dict(ss=sb("ssD", [128, 1], F32), sd=sb("sdD", [128, 1], F32), rs=sb("rsD", [128, 1], F32),
                    junk=sb("junkD", [128, D], BF16), epsc=sb("epscD", [128, 1], F32))
        lg = sb("lg", [128, 36], F32)
        r8 = sb("r8", [128, 80], F32)

        T.op(DVE, lambda: nc.vector.memset(bufs["epsc"][:], EPS), w=["epsc"])
        T.op(DVE, lambda: nc.vector.memset(tot[:], 0.0), w=["tot"])
        T.dma(POOL, wg[:, 0:4, :], w_in_v[:, 0:4, 3592:5640], w=[("wg", 0)], max_dma_last_dim=8192)
        T.dma(POOL, wg[:, 4:8, :], w_in_v[:, 4:8, 3592:5640], w=[("wg", 1)], max_dma_last_dim=8192)
        T.dma(POOL, wbm[:], w_br_m.rearrange("(k p) n -> p k n", p=128), w=["wbm"])
        T.dma(POOL, wbd[:], w_br_d.rearrange("(k p) n -> p k n", p=128), w=["wbd"])
        T.dma(SP, wr[:], wr_in.rearrange("(k p) n -> p k n", p=128), w=["wrt"])
        T.dma(SP, brb[:], br_in[0:1, :].partition_broadcast(128), w=["brb"])
        w_out_v = w_out.rearrange("(k p) n -> p k n", p=128)
        for kc in range(8):
            b = kc % 2
            T.dma(SP, wstg[b][:], w_out_v[:, kc, :], w=[("wstg", b)])
            T.op(DVE, lambda kc=kc, b=b: nc.vector.tensor_tensor(out=wo[:, kc, :], in0=wstg[b][:], in1=g1b[:], op=ALU.mult),
                 r=[("wstg", b), "g1b"], w=[("wo", kc)])

        for blk in range(NB):
            xbb = blk % 2
            bs = slice(blk * 512, (blk + 1) * 512)
            def load_blk(bk):
                bb_ = bk % 2
                sl_ = slice(bk * 512, (bk + 1) * 512)
                T.dma(SP, xb[bb_][:], xnT_v[:, :, sl_], w=[("xbD", bb_)])
                T.dma(SP, hmb_[bb_][:], hmT_v[:, :, sl_], w=[("hmD", bb_)])
                T.dma(SP, hdb_[bb_][:], hdT_v[:, :, sl_], w=[("hdD", bb_)])

            if blk == 0:
                load_blk(0)
                T.dma(SP, xt[0][:], x_in[0:128, :], w=[("xtD", 0)])
            for oc in range(8):
                st = oc % 2
                for which, (colbase, wbr, hsrc, hkey, wkey) in enumerate(((0, wbm, hmb_, "hmD", "wbm"), (1024, wbd, hdb_, "hdD", "wbd"))):
                    gb = 0 + which
                    pbk = 2 + which
                    for kc in range(8):
                        T.op(PE, lambda kc=kc, colbase=colbase, gb=gb: nc.tensor.matmul(
                            PS[gb][:, :], lhsT=wg[:, kc, colbase + oc * 128:colbase + (oc + 1) * 128], rhs=xb[xbb][:, kc, :],
                            start=(kc == 0), stop=(kc == 7)), r=[("wg",), ("xbD", xbb)], w=[("ps", gb)])
                    T.op(ACT, lambda gb=gb, which=which: nc.scalar.activation(out=sg[2 * st + which][:], in_=PS[gb][:, :], func=AF.Sigmoid),
                         r=[("ps", gb)], w=[("sg", 2 * st + which)])
                    for kc in range(4):
                        T.op(PE, lambda kc=kc, wbr=wbr, hsrc=hsrc, pbk=pbk: nc.tensor.matmul(
                            PS[pbk][:, :], lhsT=wbr[:, kc, oc * 128:(oc + 1) * 128], rhs=hsrc[xbb][:, kc, :],
                            start=(kc == 0), stop=(kc == 3)), r=[wkey, (hkey, xbb)], w=[("ps", pbk)])
                    T.op(DVE, lambda which=which, pbk=pbk: nc.vector.tensor_tensor(out=tt[2 * st + which][:], in0=PS[pbk][:, :], in1=sg[2 * st + which][:], op=ALU.mult),
                         r=[("ps", pbk), ("sg", 2 * st + which)], w=[("tt", 2 * st + which)])
                T.op(POOL, lambda st=st: nc.gpsimd.tensor_tensor(out=mT[:, oc, :], in0=tt[2 * st][:], in1=tt[2 * st + 1][:], op=ALU.add),
                     r=[("tt", 2 * st), ("tt", 2 * st + 1)], w=[("mT", oc)])
            for tl in range(4):
                t = blk * 4 + tl
                b = t % 2
                ts_ = slice(tl * 128, (tl + 1) * 128)
                if t + 1 < NT:
                    T.dma(SP, xt[(t + 1) % 2][:], x_in[(t + 1) * 128:(t + 2) * 128, :], w=[("xtD", (t + 1) % 2)])
                if tl == 3 and blk + 1 < NB:
                    load_blk(blk + 1)
                def emit_wout(tl_):
                    sl_ = slice(tl_ * 128, (tl_ + 1) * 128)
                    for half in range(2):
                        for oc in range(8):
                            T.op(PE, lambda oc=oc, half=half: nc.tensor.matmul(
                                PS[4 + half][:, :], lhsT=mT[:, oc, sl_], rhs=wo[:, oc, half * 512:(half + 1) * 512],
                                start=(oc == 0), stop=(oc == 7)), r=[("mT", oc), ("wo",)], w=[("ps", 4 + half)])

                if tl == 0:
                    emit_wout(0)
                for half in range(2):
                    T.op(DVE, lambda half=half, b=b: nc.vector.tensor_tensor(
                        out=x1t[b][:, half * 512:(half + 1) * 512], in0=PS[4 + half][:, :], in1=xt[b][:, half * 512:(half + 1) * 512], op=ALU.add),
                        r=[("ps", 4 + half), ("xtD", b)], w=[("x1t", b, half)])
                T.dma(SP, x1_d[t * 128:(t + 1) * 128, :], x1t[b][:], r=[("x1t", b)], w=[("x1d", t)])
                if tl < 3:
                    emit_wout(tl + 1)
                norm_tile(bufs, x1t[b][:], [("x1t", b), "epsc"], "n2")
                T.op(DVE, lambda b=b: nc.vector.tensor_scalar(out=xhf[:], in0=x1t[b][:], scalar1=bufs["rs"][:], scalar2=None, op0=ALU.mult),
                     r=[("x1t", b), "rs"], w=["xhf"])
                for kc in range(8):
                    T.op(PE, lambda kc=kc: nc.tensor.transpose(PS[6 + kc // 4][:, (kc % 4) * 128:(kc % 4 + 1) * 128], xhf[:, kc * 128:(kc + 1) * 128], ident_f),
                         r=["xhf", "cst"], w=[("ps", 6 + kc // 4, kc % 4)])
                for kc in range(8):
                    T.op(ACT, lambda kc=kc: nc.scalar.activation(
                        out=h2f[:, kc, :], in_=PS[6 + kc // 4][:, (kc % 4) * 128:(kc % 4 + 1) * 128], func=AF.Identity,
                        scale=a2[:, kc:kc + 1], bias=s2[:, kc:kc + 1]), r=[("ps", 6 + kc // 4, kc % 4), "a2", "modc"], w=[("h2f", kc)])
                T.op(DVE, lambda: nc.vector.tensor_tensor(out=xhf[:], in0=xhf[:], in1=a2b[:], op=ALU.mult), r=["xhf", ("a2b",)], w=["xhf"])
                T.op(POOL, lambda b=b: nc.gpsimd.tensor_tensor(out=x2t[b][:], in0=xhf[:], in1=s2b[:], op=ALU.add), r=["xhf", ("s2b",)], w=[("x2t", b)])
                T.dma(SP, X2_d[t * 128:(t + 1) * 128, :], x2t[b][:], r=[("x2t", b)], w=[("X2d", t)])
                for kc in range(8):
                    T.op(PE, lambda kc=kc: nc.tensor.matmul(PS[0][:, 0:36], lhsT=h2f[:, kc, :], rhs=wr[:, kc, :], start=(kc == 0), stop=(kc == 7)),
                         r=[("h2f", kc), "wrt"], w=[("ps", 0)])
                T.op(DVE, lambda: nc.vector.tensor_tensor(out=lg[:], in0=PS[0][:, 0:36], in1=brb[:], op=ALU.add), r=[("ps", 0), "brb"], w=["lg"])
                gl = lg[:, 0:4]
                el = lg[:, 4:36].rearrange("p (g j) -> p g j", g=4)
                R = lambda a_, b_: r8[:, a_:b_]
                T.op(DVE, lambda: nc.vector.tensor_reduce(out=R(0, 1), in_=gl, axis=AX.X, op=ALU.max), r=["lg"], w=[("r8", 0)])
                T.op(DVE, lambda: nc.vector.tensor_scalar(out=R(4, 8), in0=gl, scalar1=R(0, 1), scalar2=None, op0=ALU.is_ge), r=["lg", ("r8", 0)], w=[("r8", 1)])
                T.op(DVE, lambda: nc.vector.tensor_scalar(out=R(1, 2), in0=R(0, 1), scalar1=-1.0, scalar2=None, op0=ALU.mult), r=[("r8", 0)], w=[("r8", 2)])
                T.op(ACT, lambda: nc.scalar.activation(out=R(8, 12), in_=gl, func=AF.Exp, bias=R(1, 2), accum_out=R(2, 3)), r=["lg", ("r8", 2)], w=[("r8", 3)])
                T.op(DVE, lambda: nc.vector.reciprocal(out=R(3, 4), in_=R(2, 3)), r=[("r8", 3)], w=[("r8", 4)])
                T.op(DVE, lambda: nc.vector.tensor_tensor(out=R(16, 48).rearrange("p (g j) -> p g j", g=4), in0=el,
                                                          in1=R(4, 8).unsqueeze(2).to_broadcast([128, 4, 8]), op=ALU.mult),
                     r=["lg", ("r8", 1)], w=[("r8", 5)])
                T.op(DVE, lambda: nc.vector.tensor_reduce(out=R(48, 56), in_=R(16, 48).rearrange("p (g j) -> p j g", g=4), axis=AX.X, op=ALU.add),
                     r=[("r8", 5)], w=[("r8", 6)])
                T.op(DVE, lambda: nc.vector.max(out=R(56, 64), in_=R(48, 56)), r=[("r8", 6)], w=[("r8", 7)])
                T.op(DVE, lambda: nc.vector.tensor_scalar(out=R(64, 72), in0=R(48, 56), scalar1=R(56, 57), scalar2=None, op0=ALU.is_equal),
                     r=[("r8", 6), ("r8", 7)], w=[("r8", 9)])
                T.op(DVE, lambda: nc.vector.tensor_scalar(out=R(72, 80), in0=R(48, 56), scalar1=R(57, 58), scalar2=None, op0=ALU.is_equal),
                     r=[("r8", 6), ("r8", 7)], w=[("r8", 10)])
                T.op(DVE, lambda: nc.vector.tensor_tensor(out=R(1, 2), in0=R(57, 58), in1=R(56, 57), op=ALU.subtract), r=[("r8", 7), ("r8", 3)], w=[("r8", 2)])
                T.op(ACT, lambda: nc.scalar.activation(out=R(2, 3), in_=R(1, 2), func=AF.Exp), r=[("r8", 2), ("r8", 4)], w=[("r8", 3)])
                T.op(DVE, lambda: nc.vector.tensor_scalar(out=R(1, 2), in0=R(2, 3), scalar1=1.0, scalar2=None, op0=ALU.add), r=[("r8", 3)], w=[("r8", 2)])
                T.op(DVE, lambda: nc.vector.reciprocal(out=R(1, 2), in_=R(1, 2)), r=[("r8", 2)], w=[("r8", 2)])
                T.op(DVE, lambda t=t: nc.vector.tensor_tensor(out=w12[:, t, 0:1], in0=R(1, 2), in1=R(3, 4), op=ALU.mult), r=[("r8", 2), ("r8", 4)], w=[("w12", t, 0)])
                T.op(DVE, lambda t=t: nc.vector.tensor_tensor(out=w12[:, t, 1:2], in0=w12[:, t, 0:1], in1=R(2, 3), op=ALU.mult), r=[("w12", t, 0), ("r8", 3)], w=[("w12", t, 1)])
                for g in range(4):
                    T.op(DVE, lambda g=g, t=t: nc.vector.tensor_scalar(out=m1_all[:, t, g * 8:(g + 1) * 8], in0=R(64, 72), scalar1=R(4 + g, 5 + g), scalar2=None, op0=ALU.mult),
                         r=[("r8", 9), ("r8", 1)], w=[("m1", t, g)])
                    T.op(DVE, lambda g=g, t=t: nc.vector.tensor_scalar(out=m2_all[:, t, g * 8:(g + 1) * 8], in0=R(72, 80), scalar1=R(4 + g, 5 + g), scalar2=None, op0=ALU.mult),
                         r=[("r8", 10), ("r8", 1)], w=[("m2", t, g)])
                T.op(DVE, lambda t=t: nc.vector.tensor_tensor(out=ohb[:], in0=m1_all[:, t, :], in1=m2_all[:, t, :], op=ALU.add),
                     r=[("m1", t), ("m2", t)], w=["ohb"])
                T.op(PE, lambda: nc.tensor.matmul(PS[1][:, 0:32], lhsT=tri_b, rhs=ohb[:], start=True, stop=True), r=["ohb", "cstb"], w=[("ps", 1)])
                T.op(DVE, lambda t=t: nc.vector.tensor_tensor(out=rank_all[:, t, :], in0=PS[1][:, 0:32], in1=tot[:], op=ALU.add),
                     r=[("ps", 1), "tot"], w=[("rank", t)])
                T.op(DVE, lambda t=t: nc.vector.tensor_tensor(out=rank_all[:, t, :], in0=rank_all[:, t, :], in1=ohb[:], op=ALU.subtract),
                     r=[("rank", t), "ohb"], w=[("rank", t)])
                T.op(PE, lambda: nc.tensor.matmul(PS[1][:, 0:32], lhsT=ones_b, rhs=ohb[:], start=True, stop=True), r=["ohb", "cstb"], w=[("ps", 1)])
                T.op(DVE, lambda: nc.vector.tensor_tensor(out=tot[:], in0=PS[1][:, 0:32], in1=tot[:], op=ALU.add), r=[("ps", 1), "tot"], w=["tot"])
        T.barrier()
    if dbg and "x1" in dbg_out:
        dump_dram(dbg_out["x1"], x1_d, S, D, F32)
        T.dma(SP, dbg_out["tot"][:, :], tot[:], r=["tot"])
        T.dma(SP, dbg_out["rank"][:, :], rank_all[:].rearrange("p t e -> p (t e)"), r=[("rank",)])
        T.dma(SP, dbg_out["m1"][:, :], m1_all[:].rearrange("p t e -> p (t e)"), r=[("m1",)])
        T.dma(SP, dbg_out["m2"][:, :], m2_all[:].rearrange("p t e -> p (t e)"), r=[("m2",)])
        T.dma(SP, dbg_out["w12"][:, :], w12[:].rearrange("p t e -> p (t e)"), r=[("w12",)])
    if stage <= 5:
        return finish()

    scope_mark("phE")
    with ExitStack() as ph:
        def sb(name, shape, dt):
            return ph.enter_context(nc.sbuf_tensor(name, list(shape), dt))
        P12 = sb("P12", [128, 2, NT], I32)
        NSPL = int(os.environ.get("KSPLIT", 1))
        widx = sb("widx", [128, NSPL, NSLOT], I32)
        with ExitStack() as ph2:
            def sb2(name, shape, dt):
                return ph2.enter_context(nc.sbuf_tensor("s_" + name, list(shape), dt))
            ni = sb2("ni", [128, 32], I32)
            ntf = sb2("ntf", [128, 32], F32)
            onesr = sb2("ones32", [128, 32], F32)
            tend = sb2("tend", [128, 32], F32)
            off = sb2("off", [128, 32], F32)
            big = sb2("big", [128, NT, 32], F32)
            pf = sb2("pf", [128, 2, NT], F32)
            cmp3 = sb2("cmp3", [128, NSLOT, 32], F32)
            sidx = sb2("sidx", [128, NSLOT], F32)
            esl = sb2("esl", [128, NSLOT], F32)
            pio = sb2("pio", [128, 1], F32)
            pioi = sb2("pioi", [128, 1], I32)
            T.op(DVE, lambda: nc.vector.tensor_scalar(out=ntf[:], in0=tot[:], scalar1=127.0, scalar2=None, op0=ALU.add), r=["tot"], w=["ntf"])
            T.op(DVE, lambda: nc.vector.tensor_copy(out=ni[:], in_=ntf[:]), r=["ntf"], w=["ni"])
            T.op(DVE, lambda: nc.vector.tensor_scalar(out=ni[:], in0=ni[:], scalar1=7, scalar2=None, op0=ALU.arith_shift_right),
                 r=["ni"], w=["ni"])
            T.op(DVE, lambda: nc.vector.tensor_copy(out=ntf[:], in_=ni[:]), r=["ni"], w=["ntf"])
            T.op(DVE, lambda: nc.vector.memset(onesr[:], 1.0), w=["ones32"])
            T.op(DVE, lambda: nc.vector.tensor_tensor_scan(out=tend[:], data0=onesr[:], data1=ntf[:], initial=0.0, op0=ALU.mult, op1=ALU.add),
                 r=["ones32", "ntf"], w=["tend"])
            T.op(DVE, lambda: nc.vector.tensor_tensor(out=off[:], in0=tend[:], in1=ntf[:], op=ALU.subtract), r=["tend", "ntf"], w=["off"])
            T.op(DVE, lambda: nc.vector.tensor_scalar(out=off[:], in0=off[:], scalar1=128.0, scalar2=None, op0=ALU.mult), r=["off"], w=["off"])
            for k, mk in enumerate((m1_all, m2_all)):
                T.op(DVE, lambda: nc.vector.tensor_tensor(out=big[:], in0=rank_all[:], in1=off[:].unsqueeze(1).to_broadcast([128, NT, 32]), op=ALU.add),
                     r=[("rank",), "off"], w=["big"])
                T.op(DVE, lambda mk=mk: nc.vector.tensor_tensor(out=big[:], in0=big[:], in1=mk[:], op=ALU.mult), r=["big", ("m1",), ("m2",)], w=["big"])
                T.op(DVE, lambda k=k: nc.vector.tensor_reduce(out=pf[:, k, :], in_=big[:], axis=AX.X, op=ALU.add), r=["big"], w=[("pf", k)])
            T.op(DVE, lambda: nc.vector.tensor_scalar(out=pf[:], in0=pf[:], scalar1=0.0, scalar2=float(NSLOT * 128 - 1), op0=ALU.max, op1=ALU.min),
                 r=[("pf",)], w=[("pf",)])
            T.op(DVE, lambda: nc.vector.tensor_copy(out=P12[:], in_=pf[:]), r=[("pf",)], w=["P12"])
            T.op(POOL, lambda: nc.gpsimd.iota(out=sidx[:], pattern=[[1, NSLOT]], base=0, channel_multiplier=0, allow_small_or_imprecise_dtypes=True), w=["sidx"])
            T.op(POOL, lambda: nc.gpsimd.iota(out=pio[:], pattern=[[1, 1]], base=0, channel_multiplier=1, allow_small_or_imprecise_dtypes=True), w=["pio"])
            T.op(DVE, lambda: nc.vector.tensor_tensor(out=cmp3[:], in0=tend[:].unsqueeze(1).to_broadcast([128, NSLOT, 32]),
                                                      in1=sidx[:].unsqueeze(2).to_broadcast([128, NSLOT, 32]), op=ALU.is_le),
                 r=["tend", "sidx"], w=["cmp3"])
            T.op(DVE, lambda: nc.vector.tensor_reduce(out=esl[:], in_=cmp3[:], axis=AX.X, op=ALU.add), r=["cmp3"], w=["esl"])
            T.op(DVE, lambda: nc.vector.tensor_scalar(out=esl[:], in0=esl[:], scalar1=31.0, scalar2=128.0, op0=ALU.min, op1=ALU.mult), r=["esl"], w=["esl"])
            T.op(DVE, lambda: nc.vector.tensor_scalar(out=esl[:], in0=esl[:], scalar1=pio[:], scalar2=None, op0=ALU.add), r=["esl", "pio"], w=["esl"])
            for ci in range(NSPL):
                T.op(DVE, lambda ci=ci: nc.vector.tensor_scalar(out=sidx[:], in0=esl[:], scalar1=float(NSPL), scalar2=float(ci), op0=ALU.mult, op1=ALU.add),
                     r=["esl", "cmp3"], w=["sidx"])
                T.op(DVE, lambda ci=ci: nc.vector.tensor_copy(out=widx[:, ci, :], in_=sidx[:]), r=["sidx"], w=[("widx", ci)])
            T.barrier()
        if dbg and "P12" in dbg_out:
            T.dma(SP, dbg_out["P12"][:, :], P12[:].rearrange("p k t -> p (k t)"), r=["P12"])
            T.dma(SP, dbg_out["widx"][:, :], widx[:, 0, :], r=["widx"])

        KE = int(os.environ.get("KE", 9))
        if KE >= 2:
            x2l = [sb(f"x2l{i}", [128, D], BF16) for i in range(3)]
            for t in range(NT):
                b = t % 3
                T.dma(SP, x2l[b][:], X2_d[t * 128:(t + 1) * 128, :], w=[("x2l", b)])
                for k in range(2):
                    T.dmai(Xs_d[:, :], x2l[b][:], out_idx=P12[:, k, t:t + 1], r=[("x2l", b), "P12"], w=[("Xs", t, k)])
            T.barrier()

        if KE >= 3:
            xs = [sb(f"xs{i}", [128, D], BF16) for i in range(3)]
            wsl = [sb(f"wsl{i}", [128, 6144], BF16) for i in range(3)]
            xT = [sb(f"xTs{i}", [128, 8, 128], BF16) for i in range(2)]
            sS = [sb(f"sS{i}", [128, 256], F32) for i in range(2)]
            ac = [sb(f"ac{i}", [128, 256], BF16) for i in range(2)]
            aT = [sb(f"aTs{i}", [128, 2, 128], BF16) for i in range(2)]
            yo = [sb(f"yo{i}", [128, D], BF16) for i in range(2)]
            NSL = int(os.environ.get("KSLOTS", NSLOT))

            def load_slot(sl):
                b3 = sl % 3
                T.dma(SP, xs[b3][:], Xs_d[sl * 128:(sl + 1) * 128, :], r=[("Xs",)], w=[("xs", b3)])
                cw_ = 6144 // NSPL
                for ci in range(NSPL):
                    T.dmai(wsl[b3][:, ci * cw_:(ci + 1) * cw_], Wcat_d.rearrange("r (c w) -> (r c) w", c=NSPL), in_idx=widx[:, ci, sl:sl + 1],
                           r=[("Wcat",), "widx"], w=[("wsl", b3, ci)])

            def slot_s1(sl):
                b2, b3 = sl % 2, sl % 3
                base = 4 * b2
                xTp = psb(base).rearrange("p (k t) -> p k t", k=8)
                for kc in range(8):
                    T.op(PE, lambda kc=kc: nc.tensor.transpose(xTp[:, kc, :], xs[b3][:, kc * 128:(kc + 1) * 128], ident_b),
                         r=[("xs", b3), "cstb"], w=[("ps", base)])
                T.op(ACT, lambda: nc.scalar.copy(out=xT[b2][:, 0:4, :], in_=xTp[:, 0:4, :]), r=[("ps", base)], w=[("xTs", b2, 0)])
                T.op(DVE, lambda: nc.vector.tensor_copy(out=xT[b2][:, 4:8, :], in_=xTp[:, 4:8, :]), r=[("ps", base), ("xTs", b2, 0)], w=[("xTs", b2, 1)])
                for kc in range(8):
                    T.op(PE, lambda kc=kc: nc.tensor.matmul(PS[base + 1][:, :], lhsT=xT[b2][:, kc, :], rhs=wsl[b3][:, kc * 512:(kc + 1) * 512],
                                                            start=(kc == 0), stop=(kc == 7)), r=[("xTs", b2), ("wsl", b3)], w=[("ps", base + 1)])

            def slot_s2(sl):
                b2, b3 = sl % 2, sl % 3
                base = 4 * b2
                T.op(ACT, lambda: nc.scalar.activation(out=sS[b2][:], in_=PS[base + 1][:, 0:256], func=AF.Silu), r=[("ps", base + 1)], w=[("sS", b2)])
                T.op(DVE, lambda: nc.vector.tensor_tensor(out=ac[b2][:], in0=PS[base + 1][:, 256:512], in1=sS[b2][:], op=ALU.mult),
                     r=[("ps", base + 1), ("sS", b2)], w=[("ac", b2)])
                aTp = psb(base)[:, 0:256].rearrange("p (k t) -> p k t", k=2)
                for fc in range(2):
                    T.op(PE, lambda fc=fc: nc.tensor.transpose(aTp[:, fc, :], ac[b2][:, fc * 128:(fc + 1) * 128], ident_b),
                         r=[("ac", b2), "cstb"], w=[("ps", base)])
                T.op(ACT, lambda: nc.scalar.copy(out=aT[b2][:], in_=aTp), r=[("ps", base)], w=[("aTs", b2)])
                for cb_ in range(2):
                    for fc in range(2):
                        T.op(PE, lambda fc=fc, cb_=cb_: nc.tensor.matmul(
                            PS[base + 2 + cb_][:, :], lhsT=aT[b2][:, fc, :], rhs=wsl[b3][:, 4096 + fc * 1024 + cb_ * 512: 4096 + fc * 1024 + (cb_ + 1) * 512],
                            start=(fc == 0), stop=(fc == 1)), r=[("aTs", b2), ("wsl", b3)], w=[("ps", base + 2 + cb_)])
                T.op(ACT, lambda: nc.scalar.copy(out=yo[b2][:, 0:512], in_=PS[base + 2][:, :]), r=[("ps", base + 2)], w=[("yo", b2, 0)])
                T.op(DVE, lambda: nc.vector.tensor_copy(out=yo[b2][:, 512:1024], in_=PS[base + 3][:, :]), r=[("ps", base + 3)], w=[("yo", b2, 1)])
                T.dma(SP, Ys_d[sl * 128:(sl + 1) * 128, :], yo[b2][:], r=[("yo", b2)], w=[("Ys", sl)])

            for sl in range(min(2, NSL)):
                load_slot(sl)
            slot_s1(0)
            for sl in range(NSL):
                if sl + 2 < NSL:
                    load_slot(sl + 2)
                if sl + 1 < NSL:
                    slot_s1(sl + 1)
                slot_s2(sl)
            T.barrier()

        if KE >= 4:
            y1 = [sb(f"y1_{i}", [128, D], BF16) for i in range(3)]
            ya = [sb(f"ya_{i}", [128, D], F32) for i in range(3)]
            y2 = [sb(f"y2_{i}", [128, D], BF16) for i in range(3)]
            x1l = [sb(f"x1l{i}", [128, D], F32) for i in range(3)]
            def load_tok(t):
                b = t % 3
                T.dma(SP, x1l[b][:], x1_d[t * 128:(t + 1) * 128, :], w=[("x1l", b)])
                T.dmai(y1[b][:], Ys_d[:, :], in_idx=P12[:, 0, t:t + 1], r=[("Ys",), "P12"], w=[("y1", b)])
                T.dmai(y2[b][:], Ys_d[:, :], in_idx=P12[:, 1, t:t + 1], r=[("Ys",), "P12"], w=[("y2", b)])

            load_tok(0)
            load_tok(1)
            for t in range(NT):
                b = t % 3
                if t + 2 < NT:
                    load_tok(t + 2)
                T.op(DVE, lambda b=b, t=t: nc.vector.tensor_scalar(out=ya[b][:], in0=y1[b][:], scalar1=w12[:, t, 0:1], scalar2=None, op0=ALU.mult),
                     r=[("y1", b), ("w12",)], w=[("ya", b)])
                T.op(DVE, lambda b=b, t=t: nc.vector.scalar_tensor_tensor(out=ya[b][:], in0=y2[b][:], scalar=w12[:, t, 1:2], in1=ya[b][:], op0=ALU.mult, op1=ALU.add),
                     r=[("ya", b), ("y2", b), ("w12",)], w=[("ya", b)])
                T.op(DVE, lambda b=b: nc.vector.tensor_tensor(out=ya[b][:], in0=ya[b][:], in1=g2b[:], op=ALU.mult), r=[("ya", b), ("g2b",)], w=[("ya", b)])
                T.op(DVE, lambda b=b: nc.vector.tensor_tensor(out=x1l[b][:], in0=x1l[b][:], in1=ya[b][:], op=ALU.add), r=[("ya", b), ("x1l", b)], w=[("x1l", b)])
                T.dma(SP, out_d[t * 128:(t + 1) * 128, :], x1l[b][:], r=[("x1l", b)], w=[("out", t)])
            T.barrier()
    return finish()


def _consts():
    c = np.zeros((128, NCST), np.float32)
    c[:, 0:128] = np.eye(128, dtype=np.float32)
    s = np.arange(128)[:, None]
    t = np.arange(128)[None, :]
    c[:, 128:256] = (s <= t).astype(np.float32)
    c[:, 256:384] = ((s // 64) == (t // 64)).astype(np.float32)
    R = np.zeros((128, 128), np.float32)
    for base in (0, 64):
        for d in range(8):
            R[base + d, base + d + 8] = -1.0
            R[base + d + 8, base + d] = 1.0
    c[:, 384:512] = R.T
    c[:, 512:640] = 1.0
    inv = 500000.0 ** (-np.arange(0, 16, 2, dtype=np.float32) / 16.0)
    for p in range(128):
        d = p % 64
        c[p, 640] = inv[d % 8] if d < 16 else 0.0
    return c


_NC_CACHE = {}


def make_in_maps(inputs):
    f = lambda k: np.ascontiguousarray(np.asarray(inputs[k], dtype=np.float32))
    x = f("x")
    c = f("c")
    pos = np.ascontiguousarray(np.asarray(inputs["positions"], dtype=np.int32))
    b_ada = f("b_ada")[0]
    shared = {
        "w_ada": f("w_ada")[0],
        "b_ada_col": np.ascontiguousarray(b_ada.reshape(48, 128).T),
        "b_ada_row": np.ascontiguousarray(b_ada.reshape(1, -1)),
        "n1w": np.ascontiguousarray(f("norm1_w")[0].reshape(8, 128).T),
        "n2w": np.ascontiguousarray(f("norm2_w")[0].reshape(8, 128).T),
        "n2row": np.ascontiguousarray(f("norm2_w")[0].reshape(1, D)),
        "w_in": f("w_in")[0],
        "bgate": np.ascontiguousarray(np.stack([f("b_igate")[0], f("b_fgate")[0]], axis=1)),
        "convw": np.ascontiguousarray(f("conv_w")[0].T.reshape(8, 128, 4).transpose(1, 0, 2)),
        "convb": np.ascontiguousarray(f("conv_b")[0].reshape(8, 128).T),
        "mnw": np.ascontiguousarray(f("mlstm_norm_w")[0].reshape(1, 512)),
        "qnw": np.ascontiguousarray(np.tile(f("q_norm_w")[0], 2).reshape(128, 1)),
        "knw": np.ascontiguousarray(np.tile(f("k_norm_w")[0], 2).reshape(128, 1)),
        "qkrow": np.ascontiguousarray(np.concatenate([f("q_norm_w")[0], f("k_norm_w")[0]]).reshape(1, 128)),
        "lamv": np.ascontiguousarray(np.concatenate([f("lam_q1")[0], f("lam_k1")[0], f("lam_q2")[0], f("lam_k2")[0]]).reshape(1, 256)),
        "subw": np.ascontiguousarray(f("subln_w")[0].reshape(128, 1)),
        "w_br_m": f("w_br_m")[0],
        "w_br_d": f("w_br_d")[0],
        "w_out": f("w_out")[0],
        "wr": np.ascontiguousarray(np.concatenate([f("w_rg")[0], f("w_re")[0]], axis=1)),
        "br": np.ascontiguousarray(np.concatenate([f("b_rg")[0], f("b_re")[0]]).reshape(1, 36)),
        "w_gate": f("w_gate")[0],
        "w_up": f("w_up")[0],
        "w_down": f("w_down")[0],
        "consts": _consts(),
    }
    maps = []
    for b in range(8):
        m = dict(shared)
        m["x"] = x[b]
        m["cT"] = np.ascontiguousarray(c[b].reshape(8, 128).T)
        m["pos"] = np.ascontiguousarray(pos[b].reshape(1, S))
        maps.append(m)
    return maps


def kernel(**inputs):
    if "nc" not in _NC_CACHE:
        _NC_CACHE["nc"] = build()
    nc = _NC_CACHE["nc"]
    maps = make_in_maps(inputs)
    res = run_bass_kernel_spmd(nc, maps, core_ids=list(range(8)))
    out = np.stack([np.asarray(r["out"], dtype=np.float32) for r in res.results], axis=0)
    return out
```

```python
import math
import os
from contextlib import ExitStack

import numpy as np
import concourse.bass as bass
import concourse.mybir as mybir
from concourse.bass_utils import run_bass_kernel_spmd

F32 = mybir.dt.float32
BF16 = mybir.dt.bfloat16
I32 = mybir.dt.int32
AF = mybir.ActivationFunctionType
ALU = mybir.AluOpType
AX = mybir.AxisListType

S = 4096
D = 1024
NT = 32
NB = 8
EPS = 1e-6
NCST = 5 * 128 + 2


class _StopBuild(Exception):
    pass


class Trk:
    LIMIT = 50000
    NDSEM = 12

    def __init__(self, nc):
        self.nc = nc
        self.eng = {"pe": nc.tensor, "act": nc.scalar, "dve": nc.vector, "pool": nc.gpsimd, "sp": nc.sync}
        self.cur = {}
        self.seq = {}
        self.gen = {}
        for e in self.eng:
            self.cur[e] = [nc.alloc_semaphore(f"s_{e}_0"), 0]
            self.seq[e] = 0
            self.gen[e] = 0
        self.allsems = [(e, self.cur[e]) for e in self.eng]
        self.waited = {e: {} for e in self.eng}
        self.lastw = {}
        self.readers = {}
        self.children = {}
        self.dring = {}
        for q in ("sp", "pool", "act", "wc"):
            self.dring[q] = [[nc.alloc_semaphore(f"d_{q}_{i}"), 0] for i in range(self.NDSEM)]
        self.dpos = {q: 0 for q in self.dring}
        self.all_dma = []

    def _related(self, k):
        ks = [k[:i] for i in range(1, len(k) + 1)]
        stack = [k]
        while stack:
            p = stack.pop()
            for c in self.children.get(p, ()):
                ks.append(c)
                stack.append(c)
        return ks

    def _register(self, k):
        for i in range(1, len(k)):
            self.children.setdefault(k[:i], set()).add(k[:i + 1])

    def _wait(self, e, ev):
        sem, val, src, sq = ev
        if src == e:
            if e == "pe":
                return
            if e != "pool" and self.seq[e] - sq >= 6:
                return
        w = self.waited[e]
        if w.get(sem.name, 0) >= val:
            return
        self.eng[e].wait_ge(sem, val)
        w[sem.name] = val

    def _deps(self, e, r, w):
        evs = []
        for k in r:
            for kk in self._related(k):
                if kk in self.lastw:
                    evs.append(self.lastw[kk])
        for k in w:
            for kk in self._related(k):
                if kk in self.lastw:
                    evs.append(self.lastw[kk])
                evs.extend(self.readers.get(kk, {}).values())
        for ev in evs:
            self._wait(e, ev)

    def _record(self, ev, r, w):
        for k in r:
            self._register(k)
            self.readers.setdefault(k, {})[ev[2] if ev[2] is not None else ev[0].name] = ev
        for k in w:
            self._register(k)
            self.lastw[k] = ev
            self.readers[k] = {}
            for kk in self._related(k):
                if kk != k and len(kk) > len(k):
                    self.readers[kk] = {}
                    self.lastw.pop(kk, None)

    @staticmethod
    def _norm(ks):
        out = []
        for k in ks:
            k = tuple(k) if isinstance(k, (tuple, list)) else (k,)
            if k[0] == "ps":
                k = k[:2]
            out.append(k)
        return out

    def op(self, e, fn, r=(), w=()):
        r = self._norm(r)
        w = self._norm(w)
        w = w + [k for k in r if k[0] == "ps" and k not in w]
        self._deps(e, r, w)
        ins = fn()
        c = self.cur[e]
        c[1] += 1
        self.seq[e] += 1
        ins.then_inc(c[0], 1)
        ev = (c[0], c[1], e, self.seq[e])
        self._record(ev, r, w)
        if c[1] >= self.LIMIT:
            self.gen[e] += 1
            self.cur[e] = [self.nc.alloc_semaphore(f"s_{e}_{self.gen[e]}"), 0]
            self.allsems.append((e, self.cur[e]))
        return ins

    def dma(self, q, out, in_, r=(), w=(), ring=None, **kw):
        r = self._norm(r)
        w = self._norm(w)
        rq = ring or q
        ring = self.dring[rq]
        slot = ring[self.dpos[rq] % self.NDSEM]
        self.dpos[rq] += 1
        if slot[1] > 0:
            self._wait(q, (slot[0], slot[1], None, 0))
        self._deps(q, r, w)
        ins = self.eng[q].dma_start(out=out, in_=in_, **kw)
        slot[1] += 16
        ins.then_inc(slot[0], 16)
        ev = (slot[0], slot[1], None, 0)
        self._record(ev, r, w)
        return ins

    def dmai(self, out, in_, out_idx=None, in_idx=None, r=(), w=()):
        q = "pool"
        r = self._norm(r)
        w = self._norm(w)
        ring = self.dring[q]
        slot = ring[self.dpos[q] % self.NDSEM]
        self.dpos[q] += 1
        if slot[1] > 0:
            self._wait(q, (slot[0], slot[1], None, 0))
        self._deps(q, r, w)
        ins = self.nc.gpsimd.indirect_dma_start(
            out=out, out_offset=(bass.IndirectOffsetOnAxis(ap=out_idx, axis=0) if out_idx is not None else None),
            in_=in_, in_offset=(bass.IndirectOffsetOnAxis(ap=in_idx, axis=0) if in_idx is not None else None))
        slot[1] += 16
        ins.then_inc(slot[0], 16)
        ev = (slot[0], slot[1], None, 0)
        self._record(ev, r, w)
        return ins

    def barrier(self, full=True):
        evs = []
        for (se, c) in self.allsems:
            if c[1] > 0:
                evs.append((c[0], c[1], "__" + se, 0))
        for q, ring in self.dring.items():
            if q == "wc" and not full:
                continue
            for slot in ring:
                if slot[1] > 0:
                    evs.append((slot[0], slot[1], None, 0))
        for e in self.eng:
            for ev in evs:
                if ev[2] == "__" + e:
                    continue
                self._wait(e, ev)
        self.lastw.clear()
        self.readers.clear()
        self.children.clear()


def build(stage=99, dbg=None):
    nc = bass.Bass("TRN2", target_bir_lowering=False)
    T = Trk(nc)
    _scope = [None]

    def scope_mark(nm):
        if os.environ.get("KSCOPE") != "1":
            return
        if _scope[0] is not None:
            _scope[0].__exit__(None, None, None)
        _scope[0] = nc.named_scope(nm)
        _scope[0].__enter__()

    PE, ACT, DVE, POOL, SP = "pe", "act", "dve", "pool", "sp"

    def din(name, shape, dt=F32):
        return nc.dram_tensor(name, list(shape), dt, kind="ExternalInput").ap()

    def dscr(name, shape, dt):
        return nc.dram_tensor(name, list(shape), dt, kind="Internal").ap()

    x_in = din("x", [S, D])
    cT_in = din("cT", [128, 8])
    pos_in = din("pos", [1, S], I32)
    w_ada = din("w_ada", [D, 6 * D])
    b_ada_col = din("b_ada_col", [128, 48])
    b_ada_row = din("b_ada_row", [1, 6 * D])
    n1w_in = din("n1w", [128, 8])
    n2w_in = din("n2w", [128, 8])
    n2row_in = din("n2row", [1, D])
    w_in = din("w_in", [D, 5640])
    bgate_in = din("bgate", [4, 2])
    convw_in = din("convw", [128, 8, 4])
    convb_in = din("convb", [128, 8])
    mnw_in = din("mnw", [1, 512])
    qnw_in = din("qnw", [128, 1])
    knw_in = din("knw", [128, 1])
    qkrow_in = din("qkrow", [1, 128])
    lamv_in = din("lamv", [1, 256])
    subw_in = din("subw", [128, 1])
    w_br_m = din("w_br_m", [512, D])
    w_br_d = din("w_br_d", [512, D])
    w_out = din("w_out", [D, D])
    wr_in = din("wr", [D, 36])
    br_in = din("br", [1, 36])
    w_gate = din("w_gate", [32, D, 256])
    w_up = din("w_up", [32, D, 256])
    w_down = din("w_down", [32, 256, D])
    consts_in = din("consts", [128, NCST])
    out_d = nc.dram_tensor("out", [S, D], F32, kind="ExternalOutput").ap()

    xnT_d = dscr("xnT_d", [D, S], BF16)
    hmT_d = dscr("hmT_d", [512, S], BF16)
    hdT_d = dscr("hdT_d", [512, S], BF16)
    x1_d = dscr("x1_d", [S, D], F32)
    xn2T_d = dscr("xn2T_d", [D, S], BF16)
    combT_d = dscr("combT_d", [32, S], BF16)
    dec_d = dscr("dec_d", [4, 32], F32)
    gi_d = dscr("gi_d", [4, S], F32)
    NSLOT = 96
    X2_d = dscr("X2_d", [S, D], BF16)
    Xs_d = dscr("Xs_d", [NSLOT * 128, D], BF16)
    Ys_d = dscr("Ys_d", [NSLOT * 128, D], BF16)
    Wcat_d = dscr("Wcat_d", [32 * 128, 6144], BF16)
    gf_d = dscr("gf_d", [4, S], F32)

    dbg_out = {}
    if dbg:
        for name, (shape, dt) in dbg.items():
            dbg_out[name] = nc.dram_tensor("dbg_" + name, list(shape), dt, kind="ExternalOutput").ap()

    PS = [nc.alloc_psum_tensor(f"ps{i}", [128, 512], F32) for i in range(8)]

    def psb(i):
        return PS[i][:].bitcast(BF16)

    xnT_v = xnT_d.rearrange("(k p) t -> p k t", p=128)
    xn2T_v = xn2T_d.rearrange("(k p) t -> p k t", p=128)
    hmT_v = hmT_d.rearrange("(k p) t -> p k t", p=128)
    hdT_v = hdT_d.rearrange("(k p) t -> p k t", p=128)
    w_in_v = w_in.rearrange("(k p) n -> p k n", p=128)
    wg_v = w_gate.rearrange("e (k p) f -> e p k f", p=128)
    wu_v = w_up.rearrange("e (k p) f -> e p k f", p=128)
    wdn_v = w_down.rearrange("e (k p) n -> e p k n", p=128)

    glob = ExitStack()

    def sbg(name, shape, dt):
        return glob.enter_context(nc.sbuf_tensor("s_" + name, list(shape), dt))

    cst = sbg("cst", [128, NCST], F32)
    cstb = sbg("cstb", [128, 5 * 128], BF16)
    modc = sbg("modc", [128, 48], F32)
    a1 = sbg("a1", [128, 8], F32)
    a2 = sbg("a2", [128, 8], F32)
    g1b = sbg("g1b", [128, D], F32)
    g2b = sbg("g2b", [128, D], F32)
    a2b = sbg("a2b", [128, D], F32)
    s2b = sbg("s2b", [128, D], F32)
    rank_all = sbg("rank_all", [128, NT, 32], F32)
    m1_all = sbg("m1_all", [128, NT, 32], F32)
    m2_all = sbg("m2_all", [128, NT, 32], F32)
    w12 = sbg("w12", [128, NT, 2], F32)
    tot = sbg("tot", [128, 32], F32)
    smallc = sbg("smallc", [128, 16], F32)
    ident_f = cst[:, 0:128]
    tri_f = cst[:, 128:256]
    invf = cst[:, 640:641]
    ident_b = cstb[:, 0:128]
    tri_b = cstb[:, 128:256]
    bo64_b = cstb[:, 256:384]
    RT_b = cstb[:, 384:512]
    ones_b = cstb[:, 512:640]
    ones_f = cst[:, 512:640]

    T.dma(SP, cst[:], consts_in[:, :], w=["cst"])
    T.op(DVE, lambda: nc.vector.tensor_copy(out=cstb[:], in_=cst[:, 0:640]), r=["cst"], w=["cstb"])

    early2 = ExitStack()
    CosB = early2.enter_context(nc.sbuf_tensor("s_CosB", [128, S], BF16))
    SinB = early2.enter_context(nc.sbuf_tensor("s_SinB", [128, S], BF16))
    early2_open = [True]

    def sin_reduce(ph_sb, dst_bf, ang, n, tagk):
        ki = ph_sb["ki"]
        kf = ph_sb["kf"]
        msk = ph_sb["msk"]
        C1 = 6.28125
        C2 = 2.0 * math.pi - 6.28125
        T.op(DVE, lambda: nc.vector.tensor_scalar(out=ki[:, 0:n], in0=ang, scalar1=1.0 / (2.0 * math.pi), scalar2=None, op0=ALU.mult),
             r=[tagk], w=["ki"])
        T.op(DVE, lambda: nc.vector.tensor_copy(out=kf[:, 0:n], in_=ki[:, 0:n]), r=["ki"], w=["kf"])
        T.op(DVE, lambda: nc.vector.scalar_tensor_tensor(out=ang, in0=kf[:, 0:n], scalar=-C1, in1=ang, op0=ALU.mult, op1=ALU.add),
             r=["kf", tagk], w=[tagk])
        T.op(DVE, lambda: nc.vector.scalar_tensor_tensor(out=ang, in0=kf[:, 0:n], scalar=-C2, in1=ang, op0=ALU.mult, op1=ALU.add),
             r=["kf", tagk], w=[tagk])
        T.op(DVE, lambda: nc.vector.tensor_scalar(out=msk[:, 0:n], in0=ang, scalar1=math.pi, scalar2=-2.0 * math.pi, op0=ALU.is_gt, op1=ALU.mult),
             r=[tagk], w=["msk"])
        T.op(DVE, lambda: nc.vector.tensor_tensor(out=ang, in0=ang, in1=msk[:, 0:n], op=ALU.add), r=[tagk, "msk"], w=[tagk])
        T.op(DVE, lambda: nc.vector.tensor_scalar(out=msk[:, 0:n], in0=ang, scalar1=-math.pi, scalar2=2.0 * math.pi, op0=ALU.is_lt, op1=ALU.mult),
             r=[tagk], w=["msk"])
        T.op(DVE, lambda: nc.vector.tensor_tensor(out=ang, in0=ang, in1=msk[:, 0:n], op=ALU.add), r=[tagk, "msk"], w=[tagk])
        T.op(DVE, lambda: nc.vector.tensor_scalar(out=ang, in0=ang, scalar1=math.pi, scalar2=-math.pi, op0=ALU.min, op1=ALU.max),
             r=[tagk], w=[tagk])
        T.op(ACT, lambda: nc.scalar.activation(out=dst_bf, in_=ang, func=AF.Sin), r=[tagk], w=[tagk + "_o"])

    early = ExitStack()
    NWST = int(os.environ.get("KWST", 2))
    wst = [early.enter_context(nc.sbuf_tensor(f"s_wst{i}", [128, 6144], BF16)) for i in range(NWST)]
    for e in range(32):
        b = e % NWST
        gu = wst[b][:, 0:4096].rearrange("p (k f) -> p k f", k=8)
        T.dma(POOL, gu[:, :, 0:256], wg_v[e], w=[("wst", b, 0)], ring="wc")
        T.dma(POOL, gu[:, :, 256:512], wu_v[e], w=[("wst", b, 1)], ring="wc")
        T.dma(POOL, wst[b][:, 4096:6144].rearrange("p (k n) -> p k n", k=2), wdn_v[e], w=[("wst", b, 2)], ring="wc")
        T.dma(POOL, Wcat_d[e * 128:(e + 1) * 128, :], wst[b][:], r=[("wst", b)], w=[("Wcat", e)], ring="wc")
    early_open = [True]
    scope_mark("ph0")
    with ExitStack() as ph:
        def sb(name, shape, dt):
            return ph.enter_context(nc.sbuf_tensor("s_" + name, list(shape), dt))
        cT = sb("cT", [128, 8], F32)
        cT2 = sb("cT2", [128, 8, 2], F32)
        cbc = sb("cbc", [128, 8, 128], F32)
        wa = [sb(f"wa{i}", [128, 8, 512], F32) for i in range(2)]
        bcol = sb("bcol", [128, 48], F32)
        brow = sb("brow", [128, 2048], F32)
        nw = sb("nw", [128, 16], F32)
        tmp8 = sb("tmp8", [128, 8], F32)
        lamb = sb("lamb", [128, 256], F32)
        qkrow = sb("qkrow", [128, 128], F32)
        lt = sb("lt", [128, 128], F32)
        l2 = sb("l2", [128, 4], F32)

        T.dma(SP, cT[:], cT_in[:, :], w=["cT"])
        T.dma(SP, bcol[:], b_ada_col[:, :], w=["bcol"])
        T.dma(SP, brow[:, 0:1024], b_ada_row[0:1, 2048:3072].partition_broadcast(128), w=["brow0"])
        T.dma(SP, brow[:, 1024:2048], b_ada_row[0:1, 5120:6144].partition_broadcast(128), w=["brow1"])
        T.dma(SP, nw[:, 0:8], n1w_in[:, :], w=["nw0"])
        T.dma(SP, nw[:, 8:16], n2w_in[:, :], w=["nw1"])
        T.dma(SP, smallc[:, 0:1], qnw_in[:, :], w=["sc0"])
        T.dma(SP, smallc[:, 1:2], knw_in[:, :], w=["sc1"])
        T.dma(SP, smallc[:, 2:3], subw_in[:, :], w=["sc2"])
        T.dma(SP, lamb[:], lamv_in[0:1, :].partition_broadcast(128), w=["lamb"])
        T.dma(SP, qkrow[:], qkrow_in[0:1, :].partition_broadcast(128), w=["qkrow"])
        for k in range(2):
            T.op(DVE, lambda k=k: nc.vector.tensor_copy(out=cT2[:, :, k], in_=cT[:]), r=["cT"], w=[("cT2", k)])
        for kc in range(8):
            T.op(DVE, lambda kc=kc: nc.vector.tensor_copy(out=cbc[:, kc, :], in_=cT[:, kc:kc + 1].to_broadcast([128, 128])),
                 r=["cT"], w=[("cbc", kc)])
        if True:
            posi = ph.enter_context(nc.sbuf_tensor("s_posi", [128, 1024], I32))
            posf = ph.enter_context(nc.sbuf_tensor("s_posf", [128, 1024], F32))
            ang = ph.enter_context(nc.sbuf_tensor("s_ang", [128, 1024], F32))
            tb = dict(ki=ph.enter_context(nc.sbuf_tensor("s_ki", [128, 1024], I32)),
                      kf=ph.enter_context(nc.sbuf_tensor("s_kf", [128, 1024], F32)),
                      msk=ph.enter_context(nc.sbuf_tensor("s_msk", [128, 1024], F32)))
            for c4 in range(4):
                cs = slice(c4 * 1024, (c4 + 1) * 1024)
                T.dma(SP, posi[:], pos_in[0:1, cs].partition_broadcast(128), w=["posi"])
                T.op(DVE, lambda: nc.vector.tensor_copy(out=posf[:], in_=posi[:]), r=["posi"], w=["posf"])
                T.op(DVE, lambda: nc.vector.tensor_scalar(out=ang[:], in0=posf[:], scalar1=invf, scalar2=None, op0=ALU.mult),
                     r=["posf", "cst"], w=["ang"])
                sin_reduce(tb, SinB[:, cs], ang[:], 1024, "ang")
                T.op(DVE, lambda: nc.vector.tensor_scalar(out=ang[:], in0=posf[:], scalar1=invf, scalar2=math.pi / 2, op0=ALU.mult, op1=ALU.add),
                     r=["posf", "cst", "ang_o"], w=["ang"])
                sin_reduce(tb, CosB[:, cs], ang[:], 1024, "ang")

        w_ada_v = w_ada.rearrange("(k p) n -> p k n", p=128)
        gate_bank = {4: 1, 5: 2, 6: 5, 7: 6, 8: 1, 9: 2, 10: 3, 11: 4}
        n2rb = sb("n2rb", [128, D], F32)
        T.dma(SP, n2rb[:], n2row_in[0:1, :].partition_broadcast(128), w=["n2rb"])
        T.dma(SP, s2b[:], b_ada_row[0:1, 3072:4096].partition_broadcast(128), w=["s2b_bias"])
        T.dma(SP, a2b[:], b_ada_row[0:1, 4096:5120].partition_broadcast(128), w=["a2b_bias"])
        for blk in range(12):
            b = blk % 2
            for hh in range(2):
                T.dma(SP, wa[b][:, 4 * hh:4 * hh + 4, :], w_ada_v[:, 4 * hh:4 * hh + 4, blk * 512:(blk + 1) * 512], w=[("wa", b)])
            for m in range(4):
                j = blk * 4 + m
                for kc in range(8):
                    T.op(PE, lambda j=j, m=m, kc=kc, b=b: nc.tensor.matmul(
                        PS[0][:, 2 * j:2 * j + 2], lhsT=wa[b][:, kc, m * 128:(m + 1) * 128], rhs=cT2[:, kc, :],
                        start=(kc == 0), stop=(kc == 7)), r=[("wa", b), "cT2"], w=[("ps", 0)])
            if blk in gate_bank:
                gb = gate_bank[blk]
                for kc in range(8):
                    T.op(PE, lambda kc=kc, b=b, gb=gb: nc.tensor.matmul(
                        PS[gb][:, :], lhsT=cbc[:, kc, :], rhs=wa[b][:, kc, :], start=(kc == 0), stop=(kc == 7)),
                        r=[("wa", b), "cbc"], w=[("ps", gb)])
                if blk == 5:
                    for hh in range(2):
                        T.op(DVE, lambda hh=hh: nc.vector.tensor_tensor(out=g1b[:, hh * 512:(hh + 1) * 512], in0=PS[1 + hh][:, :],
                                                                      in1=brow[:, hh * 512:(hh + 1) * 512], op=ALU.add),
                             r=[("ps", 1 + hh), "brow0"], w=[("g1b", hh)])
                if blk == 7:
                    for hh in range(2):
                        T.op(DVE, lambda hh=hh: nc.vector.tensor_tensor(out=s2b[:, hh * 512:(hh + 1) * 512], in0=PS[5 + hh][:, :],
                                                                      in1=s2b[:, hh * 512:(hh + 1) * 512], op=ALU.add),
                             r=[("ps", 5 + hh), "s2b_bias"], w=[("s2b", hh)])
                if blk == 9:
                    for hh in range(2):
                        sl = slice(hh * 512, (hh + 1) * 512)
                        T.op(DVE, lambda hh=hh, sl=sl: nc.vector.tensor_tensor(out=a2b[:, sl], in0=PS[1 + hh][:, :], in1=a2b[:, sl], op=ALU.add),
                             r=[("ps", 1 + hh), "a2b_bias"], w=[("a2b", hh)])
                        T.op(DVE, lambda sl=sl: nc.vector.scalar_tensor_tensor(out=a2b[:, sl], in0=a2b[:, sl], scalar=1.0, in1=n2rb[:, sl], op0=ALU.add, op1=ALU.mult),
                             r=[("a2b", hh), "n2rb"], w=[("a2b", hh)])
        T.op(DVE, lambda: nc.vector.tensor_tensor(
            out=modc[:], in0=PS[0][:, 0:96].rearrange("p (m two) -> p m two", two=2)[:, :, 0], in1=bcol[:], op=ALU.add),
            r=[("ps", 0), "bcol"], w=["modc"])
        for gi, (gt, b0) in enumerate(((g1b, 1), (g2b, 3))):
            if gi == 0:
                continue
            for hh in range(2):
                T.op(DVE, lambda gt=gt, b0=b0, hh=hh, gi=gi: nc.vector.tensor_tensor(
                    out=gt[:, hh * 512:(hh + 1) * 512], in0=PS[b0 + hh][:, :],
                    in1=brow[:, gi * 1024 + hh * 512: gi * 1024 + (hh + 1) * 512], op=ALU.add),
                    r=[("ps", b0 + hh), f"brow{gi}"], w=[(f"g{gi + 1}b", hh)])
        for (av, sc0, nwo, akey) in ((a1, 8, 0, "a1"), (a2, 32, 8, "a2")):
            T.op(DVE, lambda sc0=sc0: nc.vector.tensor_scalar(out=tmp8[:], in0=modc[:, sc0:sc0 + 8], scalar1=1.0, scalar2=None, op0=ALU.add),
                 r=["modc"], w=["tmp8"])
            T.op(DVE, lambda av=av, nwo=nwo: nc.vector.tensor_tensor(out=av[:], in0=tmp8[:], in1=nw[:, nwo:nwo + 8], op=ALU.mult),
                 r=["tmp8", "nw0", "nw1"], w=[akey])
        T.op(DVE, lambda: nc.vector.tensor_scalar(out=smallc[:, 2:3], in0=smallc[:, 2:3], scalar1=0.8, scalar2=None, op0=ALU.mult),
             r=["sc2"], w=["sc2"])
        T.op(DVE, lambda: nc.vector.tensor_reduce(out=l2[:, 0:2], in_=qkrow[:].rearrange("p (a b) -> p a b", a=2), axis=AX.X, op=ALU.max,
                                                  apply_absolute_value=True), r=["qkrow"], w=["l2a"])
        T.op(DVE, lambda: nc.vector.tensor_tensor(out=l2[:, 2:3], in0=l2[:, 0:1], in1=l2[:, 1:2], op=ALU.mult), r=["l2a"], w=["l2b"])
        T.op(DVE, lambda: nc.vector.tensor_scalar(out=smallc[:, 3:4], in0=l2[:, 2:3], scalar1=-8.0, scalar2=None, op0=ALU.mult),
             r=["l2b"], w=["sc3"])
        lv = lamb[:].rearrange("p (a b) -> p a b", a=4)
        T.op(DVE, lambda: nc.vector.tensor_tensor(out=lt[:].rearrange("p (a b) -> p a b", a=2), in0=lv[:, 0:4:2, :], in1=lv[:, 1:4:2, :], op=ALU.mult),
             r=["lamb"], w=["lt"])
        T.op(DVE, lambda: nc.vector.tensor_reduce(out=l2[:, 0:2], in_=lt[:].rearrange("p (a b) -> p a b", a=2), axis=AX.X, op=ALU.add),
             r=["lt", "l2a", "l2b"], w=["l2a"])
        T.op(ACT, lambda: nc.scalar.activation(out=l2[:, 2:4], in_=l2[:, 0:2], func=AF.Exp), r=["l2a"], w=["l2b"])
        T.op(DVE, lambda: nc.vector.tensor_tensor(out=l2[:, 0:1], in0=l2[:, 3:4], in1=l2[:, 2:3], op=ALU.subtract), r=["l2b"], w=["l2a"])
        T.op(DVE, lambda: nc.vector.tensor_scalar(out=smallc[:, 4:5], in0=l2[:, 0:1], scalar1=-0.2, scalar2=None, op0=ALU.add),
             r=["l2a"], w=["sc4"])
        T.barrier(full=False)
    if dbg and "modc" in dbg_out:
        T.dma(SP, dbg_out["modc"][:, :], modc[:], r=["modc"])
        T.dma(SP, dbg_out["g1b"][:, :], g1b[:], r=["g1b"])
        T.dma(SP, dbg_out["smallc"][:, :], smallc[:], r=["sc0"])

    def dump_dram(dst, src, rows, cols, dt):
        with nc.sbuf_tensor("s_dump_" + dst.name.replace(".", "_"), [128, cols], dt) as tmp:
            for r0 in range(0, rows, 128):
                n = min(128, rows - r0)
                T.dma(SP, tmp[0:n, :], src[r0:r0 + n, :], w=["dumptmp"])
                T.dma(SP, dst[r0:r0 + n, :], tmp[0:n, :], r=["dumptmp"], w=[("dumpdst", r0)])
            T.barrier()

    def finish():
        if _scope[0] is not None:
            _scope[0].__exit__(None, None, None)
            _scope[0] = None
        T.barrier()
        if early_open[0]:
            early.close()
            early_open[0] = False
        if early2_open[0]:
            early2.close()
            early2_open[0] = False
        glob.close()
        return nc

    if stage <= 0:
        return finish()

    s1 = modc[:, 0:8]
    s2 = modc[:, 24:32]

    scope_mark("ph1")
    def norm_tile(ph_bufs, src_ap, rkeys, tag):
        ss, sd, junk = ph_bufs["ss"], ph_bufs["sd"], ph_bufs["junk"]
        T.op(ACT, lambda: nc.scalar.activation(out=junk[:], in_=src_ap, func=AF.Square, accum_out=ss[:]), r=rkeys, w=["junk", "ss"])
        T.op(ACT, lambda: nc.scalar.activation(out=sd[:], in_=ss[:], func=AF.Ln, bias=ph_bufs["epsc"][:], scale=1.0 / D), r=["ss"], w=["sd"])
        T.op(ACT, lambda: nc.scalar.activation(out=ph_bufs["rs"][:], in_=sd[:], func=AF.Exp, scale=-0.5), r=["sd"], w=["rs"])

    with ExitStack() as ph:
        def sb(name, shape, dt):
            return ph.enter_context(nc.sbuf_tensor("s_" + name, list(shape), dt))
        xt = [sb(f"xt{i}", [128, D], F32) for i in range(2)]
        xh = [sb(f"xh{i}", [128, D], BF16) for i in range(2)]
        hTb = [sb(f"hTb{i}", [128, 8, 512], BF16) for i in range(2)]
        bufs = dict(ss=sb("ss", [128, 1], F32), sd=sb("sd", [128, 1], F32), rs=sb("rs", [128, 1], F32),
                    junk=sb("junk", [128, D], BF16), epsc=sb("epsc", [128, 1], F32))
        T.op(DVE, lambda: nc.vector.memset(bufs["epsc"][:], EPS), w=["epsc"])
        pass
        for t in range(int(os.environ.get('K1_TILES', NT))):
            b = t % 2
            blk, tl = t // 4, t % 4
            hb = blk % 2
            T.dma(SP, xt[b][:], x_in[t * 128:(t + 1) * 128, :], w=[("xt", b)])
            KO = int(os.environ.get('K1_OPS', 9))
            if KO < 1:
                continue
            norm_tile(bufs, xt[b][:], [("xt", b), "epsc"], "n1")
            if KO < 2:
                continue
            T.op(DVE, lambda b=b: nc.vector.tensor_scalar(out=xh[b][:], in0=xt[b][:], scalar1=bufs["rs"][:], scalar2=None, op0=ALU.mult),
                 r=[("xt", b), "rs"], w=[("xh", b)])
            if KO < 3:
                continue
            pb = t % 2
            pv = psb(pb).rearrange("p (k t) -> p k t", k=8)
            for kc in range(8):
                T.op(PE, lambda kc=kc, b=b, pv=pv: nc.tensor.transpose(pv[:, kc, :], xh[b][:, kc * 128:(kc + 1) * 128], ident_b),
                     r=[("xh", b), "cstb"], w=[("ps", pb)])
            if KO < 4:
                continue
            for kc in range(8):
                T.op(ACT, lambda kc=kc, pv=pv, hb=hb, tl=tl: nc.scalar.activation(
                    out=hTb[hb][:, kc, tl * 128:(tl + 1) * 128], in_=pv[:, kc, :], func=AF.Identity,
                    **({} if os.environ.get('K1_NOAP') == '1' else ({'scale': a1[:, kc:kc + 1]} if os.environ.get('K1_NOAP') == '2' else
                        ({'bias': s1[:, kc:kc + 1]} if os.environ.get('K1_NOAP') == '3' else dict(scale=a1[:, kc:kc + 1], bias=s1[:, kc:kc + 1]))))),
                    r=[("ps", pb), "a1", "modc"], w=[("hTb", hb, tl)])
            if tl == 3:
                T.dma(SP, xnT_v[:, :, blk * 512:(blk + 1) * 512], hTb[hb][:], r=[("hTb", hb)], w=[("xnT", blk)])
        T.barrier()
    early.close()
    early_open[0] = False
    if dbg and "xnT" in dbg_out:
        dump_dram(dbg_out["xnT"], xnT_d, 1024, S, BF16)
    if stage <= 1:
        return finish()

    scope_mark("phA")
    persist = ExitStack()

    def sbp(name, shape, dt):
        return persist.enter_context(nc.sbuf_tensor("s_" + name, list(shape), dt))

    with ExitStack() as ph:
        def sb(name, shape, dt):
            return ph.enter_context(nc.sbuf_tensor("s_" + name, list(shape), dt))
        wqkv = sb("wqkv", [128, 8, 1536], BF16)
        wgt = sb("wgt", [128, 8, 8], BF16)
        bg = sb("bg", [4, 2], F32)
        nbf = sb("nbf", [4, 1], F32)
        epsc = sb("epscA", [128, 1], F32)
        gst = [[sb(f"gst{g}_{i}", [4, 512], F32) for i in range(2)] for g in range(2)]
        kT = sb("kT", [128, 4, S], BF16)
        v1 = sb("v1", [128, NT, 4, 128], BF16)
        xb = [sb(f"xbA{i}", [128, 8, 512], BF16) for i in range(2)]
        qT = sb("qT", [128, 4, 512], BF16)
        hdTb = sb("hdTb", [128, 4, 512], BF16)
        Pb = [[sb(f"P{m}_{i}", [128, 512], BF16) for i in range(2)] for m in range(2)]
        sqb = [sb(f"sqb{i}", [128, 512], BF16) for i in range(2)]
        qsf = [sb(f"qsf{i}", [128, 512], F32) for i in range(2)]
        sdf = [sb(f"sdf{i}", [128, 512], F32) for i in range(2)]
        qnb = [sb(f"qnb{i}", [128, 512], BF16) for i in range(2)]
        t1f = [sb(f"t1f{i}", [128, 512], F32) for i in range(2)]
        t2f = [sb(f"t2f{i}", [128, 512], F32) for i in range(2)]
        fo = [sb(f"fo{i}", [128, 512], F32) for i in range(4)]

        T.op(DVE, lambda: nc.vector.memset(epsc[:], EPS), w=["epscA"])
        T.dma(POOL, wqkv[:, 0:4, :], w_in_v[:, 0:4, 2056:3592], w=[("wqkv", 0)])
        T.dma(POOL, wqkv[:, 4:8, :], w_in_v[:, 4:8, 2056:3592], w=[("wqkv", 1)])
        T.dma(POOL, wgt[:], w_in_v[:, :, 2048:2056], w=["wgt"])
        T.dma(SP, bg[:], bgate_in[:, :], w=["bg"])
        T.op(DVE, lambda: nc.vector.tensor_scalar(out=nbf[:], in0=bg[:, 1:2], scalar1=-1.0, scalar2=None, op0=ALU.mult), r=["bg"], w=["nbf"])

        negc = smallc[:, 3:4]
        neglam = smallc[:, 4:5]
        subw8 = smallc[:, 2:3]

        for blk in range(NB):
            xbb = blk % 2
            bs = slice(blk * 512, (blk + 1) * 512)
            T.dma(SP, xb[xbb][:], xnT_v[:, :, bs], w=[("xbA", xbb)])
            for c in range(8):
                st = c % 2
                isq = c < 4
                hh = c % 4
                col0 = (0 if isq else 512) + hh * 128
                for kc in range(8):
                    T.op(PE, lambda kc=kc, col0=col0, st=st: nc.tensor.matmul(
                        PS[st][:, :], lhsT=wqkv[:, kc, col0:col0 + 128], rhs=xb[xbb][:, kc, :], start=(kc == 0), stop=(kc == 7)),
                        r=[("wqkv",), ("xbA", xbb)], w=[("ps", st)])
                wcol = smallc[:, 0:1] if isq else smallc[:, 1:2]
                T.op(ACT, lambda st=st: nc.scalar.activation(out=sqb[st][:], in_=PS[st][:, :], func=AF.Square), r=[("ps", st)], w=[("sqb", st)])
                T.op(ACT, lambda st=st, wcol=wcol: nc.scalar.activation(out=qsf[st][:], in_=PS[st][:, :], func=AF.Identity, scale=wcol),
                     r=[("ps", st), "sc0", "sc1"], w=[("qsf", st)])
                T.op(PE, lambda st=st: nc.tensor.matmul(PS[2 + st][:, :], lhsT=bo64_b, rhs=sqb[st][:], start=True, stop=True),
                     r=[("sqb", st), "cstb"], w=[("ps", 2 + st)])
                T.op(ACT, lambda st=st: nc.scalar.activation(out=sdf[st][:], in_=PS[2 + st][:, :], func=AF.Ln, bias=epsc[:], scale=1.0 / 64),
                     r=[("ps", 2 + st), "epscA"], w=[("sdf", st)])
                T.op(ACT, lambda st=st: nc.scalar.activation(out=sdf[st][:], in_=sdf[st][:], func=AF.Exp, scale=-0.5),
                     r=[("sdf", st)], w=[("sdf", st)])
                T.op(DVE, lambda st=st: nc.vector.tensor_tensor(out=qnb[st][:], in0=qsf[st][:], in1=sdf[st][:], op=ALU.mult),
                     r=[("qsf", st), ("sdf", st)], w=[("qnb", st)])
                T.op(PE, lambda st=st: nc.tensor.matmul(PS[2 + st][:, :], lhsT=RT_b, rhs=qnb[st][:], start=True, stop=True),
                     r=[("qnb", st), "cstb"], w=[("ps", 2 + st)])
                T.op(POOL, lambda st=st: nc.gpsimd.tensor_tensor(out=t1f[st][:], in0=qnb[st][:], in1=CosB[:, bs], op=ALU.mult),
                     r=[("qnb", st), "CosB"], w=[("t1f", st)])
                T.op(DVE, lambda st=st: nc.vector.tensor_tensor(out=t2f[st][:], in0=PS[2 + st][:, :], in1=SinB[:, bs], op=ALU.mult),
                     r=[("ps", 2 + st), "SinB"], w=[("t2f", st)])
                if isq:
                    dst, dk = qT[:, hh, :], ("qT", hh)
                else:
                    dst, dk = kT[:, hh, bs], ("kT", hh, blk)
                T.op(POOL, lambda st=st, dst=dst: nc.gpsimd.tensor_tensor(out=dst, in0=t1f[st][:], in1=t2f[st][:], op=ALU.add),
                     r=[("t1f", st), ("t2f", st)], w=[dk])
            for tl in range(4):
                t = blk * 4 + tl
                for kc in range(8):
                    T.op(PE, lambda kc=kc, tl=tl: nc.tensor.matmul(
                        PS[tl % 2][:, :], lhsT=xb[xbb][:, kc, tl * 128:(tl + 1) * 128], rhs=wqkv[:, kc, 1024:1536], start=(kc == 0), stop=(kc == 7)),
                        r=[("wqkv",), ("xbA", xbb)], w=[("ps", tl % 2)])
                T.op(ACT, lambda t=t, tl=tl: nc.scalar.copy(out=v1[:, t, :, :].rearrange("p h d -> p (h d)"), in_=PS[tl % 2][:, :]),
                     r=[("ps", tl % 2)], w=[("v1", t)])
            for gi in range(2):
                for kc in range(8):
                    T.op(PE, lambda kc=kc, gi=gi: nc.tensor.matmul(
                        PS[1][0:4, :], lhsT=wgt[:, kc, 4 * gi:4 * gi + 4], rhs=xb[xbb][:, kc, :], start=(kc == 0), stop=(kc == 7)),
                        r=["wgt", ("xbA", xbb)], w=[("ps", 1)])
                if gi == 0:
                    T.op(ACT, lambda: nc.scalar.activation(out=gst[0][xbb][:], in_=PS[1][0:4, :], func=AF.Identity, bias=bg[:, 0:1]),
                         r=[("ps", 1), "bg"], w=[("gst", 0, xbb)])
                    T.dma(SP, gi_d[:, bs], gst[0][xbb][:], r=[("gst", 0, xbb)], w=[("gi_d", blk)])
                else:
                    T.op(ACT, lambda: nc.scalar.activation(out=gst[1][xbb][:], in_=PS[1][0:4, :], func=AF.Exp, bias=nbf[:], scale=-1.0),
                         r=[("ps", 1), "nbf"], w=[("gst", 1, xbb)])
                    T.dma(SP, gf_d[:, bs], gst[1][xbb][:], r=[("gst", 1, xbb)], w=[("gf_d", blk)])
            for hh in range(4):
                nkt = blk * 4 + 4
                prev = None

                def pv_step(kt, c0, pbuf):
                    first = (kt == 0)
                    last = (kt == nkt - 1)
                    for m in range(2):
                        T.op(PE, lambda m=m: nc.tensor.matmul(
                            PS[4 + 2 * m][:, c0:512], lhsT=v1[:, kt, hh, :], rhs=Pb[m][pbuf][:, c0:512], start=first, stop=last,
                            skip_group_check=True), r=[("v1", kt), ("P", m, pbuf)], w=[("ps", 4 + 2 * m)])
                        T.op(PE, lambda m=m: nc.tensor.matmul(
                            PS[5 + 2 * m][:, c0:512], lhsT=ones_b, rhs=Pb[m][pbuf][:, c0:512], start=first, stop=last,
                            skip_group_check=True), r=["cstb", ("P", m, pbuf)], w=[("ps", 5 + 2 * m)])

                for kt in range(nkt):
                    ktl = kt - blk * 4
                    c0 = ktl * 128 if ktl > 0 else 0
                    pbuf = kt % 2
                    sbk = 2 * (kt % 2)
                    for m in range(2):
                        T.op(PE, lambda m=m, kt=kt, c0=c0, sbk=sbk: nc.tensor.matmul(
                            PS[sbk + m][:, c0:512], lhsT=kT[64 * m:64 * m + 64, hh, kt * 128:(kt + 1) * 128],
                            rhs=qT[64 * m:64 * m + 64, hh, c0:512], start=True, stop=True),
                            r=[("kT", hh, kt // 4), ("qT", hh)], w=[("ps", sbk + m)])
                    if prev is not None:
                        pv_step(*prev)
                    for m in range(2):
                        T.op(ACT, lambda m=m, c0=c0, pbuf=pbuf, sbk=sbk: nc.scalar.activation(
                            out=Pb[m][pbuf][:, c0:512], in_=PS[sbk + m][:, c0:512], func=AF.Exp, bias=negc, scale=0.125),
                            r=[("ps", sbk + m), "sc3"], w=[("P", m, pbuf)])
                        if ktl >= 0:
                            T.op(POOL, lambda m=m, c0=c0, pbuf=pbuf: nc.gpsimd.affine_select(
                                out=Pb[m][pbuf][:, c0:c0 + 128], in_=Pb[m][pbuf][:, c0:c0 + 128], pattern=[[1, 128]],
                                compare_op=ALU.is_ge, fill=0.0, base=0, channel_multiplier=-1),
                                r=[("P", m, pbuf)], w=[("P", m, pbuf)])
                    prev = (kt, c0, pbuf)
                pv_step(*prev)
                T.op(ACT, lambda: nc.scalar.activation(out=fo[0][:], in_=PS[5][:, :], func=AF.Ln), r=[("ps", 5)], w=[("fo", 0)])
                T.op(ACT, lambda: nc.scalar.activation(out=fo[0][:], in_=fo[0][:], func=AF.Exp, scale=-1.0), r=[("fo", 0)], w=[("fo", 0)])
                T.op(DVE, lambda: nc.vector.tensor_tensor(out=fo[1][:], in0=PS[4][:, :], in1=fo[0][:], op=ALU.mult),
                     r=[("ps", 4), ("fo", 0)], w=[("fo", 1)])
                T.op(ACT, lambda: nc.scalar.activation(out=fo[2][:], in_=PS[7][:, :], func=AF.Ln), r=[("ps", 7)], w=[("fo", 2)])
                T.op(ACT, lambda: nc.scalar.activation(out=fo[0][:], in_=fo[2][:], func=AF.Exp, scale=-1.0), r=[("fo", 2), ("fo", 0)], w=[("fo", 0)])
                T.op(DVE, lambda: nc.vector.tensor_tensor(out=fo[2][:], in0=PS[6][:, :], in1=fo[0][:], op=ALU.mult),
                     r=[("ps", 6), ("fo", 0)], w=[("fo", 2)])
                T.op(DVE, lambda: nc.vector.scalar_tensor_tensor(out=fo[3][:], in0=fo[2][:], scalar=neglam, in1=fo[1][:], op0=ALU.mult, op1=ALU.add),
                     r=[("fo", 1), ("fo", 2), "sc4"], w=[("fo", 3)])
                T.op(ACT, lambda: nc.scalar.activation(out=sqb[0][:], in_=fo[3][:], func=AF.Square), r=[("fo", 3)], w=[("sqb", 0)])
                T.op(PE, lambda: nc.tensor.matmul(PS[0][:, :], lhsT=ones_b, rhs=sqb[0][:], start=True, stop=True),
                     r=[("sqb", 0), "cstb"], w=[("ps", 0)])
                T.op(ACT, lambda: nc.scalar.activation(out=fo[0][:], in_=PS[0][:, :], func=AF.Ln, bias=epsc[:], scale=1.0 / 128),
                     r=[("ps", 0), "epscA"], w=[("fo", 0)])
                T.op(ACT, lambda: nc.scalar.activation(out=fo[0][:], in_=fo[0][:], func=AF.Exp, scale=-0.5), r=[("fo", 0)], w=[("fo", 0)])
                T.op(DVE, lambda: nc.vector.scalar_tensor_tensor(out=hdTb[:, hh, :], in0=fo[3][:], scalar=subw8, in1=fo[0][:], op0=ALU.mult, op1=ALU.mult),
                     r=[("fo", 3), ("fo", 0), "sc2"], w=[("hdTb", hh)])
            T.dma(SP, hdT_v[:, :, bs], hdTb[:], r=[("hdTb",)], w=[("hdT", blk)])
        T.barrier()
    early2.close()
    early2_open[0] = False
    if dbg and "hdT" in dbg_out:
        dump_dram(dbg_out["hdT"], hdT_d, 512, S, BF16)
        dump_dram(dbg_out["irow"], gi_d, 4, S, F32)
        dump_dram(dbg_out["frow"], gf_d, 4, S, F32)
    if stage <= 2:
        persist.close()
        return finish()

    scope_mark("phB")
    wcol = sbp("wcol", [128, NT, 8], F32)
    decb = sbp("decb", [128, 128], F32)
    with ExitStack() as ph:
        def sb(name, shape, dt):
            return ph.enter_context(nc.sbuf_tensor("s_" + name, list(shape), dt))
        irow = sb("irow", [4, S], F32)
        frow = sb("frow", [4, S], F32)
        T.dma(SP, irow[:], gi_d[:, :], w=[("irow",)])
        T.dma(SP, frow[:], gf_d[:, :], w=[("frow",)])
        onesr = sb("onesr", [4, S], F32)
        csr = sb("csr", [4, S], F32)
        nbr = sb("nbr", [4, S], F32)
        gr = sb("gr", [4, S], F32)
        pe_ = sb("pe_", [4, 33], F32)
        G = sb("G", [4, 32], F32)
        Ms = sb("Ms", [4, 32], F32)
        mp = sb("mp", [4, 33], F32)
        dec = sb("dec", [4, 32], F32)
        T.op(ACT, lambda: nc.scalar.activation(out=frow[:], in_=frow[:], func=AF.Ln, bias=1.0), r=[("frow",)], w=[("frow",)])
        T.op(DVE, lambda: nc.vector.memset(onesr[:], 1.0), w=["onesr"])
        T.op(DVE, lambda: nc.vector.tensor_tensor_scan(out=csr[:], data0=onesr[:], data1=frow[:], initial=0.0, op0=ALU.mult, op1=ALU.add),
             r=["onesr", ("frow",)], w=["csr"])
        T.op(DVE, lambda: nc.vector.memset(pe_[:, 0:1], 0.0), w=[("pe_", 0)])
        T.op(DVE, lambda: nc.vector.tensor_copy(out=pe_[:, 1:33], in_=csr[:].rearrange("p (j s) -> p j s", s=128)[:, :, 127]),
             r=["csr"], w=[("pe_", 1)])
        T.op(DVE, lambda: nc.vector.tensor_tensor(out=nbr[:].rearrange("p (j s) -> p j s", s=128), in0=csr[:].rearrange("p (j s) -> p j s", s=128),
                                                  in1=pe_[:, 0:32].unsqueeze(2).to_broadcast([4, 32, 128]), op=ALU.subtract),
             r=["csr", ("pe_",)], w=["nbr"])
        T.op(DVE, lambda: nc.vector.tensor_tensor(out=gr[:], in0=irow[:], in1=nbr[:], op=ALU.add), r=[("irow",), "nbr"], w=["gr"])
        T.op(DVE, lambda: nc.vector.tensor_reduce(out=G[:], in_=gr[:].rearrange("p (j s) -> p j s", s=128), axis=AX.X, op=ALU.max),
             r=["gr"], w=["G"])
        T.op(DVE, lambda: nc.vector.memset(mp[:, 0:1], 0.0), w=[("mp", 0)])
        nbl = nbr[:].rearrange("p (j s) -> p j s", s=128)[:, :, 127]
        for j in range(NT):
            T.op(DVE, lambda j=j: nc.vector.tensor_tensor(out=Ms[:, j:j + 1], in0=mp[:, j:j + 1], in1=G[:, j:j + 1], op=ALU.max),
                 r=[("mp", j), "G"], w=[("Ms", j)])
            T.op(DVE, lambda j=j: nc.vector.tensor_tensor(out=mp[:, j + 1:j + 2], in0=Ms[:, j:j + 1], in1=nbl[:, j:j + 1], op=ALU.subtract),
                 r=[("Ms", j), "nbr"], w=[("mp", j + 1)])
        Msb = Ms[:].unsqueeze(2).to_broadcast([4, 32, 128])
        T.op(DVE, lambda: nc.vector.tensor_tensor(out=gr[:].rearrange("p (j s) -> p j s", s=128), in0=gr[:].rearrange("p (j s) -> p j s", s=128),
                                                  in1=Msb, op=ALU.subtract), r=["gr", ("Ms",)], w=["gr"])
        T.op(DVE, lambda: nc.vector.tensor_tensor(out=nbr[:].rearrange("p (j s) -> p j s", s=128), in0=nbr[:].rearrange("p (j s) -> p j s", s=128),
                                                  in1=Msb, op=ALU.subtract), r=["nbr", ("Ms",)], w=["nbr"])
        T.op(ACT, lambda: nc.scalar.activation(out=gr[:], in_=gr[:], func=AF.Exp), r=["gr"], w=["gr"])
        T.op(ACT, lambda: nc.scalar.activation(out=nbr[:], in_=nbr[:], func=AF.Exp), r=["nbr"], w=["nbr"])
        T.op(DVE, lambda: nc.vector.tensor_scalar(out=gr[:], in0=gr[:], scalar1=128.0 ** -0.5, scalar2=None, op0=ALU.mult), r=["gr"], w=["gr"])
        T.op(DVE, lambda: nc.vector.tensor_tensor(out=dec[:], in0=mp[:, 0:32], in1=Ms[:], op=ALU.subtract), r=[("mp",), ("Ms",)], w=["dec"])
        T.op(ACT, lambda: nc.scalar.activation(out=dec[:], in_=dec[:], func=AF.Exp), r=["dec"], w=["dec"])
        T.dma(SP, dec_d[:, :], dec[:], r=["dec"], w=["dec_d"])
        T.dma(SP, decb[:], dec_d.rearrange("h j -> (h j)").unsqueeze(0).partition_broadcast(128), r=["dec_d"], w=["decb"])
        wv = PS[0][:, 0:256].rearrange("p (t e) -> p t e", e=8)
        for t in range(NT):
            T.op(PE, lambda t=t: nc.tensor.transpose(wv[:, t, 0:4], gr[:, t * 128:(t + 1) * 128], ident_f[0:4, 0:4]),
                 r=["gr", "cst"], w=[("ps", 0, t, 0)])
            T.op(PE, lambda t=t: nc.tensor.transpose(wv[:, t, 4:8], nbr[:, t * 128:(t + 1) * 128], ident_f[0:4, 0:4]),
                 r=["nbr", "cst"], w=[("ps", 0, t, 1)])
        T.op(DVE, lambda: nc.vector.tensor_copy(out=wcol[:], in_=wv), r=[("ps", 0)], w=["wcol"])
        T.barrier()
    if dbg and "wcol" in dbg_out:
        T.dma(SP, dbg_out["wcol"][:, :], wcol[:].rearrange("p t e -> p (t e)"), r=["wcol"])
        T.dma(SP, dbg_out["decb"][:, :], decb[:], r=["decb"])
    if stage <= 3:
        persist.close()
        return finish()

    scope_mark("phC")
    with ExitStack() as ph:
        def sb(name, shape, dt):
            return ph.enter_context(nc.sbuf_tensor("s_" + name, list(shape), dt))
        wm = sb("wm", [128, 8, 2048], BF16)
        cw = sb("cw", [128, 8, 4], F32)
        cbias = sb("cbias", [128, 8], F32)
        dg = sb("dg", [128, 8, 4, 128], BF16)
        mnwb = sb("mnwb", [128, 512], F32)
        epsc = sb("epscC", [128, 1], F32)
        xb = [sb(f"xbC{i}", [128, 8, 512], BF16) for i in range(2)]
        pre = sb("pre", [128, 8, 516], BF16)
        qkc = sb("qkc", [128, 8, 512], BF16)
        vm1 = [sb(f"vm1_{i}", [128, 4, 130], BF16) for i in range(2)]
        so = [sb(f"so{i}", [128, 512], F32) for i in range(2)]
        ST = [sb(f"ST{i}", [128, 128], BF16) for i in range(4)]
        kw = [sb(f"kw{i}", [128, 128], BF16) for i in range(4)]
        Cst = sb("Cst", [128, 4, 130], F32)
        Cd = sb("Cd", [128, 4, 130], BF16)
        hbuf = sb("hbuf", [128, 512], F32)
        sqh = sb("sqh", [128, 512], F32)
        hmb = sb("hmb", [128, 512], BF16)
        hmTb = [sb(f"hmTb{i}", [128, 4, 512], BF16) for i in range(2)]
        dn = sb("dn", [128, 8], F32)
        ssh = sb("ssh", [128, 8], F32)

        T.op(DVE, lambda: nc.vector.memset(epsc[:], EPS), w=["epscC"])
        T.dma(POOL, wm[:, 0:4, :], w_in_v[:, 0:4, 0:2048], w=[("wm", 0)], max_dma_last_dim=8192)
        T.dma(POOL, wm[:, 4:8, :], w_in_v[:, 4:8, 0:2048], w=[("wm", 1)], max_dma_last_dim=8192)
        T.dma(SP, cw[:], convw_in[:, :, :], w=["cw"])
        T.dma(SP, cbias[:], convb_in[:, :], w=["cbias"])
        T.dma(SP, mnwb[:], mnw_in[0:1, :].partition_broadcast(128), w=["mnwb"])
        for c in range(8):
            for j in range(4):
                T.op(DVE, lambda c=c, j=j: nc.vector.tensor_scalar(out=dg[:, c, j, :], in0=ident_f, scalar1=cw[:, c, j:j + 1], scalar2=None, op0=ALU.mult),
                     r=["cw", "cst"], w=[("dg", c, j)])
        T.op(DVE, lambda: nc.vector.memset(pre[:, :, 0:4], 0.0), w=[("pre", "halo")])
        T.op(DVE, lambda: nc.vector.memset(Cst[:], 0.0), w=["Cst"])
        T.op(DVE, lambda: nc.vector.memset(Cd[:], 0.0), w=["Cd"])
        for i in range(2):
            T.op(DVE, lambda i=i: nc.vector.memset(vm1[i][:, :, 128:130], 1.0), w=[("vm1", i)])

        for blk in range(NB):
            xbb = blk % 2
            bs = slice(blk * 512, (blk + 1) * 512)
            T.dma(SP, xb[xbb][:], xnT_v[:, :, bs], w=[("xbC", xbb)])
            for c in range(8):
                pb = c % 2
                for kc in range(8):
                    T.op(PE, lambda kc=kc, c=c, pb=pb: nc.tensor.matmul(
                        PS[pb][:, :], lhsT=wm[:, kc, c * 128:(c + 1) * 128], rhs=xb[xbb][:, kc, :], start=(kc == 0), stop=(kc == 7)),
                        r=[("wm",), ("xbC", xbb)], w=[("ps", pb)])
                if blk > 0:
                    T.op(DVE, lambda c=c: nc.vector.tensor_copy(out=pre[:, c, 1:4], in_=pre[:, c, 513:516]),
                         r=[("pre", c)], w=[("pre", "halo", c)])
                T.op(ACT, lambda c=c, pb=pb: nc.scalar.copy(out=pre[:, c, 4:516], in_=PS[pb][:, :]),
                     r=[("ps", pb), ("pre", "halo", c)], w=[("pre", c)])
                cb2 = 2 + c % 2
                for j in range(4):
                    T.op(PE, lambda c=c, j=j, cb2=cb2: nc.tensor.matmul(
                        PS[cb2][:, :], lhsT=dg[:, c, j, :], rhs=pre[:, c, 1 + j:513 + j], start=(j == 0), stop=(j == 3)),
                        r=[("dg", c), ("pre", c), ("pre", "halo", c)], w=[("ps", cb2)])
                T.op(ACT, lambda c=c, cb2=cb2: nc.scalar.activation(out=qkc[:, c, :], in_=PS[cb2][:, :], func=AF.Silu, bias=cbias[:, c:c + 1]),
                     r=[("ps", cb2), "cbias"], w=[("qkc", c)])
            for tl in range(4):
                t = blk * 4 + tl
                vb = t % 2
                ts_ = slice(tl * 128, (tl + 1) * 128)
                def emit_vo(tl_):
                    t_ = blk * 4 + tl_
                    vb_ = t_ % 2
                    sl_ = slice(tl_ * 128, (tl_ + 1) * 128)
                    for half in range(2):
                        for kc in range(8):
                            T.op(PE, lambda kc=kc, half=half: nc.tensor.matmul(
                                PS[4 + half][:, :], lhsT=xb[xbb][:, kc, sl_], rhs=wm[:, kc, 1024 + half * 512:1536 + half * 512],
                                start=(kc == 0), stop=(kc == 7)), r=[("wm",), ("xbC", xbb)], w=[("ps", 4 + half)])
                    T.op(ACT, lambda: nc.scalar.copy(out=vm1[vb_][:, :, 0:128], in_=PS[4][:, :].rearrange("p (h d) -> p h d", h=4)),
                         r=[("ps", 4)], w=[("vm1", vb_)])
                    T.op(ACT, lambda: nc.scalar.activation(out=so[vb_][:], in_=PS[5][:, :], func=AF.Sigmoid), r=[("ps", 5)], w=[("so", vb_)])

                if tl == 0:
                    emit_vo(0)
                if tl < 3:
                    emit_vo(tl + 1)
                def head_ops(hh):
                    wc = wcol[:, t, hh:hh + 1]
                    cc = wcol[:, t, 4 + hh:5 + hh]
                    dcol = decb[:, hh * 32 + t:hh * 32 + t + 1]
                    if hh % 2 == 0:
                        bA, bK, bU, bN = 6, 1, 7, 0
                    else:
                        bA, bK, bU, bN = 2, 3, 4, 5
                    kps = psb(bK)[:, 0:128]
                    ops = []
                    ops.append(lambda: T.op(PE, lambda: nc.tensor.matmul(PS[bA][:, 0:128], lhsT=qkc[:, 4 + hh, ts_], rhs=qkc[:, hh, ts_], start=True, stop=True),
                                            r=[("qkc", 4 + hh), ("qkc", hh)], w=[("ps", bA)]))
                    ops.append(lambda: T.op(DVE, lambda: nc.vector.scalar_tensor_tensor(
                        out=ST[hh][:], in0=PS[bA][:, 0:128], scalar=wc, in1=tri_f, op0=ALU.mult, op1=ALU.mult),
                        r=[("ps", bA), "wcol", "cst"], w=[("ST", hh)]))
                    ops.append(lambda: T.op(PE, lambda: nc.tensor.transpose(kps, qkc[:, 4 + hh, ts_], ident_b),
                                            r=[("qkc", 4 + hh), "cstb"], w=[("ps", bK)]))
                    ops.append(lambda: T.op(DVE, lambda: nc.vector.tensor_scalar(out=kw[hh][:], in0=kps, scalar1=wc, scalar2=None, op0=ALU.mult),
                                            r=[("ps", bK), "wcol"], w=[("kw", hh)]))
                    ops.append(lambda: T.op(ACT, lambda: nc.scalar.activation(out=Cd[:, hh, :], in_=Cst[:, hh, :], func=AF.Identity, scale=dcol),
                                            r=[("Cst", hh), "decb"], w=[("Cd", hh)]))
                    ops.append(lambda: T.op(PE, lambda: nc.tensor.matmul(PS[bU][:, 0:129], lhsT=kw[hh][:], rhs=vm1[vb][:, hh, 0:129], start=True, stop=True),
                                            r=[("kw", hh), ("vm1", vb)], w=[("ps", bU)]))
                    ops.append(lambda: T.op(PE, lambda: nc.tensor.matmul(PS[bN][:, 0:129], lhsT=ST[hh][:], rhs=vm1[vb][:, hh, 0:129], start=True, stop=False),
                                            r=[("ST", hh), ("vm1", vb)], w=[("ps", bN)]))
                    ops.append(lambda: T.op(PE, lambda: nc.tensor.matmul(PS[bN][:, 0:129], lhsT=qkc[:, hh, ts_], rhs=Cd[:, hh, 0:129], start=False, stop=True),
                                            r=[("qkc", hh), ("Cd", hh)], w=[("ps", bN)]))
                    ops.append(lambda: T.op(DVE, lambda: nc.vector.scalar_tensor_tensor(
                        out=Cst[:, hh, 0:129], in0=Cst[:, hh, 0:129], scalar=dcol, in1=PS[bU][:, 0:129], op0=ALU.mult, op1=ALU.add),
                        r=[("Cst", hh), "decb", ("ps", bU)], w=[("Cst", hh)]))
                    ops.append(lambda: T.op(DVE, lambda: nc.vector.tensor_reduce(
                        out=dn[:, hh:hh + 1], in_=PS[bN][:, 128:129], axis=AX.X, op=ALU.max, apply_absolute_value=True),
                        r=[("ps", bN)], w=[("dn", hh)]))
                    ops.append(lambda: T.op(DVE, lambda: nc.vector.tensor_scalar(
                        out=dn[:, hh:hh + 1], in0=dn[:, hh:hh + 1], scalar1=cc, scalar2=None, op0=ALU.max),
                        r=[("dn", hh), "wcol"], w=[("dn", hh)]))
                    ops.append(lambda: T.op(DVE, lambda: nc.vector.reciprocal(out=dn[:, 4 + hh:5 + hh], in_=dn[:, hh:hh + 1]), r=[("dn", hh)], w=[("dn", 4 + hh)]))
                    ops.append(lambda: T.op(ACT, lambda: nc.scalar.activation(out=hbuf[:, hh * 128:(hh + 1) * 128], in_=PS[bN][:, 0:128], func=AF.Copy,
                                                                              scale=dn[:, 4 + hh:5 + hh]),
                                            r=[("ps", bN), ("dn", 4 + hh)], w=[("hbuf", hh)]))
                    return ops

                for pair in ((0, 1), (2, 3)):
                    pops = [head_ops(hh) for hh in pair]
                    for i in range(len(pops[0])):
                        for po in pops:
                            po[i]()
                T.op(DVE, lambda: nc.vector.tensor_tensor(out=sqh[:], in0=hbuf[:], in1=hbuf[:], op=ALU.mult), r=[("hbuf",)], w=["sqh"])
                T.op(DVE, lambda: nc.vector.tensor_reduce(out=ssh[:, 0:4], in_=sqh[:].rearrange("p (h d) -> p h d", h=4), axis=AX.X, op=ALU.add),
                     r=["sqh"], w=[("ssh", 0)])
                T.op(ACT, lambda: nc.scalar.activation(out=ssh[:, 4:8], in_=ssh[:, 0:4], func=AF.Sqrt, bias=epsc[:], scale=1.0 / 128),
                     r=[("ssh", 0), "epscC"], w=[("ssh", 1)])
                T.op(DVE, lambda: nc.vector.reciprocal(out=ssh[:, 4:8], in_=ssh[:, 4:8]), r=[("ssh", 1)], w=[("ssh", 1)])
                T.op(DVE, lambda: nc.vector.tensor_tensor(out=sqh[:].rearrange("p (h d) -> p h d", h=4), in0=hbuf[:].rearrange("p (h d) -> p h d", h=4),
                                                          in1=ssh[:, 4:8].unsqueeze(2).to_broadcast([128, 4, 128]), op=ALU.mult),
                     r=[("hbuf",), ("ssh", 1)], w=["sqh"])
                T.op(DVE, lambda: nc.vector.tensor_tensor(out=sqh[:], in0=sqh[:], in1=mnwb[:], op=ALU.mult), r=["sqh", "mnwb"], w=["sqh"])
                T.op(DVE, lambda vb=vb: nc.vector.tensor_tensor(out=hmb[:], in0=sqh[:], in1=so[vb][:], op=ALU.mult), r=["sqh", ("so", vb)], w=["hmb"])
                hb = blk % 2
                tp = psb(3).rearrange("p (k t) -> p k t", k=8)
                for hh in range(4):
                    T.op(PE, lambda hh=hh: nc.tensor.transpose(tp[:, hh, :], hmb[:, hh * 128:(hh + 1) * 128], ident_b),
                         r=["hmb", "cstb"], w=[("ps", 3)])
                T.op(ACT, lambda hb=hb: nc.scalar.copy(out=hmTb[hb][:, :, ts_], in_=tp[:, 0:4, :]), r=[("ps", 3)], w=[("hmTb", hb, tl)])
            T.dma(SP, hmT_v[:, :, bs], hmTb[blk % 2][:], r=[("hmTb", blk % 2)], w=[("hmT", blk)])
        T.barrier()
    persist.close()
    if dbg and "hmT" in dbg_out:
        dump_dram(dbg_out["hmT"], hmT_d, 512, S, BF16)
    if stage <= 4:
        return finish()

    scope_mark("phD")
    with ExitStack() as ph:
        def sb(name, shape, dt):
            return ph.enter_context(nc.sbuf_tensor("s_" + name, list(shape), dt))
        wg = sb("wg", [128, 8, 2048], BF16)
        wbm = sb("wbm", [128, 4, D], BF16)
        wbd = sb("wbd", [128, 4, D], BF16)
        wo = sb("wo", [128, 8, D], BF16)
        wstg = [sb(f"wstg{i}", [128, D], F32) for i in range(2)]
        wr = sb("wrt", [128, 8, 36], F32)
        brb = sb("brb", [128, 36], F32)
        xb = [sb(f"xbD{i}", [128, 8, 512], BF16) for i in range(2)]
        hmb_ = [sb(f"hmD{i}", [128, 4, 512], BF16) for i in range(2)]
        hdb_ = [sb(f"hdD{i}", [128, 4, 512], BF16) for i in range(2)]
        sg = [sb(f"sg{i}", [128, 512], F32) for i in range(4)]
        tt = [sb(f"tt{i}", [128, 512], F32) for i in range(4)]
        mT = sb("mT", [128, 8, 512], BF16)
        xt = [sb(f"xtD{i}", [128, D], F32) for i in range(2)]
        x1t = [sb(f"x1t{i}", [128, D], F32) for i in range(2)]
        xhf = sb("xhf", [128, D], F32)
        h2f = sb("h2f", [128, 8, 128], F32)
        x2t = [sb(f"x2t{i}", [128, D], BF16) for i in range(2)]
        ohb = sb("ohb", [128, 32], BF16)
        bufs = dict(ss=sb("ssD", [128, 1], F32), sd=sb("sdD", [128, 1], F32), rs=sb("rsD", [128, 1], F32),
                    junk=sb("junkD", [128, D], BF16), epsc=sb("epscD", [128, 1], F32))
        lg = sb("lg", [128, 36], F32)
        r8 = sb("r8", [128, 80], F32)

        T.op(DVE, lambda: nc.vector.memset(bufs["epsc"][:], EPS), w=["epsc"])
        T.op(DVE, lambda: nc.vector.memset(tot[:], 0.0), w=["tot"])
        T.dma(POOL, wg[:, 0:4, :], w_in_v[:, 0:4, 3592:5640], w=[("wg", 0)], max_dma_last_dim=8192)
        T.dma(POOL, wg[:, 4:8, :], w_in_v[:, 4:8, 3592:5640], w=[("wg", 1)], max_dma_last_dim=8192)
        T.dma(POOL, wbm[:], w_br_m.rearrange("(k p) n -> p k n", p=128), w=["wbm"])
        T.dma(POOL, wbd[:], w_br_d.rearrange("(k p) n -> p k n", p=128), w=["wbd"])
        T.dma(SP, wr[:], wr_in.rearrange("(k p) n -> p k n", p=128), w=["wrt"])
        T.dma(SP, brb[:], br_in[0:1, :].partition_broadcast(128), w=["brb"])
        w_out_v = w_out.rearrange("(k p) n -> p k n", p=128)
        for kc in range(8):
            b = kc % 2
            T.dma(SP, wstg[b][:], w_out_v[:, kc, :], w=[("wstg", b)])
            T.op(DVE, lambda kc=kc, b=b: nc.vector.tensor_tensor(out=wo[:, kc, :], in0=wstg[b][:], in1=g1b[:], op=ALU.mult),
                 r=[("wstg", b), "g1b"], w=[("wo", kc)])

        for blk in range(NB):
            xbb = blk % 2
            bs = slice(blk * 512, (blk + 1) * 512)
            def load_blk(bk):
                bb_ = bk % 2
                sl_ = slice(bk * 512, (bk + 1) * 512)
                T.dma(SP, xb[bb_][:], xnT_v[:, :, sl_], w=[("xbD", bb_)])
                T.dma(SP, hmb_[bb_][:], hmT_v[:, :, sl_], w=[("hmD", bb_)])
                T.dma(SP, hdb_[bb_][:], hdT_v[:, :, sl_], w=[("hdD", bb_)])

            if blk == 0:
                load_blk(0)
                T.dma(SP, xt[0][:], x_in[0:128, :], w=[("xtD", 0)])
            for oc in range(8):
                st = oc % 2
                for which, (colbase, wbr, hsrc, hkey, wkey) in enumerate(((0, wbm, hmb_, "hmD", "wbm"), (1024, wbd, hdb_, "hdD", "wbd"))):
                    gb = 0 + which
                    pbk = 2 + which
                    for kc in range(8):
                        T.op(PE, lambda kc=kc, colbase=colbase, gb=gb: nc.tensor.matmul(
                            PS[gb][:, :], lhsT=wg[:, kc, colbase + oc * 128:colbase + (oc + 1) * 128], rhs=xb[xbb][:, kc, :],
                            start=(kc == 0), stop=(kc == 7)), r=[("wg",), ("xbD", xbb)], w=[("ps", gb)])
                    T.op(ACT, lambda gb=gb, which=which: nc.scalar.activation(out=sg[2 * st + which][:], in_=PS[gb][:, :], func=AF.Sigmoid),
                         r=[("ps", gb)], w=[("sg", 2 * st + which)])
                    for kc in range(4):
                        T.op(PE, lambda kc=kc, wbr=wbr, hsrc=hsrc, pbk=pbk: nc.tensor.matmul(
                            PS[pbk][:, :], lhsT=wbr[:, kc, oc * 128:(oc + 1) * 128], rhs=hsrc[xbb][:, kc, :],
                            start=(kc == 0), stop=(kc == 3)), r=[wkey, (hkey, xbb)], w=[("ps", pbk)])
                    T.op(DVE, lambda which=which, pbk=pbk: nc.vector.tensor_tensor(out=tt[2 * st + which][:], in0=PS[pbk][:, :], in1=sg[2 * st + which][:], op=ALU.mult),
                         r=[("ps", pbk), ("sg", 2 * st + which)], w=[("tt", 2 * st + which)])
                T.op(POOL, lambda st=st: nc.gpsimd.tensor_tensor(out=mT[:, oc, :], in0=tt[2 * st][:], in1=tt[2 * st + 1][:], op=ALU.add),
                     r=[("tt", 2 * st), ("tt", 2 * st + 1)], w=[("mT", oc)])
            for tl in range(4):
                t = blk * 4 + tl
                b = t % 2
                ts_ = slice(tl * 128, (tl + 1) * 128)
                if t + 1 < NT:
                    T.dma(SP, xt[(t + 1) % 2][:], x_in[(t + 1) * 128:(t + 2) * 128, :], w=[("xtD", (t + 1) % 2)])
                if tl == 3 and blk + 1 < NB:
                    load_blk(blk + 1)
                def emit_wout(tl_):
                    sl_ = slice(tl_ * 128, (tl_ + 1) * 128)
                    for half in range(2):
                        for oc in range(8):
                            T.op(PE, lambda oc=oc, half=half: nc.tensor.matmul(
                                PS[4 + half][:, :], lhsT=mT[:, oc, sl_], rhs=wo[:, oc, half * 512:(half + 1) * 512],
                                start=(oc == 0), stop=(oc == 7)), r=[("mT", oc), ("wo",)], w=[("ps", 4 + half)])

                if tl == 0:
                    emit_wout(0)
                for half in range(2):
                    T.op(DVE, lambda half=half, b=b: nc.vector.tensor_tensor(
                        out=x1t[b][:, half * 512:(half + 1) * 512], in0=PS[4 + half][:, :], in1=xt[b][:, half * 512:(half + 1) * 512], op=ALU.add),
                        r=[("ps", 4 + half), ("xtD", b)], w=[("x1t", b, half)])
                T.dma(SP, x1_d[t * 128:(t + 1) * 128, :], x1t[b][:], r=[("x1t", b)], w=[("x1d", t)])
                if tl < 3:
                    emit_wout(tl + 1)
                norm_tile(bufs, x1t[b][:], [("x1t", b), "epsc"], "n2")
                T.op(DVE, lambda b=b: nc.vector.tensor_scalar(out=xhf[:], in0=x1t[b][:], scalar1=bufs["rs"][:], scalar2=None, op0=ALU.mult),
                     r=[("x1t", b), "rs"], w=["xhf"])
                for kc in range(8):
                    T.op(PE, lambda kc=kc: nc.tensor.transpose(PS[6 + kc // 4][:, (kc % 4) * 128:(kc % 4 + 1) * 128], xhf[:, kc * 128:(kc + 1) * 128], ident_f),
                         r=["xhf", "cst"], w=[("ps", 6 + kc // 4, kc % 4)])
                for kc in range(8):
                    T.op(ACT, lambda kc=kc: nc.scalar.activation(
                        out=h2f[:, kc, :], in_=PS[6 + kc // 4][:, (kc % 4) * 128:(kc % 4 + 1) * 128], func=AF.Identity,
                        scale=a2[:, kc:kc + 1], bias=s2[:, kc:kc + 1]), r=[("ps", 6 + kc // 4, kc % 4), "a2", "modc"], w=[("h2f", kc)])
                T.op(DVE, lambda: nc.vector.tensor_tensor(out=xhf[:], in0=xhf[:], in1=a2b[:], op=ALU.mult), r=["xhf", ("a2b",)], w=["xhf"])
                T.op(POOL, lambda b=b: nc.gpsimd.tensor_tensor(out=x2t[b][:], in0=xhf[:], in1=s2b[:], op=ALU.add), r=["xhf", ("s2b",)], w=[("x2t", b)])
                T.dma(SP, X2_d[t * 128:(t + 1) * 128, :], x2t[b][:], r=[("x2t", b)], w=[("X2d", t)])
                for kc in range(8):
                    T.op(PE, lambda kc=kc: nc.tensor.matmul(PS[0][:, 0:36], lhsT=h2f[:, kc, :], rhs=wr[:, kc, :], start=(kc == 0), stop=(kc == 7)),
                         r=[("h2f", kc), "wrt"], w=[("ps", 0)])
                T.op(DVE, lambda: nc.vector.tensor_tensor(out=lg[:], in0=PS[0][:, 0:36], in1=brb[:], op=ALU.add), r=[("ps", 0), "brb"], w=["lg"])
                gl = lg[:, 0:4]
                el = lg[:, 4:36].rearrange("p (g j) -> p g j", g=4)
                R = lambda a_, b_: r8[:, a_:b_]
                T.op(DVE, lambda: nc.vector.tensor_reduce(out=R(0, 1), in_=gl, axis=AX.X, op=ALU.max), r=["lg"], w=[("r8", 0)])
                T.op(DVE, lambda: nc.vector.tensor_scalar(out=R(4, 8), in0=gl, scalar1=R(0, 1), scalar2=None, op0=ALU.is_ge), r=["lg", ("r8", 0)], w=[("r8", 1)])
                T.op(DVE, lambda: nc.vector.tensor_scalar(out=R(1, 2), in0=R(0, 1), scalar1=-1.0, scalar2=None, op0=ALU.mult), r=[("r8", 0)], w=[("r8", 2)])
                T.op(ACT, lambda: nc.scalar.activation(out=R(8, 12), in_=gl, func=AF.Exp, bias=R(1, 2), accum_out=R(2, 3)), r=["lg", ("r8", 2)], w=[("r8", 3)])
                T.op(DVE, lambda: nc.vector.reciprocal(out=R(3, 4), in_=R(2, 3)), r=[("r8", 3)], w=[("r8", 4)])
                T.op(DVE, lambda: nc.vector.tensor_tensor(out=R(16, 48).rearrange("p (g j) -> p g j", g=4), in0=el,
                                                          in1=R(4, 8).unsqueeze(2).to_broadcast([128, 4, 8]), op=ALU.mult),
                     r=["lg", ("r8", 1)], w=[("r8", 5)])
                T.op(DVE, lambda: nc.vector.tensor_reduce(out=R(48, 56), in_=R(16, 48).rearrange("p (g j) -> p j g", g=4), axis=AX.X, op=ALU.add),
                     r=[("r8", 5)], w=[("r8", 6)])
                T.op(DVE, lambda: nc.vector.max(out=R(56, 64), in_=R(48, 56)), r=[("r8", 6)], w=[("r8", 7)])
                T.op(DVE, lambda: nc.vector.tensor_scalar(out=R(64, 72), in0=R(48, 56), scalar1=R(56, 57), scalar2=None, op0=ALU.is_equal),
                     r=[("r8", 6), ("r8", 7)], w=[("r8", 9)])
                T.op(DVE, lambda: nc.vector.tensor_scalar(out=R(72, 80), in0=R(48, 56), scalar1=R(57, 58), scalar2=None, op0=ALU.is_equal),
                     r=[("r8", 6), ("r8", 7)], w=[("r8", 10)])
                T.op(DVE, lambda: nc.vector.tensor_tensor(out=R(1, 2), in0=R(57, 58), in1=R(56, 57), op=ALU.subtract), r=[("r8", 7), ("r8", 3)], w=[("r8", 2)])
                T.op(ACT, lambda: nc.scalar.activation(out=R(2, 3), in_=R(1, 2), func=AF.Exp), r=[("r8", 2), ("r8", 4)], w=[("r8", 3)])
                T.op(DVE, lambda: nc.vector.tensor_scalar(out=R(1, 2), in0=R(2, 3), scalar1=1.0, scalar2=None, op0=ALU.add), r=[("r8", 3)], w=[("r8", 2)])
                T.op(DVE, lambda: nc.vector.reciprocal(out=R(1, 2), in_=R(1, 2)), r=[("r8", 2)], w=[("r8", 2)])
                T.op(DVE, lambda t=t: nc.vector.tensor_tensor(out=w12[:, t, 0:1], in0=R(1, 2), in1=R(3, 4), op=ALU.mult), r=[("r8", 2), ("r8", 4)], w=[("w12", t, 0)])
                T.op(DVE, lambda t=t: nc.vector.tensor_tensor(out=w12[:, t, 1:2], in0=w12[:, t, 0:1], in1=R(2, 3), op=ALU.mult), r=[("w12", t, 0), ("r8", 3)], w=[("w12", t, 1)])
                for g in range(4):
                    T.op(DVE, lambda g=g, t=t: nc.vector.tensor_scalar(out=m1_all[:, t, g * 8:(g + 1) * 8], in0=R(64, 72), scalar1=R(4 + g, 5 + g), scalar2=None, op0=ALU.mult),
                         r=[("r8", 9), ("r8", 1)], w=[("m1", t, g)])
                    T.op(DVE, lambda g=g, t=t: nc.vector.tensor_scalar(out=m2_all[:, t, g * 8:(g + 1) * 8], in0=R(72, 80), scalar1=R(4 + g, 5 + g), scalar2=None, op0=ALU.mult),
                         r=[("r8", 10), ("r8", 1)], w=[("m2", t, g)])
                T.op(DVE, lambda t=t: nc.vector.tensor_tensor(out=ohb[:], in0=m1_all[:, t, :], in1=m2_all[:, t, :], op=ALU.add),
                     r=[("m1", t), ("m2", t)], w=["ohb"])
                T.op(PE, lambda: nc.tensor.matmul(PS[1][:, 0:32], lhsT=tri_b, rhs=ohb[:], start=True, stop=True), r=["ohb", "cstb"], w=[("ps", 1)])
                T.op(DVE, lambda t=t: nc.vector.tensor_tensor(out=rank_all[:, t, :], in0=PS[1][:, 0:32], in1=tot[:], op=ALU.add),
                     r=[("ps", 1), "tot"], w=[("rank", t)])
                T.op(DVE, lambda t=t: nc.vector.tensor_tensor(out=rank_all[:, t, :], in0=rank_all[:, t, :], in1=ohb[:], op=ALU.subtract),
                     r=[("rank", t), "ohb"], w=[("rank", t)])
                T.op(PE, lambda: nc.tensor.matmul(PS[1][:, 0:32], lhsT=ones_b, rhs=ohb[:], start=True, stop=True), r=["ohb", "cstb"], w=[("ps", 1)])
                T.op(DVE, lambda: nc.vector.tensor_tensor(out=tot[:], in0=PS[1][:, 0:32], in1=tot[:], op=ALU.add), r=[("ps", 1), "tot"], w=["tot"])
        T.barrier()
    if dbg and "x1" in dbg_out:
        dump_dram(dbg_out["x1"], x1_d, S, D, F32)
        T.dma(SP, dbg_out["tot"][:, :], tot[:], r=["tot"])
        T.dma(SP, dbg_out["rank"][:, :], rank_all[:].rearrange("p t e -> p (t e)"), r=[("rank",)])
        T.dma(SP, dbg_out["m1"][:, :], m1_all[:].rearrange("p t e -> p (t e)"), r=[("m1",)])
        T.dma(SP, dbg_out["m2"][:, :], m2_all[:].rearrange("p t e -> p (t e)"), r=[("m2",)])
        T.dma(SP, dbg_out["w12"][:, :], w12[:].rearrange("p t e -> p (t e)"), r=[("w12",)])
    if stage <= 5:
        return finish()

    scope_mark("phE")
    with ExitStack() as ph:
        def sb(name, shape, dt):
            return ph.enter_context(nc.sbuf_tensor(name, list(shape), dt))
        P12 = sb("P12", [128, 2, NT], I32)
        NSPL = int(os.environ.get("KSPLIT", 1))
        widx = sb("widx", [128, NSPL, NSLOT], I32)
        with ExitStack() as ph2:
            def sb2(name, shape, dt):
                return ph2.enter_context(nc.sbuf_tensor("s_" + name, list(shape), dt))
            ni = sb2("ni", [128, 32], I32)
            ntf = sb2("ntf", [128, 32], F32)
            onesr = sb2("ones32", [128, 32], F32)
            tend = sb2("tend", [128, 32], F32)
            off = sb2("off", [128, 32], F32)
            big = sb2("big", [128, NT, 32], F32)
            pf = sb2("pf", [128, 2, NT], F32)
            cmp3 = sb2("cmp3", [128, NSLOT, 32], F32)
            sidx = sb2("sidx", [128, NSLOT], F32)
            esl = sb2("esl", [128, NSLOT], F32)
            pio = sb2("pio", [128, 1], F32)
            pioi = sb2("pioi", [128, 1], I32)
            T.op(DVE, lambda: nc.vector.tensor_scalar(out=ntf[:], in0=tot[:], scalar1=127.0, scalar2=None, op0=ALU.add), r=["tot"], w=["ntf"])
            T.op(DVE, lambda: nc.vector.tensor_copy(out=ni[:], in_=ntf[:]), r=["ntf"], w=["ni"])
            T.op(DVE, lambda: nc.vector.tensor_scalar(out=ni[:], in0=ni[:], scalar1=7, scalar2=None, op0=ALU.arith_shift_right),
                 r=["ni"], w=["ni"])
            T.op(DVE, lambda: nc.vector.tensor_copy(out=ntf[:], in_=ni[:]), r=["ni"], w=["ntf"])
            T.op(DVE, lambda: nc.vector.memset(onesr[:], 1.0), w=["ones32"])
            T.op(DVE, lambda: nc.vector.tensor_tensor_scan(out=tend[:], data0=onesr[:], data1=ntf[:], initial=0.0, op0=ALU.mult, op1=ALU.add),
                 r=["ones32", "ntf"], w=["tend"])
            T.op(DVE, lambda: nc.vector.tensor_tensor(out=off[:], in0=tend[:], in1=ntf[:], op=ALU.subtract), r=["tend", "ntf"], w=["off"])
            T.op(DVE, lambda: nc.vector.tensor_scalar(out=off[:], in0=off[:], scalar1=128.0, scalar2=None, op0=ALU.mult), r=["off"], w=["off"])
            for k, mk in enumerate((m1_all, m2_all)):
                T.op(DVE, lambda: nc.vector.tensor_tensor(out=big[:], in0=rank_all[:], in1=off[:].unsqueeze(1).to_broadcast([128, NT, 32]), op=ALU.add),
                     r=[("rank",), "off"], w=["big"])
                T.op(DVE, lambda mk=mk: nc.vector.tensor_tensor(out=big[:], in0=big[:], in1=mk[:], op=ALU.mult), r=["big", ("m1",), ("m2",)], w=["big"])
                T.op(DVE, lambda k=k: nc.vector.tensor_reduce(out=pf[:, k, :], in_=big[:], axis=AX.X, op=ALU.add), r=["big"], w=[("pf", k)])
            T.op(DVE, lambda: nc.vector.tensor_scalar(out=pf[:], in0=pf[:], scalar1=0.0, scalar2=float(NSLOT * 128 - 1), op0=ALU.max, op1=ALU.min),
                 r=[("pf",)], w=[("pf",)])
            T.op(DVE, lambda: nc.vector.tensor_copy(out=P12[:], in_=pf[:]), r=[("pf",)], w=["P12"])
            T.op(POOL, lambda: nc.gpsimd.iota(out=sidx[:], pattern=[[1, NSLOT]], base=0, channel_multiplier=0, allow_small_or_imprecise_dtypes=True), w=["sidx"])
            T.op(POOL, lambda: nc.gpsimd.iota(out=pio[:], pattern=[[1, 1]], base=0, channel_multiplier=1, allow_small_or_imprecise_dtypes=True), w=["pio"])
            T.op(DVE, lambda: nc.vector.tensor_tensor(out=cmp3[:], in0=tend[:].unsqueeze(1).to_broadcast([128, NSLOT, 32]),
                                                      in1=sidx[:].unsqueeze(2).to_broadcast([128, NSLOT, 32]), op=ALU.is_le),
                 r=["tend", "sidx"], w=["cmp3"])
            T.op(DVE, lambda: nc.vector.tensor_reduce(out=esl[:], in_=cmp3[:], axis=AX.X, op=ALU.add), r=["cmp3"], w=["esl"])
            T.op(DVE, lambda: nc.vector.tensor_scalar(out=esl[:], in0=esl[:], scalar1=31.0, scalar2=128.0, op0=ALU.min, op1=ALU.mult), r=["esl"], w=["esl"])
            T.op(DVE, lambda: nc.vector.tensor_scalar(out=esl[:], in0=esl[:], scalar1=pio[:], scalar2=None, op0=ALU.add), r=["esl", "pio"], w=["esl"])
            for ci in range(NSPL):
                T.op(DVE, lambda ci=ci: nc.vector.tensor_scalar(out=sidx[:], in0=esl[:], scalar1=float(NSPL), scalar2=float(ci), op0=ALU.mult, op1=ALU.add),
                     r=["esl", "cmp3"], w=["sidx"])
                T.op(DVE, lambda ci=ci: nc.vector.tensor_copy(out=widx[:, ci, :], in_=sidx[:]), r=["sidx"], w=[("widx", ci)])
            T.barrier()
        if dbg and "P12" in dbg_out:
            T.dma(SP, dbg_out["P12"][:, :], P12[:].rearrange("p k t -> p (k t)"), r=["P12"])
            T.dma(SP, dbg_out["widx"][:, :], widx[:, 0, :], r=["widx"])

        KE = int(os.environ.get("KE", 9))
        if KE >= 2:
            x2l = [sb(f"x2l{i}", [128, D], BF16) for i in range(3)]
            for t in range(NT):
                b = t % 3
                T.dma(SP, x2l[b][:], X2_d[t * 128:(t + 1) * 128, :], w=[("x2l", b)])
                for k in range(2):
                    T.dmai(Xs_d[:, :], x2l[b][:], out_idx=P12[:, k, t:t + 1], r=[("x2l", b), "P12"], w=[("Xs", t, k)])
            T.barrier()

        if KE >= 3:
            xs = [sb(f"xs{i}", [128, D], BF16) for i in range(3)]
            wsl = [sb(f"wsl{i}", [128, 6144], BF16) for i in range(3)]
            xT = [sb(f"xTs{i}", [128, 8, 128], BF16) for i in range(2)]
            sS = [sb(f"sS{i}", [128, 256], F32) for i in range(2)]
            ac = [sb(f"ac{i}", [128, 256], BF16) for i in range(2)]
            aT = [sb(f"aTs{i}", [128, 2, 128], BF16) for i in range(2)]
            yo = [sb(f"yo{i}", [128, D], BF16) for i in range(2)]
            NSL = int(os.environ.get("KSLOTS", NSLOT))

            def load_slot(sl):
                b3 = sl % 3
                T.dma(SP, xs[b3][:], Xs_d[sl * 128:(sl + 1) * 128, :], r=[("Xs",)], w=[("xs", b3)])
                cw_ = 6144 // NSPL
                for ci in range(NSPL):
                    T.dmai(wsl[b3][:, ci * cw_:(ci + 1) * cw_], Wcat_d.rearrange("r (c w) -> (r c) w", c=NSPL), in_idx=widx[:, ci, sl:sl + 1],
                           r=[("Wcat",), "widx"], w=[("wsl", b3, ci)])

            def slot_s1(sl):
                b2, b3 = sl % 2, sl % 3
                base = 4 * b2
                xTp = psb(base).rearrange("p (k t) -> p k t", k=8)
                for kc in range(8):
                    T.op(PE, lambda kc=kc: nc.tensor.transpose(xTp[:, kc, :], xs[b3][:, kc * 128:(kc + 1) * 128], ident_b),
                         r=[("xs", b3), "cstb"], w=[("ps", base)])
                T.op(ACT, lambda: nc.scalar.copy(out=xT[b2][:, 0:4, :], in_=xTp[:, 0:4, :]), r=[("ps", base)], w=[("xTs", b2, 0)])
                T.op(DVE, lambda: nc.vector.tensor_copy(out=xT[b2][:, 4:8, :], in_=xTp[:, 4:8, :]), r=[("ps", base), ("xTs", b2, 0)], w=[("xTs", b2, 1)])
                for kc in range(8):
                    T.op(PE, lambda kc=kc: nc.tensor.matmul(PS[base + 1][:, :], lhsT=xT[b2][:, kc, :], rhs=wsl[b3][:, kc * 512:(kc + 1) * 512],
                                                            start=(kc == 0), stop=(kc == 7)), r=[("xTs", b2), ("wsl", b3)], w=[("ps", base + 1)])

            def slot_s2(sl):
                b2, b3 = sl % 2, sl % 3
                base = 4 * b2
                T.op(ACT, lambda: nc.scalar.activation(out=sS[b2][:], in_=PS[base + 1][:, 0:256], func=AF.Silu), r=[("ps", base + 1)], w=[("sS", b2)])
                T.op(DVE, lambda: nc.vector.tensor_tensor(out=ac[b2][:], in0=PS[base + 1][:, 256:512], in1=sS[b2][:], op=ALU.mult),
                     r=[("ps", base + 1), ("sS", b2)], w=[("ac", b2)])
                aTp = psb(base)[:, 0:256].rearrange("p (k t) -> p k t", k=2)
                for fc in range(2):
                    T.op(PE, lambda fc=fc: nc.tensor.transpose(aTp[:, fc, :], ac[b2][:, fc * 128:(fc + 1) * 128], ident_b),
                         r=[("ac", b2), "cstb"], w=[("ps", base)])
                T.op(ACT, lambda: nc.scalar.copy(out=aT[b2][:], in_=aTp), r=[("ps", base)], w=[("aTs", b2)])
                for cb_ in range(2):
                    for fc in range(2):
                        T.op(PE, lambda fc=fc, cb_=cb_: nc.tensor.matmul(
                            PS[base + 2 + cb_][:, :], lhsT=aT[b2][:, fc, :], rhs=wsl[b3][:, 4096 + fc * 1024 + cb_ * 512: 4096 + fc * 1024 + (cb_ + 1) * 512],
                            start=(fc == 0), stop=(fc == 1)), r=[("aTs", b2), ("wsl", b3)], w=[("ps", base + 2 + cb_)])
                T.op(ACT, lambda: nc.scalar.copy(out=yo[b2][:, 0:512], in_=PS[base + 2][:, :]), r=[("ps", base + 2)], w=[("yo", b2, 0)])
                T.op(DVE, lambda: nc.vector.tensor_copy(out=yo[b2][:, 512:1024], in_=PS[base + 3][:, :]), r=[("ps", base + 3)], w=[("yo", b2, 1)])
                T.dma(SP, Ys_d[sl * 128:(sl + 1) * 128, :], yo[b2][:], r=[("yo", b2)], w=[("Ys", sl)])

            for sl in range(min(2, NSL)):
                load_slot(sl)
            slot_s1(0)
            for sl in range(NSL):
                if sl + 2 < NSL:
                    load_slot(sl + 2)
                if sl + 1 < NSL:
                    slot_s1(sl + 1)
                slot_s2(sl)
            T.barrier()

        if KE >= 4:
            y1 = [sb(f"y1_{i}", [128, D], BF16) for i in range(3)]
            ya = [sb(f"ya_{i}", [128, D], F32) for i in range(3)]
            y2 = [sb(f"y2_{i}", [128, D], BF16) for i in range(3)]
            x1l = [sb(f"x1l{i}", [128, D], F32) for i in range(3)]
            def load_tok(t):
                b = t % 3
                T.dma(SP, x1l[b][:], x1_d[t * 128:(t + 1) * 128, :], w=[("x1l", b)])
                T.dmai(y1[b][:], Ys_d[:, :], in_idx=P12[:, 0, t:t + 1], r=[("Ys",), "P12"], w=[("y1", b)])
                T.dmai(y2[b][:], Ys_d[:, :], in_idx=P12[:, 1, t:t + 1], r=[("Ys",), "P12"], w=[("y2", b)])

            load_tok(0)
            load_tok(1)
            for t in range(NT):
                b = t % 3
                if t + 2 < NT:
                    load_tok(t + 2)
                T.op(DVE, lambda b=b, t=t: nc.vector.tensor_scalar(out=ya[b][:], in0=y1[b][:], scalar1=w12[:, t, 0:1], scalar2=None, op0=ALU.mult),
                     r=[("y1", b), ("w12",)], w=[("ya", b)])
                T.op(DVE, lambda b=b, t=t: nc.vector.scalar_tensor_tensor(out=ya[b][:], in0=y2[b][:], scalar=w12[:, t, 1:2], in1=ya[b][:], op0=ALU.mult, op1=ALU.add),
                     r=[("ya", b), ("y2", b), ("w12",)], w=[("ya", b)])
                T.op(DVE, lambda b=b: nc.vector.tensor_tensor(out=ya[b][:], in0=ya[b][:], in1=g2b[:], op=ALU.mult), r=[("ya", b), ("g2b",)], w=[("ya", b)])
                T.op(DVE, lambda b=b: nc.vector.tensor_tensor(out=x1l[b][:], in0=x1l[b][:], in1=ya[b][:], op=ALU.add), r=[("ya", b), ("x1l", b)], w=[("x1l", b)])
                T.dma(SP, out_d[t * 128:(t + 1) * 128, :], x1l[b][:], r=[("x1l", b)], w=[("out", t)])
            T.barrier()
    return finish()


def _consts():
    c = np.zeros((128, NCST), np.float32)
    c[:, 0:128] = np.eye(128, dtype=np.float32)
    s = np.arange(128)[:, None]
    t = np.arange(128)[None, :]
    c[:, 128:256] = (s <= t).astype(np.float32)
    c[:, 256:384] = ((s // 64) == (t // 64)).astype(np.float32)
    R = np.zeros((128, 128), np.float32)
    for base in (0, 64):
        for d in range(8):
            R[base + d, base + d + 8] = -1.0
            R[base + d + 8, base + d] = 1.0
    c[:, 384:512] = R.T
    c[:, 512:640] = 1.0
    inv = 500000.0 ** (-np.arange(0, 16, 2, dtype=np.float32) / 16.0)
    for p in range(128):
        d = p % 64
        c[p, 640] = inv[d % 8] if d < 16 else 0.0
    return c


_NC_CACHE = {}


def make_in_maps(inputs):
    f = lambda k: np.ascontiguousarray(np.asarray(inputs[k], dtype=np.float32))
    x = f("x")
    c = f("c")
    pos = np.ascontiguousarray(np.asarray(inputs["positions"], dtype=np.int32))
    b_ada = f("b_ada")[0]
    shared = {
        "w_ada": f("w_ada")[0],
        "b_ada_col": np.ascontiguousarray(b_ada.reshape(48, 128).T),
        "b_ada_row": np.ascontiguousarray(b_ada.reshape(1, -1)),
        "n1w": np.ascontiguousarray(f("norm1_w")[0].reshape(8, 128).T),
        "n2w": np.ascontiguousarray(f("norm2_w")[0].reshape(8, 128).T),
        "n2row": np.ascontiguousarray(f("norm2_w")[0].reshape(1, D)),
        "w_in": f("w_in")[0],
        "bgate": np.ascontiguousarray(np.stack([f("b_igate")[0], f("b_fgate")[0]], axis=1)),
        "convw": np.ascontiguousarray(f("conv_w")[0].T.reshape(8, 128, 4).transpose(1, 0, 2)),
        "convb": np.ascontiguousarray(f("conv_b")[0].reshape(8, 128).T),
        "mnw": np.ascontiguousarray(f("mlstm_norm_w")[0].reshape(1, 512)),
        "qnw": np.ascontiguousarray(np.tile(f("q_norm_w")[0], 2).reshape(128, 1)),
        "knw": np.ascontiguousarray(np.tile(f("k_norm_w")[0], 2).reshape(128, 1)),
        "qkrow": np.ascontiguousarray(np.concatenate([f("q_norm_w")[0], f("k_norm_w")[0]]).reshape(1, 128)),
        "lamv": np.ascontiguousarray(np.concatenate([f("lam_q1")[0], f("lam_k1")[0], f("lam_q2")[0], f("lam_k2")[0]]).reshape(1, 256)),
        "subw": np.ascontiguousarray(f("subln_w")[0].reshape(128, 1)),
        "w_br_m": f("w_br_m")[0],
        "w_br_d": f("w_br_d")[0],
        "w_out": f("w_out")[0],
        "wr": np.ascontiguousarray(np.concatenate([f("w_rg")[0], f("w_re")[0]], axis=1)),
        "br": np.ascontiguousarray(np.concatenate([f("b_rg")[0], f("b_re")[0]]).reshape(1, 36)),
        "w_gate": f("w_gate")[0],
        "w_up": f("w_up")[0],
        "w_down": f("w_down")[0],
        "consts": _consts(),
    }
    maps = []
    for b in range(8):
        m = dict(shared)
        m["x"] = x[b]
        m["cT"] = np.ascontiguousarray(c[b].reshape(8, 128).T)
        m["pos"] = np.ascontiguousarray(pos[b].reshape(1, S))
        maps.append(m)
    return maps


def kernel(**inputs):
    if "nc" not in _NC_CACHE:
        _NC_CACHE["nc"] = build()
    nc = _NC_CACHE["nc"]
    maps = make_in_maps(inputs)
    res = run_bass_kernel_spmd(nc, maps, core_ids=list(range(8)))
    out = np.stack([np.asarray(r["out"], dtype=np.float32) for r in res.results], axis=0)
    return out
```

```python
import math
import os
from contextlib import ExitStack

import numpy as np
import concourse.bass as bass
import concourse.mybir as mybir
from concourse.bass_utils import run_bass_kernel_spmd

F32 = mybir.dt.float32
BF16 = mybir.dt.bfloat16
I32 = mybir.dt.int32
AF = mybir.ActivationFunctionType
ALU = mybir.AluOpType
AX = mybir.AxisListType

S = 4096
D = 1024
NT = 32
NB = 8
EPS = 1e-6
NCST = 5 * 128 + 2


class _StopBuild(Exception):
    pass


class Trk:
    LIMIT = 50000
    NDSEM = 12

    def __init__(self, nc):
        self.nc = nc
        self.eng = {"pe": nc.tensor, "act": nc.scalar, "dve": nc.vector, "pool": nc.gpsimd, "sp": nc.sync}
        self.cur = {}
        self.seq = {}
        self.gen = {}
        for e in self.eng:
            self.cur[e] = [nc.alloc_semaphore(f"s_{e}_0"), 0]
            self.seq[e] = 0
            self.gen[e] = 0
        self.allsems = [(e, self.cur[e]) for e in self.eng]
        self.waited = {e: {} for e in self.eng}
        self.lastw = {}
        self.readers = {}
        self.children = {}
        self.dring = {}
        for q in ("sp", "pool", "act", "wc"):
            self.dring[q] = [[nc.alloc_semaphore(f"d_{q}_{i}"), 0] for i in range(self.NDSEM)]
        self.dpos = {q: 0 for q in self.dring}
        self.all_dma = []

    def _related(self, k):
        ks = [k[:i] for i in range(1, len(k) + 1)]
        stack = [k]
        while stack:
            p = stack.pop()
            for c in self.children.get(p, ()):
                ks.append(c)
                stack.append(c)
        return ks

    def _register(self, k):
        for i in range(1, len(k)):
            self.children.setdefault(k[:i], set()).add(k[:i + 1])

    def _wait(self, e, ev):
        sem, val, src, sq = ev
        if src == e:
            if e == "pe":
                return
            if e != "pool" and self.seq[e] - sq >= 6:
                return
        w = self.waited[e]
        if w.get(sem.name, 0) >= val:
            return
        self.eng[e].wait_ge(sem, val)
        w[sem.name] = val

    def _deps(self, e, r, w):
        evs = []
        for k in r:
            for kk in self._related(k):
                if kk in self.lastw:
                    evs.append(self.lastw[kk])
        for k in w:
            for kk in self._related(k):
                if kk in self.lastw:
                    evs.append(self.lastw[kk])
                evs.extend(self.readers.get(kk, {}).values())
        for ev in evs:
            self._wait(e, ev)

    def _record(self, ev, r, w):
        for k in r:
            self._register(k)
            self.readers.setdefault(k, {})[ev[2] if ev[2] is not None else ev[0].name] = ev
        for k in w:
            self._register(k)
            self.lastw[k] = ev
            self.readers[k] = {}
            for kk in self._related(k):
                if kk != k and len(kk) > len(k):
                    self.readers[kk] = {}
                    self.lastw.pop(kk, None)

    @staticmethod
    def _norm(ks):
        out = []
        for k in ks:
            k = tuple(k) if isinstance(k, (tuple, list)) else (k,)
            if k[0] == "ps":
                k = k[:2]
            out.append(k)
        return out

    def start_record(self):
        self._rec = []

    def stop_record(self):
        rec, self._rec = self._rec, None
        return rec

    def op(self, e, fn, r=(), w=()):
        if getattr(self, "_rec", None) is not None:
            self._rec.append((e, fn, r, w))
            return None
        r = self._norm(r)
        w = self._norm(w)
        w = w + [k for k in r if k[0] == "ps" and k not in w]
        self._deps(e, r, w)
        ins = fn()
        c = self.cur[e]
        c[1] += 1
        self.seq[e] += 1
        ins.then_inc(c[0], 1)
        ev = (c[0], c[1], e, self.seq[e])
        self._record(ev, r, w)
        if c[1] >= self.LIMIT:
            self.gen[e] += 1
            self.cur[e] = [self.nc.alloc_semaphore(f"s_{e}_{self.gen[e]}"), 0]
            self.allsems.append((e, self.cur[e]))
        return ins

    def dma(self, q, out, in_, r=(), w=(), ring=None, **kw):
        r = self._norm(r)
        w = self._norm(w)
        rq = ring or q
        ring = self.dring[rq]
        slot = ring[self.dpos[rq] % self.NDSEM]
        self.dpos[rq] += 1
        if slot[1] > 0:
            self._wait(q, (slot[0], slot[1], None, 0))
        self._deps(q, r, w)
        ins = self.eng[q].dma_start(out=out, in_=in_, **kw)
        slot[1] += 16
        ins.then_inc(slot[0], 16)
        ev = (slot[0], slot[1], None, 0)
        self._record(ev, r, w)
        return ins

    def dmai(self, out, in_, out_idx=None, in_idx=None, r=(), w=()):
        q = "pool"
        r = self._norm(r)
        w = self._norm(w)
        ring = self.dring[q]
        slot = ring[self.dpos[q] % self.NDSEM]
        self.dpos[q] += 1
        if slot[1] > 0:
            self._wait(q, (slot[0], slot[1], None, 0))
        self._deps(q, r, w)
        ins = self.nc.gpsimd.indirect_dma_start(
            out=out, out_offset=(bass.IndirectOffsetOnAxis(ap=out_idx, axis=0) if out_idx is not None else None),
            in_=in_, in_offset=(bass.IndirectOffsetOnAxis(ap=in_idx, axis=0) if in_idx is not None else None))
        slot[1] += 16
        ins.then_inc(slot[0], 16)
        ev = (slot[0], slot[1], None, 0)
        self._record(ev, r, w)
        return ins

    def barrier(self, full=True):
        evs = []
        for (se, c) in self.allsems:
            if c[1] > 0:
                evs.append((c[0], c[1], "__" + se, 0))
        for q, ring in self.dring.items():
            if q == "wc" and not full:
                continue
            for slot in ring:
                if slot[1] > 0:
                    evs.append((slot[0], slot[1], None, 0))
        for e in self.eng:
            for ev in evs:
                if ev[2] == "__" + e:
                    continue
                self._wait(e, ev)
        self.lastw.clear()
        self.readers.clear()
        self.children.clear()


def build(stage=99, dbg=None):
    nc = bass.Bass("TRN2", target_bir_lowering=False)
    T = Trk(nc)
    _scope = [None]

    def scope_mark(nm):
        if os.environ.get("KSCOPE") != "1":
            return
        if _scope[0] is not None:
            _scope[0].__exit__(None, None, None)
        _scope[0] = nc.named_scope(nm)
        _scope[0].__enter__()

    PE, ACT, DVE, POOL, SP = "pe", "act", "dve", "pool", "sp"

    def din(name, shape, dt=F32):
        return nc.dram_tensor(name, list(shape), dt, kind="ExternalInput").ap()

    def dscr(name, shape, dt):
        return nc.dram_tensor(name, list(shape), dt, kind="Internal").ap()

    x_in = din("x", [S, D])
    cT_in = din("cT", [128, 8])
    pos_in = din("pos", [1, S], I32)
    w_ada = din("w_ada", [D, 6 * D])
    b_ada_col = din("b_ada_col", [128, 48])
    b_ada_row = din("b_ada_row", [1, 6 * D])
    n1w_in = din("n1w", [128, 8])
    n2w_in = din("n2w", [128, 8])
    n2row_in = din("n2row", [1, D])
    w_in = din("w_in", [D, 5640])
    bgate_in = din("bgate", [4, 2])
    convw_in = din("convw", [128, 8, 4])
    convb_in = din("convb", [128, 8])
    mnw_in = din("mnw", [1, 512])
    qnw_in = din("qnw", [128, 1])
    knw_in = din("knw", [128, 1])
    qkrow_in = din("qkrow", [1, 128])
    lamv_in = din("lamv", [1, 256])
    subw_in = din("subw", [128, 1])
    w_br_m = din("w_br_m", [512, D])
    w_br_d = din("w_br_d", [512, D])
    w_out = din("w_out", [D, D])
    wr_in = din("wr", [D, 36])
    br_in = din("br", [1, 36])
    w_gate = din("w_gate", [32, D, 256])
    w_up = din("w_up", [32, D, 256])
    w_down = din("w_down", [32, 256, D])
    consts_in = din("consts", [128, NCST])
    out_d = nc.dram_tensor("out", [S, D], F32, kind="ExternalOutput").ap()

    xnT_d = dscr("xnT_d", [D, S], BF16)
    hmT_d = dscr("hmT_d", [512, S], BF16)
    hdT_d = dscr("hdT_d", [512, S], BF16)
    x1_d = dscr("x1_d", [S, D], F32)
    xn2T_d = dscr("xn2T_d", [D, S], BF16)
    combT_d = dscr("combT_d", [32, S], BF16)
    dec_d = dscr("dec_d", [4, 32], F32)
    gi_d = dscr("gi_d", [4, S], F32)
    NSLOT = 96
    X2_d = dscr("X2_d", [S, D], BF16)
    Xs_d = dscr("Xs_d", [NSLOT * 128, D], BF16)
    Ys_d = dscr("Ys_d", [NSLOT * 128, D], BF16)
    Wcat_d = dscr("Wcat_d", [32 * 128, 6144], BF16)
    gf_d = dscr("gf_d", [4, S], F32)

    dbg_out = {}
    if dbg:
        for name, (shape, dt) in dbg.items():
            dbg_out[name] = nc.dram_tensor("dbg_" + name, list(shape), dt, kind="ExternalOutput").ap()

    PS = [nc.alloc_psum_tensor(f"ps{i}", [128, 512], F32) for i in range(8)]

    def psb(i):
        return PS[i][:].bitcast(BF16)

    xnT_v = xnT_d.rearrange("(k p) t -> p k t", p=128)
    xn2T_v = xn2T_d.rearrange("(k p) t -> p k t", p=128)
    hmT_v = hmT_d.rearrange("(k p) t -> p k t", p=128)
    hdT_v = hdT_d.rearrange("(k p) t -> p k t", p=128)
    w_in_v = w_in.rearrange("(k p) n -> p k n", p=128)
    wg_v = w_gate.rearrange("e (k p) f -> e p k f", p=128)
    wu_v = w_up.rearrange("e (k p) f -> e p k f", p=128)
    wdn_v = w_down.rearrange("e (k p) n -> e p k n", p=128)

    glob = ExitStack()

    def sbg(name, shape, dt):
        return glob.enter_context(nc.sbuf_tensor("s_" + name, list(shape), dt))

    cst = sbg("cst", [128, NCST], F32)
    cstb = sbg("cstb", [128, 5 * 128], BF16)
    modc = sbg("modc", [128, 48], F32)
    a1 = sbg("a1", [128, 8], F32)
    a2 = sbg("a2", [128, 8], F32)
    g1b = sbg("g1b", [128, D], F32)
    g2b = sbg("g2b", [128, D], F32)
    a2b = sbg("a2b", [128, D], F32)
    s2b = sbg("s2b", [128, D], F32)
    rank_all = sbg("rank_all", [128, NT, 32], F32)
    m1_all = sbg("m1_all", [128, NT, 32], F32)
    m2_all = sbg("m2_all", [128, NT, 32], F32)
    w12 = sbg("w12", [128, NT, 2], F32)
    tot = sbg("tot", [128, 32], F32)
    smallc = sbg("smallc", [128, 16], F32)
    ident_f = cst[:, 0:128]
    tri_f = cst[:, 128:256]
    invf = cst[:, 640:641]
    ident_b = cstb[:, 0:128]
    tri_b = cstb[:, 128:256]
    bo64_b = cstb[:, 256:384]
    RT_b = cstb[:, 384:512]
    ones_b = cstb[:, 512:640]
    ones_f = cst[:, 512:640]

    T.dma(SP, cst[:], consts_in[:, :], w=["cst"])
    T.op(DVE, lambda: nc.vector.tensor_copy(out=cstb[:], in_=cst[:, 0:640]), r=["cst"], w=["cstb"])

    early2 = ExitStack()
    CosB = early2.enter_context(nc.sbuf_tensor("s_CosB", [128, S], BF16))
    SinB = early2.enter_context(nc.sbuf_tensor("s_SinB", [128, S], BF16))
    early2_open = [True]

    def sin_reduce(ph_sb, dst_bf, ang, n, tagk):
        ki = ph_sb["ki"]
        kf = ph_sb["kf"]
        msk = ph_sb["msk"]
        C1 = 6.28125
        C2 = 2.0 * math.pi - 6.28125
        T.op(DVE, lambda: nc.vector.tensor_scalar(out=ki[:, 0:n], in0=ang, scalar1=1.0 / (2.0 * math.pi), scalar2=None, op0=ALU.mult),
             r=[tagk], w=["ki"])
        T.op(DVE, lambda: nc.vector.tensor_copy(out=kf[:, 0:n], in_=ki[:, 0:n]), r=["ki"], w=["kf"])
        T.op(DVE, lambda: nc.vector.scalar_tensor_tensor(out=ang, in0=kf[:, 0:n], scalar=-C1, in1=ang, op0=ALU.mult, op1=ALU.add),
             r=["kf", tagk], w=[tagk])
        T.op(DVE, lambda: nc.vector.scalar_tensor_tensor(out=ang, in0=kf[:, 0:n], scalar=-C2, in1=ang, op0=ALU.mult, op1=ALU.add),
             r=["kf", tagk], w=[tagk])
        T.op(DVE, lambda: nc.vector.tensor_scalar(out=msk[:, 0:n], in0=ang, scalar1=math.pi, scalar2=-2.0 * math.pi, op0=ALU.is_gt, op1=ALU.mult),
             r=[tagk], w=["msk"])
        T.op(DVE, lambda: nc.vector.tensor_tensor(out=ang, in0=ang, in1=msk[:, 0:n], op=ALU.add), r=[tagk, "msk"], w=[tagk])
        T.op(DVE, lambda: nc.vector.tensor_scalar(out=msk[:, 0:n], in0=ang, scalar1=-math.pi, scalar2=2.0 * math.pi, op0=ALU.is_lt, op1=ALU.mult),
             r=[tagk], w=["msk"])
        T.op(DVE, lambda: nc.vector.tensor_tensor(out=ang, in0=ang, in1=msk[:, 0:n], op=ALU.add), r=[tagk, "msk"], w=[tagk])
        T.op(DVE, lambda: nc.vector.tensor_scalar(out=ang, in0=ang, scalar1=math.pi, scalar2=-math.pi, op0=ALU.min, op1=ALU.max),
             r=[tagk], w=[tagk])
        T.op(ACT, lambda: nc.scalar.activation(out=dst_bf, in_=ang, func=AF.Sin), r=[tagk], w=[tagk + "_o"])

    early = ExitStack()
    NWST = int(os.environ.get("KWST", 2))
    wst = [early.enter_context(nc.sbuf_tensor(f"s_wst{i}", [128, 6144], BF16)) for i in range(NWST)]
    for e in range(32):
        b = e % NWST
        gu = wst[b][:, 0:4096].rearrange("p (k f) -> p k f", k=8)
        T.dma(POOL, gu[:, :, 0:256], wg_v[e], w=[("wst", b, 0)], ring="wc")
        T.dma(POOL, gu[:, :, 256:512], wu_v[e], w=[("wst", b, 1)], ring="wc")
        T.dma(POOL, wst[b][:, 4096:6144].rearrange("p (k n) -> p k n", k=2), wdn_v[e], w=[("wst", b, 2)], ring="wc")
        T.dma(POOL, Wcat_d[e * 128:(e + 1) * 128, :], wst[b][:], r=[("wst", b)], w=[("Wcat", e)], ring="wc")
    early_open = [True]
    scope_mark("ph0")
    with ExitStack() as ph:
        def sb(name, shape, dt):
            return ph.enter_context(nc.sbuf_tensor("s_" + name, list(shape), dt))
        cT = sb("cT", [128, 8], F32)
        cT2 = sb("cT2", [128, 8, 2], F32)
        cbc = sb("cbc", [128, 8, 128], F32)
        wa = [sb(f"wa{i}", [128, 8, 512], F32) for i in range(2)]
        bcol = sb("bcol", [128, 48], F32)
        brow = sb("brow", [128, 2048], F32)
        nw = sb("nw", [128, 16], F32)
        tmp8 = sb("tmp8", [128, 8], F32)
        lamb = sb("lamb", [128, 256], F32)
        qkrow = sb("qkrow", [128, 128], F32)
        lt = sb("lt", [128, 128], F32)
        l2 = sb("l2", [128, 4], F32)

        T.dma(SP, cT[:], cT_in[:, :], w=["cT"])
        T.dma(SP, bcol[:], b_ada_col[:, :], w=["bcol"])
        T.dma(SP, brow[:, 0:1024], b_ada_row[0:1, 2048:3072].partition_broadcast(128), w=["brow0"])
        T.dma(SP, brow[:, 1024:2048], b_ada_row[0:1, 5120:6144].partition_broadcast(128), w=["brow1"])
        T.dma(SP, nw[:, 0:8], n1w_in[:, :], w=["nw0"])
        T.dma(SP, nw[:, 8:16], n2w_in[:, :], w=["nw1"])
        T.dma(SP, smallc[:, 0:1], qnw_in[:, :], w=["sc0"])
        T.dma(SP, smallc[:, 1:2], knw_in[:, :], w=["sc1"])
        T.dma(SP, smallc[:, 2:3], subw_in[:, :], w=["sc2"])
        T.dma(SP, lamb[:], lamv_in[0:1, :].partition_broadcast(128), w=["lamb"])
        T.dma(SP, qkrow[:], qkrow_in[0:1, :].partition_broadcast(128), w=["qkrow"])
        for k in range(2):
            T.op(DVE, lambda k=k: nc.vector.tensor_copy(out=cT2[:, :, k], in_=cT[:]), r=["cT"], w=[("cT2", k)])
        for kc in range(8):
            T.op(DVE, lambda kc=kc: nc.vector.tensor_copy(out=cbc[:, kc, :], in_=cT[:, kc:kc + 1].to_broadcast([128, 128])),
                 r=["cT"], w=[("cbc", kc)])
        if True:
            posi = ph.enter_context(nc.sbuf_tensor("s_posi", [128, 1024], I32))
            posf = ph.enter_context(nc.sbuf_tensor("s_posf", [128, 1024], F32))
            ang = ph.enter_context(nc.sbuf_tensor("s_ang", [128, 1024], F32))
            tb = dict(ki=ph.enter_context(nc.sbuf_tensor("s_ki", [128, 1024], I32)),
                      kf=ph.enter_context(nc.sbuf_tensor("s_kf", [128, 1024], F32)),
                      msk=ph.enter_context(nc.sbuf_tensor("s_msk", [128, 1024], F32)))
            for c4 in range(4):
                cs = slice(c4 * 1024, (c4 + 1) * 1024)
                T.dma(SP, posi[:], pos_in[0:1, cs].partition_broadcast(128), w=["posi"])
                T.op(DVE, lambda: nc.vector.tensor_copy(out=posf[:], in_=posi[:]), r=["posi"], w=["posf"])
                T.op(DVE, lambda: nc.vector.tensor_scalar(out=ang[:], in0=posf[:], scalar1=invf, scalar2=None, op0=ALU.mult),
                     r=["posf", "cst"], w=["ang"])
                sin_reduce(tb, SinB[:, cs], ang[:], 1024, "ang")
                T.op(DVE, lambda: nc.vector.tensor_scalar(out=ang[:], in0=posf[:], scalar1=invf, scalar2=math.pi / 2, op0=ALU.mult, op1=ALU.add),
                     r=["posf", "cst", "ang_o"], w=["ang"])
                sin_reduce(tb, CosB[:, cs], ang[:], 1024, "ang")

        w_ada_v = w_ada.rearrange("(k p) n -> p k n", p=128)
        gate_bank = {4: 1, 5: 2, 6: 5, 7: 6, 8: 1, 9: 2, 10: 3, 11: 4}
        n2rb = sb("n2rb", [128, D], F32)
        T.dma(SP, n2rb[:], n2row_in[0:1, :].partition_broadcast(128), w=["n2rb"])
        T.dma(SP, s2b[:], b_ada_row[0:1, 3072:4096].partition_broadcast(128), w=["s2b_bias"])
        T.dma(SP, a2b[:], b_ada_row[0:1, 4096:5120].partition_broadcast(128), w=["a2b_bias"])
        for blk in range(12):
            b = blk % 2
            for hh in range(2):
                T.dma(SP, wa[b][:, 4 * hh:4 * hh + 4, :], w_ada_v[:, 4 * hh:4 * hh + 4, blk * 512:(blk + 1) * 512], w=[("wa", b)])
            for m in range(4):
                j = blk * 4 + m
                for kc in range(8):
                    T.op(PE, lambda j=j, m=m, kc=kc, b=b: nc.tensor.matmul(
                        PS[0][:, 2 * j:2 * j + 2], lhsT=wa[b][:, kc, m * 128:(m + 1) * 128], rhs=cT2[:, kc, :],
                        start=(kc == 0), stop=(kc == 7)), r=[("wa", b), "cT2"], w=[("ps", 0)])
            if blk in gate_bank:
                gb = gate_bank[blk]
                for kc in range(8):
                    T.op(PE, lambda kc=kc, b=b, gb=gb: nc.tensor.matmul(
                        PS[gb][:, :], lhsT=cbc[:, kc, :], rhs=wa[b][:, kc, :], start=(kc == 0), stop=(kc == 7)),
                        r=[("wa", b), "cbc"], w=[("ps", gb)])
                if blk == 5:
                    for hh in range(2):
                        T.op(DVE, lambda hh=hh: nc.vector.tensor_tensor(out=g1b[:, hh * 512:(hh + 1) * 512], in0=PS[1 + hh][:, :],
                                                                      in1=brow[:, hh * 512:(hh + 1) * 512], op=ALU.add),
                             r=[("ps", 1 + hh), "brow0"], w=[("g1b", hh)])
                if blk == 7:
                    for hh in range(2):
                        T.op(DVE, lambda hh=hh: nc.vector.tensor_tensor(out=s2b[:, hh * 512:(hh + 1) * 512], in0=PS[5 + hh][:, :],
                                                                      in1=s2b[:, hh * 512:(hh + 1) * 512], op=ALU.add),
                             r=[("ps", 5 + hh), "s2b_bias"], w=[("s2b", hh)])
                if blk == 9:
                    for hh in range(2):
                        sl = slice(hh * 512, (hh + 1) * 512)
                        T.op(DVE, lambda hh=hh, sl=sl: nc.vector.tensor_tensor(out=a2b[:, sl], in0=PS[1 + hh][:, :], in1=a2b[:, sl], op=ALU.add),
                             r=[("ps", 1 + hh), "a2b_bias"], w=[("a2b", hh)])
                        T.op(DVE, lambda sl=sl: nc.vector.scalar_tensor_tensor(out=a2b[:, sl], in0=a2b[:, sl], scalar=1.0, in1=n2rb[:, sl], op0=ALU.add, op1=ALU.mult),
                             r=[("a2b", hh), "n2rb"], w=[("a2b", hh)])
        T.op(DVE, lambda: nc.vector.tensor_tensor(
            out=modc[:], in0=PS[0][:, 0:96].rearrange("p (m two) -> p m two", two=2)[:, :, 0], in1=bcol[:], op=ALU.add),
            r=[("ps", 0), "bcol"], w=["modc"])
        for gi, (gt, b0) in enumerate(((g1b, 1), (g2b, 3))):
            if gi == 0:
                continue
            for hh in range(2):
                T.op(DVE, lambda gt=gt, b0=b0, hh=hh, gi=gi: nc.vector.tensor_tensor(
                    out=gt[:, hh * 512:(hh + 1) * 512], in0=PS[b0 + hh][:, :],
                    in1=brow[:, gi * 1024 + hh * 512: gi * 1024 + (hh + 1) * 512], op=ALU.add),
                    r=[("ps", b0 + hh), f"brow{gi}"], w=[(f"g{gi + 1}b", hh)])
        for (av, sc0, nwo, akey) in ((a1, 8, 0, "a1"), (a2, 32, 8, "a2")):
            T.op(DVE, lambda sc0=sc0: nc.vector.tensor_scalar(out=tmp8[:], in0=modc[:, sc0:sc0 + 8], scalar1=1.0, scalar2=None, op0=ALU.add),
                 r=["modc"], w=["tmp8"])
            T.op(DVE, lambda av=av, nwo=nwo: nc.vector.tensor_tensor(out=av[:], in0=tmp8[:], in1=nw[:, nwo:nwo + 8], op=ALU.mult),
                 r=["tmp8", "nw0", "nw1"], w=[akey])
        T.op(DVE, lambda: nc.vector.tensor_scalar(out=smallc[:, 2:3], in0=smallc[:, 2:3], scalar1=0.8, scalar2=None, op0=ALU.mult),
             r=["sc2"], w=["sc2"])
        T.op(DVE, lambda: nc.vector.tensor_reduce(out=l2[:, 0:2], in_=qkrow[:].rearrange("p (a b) -> p a b", a=2), axis=AX.X, op=ALU.max,
                                                  apply_absolute_value=True), r=["qkrow"], w=["l2a"])
        T.op(DVE, lambda: nc.vector.tensor_tensor(out=l2[:, 2:3], in0=l2[:, 0:1], in1=l2[:, 1:2], op=ALU.mult), r=["l2a"], w=["l2b"])
        T.op(DVE, lambda: nc.vector.tensor_scalar(out=smallc[:, 3:4], in0=l2[:, 2:3], scalar1=-8.0, scalar2=None, op0=ALU.mult),
             r=["l2b"], w=["sc3"])
        lv = lamb[:].rearrange("p (a b) -> p a b", a=4)
        T.op(DVE, lambda: nc.vector.tensor_tensor(out=lt[:].rearrange("p (a b) -> p a b", a=2), in0=lv[:, 0:4:2, :], in1=lv[:, 1:4:2, :], op=ALU.mult),
             r=["lamb"], w=["lt"])
        T.op(DVE, lambda: nc.vector.tensor_reduce(out=l2[:, 0:2], in_=lt[:].rearrange("p (a b) -> p a b", a=2), axis=AX.X, op=ALU.add),
             r=["lt", "l2a", "l2b"], w=["l2a"])
        T.op(ACT, lambda: nc.scalar.activation(out=l2[:, 2:4], in_=l2[:, 0:2], func=AF.Exp), r=["l2a"], w=["l2b"])
        T.op(DVE, lambda: nc.vector.tensor_tensor(out=l2[:, 0:1], in0=l2[:, 3:4], in1=l2[:, 2:3], op=ALU.subtract), r=["l2b"], w=["l2a"])
        T.op(DVE, lambda: nc.vector.tensor_scalar(out=smallc[:, 4:5], in0=l2[:, 0:1], scalar1=-0.2, scalar2=None, op0=ALU.add),
             r=["l2a"], w=["sc4"])
        T.barrier(full=False)
    if dbg and "modc" in dbg_out:
        T.dma(SP, dbg_out["modc"][:, :], modc[:], r=["modc"])
        T.dma(SP, dbg_out["g1b"][:, :], g1b[:], r=["g1b"])
        T.dma(SP, dbg_out["smallc"][:, :], smallc[:], r=["sc0"])

    def dump_dram(dst, src, rows, cols, dt):
        with nc.sbuf_tensor("s_dump_" + dst.name.replace(".", "_"), [128, cols], dt) as tmp:
            for r0 in range(0, rows, 128):
                n = min(128, rows - r0)
                T.dma(SP, tmp[0:n, :], src[r0:r0 + n, :], w=["dumptmp"])
                T.dma(SP, dst[r0:r0 + n, :], tmp[0:n, :], r=["dumptmp"], w=[("dumpdst", r0)])
            T.barrier()

    def finish():
        if _scope[0] is not None:
            _scope[0].__exit__(None, None, None)
            _scope[0] = None
        T.barrier()
        if early_open[0]:
            early.close()
            early_open[0] = False
        if early2_open[0]:
            early2.close()
            early2_open[0] = False
        glob.close()
        return nc

    if stage <= 0:
        return finish()

    s1 = modc[:, 0:8]
    s2 = modc[:, 24:32]

    scope_mark("ph1")
    def norm_tile(ph_bufs, src_ap, rkeys, tag):
        ss, sd, junk = ph_bufs["ss"], ph_bufs["sd"], ph_bufs["junk"]
        T.op(ACT, lambda: nc.scalar.activation(out=junk[:], in_=src_ap, func=AF.Square, accum_out=ss[:]), r=rkeys, w=["junk", "ss"])
        T.op(ACT, lambda: nc.scalar.activation(out=sd[:], in_=ss[:], func=AF.Ln, bias=ph_bufs["epsc"][:], scale=1.0 / D), r=["ss"], w=["sd"])
        T.op(ACT, lambda: nc.scalar.activation(out=ph_bufs["rs"][:], in_=sd[:], func=AF.Exp, scale=-0.5), r=["sd"], w=["rs"])

    with ExitStack() as ph:
        def sb(name, shape, dt):
            return ph.enter_context(nc.sbuf_tensor("s_" + name, list(shape), dt))
        xt = [sb(f"xt{i}", [128, D], F32) for i in range(2)]
        xh = [sb(f"xh{i}", [128, D], BF16) for i in range(2)]
        hTb = [sb(f"hTb{i}", [128, 8, 512], BF16) for i in range(2)]
        bufs = dict(ss=sb("ss", [128, 1], F32), sd=sb("sd", [128, 1], F32), rs=sb("rs", [128, 1], F32),
                    junk=sb("junk", [128, D], BF16), epsc=sb("epsc", [128, 1], F32))
        T.op(DVE, lambda: nc.vector.memset(bufs["epsc"][:], EPS), w=["epsc"])
        pass
        for t in range(int(os.environ.get('K1_TILES', NT))):
            b = t % 2
            blk, tl = t // 4, t % 4
            hb = blk % 2
            T.dma(SP, xt[b][:], x_in[t * 128:(t + 1) * 128, :], w=[("xt", b)])
            KO = int(os.environ.get('K1_OPS', 9))
            if KO < 1:
                continue
            norm_tile(bufs, xt[b][:], [("xt", b), "epsc"], "n1")
            if KO < 2:
                continue
            T.op(DVE, lambda b=b: nc.vector.tensor_scalar(out=xh[b][:], in0=xt[b][:], scalar1=bufs["rs"][:], scalar2=None, op0=ALU.mult),
                 r=[("xt", b), "rs"], w=[("xh", b)])
            if KO < 3:
                continue
            pb = t % 2
            pv = psb(pb).rearrange("p (k t) -> p k t", k=8)
            for kc in range(8):
                T.op(PE, lambda kc=kc, b=b, pv=pv: nc.tensor.transpose(pv[:, kc, :], xh[b][:, kc * 128:(kc + 1) * 128], ident_b),
                     r=[("xh", b), "cstb"], w=[("ps", pb)])
            if KO < 4:
                continue
            for kc in range(8):
                T.op(ACT, lambda kc=kc, pv=pv, hb=hb, tl=tl: nc.scalar.activation(
                    out=hTb[hb][:, kc, tl * 128:(tl + 1) * 128], in_=pv[:, kc, :], func=AF.Identity,
                    **({} if os.environ.get('K1_NOAP') == '1' else ({'scale': a1[:, kc:kc + 1]} if os.environ.get('K1_NOAP') == '2' else
                        ({'bias': s1[:, kc:kc + 1]} if os.environ.get('K1_NOAP') == '3' else dict(scale=a1[:, kc:kc + 1], bias=s1[:, kc:kc + 1]))))),
                    r=[("ps", pb), "a1", "modc"], w=[("hTb", hb, tl)])
            if tl == 3:
                T.dma(SP, xnT_v[:, :, blk * 512:(blk + 1) * 512], hTb[hb][:], r=[("hTb", hb)], w=[("xnT", blk)])
        T.barrier()
    early.close()
    early_open[0] = False
    if dbg and "xnT" in dbg_out:
        dump_dram(dbg_out["xnT"], xnT_d, 1024, S, BF16)
    if stage <= 1:
        return finish()

    scope_mark("phA")
    persist = ExitStack()

    def sbp(name, shape, dt):
        return persist.enter_context(nc.sbuf_tensor("s_" + name, list(shape), dt))

    with ExitStack() as ph:
        def sb(name, shape, dt):
            return ph.enter_context(nc.sbuf_tensor("s_" + name, list(shape), dt))
        wqkv = sb("wqkv", [128, 8, 1536], BF16)
        wgt = sb("wgt", [128, 8, 8], BF16)
        bg = sb("bg", [4, 2], F32)
        nbf = sb("nbf", [4, 1], F32)
        epsc = sb("epscA", [128, 1], F32)
        gst = [[sb(f"gst{g}_{i}", [4, 512], F32) for i in range(2)] for g in range(2)]
        kT = sb("kT", [128, 4, S], BF16)
        v1 = sb("v1", [128, NT, 4, 128], BF16)
        xb = [sb(f"xbA{i}", [128, 8, 512], BF16) for i in range(2)]
        qT = sb("qT", [128, 4, 512], BF16)
        hdTb = sb("hdTb", [128, 4, 512], BF16)
        Pb = [[sb(f"P{m}_{i}", [128, 512], BF16) for i in range(2)] for m in range(2)]
        sqb = [sb(f"sqb{i}", [128, 512], BF16) for i in range(2)]
        qsf = [sb(f"qsf{i}", [128, 512], F32) for i in range(2)]
        sdf = [sb(f"sdf{i}", [128, 512], F32) for i in range(2)]
        qnb = [sb(f"qnb{i}", [128, 512], BF16) for i in range(2)]
        t1f = [sb(f"t1f{i}", [128, 512], F32) for i in range(2)]
        t2f = [sb(f"t2f{i}", [128, 512], F32) for i in range(2)]
        fo = [sb(f"fo{i}", [128, 512], F32) for i in range(4)]
        OL = [qsf[0], qsf[1], sdf[0], sdf[1]]
        OLK = [("qsf", 0), ("qsf", 1), ("sdf", 0), ("sdf", 1)]
        sqf = sqb[0]
        pend_fin = []

        T.op(DVE, lambda: nc.vector.memset(epsc[:], EPS), w=["epscA"])
        T.dma(POOL, wqkv[:, 0:4, :], w_in_v[:, 0:4, 2056:3592], w=[("wqkv", 0)])
        T.dma(POOL, wqkv[:, 4:8, :], w_in_v[:, 4:8, 2056:3592], w=[("wqkv", 1)])
        T.dma(POOL, wgt[:], w_in_v[:, :, 2048:2056], w=["wgt"])
        T.dma(SP, bg[:], bgate_in[:, :], w=["bg"])
        T.op(DVE, lambda: nc.vector.tensor_scalar(out=nbf[:], in0=bg[:, 1:2], scalar1=-1.0, scalar2=None, op0=ALU.mult), r=["bg"], w=["nbf"])

        negc = smallc[:, 3:4]
        neglam = smallc[:, 4:5]
        subw8 = smallc[:, 2:3]

        for blk in range(NB):
            xbb = blk % 2
            bs = slice(blk * 512, (blk + 1) * 512)
            T.dma(SP, xb[xbb][:], xnT_v[:, :, bs], w=[("xbA", xbb)])
            for c in range(8):
                st = c % 2
                isq = c < 4
                hh = c % 4
                col0 = (0 if isq else 512) + hh * 128
                for kc in range(8):
                    T.op(PE, lambda kc=kc, col0=col0, st=st: nc.tensor.matmul(
                        PS[st][:, :], lhsT=wqkv[:, kc, col0:col0 + 128], rhs=xb[xbb][:, kc, :], start=(kc == 0), stop=(kc == 7)),
                        r=[("wqkv",), ("xbA", xbb)], w=[("ps", st)])
                wcol = smallc[:, 0:1] if isq else smallc[:, 1:2]
                T.op(ACT, lambda st=st: nc.scalar.activation(out=sqb[st][:], in_=PS[st][:, :], func=AF.Square), r=[("ps", st)], w=[("sqb", st)])
                T.op(ACT, lambda st=st, wcol=wcol: nc.scalar.activation(out=qsf[st][:], in_=PS[st][:, :], func=AF.Identity, scale=wcol),
                     r=[("ps", st), "sc0", "sc1"], w=[("qsf", st)])
                T.op(PE, lambda st=st: nc.tensor.matmul(PS[2 + st][:, :], lhsT=bo64_b, rhs=sqb[st][:], start=True, stop=True),
                     r=[("sqb", st), "cstb"], w=[("ps", 2 + st)])
                T.op(ACT, lambda st=st: nc.scalar.activation(out=sdf[st][:], in_=PS[2 + st][:, :], func=AF.Ln, bias=epsc[:], scale=1.0 / 64),
                     r=[("ps", 2 + st), "epscA"], w=[("sdf", st)])
                T.op(ACT, lambda st=st: nc.scalar.activation(out=sdf[st][:], in_=sdf[st][:], func=AF.Exp, scale=-0.5),
                     r=[("sdf", st)], w=[("sdf", st)])
                T.op(DVE, lambda st=st: nc.vector.tensor_tensor(out=qnb[st][:], in0=qsf[st][:], in1=sdf[st][:], op=ALU.mult),
                     r=[("qsf", st), ("sdf", st)], w=[("qnb", st)])
                T.op(PE, lambda st=st: nc.tensor.matmul(PS[2 + st][:, :], lhsT=RT_b, rhs=qnb[st][:], start=True, stop=True),
                     r=[("qnb", st), "cstb"], w=[("ps", 2 + st)])
                T.op(POOL, lambda st=st: nc.gpsimd.tensor_tensor(out=t1f[st][:], in0=qnb[st][:], in1=CosB[:, bs], op=ALU.mult),
                     r=[("qnb", st), "CosB"], w=[("t1f", st)])
                T.op(DVE, lambda st=st: nc.vector.tensor_tensor(out=t2f[st][:], in0=PS[2 + st][:, :], in1=SinB[:, bs], op=ALU.mult),
                     r=[("ps", 2 + st), "SinB"], w=[("t2f", st)])
                if isq:
                    dst, dk = qT[:, hh, :], ("qT", hh)
                else:
                    dst, dk = kT[:, hh, bs], ("kT", hh, blk)
                T.op(POOL, lambda st=st, dst=dst: nc.gpsimd.tensor_tensor(out=dst, in0=t1f[st][:], in1=t2f[st][:], op=ALU.add),
                     r=[("t1f", st), ("t2f", st)], w=[dk])
            for tl in range(4):
                t = blk * 4 + tl
                for kc in range(8):
                    T.op(PE, lambda kc=kc, tl=tl: nc.tensor.matmul(
                        PS[tl % 2][:, :], lhsT=xb[xbb][:, kc, tl * 128:(tl + 1) * 128], rhs=wqkv[:, kc, 1024:1536], start=(kc == 0), stop=(kc == 7)),
                        r=[("wqkv",), ("xbA", xbb)], w=[("ps", tl % 2)])
                T.op(ACT, lambda t=t, tl=tl: nc.scalar.copy(out=v1[:, t, :, :].rearrange("p h d -> p (h d)"), in_=PS[tl % 2][:, :]),
                     r=[("ps", tl % 2)], w=[("v1", t)])
            for gi in range(2):
                for kc in range(8):
                    T.op(PE, lambda kc=kc, gi=gi: nc.tensor.matmul(
                        PS[1][0:4, :], lhsT=wgt[:, kc, 4 * gi:4 * gi + 4], rhs=xb[xbb][:, kc, :], start=(kc == 0), stop=(kc == 7)),
                        r=["wgt", ("xbA", xbb)], w=[("ps", 1)])
                if gi == 0:
                    T.op(ACT, lambda: nc.scalar.activation(out=gst[0][xbb][:], in_=PS[1][0:4, :], func=AF.Identity, bias=bg[:, 0:1]),
                         r=[("ps", 1), "bg"], w=[("gst", 0, xbb)])
                    T.dma(SP, gi_d[:, bs], gst[0][xbb][:], r=[("gst", 0, xbb)], w=[("gi_d", blk)])
                else:
                    T.op(ACT, lambda: nc.scalar.activation(out=gst[1][xbb][:], in_=PS[1][0:4, :], func=AF.Exp, bias=nbf[:], scale=-1.0),
                         r=[("ps", 1), "nbf"], w=[("gst", 1, xbb)])
                    T.dma(SP, gf_d[:, bs], gst[1][xbb][:], r=[("gst", 1, xbb)], w=[("gf_d", blk)])
            for hh in range(4):
                nkt = blk * 4 + 4
                prev = None

                def pv_step(kt, c0, pbuf):
                    first = (kt == 0)
                    last = (kt == nkt - 1)
                    for m in range(2):
                        T.op(PE, lambda m=m: nc.tensor.matmul(
                            PS[4 + 2 * m][:, c0:512], lhsT=v1[:, kt, hh, :], rhs=Pb[m][pbuf][:, c0:512], start=first, stop=last,
                            skip_group_check=True), r=[("v1", kt), ("P", m, pbuf)], w=[("ps", 4 + 2 * m)])
                        T.op(PE, lambda m=m: nc.tensor.matmul(
                            PS[5 + 2 * m][:, c0:512], lhsT=ones_b, rhs=Pb[m][pbuf][:, c0:512], start=first, stop=last,
                            skip_group_check=True), r=["cstb", ("P", m, pbuf)], w=[("ps", 5 + 2 * m)])

                for kt in range(nkt):
                    ktl = kt - blk * 4
                    c0 = ktl * 128 if ktl > 0 else 0
                    pbuf = kt % 2
                    sbk = 2 * (kt % 2)
                    for m in range(2):
                        T.op(PE, lambda m=m, kt=kt, c0=c0, sbk=sbk: nc.tensor.matmul(
                            PS[sbk + m][:, c0:512], lhsT=kT[64 * m:64 * m + 64, hh, kt * 128:(kt + 1) * 128],
                            rhs=qT[64 * m:64 * m + 64, hh, c0:512], start=True, stop=True),
                            r=[("kT", hh, kt // 4), ("qT", hh)], w=[("ps", sbk + m)])
                    if prev is not None:
                        pv_step(*prev)
                    for m in range(2):
                        T.op(ACT, lambda m=m, c0=c0, pbuf=pbuf, sbk=sbk: nc.scalar.activation(
                            out=Pb[m][pbuf][:, c0:512], in_=PS[sbk + m][:, c0:512], func=AF.Exp, bias=negc, scale=0.125),
                            r=[("ps", sbk + m), "sc3"], w=[("P", m, pbuf)])
                        if ktl >= 0:
                            T.op(POOL, lambda m=m, c0=c0, pbuf=pbuf: nc.gpsimd.affine_select(
                                out=Pb[m][pbuf][:, c0:c0 + 128], in_=Pb[m][pbuf][:, c0:c0 + 128], pattern=[[1, 128]],
                                compare_op=ALU.is_ge, fill=0.0, base=0, channel_multiplier=-1),
                                r=[("P", m, pbuf)], w=[("P", m, pbuf)])
                    prev = (kt, c0, pbuf)
                    if pend_fin:
                        T.op(*pend_fin.pop(0))
                pv_step(*prev)
                for rec_ in pend_fin:
                    T.op(*rec_)
                pend_fin.clear()

                def finalize_ops(hh):
                    T.op(ACT, lambda: nc.scalar.copy(out=OL[0][:], in_=PS[4][:, :]), r=[("ps", 4)], w=[OLK[0]])
                    T.op(DVE, lambda: nc.vector.tensor_copy(out=OL[1][:], in_=PS[5][:, :]), r=[("ps", 5)], w=[OLK[1]])
                    T.op(ACT, lambda: nc.scalar.copy(out=OL[2][:], in_=PS[6][:, :]), r=[("ps", 6)], w=[OLK[2]])
                    T.op(DVE, lambda: nc.vector.tensor_copy(out=OL[3][:], in_=PS[7][:, :]), r=[("ps", 7)], w=[OLK[3]])
                    T.start_record()
                    T.op(ACT, lambda: nc.scalar.activation(out=fo[0][:], in_=OL[1][:], func=AF.Ln), r=[OLK[1]], w=[("fo", 0)])
                    T.op(ACT, lambda: nc.scalar.activation(out=fo[0][:], in_=fo[0][:], func=AF.Exp, scale=-1.0), r=[("fo", 0)], w=[("fo", 0)])
                    T.op(DVE, lambda: nc.vector.tensor_tensor(out=fo[1][:], in0=OL[0][:], in1=fo[0][:], op=ALU.mult),
                         r=[OLK[0], ("fo", 0)], w=[("fo", 1)])
                    T.op(ACT, lambda: nc.scalar.activation(out=fo[2][:], in_=OL[3][:], func=AF.Ln), r=[OLK[3]], w=[("fo", 2)])
                    T.op(ACT, lambda: nc.scalar.activation(out=fo[0][:], in_=fo[2][:], func=AF.Exp, scale=-1.0), r=[("fo", 2), ("fo", 0)], w=[("fo", 0)])
                    T.op(DVE, lambda: nc.vector.tensor_tensor(out=fo[2][:], in0=OL[2][:], in1=fo[0][:], op=ALU.mult),
                         r=[OLK[2], ("fo", 0)], w=[("fo", 2)])
                    T.op(DVE, lambda: nc.vector.scalar_tensor_tensor(out=fo[3][:], in0=fo[2][:], scalar=neglam, in1=fo[1][:], op0=ALU.mult, op1=ALU.add),
                         r=[("fo", 1), ("fo", 2), "sc4"], w=[("fo", 3)])
                    T.op(ACT, lambda: nc.scalar.activation(out=sqf[:], in_=fo[3][:], func=AF.Square), r=[("fo", 3)], w=[("sqb", 0)])
                    T.op(PE, lambda: nc.tensor.matmul(PS[0][:, :], lhsT=ones_b, rhs=sqf[:], start=True, stop=True),
                         r=[("sqb", 0), "cstb"], w=[("ps", 0)])
                    T.op(ACT, lambda: nc.scalar.activation(out=fo[0][:], in_=PS[0][:, :], func=AF.Ln, bias=epsc[:], scale=1.0 / 128),
                         r=[("ps", 0), "epscA"], w=[("fo", 0)])
                    T.op(ACT, lambda: nc.scalar.activation(out=fo[0][:], in_=fo[0][:], func=AF.Exp, scale=-0.5), r=[("fo", 0)], w=[("fo", 0)])
                    T.op(DVE, lambda: nc.vector.scalar_tensor_tensor(out=hdTb[:, hh, :], in0=fo[3][:], scalar=subw8, in1=fo[0][:], op0=ALU.mult, op1=ALU.mult),
                         r=[("fo", 3), ("fo", 0), "sc2"], w=[("hdTb", hh)])
                    return T.stop_record()

                pend_fin.extend(finalize_ops(hh))
            for rec_ in pend_fin:
                T.op(*rec_)
            pend_fin.clear()
            T.dma(SP, hdT_v[:, :, bs], hdTb[:], r=[("hdTb",)], w=[("hdT", blk)])
        T.barrier()
    early2.close()
    early2_open[0] = False
    if dbg and "hdT" in dbg_out:
        dump_dram(dbg_out["hdT"], hdT_d, 512, S, BF16)
        dump_dram(dbg_out["irow"], gi_d, 4, S, F32)
        dump_dram(dbg_out["frow"], gf_d, 4, S, F32)
    if stage <= 2:
        persist.close()
        return finish()

    scope_mark("phB")
    wcol = sbp("wcol", [128, NT, 8], F32)
    decb = sbp("decb", [128, 128], F32)
    with ExitStack() as ph:
        def sb(name, shape, dt):
            return ph.enter_context(nc.sbuf_tensor("s_" + name, list(shape), dt))
        irow = sb("irow", [4, S], F32)
        frow = sb("frow", [4, S], F32)
        T.dma(SP, irow[:], gi_d[:, :], w=[("irow",)])
        T.dma(SP, frow[:], gf_d[:, :], w=[("frow",)])
        onesr = sb("onesr", [4, S], F32)
        csr = sb("csr", [4, S], F32)
        nbr = sb("nbr", [4, S], F32)
        gr = sb("gr", [4, S], F32)
        pe_ = sb("pe_", [4, 33], F32)
        G = sb("G", [4, 32], F32)
        Ms = sb("Ms", [4, 32], F32)
        mp = sb("mp", [4, 33], F32)
        dec = sb("dec", [4, 32], F32)
        T.op(ACT, lambda: nc.scalar.activation(out=frow[:], in_=frow[:], func=AF.Ln, bias=1.0), r=[("frow",)], w=[("frow",)])
        T.op(DVE, lambda: nc.vector.memset(onesr[:], 1.0), w=["onesr"])
        T.op(DVE, lambda: nc.vector.tensor_tensor_scan(out=csr[:], data0=onesr[:], data1=frow[:], initial=0.0, op0=ALU.mult, op1=ALU.add),
             r=["onesr", ("frow",)], w=["csr"])
        T.op(DVE, lambda: nc.vector.memset(pe_[:, 0:1], 0.0), w=[("pe_", 0)])
        T.op(DVE, lambda: nc.vector.tensor_copy(out=pe_[:, 1:33], in_=csr[:].rearrange("p (j s) -> p j s", s=128)[:, :, 127]),
             r=["csr"], w=[("pe_", 1)])
        T.op(DVE, lambda: nc.vector.tensor_tensor(out=nbr[:].rearrange("p (j s) -> p j s", s=128), in0=csr[:].rearrange("p (j s) -> p j s", s=128),
                                                  in1=pe_[:, 0:32].unsqueeze(2).to_broadcast([4, 32, 128]), op=ALU.subtract),
             r=["csr", ("pe_",)], w=["nbr"])
        T.op(DVE, lambda: nc.vector.tensor_tensor(out=gr[:], in0=irow[:], in1=nbr[:], op=ALU.add), r=[("irow",), "nbr"], w=["gr"])
        T.op(DVE, lambda: nc.vector.tensor_reduce(out=G[:], in_=gr[:].rearrange("p (j s) -> p j s", s=128), axis=AX.X, op=ALU.max),
             r=["gr"], w=["G"])
        T.op(DVE, lambda: nc.vector.memset(mp[:, 0:1], 0.0), w=[("mp", 0)])
        nbl = nbr[:].rearrange("p (j s) -> p j s", s=128)[:, :, 127]
        for j in range(NT):
            T.op(DVE, lambda j=j: nc.vector.tensor_tensor(out=Ms[:, j:j + 1], in0=mp[:, j:j + 1], in1=G[:, j:j + 1], op=ALU.max),
                 r=[("mp", j), "G"], w=[("Ms", j)])
            T.op(DVE, lambda j=j: nc.vector.tensor_tensor(out=mp[:, j + 1:j + 2], in0=Ms[:, j:j + 1], in1=nbl[:, j:j + 1], op=ALU.subtract),
                 r=[("Ms", j), "nbr"], w=[("mp", j + 1)])
        Msb = Ms[:].unsqueeze(2).to_broadcast([4, 32, 128])
        T.op(DVE, lambda: nc.vector.tensor_tensor(out=gr[:].rearrange("p (j s) -> p j s", s=128), in0=gr[:].rearrange("p (j s) -> p j s", s=128),
                                                  in1=Msb, op=ALU.subtract), r=["gr", ("Ms",)], w=["gr"])
        T.op(DVE, lambda: nc.vector.tensor_tensor(out=nbr[:].rearrange("p (j s) -> p j s", s=128), in0=nbr[:].rearrange("p (j s) -> p j s", s=128),
                                                  in1=Msb, op=ALU.subtract), r=["nbr", ("Ms",)], w=["nbr"])
        T.op(ACT, lambda: nc.scalar.activation(out=gr[:], in_=gr[:], func=AF.Exp), r=["gr"], w=["gr"])
        T.op(ACT, lambda: nc.scalar.activation(out=nbr[:], in_=nbr[:], func=AF.Exp), r=["nbr"], w=["nbr"])
        T.op(DVE, lambda: nc.vector.tensor_scalar(out=gr[:], in0=gr[:], scalar1=128.0 ** -0.5, scalar2=None, op0=ALU.mult), r=["gr"], w=["gr"])
        T.op(DVE, lambda: nc.vector.tensor_tensor(out=dec[:], in0=mp[:, 0:32], in1=Ms[:], op=ALU.subtract), r=[("mp",), ("Ms",)], w=["dec"])
        T.op(ACT, lambda: nc.scalar.activation(out=dec[:], in_=dec[:], func=AF.Exp), r=["dec"], w=["dec"])
        T.dma(SP, dec_d[:, :], dec[:], r=["dec"], w=["dec_d"])
        T.dma(SP, decb[:], dec_d.rearrange("h j -> (h j)").unsqueeze(0).partition_broadcast(128), r=["dec_d"], w=["decb"])
        wv = PS[0][:, 0:256].rearrange("p (t e) -> p t e", e=8)
        for t in range(NT):
            T.op(PE, lambda t=t: nc.tensor.transpose(wv[:, t, 0:4], gr[:, t * 128:(t + 1) * 128], ident_f[0:4, 0:4]),
                 r=["gr", "cst"], w=[("ps", 0, t, 0)])
            T.op(PE, lambda t=t: nc.tensor.transpose(wv[:, t, 4:8], nbr[:, t * 128:(t + 1) * 128], ident_f[0:4, 0:4]),
                 r=["nbr", "cst"], w=[("ps", 0, t, 1)])
        T.op(DVE, lambda: nc.vector.tensor_copy(out=wcol[:], in_=wv), r=[("ps", 0)], w=["wcol"])
        T.barrier()
    if dbg and "wcol" in dbg_out:
        T.dma(SP, dbg_out["wcol"][:, :], wcol[:].rearrange("p t e -> p (t e)"), r=["wcol"])
        T.dma(SP, dbg_out["decb"][:, :], decb[:], r=["decb"])
    if stage <= 3:
        persist.close()
        return finish()

    scope_mark("phC")
    with ExitStack() as ph:
        def sb(name, shape, dt):
            return ph.enter_context(nc.sbuf_tensor("s_" + name, list(shape), dt))
        wm = sb("wm", [128, 8, 2048], BF16)
        cw = sb("cw", [128, 8, 4], F32)
        cbias = sb("cbias", [128, 8], F32)
        dg = sb("dg", [128, 8, 4, 128], BF16)
        mnwb = sb("mnwb", [128, 512], F32)
        epsc = sb("epscC", [128, 1], F32)
        xb = [sb(f"xbC{i}", [128, 8, 512], BF16) for i in range(2)]
        pre = sb("pre", [128, 8, 516], BF16)
        qkc = sb("qkc", [128, 8, 512], BF16)
        vm1 = [sb(f"vm1_{i}", [128, 4, 130], BF16) for i in range(2)]
        so = [sb(f"so{i}", [128, 512], F32) for i in range(2)]
        ST = [sb(f"ST{i}", [128, 128], BF16) for i in range(4)]
        kw = [sb(f"kw{i}", [128, 128], BF16) for i in range(4)]
        Cst = sb("Cst", [128, 4, 130], F32)
        Cd = sb("Cd", [128, 4, 130], BF16)
        hbuf = sb("hbuf", [128, 512], F32)
        sqh = sb("sqh", [128, 512], F32)
        hmb = sb("hmb", [128, 512], BF16)
        hmTb = [sb(f"hmTb{i}", [128, 4, 512], BF16) for i in range(2)]
        dn = sb("dn", [128, 8], F32)
        ssh = sb("ssh", [128, 8], F32)

        T.op(DVE, lambda: nc.vector.memset(epsc[:], EPS), w=["epscC"])
        T.dma(POOL, wm[:, 0:4, :], w_in_v[:, 0:4, 0:2048], w=[("wm", 0)], max_dma_last_dim=8192)
        T.dma(POOL, wm[:, 4:8, :], w_in_v[:, 4:8, 0:2048], w=[("wm", 1)], max_dma_last_dim=8192)
        T.dma(SP, cw[:], convw_in[:, :, :], w=["cw"])
        T.dma(SP, cbias[:], convb_in[:, :], w=["cbias"])
        T.dma(SP, mnwb[:], mnw_in[0:1, :].partition_broadcast(128), w=["mnwb"])
        for c in range(8):
            for j in range(4):
                T.op(DVE, lambda c=c, j=j: nc.vector.tensor_scalar(out=dg[:, c, j, :], in0=ident_f, scalar1=cw[:, c, j:j + 1], scalar2=None, op0=ALU.mult),
                     r=["cw", "cst"], w=[("dg", c, j)])
        T.op(DVE, lambda: nc.vector.memset(pre[:, :, 0:4], 0.0), w=[("pre", "halo")])
        T.op(DVE, lambda: nc.vector.memset(Cst[:], 0.0), w=["Cst"])
        T.op(DVE, lambda: nc.vector.memset(Cd[:], 0.0), w=["Cd"])
        for i in range(2):
            T.op(DVE, lambda i=i: nc.vector.memset(vm1[i][:, :, 128:130], 1.0), w=[("vm1", i)])

        for blk in range(NB):
            xbb = blk % 2
            bs = slice(blk * 512, (blk + 1) * 512)
            T.dma(SP, xb[xbb][:], xnT_v[:, :, bs], w=[("xbC", xbb)])
            for c in range(8):
                pb = c % 2
                for kc in range(8):
                    T.op(PE, lambda kc=kc, c=c, pb=pb: nc.tensor.matmul(
                        PS[pb][:, :], lhsT=wm[:, kc, c * 128:(c + 1) * 128], rhs=xb[xbb][:, kc, :], start=(kc == 0), stop=(kc == 7)),
                        r=[("wm",), ("xbC", xbb)], w=[("ps", pb)])
                if blk > 0:
                    T.op(DVE, lambda c=c: nc.vector.tensor_copy(out=pre[:, c, 1:4], in_=pre[:, c, 513:516]),
                         r=[("pre", c)], w=[("pre", "halo", c)])
                T.op(ACT, lambda c=c, pb=pb: nc.scalar.copy(out=pre[:, c, 4:516], in_=PS[pb][:, :]),
                     r=[("ps", pb), ("pre", "halo", c)], w=[("pre", c)])
                cb2 = 2 + c % 2
                for j in range(4):
                    T.op(PE, lambda c=c, j=j, cb2=cb2: nc.tensor.matmul(
                        PS[cb2][:, :], lhsT=dg[:, c, j, :], rhs=pre[:, c, 1 + j:513 + j], start=(j == 0), stop=(j == 3)),
                        r=[("dg", c), ("pre", c), ("pre", "halo", c)], w=[("ps", cb2)])
                T.op(ACT, lambda c=c, cb2=cb2: nc.scalar.activation(out=qkc[:, c, :], in_=PS[cb2][:, :], func=AF.Silu, bias=cbias[:, c:c + 1]),
                     r=[("ps", cb2), "cbias"], w=[("qkc", c)])
            for tl in range(4):
                t = blk * 4 + tl
                vb = t % 2
                ts_ = slice(tl * 128, (tl + 1) * 128)
                def emit_vo(tl_):
                    t_ = blk * 4 + tl_
                    vb_ = t_ % 2
                    sl_ = slice(tl_ * 128, (tl_ + 1) * 128)
                    for half in range(2):
                        for kc in range(8):
                            T.op(PE, lambda kc=kc, half=half: nc.tensor.matmul(
                                PS[4 + half][:, :], lhsT=xb[xbb][:, kc, sl_], rhs=wm[:, kc, 1024 + half * 512:1536 + half * 512],
                                start=(kc == 0), stop=(kc == 7)), r=[("wm",), ("xbC", xbb)], w=[("ps", 4 + half)])
                    T.op(ACT, lambda: nc.scalar.copy(out=vm1[vb_][:, :, 0:128], in_=PS[4][:, :].rearrange("p (h d) -> p h d", h=4)),
                         r=[("ps", 4)], w=[("vm1", vb_)])
                    T.op(ACT, lambda: nc.scalar.activation(out=so[vb_][:], in_=PS[5][:, :], func=AF.Sigmoid), r=[("ps", 5)], w=[("so", vb_)])

                if tl == 0:
                    emit_vo(0)
                if tl < 3:
                    emit_vo(tl + 1)
                def head_ops(hh):
                    wc = wcol[:, t, hh:hh + 1]
                    cc = wcol[:, t, 4 + hh:5 + hh]
                    dcol = decb[:, hh * 32 + t:hh * 32 + t + 1]
                    if hh % 2 == 0:
                        bA, bK, bU, bN = 6, 1, 7, 0
                    else:
                        bA, bK, bU, bN = 2, 3, 4, 5
                    kps = psb(bK)[:, 0:128]
                    ops = []
                    ops.append(lambda: T.op(PE, lambda: nc.tensor.matmul(PS[bA][:, 0:128], lhsT=qkc[:, 4 + hh, ts_], rhs=qkc[:, hh, ts_], start=True, stop=True),
                                            r=[("qkc", 4 + hh), ("qkc", hh)], w=[("ps", bA)]))
                    ops.append(lambda: T.op(DVE, lambda: nc.vector.scalar_tensor_tensor(
                        out=ST[hh][:], in0=PS[bA][:, 0:128], scalar=wc, in1=tri_f, op0=ALU.mult, op1=ALU.mult),
                        r=[("ps", bA), "wcol", "cst"], w=[("ST", hh)]))
                    ops.append(lambda: T.op(PE, lambda: nc.tensor.transpose(kps, qkc[:, 4 + hh, ts_], ident_b),
                                            r=[("qkc", 4 + hh), "cstb"], w=[("ps", bK)]))
                    ops.append(lambda: T.op(DVE, lambda: nc.vector.tensor_scalar(out=kw[hh][:], in0=kps, scalar1=wc, scalar2=None, op0=ALU.mult),
                                            r=[("ps", bK), "wcol"], w=[("kw", hh)]))
                    ops.append(lambda: T.op(ACT, lambda: nc.scalar.activation(out=Cd[:, hh, :], in_=Cst[:, hh, :], func=AF.Identity, scale=dcol),
                                            r=[("Cst", hh), "decb"], w=[("Cd", hh)]))
                    ops.append(lambda: T.op(PE, lambda: nc.tensor.matmul(PS[bU][:, 0:129], lhsT=kw[hh][:], rhs=vm1[vb][:, hh, 0:129], start=True, stop=True),
                                            r=[("kw", hh), ("vm1", vb)], w=[("ps", bU)]))
                    ops.append(lambda: T.op(PE, lambda: nc.tensor.matmul(PS[bN][:, 0:129], lhsT=ST[hh][:], rhs=vm1[vb][:, hh, 0:129], start=True, stop=False),
                                            r=[("ST", hh), ("vm1", vb)], w=[("ps", bN)]))
                    ops.append(lambda: T.op(PE, lambda: nc.tensor.matmul(PS[bN][:, 0:129], lhsT=qkc[:, hh, ts_], rhs=Cd[:, hh, 0:129], start=False, stop=True),
                                            r=[("qkc", hh), ("Cd", hh)], w=[("ps", bN)]))
                    ops.append(lambda: T.op(DVE, lambda: nc.vector.scalar_tensor_tensor(
                        out=Cst[:, hh, 0:129], in0=Cst[:, hh, 0:129], scalar=dcol, in1=PS[bU][:, 0:129], op0=ALU.mult, op1=ALU.add),
                        r=[("Cst", hh), "decb", ("ps", bU)], w=[("Cst", hh)]))
                    ops.append(lambda: T.op(DVE, lambda: nc.vector.tensor_reduce(
                        out=dn[:, hh:hh + 1], in_=PS[bN][:, 128:129], axis=AX.X, op=ALU.max, apply_absolute_value=True),
                        r=[("ps", bN)], w=[("dn", hh)]))
                    ops.append(lambda: T.op(DVE, lambda: nc.vector.tensor_scalar(
                        out=dn[:, hh:hh + 1], in0=dn[:, hh:hh + 1], scalar1=cc, scalar2=None, op0=ALU.max),
                        r=[("dn", hh), "wcol"], w=[("dn", hh)]))
                    ops.append(lambda: T.op(DVE, lambda: nc.vector.reciprocal(out=dn[:, 4 + hh:5 + hh], in_=dn[:, hh:hh + 1]), r=[("dn", hh)], w=[("dn", 4 + hh)]))
                    ops.append(lambda: T.op(ACT, lambda: nc.scalar.activation(out=hbuf[:, hh * 128:(hh + 1) * 128], in_=PS[bN][:, 0:128], func=AF.Copy,
                                                                              scale=dn[:, 4 + hh:5 + hh]),
                                            r=[("ps", bN), ("dn", 4 + hh)], w=[("hbuf", hh)]))
                    return ops

                for pair in ((0, 1), (2, 3)):
                    pops = [head_ops(hh) for hh in pair]
                    for i in range(len(pops[0])):
                        for po in pops:
                            po[i]()
                T.op(DVE, lambda: nc.vector.tensor_tensor(out=sqh[:], in0=hbuf[:], in1=hbuf[:], op=ALU.mult), r=[("hbuf",)], w=["sqh"])
                T.op(DVE, lambda: nc.vector.tensor_reduce(out=ssh[:, 0:4], in_=sqh[:].rearrange("p (h d) -> p h d", h=4), axis=AX.X, op=ALU.add),
                     r=["sqh"], w=[("ssh", 0)])
                T.op(ACT, lambda: nc.scalar.activation(out=ssh[:, 4:8], in_=ssh[:, 0:4], func=AF.Sqrt, bias=epsc[:], scale=1.0 / 128),
                     r=[("ssh", 0), "epscC"], w=[("ssh", 1)])
                T.op(DVE, lambda: nc.vector.reciprocal(out=ssh[:, 4:8], in_=ssh[:, 4:8]), r=[("ssh", 1)], w=[("ssh", 1)])
                T.op(DVE, lambda: nc.vector.tensor_tensor(out=sqh[:].rearrange("p (h d) -> p h d", h=4), in0=hbuf[:].rearrange("p (h d) -> p h d", h=4),
                                                          in1=ssh[:, 4:8].unsqueeze(2).to_broadcast([128, 4, 128]), op=ALU.mult),
                     r=[("hbuf",), ("ssh", 1)], w=["sqh"])
                T.op(DVE, lambda: nc.vector.tensor_tensor(out=sqh[:], in0=sqh[:], in1=mnwb[:], op=ALU.mult), r=["sqh", "mnwb"], w=["sqh"])
                T.op(DVE, lambda vb=vb: nc.vector.tensor_tensor(out=hmb[:], in0=sqh[:], in1=so[vb][:], op=ALU.mult), r=["sqh", ("so", vb)], w=["hmb"])
                hb = blk % 2
                tp = psb(3).rearrange("p (k t) -> p k t", k=8)
                for hh in range(4):
                    T.op(PE, lambda hh=hh: nc.tensor.transpose(tp[:, hh, :], hmb[:, hh * 128:(hh + 1) * 128], ident_b),
                         r=["hmb", "cstb"], w=[("ps", 3)])
                T.op(ACT, lambda hb=hb: nc.scalar.copy(out=hmTb[hb][:, :, ts_], in_=tp[:, 0:4, :]), r=[("ps", 3)], w=[("hmTb", hb, tl)])
            T.dma(SP, hmT_v[:, :, bs], hmTb[blk % 2][:], r=[("hmTb", blk % 2)], w=[("hmT", blk)])
        T.barrier()
    persist.close()
    if dbg and "hmT" in dbg_out:
        dump_dram(dbg_out["hmT"], hmT_d, 512, S, BF16)
    if stage <= 4:
        return finish()

    scope_mark("phD")
    with ExitStack() as ph:
        def sb(name, shape, dt):
            return ph.enter_context(nc.sbuf_tensor("s_" + name, list(shape), dt))
        wg = sb("wg", [128, 8, 2048], BF16)
        wbm = sb("wbm", [128, 4, D], BF16)
        wbd = sb("wbd", [128, 4, D], BF16)
        wo = sb("wo", [128, 8, D], BF16)
        wstg = [sb(f"wstg{i}", [128, D], F32) for i in range(2)]
        wr = sb("wrt", [128, 8, 36], F32)
        brb = sb("brb", [128, 36], F32)
        xb = [sb(f"xbD{i}", [128, 8, 512], BF16) for i in range(2)]
        hmb_ = [sb(f"hmD{i}", [128, 4, 512], BF16) for i in range(2)]
        hdb_ = [sb(f"hdD{i}", [128, 4, 512], BF16) for i in range(2)]
        sg = [sb(f"sg{i}", [128, 512], F32) for i in range(4)]
        tt = [sb(f"tt{i}", [128, 512], F32) for i in range(4)]
        mT = sb("mT", [128, 8, 512], BF16)
        xt = [sb(f"xtD{i}", [128, D], F32) for i in range(2)]
        x1t = [sb(f"x1t{i}", [128, D], F32) for i in range(2)]
        xhf = sb("xhf", [128, D], F32)
        h2f = sb("h2f", [128, 8, 128], F32)
        x2t = [sb(f"x2t{i}", [128, D], BF16) for i in range(2)]
        ohb = sb("ohb", [128, 32], BF16)
        bufs = dict(ss=sb("ssD", [128, 1], F32), sd=sb("sdD", [128, 1], F32), rs=sb("rsD", [128, 1], F32),
                    junk=sb("junkD", [128, D], BF16), epsc=sb("epscD", [128, 1], F32))
        lg = sb("lg", [128, 36], F32)
        r8 = sb("r8", [128, 80], F32)

        T.op(DVE, lambda: nc.vector.memset(bufs["epsc"][:], EPS), w=["epsc"])
        T.op(DVE, lambda: nc.vector.memset(tot[:], 0.0), w=["tot"])
        T.dma(POOL, wg[:, 0:4, :], w_in_v[:, 0:4, 3592:5640], w=[("wg", 0)], max_dma_last_dim=8192)
        T.dma(POOL, wg[:, 4:8, :], w_in_v[:, 4:8, 3592:5640], w=[("wg", 1)], max_dma_last_dim=8192)
        T.dma(POOL, wbm[:], w_br_m.rearrange("(k p) n -> p k n", p=128), w=["wbm"])
        T.dma(POOL, wbd[:], w_br_d.rearrange("(k p) n -> p k n", p=128), w=["wbd"])
        T.dma(SP, wr[:], wr_in.rearrange("(k p) n -> p k n", p=128), w=["wrt"])
        T.dma(SP, brb[:], br_in[0:1, :].partition_broadcast(128), w=["brb"])
        w_out_v = w_out.rearrange("(k p) n -> p k n", p=128)
        for kc in range(8):
            b = kc % 2
            T.dma(SP, wstg[b][:], w_out_v[:, kc, :], w=[("wstg", b)])
            T.op(DVE, lambda kc=kc, b=b: nc.vector.tensor_tensor(out=wo[:, kc, :], in0=wstg[b][:], in1=g1b[:], op=ALU.mult),
                 r=[("wstg", b), "g1b"], w=[("wo", kc)])

        for blk in range(NB):
            xbb = blk % 2
            bs = slice(blk * 512, (blk + 1) * 512)
            def load_blk(bk):
                bb_ = bk % 2
                sl_ = slice(bk * 512, (bk + 1) * 512)
                T.dma(SP, xb[bb_][:], xnT_v[:, :, sl_], w=[("xbD", bb_)])
                T.dma(SP, hmb_[bb_][:], hmT_v[:, :, sl_], w=[("hmD", bb_)])
                T.dma(SP, hdb_[bb_][:], hdT_v[:, :, sl_], w=[("hdD", bb_)])

            if blk == 0:
                load_blk(0)
                T.dma(SP, xt[0][:], x_in[0:128, :], w=[("xtD", 0)])
            for oc in range(8):
                st = oc % 2
                for which, (colbase, wbr, hsrc, hkey, wkey) in enumerate(((0, wbm, hmb_, "hmD", "wbm"), (1024, wbd, hdb_, "hdD", "wbd"))):
                    gb = 0 + which
                    pbk = 2 + which
                    for kc in range(8):
                        T.op(PE, lambda kc=kc, colbase=colbase, gb=gb: nc.tensor.matmul(
                            PS[gb][:, :], lhsT=wg[:, kc, colbase + oc * 128:colbase + (oc + 1) * 128], rhs=xb[xbb][:, kc, :],
                            start=(kc == 0), stop=(kc == 7)), r=[("wg",), ("xbD", xbb)], w=[("ps", gb)])
                    T.op(ACT, lambda gb=gb, which=which: nc.scalar.activation(out=sg[2 * st + which][:], in_=PS[gb][:, :], func=AF.Sigmoid),
                         r=[("ps", gb)], w=[("sg", 2 * st + which)])
                    for kc in range(4):
                        T.op(PE, lambda kc=kc, wbr=wbr, hsrc=hsrc, pbk=pbk: nc.tensor.matmul(
                            PS[pbk][:, :], lhsT=wbr[:, kc, oc * 128:(oc + 1) * 128], rhs=hsrc[xbb][:, kc, :],
                            start=(kc == 0), stop=(kc == 3)), r=[wkey, (hkey, xbb)], w=[("ps", pbk)])
                    T.op(DVE, lambda which=which, pbk=pbk: nc.vector.tensor_tensor(out=tt[2 * st + which][:], in0=PS[pbk][:, :], in1=sg[2 * st + which][:], op=ALU.mult),
                         r=[("ps", pbk), ("sg", 2 * st + which)], w=[("tt", 2 * st + which)])
                T.op(POOL, lambda st=st: nc.gpsimd.tensor_tensor(out=mT[:, oc, :], in0=tt[2 * st][:], in1=tt[2 * st + 1][:], op=ALU.add),
                     r=[("tt", 2 * st), ("tt", 2 * st + 1)], w=[("mT", oc)])
            for tl in range(4):
                t = blk * 4 + tl
                b = t % 2
                ts_ = slice(tl * 128, (tl + 1) * 128)
                if t + 1 < NT:
                    T.dma(SP, xt[(t + 1) % 2][:], x_in[(t + 1) * 128:(t + 2) * 128, :], w=[("xtD", (t + 1) % 2)])
                if tl == 3 and blk + 1 < NB:
                    load_blk(blk + 1)
                def emit_wout(tl_):
                    sl_ = slice(tl_ * 128, (tl_ + 1) * 128)
                    for half in range(2):
                        for oc in range(8):
                            T.op(PE, lambda oc=oc, half=half: nc.tensor.matmul(
                                PS[4 + half][:, :], lhsT=mT[:, oc, sl_], rhs=wo[:, oc, half * 512:(half + 1) * 512],
                                start=(oc == 0), stop=(oc == 7)), r=[("mT", oc), ("wo",)], w=[("ps", 4 + half)])

                if tl == 0:
                    emit_wout(0)
                for half in range(2):
                    T.op(DVE, lambda half=half, b=b: nc.vector.tensor_tensor(
                        out=x1t[b][:, half * 512:(half + 1) * 512], in0=PS[4 + half][:, :], in1=xt[b][:, half * 512:(half + 1) * 512], op=ALU.add),
                        r=[("ps", 4 + half), ("xtD", b)], w=[("x1t", b, half)])
                T.dma(SP, x1_d[t * 128:(t + 1) * 128, :], x1t[b][:], r=[("x1t", b)], w=[("x1d", t)])
                if tl < 3:
                    emit_wout(tl + 1)
                norm_tile(bufs, x1t[b][:], [("x1t", b), "epsc"], "n2")
                T.op(DVE, lambda b=b: nc.vector.tensor_scalar(out=xhf[:], in0=x1t[b][:], scalar1=bufs["rs"][:], scalar2=None, op0=ALU.mult),
                     r=[("x1t", b), "rs"], w=["xhf"])
                for kc in range(8):
                    T.op(PE, lambda kc=kc: nc.tensor.transpose(PS[6 + kc // 4][:, (kc % 4) * 128:(kc % 4 + 1) * 128], xhf[:, kc * 128:(kc + 1) * 128], ident_f),
                         r=["xhf", "cst"], w=[("ps", 6 + kc // 4, kc % 4)])
                for kc in range(8):
                    T.op(ACT, lambda kc=kc: nc.scalar.activation(
                        out=h2f[:, kc, :], in_=PS[6 + kc // 4][:, (kc % 4) * 128:(kc % 4 + 1) * 128], func=AF.Identity,
                        scale=a2[:, kc:kc + 1], bias=s2[:, kc:kc + 1]), r=[("ps", 6 + kc // 4, kc % 4), "a2", "modc"], w=[("h2f", kc)])
                T.op(DVE, lambda: nc.vector.tensor_tensor(out=xhf[:], in0=xhf[:], in1=a2b[:], op=ALU.mult), r=["xhf", ("a2b",)], w=["xhf"])
                T.op(POOL, lambda b=b: nc.gpsimd.tensor_tensor(out=x2t[b][:], in0=xhf[:], in1=s2b[:], op=ALU.add), r=["xhf", ("s2b",)], w=[("x2t", b)])
                T.dma(SP, X2_d[t * 128:(t + 1) * 128, :], x2t[b][:], r=[("x2t", b)], w=[("X2d", t)])
                for kc in range(8):
                    T.op(PE, lambda kc=kc: nc.tensor.matmul(PS[0][:, 0:36], lhsT=h2f[:, kc, :], rhs=wr[:, kc, :], start=(kc == 0), stop=(kc == 7)),
                         r=[("h2f", kc), "wrt"], w=[("ps", 0)])
                T.op(DVE, lambda: nc.vector.tensor_tensor(out=lg[:], in0=PS[0][:, 0:36], in1=brb[:], op=ALU.add), r=[("ps", 0), "brb"], w=["lg"])
                gl = lg[:, 0:4]
                el = lg[:, 4:36].rearrange("p (g j) -> p g j", g=4)
                R = lambda a_, b_: r8[:, a_:b_]
                T.op(DVE, lambda: nc.vector.tensor_reduce(out=R(0, 1), in_=gl, axis=AX.X, op=ALU.max), r=["lg"], w=[("r8", 0)])
                T.op(DVE, lambda: nc.vector.tensor_scalar(out=R(4, 8), in0=gl, scalar1=R(0, 1), scalar2=None, op0=ALU.is_ge), r=["lg", ("r8", 0)], w=[("r8", 1)])
                T.op(DVE, lambda: nc.vector.tensor_scalar(out=R(1, 2), in0=R(0, 1), scalar1=-1.0, scalar2=None, op0=ALU.mult), r=[("r8", 0)], w=[("r8", 2)])
                T.op(ACT, lambda: nc.scalar.activation(out=R(8, 12), in_=gl, func=AF.Exp, bias=R(1, 2), accum_out=R(2, 3)), r=["lg", ("r8", 2)], w=[("r8", 3)])
                T.op(DVE, lambda: nc.vector.reciprocal(out=R(3, 4), in_=R(2, 3)), r=[("r8", 3)], w=[("r8", 4)])
                T.op(DVE, lambda: nc.vector.tensor_tensor(out=R(16, 48).rearrange("p (g j) -> p g j", g=4), in0=el,
                                                          in1=R(4, 8).unsqueeze(2).to_broadcast([128, 4, 8]), op=ALU.mult),
                     r=["lg", ("r8", 1)], w=[("r8", 5)])
                T.op(DVE, lambda: nc.vector.tensor_reduce(out=R(48, 56), in_=R(16, 48).rearrange("p (g j) -> p j g", g=4), axis=AX.X, op=ALU.add),
                     r=[("r8", 5)], w=[("r8", 6)])
                T.op(DVE, lambda: nc.vector.max(out=R(56, 64), in_=R(48, 56)), r=[("r8", 6)], w=[("r8", 7)])
                T.op(DVE, lambda: nc.vector.tensor_scalar(out=R(64, 72), in0=R(48, 56), scalar1=R(56, 57), scalar2=None, op0=ALU.is_equal),
                     r=[("r8", 6), ("r8", 7)], w=[("r8", 9)])
                T.op(DVE, lambda: nc.vector.tensor_scalar(out=R(72, 80), in0=R(48, 56), scalar1=R(57, 58), scalar2=None, op0=ALU.is_equal),
                     r=[("r8", 6), ("r8", 7)], w=[("r8", 10)])
                T.op(DVE, lambda: nc.vector.tensor_tensor(out=R(1, 2), in0=R(57, 58), in1=R(56, 57), op=ALU.subtract), r=[("r8", 7), ("r8", 3)], w=[("r8", 2)])
                T.op(ACT, lambda: nc.scalar.activation(out=R(2, 3), in_=R(1, 2), func=AF.Exp), r=[("r8", 2), ("r8", 4)], w=[("r8", 3)])
                T.op(DVE, lambda: nc.vector.tensor_scalar(out=R(1, 2), in0=R(2, 3), scalar1=1.0, scalar2=None, op0=ALU.add), r=[("r8", 3)], w=[("r8", 2)])
                T.op(DVE, lambda: nc.vector.reciprocal(out=R(1, 2), in_=R(1, 2)), r=[("r8", 2)], w=[("r8", 2)])
                T.op(DVE, lambda t=t: nc.vector.tensor_tensor(out=w12[:, t, 0:1], in0=R(1, 2), in1=R(3, 4), op=ALU.mult), r=[("r8", 2), ("r8", 4)], w=[("w12", t, 0)])
                T.op(DVE, lambda t=t: nc.vector.tensor_tensor(out=w12[:, t, 1:2], in0=w12[:, t, 0:1], in1=R(2, 3), op=ALU.mult), r=[("w12", t, 0), ("r8", 3)], w=[("w12", t, 1)])
                for g in range(4):
                    T.op(DVE, lambda g=g, t=t: nc.vector.tensor_scalar(out=m1_all[:, t, g * 8:(g + 1) * 8], in0=R(64, 72), scalar1=R(4 + g, 5 + g), scalar2=None, op0=ALU.mult),
                         r=[("r8", 9), ("r8", 1)], w=[("m1", t, g)])
                    T.op(DVE, lambda g=g, t=t: nc.vector.tensor_scalar(out=m2_all[:, t, g * 8:(g + 1) * 8], in0=R(72, 80), scalar1=R(4 + g, 5 + g), scalar2=None, op0=ALU.mult),
                         r=[("r8", 10), ("r8", 1)], w=[("m2", t, g)])
                T.op(DVE, lambda t=t: nc.vector.tensor_tensor(out=ohb[:], in0=m1_all[:, t, :], in1=m2_all[:, t, :], op=ALU.add),
                     r=[("m1", t), ("m2", t)], w=["ohb"])
                T.op(PE, lambda: nc.tensor.matmul(PS[1][:, 0:32], lhsT=tri_b, rhs=ohb[:], start=True, stop=True), r=["ohb", "cstb"], w=[("ps", 1)])
                T.op(DVE, lambda t=t: nc.vector.tensor_tensor(out=rank_all[:, t, :], in0=PS[1][:, 0:32], in1=tot[:], op=ALU.add),
                     r=[("ps", 1), "tot"], w=[("rank", t)])
                T.op(DVE, lambda t=t: nc.vector.tensor_tensor(out=rank_all[:, t, :], in0=rank_all[:, t, :], in1=ohb[:], op=ALU.subtract),
                     r=[("rank", t), "ohb"], w=[("rank", t)])
                T.op(PE, lambda: nc.tensor.matmul(PS[1][:, 0:32], lhsT=ones_b, rhs=ohb[:], start=True, stop=True), r=["ohb", "cstb"], w=[("ps", 1)])
                T.op(DVE, lambda: nc.vector.tensor_tensor(out=tot[:], in0=PS[1][:, 0:32], in1=tot[:], op=ALU.add), r=[("ps", 1), "tot"], w=["tot"])
        T.barrier()
    if dbg and "x1" in dbg_out:
        dump_dram(dbg_out["x1"], x1_d, S, D, F32)
        T.dma(SP, dbg_out["tot"][:, :], tot[:], r=["tot"])
        T.dma(SP, dbg_out["rank"][:, :], rank_all[:].rearrange("p t e -> p (t e)"), r=[("rank",)])
        T.dma(SP, dbg_out["m1"][:, :], m1_all[:].rearrange("p t e -> p (t e)"), r=[("m1",)])
        T.dma(SP, dbg_out["m2"][:, :], m2_all[:].rearrange("p t e -> p (t e)"), r=[("m2",)])
        T.dma(SP, dbg_out["w12"][:, :], w12[:].rearrange("p t e -> p (t e)"), r=[("w12",)])
    if stage <= 5:
        return finish()

    scope_mark("phE")
    with ExitStack() as ph:
        def sb(name, shape, dt):
            return ph.enter_context(nc.sbuf_tensor(name, list(shape), dt))
        P12 = sb("P12", [128, 2, NT], I32)
        NSPL = int(os.environ.get("KSPLIT", 1))
        widx = sb("widx", [128, NSPL, NSLOT], I32)
        with ExitStack() as ph2:
            def sb2(name, shape, dt):
                return ph2.enter_context(nc.sbuf_tensor("s_" + name, list(shape), dt))
            ni = sb2("ni", [128, 32], I32)
            ntf = sb2("ntf", [128, 32], F32)
            onesr = sb2("ones32", [128, 32], F32)
            tend = sb2("tend", [128, 32], F32)
            off = sb2("off", [128, 32], F32)
            big = sb2("big", [128, NT, 32], F32)
            pf = sb2("pf", [128, 2, NT], F32)
            cmp3 = sb2("cmp3", [128, NSLOT, 32], F32)
            sidx = sb2("sidx", [128, NSLOT], F32)
            esl = sb2("esl", [128, NSLOT], F32)
            pio = sb2("pio", [128, 1], F32)
            pioi = sb2("pioi", [128, 1], I32)
            T.op(DVE, lambda: nc.vector.tensor_scalar(out=ntf[:], in0=tot[:], scalar1=127.0, scalar2=None, op0=ALU.add), r=["tot"], w=["ntf"])
            T.op(DVE, lambda: nc.vector.tensor_copy(out=ni[:], in_=ntf[:]), r=["ntf"], w=["ni"])
            T.op(DVE, lambda: nc.vector.tensor_scalar(out=ni[:], in0=ni[:], scalar1=7, scalar2=None, op0=ALU.arith_shift_right),
                 r=["ni"], w=["ni"])
            T.op(DVE, lambda: nc.vector.tensor_copy(out=ntf[:], in_=ni[:]), r=["ni"], w=["ntf"])
            T.op(DVE, lambda: nc.vector.memset(onesr[:], 1.0), w=["ones32"])
            T.op(DVE, lambda: nc.vector.tensor_tensor_scan(out=tend[:], data0=onesr[:], data1=ntf[:], initial=0.0, op0=ALU.mult, op1=ALU.add),
                 r=["ones32", "ntf"], w=["tend"])
            T.op(DVE, lambda: nc.vector.tensor_tensor(out=off[:], in0=tend[:], in1=ntf[:], op=ALU.subtract), r=["tend", "ntf"], w=["off"])
            T.op(DVE, lambda: nc.vector.tensor_scalar(out=off[:], in0=off[:], scalar1=128.0, scalar2=None, op0=ALU.mult), r=["off"], w=["off"])
            for k, mk in enumerate((m1_all, m2_all)):
                T.op(DVE, lambda: nc.vector.tensor_tensor(out=big[:], in0=rank_all[:], in1=off[:].unsqueeze(1).to_broadcast([128, NT, 32]), op=ALU.add),
                     r=[("rank",), "off"], w=["big"])
                T.op(DVE, lambda mk=mk: nc.vector.tensor_tensor(out=big[:], in0=big[:], in1=mk[:], op=ALU.mult), r=["big", ("m1",), ("m2",)], w=["big"])
                T.op(DVE, lambda k=k: nc.vector.tensor_reduce(out=pf[:, k, :], in_=big[:], axis=AX.X, op=ALU.add), r=["big"], w=[("pf", k)])
            T.op(DVE, lambda: nc.vector.tensor_scalar(out=pf[:], in0=pf[:], scalar1=0.0, scalar2=float(NSLOT * 128 - 1), op0=ALU.max, op1=ALU.min),
                 r=[("pf",)], w=[("pf",)])
            T.op(DVE, lambda: nc.vector.tensor_copy(out=P12[:], in_=pf[:]), r=[("pf",)], w=["P12"])
            T.op(POOL, lambda: nc.gpsimd.iota(out=sidx[:], pattern=[[1, NSLOT]], base=0, channel_multiplier=0, allow_small_or_imprecise_dtypes=True), w=["sidx"])
            T.op(POOL, lambda: nc.gpsimd.iota(out=pio[:], pattern=[[1, 1]], base=0, channel_multiplier=1, allow_small_or_imprecise_dtypes=True), w=["pio"])
            T.op(DVE, lambda: nc.vector.tensor_tensor(out=cmp3[:], in0=tend[:].unsqueeze(1).to_broadcast([128, NSLOT, 32]),
                                                      in1=sidx[:].unsqueeze(2).to_broadcast([128, NSLOT, 32]), op=ALU.is_le),
                 r=["tend", "sidx"], w=["cmp3"])
            T.op(DVE, lambda: nc.vector.tensor_reduce(out=esl[:], in_=cmp3[:], axis=AX.X, op=ALU.add), r=["cmp3"], w=["esl"])
            T.op(DVE, lambda: nc.vector.tensor_scalar(out=esl[:], in0=esl[:], scalar1=31.0, scalar2=128.0, op0=ALU.min, op1=ALU.mult), r=["esl"], w=["esl"])
            T.op(DVE, lambda: nc.vector.tensor_scalar(out=esl[:], in0=esl[:], scalar1=pio[:], scalar2=None, op0=ALU.add), r=["esl", "pio"], w=["esl"])
            for ci in range(NSPL):
                T.op(DVE, lambda ci=ci: nc.vector.tensor_scalar(out=sidx[:], in0=esl[:], scalar1=float(NSPL), scalar2=float(ci), op0=ALU.mult, op1=ALU.add),
                     r=["esl", "cmp3"], w=["sidx"])
                T.op(DVE, lambda ci=ci: nc.vector.tensor_copy(out=widx[:, ci, :], in_=sidx[:]), r=["sidx"], w=[("widx", ci)])
            T.barrier()
        if dbg and "P12" in dbg_out:
            T.dma(SP, dbg_out["P12"][:, :], P12[:].rearrange("p k t -> p (k t)"), r=["P12"])
            T.dma(SP, dbg_out["widx"][:, :], widx[:, 0, :], r=["widx"])

        KE = int(os.environ.get("KE", 9))
        if KE >= 2:
            x2l = [sb(f"x2l{i}", [128, D], BF16) for i in range(3)]
            for t in range(NT):
                b = t % 3
                T.dma(SP, x2l[b][:], X2_d[t * 128:(t + 1) * 128, :], w=[("x2l", b)])
                for k in range(2):
                    T.dmai(Xs_d[:, :], x2l[b][:], out_idx=P12[:, k, t:t + 1], r=[("x2l", b), "P12"], w=[("Xs", t, k)])
            T.barrier()

        if KE >= 3:
            xs = [sb(f"xs{i}", [128, D], BF16) for i in range(3)]
            wsl = [sb(f"wsl{i}", [128, 6144], BF16) for i in range(3)]
            xT = [sb(f"xTs{i}", [128, 8, 128], BF16) for i in range(2)]
            sS = [sb(f"sS{i}", [128, 256], F32) for i in range(2)]
            ac = [sb(f"ac{i}", [128, 256], BF16) for i in range(2)]
            aT = [sb(f"aTs{i}", [128, 2, 128], BF16) for i in range(2)]
            yo = [sb(f"yo{i}", [128, D], BF16) for i in range(2)]
            NSL = int(os.environ.get("KSLOTS", NSLOT))

            def load_slot(sl):
                b3 = sl % 3
                T.dma(SP, xs[b3][:], Xs_d[sl * 128:(sl + 1) * 128, :], r=[("Xs",)], w=[("xs", b3)])
                cw_ = 6144 // NSPL
                for ci in range(NSPL):
                    T.dmai(wsl[b3][:, ci * cw_:(ci + 1) * cw_], Wcat_d.rearrange("r (c w) -> (r c) w", c=NSPL), in_idx=widx[:, ci, sl:sl + 1],
                           r=[("Wcat",), "widx"], w=[("wsl", b3, ci)])

            def slot_s1(sl):
                b2, b3 = sl % 2, sl % 3
                base = 4 * b2
                xTp = psb(base).rearrange("p (k t) -> p k t", k=8)
                for kc in range(8):
                    T.op(PE, lambda kc=kc: nc.tensor.transpose(xTp[:, kc, :], xs[b3][:, kc * 128:(kc + 1) * 128], ident_b),
                         r=[("xs", b3), "cstb"], w=[("ps", base)])
                T.op(ACT, lambda: nc.scalar.copy(out=xT[b2][:, 0:4, :], in_=xTp[:, 0:4, :]), r=[("ps", base)], w=[("xTs", b2, 0)])
                T.op(DVE, lambda: nc.vector.tensor_copy(out=xT[b2][:, 4:8, :], in_=xTp[:, 4:8, :]), r=[("ps", base), ("xTs", b2, 0)], w=[("xTs", b2, 1)])
                for kc in range(8):
                    T.op(PE, lambda kc=kc: nc.tensor.matmul(PS[base + 1][:, :], lhsT=xT[b2][:, kc, :], rhs=wsl[b3][:, kc * 512:(kc + 1) * 512],
                                                            start=(kc == 0), stop=(kc == 7)), r=[("xTs", b2), ("wsl", b3)], w=[("ps", base + 1)])

            def slot_s2(sl):
                b2, b3 = sl % 2, sl % 3
                base = 4 * b2
                T.op(ACT, lambda: nc.scalar.activation(out=sS[b2][:], in_=PS[base + 1][:, 0:256], func=AF.Silu), r=[("ps", base + 1)], w=[("sS", b2)])
                T.op(DVE, lambda: nc.vector.tensor_tensor(out=ac[b2][:], in0=PS[base + 1][:, 256:512], in1=sS[b2][:], op=ALU.mult),
                     r=[("ps", base + 1), ("sS", b2)], w=[("ac", b2)])
                aTp = psb(base)[:, 0:256].rearrange("p (k t) -> p k t", k=2)
                for fc in range(2):
                    T.op(PE, lambda fc=fc: nc.tensor.transpose(aTp[:, fc, :], ac[b2][:, fc * 128:(fc + 1) * 128], ident_b),
                         r=[("ac", b2), "cstb"], w=[("ps", base)])
                T.op(ACT, lambda: nc.scalar.copy(out=aT[b2][:], in_=aTp), r=[("ps", base)], w=[("aTs", b2)])
                for cb_ in range(2):
                    for fc in range(2):
                        T.op(PE, lambda fc=fc, cb_=cb_: nc.tensor.matmul(
                            PS[base + 2 + cb_][:, :], lhsT=aT[b2][:, fc, :], rhs=wsl[b3][:, 4096 + fc * 1024 + cb_ * 512: 4096 + fc * 1024 + (cb_ + 1) * 512],
                            start=(fc == 0), stop=(fc == 1)), r=[("aTs", b2), ("wsl", b3)], w=[("ps", base + 2 + cb_)])
                T.op(ACT, lambda: nc.scalar.copy(out=yo[b2][:, 0:512], in_=PS[base + 2][:, :]), r=[("ps", base + 2)], w=[("yo", b2, 0)])
                T.op(DVE, lambda: nc.vector.tensor_copy(out=yo[b2][:, 512:1024], in_=PS[base + 3][:, :]), r=[("ps", base + 3)], w=[("yo", b2, 1)])
                T.dma(SP, Ys_d[sl * 128:(sl + 1) * 128, :], yo[b2][:], r=[("yo", b2)], w=[("Ys", sl)])

            for sl in range(min(2, NSL)):
                load_slot(sl)
            slot_s1(0)
            for sl in range(NSL):
                if sl + 2 < NSL:
                    load_slot(sl + 2)
                if sl + 1 < NSL:
                    slot_s1(sl + 1)
                slot_s2(sl)
            T.barrier()

        if KE >= 4:
            y1 = [sb(f"y1_{i}", [128, D], BF16) for i in range(3)]
            ya = [sb(f"ya_{i}", [128, D], F32) for i in range(3)]
            y2 = [sb(f"y2_{i}", [128, D], BF16) for i in range(3)]
            x1l = [sb(f"x1l{i}", [128, D], F32) for i in range(3)]
            def load_tok(t):
                b = t % 3
                T.dma(SP, x1l[b][:], x1_d[t * 128:(t + 1) * 128, :], w=[("x1l", b)])
                T.dmai(y1[b][:], Ys_d[:, :], in_idx=P12[:, 0, t:t + 1], r=[("Ys",), "P12"], w=[("y1", b)])
                T.dmai(y2[b][:], Ys_d[:, :], in_idx=P12[:, 1, t:t + 1], r=[("Ys",), "P12"], w=[("y2", b)])

            load_tok(0)
            load_tok(1)
            for t in range(NT):
                b = t % 3
                if t + 2 < NT:
                    load_tok(t + 2)
                T.op(DVE, lambda b=b, t=t: nc.vector.tensor_scalar(out=ya[b][:], in0=y1[b][:], scalar1=w12[:, t, 0:1], scalar2=None, op0=ALU.mult),
                     r=[("y1", b), ("w12",)], w=[("ya", b)])
                T.op(DVE, lambda b=b, t=t: nc.vector.scalar_tensor_tensor(out=ya[b][:], in0=y2[b][:], scalar=w12[:, t, 1:2], in1=ya[b][:], op0=ALU.mult, op1=ALU.add),
                     r=[("ya", b), ("y2", b), ("w12",)], w=[("ya", b)])
                T.op(DVE, lambda b=b: nc.vector.tensor_tensor(out=ya[b][:], in0=ya[b][:], in1=g2b[:], op=ALU.mult), r=[("ya", b), ("g2b",)], w=[("ya", b)])
                T.op(DVE, lambda b=b: nc.vector.tensor_tensor(out=x1l[b][:], in0=x1l[b][:], in1=ya[b][:], op=ALU.add), r=[("ya", b), ("x1l", b)], w=[("x1l", b)])
                T.dma(SP, out_d[t * 128:(t + 1) * 128, :], x1l[b][:], r=[("x1l", b)], w=[("out", t)])
            T.barrier()
    return finish()


def _consts():
    c = np.zeros((128, NCST), np.float32)
    c[:, 0:128] = np.eye(128, dtype=np.float32)
    s = np.arange(128)[:, None]
    t = np.arange(128)[None, :]
    c[:, 128:256] = (s <= t).astype(np.float32)
    c[:, 256:384] = ((s // 64) == (t // 64)).astype(np.float32)
    R = np.zeros((128, 128), np.float32)
    for base in (0, 64):
        for d in range(8):
            R[base + d, base + d + 8] = -1.0
            R[base + d + 8, base + d] = 1.0
    c[:, 384:512] = R.T
    c[:, 512:640] = 1.0
    inv = 500000.0 ** (-np.arange(0, 16, 2, dtype=np.float32) / 16.0)
    for p in range(128):
        d = p % 64
        c[p, 640] = inv[d % 8] if d < 16 else 0.0
    return c


_NC_CACHE = {}


def make_in_maps(inputs):
    f = lambda k: np.ascontiguousarray(np.asarray(inputs[k], dtype=np.float32))
    x = f("x")
    c = f("c")
    pos = np.ascontiguousarray(np.asarray(inputs["positions"], dtype=np.int32))
    b_ada = f("b_ada")[0]
    shared = {
        "w_ada": f("w_ada")[0],
        "b_ada_col": np.ascontiguousarray(b_ada.reshape(48, 128).T),
        "b_ada_row": np.ascontiguousarray(b_ada.reshape(1, -1)),
        "n1w": np.ascontiguousarray(f("norm1_w")[0].reshape(8, 128).T),
        "n2w": np.ascontiguousarray(f("norm2_w")[0].reshape(8, 128).T),
        "n2row": np.ascontiguousarray(f("norm2_w")[0].reshape(1, D)),
        "w_in": f("w_in")[0],
        "bgate": np.ascontiguousarray(np.stack([f("b_igate")[0], f("b_fgate")[0]], axis=1)),
        "convw": np.ascontiguousarray(f("conv_w")[0].T.reshape(8, 128, 4).transpose(1, 0, 2)),
        "convb": np.ascontiguousarray(f("conv_b")[0].reshape(8, 128).T),
        "mnw": np.ascontiguousarray(f("mlstm_norm_w")[0].reshape(1, 512)),
        "qnw": np.ascontiguousarray(np.tile(f("q_norm_w")[0], 2).reshape(128, 1)),
        "knw": np.ascontiguousarray(np.tile(f("k_norm_w")[0], 2).reshape(128, 1)),
        "qkrow": np.ascontiguousarray(np.concatenate([f("q_norm_w")[0], f("k_norm_w")[0]]).reshape(1, 128)),
        "lamv": np.ascontiguousarray(np.concatenate([f("lam_q1")[0], f("lam_k1")[0], f("lam_q2")[0], f("lam_k2")[0]]).reshape(1, 256)),
        "subw": np.ascontiguousarray(f("subln_w")[0].reshape(128, 1)),
        "w_br_m": f("w_br_m")[0],
        "w_br_d": f("w_br_d")[0],
        "w_out": f("w_out")[0],
        "wr": np.ascontiguousarray(np.concatenate([f("w_rg")[0], f("w_re")[0]], axis=1)),
        "br": np.ascontiguousarray(np.concatenate([f("b_rg")[0], f("b_re")[0]]).reshape(1, 36)),
        "w_gate": f("w_gate")[0],
        "w_up": f("w_up")[0],
        "w_down": f("w_down")[0],
        "consts": _consts(),
    }
    maps = []
    for b in range(8):
        m = dict(shared)
        m["x"] = x[b]
        m["cT"] = np.ascontiguousarray(c[b].reshape(8, 128).T)
        m["pos"] = np.ascontiguousarray(pos[b].reshape(1, S))
        maps.append(m)
    return maps


def kernel(**inputs):
    if "nc" not in _NC_CACHE:
        _NC_CACHE["nc"] = build()
    nc = _NC_CACHE["nc"]
    maps = make_in_maps(inputs)
    res = run_bass_kernel_spmd(nc, maps, core_ids=list(range(8)))
    out = np.stack([np.asarray(r["out"], dtype=np.float32) for r in res.results], axis=0)
    return out
```

```python
import math
import os
from contextlib import ExitStack

import numpy as np
import concourse.bass as bass
import concourse.mybir as mybir
from concourse.bass_utils import run_bass_kernel_spmd

F32 = mybir.dt.float32
BF16 = mybir.dt.bfloat16
I32 = mybir.dt.int32
AF = mybir.ActivationFunctionType
ALU = mybir.AluOpType
AX = mybir.AxisListType

S = 4096
D = 1024
NT = 32
NB = 8
EPS = 1e-6
NCST = 5 * 128 + 2


class _StopBuild(Exception):
    pass


class Trk:
    LIMIT = 50000
    NDSEM = 12

    def __init__(self, nc):
        self.nc = nc
        self.eng = {"pe": nc.tensor, "act": nc.scalar, "dve": nc.vector, "pool": nc.gpsimd, "sp": nc.sync}
        self.cur = {}
        self.seq = {}
        self.gen = {}
        for e in self.eng:
            self.cur[e] = [nc.alloc_semaphore(f"s_{e}_0"), 0]
            self.seq[e] = 0
            self.gen[e] = 0
        self.allsems = [(e, self.cur[e]) for e in self.eng]
        self.waited = {e: {} for e in self.eng}
        self.lastw = {}
        self.readers = {}
        self.children = {}
        self.dring = {}
        for q in ("sp", "pool", "act", "wc"):
            self.dring[q] = [[nc.alloc_semaphore(f"d_{q}_{i}"), 0] for i in range(self.NDSEM)]
        self.dpos = {q: 0 for q in self.dring}
        self.all_dma = []

    def _related(self, k):
        ks = [k[:i] for i in range(1, len(k) + 1)]
        stack = [k]
        while stack:
            p = stack.pop()
            for c in self.children.get(p, ()):
                ks.append(c)
                stack.append(c)
        return ks

    def _register(self, k):
        for i in range(1, len(k)):
            self.children.setdefault(k[:i], set()).add(k[:i + 1])

    def _wait(self, e, ev):
        sem, val, src, sq = ev
        if src == e:
            if e == "pe":
                return
            if e != "pool" and self.seq[e] - sq >= 6:
                return
        w = self.waited[e]
        if w.get(sem.name, 0) >= val:
            return
        self.eng[e].wait_ge(sem, val)
        w[sem.name] = val

    def _deps(self, e, r, w):
        evs = []
        for k in r:
            for kk in self._related(k):
                if kk in self.lastw:
                    evs.append(self.lastw[kk])
        for k in w:
            for kk in self._related(k):
                if kk in self.lastw:
                    evs.append(self.lastw[kk])
                evs.extend(self.readers.get(kk, {}).values())
        for ev in evs:
            self._wait(e, ev)

    def _record(self, ev, r, w):
        for k in r:
            self._register(k)
            self.readers.setdefault(k, {})[ev[2] if ev[2] is not None else ev[0].name] = ev
        for k in w:
            self._register(k)
            self.lastw[k] = ev
            self.readers[k] = {}
            for kk in self._related(k):
                if kk != k and len(kk) > len(k):
                    self.readers[kk] = {}
                    self.lastw.pop(kk, None)

    @staticmethod
    def _norm(ks):
        out = []
        for k in ks:
            k = tuple(k) if isinstance(k, (tuple, list)) else (k,)
            if k[0] == "ps":
                k = k[:2]
            out.append(k)
        return out

    def start_record(self):
        self._rec = []

    def stop_record(self):
        rec, self._rec = self._rec, None
        return rec

    def op(self, e, fn, r=(), w=()):
        if getattr(self, "_rec", None) is not None:
            self._rec.append((e, fn, r, w))
            return None
        r = self._norm(r)
        w = self._norm(w)
        w = w + [k for k in r if k[0] == "ps" and k not in w]
        self._deps(e, r, w)
        ins = fn()
        c = self.cur[e]
        c[1] += 1
        self.seq[e] += 1
        ins.then_inc(c[0], 1)
        ev = (c[0], c[1], e, self.seq[e])
        self._record(ev, r, w)
        if c[1] >= self.LIMIT:
            self.gen[e] += 1
            self.cur[e] = [self.nc.alloc_semaphore(f"s_{e}_{self.gen[e]}"), 0]
            self.allsems.append((e, self.cur[e]))
        return ins

    def dma(self, q, out, in_, r=(), w=(), ring=None, **kw):
        r = self._norm(r)
        w = self._norm(w)
        rq = ring or q
        ring = self.dring[rq]
        slot = ring[self.dpos[rq] % self.NDSEM]
        self.dpos[rq] += 1
        if slot[1] > 0:
            self._wait(q, (slot[0], slot[1], None, 0))
        self._deps(q, r, w)
        ins = self.eng[q].dma_start(out=out, in_=in_, **kw)
        slot[1] += 16
        ins.then_inc(slot[0], 16)
        ev = (slot[0], slot[1], None, 0)
        self._record(ev, r, w)
        return ins

    def dmai(self, out, in_, out_idx=None, in_idx=None, r=(), w=()):
        q = "pool"
        r = self._norm(r)
        w = self._norm(w)
        ring = self.dring[q]
        slot = ring[self.dpos[q] % self.NDSEM]
        self.dpos[q] += 1
        if slot[1] > 0:
            self._wait(q, (slot[0], slot[1], None, 0))
        self._deps(q, r, w)
        ins = self.nc.gpsimd.indirect_dma_start(
            out=out, out_offset=(bass.IndirectOffsetOnAxis(ap=out_idx, axis=0) if out_idx is not None else None),
            in_=in_, in_offset=(bass.IndirectOffsetOnAxis(ap=in_idx, axis=0) if in_idx is not None else None))
        slot[1] += 16
        ins.then_inc(slot[0], 16)
        ev = (slot[0], slot[1], None, 0)
        self._record(ev, r, w)
        return ins

    def barrier(self, full=True):
        evs = []
        for (se, c) in self.allsems:
            if c[1] > 0:
                evs.append((c[0], c[1], "__" + se, 0))
        for q, ring in self.dring.items():
            if q == "wc" and not full:
                continue
            for slot in ring:
                if slot[1] > 0:
                    evs.append((slot[0], slot[1], None, 0))
        for e in self.eng:
            for ev in evs:
                if ev[2] == "__" + e:
                    continue
                self._wait(e, ev)
        self.lastw.clear()
        self.readers.clear()
        self.children.clear()


def build(stage=99, dbg=None):
    nc = bass.Bass("TRN2", target_bir_lowering=False)
    T = Trk(nc)
    _scope = [None]

    def scope_mark(nm):
        if os.environ.get("KSCOPE") != "1":
            return
        if _scope[0] is not None:
            _scope[0].__exit__(None, None, None)
        _scope[0] = nc.named_scope(nm)
        _scope[0].__enter__()

    PE, ACT, DVE, POOL, SP = "pe", "act", "dve", "pool", "sp"

    def din(name, shape, dt=F32):
        return nc.dram_tensor(name, list(shape), dt, kind="ExternalInput").ap()

    def dscr(name, shape, dt):
        return nc.dram_tensor(name, list(shape), dt, kind="Internal").ap()

    x_in = din("x", [S, D])
    cT_in = din("cT", [128, 8])
    pos_in = din("pos", [1, S], I32)
    w_ada = din("w_ada", [D, 6 * D])
    b_ada_col = din("b_ada_col", [128, 48])
    b_ada_row = din("b_ada_row", [1, 6 * D])
    n1w_in = din("n1w", [128, 8])
    n2w_in = din("n2w", [128, 8])
    n2row_in = din("n2row", [1, D])
    w_in = din("w_in", [D, 5640])
    bgate_in = din("bgate", [4, 2])
    convw_in = din("convw", [128, 8, 4])
    convb_in = din("convb", [128, 8])
    mnw_in = din("mnw", [1, 512])
    qnw_in = din("qnw", [128, 1])
    knw_in = din("knw", [128, 1])
    qkrow_in = din("qkrow", [1, 128])
    lamv_in = din("lamv", [1, 256])
    subw_in = din("subw", [128, 1])
    w_br_m = din("w_br_m", [512, D])
    w_br_d = din("w_br_d", [512, D])
    w_out = din("w_out", [D, D])
    wr_in = din("wr", [D, 36])
    br_in = din("br", [1, 36])
    w_gate = din("w_gate", [32, D, 256])
    w_up = din("w_up", [32, D, 256])
    w_down = din("w_down", [32, 256, D])
    consts_in = din("consts", [128, NCST])
    out_d = nc.dram_tensor("out", [S, D], F32, kind="ExternalOutput").ap()

    xnT_d = dscr("xnT_d", [D, S], BF16)
    hmT_d = dscr("hmT_d", [512, S], BF16)
    hdT_d = dscr("hdT_d", [512, S], BF16)
    x1_d = dscr("x1_d", [S, D], F32)
    xn2T_d = dscr("xn2T_d", [D, S], BF16)
    combT_d = dscr("combT_d", [32, S], BF16)
    dec_d = dscr("dec_d", [4, 32], F32)
    gi_d = dscr("gi_d", [4, S], F32)
    NSLOT = 96
    X2_d = dscr("X2_d", [S, D], BF16)
    Xs_d = dscr("Xs_d", [NSLOT * 128, D], BF16)
    Ys_d = dscr("Ys_d", [NSLOT * 128, D], BF16)
    Wcat_d = dscr("Wcat_d", [32 * 128, 6144], BF16)
    gf_d = dscr("gf_d", [4, S], F32)

    dbg_out = {}
    if dbg:
        for name, (shape, dt) in dbg.items():
            dbg_out[name] = nc.dram_tensor("dbg_" + name, list(shape), dt, kind="ExternalOutput").ap()

    PS = [nc.alloc_psum_tensor(f"ps{i}", [128, 512], F32) for i in range(8)]

    def psb(i):
        return PS[i][:].bitcast(BF16)

    xnT_v = xnT_d.rearrange("(k p) t -> p k t", p=128)
    xn2T_v = xn2T_d.rearrange("(k p) t -> p k t", p=128)
    hmT_v = hmT_d.rearrange("(k p) t -> p k t", p=128)
    hdT_v = hdT_d.rearrange("(k p) t -> p k t", p=128)
    w_in_v = w_in.rearrange("(k p) n -> p k n", p=128)
    wg_v = w_gate.rearrange("e (k p) f -> e p k f", p=128)
    wu_v = w_up.rearrange("e (k p) f -> e p k f", p=128)
    wdn_v = w_down.rearrange("e (k p) n -> e p k n", p=128)

    glob = ExitStack()

    def sbg(name, shape, dt):
        return glob.enter_context(nc.sbuf_tensor("s_" + name, list(shape), dt))

    cst = sbg("cst", [128, NCST], F32)
    cstb = sbg("cstb", [128, 5 * 128], BF16)
    modc = sbg("modc", [128, 48], F32)
    a1 = sbg("a1", [128, 8], F32)
    a2 = sbg("a2", [128, 8], F32)
    g1b = sbg("g1b", [128, D], F32)
    g2b = sbg("g2b", [128, D], F32)
    a2b = sbg("a2b", [128, D], F32)
    s2b = sbg("s2b", [128, D], F32)
    rank_all = sbg("rank_all", [128, NT, 32], F32)
    m1_all = sbg("m1_all", [128, NT, 32], F32)
    m2_all = sbg("m2_all", [128, NT, 32], F32)
    w12 = sbg("w12", [128, NT, 2], F32)
    tot = sbg("tot", [128, 32], F32)
    smallc = sbg("smallc", [128, 16], F32)
    ident_f = cst[:, 0:128]
    tri_f = cst[:, 128:256]
    invf = cst[:, 640:641]
    ident_b = cstb[:, 0:128]
    tri_b = cstb[:, 128:256]
    bo64_b = cstb[:, 256:384]
    RT_b = cstb[:, 384:512]
    ones_b = cstb[:, 512:640]
    ones_f = cst[:, 512:640]

    T.dma(SP, cst[:], consts_in[:, :], w=["cst"])
    T.op(DVE, lambda: nc.vector.tensor_copy(out=cstb[:], in_=cst[:, 0:640]), r=["cst"], w=["cstb"])

    early2 = ExitStack()
    CosB = early2.enter_context(nc.sbuf_tensor("s_CosB", [128, S], BF16))
    SinB = early2.enter_context(nc.sbuf_tensor("s_SinB", [128, S], BF16))
    early2_open = [True]

    def sin_reduce(ph_sb, dst_bf, ang, n, tagk):
        ki = ph_sb["ki"]
        kf = ph_sb["kf"]
        msk = ph_sb["msk"]
        C1 = 6.28125
        C2 = 2.0 * math.pi - 6.28125
        T.op(DVE, lambda: nc.vector.tensor_scalar(out=ki[:, 0:n], in0=ang, scalar1=1.0 / (2.0 * math.pi), scalar2=None, op0=ALU.mult),
             r=[tagk], w=["ki"])
        T.op(DVE, lambda: nc.vector.tensor_copy(out=kf[:, 0:n], in_=ki[:, 0:n]), r=["ki"], w=["kf"])
        T.op(DVE, lambda: nc.vector.scalar_tensor_tensor(out=ang, in0=kf[:, 0:n], scalar=-C1, in1=ang, op0=ALU.mult, op1=ALU.add),
             r=["kf", tagk], w=[tagk])
        T.op(DVE, lambda: nc.vector.scalar_tensor_tensor(out=ang, in0=kf[:, 0:n], scalar=-C2, in1=ang, op0=ALU.mult, op1=ALU.add),
             r=["kf", tagk], w=[tagk])
        T.op(DVE, lambda: nc.vector.tensor_scalar(out=msk[:, 0:n], in0=ang, scalar1=math.pi, scalar2=-2.0 * math.pi, op0=ALU.is_gt, op1=ALU.mult),
             r=[tagk], w=["msk"])
        T.op(DVE, lambda: nc.vector.tensor_tensor(out=ang, in0=ang, in1=msk[:, 0:n], op=ALU.add), r=[tagk, "msk"], w=[tagk])
        T.op(DVE, lambda: nc.vector.tensor_scalar(out=msk[:, 0:n], in0=ang, scalar1=-math.pi, scalar2=2.0 * math.pi, op0=ALU.is_lt, op1=ALU.mult),
             r=[tagk], w=["msk"])
        T.op(DVE, lambda: nc.vector.tensor_tensor(out=ang, in0=ang, in1=msk[:, 0:n], op=ALU.add), r=[tagk, "msk"], w=[tagk])
        T.op(DVE, lambda: nc.vector.tensor_scalar(out=ang, in0=ang, scalar1=math.pi, scalar2=-math.pi, op0=ALU.min, op1=ALU.max),
             r=[tagk], w=[tagk])
        T.op(ACT, lambda: nc.scalar.activation(out=dst_bf, in_=ang, func=AF.Sin), r=[tagk], w=[tagk + "_o"])

    early = ExitStack()
    NWST = int(os.environ.get("KWST", 2))
    wst = [early.enter_context(nc.sbuf_tensor(f"s_wst{i}", [128, 6144], BF16)) for i in range(NWST)]
    for e in range(32):
        b = e % NWST
        gu = wst[b][:, 0:4096].rearrange("p (k f) -> p k f", k=8)
        T.dma(POOL, gu[:, :, 0:256], wg_v[e], w=[("wst", b, 0)], ring="wc")
        T.dma(POOL, gu[:, :, 256:512], wu_v[e], w=[("wst", b, 1)], ring="wc")
        T.dma(POOL, wst[b][:, 4096:6144].rearrange("p (k n) -> p k n", k=2), wdn_v[e], w=[("wst", b, 2)], ring="wc")
        T.dma(POOL, Wcat_d[e * 128:(e + 1) * 128, :], wst[b][:], r=[("wst", b)], w=[("Wcat", e)], ring="wc")
    early_open = [True]
    scope_mark("ph0")
    with ExitStack() as ph:
        def sb(name, shape, dt):
            return ph.enter_context(nc.sbuf_tensor("s_" + name, list(shape), dt))
        cT = sb("cT", [128, 8], F32)
        cT2 = sb("cT2", [128, 8, 2], F32)
        cbc = sb("cbc", [128, 8, 128], F32)
        wa = [sb(f"wa{i}", [128, 8, 512], F32) for i in range(2)]
        bcol = sb("bcol", [128, 48], F32)
        brow = sb("brow", [128, 2048], F32)
        nw = sb("nw", [128, 16], F32)
        tmp8 = sb("tmp8", [128, 8], F32)
        lamb = sb("lamb", [128, 256], F32)
        qkrow = sb("qkrow", [128, 128], F32)
        lt = sb("lt", [128, 128], F32)
        l2 = sb("l2", [128, 4], F32)

        T.dma(SP, cT[:], cT_in[:, :], w=["cT"])
        T.dma(SP, bcol[:], b_ada_col[:, :], w=["bcol"])
        T.dma(SP, brow[:, 0:1024], b_ada_row[0:1, 2048:3072].partition_broadcast(128), w=["brow0"])
        T.dma(SP, brow[:, 1024:2048], b_ada_row[0:1, 5120:6144].partition_broadcast(128), w=["brow1"])
        T.dma(SP, nw[:, 0:8], n1w_in[:, :], w=["nw0"])
        T.dma(SP, nw[:, 8:16], n2w_in[:, :], w=["nw1"])
        T.dma(SP, smallc[:, 0:1], qnw_in[:, :], w=["sc0"])
        T.dma(SP, smallc[:, 1:2], knw_in[:, :], w=["sc1"])
        T.dma(SP, smallc[:, 2:3], subw_in[:, :], w=["sc2"])
        T.dma(SP, lamb[:], lamv_in[0:1, :].partition_broadcast(128), w=["lamb"])
        T.dma(SP, qkrow[:], qkrow_in[0:1, :].partition_broadcast(128), w=["qkrow"])
        for k in range(2):
            T.op(DVE, lambda k=k: nc.vector.tensor_copy(out=cT2[:, :, k], in_=cT[:]), r=["cT"], w=[("cT2", k)])
        for kc in range(8):
            T.op(DVE, lambda kc=kc: nc.vector.tensor_copy(out=cbc[:, kc, :], in_=cT[:, kc:kc + 1].to_broadcast([128, 128])),
                 r=["cT"], w=[("cbc", kc)])
        if True:
            posi = ph.enter_context(nc.sbuf_tensor("s_posi", [128, 1024], I32))
            posf = ph.enter_context(nc.sbuf_tensor("s_posf", [128, 1024], F32))
            ang = ph.enter_context(nc.sbuf_tensor("s_ang", [128, 1024], F32))
            tb = dict(ki=ph.enter_context(nc.sbuf_tensor("s_ki", [128, 1024], I32)),
                      kf=ph.enter_context(nc.sbuf_tensor("s_kf", [128, 1024], F32)),
                      msk=ph.enter_context(nc.sbuf_tensor("s_msk", [128, 1024], F32)))
            for c4 in range(4):
                cs = slice(c4 * 1024, (c4 + 1) * 1024)
                T.dma(SP, posi[:], pos_in[0:1, cs].partition_broadcast(128), w=["posi"])
                T.op(DVE, lambda: nc.vector.tensor_copy(out=posf[:], in_=posi[:]), r=["posi"], w=["posf"])
                T.op(DVE, lambda: nc.vector.tensor_scalar(out=ang[:], in0=posf[:], scalar1=invf, scalar2=None, op0=ALU.mult),
                     r=["posf", "cst"], w=["ang"])
                sin_reduce(tb, SinB[:, cs], ang[:], 1024, "ang")
                T.op(DVE, lambda: nc.vector.tensor_scalar(out=ang[:], in0=posf[:], scalar1=invf, scalar2=math.pi / 2, op0=ALU.mult, op1=ALU.add),
                     r=["posf", "cst", "ang_o"], w=["ang"])
                sin_reduce(tb, CosB[:, cs], ang[:], 1024, "ang")

        w_ada_v = w_ada.rearrange("(k p) n -> p k n", p=128)
        gate_bank = {4: 1, 5: 2, 6: 5, 7: 6, 8: 1, 9: 2, 10: 3, 11: 4}
        n2rb = sb("n2rb", [128, D], F32)
        T.dma(SP, n2rb[:], n2row_in[0:1, :].partition_broadcast(128), w=["n2rb"])
        T.dma(SP, s2b[:], b_ada_row[0:1, 3072:4096].partition_broadcast(128), w=["s2b_bias"])
        T.dma(SP, a2b[:], b_ada_row[0:1, 4096:5120].partition_broadcast(128), w=["a2b_bias"])
        for blk in range(12):
            b = blk % 2
            for hh in range(2):
                T.dma(SP, wa[b][:, 4 * hh:4 * hh + 4, :], w_ada_v[:, 4 * hh:4 * hh + 4, blk * 512:(blk + 1) * 512], w=[("wa", b)])
            for m in range(4):
                j = blk * 4 + m
                for kc in range(8):
                    T.op(PE, lambda j=j, m=m, kc=kc, b=b: nc.tensor.matmul(
                        PS[0][:, 2 * j:2 * j + 2], lhsT=wa[b][:, kc, m * 128:(m + 1) * 128], rhs=cT2[:, kc, :],
                        start=(kc == 0), stop=(kc == 7)), r=[("wa", b), "cT2"], w=[("ps", 0)])
            if blk in gate_bank:
                gb = gate_bank[blk]
                for kc in range(8):
                    T.op(PE, lambda kc=kc, b=b, gb=gb: nc.tensor.matmul(
                        PS[gb][:, :], lhsT=cbc[:, kc, :], rhs=wa[b][:, kc, :], start=(kc == 0), stop=(kc == 7)),
                        r=[("wa", b), "cbc"], w=[("ps", gb)])
                if blk == 5:
                    for hh in range(2):
                        T.op(DVE, lambda hh=hh: nc.vector.tensor_tensor(out=g1b[:, hh * 512:(hh + 1) * 512], in0=PS[1 + hh][:, :],
                                                                      in1=brow[:, hh * 512:(hh + 1) * 512], op=ALU.add),
                             r=[("ps", 1 + hh), "brow0"], w=[("g1b", hh)])
                if blk == 7:
                    for hh in range(2):
                        T.op(DVE, lambda hh=hh: nc.vector.tensor_tensor(out=s2b[:, hh * 512:(hh + 1) * 512], in0=PS[5 + hh][:, :],
                                                                      in1=s2b[:, hh * 512:(hh + 1) * 512], op=ALU.add),
                             r=[("ps", 5 + hh), "s2b_bias"], w=[("s2b", hh)])
                if blk == 9:
                    for hh in range(2):
                        sl = slice(hh * 512, (hh + 1) * 512)
                        T.op(DVE, lambda hh=hh, sl=sl: nc.vector.tensor_tensor(out=a2b[:, sl], in0=PS[1 + hh][:, :], in1=a2b[:, sl], op=ALU.add),
                             r=[("ps", 1 + hh), "a2b_bias"], w=[("a2b", hh)])
                        T.op(DVE, lambda sl=sl: nc.vector.scalar_tensor_tensor(out=a2b[:, sl], in0=a2b[:, sl], scalar=1.0, in1=n2rb[:, sl], op0=ALU.add, op1=ALU.mult),
                             r=[("a2b", hh), "n2rb"], w=[("a2b", hh)])
        T.op(DVE, lambda: nc.vector.tensor_tensor(
            out=modc[:], in0=PS[0][:, 0:96].rearrange("p (m two) -> p m two", two=2)[:, :, 0], in1=bcol[:], op=ALU.add),
            r=[("ps", 0), "bcol"], w=["modc"])
        for gi, (gt, b0) in enumerate(((g1b, 1), (g2b, 3))):
            if gi == 0:
                continue
            for hh in range(2):
                T.op(DVE, lambda gt=gt, b0=b0, hh=hh, gi=gi: nc.vector.tensor_tensor(
                    out=gt[:, hh * 512:(hh + 1) * 512], in0=PS[b0 + hh][:, :],
                    in1=brow[:, gi * 1024 + hh * 512: gi * 1024 + (hh + 1) * 512], op=ALU.add),
                    r=[("ps", b0 + hh), f"brow{gi}"], w=[(f"g{gi + 1}b", hh)])
        for (av, sc0, nwo, akey) in ((a1, 8, 0, "a1"), (a2, 32, 8, "a2")):
            T.op(DVE, lambda sc0=sc0: nc.vector.tensor_scalar(out=tmp8[:], in0=modc[:, sc0:sc0 + 8], scalar1=1.0, scalar2=None, op0=ALU.add),
                 r=["modc"], w=["tmp8"])
            T.op(DVE, lambda av=av, nwo=nwo: nc.vector.tensor_tensor(out=av[:], in0=tmp8[:], in1=nw[:, nwo:nwo + 8], op=ALU.mult),
                 r=["tmp8", "nw0", "nw1"], w=[akey])
        T.op(DVE, lambda: nc.vector.tensor_scalar(out=smallc[:, 2:3], in0=smallc[:, 2:3], scalar1=0.8, scalar2=None, op0=ALU.mult),
             r=["sc2"], w=["sc2"])
        T.op(DVE, lambda: nc.vector.tensor_reduce(out=l2[:, 0:2], in_=qkrow[:].rearrange("p (a b) -> p a b", a=2), axis=AX.X, op=ALU.max,
                                                  apply_absolute_value=True), r=["qkrow"], w=["l2a"])
        T.op(DVE, lambda: nc.vector.tensor_tensor(out=l2[:, 2:3], in0=l2[:, 0:1], in1=l2[:, 1:2], op=ALU.mult), r=["l2a"], w=["l2b"])
        T.op(DVE, lambda: nc.vector.tensor_scalar(out=smallc[:, 3:4], in0=l2[:, 2:3], scalar1=-8.0, scalar2=None, op0=ALU.mult),
             r=["l2b"], w=["sc3"])
        lv = lamb[:].rearrange("p (a b) -> p a b", a=4)
        T.op(DVE, lambda: nc.vector.tensor_tensor(out=lt[:].rearrange("p (a b) -> p a b", a=2), in0=lv[:, 0:4:2, :], in1=lv[:, 1:4:2, :], op=ALU.mult),
             r=["lamb"], w=["lt"])
        T.op(DVE, lambda: nc.vector.tensor_reduce(out=l2[:, 0:2], in_=lt[:].rearrange("p (a b) -> p a b", a=2), axis=AX.X, op=ALU.add),
             r=["lt", "l2a", "l2b"], w=["l2a"])
        T.op(ACT, lambda: nc.scalar.activation(out=l2[:, 2:4], in_=l2[:, 0:2], func=AF.Exp), r=["l2a"], w=["l2b"])
        T.op(DVE, lambda: nc.vector.tensor_tensor(out=l2[:, 0:1], in0=l2[:, 3:4], in1=l2[:, 2:3], op=ALU.subtract), r=["l2b"], w=["l2a"])
        T.op(DVE, lambda: nc.vector.tensor_scalar(out=smallc[:, 4:5], in0=l2[:, 0:1], scalar1=-0.2, scalar2=None, op0=ALU.add),
             r=["l2a"], w=["sc4"])
        T.barrier(full=False)
    if dbg and "modc" in dbg_out:
        T.dma(SP, dbg_out["modc"][:, :], modc[:], r=["modc"])
        T.dma(SP, dbg_out["g1b"][:, :], g1b[:], r=["g1b"])
        T.dma(SP, dbg_out["smallc"][:, :], smallc[:], r=["sc0"])

    def dump_dram(dst, src, rows, cols, dt):
        with nc.sbuf_tensor("s_dump_" + dst.name.replace(".", "_"), [128, cols], dt) as tmp:
            for r0 in range(0, rows, 128):
                n = min(128, rows - r0)
                T.dma(SP, tmp[0:n, :], src[r0:r0 + n, :], w=["dumptmp"])
                T.dma(SP, dst[r0:r0 + n, :], tmp[0:n, :], r=["dumptmp"], w=[("dumpdst", r0)])
            T.barrier()

    def finish():
        if _scope[0] is not None:
            _scope[0].__exit__(None, None, None)
            _scope[0] = None
        T.barrier()
        if early_open[0]:
            early.close()
            early_open[0] = False
        if early2_open[0]:
            early2.close()
            early2_open[0] = False
        glob.close()
        return nc

    if stage <= 0:
        return finish()

    s1 = modc[:, 0:8]
    s2 = modc[:, 24:32]

    scope_mark("ph1")
    def norm_tile(ph_bufs, src_ap, rkeys, tag):
        ss, sd, junk = ph_bufs["ss"], ph_bufs["sd"], ph_bufs["junk"]
        T.op(ACT, lambda: nc.scalar.activation(out=junk[:], in_=src_ap, func=AF.Square, accum_out=ss[:]), r=rkeys, w=["junk", "ss"])
        T.op(ACT, lambda: nc.scalar.activation(out=sd[:], in_=ss[:], func=AF.Ln, bias=ph_bufs["epsc"][:], scale=1.0 / D), r=["ss"], w=["sd"])
        T.op(ACT, lambda: nc.scalar.activation(out=ph_bufs["rs"][:], in_=sd[:], func=AF.Exp, scale=-0.5), r=["sd"], w=["rs"])

    with ExitStack() as ph:
        def sb(name, shape, dt):
            return ph.enter_context(nc.sbuf_tensor("s_" + name, list(shape), dt))
        xt = [sb(f"xt{i}", [128, D], F32) for i in range(2)]
        xh = [sb(f"xh{i}", [128, D], BF16) for i in range(2)]
        hTb = [sb(f"hTb{i}", [128, 8, 512], BF16) for i in range(2)]
        bufs = dict(ss=sb("ss", [128, 1], F32), sd=sb("sd", [128, 1], F32), rs=sb("rs", [128, 1], F32),
                    junk=sb("junk", [128, D], BF16), epsc=sb("epsc", [128, 1], F32))
        T.op(DVE, lambda: nc.vector.memset(bufs["epsc"][:], EPS), w=["epsc"])
        pass
        for t in range(int(os.environ.get('K1_TILES', NT))):
            b = t % 2
            blk, tl = t // 4, t % 4
            hb = blk % 2
            T.dma(SP, xt[b][:], x_in[t * 128:(t + 1) * 128, :], w=[("xt", b)])
            KO = int(os.environ.get('K1_OPS', 9))
            if KO < 1:
                continue
            norm_tile(bufs, xt[b][:], [("xt", b), "epsc"], "n1")
            if KO < 2:
                continue
            T.op(DVE, lambda b=b: nc.vector.tensor_scalar(out=xh[b][:], in0=xt[b][:], scalar1=bufs["rs"][:], scalar2=None, op0=ALU.mult),
                 r=[("xt", b), "rs"], w=[("xh", b)])
            if KO < 3:
                continue
            pb = t % 2
            pv = psb(pb).rearrange("p (k t) -> p k t", k=8)
            for kc in range(8):
                T.op(PE, lambda kc=kc, b=b, pv=pv: nc.tensor.transpose(pv[:, kc, :], xh[b][:, kc * 128:(kc + 1) * 128], ident_b),
                     r=[("xh", b), "cstb"], w=[("ps", pb)])
            if KO < 4:
                continue
            for kc in range(8):
                T.op(ACT, lambda kc=kc, pv=pv, hb=hb, tl=tl: nc.scalar.activation(
                    out=hTb[hb][:, kc, tl * 128:(tl + 1) * 128], in_=pv[:, kc, :], func=AF.Identity,
                    **({} if os.environ.get('K1_NOAP') == '1' else ({'scale': a1[:, kc:kc + 1]} if os.environ.get('K1_NOAP') == '2' else
                        ({'bias': s1[:, kc:kc + 1]} if os.environ.get('K1_NOAP') == '3' else dict(scale=a1[:, kc:kc + 1], bias=s1[:, kc:kc + 1]))))),
                    r=[("ps", pb), "a1", "modc"], w=[("hTb", hb, tl)])
            if tl == 3:
                T.dma(SP, xnT_v[:, :, blk * 512:(blk + 1) * 512], hTb[hb][:], r=[("hTb", hb)], w=[("xnT", blk)])
        T.barrier()
    early.close()
    early_open[0] = False
    if dbg and "xnT" in dbg_out:
        dump_dram(dbg_out["xnT"], xnT_d, 1024, S, BF16)
    if stage <= 1:
        return finish()

    scope_mark("phA")
    persist = ExitStack()

    def sbp(name, shape, dt):
        return persist.enter_context(nc.sbuf_tensor("s_" + name, list(shape), dt))

    with ExitStack() as ph:
        def sb(name, shape, dt):
            return ph.enter_context(nc.sbuf_tensor("s_" + name, list(shape), dt))
        wqkv = sb("wqkv", [128, 8, 1536], BF16)
        wgt = sb("wgt", [128, 8, 8], BF16)
        bg = sb("bg", [4, 2], F32)
        nbf = sb("nbf", [4, 1], F32)
        epsc = sb("epscA", [128, 1], F32)
        gst = [[sb(f"gst{g}_{i}", [4, 512], F32) for i in range(2)] for g in range(2)]
        kT = sb("kT", [128, 4, S], BF16)
        v1 = sb("v1", [128, NT, 4, 128], BF16)
        xb = [sb(f"xbA{i}", [128, 8, 512], BF16) for i in range(2)]
        qT = sb("qT", [128, 4, 512], BF16)
        hdTb = sb("hdTb", [128, 4, 512], BF16)
        Pb = [[sb(f"P{m}_{i}", [128, 512], BF16) for i in range(2)] for m in range(2)]
        sqb = [sb(f"sqb{i}", [128, 512], BF16) for i in range(2)]
        qsf = [sb(f"qsf{i}", [128, 512], F32) for i in range(2)]
        sdf = [sb(f"sdf{i}", [128, 512], F32) for i in range(2)]
        qnb = [sb(f"qnb{i}", [128, 512], BF16) for i in range(2)]
        t1f = [sb(f"t1f{i}", [128, 512], F32) for i in range(2)]
        t2f = [sb(f"t2f{i}", [128, 512], F32) for i in range(2)]
        fo = [sb(f"fo{i}", [128, 512], F32) for i in range(4)]
        OL = [qsf[0], qsf[1], sdf[0], sdf[1]]
        OLK = [("qsf", 0), ("qsf", 1), ("sdf", 0), ("sdf", 1)]
        sqf = sqb[0]
        pend_fin = []

        T.op(DVE, lambda: nc.vector.memset(epsc[:], EPS), w=["epscA"])
        T.dma(POOL, wqkv[:, 0:4, :], w_in_v[:, 0:4, 2056:3592], w=[("wqkv", 0)])
        T.dma(POOL, wqkv[:, 4:8, :], w_in_v[:, 4:8, 2056:3592], w=[("wqkv", 1)])
        T.dma(POOL, wgt[:], w_in_v[:, :, 2048:2056], w=["wgt"])
        T.dma(SP, bg[:], bgate_in[:, :], w=["bg"])
        T.op(DVE, lambda: nc.vector.tensor_scalar(out=nbf[:], in0=bg[:, 1:2], scalar1=-1.0, scalar2=None, op0=ALU.mult), r=["bg"], w=["nbf"])

        negc = smallc[:, 3:4]
        neglam = smallc[:, 4:5]
        subw8 = smallc[:, 2:3]

        for blk in range(NB):
            xbb = blk % 2
            bs = slice(blk * 512, (blk + 1) * 512)
            T.dma(SP, xb[xbb][:], xnT_v[:, :, bs], w=[("xbA", xbb)])
            def emit_v(tl):
                t = blk * 4 + tl
                bk = 4 + tl
                for kc in range(8):
                    T.op(PE, lambda kc=kc: nc.tensor.matmul(
                        PS[bk][:, :], lhsT=xb[xbb][:, kc, tl * 128:(tl + 1) * 128], rhs=wqkv[:, kc, 1024:1536], start=(kc == 0), stop=(kc == 7)),
                        r=[("wqkv",), ("xbA", xbb)], w=[("ps", bk)])
                T.op(ACT, lambda: nc.scalar.copy(out=v1[:, t, :, :].rearrange("p h d -> p (h d)"), in_=PS[bk][:, :]),
                     r=[("ps", bk)], w=[("v1", t)])

            def emit_gate(gi):
                bk = 4 + gi
                for kc in range(8):
                    T.op(PE, lambda kc=kc: nc.tensor.matmul(
                        PS[bk][0:4, :], lhsT=wgt[:, kc, 4 * gi:4 * gi + 4], rhs=xb[xbb][:, kc, :], start=(kc == 0), stop=(kc == 7)),
                        r=["wgt", ("xbA", xbb)], w=[("ps", bk)])
                if gi == 0:
                    T.op(ACT, lambda: nc.scalar.activation(out=gst[0][xbb][:], in_=PS[bk][0:4, :], func=AF.Identity, bias=bg[:, 0:1]),
                         r=[("ps", bk), "bg"], w=[("gst", 0, xbb)])
                    T.dma(SP, gi_d[:, bs], gst[0][xbb][:], r=[("gst", 0, xbb)], w=[("gi_d", blk)])
                else:
                    T.op(ACT, lambda: nc.scalar.activation(out=gst[1][xbb][:], in_=PS[bk][0:4, :], func=AF.Exp, bias=nbf[:], scale=-1.0),
                         r=[("ps", bk), "nbf"], w=[("gst", 1, xbb)])
                    T.dma(SP, gf_d[:, bs], gst[1][xbb][:], r=[("gst", 1, xbb)], w=[("gf_d", blk)])

            for c in range(8):
                st = c % 2
                isq = c < 4
                hh = c % 4
                col0 = (0 if isq else 512) + hh * 128
                for kc in range(8):
                    T.op(PE, lambda kc=kc, col0=col0, st=st: nc.tensor.matmul(
                        PS[st][:, :], lhsT=wqkv[:, kc, col0:col0 + 128], rhs=xb[xbb][:, kc, :], start=(kc == 0), stop=(kc == 7)),
                        r=[("wqkv",), ("xbA", xbb)], w=[("ps", st)])
                wcol = smallc[:, 0:1] if isq else smallc[:, 1:2]
                T.op(ACT, lambda st=st: nc.scalar.activation(out=sqb[st][:], in_=PS[st][:, :], func=AF.Square), r=[("ps", st)], w=[("sqb", st)])
                T.op(ACT, lambda st=st, wcol=wcol: nc.scalar.activation(out=qsf[st][:], in_=PS[st][:, :], func=AF.Identity, scale=wcol),
                     r=[("ps", st), "sc0", "sc1"], w=[("qsf", st)])
                if c < 4:
                    emit_v(c)
                elif c < 6:
                    emit_gate(c - 4)
                T.op(PE, lambda st=st: nc.tensor.matmul(PS[2 + st][:, :], lhsT=bo64_b, rhs=sqb[st][:], start=True, stop=True),
                     r=[("sqb", st), "cstb"], w=[("ps", 2 + st)])
                T.op(ACT, lambda st=st: nc.scalar.activation(out=sdf[st][:], in_=PS[2 + st][:, :], func=AF.Ln, bias=epsc[:], scale=1.0 / 64),
                     r=[("ps", 2 + st), "epscA"], w=[("sdf", st)])
                T.op(ACT, lambda st=st: nc.scalar.activation(out=sdf[st][:], in_=sdf[st][:], func=AF.Exp, scale=-0.5),
                     r=[("sdf", st)], w=[("sdf", st)])
                T.op(DVE, lambda st=st: nc.vector.tensor_tensor(out=qnb[st][:], in0=qsf[st][:], in1=sdf[st][:], op=ALU.mult),
                     r=[("qsf", st), ("sdf", st)], w=[("qnb", st)])
                T.op(PE, lambda st=st: nc.tensor.matmul(PS[2 + st][:, :], lhsT=RT_b, rhs=qnb[st][:], start=True, stop=True),
                     r=[("qnb", st), "cstb"], w=[("ps", 2 + st)])
                T.op(POOL, lambda st=st: nc.gpsimd.tensor_tensor(out=t1f[st][:], in0=qnb[st][:], in1=CosB[:, bs], op=ALU.mult),
                     r=[("qnb", st), "CosB"], w=[("t1f", st)])
                T.op(DVE, lambda st=st: nc.vector.tensor_tensor(out=t2f[st][:], in0=PS[2 + st][:, :], in1=SinB[:, bs], op=ALU.mult),
                     r=[("ps", 2 + st), "SinB"], w=[("t2f", st)])
                if isq:
                    dst, dk = qT[:, hh, :], ("qT", hh)
                else:
                    dst, dk = kT[:, hh, bs], ("kT", hh, blk)
                T.op(POOL, lambda st=st, dst=dst: nc.gpsimd.tensor_tensor(out=dst, in0=t1f[st][:], in1=t2f[st][:], op=ALU.add),
                     r=[("t1f", st), ("t2f", st)], w=[dk])
            for hh in range(4):
                nkt = blk * 4 + 4
                prev = None

                def pv_step(kt, c0, pbuf):
                    first = (kt == 0)
                    last = (kt == nkt - 1)
                    for m in range(2):
                        T.op(PE, lambda m=m: nc.tensor.matmul(
                            PS[4 + 2 * m][:, c0:512], lhsT=v1[:, kt, hh, :], rhs=Pb[m][pbuf][:, c0:512], start=first, stop=last,
                            skip_group_check=True), r=[("v1", kt), ("P", m, pbuf)], w=[("ps", 4 + 2 * m)])
                        T.op(PE, lambda m=m: nc.tensor.matmul(
                            PS[5 + 2 * m][:, c0:512], lhsT=ones_b, rhs=Pb[m][pbuf][:, c0:512], start=first, stop=last,
                            skip_group_check=True), r=["cstb", ("P", m, pbuf)], w=[("ps", 5 + 2 * m)])

                for kt in range(nkt):
                    ktl = kt - blk * 4
                    c0 = ktl * 128 if ktl > 0 else 0
                    pbuf = kt % 2
                    sbk = 2 * (kt % 2)
                    for m in range(2):
                        T.op(PE, lambda m=m, kt=kt, c0=c0, sbk=sbk: nc.tensor.matmul(
                            PS[sbk + m][:, c0:512], lhsT=kT[64 * m:64 * m + 64, hh, kt * 128:(kt + 1) * 128],
                            rhs=qT[64 * m:64 * m + 64, hh, c0:512], start=True, stop=True),
                            r=[("kT", hh, kt // 4), ("qT", hh)], w=[("ps", sbk + m)])
                    if prev is not None:
                        pv_step(*prev)
                    for m in range(2):
                        T.op(ACT, lambda m=m, c0=c0, pbuf=pbuf, sbk=sbk: nc.scalar.activation(
                            out=Pb[m][pbuf][:, c0:512], in_=PS[sbk + m][:, c0:512], func=AF.Exp, bias=negc, scale=0.125),
                            r=[("ps", sbk + m), "sc3"], w=[("P", m, pbuf)])
                        if ktl >= 0:
                            T.op(POOL, lambda m=m, c0=c0, pbuf=pbuf: nc.gpsimd.affine_select(
                                out=Pb[m][pbuf][:, c0:c0 + 128], in_=Pb[m][pbuf][:, c0:c0 + 128], pattern=[[1, 128]],
                                compare_op=ALU.is_ge, fill=0.0, base=0, channel_multiplier=-1),
                                r=[("P", m, pbuf)], w=[("P", m, pbuf)])
                    prev = (kt, c0, pbuf)
                    if pend_fin:
                        T.op(*pend_fin.pop(0))
                pv_step(*prev)
                for rec_ in pend_fin:
                    T.op(*rec_)
                pend_fin.clear()

                def finalize_ops(hh):
                    T.op(ACT, lambda: nc.scalar.copy(out=OL[0][:], in_=PS[4][:, :]), r=[("ps", 4)], w=[OLK[0]])
                    T.op(DVE, lambda: nc.vector.tensor_copy(out=OL[1][:], in_=PS[5][:, :]), r=[("ps", 5)], w=[OLK[1]])
                    T.op(ACT, lambda: nc.scalar.copy(out=OL[2][:], in_=PS[6][:, :]), r=[("ps", 6)], w=[OLK[2]])
                    T.op(DVE, lambda: nc.vector.tensor_copy(out=OL[3][:], in_=PS[7][:, :]), r=[("ps", 7)], w=[OLK[3]])
                    T.start_record()
                    T.op(ACT, lambda: nc.scalar.activation(out=fo[0][:], in_=OL[1][:], func=AF.Ln), r=[OLK[1]], w=[("fo", 0)])
                    T.op(ACT, lambda: nc.scalar.activation(out=fo[0][:], in_=fo[0][:], func=AF.Exp, scale=-1.0), r=[("fo", 0)], w=[("fo", 0)])
                    T.op(DVE, lambda: nc.vector.tensor_tensor(out=fo[1][:], in0=OL[0][:], in1=fo[0][:], op=ALU.mult),
                         r=[OLK[0], ("fo", 0)], w=[("fo", 1)])
                    T.op(ACT, lambda: nc.scalar.activation(out=fo[2][:], in_=OL[3][:], func=AF.Ln), r=[OLK[3]], w=[("fo", 2)])
                    T.op(ACT, lambda: nc.scalar.activation(out=fo[0][:], in_=fo[2][:], func=AF.Exp, scale=-1.0), r=[("fo", 2), ("fo", 0)], w=[("fo", 0)])
                    T.op(DVE, lambda: nc.vector.tensor_tensor(out=fo[2][:], in0=OL[2][:], in1=fo[0][:], op=ALU.mult),
                         r=[OLK[2], ("fo", 0)], w=[("fo", 2)])
                    T.op(DVE, lambda: nc.vector.scalar_tensor_tensor(out=fo[3][:], in0=fo[2][:], scalar=neglam, in1=fo[1][:], op0=ALU.mult, op1=ALU.add),
                         r=[("fo", 1), ("fo", 2), "sc4"], w=[("fo", 3)])
                    T.op(ACT, lambda: nc.scalar.activation(out=sqf[:], in_=fo[3][:], func=AF.Square), r=[("fo", 3)], w=[("sqb", 0)])
                    T.op(PE, lambda: nc.tensor.matmul(PS[0][:, :], lhsT=ones_b, rhs=sqf[:], start=True, stop=True),
                         r=[("sqb", 0), "cstb"], w=[("ps", 0)])
                    T.op(ACT, lambda: nc.scalar.activation(out=fo[0][:], in_=PS[0][:, :], func=AF.Ln, bias=epsc[:], scale=1.0 / 128),
                         r=[("ps", 0), "epscA"], w=[("fo", 0)])
                    T.op(ACT, lambda: nc.scalar.activation(out=fo[0][:], in_=fo[0][:], func=AF.Exp, scale=-0.5), r=[("fo", 0)], w=[("fo", 0)])
                    T.op(DVE, lambda: nc.vector.scalar_tensor_tensor(out=hdTb[:, hh, :], in0=fo[3][:], scalar=subw8, in1=fo[0][:], op0=ALU.mult, op1=ALU.mult),
                         r=[("fo", 3), ("fo", 0), "sc2"], w=[("hdTb", hh)])
                    return T.stop_record()

                pend_fin.extend(finalize_ops(hh))
            for rec_ in pend_fin:
                T.op(*rec_)
            pend_fin.clear()
            T.dma(SP, hdT_v[:, :, bs], hdTb[:], r=[("hdTb",)], w=[("hdT", blk)])
        T.barrier()
    early2.close()
    early2_open[0] = False
    if dbg and "hdT" in dbg_out:
        dump_dram(dbg_out["hdT"], hdT_d, 512, S, BF16)
        dump_dram(dbg_out["irow"], gi_d, 4, S, F32)
        dump_dram(dbg_out["frow"], gf_d, 4, S, F32)
    if stage <= 2:
        persist.close()
        return finish()

    scope_mark("phB")
    wcol = sbp("wcol", [128, NT, 8], F32)
    decb = sbp("decb", [128, 128], F32)
    with ExitStack() as ph:
        def sb(name, shape, dt):
            return ph.enter_context(nc.sbuf_tensor("s_" + name, list(shape), dt))
        irow = sb("irow", [4, S], F32)
        frow = sb("frow", [4, S], F32)
        T.dma(SP, irow[:], gi_d[:, :], w=[("irow",)])
        T.dma(SP, frow[:], gf_d[:, :], w=[("frow",)])
        onesr = sb("onesr", [4, S], F32)
        csr = sb("csr", [4, S], F32)
        nbr = sb("nbr", [4, S], F32)
        gr = sb("gr", [4, S], F32)
        pe_ = sb("pe_", [4, 33], F32)
        G = sb("G", [4, 32], F32)
        Ms = sb("Ms", [4, 32], F32)
        mp = sb("mp", [4, 33], F32)
        dec = sb("dec", [4, 32], F32)
        T.op(ACT, lambda: nc.scalar.activation(out=frow[:], in_=frow[:], func=AF.Ln, bias=1.0), r=[("frow",)], w=[("frow",)])
        T.op(DVE, lambda: nc.vector.memset(onesr[:], 1.0), w=["onesr"])
        T.op(DVE, lambda: nc.vector.tensor_tensor_scan(out=csr[:], data0=onesr[:], data1=frow[:], initial=0.0, op0=ALU.mult, op1=ALU.add),
             r=["onesr", ("frow",)], w=["csr"])
        T.op(DVE, lambda: nc.vector.memset(pe_[:, 0:1], 0.0), w=[("pe_", 0)])
        T.op(DVE, lambda: nc.vector.tensor_copy(out=pe_[:, 1:33], in_=csr[:].rearrange("p (j s) -> p j s", s=128)[:, :, 127]),
             r=["csr"], w=[("pe_", 1)])
        T.op(DVE, lambda: nc.vector.tensor_tensor(out=nbr[:].rearrange("p (j s) -> p j s", s=128), in0=csr[:].rearrange("p (j s) -> p j s", s=128),
                                                  in1=pe_[:, 0:32].unsqueeze(2).to_broadcast([4, 32, 128]), op=ALU.subtract),
             r=["csr", ("pe_",)], w=["nbr"])
        T.op(DVE, lambda: nc.vector.tensor_tensor(out=gr[:], in0=irow[:], in1=nbr[:], op=ALU.add), r=[("irow",), "nbr"], w=["gr"])
        T.op(DVE, lambda: nc.vector.tensor_reduce(out=G[:], in_=gr[:].rearrange("p (j s) -> p j s", s=128), axis=AX.X, op=ALU.max),
             r=["gr"], w=["G"])
        T.op(DVE, lambda: nc.vector.memset(mp[:, 0:1], 0.0), w=[("mp", 0)])
        nbl = nbr[:].rearrange("p (j s) -> p j s", s=128)[:, :, 127]
        for j in range(NT):
            T.op(DVE, lambda j=j: nc.vector.tensor_tensor(out=Ms[:, j:j + 1], in0=mp[:, j:j + 1], in1=G[:, j:j + 1], op=ALU.max),
                 r=[("mp", j), "G"], w=[("Ms", j)])
            T.op(DVE, lambda j=j: nc.vector.tensor_tensor(out=mp[:, j + 1:j + 2], in0=Ms[:, j:j + 1], in1=nbl[:, j:j + 1], op=ALU.subtract),
                 r=[("Ms", j), "nbr"], w=[("mp", j + 1)])
        Msb = Ms[:].unsqueeze(2).to_broadcast([4, 32, 128])
        T.op(DVE, lambda: nc.vector.tensor_tensor(out=gr[:].rearrange("p (j s) -> p j s", s=128), in0=gr[:].rearrange("p (j s) -> p j s", s=128),
                                                  in1=Msb, op=ALU.subtract), r=["gr", ("Ms",)], w=["gr"])
        T.op(DVE, lambda: nc.vector.tensor_tensor(out=nbr[:].rearrange("p (j s) -> p j s", s=128), in0=nbr[:].rearrange("p (j s) -> p j s", s=128),
                                                  in1=Msb, op=ALU.subtract), r=["nbr", ("Ms",)], w=["nbr"])
        T.op(ACT, lambda: nc.scalar.activation(out=gr[:], in_=gr[:], func=AF.Exp), r=["gr"], w=["gr"])
        T.op(ACT, lambda: nc.scalar.activation(out=nbr[:], in_=nbr[:], func=AF.Exp), r=["nbr"], w=["nbr"])
        T.op(DVE, lambda: nc.vector.tensor_scalar(out=gr[:], in0=gr[:], scalar1=128.0 ** -0.5, scalar2=None, op0=ALU.mult), r=["gr"], w=["gr"])
        T.op(DVE, lambda: nc.vector.tensor_tensor(out=dec[:], in0=mp[:, 0:32], in1=Ms[:], op=ALU.subtract), r=[("mp",), ("Ms",)], w=["dec"])
        T.op(ACT, lambda: nc.scalar.activation(out=dec[:], in_=dec[:], func=AF.Exp), r=["dec"], w=["dec"])
        T.dma(SP, dec_d[:, :], dec[:], r=["dec"], w=["dec_d"])
        T.dma(SP, decb[:], dec_d.rearrange("h j -> (h j)").unsqueeze(0).partition_broadcast(128), r=["dec_d"], w=["decb"])
        wv = PS[0][:, 0:256].rearrange("p (t e) -> p t e", e=8)
        for t in range(NT):
            T.op(PE, lambda t=t: nc.tensor.transpose(wv[:, t, 0:4], gr[:, t * 128:(t + 1) * 128], ident_f[0:4, 0:4]),
                 r=["gr", "cst"], w=[("ps", 0, t, 0)])
            T.op(PE, lambda t=t: nc.tensor.transpose(wv[:, t, 4:8], nbr[:, t * 128:(t + 1) * 128], ident_f[0:4, 0:4]),
                 r=["nbr", "cst"], w=[("ps", 0, t, 1)])
        T.op(DVE, lambda: nc.vector.tensor_copy(out=wcol[:], in_=wv), r=[("ps", 0)], w=["wcol"])
        T.barrier()
    if dbg and "wcol" in dbg_out:
        T.dma(SP, dbg_out["wcol"][:, :], wcol[:].rearrange("p t e -> p (t e)"), r=["wcol"])
        T.dma(SP, dbg_out["decb"][:, :], decb[:], r=["decb"])
    if stage <= 3:
        persist.close()
        return finish()

    scope_mark("phC")
    with ExitStack() as ph:
        def sb(name, shape, dt):
            return ph.enter_context(nc.sbuf_tensor("s_" + name, list(shape), dt))
        wm = sb("wm", [128, 8, 2048], BF16)
        cw = sb("cw", [128, 8, 4], F32)
        cbias = sb("cbias", [128, 8], F32)
        dg = sb("dg", [128, 8, 4, 128], BF16)
        mnwb = sb("mnwb", [128, 512], F32)
        epsc = sb("epscC", [128, 1], F32)
        xb = [sb(f"xbC{i}", [128, 8, 512], BF16) for i in range(2)]
        pre = sb("pre", [128, 8, 516], BF16)
        qkc = sb("qkc", [128, 8, 512], BF16)
        vm1 = [sb(f"vm1_{i}", [128, 4, 130], BF16) for i in range(2)]
        so = [sb(f"so{i}", [128, 512], F32) for i in range(2)]
        ST = [sb(f"ST{i}", [128, 128], BF16) for i in range(4)]
        kw = [sb(f"kw{i}", [128, 128], BF16) for i in range(4)]
        Cst = sb("Cst", [128, 4, 130], F32)
        Cd = sb("Cd", [128, 4, 130], BF16)
        hbuf = sb("hbuf", [128, 512], F32)
        sqh = sb("sqh", [128, 512], F32)
        hmb = sb("hmb", [128, 512], BF16)
        hmTb = [sb(f"hmTb{i}", [128, 4, 512], BF16) for i in range(2)]
        dn = sb("dn", [128, 8], F32)
        ssh = sb("ssh", [128, 8], F32)

        T.op(DVE, lambda: nc.vector.memset(epsc[:], EPS), w=["epscC"])
        T.dma(POOL, wm[:, 0:4, :], w_in_v[:, 0:4, 0:2048], w=[("wm", 0)], max_dma_last_dim=8192)
        T.dma(POOL, wm[:, 4:8, :], w_in_v[:, 4:8, 0:2048], w=[("wm", 1)], max_dma_last_dim=8192)
        T.dma(SP, cw[:], convw_in[:, :, :], w=["cw"])
        T.dma(SP, cbias[:], convb_in[:, :], w=["cbias"])
        T.dma(SP, mnwb[:], mnw_in[0:1, :].partition_broadcast(128), w=["mnwb"])
        for c in range(8):
            for j in range(4):
                T.op(DVE, lambda c=c, j=j: nc.vector.tensor_scalar(out=dg[:, c, j, :], in0=ident_f, scalar1=cw[:, c, j:j + 1], scalar2=None, op0=ALU.mult),
                     r=["cw", "cst"], w=[("dg", c, j)])
        T.op(DVE, lambda: nc.vector.memset(pre[:, :, 0:4], 0.0), w=[("pre", "halo")])
        T.op(DVE, lambda: nc.vector.memset(Cst[:], 0.0), w=["Cst"])
        T.op(DVE, lambda: nc.vector.memset(Cd[:], 0.0), w=["Cd"])
        for i in range(2):
            T.op(DVE, lambda i=i: nc.vector.memset(vm1[i][:, :, 128:130], 1.0), w=[("vm1", i)])

        for blk in range(NB):
            xbb = blk % 2
            bs = slice(blk * 512, (blk + 1) * 512)
            T.dma(SP, xb[xbb][:], xnT_v[:, :, bs], w=[("xbC", xbb)])
            for c in range(8):
                pb = c % 2
                for kc in range(8):
                    T.op(PE, lambda kc=kc, c=c, pb=pb: nc.tensor.matmul(
                        PS[pb][:, :], lhsT=wm[:, kc, c * 128:(c + 1) * 128], rhs=xb[xbb][:, kc, :], start=(kc == 0), stop=(kc == 7)),
                        r=[("wm",), ("xbC", xbb)], w=[("ps", pb)])
                if blk > 0:
                    T.op(DVE, lambda c=c: nc.vector.tensor_copy(out=pre[:, c, 1:4], in_=pre[:, c, 513:516]),
                         r=[("pre", c)], w=[("pre", "halo", c)])
                T.op(ACT, lambda c=c, pb=pb: nc.scalar.copy(out=pre[:, c, 4:516], in_=PS[pb][:, :]),
                     r=[("ps", pb), ("pre", "halo", c)], w=[("pre", c)])
                cb2 = 2 + c % 2
                for j in range(4):
                    T.op(PE, lambda c=c, j=j, cb2=cb2: nc.tensor.matmul(
                        PS[cb2][:, :], lhsT=dg[:, c, j, :], rhs=pre[:, c, 1 + j:513 + j], start=(j == 0), stop=(j == 3)),
                        r=[("dg", c), ("pre", c), ("pre", "halo", c)], w=[("ps", cb2)])
                T.op(ACT, lambda c=c, cb2=cb2: nc.scalar.activation(out=qkc[:, c, :], in_=PS[cb2][:, :], func=AF.Silu, bias=cbias[:, c:c + 1]),
                     r=[("ps", cb2), "cbias"], w=[("qkc", c)])
            for tl in range(4):
                t = blk * 4 + tl
                vb = t % 2
                ts_ = slice(tl * 128, (tl + 1) * 128)
                def emit_vo(tl_):
                    t_ = blk * 4 + tl_
                    vb_ = t_ % 2
                    sl_ = slice(tl_ * 128, (tl_ + 1) * 128)
                    for half in range(2):
                        for kc in range(8):
                            T.op(PE, lambda kc=kc, half=half: nc.tensor.matmul(
                                PS[4 + half][:, :], lhsT=xb[xbb][:, kc, sl_], rhs=wm[:, kc, 1024 + half * 512:1536 + half * 512],
                                start=(kc == 0), stop=(kc == 7)), r=[("wm",), ("xbC", xbb)], w=[("ps", 4 + half)])
                    T.op(ACT, lambda: nc.scalar.copy(out=vm1[vb_][:, :, 0:128], in_=PS[4][:, :].rearrange("p (h d) -> p h d", h=4)),
                         r=[("ps", 4)], w=[("vm1", vb_)])
                    T.op(ACT, lambda: nc.scalar.activation(out=so[vb_][:], in_=PS[5][:, :], func=AF.Sigmoid), r=[("ps", 5)], w=[("so", vb_)])

                if tl == 0:
                    emit_vo(0)
                if tl < 3:
                    emit_vo(tl + 1)
                def head_ops(hh):
                    wc = wcol[:, t, hh:hh + 1]
                    cc = wcol[:, t, 4 + hh:5 + hh]
                    dcol = decb[:, hh * 32 + t:hh * 32 + t + 1]
                    if hh % 2 == 0:
                        bA, bK, bU, bN = 6, 1, 7, 0
                    else:
                        bA, bK, bU, bN = 2, 3, 4, 5
                    kps = psb(bK)[:, 0:128]
                    ops = []
                    ops.append(lambda: T.op(PE, lambda: nc.tensor.matmul(PS[bA][:, 0:128], lhsT=qkc[:, 4 + hh, ts_], rhs=qkc[:, hh, ts_], start=True, stop=True),
                                            r=[("qkc", 4 + hh), ("qkc", hh)], w=[("ps", bA)]))
                    ops.append(lambda: T.op(DVE, lambda: nc.vector.scalar_tensor_tensor(
                        out=ST[hh][:], in0=PS[bA][:, 0:128], scalar=wc, in1=tri_f, op0=ALU.mult, op1=ALU.mult),
                        r=[("ps", bA), "wcol", "cst"], w=[("ST", hh)]))
                    ops.append(lambda: T.op(PE, lambda: nc.tensor.transpose(kps, qkc[:, 4 + hh, ts_], ident_b),
                                            r=[("qkc", 4 + hh), "cstb"], w=[("ps", bK)]))
                    ops.append(lambda: T.op(DVE, lambda: nc.vector.tensor_scalar(out=kw[hh][:], in0=kps, scalar1=wc, scalar2=None, op0=ALU.mult),
                                            r=[("ps", bK), "wcol"], w=[("kw", hh)]))
                    ops.append(lambda: T.op(ACT, lambda: nc.scalar.activation(out=Cd[:, hh, :], in_=Cst[:, hh, :], func=AF.Identity, scale=dcol),
                                            r=[("Cst", hh), "decb"], w=[("Cd", hh)]))
                    ops.append(lambda: T.op(PE, lambda: nc.tensor.matmul(PS[bU][:, 0:129], lhsT=kw[hh][:], rhs=vm1[vb][:, hh, 0:129], start=True, stop=True),
                                            r=[("kw", hh), ("vm1", vb)], w=[("ps", bU)]))
                    ops.append(lambda: T.op(PE, lambda: nc.tensor.matmul(PS[bN][:, 0:129], lhsT=ST[hh][:], rhs=vm1[vb][:, hh, 0:129], start=True, stop=False),
                                            r=[("ST", hh), ("vm1", vb)], w=[("ps", bN)]))
                    ops.append(lambda: T.op(PE, lambda: nc.tensor.matmul(PS[bN][:, 0:129], lhsT=qkc[:, hh, ts_], rhs=Cd[:, hh, 0:129], start=False, stop=True),
                                            r=[("qkc", hh), ("Cd", hh)], w=[("ps", bN)]))
                    ops.append(lambda: T.op(DVE, lambda: nc.vector.scalar_tensor_tensor(
                        out=Cst[:, hh, 0:129], in0=Cst[:, hh, 0:129], scalar=dcol, in1=PS[bU][:, 0:129], op0=ALU.mult, op1=ALU.add),
                        r=[("Cst", hh), "decb", ("ps", bU)], w=[("Cst", hh)]))
                    ops.append(lambda: T.op(DVE, lambda: nc.vector.tensor_reduce(
                        out=dn[:, hh:hh + 1], in_=PS[bN][:, 128:129], axis=AX.X, op=ALU.max, apply_absolute_value=True),
                        r=[("ps", bN)], w=[("dn", hh)]))
                    ops.append(lambda: T.op(DVE, lambda: nc.vector.tensor_scalar(
                        out=dn[:, hh:hh + 1], in0=dn[:, hh:hh + 1], scalar1=cc, scalar2=None, op0=ALU.max),
                        r=[("dn", hh), "wcol"], w=[("dn", hh)]))
                    ops.append(lambda: T.op(DVE, lambda: nc.vector.reciprocal(out=dn[:, 4 + hh:5 + hh], in_=dn[:, hh:hh + 1]), r=[("dn", hh)], w=[("dn", 4 + hh)]))
                    ops.append(lambda: T.op(ACT, lambda: nc.scalar.activation(out=hbuf[:, hh * 128:(hh + 1) * 128], in_=PS[bN][:, 0:128], func=AF.Copy,
                                                                              scale=dn[:, 4 + hh:5 + hh]),
                                            r=[("ps", bN), ("dn", 4 + hh)], w=[("hbuf", hh)]))
                    return ops

                for pair in ((0, 1), (2, 3)):
                    pops = [head_ops(hh) for hh in pair]
                    for i in range(len(pops[0])):
                        for po in pops:
                            po[i]()
                T.op(DVE, lambda: nc.vector.tensor_tensor(out=sqh[:], in0=hbuf[:], in1=hbuf[:], op=ALU.mult), r=[("hbuf",)], w=["sqh"])
                T.op(DVE, lambda: nc.vector.tensor_reduce(out=ssh[:, 0:4], in_=sqh[:].rearrange("p (h d) -> p h d", h=4), axis=AX.X, op=ALU.add),
                     r=["sqh"], w=[("ssh", 0)])
                T.op(ACT, lambda: nc.scalar.activation(out=ssh[:, 4:8], in_=ssh[:, 0:4], func=AF.Sqrt, bias=epsc[:], scale=1.0 / 128),
                     r=[("ssh", 0), "epscC"], w=[("ssh", 1)])
                T.op(DVE, lambda: nc.vector.reciprocal(out=ssh[:, 4:8], in_=ssh[:, 4:8]), r=[("ssh", 1)], w=[("ssh", 1)])
                T.op(DVE, lambda: nc.vector.tensor_tensor(out=sqh[:].rearrange("p (h d) -> p h d", h=4), in0=hbuf[:].rearrange("p (h d) -> p h d", h=4),
                                                          in1=ssh[:, 4:8].unsqueeze(2).to_broadcast([128, 4, 128]), op=ALU.mult),
                     r=[("hbuf",), ("ssh", 1)], w=["sqh"])
                T.op(DVE, lambda: nc.vector.tensor_tensor(out=sqh[:], in0=sqh[:], in1=mnwb[:], op=ALU.mult), r=["sqh", "mnwb"], w=["sqh"])
                T.op(DVE, lambda vb=vb: nc.vector.tensor_tensor(out=hmb[:], in0=sqh[:], in1=so[vb][:], op=ALU.mult), r=["sqh", ("so", vb)], w=["hmb"])
                hb = blk % 2
                tp = psb(3).rearrange("p (k t) -> p k t", k=8)
                for hh in range(4):
                    T.op(PE, lambda hh=hh: nc.tensor.transpose(tp[:, hh, :], hmb[:, hh * 128:(hh + 1) * 128], ident_b),
                         r=["hmb", "cstb"], w=[("ps", 3)])
                T.op(ACT, lambda hb=hb: nc.scalar.copy(out=hmTb[hb][:, :, ts_], in_=tp[:, 0:4, :]), r=[("ps", 3)], w=[("hmTb", hb, tl)])
            T.dma(SP, hmT_v[:, :, bs], hmTb[blk % 2][:], r=[("hmTb", blk % 2)], w=[("hmT", blk)])
        T.barrier()
    persist.close()
    if dbg and "hmT" in dbg_out:
        dump_dram(dbg_out["hmT"], hmT_d, 512, S, BF16)
    if stage <= 4:
        return finish()

    scope_mark("phD")
    with ExitStack() as ph:
        def sb(name, shape, dt):
            return ph.enter_context(nc.sbuf_tensor("s_" + name, list(shape), dt))
        wg = sb("wg", [128, 8, 2048], BF16)
        wbm = sb("wbm", [128, 4, D], BF16)
        wbd = sb("wbd", [128, 4, D], BF16)
        wo = sb("wo", [128, 8, D], BF16)
        wstg = [sb(f"wstg{i}", [128, D], F32) for i in range(2)]
        wr = sb("wrt", [128, 8, 36], F32)
        brb = sb("brb", [128, 36], F32)
        xb = [sb(f"xbD{i}", [128, 8, 512], BF16) for i in range(2)]
        hmb_ = [sb(f"hmD{i}", [128, 4, 512], BF16) for i in range(2)]
        hdb_ = [sb(f"hdD{i}", [128, 4, 512], BF16) for i in range(2)]
        sg = [sb(f"sg{i}", [128, 512], F32) for i in range(4)]
        tt = [sb(f"tt{i}", [128, 512], F32) for i in range(4)]
        mT = sb("mT", [128, 8, 512], BF16)
        xt = [sb(f"xtD{i}", [128, D], F32) for i in range(2)]
        x1t = [sb(f"x1t{i}", [128, D], F32) for i in range(2)]
        xhf = sb("xhf", [128, D], F32)
        h2f = sb("h2f", [128, 8, 128], F32)
        x2t = [sb(f"x2t{i}", [128, D], BF16) for i in range(2)]
        ohb = sb("ohb", [128, 32], BF16)
        bufs = dict(ss=sb("ssD", [128, 1], F32), sd=sb("sdD", [128, 1], F32), rs=sb("rsD", [128, 1], F32),
                    junk=sb("junkD", [128, D], BF16), epsc=sb("epscD", [128, 1], F32))
        lg = sb("lg", [128, 36], F32)
        r8 = sb("r8", [128, 80], F32)

        T.op(DVE, lambda: nc.vector.memset(bufs["epsc"][:], EPS), w=["epsc"])
        T.op(DVE, lambda: nc.vector.memset(tot[:], 0.0), w=["tot"])
        T.dma(POOL, wg[:, 0:4, :], w_in_v[:, 0:4, 3592:5640], w=[("wg", 0)], max_dma_last_dim=8192)
        T.dma(POOL, wg[:, 4:8, :], w_in_v[:, 4:8, 3592:5640], w=[("wg", 1)], max_dma_last_dim=8192)
        T.dma(POOL, wbm[:], w_br_m.rearrange("(k p) n -> p k n", p=128), w=["wbm"])
        T.dma(POOL, wbd[:], w_br_d.rearrange("(k p) n -> p k n", p=128), w=["wbd"])
        T.dma(SP, wr[:], wr_in.rearrange("(k p) n -> p k n", p=128), w=["wrt"])
        T.dma(SP, brb[:], br_in[0:1, :].partition_broadcast(128), w=["brb"])
        w_out_v = w_out.rearrange("(k p) n -> p k n", p=128)
        for kc in range(8):
            b = kc % 2
            T.dma(SP, wstg[b][:], w_out_v[:, kc, :], w=[("wstg", b)])
            T.op(DVE, lambda kc=kc, b=b: nc.vector.tensor_tensor(out=wo[:, kc, :], in0=wstg[b][:], in1=g1b[:], op=ALU.mult),
                 r=[("wstg", b), "g1b"], w=[("wo", kc)])

        for blk in range(NB):
            xbb = blk % 2
            bs = slice(blk * 512, (blk + 1) * 512)
            def load_blk(bk):
                bb_ = bk % 2
                sl_ = slice(bk * 512, (bk + 1) * 512)
                T.dma(SP, xb[bb_][:], xnT_v[:, :, sl_], w=[("xbD", bb_)])
                T.dma(SP, hmb_[bb_][:], hmT_v[:, :, sl_], w=[("hmD", bb_)])
                T.dma(SP, hdb_[bb_][:], hdT_v[:, :, sl_], w=[("hdD", bb_)])

            if blk == 0:
                load_blk(0)
                T.dma(SP, xt[0][:], x_in[0:128, :], w=[("xtD", 0)])
            for oc in range(8):
                st = oc % 2
                for which, (colbase, wbr, hsrc, hkey, wkey) in enumerate(((0, wbm, hmb_, "hmD", "wbm"), (1024, wbd, hdb_, "hdD", "wbd"))):
                    gb = 0 + which
                    pbk = 2 + which
                    for kc in range(8):
                        T.op(PE, lambda kc=kc, colbase=colbase, gb=gb: nc.tensor.matmul(
                            PS[gb][:, :], lhsT=wg[:, kc, colbase + oc * 128:colbase + (oc + 1) * 128], rhs=xb[xbb][:, kc, :],
                            start=(kc == 0), stop=(kc == 7)), r=[("wg",), ("xbD", xbb)], w=[("ps", gb)])
                    T.op(ACT, lambda gb=gb, which=which: nc.scalar.activation(out=sg[2 * st + which][:], in_=PS[gb][:, :], func=AF.Sigmoid),
                         r=[("ps", gb)], w=[("sg", 2 * st + which)])
                    for kc in range(4):
                        T.op(PE, lambda kc=kc, wbr=wbr, hsrc=hsrc, pbk=pbk: nc.tensor.matmul(
                            PS[pbk][:, :], lhsT=wbr[:, kc, oc * 128:(oc + 1) * 128], rhs=hsrc[xbb][:, kc, :],
                            start=(kc == 0), stop=(kc == 3)), r=[wkey, (hkey, xbb)], w=[("ps", pbk)])
                    T.op(DVE, lambda which=which, pbk=pbk: nc.vector.tensor_tensor(out=tt[2 * st + which][:], in0=PS[pbk][:, :], in1=sg[2 * st + which][:], op=ALU.mult),
                         r=[("ps", pbk), ("sg", 2 * st + which)], w=[("tt", 2 * st + which)])
                T.op(POOL, lambda st=st: nc.gpsimd.tensor_tensor(out=mT[:, oc, :], in0=tt[2 * st][:], in1=tt[2 * st + 1][:], op=ALU.add),
                     r=[("tt", 2 * st), ("tt", 2 * st + 1)], w=[("mT", oc)])
            for tl in range(4):
                t = blk * 4 + tl
                b = t % 2
                ts_ = slice(tl * 128, (tl + 1) * 128)
                if t + 1 < NT:
                    T.dma(SP, xt[(t + 1) % 2][:], x_in[(t + 1) * 128:(t + 2) * 128, :], w=[("xtD", (t + 1) % 2)])
                if tl == 3 and blk + 1 < NB:
                    load_blk(blk + 1)
                def emit_wout(tl_):
                    sl_ = slice(tl_ * 128, (tl_ + 1) * 128)
                    for half in range(2):
                        for oc in range(8):
                            T.op(PE, lambda oc=oc, half=half: nc.tensor.matmul(
                                PS[4 + half][:, :], lhsT=mT[:, oc, sl_], rhs=wo[:, oc, half * 512:(half + 1) * 512],
                                start=(oc == 0), stop=(oc == 7)), r=[("mT", oc), ("wo",)], w=[("ps", 4 + half)])

                if tl == 0:
                    emit_wout(0)
                for half in range(2):
                    T.op(DVE, lambda half=half, b=b: nc.vector.tensor_tensor(
                        out=x1t[b][:, half * 512:(half + 1) * 512], in0=PS[4 + half][:, :], in1=xt[b][:, half * 512:(half + 1) * 512], op=ALU.add),
                        r=[("ps", 4 + half), ("xtD", b)], w=[("x1t", b, half)])
                T.dma(SP, x1_d[t * 128:(t + 1) * 128, :], x1t[b][:], r=[("x1t", b)], w=[("x1d", t)])
                if tl < 3:
                    emit_wout(tl + 1)
                norm_tile(bufs, x1t[b][:], [("x1t", b), "epsc"], "n2")
                T.op(DVE, lambda b=b: nc.vector.tensor_scalar(out=xhf[:], in0=x1t[b][:], scalar1=bufs["rs"][:], scalar2=None, op0=ALU.mult),
                     r=[("x1t", b), "rs"], w=["xhf"])
                for kc in range(8):
                    T.op(PE, lambda kc=kc: nc.tensor.transpose(PS[6 + kc // 4][:, (kc % 4) * 128:(kc % 4 + 1) * 128], xhf[:, kc * 128:(kc + 1) * 128], ident_f),
                         r=["xhf", "cst"], w=[("ps", 6 + kc // 4, kc % 4)])
                for kc in range(8):
                    T.op(ACT, lambda kc=kc: nc.scalar.activation(
                        out=h2f[:, kc, :], in_=PS[6 + kc // 4][:, (kc % 4) * 128:(kc % 4 + 1) * 128], func=AF.Identity,
                        scale=a2[:, kc:kc + 1], bias=s2[:, kc:kc + 1]), r=[("ps", 6 + kc // 4, kc % 4), "a2", "modc"], w=[("h2f", kc)])
                T.op(DVE, lambda: nc.vector.tensor_tensor(out=xhf[:], in0=xhf[:], in1=a2b[:], op=ALU.mult), r=["xhf", ("a2b",)], w=["xhf"])
                T.op(POOL, lambda b=b: nc.gpsimd.tensor_tensor(out=x2t[b][:], in0=xhf[:], in1=s2b[:], op=ALU.add), r=["xhf", ("s2b",)], w=[("x2t", b)])
                T.dma(SP, X2_d[t * 128:(t + 1) * 128, :], x2t[b][:], r=[("x2t", b)], w=[("X2d", t)])
                for kc in range(8):
                    T.op(PE, lambda kc=kc: nc.tensor.matmul(PS[0][:, 0:36], lhsT=h2f[:, kc, :], rhs=wr[:, kc, :], start=(kc == 0), stop=(kc == 7)),
                         r=[("h2f", kc), "wrt"], w=[("ps", 0)])
                T.op(DVE, lambda: nc.vector.tensor_tensor(out=lg[:], in0=PS[0][:, 0:36], in1=brb[:], op=ALU.add), r=[("ps", 0), "brb"], w=["lg"])
                gl = lg[:, 0:4]
                el = lg[:, 4:36].rearrange("p (g j) -> p g j", g=4)
                R = lambda a_, b_: r8[:, a_:b_]
                T.op(DVE, lambda: nc.vector.tensor_reduce(out=R(0, 1), in_=gl, axis=AX.X, op=ALU.max), r=["lg"], w=[("r8", 0)])
                T.op(DVE, lambda: nc.vector.tensor_scalar(out=R(4, 8), in0=gl, scalar1=R(0, 1), scalar2=None, op0=ALU.is_ge), r=["lg", ("r8", 0)], w=[("r8", 1)])
                T.op(DVE, lambda: nc.vector.tensor_scalar(out=R(1, 2), in0=R(0, 1), scalar1=-1.0, scalar2=None, op0=ALU.mult), r=[("r8", 0)], w=[("r8", 2)])
                T.op(ACT, lambda: nc.scalar.activation(out=R(8, 12), in_=gl, func=AF.Exp, bias=R(1, 2), accum_out=R(2, 3)), r=["lg", ("r8", 2)], w=[("r8", 3)])
                T.op(DVE, lambda: nc.vector.reciprocal(out=R(3, 4), in_=R(2, 3)), r=[("r8", 3)], w=[("r8", 4)])
                T.op(DVE, lambda: nc.vector.tensor_tensor(out=R(16, 48).rearrange("p (g j) -> p g j", g=4), in0=el,
                                                          in1=R(4, 8).unsqueeze(2).to_broadcast([128, 4, 8]), op=ALU.mult),
                     r=["lg", ("r8", 1)], w=[("r8", 5)])
                T.op(DVE, lambda: nc.vector.tensor_reduce(out=R(48, 56), in_=R(16, 48).rearrange("p (g j) -> p j g", g=4), axis=AX.X, op=ALU.add),
                     r=[("r8", 5)], w=[("r8", 6)])
                T.op(DVE, lambda: nc.vector.max(out=R(56, 64), in_=R(48, 56)), r=[("r8", 6)], w=[("r8", 7)])
                T.op(DVE, lambda: nc.vector.tensor_scalar(out=R(64, 72), in0=R(48, 56), scalar1=R(56, 57), scalar2=None, op0=ALU.is_equal),
                     r=[("r8", 6), ("r8", 7)], w=[("r8", 9)])
                T.op(DVE, lambda: nc.vector.tensor_scalar(out=R(72, 80), in0=R(48, 56), scalar1=R(57, 58), scalar2=None, op0=ALU.is_equal),
                     r=[("r8", 6), ("r8", 7)], w=[("r8", 10)])
                T.op(DVE, lambda: nc.vector.tensor_tensor(out=R(1, 2), in0=R(57, 58), in1=R(56, 57), op=ALU.subtract), r=[("r8", 7), ("r8", 3)], w=[("r8", 2)])
                T.op(ACT, lambda: nc.scalar.activation(out=R(2, 3), in_=R(1, 2), func=AF.Exp), r=[("r8", 2), ("r8", 4)], w=[("r8", 3)])
                T.op(DVE, lambda: nc.vector.tensor_scalar(out=R(1, 2), in0=R(2, 3), scalar1=1.0, scalar2=None, op0=ALU.add), r=[("r8", 3)], w=[("r8", 2)])
                T.op(DVE, lambda: nc.vector.reciprocal(out=R(1, 2), in_=R(1, 2)), r=[("r8", 2)], w=[("r8", 2)])
                T.op(DVE, lambda t=t: nc.vector.tensor_tensor(out=w12[:, t, 0:1], in0=R(1, 2), in1=R(3, 4), op=ALU.mult), r=[("r8", 2), ("r8", 4)], w=[("w12", t, 0)])
                T.op(DVE, lambda t=t: nc.vector.tensor_tensor(out=w12[:, t, 1:2], in0=w12[:, t, 0:1], in1=R(2, 3), op=ALU.mult), r=[("w12", t, 0), ("r8", 3)], w=[("w12", t, 1)])
                for g in range(4):
                    T.op(DVE, lambda g=g, t=t: nc.vector.tensor_scalar(out=m1_all[:, t, g * 8:(g + 1) * 8], in0=R(64, 72), scalar1=R(4 + g, 5 + g), scalar2=None, op0=ALU.mult),
                         r=[("r8", 9), ("r8", 1)], w=[("m1", t, g)])
                    T.op(DVE, lambda g=g, t=t: nc.vector.tensor_scalar(out=m2_all[:, t, g * 8:(g + 1) * 8], in0=R(72, 80), scalar1=R(4 + g, 5 + g), scalar2=None, op0=ALU.mult),
                         r=[("r8", 10), ("r8", 1)], w=[("m2", t, g)])
                T.op(DVE, lambda t=t: nc.vector.tensor_tensor(out=ohb[:], in0=m1_all[:, t, :], in1=m2_all[:, t, :], op=ALU.add),
                     r=[("m1", t), ("m2", t)], w=["ohb"])
                T.op(PE, lambda: nc.tensor.matmul(PS[1][:, 0:32], lhsT=tri_b, rhs=ohb[:], start=True, stop=True), r=["ohb", "cstb"], w=[("ps", 1)])
                T.op(DVE, lambda t=t: nc.vector.tensor_tensor(out=rank_all[:, t, :], in0=PS[1][:, 0:32], in1=tot[:], op=ALU.add),
                     r=[("ps", 1), "tot"], w=[("rank", t)])
                T.op(DVE, lambda t=t: nc.vector.tensor_tensor(out=rank_all[:, t, :], in0=rank_all[:, t, :], in1=ohb[:], op=ALU.subtract),
                     r=[("rank", t), "ohb"], w=[("rank", t)])
                T.op(PE, lambda: nc.tensor.matmul(PS[1][:, 0:32], lhsT=ones_b, rhs=ohb[:], start=True, stop=True), r=["ohb", "cstb"], w=[("ps", 1)])
                T.op(DVE, lambda: nc.vector.tensor_tensor(out=tot[:], in0=PS[1][:, 0:32], in1=tot[:], op=ALU.add), r=[("ps", 1), "tot"], w=["tot"])
        T.barrier()
    if dbg and "x1" in dbg_out:
        dump_dram(dbg_out["x1"], x1_d, S, D, F32)
        T.dma(SP, dbg_out["tot"][:, :], tot[:], r=["tot"])
        T.dma(SP, dbg_out["rank"][:, :], rank_all[:].rearrange("p t e -> p (t e)"), r=[("rank",)])
        T.dma(SP, dbg_out["m1"][:, :], m1_all[:].rearrange("p t e -> p (t e)"), r=[("m1",)])
        T.dma(SP, dbg_out["m2"][:, :], m2_all[:].rearrange("p t e -> p (t e)"), r=[("m2",)])
        T.dma(SP, dbg_out["w12"][:, :], w12[:].rearrange("p t e -> p (t e)"), r=[("w12",)])
    if stage <= 5:
        return finish()

    scope_mark("phE")
    with ExitStack() as ph:
        def sb(name, shape, dt):
            return ph.enter_context(nc.sbuf_tensor(name, list(shape), dt))
        P12 = sb("P12", [128, 2, NT], I32)
        NSPL = int(os.environ.get("KSPLIT", 1))
        widx = sb("widx", [128, NSPL, NSLOT], I32)
        with ExitStack() as ph2:
            def sb2(name, shape, dt):
                return ph2.enter_context(nc.sbuf_tensor("s_" + name, list(shape), dt))
            ni = sb2("ni", [128, 32], I32)
            ntf = sb2("ntf", [128, 32], F32)
            onesr = sb2("ones32", [128, 32], F32)
            tend = sb2("tend", [128, 32], F32)
            off = sb2("off", [128, 32], F32)
            big = sb2("big", [128, NT, 32], F32)
            pf = sb2("pf", [128, 2, NT], F32)
            cmp3 = sb2("cmp3", [128, NSLOT, 32], F32)
            sidx = sb2("sidx", [128, NSLOT], F32)
            esl = sb2("esl", [128, NSLOT], F32)
            pio = sb2("pio", [128, 1], F32)
            pioi = sb2("pioi", [128, 1], I32)
            T.op(DVE, lambda: nc.vector.tensor_scalar(out=ntf[:], in0=tot[:], scalar1=127.0, scalar2=None, op0=ALU.add), r=["tot"], w=["ntf"])
            T.op(DVE, lambda: nc.vector.tensor_copy(out=ni[:], in_=ntf[:]), r=["ntf"], w=["ni"])
            T.op(DVE, lambda: nc.vector.tensor_scalar(out=ni[:], in0=ni[:], scalar1=7, scalar2=None, op0=ALU.arith_shift_right),
                 r=["ni"], w=["ni"])
            T.op(DVE, lambda: nc.vector.tensor_copy(out=ntf[:], in_=ni[:]), r=["ni"], w=["ntf"])
            T.op(DVE, lambda: nc.vector.memset(onesr[:], 1.0), w=["ones32"])
            T.op(DVE, lambda: nc.vector.tensor_tensor_scan(out=tend[:], data0=onesr[:], data1=ntf[:], initial=0.0, op0=ALU.mult, op1=ALU.add),
                 r=["ones32", "ntf"], w=["tend"])
            T.op(DVE, lambda: nc.vector.tensor_tensor(out=off[:], in0=tend[:], in1=ntf[:], op=ALU.subtract), r=["tend", "ntf"], w=["off"])
            T.op(DVE, lambda: nc.vector.tensor_scalar(out=off[:], in0=off[:], scalar1=128.0, scalar2=None, op0=ALU.mult), r=["off"], w=["off"])
            for k, mk in enumerate((m1_all, m2_all)):
                T.op(DVE, lambda: nc.vector.tensor_tensor(out=big[:], in0=rank_all[:], in1=off[:].unsqueeze(1).to_broadcast([128, NT, 32]), op=ALU.add),
                     r=[("rank",), "off"], w=["big"])
                T.op(DVE, lambda mk=mk: nc.vector.tensor_tensor(out=big[:], in0=big[:], in1=mk[:], op=ALU.mult), r=["big", ("m1",), ("m2",)], w=["big"])
                T.op(DVE, lambda k=k: nc.vector.tensor_reduce(out=pf[:, k, :], in_=big[:], axis=AX.X, op=ALU.add), r=["big"], w=[("pf", k)])
            T.op(DVE, lambda: nc.vector.tensor_scalar(out=pf[:], in0=pf[:], scalar1=0.0, scalar2=float(NSLOT * 128 - 1), op0=ALU.max, op1=ALU.min),
                 r=[("pf",)], w=[("pf",)])
            T.op(DVE, lambda: nc.vector.tensor_copy(out=P12[:], in_=pf[:]), r=[("pf",)], w=["P12"])
            T.op(POOL, lambda: nc.gpsimd.iota(out=sidx[:], pattern=[[1, NSLOT]], base=0, channel_multiplier=0, allow_small_or_imprecise_dtypes=True), w=["sidx"])
            T.op(POOL, lambda: nc.gpsimd.iota(out=pio[:], pattern=[[1, 1]], base=0, channel_multiplier=1, allow_small_or_imprecise_dtypes=True), w=["pio"])
            T.op(DVE, lambda: nc.vector.tensor_tensor(out=cmp3[:], in0=tend[:].unsqueeze(1).to_broadcast([128, NSLOT, 32]),
                                                      in1=sidx[:].unsqueeze(2).to_broadcast([128, NSLOT, 32]), op=ALU.is_le),
                 r=["tend", "sidx"], w=["cmp3"])
            T.op(DVE, lambda: nc.vector.tensor_reduce(out=esl[:], in_=cmp3[:], axis=AX.X, op=ALU.add), r=["cmp3"], w=["esl"])
            T.op(DVE, lambda: nc.vector.tensor_scalar(out=esl[:], in0=esl[:], scalar1=31.0, scalar2=128.0, op0=ALU.min, op1=ALU.mult), r=["esl"], w=["esl"])
            T.op(DVE, lambda: nc.vector.tensor_scalar(out=esl[:], in0=esl[:], scalar1=pio[:], scalar2=None, op0=ALU.add), r=["esl", "pio"], w=["esl"])
            for ci in range(NSPL):
                T.op(DVE, lambda ci=ci: nc.vector.tensor_scalar(out=sidx[:], in0=esl[:], scalar1=float(NSPL), scalar2=float(ci), op0=ALU.mult, op1=ALU.add),
                     r=["esl", "cmp3"], w=["sidx"])
                T.op(DVE, lambda ci=ci: nc.vector.tensor_copy(out=widx[:, ci, :], in_=sidx[:]), r=["sidx"], w=[("widx", ci)])
            T.barrier()
        if dbg and "P12" in dbg_out:
            T.dma(SP, dbg_out["P12"][:, :], P12[:].rearrange("p k t -> p (k t)"), r=["P12"])
            T.dma(SP, dbg_out["widx"][:, :], widx[:, 0, :], r=["widx"])

        KE = int(os.environ.get("KE", 9))
        if KE >= 2:
            x2l = [sb(f"x2l{i}", [128, D], BF16) for i in range(3)]
            for t in range(NT):
                b = t % 3
                T.dma(SP, x2l[b][:], X2_d[t * 128:(t + 1) * 128, :], w=[("x2l", b)])
                for k in range(2):
                    T.dmai(Xs_d[:, :], x2l[b][:], out_idx=P12[:, k, t:t + 1], r=[("x2l", b), "P12"], w=[("Xs", t, k)])
            T.barrier()

        if KE >= 3:
            xs = [sb(f"xs{i}", [128, D], BF16) for i in range(3)]
            wsl = [sb(f"wsl{i}", [128, 6144], BF16) for i in range(3)]
            xT = [sb(f"xTs{i}", [128, 8, 128], BF16) for i in range(2)]
            sS = [sb(f"sS{i}", [128, 256], F32) for i in range(2)]
            ac = [sb(f"ac{i}", [128, 256], BF16) for i in range(2)]
            aT = [sb(f"aTs{i}", [128, 2, 128], BF16) for i in range(2)]
            yo = [sb(f"yo{i}", [128, D], BF16) for i in range(2)]
            NSL = int(os.environ.get("KSLOTS", NSLOT))

            def load_slot(sl):
                b3 = sl % 3
                T.dma(SP, xs[b3][:], Xs_d[sl * 128:(sl + 1) * 128, :], r=[("Xs",)], w=[("xs", b3)])
                cw_ = 6144 // NSPL
                for ci in range(NSPL):
                    T.dmai(wsl[b3][:, ci * cw_:(ci + 1) * cw_], Wcat_d.rearrange("r (c w) -> (r c) w", c=NSPL), in_idx=widx[:, ci, sl:sl + 1],
                           r=[("Wcat",), "widx"], w=[("wsl", b3, ci)])

            def slot_s1(sl):
                b2, b3 = sl % 2, sl % 3
                base = 4 * b2
                xTp = psb(base).rearrange("p (k t) -> p k t", k=8)
                for kc in range(8):
                    T.op(PE, lambda kc=kc: nc.tensor.transpose(xTp[:, kc, :], xs[b3][:, kc * 128:(kc + 1) * 128], ident_b),
                         r=[("xs", b3), "cstb"], w=[("ps", base)])
                T.op(ACT, lambda: nc.scalar.copy(out=xT[b2][:, 0:4, :], in_=xTp[:, 0:4, :]), r=[("ps", base)], w=[("xTs", b2, 0)])
                T.op(DVE, lambda: nc.vector.tensor_copy(out=xT[b2][:, 4:8, :], in_=xTp[:, 4:8, :]), r=[("ps", base), ("xTs", b2, 0)], w=[("xTs", b2, 1)])
                for kc in range(8):
                    T.op(PE, lambda kc=kc: nc.tensor.matmul(PS[base + 1][:, :], lhsT=xT[b2][:, kc, :], rhs=wsl[b3][:, kc * 512:(kc + 1) * 512],
                                                            start=(kc == 0), stop=(kc == 7)), r=[("xTs", b2), ("wsl", b3)], w=[("ps", base + 1)])

            def slot_s2(sl):
                b2, b3 = sl % 2, sl % 3
                base = 4 * b2
                T.op(ACT, lambda: nc.scalar.activation(out=sS[b2][:], in_=PS[base + 1][:, 0:256], func=AF.Silu), r=[("ps", base + 1)], w=[("sS", b2)])
                T.op(DVE, lambda: nc.vector.tensor_tensor(out=ac[b2][:], in0=PS[base + 1][:, 256:512], in1=sS[b2][:], op=ALU.mult),
                     r=[("ps", base + 1), ("sS", b2)], w=[("ac", b2)])
                aTp = psb(base)[:, 0:256].rearrange("p (k t) -> p k t", k=2)
                for fc in range(2):
                    T.op(PE, lambda fc=fc: nc.tensor.transpose(aTp[:, fc, :], ac[b2][:, fc * 128:(fc + 1) * 128], ident_b),
                         r=[("ac", b2), "cstb"], w=[("ps", base)])
                T.op(ACT, lambda: nc.scalar.copy(out=aT[b2][:], in_=aTp), r=[("ps", base)], w=[("aTs", b2)])
                for cb_ in range(2):
                    for fc in range(2):
                        T.op(PE, lambda fc=fc, cb_=cb_: nc.tensor.matmul(
                            PS[base + 2 + cb_][:, :], lhsT=aT[b2][:, fc, :], rhs=wsl[b3][:, 4096 + fc * 1024 + cb_ * 512: 4096 + fc * 1024 + (cb_ + 1) * 512],
                            start=(fc == 0), stop=(fc == 1)), r=[("aTs", b2), ("wsl", b3)], w=[("ps", base + 2 + cb_)])
                T.op(ACT, lambda: nc.scalar.copy(out=yo[b2][:, 0:512], in_=PS[base + 2][:, :]), r=[("ps", base + 2)], w=[("yo", b2, 0)])
                T.op(DVE, lambda: nc.vector.tensor_copy(out=yo[b2][:, 512:1024], in_=PS[base + 3][:, :]), r=[("ps", base + 3)], w=[("yo", b2, 1)])
                T.dma(SP, Ys_d[sl * 128:(sl + 1) * 128, :], yo[b2][:], r=[("yo", b2)], w=[("Ys", sl)])

            for sl in range(min(2, NSL)):
                load_slot(sl)
            slot_s1(0)
            for sl in range(NSL):
                if sl + 2 < NSL:
                    load_slot(sl + 2)
                if sl + 1 < NSL:
                    slot_s1(sl + 1)
                slot_s2(sl)
            T.barrier()

        if KE >= 4:
            y1 = [sb(f"y1_{i}", [128, D], BF16) for i in range(3)]
            ya = [sb(f"ya_{i}", [128, D], F32) for i in range(3)]
            y2 = [sb(f"y2_{i}", [128, D], BF16) for i in range(3)]
            x1l = [sb(f"x1l{i}", [128, D], F32) for i in range(3)]
            def load_tok(t):
                b = t % 3
                T.dma(SP, x1l[b][:], x1_d[t * 128:(t + 1) * 128, :], w=[("x1l", b)])
                T.dmai(y1[b][:], Ys_d[:, :], in_idx=P12[:, 0, t:t + 1], r=[("Ys",), "P12"], w=[("y1", b)])
                T.dmai(y2[b][:], Ys_d[:, :], in_idx=P12[:, 1, t:t + 1], r=[("Ys",), "P12"], w=[("y2", b)])

            load_tok(0)
            load_tok(1)
            for t in range(NT):
                b = t % 3
                if t + 2 < NT:
                    load_tok(t + 2)
                T.op(DVE, lambda b=b, t=t: nc.vector.tensor_scalar(out=ya[b][:], in0=y1[b][:], scalar1=w12[:, t, 0:1], scalar2=None, op0=ALU.mult),
                     r=[("y1", b), ("w12",)], w=[("ya", b)])
                T.op(DVE, lambda b=b, t=t: nc.vector.scalar_tensor_tensor(out=ya[b][:], in0=y2[b][:], scalar=w12[:, t, 1:2], in1=ya[b][:], op0=ALU.mult, op1=ALU.add),
                     r=[("ya", b), ("y2", b), ("w12",)], w=[("ya", b)])
                T.op(DVE, lambda b=b: nc.vector.tensor_tensor(out=ya[b][:], in0=ya[b][:], in1=g2b[:], op=ALU.mult), r=[("ya", b), ("g2b",)], w=[("ya", b)])
                T.op(DVE, lambda b=b: nc.vector.tensor_tensor(out=x1l[b][:], in0=x1l[b][:], in1=ya[b][:], op=ALU.add), r=[("ya", b), ("x1l", b)], w=[("x1l", b)])
                T.dma(SP, out_d[t * 128:(t + 1) * 128, :], x1l[b][:], r=[("x1l", b)], w=[("out", t)])
            T.barrier()
    return finish()


def _consts():
    c = np.zeros((128, NCST), np.float32)
    c[:, 0:128] = np.eye(128, dtype=np.float32)
    s = np.arange(128)[:, None]
    t = np.arange(128)[None, :]
    c[:, 128:256] = (s <= t).astype(np.float32)
    c[:, 256:384] = ((s // 64) == (t // 64)).astype(np.float32)
    R = np.zeros((128, 128), np.float32)
    for base in (0, 64):
        for d in range(8):
            R[base + d, base + d + 8] = -1.0
            R[base + d + 8, base + d] = 1.0
    c[:, 384:512] = R.T
    c[:, 512:640] = 1.0
    inv = 500000.0 ** (-np.arange(0, 16, 2, dtype=np.float32) / 16.0)
    for p in range(128):
        d = p % 64
        c[p, 640] = inv[d % 8] if d < 16 else 0.0
    return c


_NC_CACHE = {}


def make_in_maps(inputs):
    f = lambda k: np.ascontiguousarray(np.asarray(inputs[k], dtype=np.float32))
    x = f("x")
    c = f("c")
    pos = np.ascontiguousarray(np.asarray(inputs["positions"], dtype=np.int32))
    b_ada = f("b_ada")[0]
    shared = {
        "w_ada": f("w_ada")[0],
        "b_ada_col": np.ascontiguousarray(b_ada.reshape(48, 128).T),
        "b_ada_row": np.ascontiguousarray(b_ada.reshape(1, -1)),
        "n1w": np.ascontiguousarray(f("norm1_w")[0].reshape(8, 128).T),
        "n2w": np.ascontiguousarray(f("norm2_w")[0].reshape(8, 128).T),
        "n2row": np.ascontiguousarray(f("norm2_w")[0].reshape(1, D)),
        "w_in": f("w_in")[0],
        "bgate": np.ascontiguousarray(np.stack([f("b_igate")[0], f("b_fgate")[0]], axis=1)),
        "convw": np.ascontiguousarray(f("conv_w")[0].T.reshape(8, 128, 4).transpose(1, 0, 2)),
        "convb": np.ascontiguousarray(f("conv_b")[0].reshape(8, 128).T),
        "mnw": np.ascontiguousarray(f("mlstm_norm_w")[0].reshape(1, 512)),
        "qnw": np.ascontiguousarray(np.tile(f("q_norm_w")[0], 2).reshape(128, 1)),
        "knw": np.ascontiguousarray(np.tile(f("k_norm_w")[0], 2).reshape(128, 1)),
        "qkrow": np.ascontiguousarray(np.concatenate([f("q_norm_w")[0], f("k_norm_w")[0]]).reshape(1, 128)),
        "lamv": np.ascontiguousarray(np.concatenate([f("lam_q1")[0], f("lam_k1")[0], f("lam_q2")[0], f("lam_k2")[0]]).reshape(1, 256)),
        "subw": np.ascontiguousarray(f("subln_w")[0].reshape(128, 1)),
        "w_br_m": f("w_br_m")[0],
        "w_br_d": f("w_br_d")[0],
        "w_out": f("w_out")[0],
        "wr": np.ascontiguousarray(np.concatenate([f("w_rg")[0], f("w_re")[0]], axis=1)),
        "br": np.ascontiguousarray(np.concatenate([f("b_rg")[0], f("b_re")[0]]).reshape(1, 36)),
        "w_gate": f("w_gate")[0],
        "w_up": f("w_up")[0],
        "w_down": f("w_down")[0],
        "consts": _consts(),
    }
    maps = []
    for b in range(8):
        m = dict(shared)
        m["x"] = x[b]
        m["cT"] = np.ascontiguousarray(c[b].reshape(8, 128).T)
        m["pos"] = np.ascontiguousarray(pos[b].reshape(1, S))
        maps.append(m)
    return maps


def kernel(**inputs):
    if "nc" not in _NC_CACHE:
        _NC_CACHE["nc"] = build()
    nc = _NC_CACHE["nc"]
    maps = make_in_maps(inputs)
    res = run_bass_kernel_spmd(nc, maps, core_ids=list(range(8)))
    out = np.stack([np.asarray(r["out"], dtype=np.float32) for r in res.results], axis=0)
    return out
```

```python
import math
import os
from contextlib import ExitStack

import numpy as np
import concourse.bass as bass
import concourse.mybir as mybir
from concourse.bass_utils import run_bass_kernel_spmd

F32 = mybir.dt.float32
BF16 = mybir.dt.bfloat16
I32 = mybir.dt.int32
AF = mybir.ActivationFunctionType
ALU = mybir.AluOpType
AX = mybir.AxisListType

S = 4096
D = 1024
NT = 32
NB = 8
EPS = 1e-6
NCST = 5 * 128 + 2


class _StopBuild(Exception):
    pass


class Trk:
    LIMIT = 50000
    NDSEM = 12

    def __init__(self, nc):
        self.nc = nc
        self.eng = {"pe": nc.tensor, "act": nc.scalar, "dve": nc.vector, "pool": nc.gpsimd, "sp": nc.sync}
        self.cur = {}
        self.seq = {}
        self.gen = {}
        for e in self.eng:
            self.cur[e] = [nc.alloc_semaphore(f"s_{e}_0"), 0]
            self.seq[e] = 0
            self.gen[e] = 0
        self.allsems = [(e, self.cur[e]) for e in self.eng]
        self.waited = {e: {} for e in self.eng}
        self.lastw = {}
        self.readers = {}
        self.children = {}
        self.dring = {}
        for q in ("sp", "pool", "act", "wc"):
            self.dring[q] = [[nc.alloc_semaphore(f"d_{q}_{i}"), 0] for i in range(self.NDSEM)]
        self.dpos = {q: 0 for q in self.dring}
        self.all_dma = []

    def _related(self, k):
        ks = [k[:i] for i in range(1, len(k) + 1)]
        stack = [k]
        while stack:
            p = stack.pop()
            for c in self.children.get(p, ()):
                ks.append(c)
                stack.append(c)
        return ks

    def _register(self, k):
        for i in range(1, len(k)):
            self.children.setdefault(k[:i], set()).add(k[:i + 1])

    def _wait(self, e, ev):
        sem, val, src, sq = ev
        if src == e:
            if e == "pe":
                return
            if e != "pool" and self.seq[e] - sq >= 6:
                return
        w = self.waited[e]
        if w.get(sem.name, 0) >= val:
            return
        self.eng[e].wait_ge(sem, val)
        w[sem.name] = val

    def _deps(self, e, r, w):
        evs = []
        for k in r:
            for kk in self._related(k):
                if kk in self.lastw:
                    evs.append(self.lastw[kk])
        for k in w:
            for kk in self._related(k):
                if kk in self.lastw:
                    evs.append(self.lastw[kk])
                evs.extend(self.readers.get(kk, {}).values())
        for ev in evs:
            self._wait(e, ev)

    def _record(self, ev, r, w):
        for k in r:
            self._register(k)
            self.readers.setdefault(k, {})[ev[2] if ev[2] is not None else ev[0].name] = ev
        for k in w:
            self._register(k)
            self.lastw[k] = ev
            self.readers[k] = {}
            for kk in self._related(k):
                if kk != k and len(kk) > len(k):
                    self.readers[kk] = {}
                    self.lastw.pop(kk, None)

    @staticmethod
    def _norm(ks):
        out = []
        for k in ks:
            k = tuple(k) if isinstance(k, (tuple, list)) else (k,)
            if k[0] == "ps":
                k = k[:2]
            out.append(k)
        return out

    def start_record(self):
        self._rec = []

    def stop_record(self):
        rec, self._rec = self._rec, None
        return rec

    def op(self, e, fn, r=(), w=()):
        if getattr(self, "_rec", None) is not None:
            self._rec.append((e, fn, r, w))
            return None
        r = self._norm(r)
        w = self._norm(w)
        w = w + [k for k in r if k[0] == "ps" and k not in w]
        self._deps(e, r, w)
        ins = fn()
        c = self.cur[e]
        c[1] += 1
        self.seq[e] += 1
        ins.then_inc(c[0], 1)
        ev = (c[0], c[1], e, self.seq[e])
        self._record(ev, r, w)
        if c[1] >= self.LIMIT:
            self.gen[e] += 1
            self.cur[e] = [self.nc.alloc_semaphore(f"s_{e}_{self.gen[e]}"), 0]
            self.allsems.append((e, self.cur[e]))
        return ins

    def dma(self, q, out, in_, r=(), w=(), ring=None, **kw):
        r = self._norm(r)
        w = self._norm(w)
        rq = ring or q
        ring = self.dring[rq]
        slot = ring[self.dpos[rq] % self.NDSEM]
        self.dpos[rq] += 1
        if slot[1] > 0:
            self._wait(q, (slot[0], slot[1], None, 0))
        self._deps(q, r, w)
        ins = self.eng[q].dma_start(out=out, in_=in_, **kw)
        slot[1] += 16
        ins.then_inc(slot[0], 16)
        ev = (slot[0], slot[1], None, 0)
        self._record(ev, r, w)
        return ins

    def dmai(self, out, in_, out_idx=None, in_idx=None, r=(), w=()):
        q = "pool"
        r = self._norm(r)
        w = self._norm(w)
        ring = self.dring[q]
        slot = ring[self.dpos[q] % self.NDSEM]
        self.dpos[q] += 1
        if slot[1] > 0:
            self._wait(q, (slot[0], slot[1], None, 0))
        self._deps(q, r, w)
        ins = self.nc.gpsimd.indirect_dma_start(
            out=out, out_offset=(bass.IndirectOffsetOnAxis(ap=out_idx, axis=0) if out_idx is not None else None),
            in_=in_, in_offset=(bass.IndirectOffsetOnAxis(ap=in_idx, axis=0) if in_idx is not None else None))
        slot[1] += 16
        ins.then_inc(slot[0], 16)
        ev = (slot[0], slot[1], None, 0)
        self._record(ev, r, w)
        return ins

    def barrier(self, full=True):
        evs = []
        for (se, c) in self.allsems:
            if c[1] > 0:
                evs.append((c[0], c[1], "__" + se, 0))
        for q, ring in self.dring.items():
            if q == "wc" and not full:
                continue
            for slot in ring:
                if slot[1] > 0:
                    evs.append((slot[0], slot[1], None, 0))
        for e in self.eng:
            for ev in evs:
                if ev[2] == "__" + e:
                    continue
                self._wait(e, ev)
        self.lastw.clear()
        self.readers.clear()
        self.children.clear()


def build(stage=99, dbg=None):
    nc = bass.Bass("TRN2", target_bir_lowering=False)
    T = Trk(nc)
    _scope = [None]

    def scope_mark(nm):
        if os.environ.get("KSCOPE") != "1":
            return
        if _scope[0] is not None:
            _scope[0].__exit__(None, None, None)
        _scope[0] = nc.named_scope(nm)
        _scope[0].__enter__()

    PE, ACT, DVE, POOL, SP = "pe", "act", "dve", "pool", "sp"

    def din(name, shape, dt=F32):
        return nc.dram_tensor(name, list(shape), dt, kind="ExternalInput").ap()

    def dscr(name, shape, dt):
        return nc.dram_tensor(name, list(shape), dt, kind="Internal").ap()

    x_in = din("x", [S, D])
    cT_in = din("cT", [128, 8])
    pos_in = din("pos", [1, S], I32)
    w_ada = din("w_ada", [D, 6 * D])
    b_ada_col = din("b_ada_col", [128, 48])
    b_ada_row = din("b_ada_row", [1, 6 * D])
    n1w_in = din("n1w", [128, 8])
    n2w_in = din("n2w", [128, 8])
    n2row_in = din("n2row", [1, D])
    w_in = din("w_in", [D, 5640])
    bgate_in = din("bgate", [4, 2])
    convw_in = din("convw", [128, 8, 4])
    convb_in = din("convb", [128, 8])
    mnw_in = din("mnw", [1, 512])
    qnw_in = din("qnw", [128, 1])
    knw_in = din("knw", [128, 1])
    qkrow_in = din("qkrow", [1, 128])
    lamv_in = din("lamv", [1, 256])
    subw_in = din("subw", [128, 1])
    w_br_m = din("w_br_m", [512, D])
    w_br_d = din("w_br_d", [512, D])
    w_out = din("w_out", [D, D])
    wr_in = din("wr", [D, 36])
    br_in = din("br", [1, 36])
    w_gate = din("w_gate", [32, D, 256])
    w_up = din("w_up", [32, D, 256])
    w_down = din("w_down", [32, 256, D])
    consts_in = din("consts", [128, NCST])
    out_d = nc.dram_tensor("out", [S, D], F32, kind="ExternalOutput").ap()

    xnT_d = dscr("xnT_d", [D, S], BF16)
    hmT_d = dscr("hmT_d", [512, S], BF16)
    hdT_d = dscr("hdT_d", [512, S], BF16)
    x1_d = dscr("x1_d", [S, D], F32)
    xn2T_d = dscr("xn2T_d", [D, S], BF16)
    combT_d = dscr("combT_d", [32, S], BF16)
    dec_d = dscr("dec_d", [4, 32], F32)
    gi_d = dscr("gi_d", [4, S], F32)
    NSLOT = 96
    X2_d = dscr("X2_d", [S, D], BF16)
    Xs_d = dscr("Xs_d", [NSLOT * 128, D], BF16)
    Ys_d = dscr("Ys_d", [NSLOT * 128, D], BF16)
    Wcat_d = dscr("Wcat_d", [32 * 128, 6144], BF16)
    gf_d = dscr("gf_d", [4, S], F32)

    dbg_out = {}
    if dbg:
        for name, (shape, dt) in dbg.items():
            dbg_out[name] = nc.dram_tensor("dbg_" + name, list(shape), dt, kind="ExternalOutput").ap()

    PS = [nc.alloc_psum_tensor(f"ps{i}", [128, 512], F32) for i in range(8)]

    def psb(i):
        return PS[i][:].bitcast(BF16)

    xnT_v = xnT_d.rearrange("(k p) t -> p k t", p=128)
    xn2T_v = xn2T_d.rearrange("(k p) t -> p k t", p=128)
    hmT_v = hmT_d.rearrange("(k p) t -> p k t", p=128)
    hdT_v = hdT_d.rearrange("(k p) t -> p k t", p=128)
    w_in_v = w_in.rearrange("(k p) n -> p k n", p=128)
    wg_v = w_gate.rearrange("e (k p) f -> e p k f", p=128)
    wu_v = w_up.rearrange("e (k p) f -> e p k f", p=128)
    wdn_v = w_down.rearrange("e (k p) n -> e p k n", p=128)

    glob = ExitStack()

    def sbg(name, shape, dt):
        return glob.enter_context(nc.sbuf_tensor("s_" + name, list(shape), dt))

    cst = sbg("cst", [128, NCST], F32)
    cstb = sbg("cstb", [128, 5 * 128], BF16)
    modc = sbg("modc", [128, 48], F32)
    a1 = sbg("a1", [128, 8], F32)
    a2 = sbg("a2", [128, 8], F32)
    g1b = sbg("g1b", [128, D], F32)
    g2b = sbg("g2b", [128, D], F32)
    a2b = sbg("a2b", [128, D], F32)
    s2b = sbg("s2b", [128, D], F32)
    rank_all = sbg("rank_all", [128, NT, 32], F32)
    m1_all = sbg("m1_all", [128, NT, 32], F32)
    m2_all = sbg("m2_all", [128, NT, 32], F32)
    w12 = sbg("w12", [128, NT, 2], F32)
    tot = sbg("tot", [128, 32], F32)
    smallc = sbg("smallc", [128, 16], F32)
    ident_f = cst[:, 0:128]
    tri_f = cst[:, 128:256]
    invf = cst[:, 640:641]
    ident_b = cstb[:, 0:128]
    tri_b = cstb[:, 128:256]
    bo64_b = cstb[:, 256:384]
    RT_b = cstb[:, 384:512]
    ones_b = cstb[:, 512:640]
    ones_f = cst[:, 512:640]

    T.dma(SP, cst[:], consts_in[:, :], w=["cst"])
    T.op(DVE, lambda: nc.vector.tensor_copy(out=cstb[:], in_=cst[:, 0:640]), r=["cst"], w=["cstb"])

    early2 = ExitStack()
    CosB = early2.enter_context(nc.sbuf_tensor("s_CosB", [128, S], BF16))
    SinB = early2.enter_context(nc.sbuf_tensor("s_SinB", [128, S], BF16))
    early2_open = [True]

    def sin_reduce(ph_sb, dst_bf, ang, n, tagk):
        ki = ph_sb["ki"]
        kf = ph_sb["kf"]
        msk = ph_sb["msk"]
        C1 = 6.28125
        C2 = 2.0 * math.pi - 6.28125
        T.op(DVE, lambda: nc.vector.tensor_scalar(out=ki[:, 0:n], in0=ang, scalar1=1.0 / (2.0 * math.pi), scalar2=None, op0=ALU.mult),
             r=[tagk], w=["ki"])
        T.op(DVE, lambda: nc.vector.tensor_copy(out=kf[:, 0:n], in_=ki[:, 0:n]), r=["ki"], w=["kf"])
        T.op(DVE, lambda: nc.vector.scalar_tensor_tensor(out=ang, in0=kf[:, 0:n], scalar=-C1, in1=ang, op0=ALU.mult, op1=ALU.add),
             r=["kf", tagk], w=[tagk])
        T.op(DVE, lambda: nc.vector.scalar_tensor_tensor(out=ang, in0=kf[:, 0:n], scalar=-C2, in1=ang, op0=ALU.mult, op1=ALU.add),
             r=["kf", tagk], w=[tagk])
        T.op(DVE, lambda: nc.vector.tensor_scalar(out=msk[:, 0:n], in0=ang, scalar1=math.pi, scalar2=-2.0 * math.pi, op0=ALU.is_gt, op1=ALU.mult),
             r=[tagk], w=["msk"])
        T.op(DVE, lambda: nc.vector.tensor_tensor(out=ang, in0=ang, in1=msk[:, 0:n], op=ALU.add), r=[tagk, "msk"], w=[tagk])
        T.op(DVE, lambda: nc.vector.tensor_scalar(out=msk[:, 0:n], in0=ang, scalar1=-math.pi, scalar2=2.0 * math.pi, op0=ALU.is_lt, op1=ALU.mult),
             r=[tagk], w=["msk"])
        T.op(DVE, lambda: nc.vector.tensor_tensor(out=ang, in0=ang, in1=msk[:, 0:n], op=ALU.add), r=[tagk, "msk"], w=[tagk])
        T.op(DVE, lambda: nc.vector.tensor_scalar(out=ang, in0=ang, scalar1=math.pi, scalar2=-math.pi, op0=ALU.min, op1=ALU.max),
             r=[tagk], w=[tagk])
        T.op(ACT, lambda: nc.scalar.activation(out=dst_bf, in_=ang, func=AF.Sin), r=[tagk], w=[tagk + "_o"])

    early = ExitStack()
    NWST = int(os.environ.get("KWST", 2))
    wst = [early.enter_context(nc.sbuf_tensor(f"s_wst{i}", [128, 6144], BF16)) for i in range(NWST)]
    for e in range(32):
        b = e % NWST
        gu = wst[b][:, 0:4096].rearrange("p (k f) -> p k f", k=8)
        T.dma(POOL, gu[:, :, 0:256], wg_v[e], w=[("wst", b, 0)], ring="wc")
        T.dma(POOL, gu[:, :, 256:512], wu_v[e], w=[("wst", b, 1)], ring="wc")
        T.dma(POOL, wst[b][:, 4096:6144].rearrange("p (k n) -> p k n", k=2), wdn_v[e], w=[("wst", b, 2)], ring="wc")
        T.dma(POOL, Wcat_d[e * 128:(e + 1) * 128, :], wst[b][:], r=[("wst", b)], w=[("Wcat", e)], ring="wc")
    early_open = [True]
    scope_mark("ph0")
    with ExitStack() as ph:
        def sb(name, shape, dt):
            return ph.enter_context(nc.sbuf_tensor("s_" + name, list(shape), dt))
        cT = sb("cT", [128, 8], F32)
        cT2 = sb("cT2", [128, 8, 2], F32)
        cbc = sb("cbc", [128, 8, 128], F32)
        wa = [sb(f"wa{i}", [128, 8, 512], F32) for i in range(2)]
        bcol = sb("bcol", [128, 48], F32)
        brow = sb("brow", [128, 2048], F32)
        nw = sb("nw", [128, 16], F32)
        tmp8 = sb("tmp8", [128, 8], F32)
        lamb = sb("lamb", [128, 256], F32)
        qkrow = sb("qkrow", [128, 128], F32)
        lt = sb("lt", [128, 128], F32)
        l2 = sb("l2", [128, 4], F32)

        T.dma(SP, cT[:], cT_in[:, :], w=["cT"])
        T.dma(SP, bcol[:], b_ada_col[:, :], w=["bcol"])
        T.dma(SP, brow[:, 0:1024], b_ada_row[0:1, 2048:3072].partition_broadcast(128), w=["brow0"])
        T.dma(SP, brow[:, 1024:2048], b_ada_row[0:1, 5120:6144].partition_broadcast(128), w=["brow1"])
        T.dma(SP, nw[:, 0:8], n1w_in[:, :], w=["nw0"])
        T.dma(SP, nw[:, 8:16], n2w_in[:, :], w=["nw1"])
        T.dma(SP, smallc[:, 0:1], qnw_in[:, :], w=["sc0"])
        T.dma(SP, smallc[:, 1:2], knw_in[:, :], w=["sc1"])
        T.dma(SP, smallc[:, 2:3], subw_in[:, :], w=["sc2"])
        T.dma(SP, lamb[:], lamv_in[0:1, :].partition_broadcast(128), w=["lamb"])
        T.dma(SP, qkrow[:], qkrow_in[0:1, :].partition_broadcast(128), w=["qkrow"])
        for k in range(2):
            T.op(DVE, lambda k=k: nc.vector.tensor_copy(out=cT2[:, :, k], in_=cT[:]), r=["cT"], w=[("cT2", k)])
        for kc in range(8):
            T.op(DVE, lambda kc=kc: nc.vector.tensor_copy(out=cbc[:, kc, :], in_=cT[:, kc:kc + 1].to_broadcast([128, 128])),
                 r=["cT"], w=[("cbc", kc)])
        if True:
            posi = ph.enter_context(nc.sbuf_tensor("s_posi", [128, 1024], I32))
            posf = ph.enter_context(nc.sbuf_tensor("s_posf", [128, 1024], F32))
            ang = ph.enter_context(nc.sbuf_tensor("s_ang", [128, 1024], F32))
            tb = dict(ki=ph.enter_context(nc.sbuf_tensor("s_ki", [128, 1024], I32)),
                      kf=ph.enter_context(nc.sbuf_tensor("s_kf", [128, 1024], F32)),
                      msk=ph.enter_context(nc.sbuf_tensor("s_msk", [128, 1024], F32)))
            for c4 in range(4):
                cs = slice(c4 * 1024, (c4 + 1) * 1024)
                T.dma(SP, posi[:], pos_in[0:1, cs].partition_broadcast(128), w=["posi"])
                T.op(DVE, lambda: nc.vector.tensor_copy(out=posf[:], in_=posi[:]), r=["posi"], w=["posf"])
                T.op(DVE, lambda: nc.vector.tensor_scalar(out=ang[:], in0=posf[:], scalar1=invf, scalar2=None, op0=ALU.mult),
                     r=["posf", "cst"], w=["ang"])
                sin_reduce(tb, SinB[:, cs], ang[:], 1024, "ang")
                T.op(DVE, lambda: nc.vector.tensor_scalar(out=ang[:], in0=posf[:], scalar1=invf, scalar2=math.pi / 2, op0=ALU.mult, op1=ALU.add),
                     r=["posf", "cst", "ang_o"], w=["ang"])
                sin_reduce(tb, CosB[:, cs], ang[:], 1024, "ang")

        w_ada_v = w_ada.rearrange("(k p) n -> p k n", p=128)
        gate_bank = {4: 1, 5: 2, 6: 5, 7: 6, 8: 1, 9: 2, 10: 3, 11: 4}
        n2rb = sb("n2rb", [128, D], F32)
        T.dma(SP, n2rb[:], n2row_in[0:1, :].partition_broadcast(128), w=["n2rb"])
        T.dma(SP, s2b[:], b_ada_row[0:1, 3072:4096].partition_broadcast(128), w=["s2b_bias"])
        T.dma(SP, a2b[:], b_ada_row[0:1, 4096:5120].partition_broadcast(128), w=["a2b_bias"])
        for blk in range(12):
            b = blk % 2
            for hh in range(2):
                T.dma(SP, wa[b][:, 4 * hh:4 * hh + 4, :], w_ada_v[:, 4 * hh:4 * hh + 4, blk * 512:(blk + 1) * 512], w=[("wa", b)])
            for m in range(4):
                j = blk * 4 + m
                for kc in range(8):
                    T.op(PE, lambda j=j, m=m, kc=kc, b=b: nc.tensor.matmul(
                        PS[0][:, 2 * j:2 * j + 2], lhsT=wa[b][:, kc, m * 128:(m + 1) * 128], rhs=cT2[:, kc, :],
                        start=(kc == 0), stop=(kc == 7)), r=[("wa", b), "cT2"], w=[("ps", 0)])
            if blk in gate_bank:
                gb = gate_bank[blk]
                for kc in range(8):
                    T.op(PE, lambda kc=kc, b=b, gb=gb: nc.tensor.matmul(
                        PS[gb][:, :], lhsT=cbc[:, kc, :], rhs=wa[b][:, kc, :], start=(kc == 0), stop=(kc == 7)),
                        r=[("wa", b), "cbc"], w=[("ps", gb)])
                if blk == 5:
                    for hh in range(2):
                        T.op(DVE, lambda hh=hh: nc.vector.tensor_tensor(out=g1b[:, hh * 512:(hh + 1) * 512], in0=PS[1 + hh][:, :],
                                                                      in1=brow[:, hh * 512:(hh + 1) * 512], op=ALU.add),
                             r=[("ps", 1 + hh), "brow0"], w=[("g1b", hh)])
                if blk == 7:
                    for hh in range(2):
                        T.op(DVE, lambda hh=hh: nc.vector.tensor_tensor(out=s2b[:, hh * 512:(hh + 1) * 512], in0=PS[5 + hh][:, :],
                                                                      in1=s2b[:, hh * 512:(hh + 1) * 512], op=ALU.add),
                             r=[("ps", 5 + hh), "s2b_bias"], w=[("s2b", hh)])
                if blk == 9:
                    for hh in range(2):
                        sl = slice(hh * 512, (hh + 1) * 512)
                        T.op(DVE, lambda hh=hh, sl=sl: nc.vector.tensor_tensor(out=a2b[:, sl], in0=PS[1 + hh][:, :], in1=a2b[:, sl], op=ALU.add),
                             r=[("ps", 1 + hh), "a2b_bias"], w=[("a2b", hh)])
                        T.op(DVE, lambda sl=sl: nc.vector.scalar_tensor_tensor(out=a2b[:, sl], in0=a2b[:, sl], scalar=1.0, in1=n2rb[:, sl], op0=ALU.add, op1=ALU.mult),
                             r=[("a2b", hh), "n2rb"], w=[("a2b", hh)])
        T.op(DVE, lambda: nc.vector.tensor_tensor(
            out=modc[:], in0=PS[0][:, 0:96].rearrange("p (m two) -> p m two", two=2)[:, :, 0], in1=bcol[:], op=ALU.add),
            r=[("ps", 0), "bcol"], w=["modc"])
        for gi, (gt, b0) in enumerate(((g1b, 1), (g2b, 3))):
            if gi == 0:
                continue
            for hh in range(2):
                T.op(DVE, lambda gt=gt, b0=b0, hh=hh, gi=gi: nc.vector.tensor_tensor(
                    out=gt[:, hh * 512:(hh + 1) * 512], in0=PS[b0 + hh][:, :],
                    in1=brow[:, gi * 1024 + hh * 512: gi * 1024 + (hh + 1) * 512], op=ALU.add),
                    r=[("ps", b0 + hh), f"brow{gi}"], w=[(f"g{gi + 1}b", hh)])
        for (av, sc0, nwo, akey) in ((a1, 8, 0, "a1"), (a2, 32, 8, "a2")):
            T.op(DVE, lambda sc0=sc0: nc.vector.tensor_scalar(out=tmp8[:], in0=modc[:, sc0:sc0 + 8], scalar1=1.0, scalar2=None, op0=ALU.add),
                 r=["modc"], w=["tmp8"])
            T.op(DVE, lambda av=av, nwo=nwo: nc.vector.tensor_tensor(out=av[:], in0=tmp8[:], in1=nw[:, nwo:nwo + 8], op=ALU.mult),
                 r=["tmp8", "nw0", "nw1"], w=[akey])
        T.op(DVE, lambda: nc.vector.tensor_scalar(out=smallc[:, 2:3], in0=smallc[:, 2:3], scalar1=0.8, scalar2=None, op0=ALU.mult),
             r=["sc2"], w=["sc2"])
        T.op(DVE, lambda: nc.vector.tensor_reduce(out=l2[:, 0:2], in_=qkrow[:].rearrange("p (a b) -> p a b", a=2), axis=AX.X, op=ALU.max,
                                                  apply_absolute_value=True), r=["qkrow"], w=["l2a"])
        T.op(DVE, lambda: nc.vector.tensor_tensor(out=l2[:, 2:3], in0=l2[:, 0:1], in1=l2[:, 1:2], op=ALU.mult), r=["l2a"], w=["l2b"])
        T.op(DVE, lambda: nc.vector.tensor_scalar(out=smallc[:, 3:4], in0=l2[:, 2:3], scalar1=-8.0, scalar2=None, op0=ALU.mult),
             r=["l2b"], w=["sc3"])
        lv = lamb[:].rearrange("p (a b) -> p a b", a=4)
        T.op(DVE, lambda: nc.vector.tensor_tensor(out=lt[:].rearrange("p (a b) -> p a b", a=2), in0=lv[:, 0:4:2, :], in1=lv[:, 1:4:2, :], op=ALU.mult),
             r=["lamb"], w=["lt"])
        T.op(DVE, lambda: nc.vector.tensor_reduce(out=l2[:, 0:2], in_=lt[:].rearrange("p (a b) -> p a b", a=2), axis=AX.X, op=ALU.add),
             r=["lt", "l2a", "l2b"], w=["l2a"])
        T.op(ACT, lambda: nc.scalar.activation(out=l2[:, 2:4], in_=l2[:, 0:2], func=AF.Exp), r=["l2a"], w=["l2b"])
        T.op(DVE, lambda: nc.vector.tensor_tensor(out=l2[:, 0:1], in0=l2[:, 3:4], in1=l2[:, 2:3], op=ALU.subtract), r=["l2b"], w=["l2a"])
        T.op(DVE, lambda: nc.vector.tensor_scalar(out=smallc[:, 4:5], in0=l2[:, 0:1], scalar1=-0.2, scalar2=None, op0=ALU.add),
             r=["l2a"], w=["sc4"])
        T.barrier(full=False)
    if dbg and "modc" in dbg_out:
        T.dma(SP, dbg_out["modc"][:, :], modc[:], r=["modc"])
        T.dma(SP, dbg_out["g1b"][:, :], g1b[:], r=["g1b"])
        T.dma(SP, dbg_out["smallc"][:, :], smallc[:], r=["sc0"])

    def dump_dram(dst, src, rows, cols, dt):
        with nc.sbuf_tensor("s_dump_" + dst.name.replace(".", "_"), [128, cols], dt) as tmp:
            for r0 in range(0, rows, 128):
                n = min(128, rows - r0)
                T.dma(SP, tmp[0:n, :], src[r0:r0 + n, :], w=["dumptmp"])
                T.dma(SP, dst[r0:r0 + n, :], tmp[0:n, :], r=["dumptmp"], w=[("dumpdst", r0)])
            T.barrier()

    def finish():
        if _scope[0] is not None:
            _scope[0].__exit__(None, None, None)
            _scope[0] = None
        T.barrier()
        if early_open[0]:
            early.close()
            early_open[0] = False
        if early2_open[0]:
            early2.close()
            early2_open[0] = False
        glob.close()
        return nc

    if stage <= 0:
        return finish()

    s1 = modc[:, 0:8]
    s2 = modc[:, 24:32]

    scope_mark("ph1")
    def norm_tile(ph_bufs, src_ap, rkeys, tag):
        ss, sd, junk = ph_bufs["ss"], ph_bufs["sd"], ph_bufs["junk"]
        T.op(ACT, lambda: nc.scalar.activation(out=junk[:], in_=src_ap, func=AF.Square, accum_out=ss[:]), r=rkeys, w=["junk", "ss"])
        T.op(ACT, lambda: nc.scalar.activation(out=sd[:], in_=ss[:], func=AF.Ln, bias=ph_bufs["epsc"][:], scale=1.0 / D), r=["ss"], w=["sd"])
        T.op(ACT, lambda: nc.scalar.activation(out=ph_bufs["rs"][:], in_=sd[:], func=AF.Exp, scale=-0.5), r=["sd"], w=["rs"])

    with ExitStack() as ph:
        def sb(name, shape, dt):
            return ph.enter_context(nc.sbuf_tensor("s_" + name, list(shape), dt))
        xt = [sb(f"xt{i}", [128, D], F32) for i in range(2)]
        xh = [sb(f"xh{i}", [128, D], BF16) for i in range(2)]
        hTb = [sb(f"hTb{i}", [128, 8, 512], BF16) for i in range(2)]
        bufs = dict(ss=sb("ss", [128, 1], F32), sd=sb("sd", [128, 1], F32), rs=sb("rs", [128, 1], F32),
                    junk=sb("junk", [128, D], BF16), epsc=sb("epsc", [128, 1], F32))
        T.op(DVE, lambda: nc.vector.memset(bufs["epsc"][:], EPS), w=["epsc"])
        pass
        for t in range(int(os.environ.get('K1_TILES', NT))):
            b = t % 2
            blk, tl = t // 4, t % 4
            hb = blk % 2
            T.dma(SP, xt[b][:], x_in[t * 128:(t + 1) * 128, :], w=[("xt", b)])
            KO = int(os.environ.get('K1_OPS', 9))
            if KO < 1:
                continue
            norm_tile(bufs, xt[b][:], [("xt", b), "epsc"], "n1")
            if KO < 2:
                continue
            T.op(DVE, lambda b=b: nc.vector.tensor_scalar(out=xh[b][:], in0=xt[b][:], scalar1=bufs["rs"][:], scalar2=None, op0=ALU.mult),
                 r=[("xt", b), "rs"], w=[("xh", b)])
            if KO < 3:
                continue
            pb = t % 2
            pv = psb(pb).rearrange("p (k t) -> p k t", k=8)
            for kc in range(8):
                T.op(PE, lambda kc=kc, b=b, pv=pv: nc.tensor.transpose(pv[:, kc, :], xh[b][:, kc * 128:(kc + 1) * 128], ident_b),
                     r=[("xh", b), "cstb"], w=[("ps", pb)])
            if KO < 4:
                continue
            for kc in range(8):
                T.op(ACT, lambda kc=kc, pv=pv, hb=hb, tl=tl: nc.scalar.activation(
                    out=hTb[hb][:, kc, tl * 128:(tl + 1) * 128], in_=pv[:, kc, :], func=AF.Identity,
                    **({} if os.environ.get('K1_NOAP') == '1' else ({'scale': a1[:, kc:kc + 1]} if os.environ.get('K1_NOAP') == '2' else
                        ({'bias': s1[:, kc:kc + 1]} if os.environ.get('K1_NOAP') == '3' else dict(scale=a1[:, kc:kc + 1], bias=s1[:, kc:kc + 1]))))),
                    r=[("ps", pb), "a1", "modc"], w=[("hTb", hb, tl)])
            if tl == 3:
                T.dma(SP, xnT_v[:, :, blk * 512:(blk + 1) * 512], hTb[hb][:], r=[("hTb", hb)], w=[("xnT", blk)])
        T.barrier()
    early.close()
    early_open[0] = False
    if dbg and "xnT" in dbg_out:
        dump_dram(dbg_out["xnT"], xnT_d, 1024, S, BF16)
    if stage <= 1:
        return finish()

    scope_mark("phA")
    persist = ExitStack()

    def sbp(name, shape, dt):
        return persist.enter_context(nc.sbuf_tensor("s_" + name, list(shape), dt))

    with ExitStack() as ph:
        def sb(name, shape, dt):
            return ph.enter_context(nc.sbuf_tensor("s_" + name, list(shape), dt))
        wqkv = sb("wqkv", [128, 8, 1536], BF16)
        wgt = sb("wgt", [128, 8, 8], BF16)
        bg = sb("bg", [4, 2], F32)
        nbf = sb("nbf", [4, 1], F32)
        epsc = sb("epscA", [128, 1], F32)
        gst = [[sb(f"gst{g}_{i}", [4, 512], F32) for i in range(2)] for g in range(2)]
        kT = sb("kT", [128, 4, S], BF16)
        v1 = sb("v1", [128, NT, 4, 128], BF16)
        xb = [sb(f"xbA{i}", [128, 8, 512], BF16) for i in range(2)]
        qT = sb("qT", [128, 4, 512], BF16)
        hdTb = sb("hdTb", [128, 4, 512], BF16)
        Pb = [[sb(f"P{m}_{i}", [128, 512], BF16) for i in range(2)] for m in range(2)]
        sqb = [sb(f"sqb{i}", [128, 512], BF16) for i in range(2)]
        qsf = [sb(f"qsf{i}", [128, 512], F32) for i in range(2)]
        sdf = [sb(f"sdf{i}", [128, 512], F32) for i in range(2)]
        qnb = [sb(f"qnb{i}", [128, 512], BF16) for i in range(2)]
        t1f = [sb(f"t1f{i}", [128, 512], F32) for i in range(2)]
        t2f = [sb(f"t2f{i}", [128, 512], F32) for i in range(2)]
        fo = [sb(f"fo{i}", [128, 512], F32) for i in range(4)]
        OL = [qsf[0], qsf[1], sdf[0], sdf[1]]
        OLK = [("qsf", 0), ("qsf", 1), ("sdf", 0), ("sdf", 1)]
        sqf = sqb[0]
        pend_fin = []

        T.op(DVE, lambda: nc.vector.memset(epsc[:], EPS), w=["epscA"])
        T.dma(POOL, wqkv[:, 0:4, :], w_in_v[:, 0:4, 2056:3592], w=[("wqkv", 0)])
        T.dma(POOL, wqkv[:, 4:8, :], w_in_v[:, 4:8, 2056:3592], w=[("wqkv", 1)])
        T.dma(POOL, wgt[:], w_in_v[:, :, 2048:2056], w=["wgt"])
        T.dma(SP, bg[:], bgate_in[:, :], w=["bg"])
        T.op(DVE, lambda: nc.vector.tensor_scalar(out=nbf[:], in0=bg[:, 1:2], scalar1=-1.0, scalar2=None, op0=ALU.mult), r=["bg"], w=["nbf"])

        negc = smallc[:, 3:4]
        neglam = smallc[:, 4:5]
        subw8 = smallc[:, 2:3]

        for blk in range(NB):
            xbb = blk % 2
            bs = slice(blk * 512, (blk + 1) * 512)
            T.dma(SP, xb[xbb][:], xnT_v[:, :, bs], w=[("xbA", xbb)])
            def emit_v(tl):
                t = blk * 4 + tl
                bk = 4 + tl
                for kc in range(8):
                    T.op(PE, lambda kc=kc: nc.tensor.matmul(
                        PS[bk][:, :], lhsT=xb[xbb][:, kc, tl * 128:(tl + 1) * 128], rhs=wqkv[:, kc, 1024:1536], start=(kc == 0), stop=(kc == 7)),
                        r=[("wqkv",), ("xbA", xbb)], w=[("ps", bk)])
                T.op(ACT, lambda: nc.scalar.copy(out=v1[:, t, :, :].rearrange("p h d -> p (h d)"), in_=PS[bk][:, :]),
                     r=[("ps", bk)], w=[("v1", t)])

            def emit_gate(gi):
                bk = 4 + gi
                for kc in range(8):
                    T.op(PE, lambda kc=kc: nc.tensor.matmul(
                        PS[bk][0:4, :], lhsT=wgt[:, kc, 4 * gi:4 * gi + 4], rhs=xb[xbb][:, kc, :], start=(kc == 0), stop=(kc == 7)),
                        r=["wgt", ("xbA", xbb)], w=[("ps", bk)])
                if gi == 0:
                    T.op(ACT, lambda: nc.scalar.activation(out=gst[0][xbb][:], in_=PS[bk][0:4, :], func=AF.Identity, bias=bg[:, 0:1]),
                         r=[("ps", bk), "bg"], w=[("gst", 0, xbb)])
                    T.dma(SP, gi_d[:, bs], gst[0][xbb][:], r=[("gst", 0, xbb)], w=[("gi_d", blk)])
                else:
                    T.op(ACT, lambda: nc.scalar.activation(out=gst[1][xbb][:], in_=PS[bk][0:4, :], func=AF.Exp, bias=nbf[:], scale=-1.0),
                         r=[("ps", bk), "nbf"], w=[("gst", 1, xbb)])
                    T.dma(SP, gf_d[:, bs], gst[1][xbb][:], r=[("gst", 1, xbb)], w=[("gf_d", blk)])

            for c in range(8):
                st = c % 2
                isq = c < 4
                hh = c % 4
                col0 = (0 if isq else 512) + hh * 128
                for kc in range(8):
                    T.op(PE, lambda kc=kc, col0=col0, st=st: nc.tensor.matmul(
                        PS[st][:, :], lhsT=wqkv[:, kc, col0:col0 + 128], rhs=xb[xbb][:, kc, :], start=(kc == 0), stop=(kc == 7)),
                        r=[("wqkv",), ("xbA", xbb)], w=[("ps", st)])
                wcol = smallc[:, 0:1] if isq else smallc[:, 1:2]
                T.op(ACT, lambda st=st: nc.scalar.activation(out=sqb[st][:], in_=PS[st][:, :], func=AF.Square), r=[("ps", st)], w=[("sqb", st)])
                T.op(ACT, lambda st=st, wcol=wcol: nc.scalar.activation(out=qsf[st][:], in_=PS[st][:, :], func=AF.Identity, scale=wcol),
                     r=[("ps", st), "sc0", "sc1"], w=[("qsf", st)])
                if c < 4:
                    emit_v(c)
                elif c < 6:
                    emit_gate(c - 4)
                T.op(PE, lambda st=st: nc.tensor.matmul(PS[2 + st][:, :], lhsT=bo64_b, rhs=sqb[st][:], start=True, stop=True),
                     r=[("sqb", st), "cstb"], w=[("ps", 2 + st)])
                T.op(ACT, lambda st=st: nc.scalar.activation(out=sdf[st][:], in_=PS[2 + st][:, :], func=AF.Ln, bias=epsc[:], scale=1.0 / 64),
                     r=[("ps", 2 + st), "epscA"], w=[("sdf", st)])
                T.op(ACT, lambda st=st: nc.scalar.activation(out=sdf[st][:], in_=sdf[st][:], func=AF.Exp, scale=-0.5),
                     r=[("sdf", st)], w=[("sdf", st)])
                T.op(DVE, lambda st=st: nc.vector.tensor_tensor(out=qnb[st][:], in0=qsf[st][:], in1=sdf[st][:], op=ALU.mult),
                     r=[("qsf", st), ("sdf", st)], w=[("qnb", st)])
                T.op(PE, lambda st=st: nc.tensor.matmul(PS[2 + st][:, :], lhsT=RT_b, rhs=qnb[st][:], start=True, stop=True),
                     r=[("qnb", st), "cstb"], w=[("ps", 2 + st)])
                T.op(POOL, lambda st=st: nc.gpsimd.tensor_tensor(out=t1f[st][:], in0=qnb[st][:], in1=CosB[:, bs], op=ALU.mult),
                     r=[("qnb", st), "CosB"], w=[("t1f", st)])
                T.op(DVE, lambda st=st: nc.vector.tensor_tensor(out=t2f[st][:], in0=PS[2 + st][:, :], in1=SinB[:, bs], op=ALU.mult),
                     r=[("ps", 2 + st), "SinB"], w=[("t2f", st)])
                if isq:
                    dst, dk = qT[:, hh, :], ("qT", hh)
                else:
                    dst, dk = kT[:, hh, bs], ("kT", hh, blk)
                T.op(POOL, lambda st=st, dst=dst: nc.gpsimd.tensor_tensor(out=dst, in0=t1f[st][:], in1=t2f[st][:], op=ALU.add),
                     r=[("t1f", st), ("t2f", st)], w=[dk])
            for hh in range(4):
                nkt = blk * 4 + 4
                prev = None

                def pv_step(kt, c0, pbuf):
                    first = (kt == 0)
                    last = (kt == nkt - 1)
                    for m in range(2):
                        T.op(PE, lambda m=m: nc.tensor.matmul(
                            PS[4 + 2 * m][:, c0:512], lhsT=v1[:, kt, hh, :], rhs=Pb[m][pbuf][:, c0:512], start=first, stop=last,
                            skip_group_check=True), r=[("v1", kt), ("P", m, pbuf)], w=[("ps", 4 + 2 * m)])
                        T.op(PE, lambda m=m: nc.tensor.matmul(
                            PS[5 + 2 * m][:, c0:512], lhsT=ones_b, rhs=Pb[m][pbuf][:, c0:512], start=first, stop=last,
                            skip_group_check=True), r=["cstb", ("P", m, pbuf)], w=[("ps", 5 + 2 * m)])

                for kt in range(nkt):
                    ktl = kt - blk * 4
                    c0 = ktl * 128 if ktl > 0 else 0
                    pbuf = kt % 2
                    sbk = 2 * (kt % 2)
                    for m in range(2):
                        T.op(PE, lambda m=m, kt=kt, c0=c0, sbk=sbk: nc.tensor.matmul(
                            PS[sbk + m][:, c0:512], lhsT=kT[64 * m:64 * m + 64, hh, kt * 128:(kt + 1) * 128],
                            rhs=qT[64 * m:64 * m + 64, hh, c0:512], start=True, stop=True),
                            r=[("kT", hh, kt // 4), ("qT", hh)], w=[("ps", sbk + m)])
                    if prev is not None:
                        pv_step(*prev)
                    for m in range(2):
                        T.op(ACT, lambda m=m, c0=c0, pbuf=pbuf, sbk=sbk: nc.scalar.activation(
                            out=Pb[m][pbuf][:, c0:512], in_=PS[sbk + m][:, c0:512], func=AF.Exp, bias=negc, scale=0.125),
                            r=[("ps", sbk + m), "sc3"], w=[("P", m, pbuf)])
                        if ktl >= 0:
                            T.op(POOL, lambda m=m, c0=c0, pbuf=pbuf: nc.gpsimd.affine_select(
                                out=Pb[m][pbuf][:, c0:c0 + 128], in_=Pb[m][pbuf][:, c0:c0 + 128], pattern=[[1, 128]],
                                compare_op=ALU.is_ge, fill=0.0, base=0, channel_multiplier=-1),
                                r=[("P", m, pbuf)], w=[("P", m, pbuf)])
                    prev = (kt, c0, pbuf)
                    if pend_fin:
                        T.op(*pend_fin.pop(0))
                pv_step(*prev)
                for rec_ in pend_fin:
                    T.op(*rec_)
                pend_fin.clear()

                def finalize_ops(hh):
                    T.op(ACT, lambda: nc.scalar.copy(out=OL[0][:], in_=PS[4][:, :]), r=[("ps", 4)], w=[OLK[0]])
                    T.op(DVE, lambda: nc.vector.tensor_copy(out=OL[1][:], in_=PS[5][:, :]), r=[("ps", 5)], w=[OLK[1]])
                    T.op(ACT, lambda: nc.scalar.copy(out=OL[2][:], in_=PS[6][:, :]), r=[("ps", 6)], w=[OLK[2]])
                    T.op(DVE, lambda: nc.vector.tensor_copy(out=OL[3][:], in_=PS[7][:, :]), r=[("ps", 7)], w=[OLK[3]])
                    T.start_record()
                    T.op(ACT, lambda: nc.scalar.activation(out=fo[0][:], in_=OL[1][:], func=AF.Ln), r=[OLK[1]], w=[("fo", 0)])
                    T.op(ACT, lambda: nc.scalar.activation(out=fo[0][:], in_=fo[0][:], func=AF.Exp, scale=-1.0), r=[("fo", 0)], w=[("fo", 0)])
                    T.op(DVE, lambda: nc.vector.tensor_tensor(out=fo[1][:], in0=OL[0][:], in1=fo[0][:], op=ALU.mult),
                         r=[OLK[0], ("fo", 0)], w=[("fo", 1)])
                    T.op(ACT, lambda: nc.scalar.activation(out=fo[2][:], in_=OL[3][:], func=AF.Ln), r=[OLK[3]], w=[("fo", 2)])
                    T.op(ACT, lambda: nc.scalar.activation(out=fo[0][:], in_=fo[2][:], func=AF.Exp, scale=-1.0), r=[("fo", 2), ("fo", 0)], w=[("fo", 0)])
                    T.op(DVE, lambda: nc.vector.tensor_tensor(out=fo[2][:], in0=OL[2][:], in1=fo[0][:], op=ALU.mult),
                         r=[OLK[2], ("fo", 0)], w=[("fo", 2)])
                    T.op(DVE, lambda: nc.vector.scalar_tensor_tensor(out=fo[3][:], in0=fo[2][:], scalar=neglam, in1=fo[1][:], op0=ALU.mult, op1=ALU.add),
                         r=[("fo", 1), ("fo", 2), "sc4"], w=[("fo", 3)])
                    T.op(ACT, lambda: nc.scalar.activation(out=sqf[:], in_=fo[3][:], func=AF.Square), r=[("fo", 3)], w=[("sqb", 0)])
                    T.op(PE, lambda: nc.tensor.matmul(PS[0][:, :], lhsT=ones_b, rhs=sqf[:], start=True, stop=True),
                         r=[("sqb", 0), "cstb"], w=[("ps", 0)])
                    T.op(ACT, lambda: nc.scalar.activation(out=fo[0][:], in_=PS[0][:, :], func=AF.Ln, bias=epsc[:], scale=1.0 / 128),
                         r=[("ps", 0), "epscA"], w=[("fo", 0)])
                    T.op(ACT, lambda: nc.scalar.activation(out=fo[0][:], in_=fo[0][:], func=AF.Exp, scale=-0.5), r=[("fo", 0)], w=[("fo", 0)])
                    T.op(DVE, lambda: nc.vector.scalar_tensor_tensor(out=hdTb[:, hh, :], in0=fo[3][:], scalar=subw8, in1=fo[0][:], op0=ALU.mult, op1=ALU.mult),
                         r=[("fo", 3), ("fo", 0), "sc2"], w=[("hdTb", hh)])
                    return T.stop_record()

                pend_fin.extend(finalize_ops(hh))
            for rec_ in pend_fin:
                T.op(*rec_)
            pend_fin.clear()
            T.dma(SP, hdT_v[:, :, bs], hdTb[:], r=[("hdTb",)], w=[("hdT", blk)])
        T.barrier()
    early2.close()
    early2_open[0] = False
    if dbg and "hdT" in dbg_out:
        dump_dram(dbg_out["hdT"], hdT_d, 512, S, BF16)
        dump_dram(dbg_out["irow"], gi_d, 4, S, F32)
        dump_dram(dbg_out["frow"], gf_d, 4, S, F32)
    if stage <= 2:
        persist.close()
        return finish()

    scope_mark("phB")
    wcol = sbp("wcol", [128, NT, 8], F32)
    decb = sbp("decb", [128, 128], F32)
    with ExitStack() as ph:
        def sb(name, shape, dt):
            return ph.enter_context(nc.sbuf_tensor("s_" + name, list(shape), dt))
        irow = sb("irow", [4, S], F32)
        frow = sb("frow", [4, S], F32)
        T.dma(SP, irow[:], gi_d[:, :], w=[("irow",)])
        T.dma(SP, frow[:], gf_d[:, :], w=[("frow",)])
        onesr = sb("onesr", [4, S], F32)
        csr = sb("csr", [4, S], F32)
        nbr = sb("nbr", [4, S], F32)
        gr = sb("gr", [4, S], F32)
        pe_ = sb("pe_", [4, 33], F32)
        G = sb("G", [4, 32], F32)
        Ms = sb("Ms", [4, 32], F32)
        mp = sb("mp", [4, 33], F32)
        dec = sb("dec", [4, 32], F32)
        T.op(ACT, lambda: nc.scalar.activation(out=frow[:], in_=frow[:], func=AF.Ln, bias=1.0), r=[("frow",)], w=[("frow",)])
        T.op(DVE, lambda: nc.vector.memset(onesr[:], 1.0), w=["onesr"])
        T.op(DVE, lambda: nc.vector.tensor_tensor_scan(out=csr[:], data0=onesr[:], data1=frow[:], initial=0.0, op0=ALU.mult, op1=ALU.add),
             r=["onesr", ("frow",)], w=["csr"])
        T.op(DVE, lambda: nc.vector.memset(pe_[:, 0:1], 0.0), w=[("pe_", 0)])
        T.op(DVE, lambda: nc.vector.tensor_copy(out=pe_[:, 1:33], in_=csr[:].rearrange("p (j s) -> p j s", s=128)[:, :, 127]),
             r=["csr"], w=[("pe_", 1)])
        T.op(DVE, lambda: nc.vector.tensor_tensor(out=nbr[:].rearrange("p (j s) -> p j s", s=128), in0=csr[:].rearrange("p (j s) -> p j s", s=128),
                                                  in1=pe_[:, 0:32].unsqueeze(2).to_broadcast([4, 32, 128]), op=ALU.subtract),
             r=["csr", ("pe_",)], w=["nbr"])
        T.op(DVE, lambda: nc.vector.tensor_tensor(out=gr[:], in0=irow[:], in1=nbr[:], op=ALU.add), r=[("irow",), "nbr"], w=["gr"])
        T.op(DVE, lambda: nc.vector.tensor_reduce(out=G[:], in_=gr[:].rearrange("p (j s) -> p j s", s=128), axis=AX.X, op=ALU.max),
             r=["gr"], w=["G"])
        T.op(DVE, lambda: nc.vector.memset(mp[:, 0:1], 0.0), w=[("mp", 0)])
        nbl = nbr[:].rearrange("p (j s) -> p j s", s=128)[:, :, 127]
        for j in range(NT):
            T.op(DVE, lambda j=j: nc.vector.tensor_tensor(out=Ms[:, j:j + 1], in0=mp[:, j:j + 1], in1=G[:, j:j + 1], op=ALU.max),
                 r=[("mp", j), "G"], w=[("Ms", j)])
            T.op(DVE, lambda j=j: nc.vector.tensor_tensor(out=mp[:, j + 1:j + 2], in0=Ms[:, j:j + 1], in1=nbl[:, j:j + 1], op=ALU.subtract),
                 r=[("Ms", j), "nbr"], w=[("mp", j + 1)])
        Msb = Ms[:].unsqueeze(2).to_broadcast([4, 32, 128])
        T.op(DVE, lambda: nc.vector.tensor_tensor(out=gr[:].rearrange("p (j s) -> p j s", s=128), in0=gr[:].rearrange("p (j s) -> p j s", s=128),
                                                  in1=Msb, op=ALU.subtract), r=["gr", ("Ms",)], w=["gr"])
        T.op(DVE, lambda: nc.vector.tensor_tensor(out=nbr[:].rearrange("p (j s) -> p j s", s=128), in0=nbr[:].rearrange("p (j s) -> p j s", s=128),
                                                  in1=Msb, op=ALU.subtract), r=["nbr", ("Ms",)], w=["nbr"])
        T.op(ACT, lambda: nc.scalar.activation(out=gr[:], in_=gr[:], func=AF.Exp), r=["gr"], w=["gr"])
        T.op(ACT, lambda: nc.scalar.activation(out=nbr[:], in_=nbr[:], func=AF.Exp), r=["nbr"], w=["nbr"])
        T.op(DVE, lambda: nc.vector.tensor_scalar(out=gr[:], in0=gr[:], scalar1=128.0 ** -0.5, scalar2=None, op0=ALU.mult), r=["gr"], w=["gr"])
        T.op(DVE, lambda: nc.vector.tensor_tensor(out=dec[:], in0=mp[:, 0:32], in1=Ms[:], op=ALU.subtract), r=[("mp",), ("Ms",)], w=["dec"])
        T.op(ACT, lambda: nc.scalar.activation(out=dec[:], in_=dec[:], func=AF.Exp), r=["dec"], w=["dec"])
        T.dma(SP, dec_d[:, :], dec[:], r=["dec"], w=["dec_d"])
        T.dma(SP, decb[:], dec_d.rearrange("h j -> (h j)").unsqueeze(0).partition_broadcast(128), r=["dec_d"], w=["decb"])
        wv = PS[0][:, 0:256].rearrange("p (t e) -> p t e", e=8)
        for t in range(NT):
            T.op(PE, lambda t=t: nc.tensor.transpose(wv[:, t, 0:4], gr[:, t * 128:(t + 1) * 128], ident_f[0:4, 0:4]),
                 r=["gr", "cst"], w=[("ps", 0, t, 0)])
            T.op(PE, lambda t=t: nc.tensor.transpose(wv[:, t, 4:8], nbr[:, t * 128:(t + 1) * 128], ident_f[0:4, 0:4]),
                 r=["nbr", "cst"], w=[("ps", 0, t, 1)])
        T.op(DVE, lambda: nc.vector.tensor_copy(out=wcol[:], in_=wv), r=[("ps", 0)], w=["wcol"])
        T.barrier()
    if dbg and "wcol" in dbg_out:
        T.dma(SP, dbg_out["wcol"][:, :], wcol[:].rearrange("p t e -> p (t e)"), r=["wcol"])
        T.dma(SP, dbg_out["decb"][:, :], decb[:], r=["decb"])
    if stage <= 3:
        persist.close()
        return finish()

    scope_mark("phC")
    with ExitStack() as ph:
        def sb(name, shape, dt):
            return ph.enter_context(nc.sbuf_tensor("s_" + name, list(shape), dt))
        wm = sb("wm", [128, 8, 2048], BF16)
        cw = sb("cw", [128, 8, 4], F32)
        cbias = sb("cbias", [128, 8], F32)
        dg = sb("dg", [128, 8, 4, 128], BF16)
        mnwb = sb("mnwb", [128, 512], F32)
        epsc = sb("epscC", [128, 1], F32)
        xb = [sb(f"xbC{i}", [128, 8, 512], BF16) for i in range(2)]
        pre = sb("pre", [128, 8, 516], BF16)
        qkc = sb("qkc", [128, 8, 512], BF16)
        vm1 = [sb(f"vm1_{i}", [128, 4, 130], BF16) for i in range(2)]
        so = [sb(f"so{i}", [128, 512], F32) for i in range(3)]
        pend_tail = []
        ST = [sb(f"ST{i}", [128, 128], BF16) for i in range(4)]
        kw = [sb(f"kw{i}", [128, 128], BF16) for i in range(4)]
        Cst = sb("Cst", [128, 4, 130], F32)
        Cd = sb("Cd", [128, 4, 130], BF16)
        hbuf = sb("hbuf", [128, 512], F32)
        sqh = sb("sqh", [128, 512], F32)
        hmb = sb("hmb", [128, 512], BF16)
        hmTb = [sb(f"hmTb{i}", [128, 4, 512], BF16) for i in range(2)]
        dn = sb("dn", [128, 8], F32)
        ssh = sb("ssh", [128, 8], F32)

        T.op(DVE, lambda: nc.vector.memset(epsc[:], EPS), w=["epscC"])
        T.dma(POOL, wm[:, 0:4, :], w_in_v[:, 0:4, 0:2048], w=[("wm", 0)], max_dma_last_dim=8192)
        T.dma(POOL, wm[:, 4:8, :], w_in_v[:, 4:8, 0:2048], w=[("wm", 1)], max_dma_last_dim=8192)
        T.dma(SP, cw[:], convw_in[:, :, :], w=["cw"])
        T.dma(SP, cbias[:], convb_in[:, :], w=["cbias"])
        T.dma(SP, mnwb[:], mnw_in[0:1, :].partition_broadcast(128), w=["mnwb"])
        for c in range(8):
            for j in range(4):
                T.op(DVE, lambda c=c, j=j: nc.vector.tensor_scalar(out=dg[:, c, j, :], in0=ident_f, scalar1=cw[:, c, j:j + 1], scalar2=None, op0=ALU.mult),
                     r=["cw", "cst"], w=[("dg", c, j)])
        T.op(DVE, lambda: nc.vector.memset(pre[:, :, 0:4], 0.0), w=[("pre", "halo")])
        T.op(DVE, lambda: nc.vector.memset(Cst[:], 0.0), w=["Cst"])
        T.op(DVE, lambda: nc.vector.memset(Cd[:], 0.0), w=["Cd"])
        for i in range(2):
            T.op(DVE, lambda i=i: nc.vector.memset(vm1[i][:, :, 128:130], 1.0), w=[("vm1", i)])

        for blk in range(NB):
            xbb = blk % 2
            bs = slice(blk * 512, (blk + 1) * 512)
            T.dma(SP, xb[xbb][:], xnT_v[:, :, bs], w=[("xbC", xbb)])
            for c in range(8):
                pb = c % 2
                for kc in range(8):
                    T.op(PE, lambda kc=kc, c=c, pb=pb: nc.tensor.matmul(
                        PS[pb][:, :], lhsT=wm[:, kc, c * 128:(c + 1) * 128], rhs=xb[xbb][:, kc, :], start=(kc == 0), stop=(kc == 7)),
                        r=[("wm",), ("xbC", xbb)], w=[("ps", pb)])
                if blk > 0:
                    T.op(DVE, lambda c=c: nc.vector.tensor_copy(out=pre[:, c, 1:4], in_=pre[:, c, 513:516]),
                         r=[("pre", c)], w=[("pre", "halo", c)])
                T.op(ACT, lambda c=c, pb=pb: nc.scalar.copy(out=pre[:, c, 4:516], in_=PS[pb][:, :]),
                     r=[("ps", pb), ("pre", "halo", c)], w=[("pre", c)])
                cb2 = 2 + c % 2
                for j in range(4):
                    T.op(PE, lambda c=c, j=j, cb2=cb2: nc.tensor.matmul(
                        PS[cb2][:, :], lhsT=dg[:, c, j, :], rhs=pre[:, c, 1 + j:513 + j], start=(j == 0), stop=(j == 3)),
                        r=[("dg", c), ("pre", c), ("pre", "halo", c)], w=[("ps", cb2)])
                T.op(ACT, lambda c=c, cb2=cb2: nc.scalar.activation(out=qkc[:, c, :], in_=PS[cb2][:, :], func=AF.Silu, bias=cbias[:, c:c + 1]),
                     r=[("ps", cb2), "cbias"], w=[("qkc", c)])
            for tl in range(4):
                t = blk * 4 + tl
                vb = t % 2
                ts_ = slice(tl * 128, (tl + 1) * 128)
                def emit_vo(tl_):
                    t_ = blk * 4 + tl_
                    vb_ = t_ % 2
                    sl_ = slice(tl_ * 128, (tl_ + 1) * 128)
                    for half in range(2):
                        for kc in range(8):
                            T.op(PE, lambda kc=kc, half=half: nc.tensor.matmul(
                                PS[4 + half][:, :], lhsT=xb[xbb][:, kc, sl_], rhs=wm[:, kc, 1024 + half * 512:1536 + half * 512],
                                start=(kc == 0), stop=(kc == 7)), r=[("wm",), ("xbC", xbb)], w=[("ps", 4 + half)])
                    T.op(ACT, lambda: nc.scalar.copy(out=vm1[vb_][:, :, 0:128], in_=PS[4][:, :].rearrange("p (h d) -> p h d", h=4)),
                         r=[("ps", 4)], w=[("vm1", vb_)])
                    T.op(ACT, lambda: nc.scalar.activation(out=so[t_ % 3][:], in_=PS[5][:, :], func=AF.Sigmoid), r=[("ps", 5)], w=[("so", t_ % 3)])

                if tl == 0:
                    emit_vo(0)
                if tl < 3:
                    emit_vo(tl + 1)
                def head_ops(hh):
                    wc = wcol[:, t, hh:hh + 1]
                    cc = wcol[:, t, 4 + hh:5 + hh]
                    dcol = decb[:, hh * 32 + t:hh * 32 + t + 1]
                    if hh % 2 == 0:
                        bA, bK, bU, bN = 6, 1, 7, 0
                    else:
                        bA, bK, bU, bN = 2, 3, 4, 5
                    kps = psb(bK)[:, 0:128]
                    ops = []
                    ops.append(lambda: T.op(PE, lambda: nc.tensor.matmul(PS[bA][:, 0:128], lhsT=qkc[:, 4 + hh, ts_], rhs=qkc[:, hh, ts_], start=True, stop=True),
                                            r=[("qkc", 4 + hh), ("qkc", hh)], w=[("ps", bA)]))
                    ops.append(lambda: T.op(DVE, lambda: nc.vector.scalar_tensor_tensor(
                        out=ST[hh][:], in0=PS[bA][:, 0:128], scalar=wc, in1=tri_f, op0=ALU.mult, op1=ALU.mult),
                        r=[("ps", bA), "wcol", "cst"], w=[("ST", hh)]))
                    ops.append(lambda: T.op(PE, lambda: nc.tensor.transpose(kps, qkc[:, 4 + hh, ts_], ident_b),
                                            r=[("qkc", 4 + hh), "cstb"], w=[("ps", bK)]))
                    ops.append(lambda: T.op(DVE, lambda: nc.vector.tensor_scalar(out=kw[hh][:], in0=kps, scalar1=wc, scalar2=None, op0=ALU.mult),
                                            r=[("ps", bK), "wcol"], w=[("kw", hh)]))
                    ops.append(lambda: T.op(ACT, lambda: nc.scalar.activation(out=Cd[:, hh, :], in_=Cst[:, hh, :], func=AF.Identity, scale=dcol),
                                            r=[("Cst", hh), "decb"], w=[("Cd", hh)]))
                    ops.append(lambda: T.op(PE, lambda: nc.tensor.matmul(PS[bU][:, 0:129], lhsT=kw[hh][:], rhs=vm1[vb][:, hh, 0:129], start=True, stop=True),
                                            r=[("kw", hh), ("vm1", vb)], w=[("ps", bU)]))
                    ops.append(lambda: T.op(PE, lambda: nc.tensor.matmul(PS[bN][:, 0:129], lhsT=ST[hh][:], rhs=vm1[vb][:, hh, 0:129], start=True, stop=False),
                                            r=[("ST", hh), ("vm1", vb)], w=[("ps", bN)]))
                    ops.append(lambda: T.op(PE, lambda: nc.tensor.matmul(PS[bN][:, 0:129], lhsT=qkc[:, hh, ts_], rhs=Cd[:, hh, 0:129], start=False, stop=True),
                                            r=[("qkc", hh), ("Cd", hh)], w=[("ps", bN)]))
                    ops.append(lambda: T.op(DVE, lambda: nc.vector.scalar_tensor_tensor(
                        out=Cst[:, hh, 0:129], in0=Cst[:, hh, 0:129], scalar=dcol, in1=PS[bU][:, 0:129], op0=ALU.mult, op1=ALU.add),
                        r=[("Cst", hh), "decb", ("ps", bU)], w=[("Cst", hh)]))
                    ops.append(lambda: T.op(DVE, lambda: nc.vector.tensor_reduce(
                        out=dn[:, hh:hh + 1], in_=PS[bN][:, 128:129], axis=AX.X, op=ALU.max, apply_absolute_value=True),
                        r=[("ps", bN)], w=[("dn", hh)]))
                    ops.append(lambda: T.op(DVE, lambda: nc.vector.tensor_scalar(
                        out=dn[:, hh:hh + 1], in0=dn[:, hh:hh + 1], scalar1=cc, scalar2=None, op0=ALU.max),
                        r=[("dn", hh), "wcol"], w=[("dn", hh)]))
                    ops.append(lambda: T.op(DVE, lambda: nc.vector.reciprocal(out=dn[:, 4 + hh:5 + hh], in_=dn[:, hh:hh + 1]), r=[("dn", hh)], w=[("dn", 4 + hh)]))
                    ops.append(lambda: T.op(ACT, lambda: nc.scalar.activation(out=hbuf[:, hh * 128:(hh + 1) * 128], in_=PS[bN][:, 0:128], func=AF.Copy,
                                                                              scale=dn[:, 4 + hh:5 + hh]),
                                            r=[("ps", bN), ("dn", 4 + hh)], w=[("hbuf", hh)]))
                    return ops

                for pair in ((0, 1), (2, 3)):
                    pops = [head_ops(hh) for hh in pair]
                    for i in range(len(pops[0])):
                        for po in pops:
                            po[i]()
                        if pend_tail:
                            T.op(*pend_tail.pop(0))
                    for rec_ in pend_tail:
                        T.op(*rec_)
                    pend_tail.clear()
                T.start_record()
                T.op(DVE, lambda: nc.vector.tensor_tensor(out=sqh[:], in0=hbuf[:], in1=hbuf[:], op=ALU.mult), r=[("hbuf",)], w=["sqh"])
                T.op(DVE, lambda: nc.vector.tensor_reduce(out=ssh[:, 0:4], in_=sqh[:].rearrange("p (h d) -> p h d", h=4), axis=AX.X, op=ALU.add),
                     r=["sqh"], w=[("ssh", 0)])
                T.op(ACT, lambda: nc.scalar.activation(out=ssh[:, 4:8], in_=ssh[:, 0:4], func=AF.Sqrt, bias=epsc[:], scale=1.0 / 128),
                     r=[("ssh", 0), "epscC"], w=[("ssh", 1)])
                T.op(DVE, lambda: nc.vector.reciprocal(out=ssh[:, 4:8], in_=ssh[:, 4:8]), r=[("ssh", 1)], w=[("ssh", 1)])
                T.op(DVE, lambda: nc.vector.tensor_tensor(out=sqh[:].rearrange("p (h d) -> p h d", h=4), in0=hbuf[:].rearrange("p (h d) -> p h d", h=4),
                                                          in1=ssh[:, 4:8].unsqueeze(2).to_broadcast([128, 4, 128]), op=ALU.mult),
                     r=[("hbuf",), ("ssh", 1)], w=["sqh"])
                T.op(DVE, lambda: nc.vector.tensor_tensor(out=sqh[:], in0=sqh[:], in1=mnwb[:], op=ALU.mult), r=["sqh", "mnwb"], w=["sqh"])
                T.op(DVE, lambda t=t: nc.vector.tensor_tensor(out=hmb[:], in0=sqh[:], in1=so[t % 3][:], op=ALU.mult), r=["sqh", ("so", t % 3)], w=["hmb"])
                hb = blk % 2
                tp = psb(3).rearrange("p (k t) -> p k t", k=8)
                for hh in range(4):
                    T.op(PE, lambda hh=hh, tp=tp: nc.tensor.transpose(tp[:, hh, :], hmb[:, hh * 128:(hh + 1) * 128], ident_b),
                         r=["hmb", "cstb"], w=[("ps", 3)])
                T.op(ACT, lambda hb=hb, ts_=ts_, tp=tp: nc.scalar.copy(out=hmTb[hb][:, :, ts_], in_=tp[:, 0:4, :]), r=[("ps", 3)], w=[("hmTb", hb, tl)])
                pend_tail.extend(T.stop_record())
            for rec_ in pend_tail:
                T.op(*rec_)
            pend_tail.clear()
            T.dma(SP, hmT_v[:, :, bs], hmTb[blk % 2][:], r=[("hmTb", blk % 2)], w=[("hmT", blk)])
        T.barrier()
    persist.close()
    if dbg and "hmT" in dbg_out:
        dump_dram(dbg_out["hmT"], hmT_d, 512, S, BF16)
    if stage <= 4:
        return finish()

    scope_mark("phD")
    with ExitStack() as ph:
        def sb(name, shape, dt):
            return ph.enter_context(nc.sbuf_tensor("s_" + name, list(shape), dt))
        wg = sb("wg", [128, 8, 2048], BF16)
        wbm = sb("wbm", [128, 4, D], BF16)
        wbd = sb("wbd", [128, 4, D], BF16)
        wo = sb("wo", [128, 8, D], BF16)
        wstg = [sb(f"wstg{i}", [128, D], F32) for i in range(2)]
        wr = sb("wrt", [128, 8, 36], F32)
        brb = sb("brb", [128, 36], F32)
        xb = [sb(f"xbD{i}", [128, 8, 512], BF16) for i in range(2)]
        hmb_ = [sb(f"hmD{i}", [128, 4, 512], BF16) for i in range(2)]
        hdb_ = [sb(f"hdD{i}", [128, 4, 512], BF16) for i in range(2)]
        sg = [sb(f"sg{i}", [128, 512], F32) for i in range(4)]
        tt = [sb(f"tt{i}", [128, 512], F32) for i in range(4)]
        mT = sb("mT", [128, 8, 512], BF16)
        xt = [sb(f"xtD{i}", [128, D], F32) for i in range(2)]
        x1t = [sb(f"x1t{i}", [128, D], F32) for i in range(2)]
        xhf = sb("xhf", [128, D], F32)
        h2f = sb("h2f", [128, 8, 128], F32)
        x2t = [sb(f"x2t{i}", [128, D], BF16) for i in range(2)]
        ohb = sb("ohb", [128, 32], BF16)
        bufs = dict(ss=sb("ssD", [128, 1], F32), sd=sb("sdD", [128, 1], F32), rs=sb("rsD", [128, 1], F32),
                    junk=sb("junkD", [128, D], BF16), epsc=sb("epscD", [128, 1], F32))
        lg = sb("lg", [128, 36], F32)
        r8 = sb("r8", [128, 80], F32)

        T.op(DVE, lambda: nc.vector.memset(bufs["epsc"][:], EPS), w=["epsc"])
        T.op(DVE, lambda: nc.vector.memset(tot[:], 0.0), w=["tot"])
        T.dma(POOL, wg[:, 0:4, :], w_in_v[:, 0:4, 3592:5640], w=[("wg", 0)], max_dma_last_dim=8192)
        T.dma(POOL, wg[:, 4:8, :], w_in_v[:, 4:8, 3592:5640], w=[("wg", 1)], max_dma_last_dim=8192)
        T.dma(POOL, wbm[:], w_br_m.rearrange("(k p) n -> p k n", p=128), w=["wbm"])
        T.dma(POOL, wbd[:], w_br_d.rearrange("(k p) n -> p k n", p=128), w=["wbd"])
        T.dma(SP, wr[:], wr_in.rearrange("(k p) n -> p k n", p=128), w=["wrt"])
        T.dma(SP, brb[:], br_in[0:1, :].partition_broadcast(128), w=["brb"])
        w_out_v = w_out.rearrange("(k p) n -> p k n", p=128)
        for kc in range(8):
            b = kc % 2
            T.dma(SP, wstg[b][:], w_out_v[:, kc, :], w=[("wstg", b)])
            T.op(DVE, lambda kc=kc, b=b: nc.vector.tensor_tensor(out=wo[:, kc, :], in0=wstg[b][:], in1=g1b[:], op=ALU.mult),
                 r=[("wstg", b), "g1b"], w=[("wo", kc)])

        for blk in range(NB):
            xbb = blk % 2
            bs = slice(blk * 512, (blk + 1) * 512)
            def load_blk(bk):
                bb_ = bk % 2
                sl_ = slice(bk * 512, (bk + 1) * 512)
                T.dma(SP, xb[bb_][:], xnT_v[:, :, sl_], w=[("xbD", bb_)])
                T.dma(SP, hmb_[bb_][:], hmT_v[:, :, sl_], w=[("hmD", bb_)])
                T.dma(SP, hdb_[bb_][:], hdT_v[:, :, sl_], w=[("hdD", bb_)])

            if blk == 0:
                load_blk(0)
                T.dma(SP, xt[0][:], x_in[0:128, :], w=[("xtD", 0)])
            for oc in range(8):
                st = oc % 2
                for which, (colbase, wbr, hsrc, hkey, wkey) in enumerate(((0, wbm, hmb_, "hmD", "wbm"), (1024, wbd, hdb_, "hdD", "wbd"))):
                    gb = 0 + which
                    pbk = 2 + which
                    for kc in range(8):
                        T.op(PE, lambda kc=kc, colbase=colbase, gb=gb: nc.tensor.matmul(
                            PS[gb][:, :], lhsT=wg[:, kc, colbase + oc * 128:colbase + (oc + 1) * 128], rhs=xb[xbb][:, kc, :],
                            start=(kc == 0), stop=(kc == 7)), r=[("wg",), ("xbD", xbb)], w=[("ps", gb)])
                    T.op(ACT, lambda gb=gb, which=which: nc.scalar.activation(out=sg[2 * st + which][:], in_=PS[gb][:, :], func=AF.Sigmoid),
                         r=[("ps", gb)], w=[("sg", 2 * st + which)])
                    for kc in range(4):
                        T.op(PE, lambda kc=kc, wbr=wbr, hsrc=hsrc, pbk=pbk: nc.tensor.matmul(
                            PS[pbk][:, :], lhsT=wbr[:, kc, oc * 128:(oc + 1) * 128], rhs=hsrc[xbb][:, kc, :],
                            start=(kc == 0), stop=(kc == 3)), r=[wkey, (hkey, xbb)], w=[("ps", pbk)])
                    T.op(DVE, lambda which=which, pbk=pbk: nc.vector.tensor_tensor(out=tt[2 * st + which][:], in0=PS[pbk][:, :], in1=sg[2 * st + which][:], op=ALU.mult),
                         r=[("ps", pbk), ("sg", 2 * st + which)], w=[("tt", 2 * st + which)])
                T.op(POOL, lambda st=st: nc.gpsimd.tensor_tensor(out=mT[:, oc, :], in0=tt[2 * st][:], in1=tt[2 * st + 1][:], op=ALU.add),
                     r=[("tt", 2 * st), ("tt", 2 * st + 1)], w=[("mT", oc)])
            for tl in range(4):
                t = blk * 4 + tl
                b = t % 2
                ts_ = slice(tl * 128, (tl + 1) * 128)
                if t + 1 < NT:
                    T.dma(SP, xt[(t + 1) % 2][:], x_in[(t + 1) * 128:(t + 2) * 128, :], w=[("xtD", (t + 1) % 2)])
                if tl == 3 and blk + 1 < NB:
                    load_blk(blk + 1)
                def emit_wout(tl_):
                    sl_ = slice(tl_ * 128, (tl_ + 1) * 128)
                    for half in range(2):
                        for oc in range(8):
                            T.op(PE, lambda oc=oc, half=half: nc.tensor.matmul(
                                PS[4 + half][:, :], lhsT=mT[:, oc, sl_], rhs=wo[:, oc, half * 512:(half + 1) * 512],
                                start=(oc == 0), stop=(oc == 7)), r=[("mT", oc), ("wo",)], w=[("ps", 4 + half)])

                if tl == 0:
                    emit_wout(0)
                for half in range(2):
                    T.op(DVE, lambda half=half, b=b: nc.vector.tensor_tensor(
                        out=x1t[b][:, half * 512:(half + 1) * 512], in0=PS[4 + half][:, :], in1=xt[b][:, half * 512:(half + 1) * 512], op=ALU.add),
                        r=[("ps", 4 + half), ("xtD", b)], w=[("x1t", b, half)])
                T.dma(SP, x1_d[t * 128:(t + 1) * 128, :], x1t[b][:], r=[("x1t", b)], w=[("x1d", t)])
                if tl < 3:
                    emit_wout(tl + 1)
                norm_tile(bufs, x1t[b][:], [("x1t", b), "epsc"], "n2")
                T.op(DVE, lambda b=b: nc.vector.tensor_scalar(out=xhf[:], in0=x1t[b][:], scalar1=bufs["rs"][:], scalar2=None, op0=ALU.mult),
                     r=[("x1t", b), "rs"], w=["xhf"])
                for kc in range(8):
                    T.op(PE, lambda kc=kc: nc.tensor.transpose(PS[6 + kc // 4][:, (kc % 4) * 128:(kc % 4 + 1) * 128], xhf[:, kc * 128:(kc + 1) * 128], ident_f),
                         r=["xhf", "cst"], w=[("ps", 6 + kc // 4, kc % 4)])
                for kc in range(8):
                    T.op(ACT, lambda kc=kc: nc.scalar.activation(
                        out=h2f[:, kc, :], in_=PS[6 + kc // 4][:, (kc % 4) * 128:(kc % 4 + 1) * 128], func=AF.Identity,
                        scale=a2[:, kc:kc + 1], bias=s2[:, kc:kc + 1]), r=[("ps", 6 + kc // 4, kc % 4), "a2", "modc"], w=[("h2f", kc)])
                T.op(DVE, lambda: nc.vector.tensor_tensor(out=xhf[:], in0=xhf[:], in1=a2b[:], op=ALU.mult), r=["xhf", ("a2b",)], w=["xhf"])
                T.op(POOL, lambda b=b: nc.gpsimd.tensor_tensor(out=x2t[b][:], in0=xhf[:], in1=s2b[:], op=ALU.add), r=["xhf", ("s2b",)], w=[("x2t", b)])
                T.dma(SP, X2_d[t * 128:(t + 1) * 128, :], x2t[b][:], r=[("x2t", b)], w=[("X2d", t)])
                for kc in range(8):
                    T.op(PE, lambda kc=kc: nc.tensor.matmul(PS[0][:, 0:36], lhsT=h2f[:, kc, :], rhs=wr[:, kc, :], start=(kc == 0), stop=(kc == 7)),
                         r=[("h2f", kc), "wrt"], w=[("ps", 0)])
                T.op(DVE, lambda: nc.vector.tensor_tensor(out=lg[:], in0=PS[0][:, 0:36], in1=brb[:], op=ALU.add), r=[("ps", 0), "brb"], w=["lg"])
                gl = lg[:, 0:4]
                el = lg[:, 4:36].rearrange("p (g j) -> p g j", g=4)
                R = lambda a_, b_: r8[:, a_:b_]
                T.op(DVE, lambda: nc.vector.tensor_reduce(out=R(0, 1), in_=gl, axis=AX.X, op=ALU.max), r=["lg"], w=[("r8", 0)])
                T.op(DVE, lambda: nc.vector.tensor_scalar(out=R(4, 8), in0=gl, scalar1=R(0, 1), scalar2=None, op0=ALU.is_ge), r=["lg", ("r8", 0)], w=[("r8", 1)])
                T.op(DVE, lambda: nc.vector.tensor_scalar(out=R(1, 2), in0=R(0, 1), scalar1=-1.0, scalar2=None, op0=ALU.mult), r=[("r8", 0)], w=[("r8", 2)])
                T.op(ACT, lambda: nc.scalar.activation(out=R(8, 12), in_=gl, func=AF.Exp, bias=R(1, 2), accum_out=R(2, 3)), r=["lg", ("r8", 2)], w=[("r8", 3)])
                T.op(DVE, lambda: nc.vector.reciprocal(out=R(3, 4), in_=R(2, 3)), r=[("r8", 3)], w=[("r8", 4)])
                T.op(DVE, lambda: nc.vector.tensor_tensor(out=R(16, 48).rearrange("p (g j) -> p g j", g=4), in0=el,
                                                          in1=R(4, 8).unsqueeze(2).to_broadcast([128, 4, 8]), op=ALU.mult),
                     r=["lg", ("r8", 1)], w=[("r8", 5)])
                T.op(DVE, lambda: nc.vector.tensor_reduce(out=R(48, 56), in_=R(16, 48).rearrange("p (g j) -> p j g", g=4), axis=AX.X, op=ALU.add),
                     r=[("r8", 5)], w=[("r8", 6)])
                T.op(DVE, lambda: nc.vector.max(out=R(56, 64), in_=R(48, 56)), r=[("r8", 6)], w=[("r8", 7)])
                T.op(DVE, lambda: nc.vector.tensor_scalar(out=R(64, 72), in0=R(48, 56), scalar1=R(56, 57), scalar2=None, op0=ALU.is_equal),
                     r=[("r8", 6), ("r8", 7)], w=[("r8", 9)])
                T.op(DVE, lambda: nc.vector.tensor_scalar(out=R(72, 80), in0=R(48, 56), scalar1=R(57, 58), scalar2=None, op0=ALU.is_equal),
                     r=[("r8", 6), ("r8", 7)], w=[("r8", 10)])
                T.op(DVE, lambda: nc.vector.tensor_tensor(out=R(1, 2), in0=R(57, 58), in1=R(56, 57), op=ALU.subtract), r=[("r8", 7), ("r8", 3)], w=[("r8", 2)])
                T.op(ACT, lambda: nc.scalar.activation(out=R(2, 3), in_=R(1, 2), func=AF.Exp), r=[("r8", 2), ("r8", 4)], w=[("r8", 3)])
                T.op(DVE, lambda: nc.vector.tensor_scalar(out=R(1, 2), in0=R(2, 3), scalar1=1.0, scalar2=None, op0=ALU.add), r=[("r8", 3)], w=[("r8", 2)])
                T.op(DVE, lambda: nc.vector.reciprocal(out=R(1, 2), in_=R(1, 2)), r=[("r8", 2)], w=[("r8", 2)])
                T.op(DVE, lambda t=t: nc.vector.tensor_tensor(out=w12[:, t, 0:1], in0=R(1, 2), in1=R(3, 4), op=ALU.mult), r=[("r8", 2), ("r8", 4)], w=[("w12", t, 0)])
                T.op(DVE, lambda t=t: nc.vector.tensor_tensor(out=w12[:, t, 1:2], in0=w12[:, t, 0:1], in1=R(2, 3), op=ALU.mult), r=[("w12", t, 0), ("r8", 3)], w=[("w12", t, 1)])
                for g in range(4):
                    T.op(DVE, lambda g=g, t=t: nc.vector.tensor_scalar(out=m1_all[:, t, g * 8:(g + 1) * 8], in0=R(64, 72), scalar1=R(4 + g, 5 + g), scalar2=None, op0=ALU.mult),
                         r=[("r8", 9), ("r8", 1)], w=[("m1", t, g)])
                    T.op(DVE, lambda g=g, t=t: nc.vector.tensor_scalar(out=m2_all[:, t, g * 8:(g + 1) * 8], in0=R(72, 80), scalar1=R(4 + g, 5 + g), scalar2=None, op0=ALU.mult),
                         r=[("r8", 10), ("r8", 1)], w=[("m2", t, g)])
                T.op(DVE, lambda t=t: nc.vector.tensor_tensor(out=ohb[:], in0=m1_all[:, t, :], in1=m2_all[:, t, :], op=ALU.add),
                     r=[("m1", t), ("m2", t)], w=["ohb"])
                T.op(PE, lambda: nc.tensor.matmul(PS[1][:, 0:32], lhsT=tri_b, rhs=ohb[:], start=True, stop=True), r=["ohb", "cstb"], w=[("ps", 1)])
                T.op(DVE, lambda t=t: nc.vector.tensor_tensor(out=rank_all[:, t, :], in0=PS[1][:, 0:32], in1=tot[:], op=ALU.add),
                     r=[("ps", 1), "tot"], w=[("rank", t)])
                T.op(DVE, lambda t=t: nc.vector.tensor_tensor(out=rank_all[:, t, :], in0=rank_all[:, t, :], in1=ohb[:], op=ALU.subtract),
                     r=[("rank", t), "ohb"], w=[("rank", t)])
                T.op(PE, lambda: nc.tensor.matmul(PS[1][:, 0:32], lhsT=ones_b, rhs=ohb[:], start=True, stop=True), r=["ohb", "cstb"], w=[("ps", 1)])
                T.op(DVE, lambda: nc.vector.tensor_tensor(out=tot[:], in0=PS[1][:, 0:32], in1=tot[:], op=ALU.add), r=[("ps", 1), "tot"], w=["tot"])
        T.barrier()
    if dbg and "x1" in dbg_out:
        dump_dram(dbg_out["x1"], x1_d, S, D, F32)
        T.dma(SP, dbg_out["tot"][:, :], tot[:], r=["tot"])
        T.dma(SP, dbg_out["rank"][:, :], rank_all[:].rearrange("p t e -> p (t e)"), r=[("rank",)])
        T.dma(SP, dbg_out["m1"][:, :], m1_all[:].rearrange("p t e -> p (t e)"), r=[("m1",)])
        T.dma(SP, dbg_out["m2"][:, :], m2_all[:].rearrange("p t e -> p (t e)"), r=[("m2",)])
        T.dma(SP, dbg_out["w12"][:, :], w12[:].rearrange("p t e -> p (t e)"), r=[("w12",)])
    if stage <= 5:
        return finish()

    scope_mark("phE")
    with ExitStack() as ph:
        def sb(name, shape, dt):
            return ph.enter_context(nc.sbuf_tensor(name, list(shape), dt))
        P12 = sb("P12", [128, 2, NT], I32)
        NSPL = int(os.environ.get("KSPLIT", 1))
        widx = sb("widx", [128, NSPL, NSLOT], I32)
        with ExitStack() as ph2:
            def sb2(name, shape, dt):
                return ph2.enter_context(nc.sbuf_tensor("s_" + name, list(shape), dt))
            ni = sb2("ni", [128, 32], I32)
            ntf = sb2("ntf", [128, 32], F32)
            onesr = sb2("ones32", [128, 32], F32)
            tend = sb2("tend", [128, 32], F32)
            off = sb2("off", [128, 32], F32)
            big = sb2("big", [128, NT, 32], F32)
            pf = sb2("pf", [128, 2, NT], F32)
            cmp3 = sb2("cmp3", [128, NSLOT, 32], F32)
            sidx = sb2("sidx", [128, NSLOT], F32)
            esl = sb2("esl", [128, NSLOT], F32)
            pio = sb2("pio", [128, 1], F32)
            pioi = sb2("pioi", [128, 1], I32)
            T.op(DVE, lambda: nc.vector.tensor_scalar(out=ntf[:], in0=tot[:], scalar1=127.0, scalar2=None, op0=ALU.add), r=["tot"], w=["ntf"])
            T.op(DVE, lambda: nc.vector.tensor_copy(out=ni[:], in_=ntf[:]), r=["ntf"], w=["ni"])
            T.op(DVE, lambda: nc.vector.tensor_scalar(out=ni[:], in0=ni[:], scalar1=7, scalar2=None, op0=ALU.arith_shift_right),
                 r=["ni"], w=["ni"])
            T.op(DVE, lambda: nc.vector.tensor_copy(out=ntf[:], in_=ni[:]), r=["ni"], w=["ntf"])
            T.op(DVE, lambda: nc.vector.memset(onesr[:], 1.0), w=["ones32"])
            T.op(DVE, lambda: nc.vector.tensor_tensor_scan(out=tend[:], data0=onesr[:], data1=ntf[:], initial=0.0, op0=ALU.mult, op1=ALU.add),
                 r=["ones32", "ntf"], w=["tend"])
            T.op(DVE, lambda: nc.vector.tensor_tensor(out=off[:], in0=tend[:], in1=ntf[:], op=ALU.subtract), r=["tend", "ntf"], w=["off"])
            T.op(DVE, lambda: nc.vector.tensor_scalar(out=off[:], in0=off[:], scalar1=128.0, scalar2=None, op0=ALU.mult), r=["off"], w=["off"])
            for k, mk in enumerate((m1_all, m2_all)):
                T.op(DVE, lambda: nc.vector.tensor_tensor(out=big[:], in0=rank_all[:], in1=off[:].unsqueeze(1).to_broadcast([128, NT, 32]), op=ALU.add),
                     r=[("rank",), "off"], w=["big"])
                T.op(DVE, lambda mk=mk: nc.vector.tensor_tensor(out=big[:], in0=big[:], in1=mk[:], op=ALU.mult), r=["big", ("m1",), ("m2",)], w=["big"])
                T.op(DVE, lambda k=k: nc.vector.tensor_reduce(out=pf[:, k, :], in_=big[:], axis=AX.X, op=ALU.add), r=["big"], w=[("pf", k)])
            T.op(DVE, lambda: nc.vector.tensor_scalar(out=pf[:], in0=pf[:], scalar1=0.0, scalar2=float(NSLOT * 128 - 1), op0=ALU.max, op1=ALU.min),
                 r=[("pf",)], w=[("pf",)])
            T.op(DVE, lambda: nc.vector.tensor_copy(out=P12[:], in_=pf[:]), r=[("pf",)], w=["P12"])
            T.op(POOL, lambda: nc.gpsimd.iota(out=sidx[:], pattern=[[1, NSLOT]], base=0, channel_multiplier=0, allow_small_or_imprecise_dtypes=True), w=["sidx"])
            T.op(POOL, lambda: nc.gpsimd.iota(out=pio[:], pattern=[[1, 1]], base=0, channel_multiplier=1, allow_small_or_imprecise_dtypes=True), w=["pio"])
            T.op(DVE, lambda: nc.vector.tensor_tensor(out=cmp3[:], in0=tend[:].unsqueeze(1).to_broadcast([128, NSLOT, 32]),
                                                      in1=sidx[:].unsqueeze(2).to_broadcast([128, NSLOT, 32]), op=ALU.is_le),
                 r=["tend", "sidx"], w=["cmp3"])
            T.op(DVE, lambda: nc.vector.tensor_reduce(out=esl[:], in_=cmp3[:], axis=AX.X, op=ALU.add), r=["cmp3"], w=["esl"])
            T.op(DVE, lambda: nc.vector.tensor_scalar(out=esl[:], in0=esl[:], scalar1=31.0, scalar2=128.0, op0=ALU.min, op1=ALU.mult), r=["esl"], w=["esl"])
            T.op(DVE, lambda: nc.vector.tensor_scalar(out=esl[:], in0=esl[:], scalar1=pio[:], scalar2=None, op0=ALU.add), r=["esl", "pio"], w=["esl"])
            for ci in range(NSPL):
                T.op(DVE, lambda ci=ci: nc.vector.tensor_scalar(out=sidx[:], in0=esl[:], scalar1=float(NSPL), scalar2=float(ci), op0=ALU.mult, op1=ALU.add),
                     r=["esl", "cmp3"], w=["sidx"])
                T.op(DVE, lambda ci=ci: nc.vector.tensor_copy(out=widx[:, ci, :], in_=sidx[:]), r=["sidx"], w=[("widx", ci)])
            T.barrier()
        if dbg and "P12" in dbg_out:
            T.dma(SP, dbg_out["P12"][:, :], P12[:].rearrange("p k t -> p (k t)"), r=["P12"])
            T.dma(SP, dbg_out["widx"][:, :], widx[:, 0, :], r=["widx"])

        KE = int(os.environ.get("KE", 9))
        if KE >= 2:
            x2l = [sb(f"x2l{i}", [128, D], BF16) for i in range(3)]
            for t in range(NT):
                b = t % 3
                T.dma(SP, x2l[b][:], X2_d[t * 128:(t + 1) * 128, :], w=[("x2l", b)])
                for k in range(2):
                    T.dmai(Xs_d[:, :], x2l[b][:], out_idx=P12[:, k, t:t + 1], r=[("x2l", b), "P12"], w=[("Xs", t, k)])
            T.barrier()

        if KE >= 3:
            xs = [sb(f"xs{i}", [128, D], BF16) for i in range(3)]
            wsl = [sb(f"wsl{i}", [128, 6144], BF16) for i in range(3)]
            xT = [sb(f"xTs{i}", [128, 8, 128], BF16) for i in range(2)]
            sS = [sb(f"sS{i}", [128, 256], F32) for i in range(2)]
            ac = [sb(f"ac{i}", [128, 256], BF16) for i in range(2)]
            aT = [sb(f"aTs{i}", [128, 2, 128], BF16) for i in range(2)]
            yo = [sb(f"yo{i}", [128, D], BF16) for i in range(2)]
            NSL = int(os.environ.get("KSLOTS", NSLOT))

            def load_slot(sl):
                b3 = sl % 3
                T.dma(SP, xs[b3][:], Xs_d[sl * 128:(sl + 1) * 128, :], r=[("Xs",)], w=[("xs", b3)])
                cw_ = 6144 // NSPL
                for ci in range(NSPL):
                    T.dmai(wsl[b3][:, ci * cw_:(ci + 1) * cw_], Wcat_d.rearrange("r (c w) -> (r c) w", c=NSPL), in_idx=widx[:, ci, sl:sl + 1],
                           r=[("Wcat",), "widx"], w=[("wsl", b3, ci)])

            def slot_s1(sl):
                b2, b3 = sl % 2, sl % 3
                base = 4 * b2
                xTp = psb(base).rearrange("p (k t) -> p k t", k=8)
                for kc in range(8):
                    T.op(PE, lambda kc=kc: nc.tensor.transpose(xTp[:, kc, :], xs[b3][:, kc * 128:(kc + 1) * 128], ident_b),
                         r=[("xs", b3), "cstb"], w=[("ps", base)])
                T.op(ACT, lambda: nc.scalar.copy(out=xT[b2][:, 0:4, :], in_=xTp[:, 0:4, :]), r=[("ps", base)], w=[("xTs", b2, 0)])
                T.op(DVE, lambda: nc.vector.tensor_copy(out=xT[b2][:, 4:8, :], in_=xTp[:, 4:8, :]), r=[("ps", base), ("xTs", b2, 0)], w=[("xTs", b2, 1)])
                for kc in range(8):
                    T.op(PE, lambda kc=kc: nc.tensor.matmul(PS[base + 1][:, :], lhsT=xT[b2][:, kc, :], rhs=wsl[b3][:, kc * 512:(kc + 1) * 512],
                                                            start=(kc == 0), stop=(kc == 7)), r=[("xTs", b2), ("wsl", b3)], w=[("ps", base + 1)])

            def slot_s2(sl):
                b2, b3 = sl % 2, sl % 3
                base = 4 * b2
                T.op(ACT, lambda: nc.scalar.activation(out=sS[b2][:], in_=PS[base + 1][:, 0:256], func=AF.Silu), r=[("ps", base + 1)], w=[("sS", b2)])
                T.op(DVE, lambda: nc.vector.tensor_tensor(out=ac[b2][:], in0=PS[base + 1][:, 256:512], in1=sS[b2][:], op=ALU.mult),
                     r=[("ps", base + 1), ("sS", b2)], w=[("ac", b2)])
                aTp = psb(base)[:, 0:256].rearrange("p (k t) -> p k t", k=2)
                for fc in range(2):
                    T.op(PE, lambda fc=fc: nc.tensor.transpose(aTp[:, fc, :], ac[b2][:, fc * 128:(fc + 1) * 128], ident_b),
                         r=[("ac", b2), "cstb"], w=[("ps", base)])
                T.op(ACT, lambda: nc.scalar.copy(out=aT[b2][:], in_=aTp), r=[("ps", base)], w=[("aTs", b2)])
                for cb_ in range(2):
                    for fc in range(2):
                        T.op(PE, lambda fc=fc, cb_=cb_: nc.tensor.matmul(
                            PS[base + 2 + cb_][:, :], lhsT=aT[b2][:, fc, :], rhs=wsl[b3][:, 4096 + fc * 1024 + cb_ * 512: 4096 + fc * 1024 + (cb_ + 1) * 512],
                            start=(fc == 0), stop=(fc == 1)), r=[("aTs", b2), ("wsl", b3)], w=[("ps", base + 2 + cb_)])
                T.op(ACT, lambda: nc.scalar.copy(out=yo[b2][:, 0:512], in_=PS[base + 2][:, :]), r=[("ps", base + 2)], w=[("yo", b2, 0)])
                T.op(DVE, lambda: nc.vector.tensor_copy(out=yo[b2][:, 512:1024], in_=PS[base + 3][:, :]), r=[("ps", base + 3)], w=[("yo", b2, 1)])
                T.dma(SP, Ys_d[sl * 128:(sl + 1) * 128, :], yo[b2][:], r=[("yo", b2)], w=[("Ys", sl)])

            for sl in range(min(2, NSL)):
                load_slot(sl)
            slot_s1(0)
            for sl in range(NSL):
                if sl + 2 < NSL:
                    load_slot(sl + 2)
                if sl + 1 < NSL:
                    slot_s1(sl + 1)
                slot_s2(sl)
            T.barrier()

        if KE >= 4:
            y1 = [sb(f"y1_{i}", [128, D], BF16) for i in range(3)]
            ya = [sb(f"ya_{i}", [128, D], F32) for i in range(3)]
            y2 = [sb(f"y2_{i}", [128, D], BF16) for i in range(3)]
            x1l = [sb(f"x1l{i}", [128, D], F32) for i in range(3)]
            def load_tok(t):
                b = t % 3
                T.dma(SP, x1l[b][:], x1_d[t * 128:(t + 1) * 128, :], w=[("x1l", b)])
                T.dmai(y1[b][:], Ys_d[:, :], in_idx=P12[:, 0, t:t + 1], r=[("Ys",), "P12"], w=[("y1", b)])
                T.dmai(y2[b][:], Ys_d[:, :], in_idx=P12[:, 1, t:t + 1], r=[("Ys",), "P12"], w=[("y2", b)])

            load_tok(0)
            load_tok(1)
            for t in range(NT):
                b = t % 3
                if t + 2 < NT:
                    load_tok(t + 2)
                T.op(DVE, lambda b=b, t=t: nc.vector.tensor_scalar(out=ya[b][:], in0=y1[b][:], scalar1=w12[:, t, 0:1], scalar2=None, op0=ALU.mult),
                     r=[("y1", b), ("w12",)], w=[("ya", b)])
                T.op(DVE, lambda b=b, t=t: nc.vector.scalar_tensor_tensor(out=ya[b][:], in0=y2[b][:], scalar=w12[:, t, 1:2], in1=ya[b][:], op0=ALU.mult, op1=ALU.add),
                     r=[("ya", b), ("y2", b), ("w12",)], w=[("ya", b)])
                T.op(DVE, lambda b=b: nc.vector.tensor_tensor(out=ya[b][:], in0=ya[b][:], in1=g2b[:], op=ALU.mult), r=[("ya", b), ("g2b",)], w=[("ya", b)])
                T.op(DVE, lambda b=b: nc.vector.tensor_tensor(out=x1l[b][:], in0=x1l[b][:], in1=ya[b][:], op=ALU.add), r=[("ya", b), ("x1l", b)], w=[("x1l", b)])
                T.dma(SP, out_d[t * 128:(t + 1) * 128, :], x1l[b][:], r=[("x1l", b)], w=[("out", t)])
            T.barrier()
    return finish()


def _consts():
    c = np.zeros((128, NCST), np.float32)
    c[:, 0:128] = np.eye(128, dtype=np.float32)
    s = np.arange(128)[:, None]
    t = np.arange(128)[None, :]
    c[:, 128:256] = (s <= t).astype(np.float32)
    c[:, 256:384] = ((s // 64) == (t // 64)).astype(np.float32)
    R = np.zeros((128, 128), np.float32)
    for base in (0, 64):
        for d in range(8):
            R[base + d, base + d + 8] = -1.0
            R[base + d + 8, base + d] = 1.0
    c[:, 384:512] = R.T
    c[:, 512:640] = 1.0
    inv = 500000.0 ** (-np.arange(0, 16, 2, dtype=np.float32) / 16.0)
    for p in range(128):
        d = p % 64
        c[p, 640] = inv[d % 8] if d < 16 else 0.0
    return c


_NC_CACHE = {}


def make_in_maps(inputs):
    f = lambda k: np.ascontiguousarray(np.asarray(inputs[k], dtype=np.float32))
    x = f("x")
    c = f("c")
    pos = np.ascontiguousarray(np.asarray(inputs["positions"], dtype=np.int32))
    b_ada = f("b_ada")[0]
    shared = {
        "w_ada": f("w_ada")[0],
        "b_ada_col": np.ascontiguousarray(b_ada.reshape(48, 128).T),
        "b_ada_row": np.ascontiguousarray(b_ada.reshape(1, -1)),
        "n1w": np.ascontiguousarray(f("norm1_w")[0].reshape(8, 128).T),
        "n2w": np.ascontiguousarray(f("norm2_w")[0].reshape(8, 128).T),
        "n2row": np.ascontiguousarray(f("norm2_w")[0].reshape(1, D)),
        "w_in": f("w_in")[0],
        "bgate": np.ascontiguousarray(np.stack([f("b_igate")[0], f("b_fgate")[0]], axis=1)),
        "convw": np.ascontiguousarray(f("conv_w")[0].T.reshape(8, 128, 4).transpose(1, 0, 2)),
        "convb": np.ascontiguousarray(f("conv_b")[0].reshape(8, 128).T),
        "mnw": np.ascontiguousarray(f("mlstm_norm_w")[0].reshape(1, 512)),
        "qnw": np.ascontiguousarray(np.tile(f("q_norm_w")[0], 2).reshape(128, 1)),
        "knw": np.ascontiguousarray(np.tile(f("k_norm_w")[0], 2).reshape(128, 1)),
        "qkrow": np.ascontiguousarray(np.concatenate([f("q_norm_w")[0], f("k_norm_w")[0]]).reshape(1, 128)),
        "lamv": np.ascontiguousarray(np.concatenate([f("lam_q1")[0], f("lam_k1")[0], f("lam_q2")[0], f("lam_k2")[0]]).reshape(1, 256)),
        "subw": np.ascontiguousarray(f("subln_w")[0].reshape(128, 1)),
        "w_br_m": f("w_br_m")[0],
        "w_br_d": f("w_br_d")[0],
        "w_out": f("w_out")[0],
        "wr": np.ascontiguousarray(np.concatenate([f("w_rg")[0], f("w_re")[0]], axis=1)),
        "br": np.ascontiguousarray(np.concatenate([f("b_rg")[0], f("b_re")[0]]).reshape(1, 36)),
        "w_gate": f("w_gate")[0],
        "w_up": f("w_up")[0],
        "w_down": f("w_down")[0],
        "consts": _consts(),
    }
    maps = []
    for b in range(8):
        m = dict(shared)
        m["x"] = x[b]
        m["cT"] = np.ascontiguousarray(c[b].reshape(8, 128).T)
        m["pos"] = np.ascontiguousarray(pos[b].reshape(1, S))
        maps.append(m)
    return maps


def kernel(**inputs):
    if "nc" not in _NC_CACHE:
        _NC_CACHE["nc"] = build()
    nc = _NC_CACHE["nc"]
    maps = make_in_maps(inputs)
    res = run_bass_kernel_spmd(nc, maps, core_ids=list(range(8)))
    out = np.stack([np.asarray(r["out"], dtype=np.float32) for r in res.results], axis=0)
    return out
```

```python
import math
import os
from contextlib import ExitStack

import numpy as np
import concourse.bass as bass
import concourse.mybir as mybir
from concourse.bass_utils import run_bass_kernel_spmd

F32 = mybir.dt.float32
BF16 = mybir.dt.bfloat16
I32 = mybir.dt.int32
AF = mybir.ActivationFunctionType
ALU = mybir.AluOpType
AX = mybir.AxisListType

S = 4096
D = 1024
NT = 32
NB = 8
EPS = 1e-6
NCST = 5 * 128 + 2


class _StopBuild(Exception):
    pass


class Trk:
    LIMIT = 50000
    NDSEM = 12

    def __init__(self, nc):
        self.nc = nc
        self.eng = {"pe": nc.tensor, "act": nc.scalar, "dve": nc.vector, "pool": nc.gpsimd, "sp": nc.sync}
        self.cur = {}
        self.seq = {}
        self.gen = {}
        for e in self.eng:
            self.cur[e] = [nc.alloc_semaphore(f"s_{e}_0"), 0]
            self.seq[e] = 0
            self.gen[e] = 0
        self.allsems = [(e, self.cur[e]) for e in self.eng]
        self.waited = {e: {} for e in self.eng}
        self.lastw = {}
        self.readers = {}
        self.children = {}
        self.dring = {}
        for q in ("sp", "pool", "act", "wc"):
            self.dring[q] = [[nc.alloc_semaphore(f"d_{q}_{i}"), 0] for i in range(self.NDSEM)]
        self.dpos = {q: 0 for q in self.dring}
        self.all_dma = []

    def _related(self, k):
        ks = [k[:i] for i in range(1, len(k) + 1)]
        stack = [k]
        while stack:
            p = stack.pop()
            for c in self.children.get(p, ()):
                ks.append(c)
                stack.append(c)
        return ks

    def _register(self, k):
        for i in range(1, len(k)):
            self.children.setdefault(k[:i], set()).add(k[:i + 1])

    def _wait(self, e, ev):
        sem, val, src, sq = ev
        if src == e:
            if e == "pe":
                return
            if e != "pool" and self.seq[e] - sq >= 6:
                return
        w = self.waited[e]
        if w.get(sem.name, 0) >= val:
            return
        self.eng[e].wait_ge(sem, val)
        w[sem.name] = val

    def _deps(self, e, r, w):
        evs = []
        for k in r:
            for kk in self._related(k):
                if kk in self.lastw:
                    evs.append(self.lastw[kk])
        for k in w:
            for kk in self._related(k):
                if kk in self.lastw:
                    evs.append(self.lastw[kk])
                evs.extend(self.readers.get(kk, {}).values())
        for ev in evs:
            self._wait(e, ev)

    def _record(self, ev, r, w):
        for k in r:
            self._register(k)
            self.readers.setdefault(k, {})[ev[2] if ev[2] is not None else ev[0].name] = ev
        for k in w:
            self._register(k)
            self.lastw[k] = ev
            self.readers[k] = {}
            for kk in self._related(k):
                if kk != k and len(kk) > len(k):
                    self.readers[kk] = {}
                    self.lastw.pop(kk, None)

    @staticmethod
    def _norm(ks):
        out = []
        for k in ks:
            k = tuple(k) if isinstance(k, (tuple, list)) else (k,)
            if k[0] == "ps":
                k = k[:2]
            out.append(k)
        return out

    def start_record(self):
        self._rec = []

    def stop_record(self):
        rec, self._rec = self._rec, None
        return rec

    def op(self, e, fn, r=(), w=()):
        if getattr(self, "_rec", None) is not None:
            self._rec.append((e, fn, r, w))
            return None
        r = self._norm(r)
        w = self._norm(w)
        w = w + [k for k in r if k[0] == "ps" and k not in w]
        self._deps(e, r, w)
        ins = fn()
        c = self.cur[e]
        c[1] += 1
        self.seq[e] += 1
        ins.then_inc(c[0], 1)
        ev = (c[0], c[1], e, self.seq[e])
        self._record(ev, r, w)
        if c[1] >= self.LIMIT:
            self.gen[e] += 1
            self.cur[e] = [self.nc.alloc_semaphore(f"s_{e}_{self.gen[e]}"), 0]
            self.allsems.append((e, self.cur[e]))
        return ins

    def dma(self, q, out, in_, r=(), w=(), ring=None, **kw):
        r = self._norm(r)
        w = self._norm(w)
        rq = ring or q
        ring = self.dring[rq]
        slot = ring[self.dpos[rq] % self.NDSEM]
        self.dpos[rq] += 1
        if slot[1] > 0:
            self._wait(q, (slot[0], slot[1], None, 0))
        self._deps(q, r, w)
        ins = self.eng[q].dma_start(out=out, in_=in_, **kw)
        slot[1] += 16
        ins.then_inc(slot[0], 16)
        ev = (slot[0], slot[1], None, 0)
        self._record(ev, r, w)
        return ins

    def dmai(self, out, in_, out_idx=None, in_idx=None, r=(), w=()):
        q = "pool"
        r = self._norm(r)
        w = self._norm(w)
        ring = self.dring[q]
        slot = ring[self.dpos[q] % self.NDSEM]
        self.dpos[q] += 1
        if slot[1] > 0:
            self._wait(q, (slot[0], slot[1], None, 0))
        self._deps(q, r, w)
        ins = self.nc.gpsimd.indirect_dma_start(
            out=out, out_offset=(bass.IndirectOffsetOnAxis(ap=out_idx, axis=0) if out_idx is not None else None),
            in_=in_, in_offset=(bass.IndirectOffsetOnAxis(ap=in_idx, axis=0) if in_idx is not None else None))
        slot[1] += 16
        ins.then_inc(slot[0], 16)
        ev = (slot[0], slot[1], None, 0)
        self._record(ev, r, w)
        return ins

    def barrier(self, full=True):
        evs = []
        for (se, c) in self.allsems:
            if c[1] > 0:
                evs.append((c[0], c[1], "__" + se, 0))
        for q, ring in self.dring.items():
            if q == "wc" and not full:
                continue
            for slot in ring:
                if slot[1] > 0:
                    evs.append((slot[0], slot[1], None, 0))
        for e in self.eng:
            for ev in evs:
                if ev[2] == "__" + e:
                    continue
                self._wait(e, ev)
        self.lastw.clear()
        self.readers.clear()
        self.children.clear()


def build(stage=99, dbg=None):
    nc = bass.Bass("TRN2", target_bir_lowering=False)
    T = Trk(nc)
    _scope = [None]

    def scope_mark(nm):
        if os.environ.get("KSCOPE") != "1":
            return
        if _scope[0] is not None:
            _scope[0].__exit__(None, None, None)
        _scope[0] = nc.named_scope(nm)
        _scope[0].__enter__()

    PE, ACT, DVE, POOL, SP = "pe", "act", "dve", "pool", "sp"

    def din(name, shape, dt=F32):
        return nc.dram_tensor(name, list(shape), dt, kind="ExternalInput").ap()

    def dscr(name, shape, dt):
        return nc.dram_tensor(name, list(shape), dt, kind="Internal").ap()

    x_in = din("x", [S, D])
    cT_in = din("cT", [128, 8])
    pos_in = din("pos", [1, S], I32)
    w_ada = din("w_ada", [D, 6 * D])
    b_ada_col = din("b_ada_col", [128, 48])
    b_ada_row = din("b_ada_row", [1, 6 * D])
    n1w_in = din("n1w", [128, 8])
    n2w_in = din("n2w", [128, 8])
    n2row_in = din("n2row", [1, D])
    w_in = din("w_in", [D, 5640])
    bgate_in = din("bgate", [4, 2])
    convw_in = din("convw", [128, 8, 4])
    convb_in = din("convb", [128, 8])
    mnw_in = din("mnw", [1, 512])
    qnw_in = din("qnw", [128, 1])
    knw_in = din("knw", [128, 1])
    qkrow_in = din("qkrow", [1, 128])
    lamv_in = din("lamv", [1, 256])
    subw_in = din("subw", [128, 1])
    w_br_m = din("w_br_m", [512, D])
    w_br_d = din("w_br_d", [512, D])
    w_out = din("w_out", [D, D])
    wr_in = din("wr", [D, 36])
    br_in = din("br", [1, 36])
    w_gate = din("w_gate", [32, D, 256])
    w_up = din("w_up", [32, D, 256])
    w_down = din("w_down", [32, 256, D])
    consts_in = din("consts", [128, NCST])
    out_d = nc.dram_tensor("out", [S, D], F32, kind="ExternalOutput").ap()

    xnT_d = dscr("xnT_d", [D, S], BF16)
    hmT_d = dscr("hmT_d", [512, S], BF16)
    hdT_d = dscr("hdT_d", [512, S], BF16)
    x1_d = dscr("x1_d", [S, D], F32)
    xn2T_d = dscr("xn2T_d", [D, S], BF16)
    combT_d = dscr("combT_d", [32, S], BF16)
    dec_d = dscr("dec_d", [4, 32], F32)
    gi_d = dscr("gi_d", [4, S], F32)
    NSLOT = 96
    X2_d = dscr("X2_d", [S, D], BF16)
    Xs_d = dscr("Xs_d", [NSLOT * 128, D], BF16)
    Ys_d = dscr("Ys_d", [NSLOT * 128, D], BF16)
    Wcat_d = dscr("Wcat_d", [32 * 128, 6144], BF16)
    gf_d = dscr("gf_d", [4, S], F32)

    dbg_out = {}
    if dbg:
        for name, (shape, dt) in dbg.items():
            dbg_out[name] = nc.dram_tensor("dbg_" + name, list(shape), dt, kind="ExternalOutput").ap()

    PS = [nc.alloc_psum_tensor(f"ps{i}", [128, 512], F32) for i in range(8)]

    def psb(i):
        return PS[i][:].bitcast(BF16)

    xnT_v = xnT_d.rearrange("(k p) t -> p k t", p=128)
    xn2T_v = xn2T_d.rearrange("(k p) t -> p k t", p=128)
    hmT_v = hmT_d.rearrange("(k p) t -> p k t", p=128)
    hdT_v = hdT_d.rearrange("(k p) t -> p k t", p=128)
    w_in_v = w_in.rearrange("(k p) n -> p k n", p=128)
    wg_v = w_gate.rearrange("e (k p) f -> e p k f", p=128)
    wu_v = w_up.rearrange("e (k p) f -> e p k f", p=128)
    wdn_v = w_down.rearrange("e (k p) n -> e p k n", p=128)

    glob = ExitStack()

    def sbg(name, shape, dt):
        return glob.enter_context(nc.sbuf_tensor("s_" + name, list(shape), dt))

    cst = sbg("cst", [128, NCST], F32)
    cstb = sbg("cstb", [128, 5 * 128], BF16)
    modc = sbg("modc", [128, 48], F32)
    a1 = sbg("a1", [128, 8], F32)
    a2 = sbg("a2", [128, 8], F32)
    g1b = sbg("g1b", [128, D], F32)
    g2b = sbg("g2b", [128, D], F32)
    a2b = sbg("a2b", [128, D], F32)
    s2b = sbg("s2b", [128, D], F32)
    rank_all = sbg("rank_all", [128, NT, 32], F32)
    m1_all = sbg("m1_all", [128, NT, 32], F32)
    m2_all = sbg("m2_all", [128, NT, 32], F32)
    w12 = sbg("w12", [128, NT, 2], F32)
    tot = sbg("tot", [128, 32], F32)
    smallc = sbg("smallc", [128, 16], F32)
    ident_f = cst[:, 0:128]
    tri_f = cst[:, 128:256]
    invf = cst[:, 640:641]
    ident_b = cstb[:, 0:128]
    tri_b = cstb[:, 128:256]
    bo64_b = cstb[:, 256:384]
    RT_b = cstb[:, 384:512]
    ones_b = cstb[:, 512:640]
    ones_f = cst[:, 512:640]

    T.dma(SP, cst[:], consts_in[:, :], w=["cst"])
    T.op(DVE, lambda: nc.vector.tensor_copy(out=cstb[:], in_=cst[:, 0:640]), r=["cst"], w=["cstb"])

    early2 = ExitStack()
    CosB = early2.enter_context(nc.sbuf_tensor("s_CosB", [128, S], BF16))
    SinB = early2.enter_context(nc.sbuf_tensor("s_SinB", [128, S], BF16))
    early2_open = [True]

    def sin_reduce(ph_sb, dst_bf, ang, n, tagk):
        ki = ph_sb["ki"]
        kf = ph_sb["kf"]
        msk = ph_sb["msk"]
        C1 = 6.28125
        C2 = 2.0 * math.pi - 6.28125
        T.op(DVE, lambda: nc.vector.tensor_scalar(out=ki[:, 0:n], in0=ang, scalar1=1.0 / (2.0 * math.pi), scalar2=None, op0=ALU.mult),
             r=[tagk], w=["ki"])
        T.op(DVE, lambda: nc.vector.tensor_copy(out=kf[:, 0:n], in_=ki[:, 0:n]), r=["ki"], w=["kf"])
        T.op(DVE, lambda: nc.vector.scalar_tensor_tensor(out=ang, in0=kf[:, 0:n], scalar=-C1, in1=ang, op0=ALU.mult, op1=ALU.add),
             r=["kf", tagk], w=[tagk])
        T.op(DVE, lambda: nc.vector.scalar_tensor_tensor(out=ang, in0=kf[:, 0:n], scalar=-C2, in1=ang, op0=ALU.mult, op1=ALU.add),
             r=["kf", tagk], w=[tagk])
        T.op(DVE, lambda: nc.vector.tensor_scalar(out=msk[:, 0:n], in0=ang, scalar1=math.pi, scalar2=-2.0 * math.pi, op0=ALU.is_gt, op1=ALU.mult),
             r=[tagk], w=["msk"])
        T.op(DVE, lambda: nc.vector.tensor_tensor(out=ang, in0=ang, in1=msk[:, 0:n], op=ALU.add), r=[tagk, "msk"], w=[tagk])
        T.op(DVE, lambda: nc.vector.tensor_scalar(out=msk[:, 0:n], in0=ang, scalar1=-math.pi, scalar2=2.0 * math.pi, op0=ALU.is_lt, op1=ALU.mult),
             r=[tagk], w=["msk"])
        T.op(DVE, lambda: nc.vector.tensor_tensor(out=ang, in0=ang, in1=msk[:, 0:n], op=ALU.add), r=[tagk, "msk"], w=[tagk])
        T.op(DVE, lambda: nc.vector.tensor_scalar(out=ang, in0=ang, scalar1=math.pi, scalar2=-math.pi, op0=ALU.min, op1=ALU.max),
             r=[tagk], w=[tagk])
        T.op(ACT, lambda: nc.scalar.activation(out=dst_bf, in_=ang, func=AF.Sin), r=[tagk], w=[tagk + "_o"])

    early = ExitStack()
    NWST = int(os.environ.get("KWST", 2))
    wst = [early.enter_context(nc.sbuf_tensor(f"s_wst{i}", [128, 6144], BF16)) for i in range(NWST)]
    for e in range(32):
        b = e % NWST
        gu = wst[b][:, 0:4096].rearrange("p (k f) -> p k f", k=8)
        T.dma(POOL, gu[:, :, 0:256], wg_v[e], w=[("wst", b, 0)], ring="wc")
        T.dma(POOL, gu[:, :, 256:512], wu_v[e], w=[("wst", b, 1)], ring="wc")
        T.dma(POOL, wst[b][:, 4096:6144].rearrange("p (k n) -> p k n", k=2), wdn_v[e], w=[("wst", b, 2)], ring="wc")
        T.dma(POOL, Wcat_d[e * 128:(e + 1) * 128, :], wst[b][:], r=[("wst", b)], w=[("Wcat", e)], ring="wc")
    early_open = [True]
    scope_mark("ph0")
    with ExitStack() as ph:
        def sb(name, shape, dt):
            return ph.enter_context(nc.sbuf_tensor("s_" + name, list(shape), dt))
        cT = sb("cT", [128, 8], F32)
        cT2 = sb("cT2", [128, 8, 2], F32)
        cbc = sb("cbc", [128, 8, 128], F32)
        wa = [sb(f"wa{i}", [128, 8, 512], F32) for i in range(2)]
        bcol = sb("bcol", [128, 48], F32)
        brow = sb("brow", [128, 2048], F32)
        nw = sb("nw", [128, 16], F32)
        tmp8 = sb("tmp8", [128, 8], F32)
        lamb = sb("lamb", [128, 256], F32)
        qkrow = sb("qkrow", [128, 128], F32)
        lt = sb("lt", [128, 128], F32)
        l2 = sb("l2", [128, 4], F32)

        T.dma(SP, cT[:], cT_in[:, :], w=["cT"])
        T.dma(SP, bcol[:], b_ada_col[:, :], w=["bcol"])
        T.dma(SP, brow[:, 0:1024], b_ada_row[0:1, 2048:3072].partition_broadcast(128), w=["brow0"])
        T.dma(SP, brow[:, 1024:2048], b_ada_row[0:1, 5120:6144].partition_broadcast(128), w=["brow1"])
        T.dma(SP, nw[:, 0:8], n1w_in[:, :], w=["nw0"])
        T.dma(SP, nw[:, 8:16], n2w_in[:, :], w=["nw1"])
        T.dma(SP, smallc[:, 0:1], qnw_in[:, :], w=["sc0"])
        T.dma(SP, smallc[:, 1:2], knw_in[:, :], w=["sc1"])
        T.dma(SP, smallc[:, 2:3], subw_in[:, :], w=["sc2"])
        T.dma(SP, lamb[:], lamv_in[0:1, :].partition_broadcast(128), w=["lamb"])
        T.dma(SP, qkrow[:], qkrow_in[0:1, :].partition_broadcast(128), w=["qkrow"])
        for k in range(2):
            T.op(DVE, lambda k=k: nc.vector.tensor_copy(out=cT2[:, :, k], in_=cT[:]), r=["cT"], w=[("cT2", k)])
        for kc in range(8):
            T.op(DVE, lambda kc=kc: nc.vector.tensor_copy(out=cbc[:, kc, :], in_=cT[:, kc:kc + 1].to_broadcast([128, 128])),
                 r=["cT"], w=[("cbc", kc)])
        if True:
            posi = ph.enter_context(nc.sbuf_tensor("s_posi", [128, 1024], I32))
            posf = ph.enter_context(nc.sbuf_tensor("s_posf", [128, 1024], F32))
            ang = ph.enter_context(nc.sbuf_tensor("s_ang", [128, 1024], F32))
            tb = dict(ki=ph.enter_context(nc.sbuf_tensor("s_ki", [128, 1024], I32)),
                      kf=ph.enter_context(nc.sbuf_tensor("s_kf", [128, 1024], F32)),
                      msk=ph.enter_context(nc.sbuf_tensor("s_msk", [128, 1024], F32)))
            for c4 in range(4):
                cs = slice(c4 * 1024, (c4 + 1) * 1024)
                T.dma(SP, posi[:], pos_in[0:1, cs].partition_broadcast(128), w=["posi"])
                T.op(DVE, lambda: nc.vector.tensor_copy(out=posf[:], in_=posi[:]), r=["posi"], w=["posf"])
                T.op(DVE, lambda: nc.vector.tensor_scalar(out=ang[:], in0=posf[:], scalar1=invf, scalar2=None, op0=ALU.mult),
                     r=["posf", "cst"], w=["ang"])
                sin_reduce(tb, SinB[:, cs], ang[:], 1024, "ang")
                T.op(DVE, lambda: nc.vector.tensor_scalar(out=ang[:], in0=posf[:], scalar1=invf, scalar2=math.pi / 2, op0=ALU.mult, op1=ALU.add),
                     r=["posf", "cst", "ang_o"], w=["ang"])
                sin_reduce(tb, CosB[:, cs], ang[:], 1024, "ang")

        w_ada_v = w_ada.rearrange("(k p) n -> p k n", p=128)
        gate_bank = {4: 1, 5: 2, 6: 5, 7: 6, 8: 1, 9: 2, 10: 3, 11: 4}
        n2rb = sb("n2rb", [128, D], F32)
        T.dma(SP, n2rb[:], n2row_in[0:1, :].partition_broadcast(128), w=["n2rb"])
        T.dma(SP, s2b[:], b_ada_row[0:1, 3072:4096].partition_broadcast(128), w=["s2b_bias"])
        T.dma(SP, a2b[:], b_ada_row[0:1, 4096:5120].partition_broadcast(128), w=["a2b_bias"])
        for blk in range(12):
            b = blk % 2
            for hh in range(2):
                T.dma(SP, wa[b][:, 4 * hh:4 * hh + 4, :], w_ada_v[:, 4 * hh:4 * hh + 4, blk * 512:(blk + 1) * 512], w=[("wa", b)])
            for m in range(4):
                j = blk * 4 + m
                for kc in range(8):
                    T.op(PE, lambda j=j, m=m, kc=kc, b=b: nc.tensor.matmul(
                        PS[0][:, 2 * j:2 * j + 2], lhsT=wa[b][:, kc, m * 128:(m + 1) * 128], rhs=cT2[:, kc, :],
                        start=(kc == 0), stop=(kc == 7)), r=[("wa", b), "cT2"], w=[("ps", 0)])
            if blk in gate_bank:
                gb = gate_bank[blk]
                for kc in range(8):
                    T.op(PE, lambda kc=kc, b=b, gb=gb: nc.tensor.matmul(
                        PS[gb][:, :], lhsT=cbc[:, kc, :], rhs=wa[b][:, kc, :], start=(kc == 0), stop=(kc == 7)),
                        r=[("wa", b), "cbc"], w=[("ps", gb)])
                if blk == 5:
                    for hh in range(2):
                        T.op(DVE, lambda hh=hh: nc.vector.tensor_tensor(out=g1b[:, hh * 512:(hh + 1) * 512], in0=PS[1 + hh][:, :],
                                                                      in1=brow[:, hh * 512:(hh + 1) * 512], op=ALU.add),
                             r=[("ps", 1 + hh), "brow0"], w=[("g1b", hh)])
                if blk == 7:
                    for hh in range(2):
                        T.op(DVE, lambda hh=hh: nc.vector.tensor_tensor(out=s2b[:, hh * 512:(hh + 1) * 512], in0=PS[5 + hh][:, :],
                                                                      in1=s2b[:, hh * 512:(hh + 1) * 512], op=ALU.add),
                             r=[("ps", 5 + hh), "s2b_bias"], w=[("s2b", hh)])
                if blk == 9:
                    for hh in range(2):
                        sl = slice(hh * 512, (hh + 1) * 512)
                        T.op(DVE, lambda hh=hh, sl=sl: nc.vector.tensor_tensor(out=a2b[:, sl], in0=PS[1 + hh][:, :], in1=a2b[:, sl], op=ALU.add),
                             r=[("ps", 1 + hh), "a2b_bias"], w=[("a2b", hh)])
                        T.op(DVE, lambda sl=sl: nc.vector.scalar_tensor_tensor(out=a2b[:, sl], in0=a2b[:, sl], scalar=1.0, in1=n2rb[:, sl], op0=ALU.add, op1=ALU.mult),
                             r=[("a2b", hh), "n2rb"], w=[("a2b", hh)])
        T.op(DVE, lambda: nc.vector.tensor_tensor(
            out=modc[:], in0=PS[0][:, 0:96].rearrange("p (m two) -> p m two", two=2)[:, :, 0], in1=bcol[:], op=ALU.add),
            r=[("ps", 0), "bcol"], w=["modc"])
        for gi, (gt, b0) in enumerate(((g1b, 1), (g2b, 3))):
            if gi == 0:
                continue
            for hh in range(2):
                T.op(DVE, lambda gt=gt, b0=b0, hh=hh, gi=gi: nc.vector.tensor_tensor(
                    out=gt[:, hh * 512:(hh + 1) * 512], in0=PS[b0 + hh][:, :],
                    in1=brow[:, gi * 1024 + hh * 512: gi * 1024 + (hh + 1) * 512], op=ALU.add),
                    r=[("ps", b0 + hh), f"brow{gi}"], w=[(f"g{gi + 1}b", hh)])
        for (av, sc0, nwo, akey) in ((a1, 8, 0, "a1"), (a2, 32, 8, "a2")):
            T.op(DVE, lambda sc0=sc0: nc.vector.tensor_scalar(out=tmp8[:], in0=modc[:, sc0:sc0 + 8], scalar1=1.0, scalar2=None, op0=ALU.add),
                 r=["modc"], w=["tmp8"])
            T.op(DVE, lambda av=av, nwo=nwo: nc.vector.tensor_tensor(out=av[:], in0=tmp8[:], in1=nw[:, nwo:nwo + 8], op=ALU.mult),
                 r=["tmp8", "nw0", "nw1"], w=[akey])
        T.op(DVE, lambda: nc.vector.tensor_scalar(out=smallc[:, 2:3], in0=smallc[:, 2:3], scalar1=0.8, scalar2=None, op0=ALU.mult),
             r=["sc2"], w=["sc2"])
        T.op(DVE, lambda: nc.vector.tensor_reduce(out=l2[:, 0:2], in_=qkrow[:].rearrange("p (a b) -> p a b", a=2), axis=AX.X, op=ALU.max,
                                                  apply_absolute_value=True), r=["qkrow"], w=["l2a"])
        T.op(DVE, lambda: nc.vector.tensor_tensor(out=l2[:, 2:3], in0=l2[:, 0:1], in1=l2[:, 1:2], op=ALU.mult), r=["l2a"], w=["l2b"])
        T.op(DVE, lambda: nc.vector.tensor_scalar(out=smallc[:, 3:4], in0=l2[:, 2:3], scalar1=-8.0, scalar2=None, op0=ALU.mult),
             r=["l2b"], w=["sc3"])
        lv = lamb[:].rearrange("p (a b) -> p a b", a=4)
        T.op(DVE, lambda: nc.vector.tensor_tensor(out=lt[:].rearrange("p (a b) -> p a b", a=2), in0=lv[:, 0:4:2, :], in1=lv[:, 1:4:2, :], op=ALU.mult),
             r=["lamb"], w=["lt"])
        T.op(DVE, lambda: nc.vector.tensor_reduce(out=l2[:, 0:2], in_=lt[:].rearrange("p (a b) -> p a b", a=2), axis=AX.X, op=ALU.add),
             r=["lt", "l2a", "l2b"], w=["l2a"])
        T.op(ACT, lambda: nc.scalar.activation(out=l2[:, 2:4], in_=l2[:, 0:2], func=AF.Exp), r=["l2a"], w=["l2b"])
        T.op(DVE, lambda: nc.vector.tensor_tensor(out=l2[:, 0:1], in0=l2[:, 3:4], in1=l2[:, 2:3], op=ALU.subtract), r=["l2b"], w=["l2a"])
        T.op(DVE, lambda: nc.vector.tensor_scalar(out=smallc[:, 4:5], in0=l2[:, 0:1], scalar1=-0.2, scalar2=None, op0=ALU.add),
             r=["l2a"], w=["sc4"])
        T.barrier(full=False)
    if dbg and "modc" in dbg_out:
        T.dma(SP, dbg_out["modc"][:, :], modc[:], r=["modc"])
        T.dma(SP, dbg_out["g1b"][:, :], g1b[:], r=["g1b"])
        T.dma(SP, dbg_out["smallc"][:, :], smallc[:], r=["sc0"])

    def dump_dram(dst, src, rows, cols, dt):
        with nc.sbuf_tensor("s_dump_" + dst.name.replace(".", "_"), [128, cols], dt) as tmp:
            for r0 in range(0, rows, 128):
                n = min(128, rows - r0)
                T.dma(SP, tmp[0:n, :], src[r0:r0 + n, :], w=["dumptmp"])
                T.dma(SP, dst[r0:r0 + n, :], tmp[0:n, :], r=["dumptmp"], w=[("dumpdst", r0)])
            T.barrier()

    def finish():
        if _scope[0] is not None:
            _scope[0].__exit__(None, None, None)
            _scope[0] = None
        T.barrier()
        if early_open[0]:
            early.close()
            early_open[0] = False
        if early2_open[0]:
            early2.close()
            early2_open[0] = False
        glob.close()
        return nc

    if stage <= 0:
        return finish()

    s1 = modc[:, 0:8]
    s2 = modc[:, 24:32]

    scope_mark("ph1")
    def norm_tile(ph_bufs, src_ap, rkeys, tag):
        ss, sd, junk = ph_bufs["ss"], ph_bufs["sd"], ph_bufs["junk"]
        T.op(ACT, lambda: nc.scalar.activation(out=junk[:], in_=src_ap, func=AF.Square, accum_out=ss[:]), r=rkeys, w=["junk", "ss"])
        T.op(ACT, lambda: nc.scalar.activation(out=sd[:], in_=ss[:], func=AF.Ln, bias=ph_bufs["epsc"][:], scale=1.0 / D), r=["ss"], w=["sd"])
        T.op(ACT, lambda: nc.scalar.activation(out=ph_bufs["rs"][:], in_=sd[:], func=AF.Exp, scale=-0.5), r=["sd"], w=["rs"])

    with ExitStack() as ph:
        def sb(name, shape, dt):
            return ph.enter_context(nc.sbuf_tensor("s_" + name, list(shape), dt))
        xt = [sb(f"xt{i}", [128, D], F32) for i in range(2)]
        xh = [sb(f"xh{i}", [128, D], BF16) for i in range(2)]
        hTb = [sb(f"hTb{i}", [128, 8, 512], BF16) for i in range(2)]
        bufs = dict(ss=sb("ss", [128, 1], F32), sd=sb("sd", [128, 1], F32), rs=sb("rs", [128, 1], F32),
                    junk=sb("junk", [128, D], BF16), epsc=sb("epsc", [128, 1], F32))
        T.op(DVE, lambda: nc.vector.memset(bufs["epsc"][:], EPS), w=["epsc"])
        pass
        for t in range(int(os.environ.get('K1_TILES', NT))):
            b = t % 2
            blk, tl = t // 4, t % 4
            hb = blk % 2
            T.dma(SP, xt[b][:], x_in[t * 128:(t + 1) * 128, :], w=[("xt", b)])
            KO = int(os.environ.get('K1_OPS', 9))
            if KO < 1:
                continue
            norm_tile(bufs, xt[b][:], [("xt", b), "epsc"], "n1")
            if KO < 2:
                continue
            T.op(DVE, lambda b=b: nc.vector.tensor_scalar(out=xh[b][:], in0=xt[b][:], scalar1=bufs["rs"][:], scalar2=None, op0=ALU.mult),
                 r=[("xt", b), "rs"], w=[("xh", b)])
            if KO < 3:
                continue
            pb = t % 2
            pv = psb(pb).rearrange("p (k t) -> p k t", k=8)
            for kc in range(8):
                T.op(PE, lambda kc=kc, b=b, pv=pv: nc.tensor.transpose(pv[:, kc, :], xh[b][:, kc * 128:(kc + 1) * 128], ident_b),
                     r=[("xh", b), "cstb"], w=[("ps", pb)])
            if KO < 4:
                continue
            for kc in range(8):
                T.op(ACT, lambda kc=kc, pv=pv, hb=hb, tl=tl: nc.scalar.activation(
                    out=hTb[hb][:, kc, tl * 128:(tl + 1) * 128], in_=pv[:, kc, :], func=AF.Identity,
                    **({} if os.environ.get('K1_NOAP') == '1' else ({'scale': a1[:, kc:kc + 1]} if os.environ.get('K1_NOAP') == '2' else
                        ({'bias': s1[:, kc:kc + 1]} if os.environ.get('K1_NOAP') == '3' else dict(scale=a1[:, kc:kc + 1], bias=s1[:, kc:kc + 1]))))),
                    r=[("ps", pb), "a1", "modc"], w=[("hTb", hb, tl)])
            if tl == 3:
                T.dma(SP, xnT_v[:, :, blk * 512:(blk + 1) * 512], hTb[hb][:], r=[("hTb", hb)], w=[("xnT", blk)])
        T.barrier()
    early.close()
    early_open[0] = False
    if dbg and "xnT" in dbg_out:
        dump_dram(dbg_out["xnT"], xnT_d, 1024, S, BF16)
    if stage <= 1:
        return finish()

    scope_mark("phA")
    persist = ExitStack()

    def sbp(name, shape, dt):
        return persist.enter_context(nc.sbuf_tensor("s_" + name, list(shape), dt))

    with ExitStack() as ph:
        def sb(name, shape, dt):
            return ph.enter_context(nc.sbuf_tensor("s_" + name, list(shape), dt))
        wqkv = sb("wqkv", [128, 8, 1536], BF16)
        wgt = sb("wgt", [128, 8, 8], BF16)
        bg = sb("bg", [4, 2], F32)
        nbf = sb("nbf", [4, 1], F32)
        epsc = sb("epscA", [128, 1], F32)
        gst = [[sb(f"gst{g}_{i}", [4, 512], F32) for i in range(2)] for g in range(2)]
        kT = sb("kT", [128, 4, S], BF16)
        v1 = sb("v1", [128, NT, 4, 128], BF16)
        xb = [sb(f"xbA{i}", [128, 8, 512], BF16) for i in range(2)]
        qT = sb("qT", [128, 4, 512], BF16)
        hdTb = sb("hdTb", [128, 4, 512], BF16)
        Pb = [[sb(f"P{m}_{i}", [128, 512], BF16) for i in range(2)] for m in range(2)]
        sqb = [sb(f"sqb{i}", [128, 512], BF16) for i in range(2)]
        qsf = [sb(f"qsf{i}", [128, 512], F32) for i in range(2)]
        sdf = [sb(f"sdf{i}", [128, 512], F32) for i in range(2)]
        qnb = [sb(f"qnb{i}", [128, 512], BF16) for i in range(2)]
        t1f = [sb(f"t1f{i}", [128, 512], F32) for i in range(2)]
        t2f = [sb(f"t2f{i}", [128, 512], F32) for i in range(2)]
        fo = [sb(f"fo{i}", [128, 512], F32) for i in range(4)]
        OL = [qsf[0], qsf[1], sdf[0], sdf[1]]
        OLK = [("qsf", 0), ("qsf", 1), ("sdf", 0), ("sdf", 1)]
        sqf = sqb[0]
        pend_fin = []

        T.op(DVE, lambda: nc.vector.memset(epsc[:], EPS), w=["epscA"])
        T.dma(POOL, wqkv[:, 0:4, :], w_in_v[:, 0:4, 2056:3592], w=[("wqkv", 0)])
        T.dma(POOL, wqkv[:, 4:8, :], w_in_v[:, 4:8, 2056:3592], w=[("wqkv", 1)])
        T.dma(POOL, wgt[:], w_in_v[:, :, 2048:2056], w=["wgt"])
        T.dma(SP, bg[:], bgate_in[:, :], w=["bg"])
        T.op(DVE, lambda: nc.vector.tensor_scalar(out=nbf[:], in0=bg[:, 1:2], scalar1=-1.0, scalar2=None, op0=ALU.mult), r=["bg"], w=["nbf"])

        negc = smallc[:, 3:4]
        neglam = smallc[:, 4:5]
        subw8 = smallc[:, 2:3]

        for blk in range(NB):
            xbb = blk % 2
            bs = slice(blk * 512, (blk + 1) * 512)
            T.dma(SP, xb[xbb][:], xnT_v[:, :, bs], w=[("xbA", xbb)])
            def emit_v(tl):
                t = blk * 4 + tl
                bk = 4 + tl
                for kc in range(8):
                    T.op(PE, lambda kc=kc: nc.tensor.matmul(
                        PS[bk][:, :], lhsT=xb[xbb][:, kc, tl * 128:(tl + 1) * 128], rhs=wqkv[:, kc, 1024:1536], start=(kc == 0), stop=(kc == 7)),
                        r=[("wqkv",), ("xbA", xbb)], w=[("ps", bk)])
                T.op(ACT, lambda: nc.scalar.copy(out=v1[:, t, :, :].rearrange("p h d -> p (h d)"), in_=PS[bk][:, :]),
                     r=[("ps", bk)], w=[("v1", t)])

            def emit_gate(gi):
                bk = 4 + gi
                for kc in range(8):
                    T.op(PE, lambda kc=kc: nc.tensor.matmul(
                        PS[bk][0:4, :], lhsT=wgt[:, kc, 4 * gi:4 * gi + 4], rhs=xb[xbb][:, kc, :], start=(kc == 0), stop=(kc == 7)),
                        r=["wgt", ("xbA", xbb)], w=[("ps", bk)])
                if gi == 0:
                    T.op(ACT, lambda: nc.scalar.activation(out=gst[0][xbb][:], in_=PS[bk][0:4, :], func=AF.Identity, bias=bg[:, 0:1]),
                         r=[("ps", bk), "bg"], w=[("gst", 0, xbb)])
                    T.dma(SP, gi_d[:, bs], gst[0][xbb][:], r=[("gst", 0, xbb)], w=[("gi_d", blk)])
                else:
                    T.op(ACT, lambda: nc.scalar.activation(out=gst[1][xbb][:], in_=PS[bk][0:4, :], func=AF.Exp, bias=nbf[:], scale=-1.0),
                         r=[("ps", bk), "nbf"], w=[("gst", 1, xbb)])
                    T.dma(SP, gf_d[:, bs], gst[1][xbb][:], r=[("gst", 1, xbb)], w=[("gf_d", blk)])

            for c in range(8):
                st = c % 2
                isq = c < 4
                hh = c % 4
                col0 = (0 if isq else 512) + hh * 128
                for kc in range(8):
                    T.op(PE, lambda kc=kc, col0=col0, st=st: nc.tensor.matmul(
                        PS[st][:, :], lhsT=wqkv[:, kc, col0:col0 + 128], rhs=xb[xbb][:, kc, :], start=(kc == 0), stop=(kc == 7)),
                        r=[("wqkv",), ("xbA", xbb)], w=[("ps", st)])
                wcol = smallc[:, 0:1] if isq else smallc[:, 1:2]
                T.op(ACT, lambda st=st: nc.scalar.activation(out=sqb[st][:], in_=PS[st][:, :], func=AF.Square), r=[("ps", st)], w=[("sqb", st)])
                T.op(ACT, lambda st=st, wcol=wcol: nc.scalar.activation(out=qsf[st][:], in_=PS[st][:, :], func=AF.Identity, scale=wcol),
                     r=[("ps", st), "sc0", "sc1"], w=[("qsf", st)])
                if c < 4:
                    emit_v(c)
                elif c < 6:
                    emit_gate(c - 4)
                T.op(PE, lambda st=st: nc.tensor.matmul(PS[2 + st][:, :], lhsT=bo64_b, rhs=sqb[st][:], start=True, stop=True),
                     r=[("sqb", st), "cstb"], w=[("ps", 2 + st)])
                T.op(ACT, lambda st=st: nc.scalar.activation(out=sdf[st][:], in_=PS[2 + st][:, :], func=AF.Ln, bias=epsc[:], scale=1.0 / 64),
                     r=[("ps", 2 + st), "epscA"], w=[("sdf", st)])
                T.op(ACT, lambda st=st: nc.scalar.activation(out=sdf[st][:], in_=sdf[st][:], func=AF.Exp, scale=-0.5),
                     r=[("sdf", st)], w=[("sdf", st)])
                T.op(DVE, lambda st=st: nc.vector.tensor_tensor(out=qnb[st][:], in0=qsf[st][:], in1=sdf[st][:], op=ALU.mult),
                     r=[("qsf", st), ("sdf", st)], w=[("qnb", st)])
                T.op(PE, lambda st=st: nc.tensor.matmul(PS[2 + st][:, :], lhsT=RT_b, rhs=qnb[st][:], start=True, stop=True),
                     r=[("qnb", st), "cstb"], w=[("ps", 2 + st)])
                T.op(POOL, lambda st=st: nc.gpsimd.tensor_tensor(out=t1f[st][:], in0=qnb[st][:], in1=CosB[:, bs], op=ALU.mult),
                     r=[("qnb", st), "CosB"], w=[("t1f", st)])
                T.op(DVE, lambda st=st: nc.vector.tensor_tensor(out=t2f[st][:], in0=PS[2 + st][:, :], in1=SinB[:, bs], op=ALU.mult),
                     r=[("ps", 2 + st), "SinB"], w=[("t2f", st)])
                if isq:
                    dst, dk = qT[:, hh, :], ("qT", hh)
                else:
                    dst, dk = kT[:, hh, bs], ("kT", hh, blk)
                T.op(POOL, lambda st=st, dst=dst: nc.gpsimd.tensor_tensor(out=dst, in0=t1f[st][:], in1=t2f[st][:], op=ALU.add),
                     r=[("t1f", st), ("t2f", st)], w=[dk])
            for hh in range(4):
                nkt = blk * 4 + 4
                prev = None

                def pv_step(kt, c0, pbuf):
                    first = (kt == 0)
                    last = (kt == nkt - 1)
                    for m in range(2):
                        T.op(PE, lambda m=m: nc.tensor.matmul(
                            PS[4 + 2 * m][:, c0:512], lhsT=v1[:, kt, hh, :], rhs=Pb[m][pbuf][:, c0:512], start=first, stop=last,
                            skip_group_check=True), r=[("v1", kt), ("P", m, pbuf)], w=[("ps", 4 + 2 * m)])
                        T.op(PE, lambda m=m: nc.tensor.matmul(
                            PS[5 + 2 * m][:, c0:512], lhsT=ones_b, rhs=Pb[m][pbuf][:, c0:512], start=first, stop=last,
                            skip_group_check=True), r=["cstb", ("P", m, pbuf)], w=[("ps", 5 + 2 * m)])

                for kt in range(nkt):
                    ktl = kt - blk * 4
                    c0 = ktl * 128 if ktl > 0 else 0
                    pbuf = kt % 2
                    sbk = 2 * (kt % 2)
                    for m in range(2):
                        T.op(PE, lambda m=m, kt=kt, c0=c0, sbk=sbk: nc.tensor.matmul(
                            PS[sbk + m][:, c0:512], lhsT=kT[64 * m:64 * m + 64, hh, kt * 128:(kt + 1) * 128],
                            rhs=qT[64 * m:64 * m + 64, hh, c0:512], start=True, stop=True),
                            r=[("kT", hh, kt // 4), ("qT", hh)], w=[("ps", sbk + m)])
                    if prev is not None:
                        pv_step(*prev)
                    for m in range(2):
                        T.op(ACT, lambda m=m, c0=c0, pbuf=pbuf, sbk=sbk: nc.scalar.activation(
                            out=Pb[m][pbuf][:, c0:512], in_=PS[sbk + m][:, c0:512], func=AF.Exp, bias=negc, scale=0.125),
                            r=[("ps", sbk + m), "sc3"], w=[("P", m, pbuf)])
                        if ktl >= 0:
                            T.op(POOL, lambda m=m, c0=c0, pbuf=pbuf: nc.gpsimd.affine_select(
                                out=Pb[m][pbuf][:, c0:c0 + 128], in_=Pb[m][pbuf][:, c0:c0 + 128], pattern=[[1, 128]],
                                compare_op=ALU.is_ge, fill=0.0, base=0, channel_multiplier=-1),
                                r=[("P", m, pbuf)], w=[("P", m, pbuf)])
                    prev = (kt, c0, pbuf)
                    if pend_fin:
                        T.op(*pend_fin.pop(0))
                pv_step(*prev)
                for rec_ in pend_fin:
                    T.op(*rec_)
                pend_fin.clear()

                def finalize_ops(hh):
                    T.op(ACT, lambda: nc.scalar.copy(out=OL[0][:], in_=PS[4][:, :]), r=[("ps", 4)], w=[OLK[0]])
                    T.op(DVE, lambda: nc.vector.tensor_copy(out=OL[1][:], in_=PS[5][:, :]), r=[("ps", 5)], w=[OLK[1]])
                    T.op(ACT, lambda: nc.scalar.copy(out=OL[2][:], in_=PS[6][:, :]), r=[("ps", 6)], w=[OLK[2]])
                    T.op(DVE, lambda: nc.vector.tensor_copy(out=OL[3][:], in_=PS[7][:, :]), r=[("ps", 7)], w=[OLK[3]])
                    T.start_record()
                    T.op(ACT, lambda: nc.scalar.activation(out=fo[0][:], in_=OL[1][:], func=AF.Ln), r=[OLK[1]], w=[("fo", 0)])
                    T.op(ACT, lambda: nc.scalar.activation(out=fo[0][:], in_=fo[0][:], func=AF.Exp, scale=-1.0), r=[("fo", 0)], w=[("fo", 0)])
                    T.op(DVE, lambda: nc.vector.tensor_tensor(out=fo[1][:], in0=OL[0][:], in1=fo[0][:], op=ALU.mult),
                         r=[OLK[0], ("fo", 0)], w=[("fo", 1)])
                    T.op(ACT, lambda: nc.scalar.activation(out=fo[2][:], in_=OL[3][:], func=AF.Ln), r=[OLK[3]], w=[("fo", 2)])
                    T.op(ACT, lambda: nc.scalar.activation(out=fo[0][:], in_=fo[2][:], func=AF.Exp, scale=-1.0), r=[("fo", 2), ("fo", 0)], w=[("fo", 0)])
                    T.op(DVE, lambda: nc.vector.tensor_tensor(out=fo[2][:], in0=OL[2][:], in1=fo[0][:], op=ALU.mult),
                         r=[OLK[2], ("fo", 0)], w=[("fo", 2)])
                    T.op(DVE, lambda: nc.vector.scalar_tensor_tensor(out=fo[3][:], in0=fo[2][:], scalar=neglam, in1=fo[1][:], op0=ALU.mult, op1=ALU.add),
                         r=[("fo", 1), ("fo", 2), "sc4"], w=[("fo", 3)])
                    T.op(ACT, lambda: nc.scalar.activation(out=sqf[:], in_=fo[3][:], func=AF.Square), r=[("fo", 3)], w=[("sqb", 0)])
                    T.op(PE, lambda: nc.tensor.matmul(PS[0][:, :], lhsT=ones_b, rhs=sqf[:], start=True, stop=True),
                         r=[("sqb", 0), "cstb"], w=[("ps", 0)])
                    T.op(ACT, lambda: nc.scalar.activation(out=fo[0][:], in_=PS[0][:, :], func=AF.Ln, bias=epsc[:], scale=1.0 / 128),
                         r=[("ps", 0), "epscA"], w=[("fo", 0)])
                    T.op(ACT, lambda: nc.scalar.activation(out=fo[0][:], in_=fo[0][:], func=AF.Exp, scale=-0.5), r=[("fo", 0)], w=[("fo", 0)])
                    T.op(DVE, lambda: nc.vector.scalar_tensor_tensor(out=hdTb[:, hh, :], in0=fo[3][:], scalar=subw8, in1=fo[0][:], op0=ALU.mult, op1=ALU.mult),
                         r=[("fo", 3), ("fo", 0), "sc2"], w=[("hdTb", hh)])
                    return T.stop_record()

                pend_fin.extend(finalize_ops(hh))
            for rec_ in pend_fin:
                T.op(*rec_)
            pend_fin.clear()
            T.dma(SP, hdT_v[:, :, bs], hdTb[:], r=[("hdTb",)], w=[("hdT", blk)])
        T.barrier()
    early2.close()
    early2_open[0] = False
    if dbg and "hdT" in dbg_out:
        dump_dram(dbg_out["hdT"], hdT_d, 512, S, BF16)
        dump_dram(dbg_out["irow"], gi_d, 4, S, F32)
        dump_dram(dbg_out["frow"], gf_d, 4, S, F32)
    if stage <= 2:
        persist.close()
        return finish()

    scope_mark("phB")
    wcol = sbp("wcol", [128, NT, 8], F32)
    decb = sbp("decb", [128, 128], F32)
    with ExitStack() as ph:
        def sb(name, shape, dt):
            return ph.enter_context(nc.sbuf_tensor("s_" + name, list(shape), dt))
        irow = sb("irow", [4, S], F32)
        frow = sb("frow", [4, S], F32)
        T.dma(SP, irow[:], gi_d[:, :], w=[("irow",)])
        T.dma(SP, frow[:], gf_d[:, :], w=[("frow",)])
        onesr = sb("onesr", [4, S], F32)
        csr = sb("csr", [4, S], F32)
        nbr = sb("nbr", [4, S], F32)
        gr = sb("gr", [4, S], F32)
        pe_ = sb("pe_", [4, 33], F32)
        G = sb("G", [4, 32], F32)
        Ms = sb("Ms", [4, 32], F32)
        mp = sb("mp", [4, 33], F32)
        dec = sb("dec", [4, 32], F32)
        T.op(ACT, lambda: nc.scalar.activation(out=frow[:], in_=frow[:], func=AF.Ln, bias=1.0), r=[("frow",)], w=[("frow",)])
        T.op(DVE, lambda: nc.vector.memset(onesr[:], 1.0), w=["onesr"])
        T.op(DVE, lambda: nc.vector.tensor_tensor_scan(out=csr[:], data0=onesr[:], data1=frow[:], initial=0.0, op0=ALU.mult, op1=ALU.add),
             r=["onesr", ("frow",)], w=["csr"])
        T.op(DVE, lambda: nc.vector.memset(pe_[:, 0:1], 0.0), w=[("pe_", 0)])
        T.op(DVE, lambda: nc.vector.tensor_copy(out=pe_[:, 1:33], in_=csr[:].rearrange("p (j s) -> p j s", s=128)[:, :, 127]),
             r=["csr"], w=[("pe_", 1)])
        T.op(DVE, lambda: nc.vector.tensor_tensor(out=nbr[:].rearrange("p (j s) -> p j s", s=128), in0=csr[:].rearrange("p (j s) -> p j s", s=128),
                                                  in1=pe_[:, 0:32].unsqueeze(2).to_broadcast([4, 32, 128]), op=ALU.subtract),
             r=["csr", ("pe_",)], w=["nbr"])
        T.op(DVE, lambda: nc.vector.tensor_tensor(out=gr[:], in0=irow[:], in1=nbr[:], op=ALU.add), r=[("irow",), "nbr"], w=["gr"])
        T.op(DVE, lambda: nc.vector.tensor_reduce(out=G[:], in_=gr[:].rearrange("p (j s) -> p j s", s=128), axis=AX.X, op=ALU.max),
             r=["gr"], w=["G"])
        T.op(DVE, lambda: nc.vector.memset(mp[:, 0:1], 0.0), w=[("mp", 0)])
        nbl = nbr[:].rearrange("p (j s) -> p j s", s=128)[:, :, 127]
        for j in range(NT):
            T.op(DVE, lambda j=j: nc.vector.tensor_tensor(out=Ms[:, j:j + 1], in0=mp[:, j:j + 1], in1=G[:, j:j + 1], op=ALU.max),
                 r=[("mp", j), "G"], w=[("Ms", j)])
            T.op(DVE, lambda j=j: nc.vector.tensor_tensor(out=mp[:, j + 1:j + 2], in0=Ms[:, j:j + 1], in1=nbl[:, j:j + 1], op=ALU.subtract),
                 r=[("Ms", j), "nbr"], w=[("mp", j + 1)])
        Msb = Ms[:].unsqueeze(2).to_broadcast([4, 32, 128])
        T.op(DVE, lambda: nc.vector.tensor_tensor(out=gr[:].rearrange("p (j s) -> p j s", s=128), in0=gr[:].rearrange("p (j s) -> p j s", s=128),
                                                  in1=Msb, op=ALU.subtract), r=["gr", ("Ms",)], w=["gr"])
        T.op(DVE, lambda: nc.vector.tensor_tensor(out=nbr[:].rearrange("p (j s) -> p j s", s=128), in0=nbr[:].rearrange("p (j s) -> p j s", s=128),
                                                  in1=Msb, op=ALU.subtract), r=["nbr", ("Ms",)], w=["nbr"])
        T.op(ACT, lambda: nc.scalar.activation(out=gr[:], in_=gr[:], func=AF.Exp), r=["gr"], w=["gr"])
        T.op(ACT, lambda: nc.scalar.activation(out=nbr[:], in_=nbr[:], func=AF.Exp), r=["nbr"], w=["nbr"])
        T.op(DVE, lambda: nc.vector.tensor_scalar(out=gr[:], in0=gr[:], scalar1=128.0 ** -0.5, scalar2=None, op0=ALU.mult), r=["gr"], w=["gr"])
        T.op(DVE, lambda: nc.vector.tensor_tensor(out=dec[:], in0=mp[:, 0:32], in1=Ms[:], op=ALU.subtract), r=[("mp",), ("Ms",)], w=["dec"])
        T.op(ACT, lambda: nc.scalar.activation(out=dec[:], in_=dec[:], func=AF.Exp), r=["dec"], w=["dec"])
        T.dma(SP, dec_d[:, :], dec[:], r=["dec"], w=["dec_d"])
        T.dma(SP, decb[:], dec_d.rearrange("h j -> (h j)").unsqueeze(0).partition_broadcast(128), r=["dec_d"], w=["decb"])
        wv = PS[0][:, 0:256].rearrange("p (t e) -> p t e", e=8)
        for t in range(NT):
            T.op(PE, lambda t=t: nc.tensor.transpose(wv[:, t, 0:4], gr[:, t * 128:(t + 1) * 128], ident_f[0:4, 0:4]),
                 r=["gr", "cst"], w=[("ps", 0, t, 0)])
            T.op(PE, lambda t=t: nc.tensor.transpose(wv[:, t, 4:8], nbr[:, t * 128:(t + 1) * 128], ident_f[0:4, 0:4]),
                 r=["nbr", "cst"], w=[("ps", 0, t, 1)])
        T.op(DVE, lambda: nc.vector.tensor_copy(out=wcol[:], in_=wv), r=[("ps", 0)], w=["wcol"])
        T.barrier()
    if dbg and "wcol" in dbg_out:
        T.dma(SP, dbg_out["wcol"][:, :], wcol[:].rearrange("p t e -> p (t e)"), r=["wcol"])
        T.dma(SP, dbg_out["decb"][:, :], decb[:], r=["decb"])
    if stage <= 3:
        persist.close()
        return finish()

    scope_mark("phC")
    with ExitStack() as ph:
        def sb(name, shape, dt):
            return ph.enter_context(nc.sbuf_tensor("s_" + name, list(shape), dt))
        wm = sb("wm", [128, 8, 2048], BF16)
        cw = sb("cw", [128, 8, 4], F32)
        cbias = sb("cbias", [128, 8], F32)
        dg = sb("dg", [128, 8, 4, 128], BF16)
        mnwb = sb("mnwb", [128, 512], F32)
        epsc = sb("epscC", [128, 1], F32)
        xb = [sb(f"xbC{i}", [128, 8, 512], BF16) for i in range(2)]
        pre = sb("pre", [128, 8, 516], BF16)
        qkc = sb("qkc", [128, 8, 512], BF16)
        vm1 = [sb(f"vm1_{i}", [128, 4, 130], BF16) for i in range(2)]
        so = [sb(f"so{i}", [128, 512], F32) for i in range(3)]
        pend_tail = []
        ST = [sb(f"ST{i}", [128, 128], BF16) for i in range(4)]
        kw = [sb(f"kw{i}", [128, 128], BF16) for i in range(4)]
        Cst = sb("Cst", [128, 4, 130], F32)
        Cd = sb("Cd", [128, 4, 130], BF16)
        hbuf = sb("hbuf", [128, 512], F32)
        sqh = sb("sqh", [128, 512], F32)
        hmb = sb("hmb", [128, 512], BF16)
        hmTb = [sb(f"hmTb{i}", [128, 4, 512], BF16) for i in range(2)]
        dn = sb("dn", [128, 8], F32)
        ssh = sb("ssh", [128, 8], F32)

        T.op(DVE, lambda: nc.vector.memset(epsc[:], EPS), w=["epscC"])
        T.dma(POOL, wm[:, 0:4, :], w_in_v[:, 0:4, 0:2048], w=[("wm", 0)], max_dma_last_dim=8192)
        T.dma(POOL, wm[:, 4:8, :], w_in_v[:, 4:8, 0:2048], w=[("wm", 1)], max_dma_last_dim=8192)
        T.dma(SP, cw[:], convw_in[:, :, :], w=["cw"])
        T.dma(SP, cbias[:], convb_in[:, :], w=["cbias"])
        T.dma(SP, mnwb[:], mnw_in[0:1, :].partition_broadcast(128), w=["mnwb"])
        for c in range(8):
            for j in range(4):
                T.op(DVE, lambda c=c, j=j: nc.vector.tensor_scalar(out=dg[:, c, j, :], in0=ident_f, scalar1=cw[:, c, j:j + 1], scalar2=None, op0=ALU.mult),
                     r=["cw", "cst"], w=[("dg", c, j)])
        T.op(DVE, lambda: nc.vector.memset(pre[:, :, 0:4], 0.0), w=[("pre", "halo")])
        T.op(DVE, lambda: nc.vector.memset(Cst[:], 0.0), w=["Cst"])
        T.op(DVE, lambda: nc.vector.memset(Cd[:], 0.0), w=["Cd"])
        for i in range(2):
            T.op(DVE, lambda i=i: nc.vector.memset(vm1[i][:, :, 128:130], 1.0), w=[("vm1", i)])

        for blk in range(NB):
            xbb = blk % 2
            bs = slice(blk * 512, (blk + 1) * 512)
            T.dma(SP, xb[xbb][:], xnT_v[:, :, bs], w=[("xbC", xbb)])
            for c in range(8):
                pb = c % 2
                for kc in range(8):
                    T.op(PE, lambda kc=kc, c=c, pb=pb: nc.tensor.matmul(
                        PS[pb][:, :], lhsT=wm[:, kc, c * 128:(c + 1) * 128], rhs=xb[xbb][:, kc, :], start=(kc == 0), stop=(kc == 7)),
                        r=[("wm",), ("xbC", xbb)], w=[("ps", pb)])
                if blk > 0:
                    T.op(DVE, lambda c=c: nc.vector.tensor_copy(out=pre[:, c, 1:4], in_=pre[:, c, 513:516]),
                         r=[("pre", c)], w=[("pre", "halo", c)])
                T.op(ACT, lambda c=c, pb=pb: nc.scalar.copy(out=pre[:, c, 4:516], in_=PS[pb][:, :]),
                     r=[("ps", pb), ("pre", "halo", c)], w=[("pre", c)])
                cb2 = 2 + c % 2
                for j in range(4):
                    T.op(PE, lambda c=c, j=j, cb2=cb2: nc.tensor.matmul(
                        PS[cb2][:, :], lhsT=dg[:, c, j, :], rhs=pre[:, c, 1 + j:513 + j], start=(j == 0), stop=(j == 3)),
                        r=[("dg", c), ("pre", c), ("pre", "halo", c)], w=[("ps", cb2)])
                T.op(ACT, lambda c=c, cb2=cb2: nc.scalar.activation(out=qkc[:, c, :], in_=PS[cb2][:, :], func=AF.Silu, bias=cbias[:, c:c + 1]),
                     r=[("ps", cb2), "cbias"], w=[("qkc", c)])
            for tl in range(4):
                t = blk * 4 + tl
                vb = t % 2
                ts_ = slice(tl * 128, (tl + 1) * 128)
                def emit_vo(tl_):
                    t_ = blk * 4 + tl_
                    vb_ = t_ % 2
                    sl_ = slice(tl_ * 128, (tl_ + 1) * 128)
                    for half in range(2):
                        for kc in range(8):
                            T.op(PE, lambda kc=kc, half=half: nc.tensor.matmul(
                                PS[4 + half][:, :], lhsT=xb[xbb][:, kc, sl_], rhs=wm[:, kc, 1024 + half * 512:1536 + half * 512],
                                start=(kc == 0), stop=(kc == 7)), r=[("wm",), ("xbC", xbb)], w=[("ps", 4 + half)])
                    T.op(ACT, lambda: nc.scalar.copy(out=vm1[vb_][:, :, 0:128], in_=PS[4][:, :].rearrange("p (h d) -> p h d", h=4)),
                         r=[("ps", 4)], w=[("vm1", vb_)])
                    T.op(ACT, lambda: nc.scalar.activation(out=so[t_ % 3][:], in_=PS[5][:, :], func=AF.Sigmoid), r=[("ps", 5)], w=[("so", t_ % 3)])

                if tl == 0:
                    emit_vo(0)
                if tl < 3:
                    emit_vo(tl + 1)
                def head_ops(hh):
                    wc = wcol[:, t, hh:hh + 1]
                    cc = wcol[:, t, 4 + hh:5 + hh]
                    dcol = decb[:, hh * 32 + t:hh * 32 + t + 1]
                    if hh % 2 == 0:
                        bA, bK, bU, bN = 6, 1, 7, 0
                    else:
                        bA, bK, bU, bN = 2, 3, 4, 5
                    kps = psb(bK)[:, 0:128]
                    ops = []
                    ops.append(lambda: T.op(PE, lambda: nc.tensor.matmul(PS[bA][:, 0:128], lhsT=qkc[:, 4 + hh, ts_], rhs=qkc[:, hh, ts_], start=True, stop=True),
                                            r=[("qkc", 4 + hh), ("qkc", hh)], w=[("ps", bA)]))
                    ops.append(lambda: T.op(DVE, lambda: nc.vector.scalar_tensor_tensor(
                        out=ST[hh][:], in0=PS[bA][:, 0:128], scalar=wc, in1=tri_f, op0=ALU.mult, op1=ALU.mult),
                        r=[("ps", bA), "wcol", "cst"], w=[("ST", hh)]))
                    ops.append(lambda: T.op(PE, lambda: nc.tensor.transpose(kps, qkc[:, 4 + hh, ts_], ident_b),
                                            r=[("qkc", 4 + hh), "cstb"], w=[("ps", bK)]))
                    ops.append(lambda: T.op(DVE, lambda: nc.vector.tensor_scalar(out=kw[hh][:], in0=kps, scalar1=wc, scalar2=None, op0=ALU.mult),
                                            r=[("ps", bK), "wcol"], w=[("kw", hh)]))
                    ops.append(lambda: T.op(ACT, lambda: nc.scalar.activation(out=Cd[:, hh, :], in_=Cst[:, hh, :], func=AF.Identity, scale=dcol),
                                            r=[("Cst", hh), "decb"], w=[("Cd", hh)]))
                    ops.append(lambda: T.op(PE, lambda: nc.tensor.matmul(PS[bU][:, 0:129], lhsT=kw[hh][:], rhs=vm1[vb][:, hh, 0:129], start=True, stop=True),
                                            r=[("kw", hh), ("vm1", vb)], w=[("ps", bU)]))
                    ops.append(lambda: T.op(PE, lambda: nc.tensor.matmul(PS[bN][:, 0:129], lhsT=ST[hh][:], rhs=vm1[vb][:, hh, 0:129], start=True, stop=False),
                                            r=[("ST", hh), ("vm1", vb)], w=[("ps", bN)]))
                    ops.append(lambda: T.op(PE, lambda: nc.tensor.matmul(PS[bN][:, 0:129], lhsT=qkc[:, hh, ts_], rhs=Cd[:, hh, 0:129], start=False, stop=True),
                                            r=[("qkc", hh), ("Cd", hh)], w=[("ps", bN)]))
                    ops.append(lambda: T.op(DVE, lambda: nc.vector.scalar_tensor_tensor(
                        out=Cst[:, hh, 0:129], in0=Cst[:, hh, 0:129], scalar=dcol, in1=PS[bU][:, 0:129], op0=ALU.mult, op1=ALU.add),
                        r=[("Cst", hh), "decb", ("ps", bU)], w=[("Cst", hh)]))
                    ops.append(lambda: T.op(DVE, lambda: nc.vector.tensor_reduce(
                        out=dn[:, hh:hh + 1], in_=PS[bN][:, 128:129], axis=AX.X, op=ALU.max, apply_absolute_value=True),
                        r=[("ps", bN)], w=[("dn", hh)]))
                    ops.append(lambda: T.op(DVE, lambda: nc.vector.tensor_scalar(
                        out=dn[:, hh:hh + 1], in0=dn[:, hh:hh + 1], scalar1=cc, scalar2=None, op0=ALU.max),
                        r=[("dn", hh), "wcol"], w=[("dn", hh)]))
                    ops.append(lambda: T.op(DVE, lambda: nc.vector.reciprocal(out=dn[:, 4 + hh:5 + hh], in_=dn[:, hh:hh + 1]), r=[("dn", hh)], w=[("dn", 4 + hh)]))
                    ops.append(lambda: T.op(ACT, lambda: nc.scalar.activation(out=hbuf[:, hh * 128:(hh + 1) * 128], in_=PS[bN][:, 0:128], func=AF.Copy,
                                                                              scale=dn[:, 4 + hh:5 + hh]),
                                            r=[("ps", bN), ("dn", 4 + hh)], w=[("hbuf", hh)]))
                    return ops

                for pair in ((0, 1), (2, 3)):
                    pops = [head_ops(hh) for hh in pair]
                    for i in range(len(pops[0])):
                        for po in pops:
                            po[i]()
                        if pend_tail:
                            T.op(*pend_tail.pop(0))
                    for rec_ in pend_tail:
                        T.op(*rec_)
                    pend_tail.clear()
                T.start_record()
                T.op(DVE, lambda: nc.vector.tensor_tensor(out=sqh[:], in0=hbuf[:], in1=hbuf[:], op=ALU.mult), r=[("hbuf",)], w=["sqh"])
                T.op(DVE, lambda: nc.vector.tensor_reduce(out=ssh[:, 0:4], in_=sqh[:].rearrange("p (h d) -> p h d", h=4), axis=AX.X, op=ALU.add),
                     r=["sqh"], w=[("ssh", 0)])
                T.op(ACT, lambda: nc.scalar.activation(out=ssh[:, 4:8], in_=ssh[:, 0:4], func=AF.Sqrt, bias=epsc[:], scale=1.0 / 128),
                     r=[("ssh", 0), "epscC"], w=[("ssh", 1)])
                T.op(DVE, lambda: nc.vector.reciprocal(out=ssh[:, 4:8], in_=ssh[:, 4:8]), r=[("ssh", 1)], w=[("ssh", 1)])
                T.op(DVE, lambda: nc.vector.tensor_tensor(out=sqh[:].rearrange("p (h d) -> p h d", h=4), in0=hbuf[:].rearrange("p (h d) -> p h d", h=4),
                                                          in1=ssh[:, 4:8].unsqueeze(2).to_broadcast([128, 4, 128]), op=ALU.mult),
                     r=[("hbuf",), ("ssh", 1)], w=["sqh"])
                T.op(DVE, lambda: nc.vector.tensor_tensor(out=sqh[:], in0=sqh[:], in1=mnwb[:], op=ALU.mult), r=["sqh", "mnwb"], w=["sqh"])
                T.op(DVE, lambda t=t: nc.vector.tensor_tensor(out=hmb[:], in0=sqh[:], in1=so[t % 3][:], op=ALU.mult), r=["sqh", ("so", t % 3)], w=["hmb"])
                hb = blk % 2
                tp = psb(3).rearrange("p (k t) -> p k t", k=8)
                for hh in range(4):
                    T.op(PE, lambda hh=hh, tp=tp: nc.tensor.transpose(tp[:, hh, :], hmb[:, hh * 128:(hh + 1) * 128], ident_b),
                         r=["hmb", "cstb"], w=[("ps", 3)])
                T.op(ACT, lambda hb=hb, ts_=ts_, tp=tp: nc.scalar.copy(out=hmTb[hb][:, :, ts_], in_=tp[:, 0:4, :]), r=[("ps", 3)], w=[("hmTb", hb, tl)])
                pend_tail.extend(T.stop_record())
            for rec_ in pend_tail:
                T.op(*rec_)
            pend_tail.clear()
            T.dma(SP, hmT_v[:, :, bs], hmTb[blk % 2][:], r=[("hmTb", blk % 2)], w=[("hmT", blk)])
        T.barrier()
    persist.close()
    if dbg and "hmT" in dbg_out:
        dump_dram(dbg_out["hmT"], hmT_d, 512, S, BF16)
    if stage <= 4:
        return finish()

    scope_mark("phD")
    with ExitStack() as ph:
        def sb(name, shape, dt):
            return ph.enter_context(nc.sbuf_tensor("s_" + name, list(shape), dt))
        wg = sb("wg", [128, 8, 2048], BF16)
        wbm = sb("wbm", [128, 4, D], BF16)
        wbd = sb("wbd", [128, 4, D], BF16)
        wo = sb("wo", [128, 8, D], BF16)
        wstg = [sb(f"wstg{i}", [128, D], F32) for i in range(2)]
        wr = sb("wrt", [128, 8, 36], F32)
        brb = sb("brb", [128, 36], F32)
        xb = [sb(f"xbD{i}", [128, 8, 512], BF16) for i in range(2)]
        hmb_ = [sb(f"hmD{i}", [128, 4, 512], BF16) for i in range(2)]
        hdb_ = [sb(f"hdD{i}", [128, 4, 512], BF16) for i in range(2)]
        sg = [sb(f"sg{i}", [128, 512], F32) for i in range(4)]
        tt = [sb(f"tt{i}", [128, 512], F32) for i in range(4)]
        mT = sb("mT", [128, 8, 512], BF16)
        xt = [sb(f"xtD{i}", [128, D], F32) for i in range(2)]
        x1t = [sb(f"x1t{i}", [128, D], F32) for i in range(2)]
        xhf = sb("xhf", [128, D], F32)
        h2f = sb("h2f", [128, 8, 128], F32)
        x2t = [sb(f"x2t{i}", [128, D], BF16) for i in range(2)]
        ohb = sb("ohb", [128, 32], BF16)
        bufs = dict(ss=sb("ssD", [128, 1], F32), sd=sb("sdD", [128, 1], F32), rs=sb("rsD", [128, 1], F32),
                    junk=sb("junkD", [128, D], BF16), epsc=sb("epscD", [128, 1], F32))
        lg = sb("lg", [128, 36], F32)
        r8 = sb("r8", [128, 80], F32)

        T.op(DVE, lambda: nc.vector.memset(bufs["epsc"][:], EPS), w=["epsc"])
        T.op(DVE, lambda: nc.vector.memset(tot[:], 0.0), w=["tot"])
        T.dma(POOL, wg[:, 0:4, :], w_in_v[:, 0:4, 3592:5640], w=[("wg", 0)], max_dma_last_dim=8192)
        T.dma(POOL, wg[:, 4:8, :], w_in_v[:, 4:8, 3592:5640], w=[("wg", 1)], max_dma_last_dim=8192)
        T.dma(POOL, wbm[:], w_br_m.rearrange("(k p) n -> p k n", p=128), w=["wbm"])
        T.dma(POOL, wbd[:], w_br_d.rearrange("(k p) n -> p k n", p=128), w=["wbd"])
        T.dma(SP, wr[:], wr_in.rearrange("(k p) n -> p k n", p=128), w=["wrt"])
        T.dma(SP, brb[:], br_in[0:1, :].partition_broadcast(128), w=["brb"])
        w_out_v = w_out.rearrange("(k p) n -> p k n", p=128)
        for kc in range(8):
            b = kc % 2
            T.dma(SP, wstg[b][:], w_out_v[:, kc, :], w=[("wstg", b)])
            T.op(DVE, lambda kc=kc, b=b: nc.vector.tensor_tensor(out=wo[:, kc, :], in0=wstg[b][:], in1=g1b[:], op=ALU.mult),
                 r=[("wstg", b), "g1b"], w=[("wo", kc)])

        for blk in range(NB):
            xbb = blk % 2
            bs = slice(blk * 512, (blk + 1) * 512)
            def load_blk(bk):
                bb_ = bk % 2
                sl_ = slice(bk * 512, (bk + 1) * 512)
                T.dma(SP, xb[bb_][:], xnT_v[:, :, sl_], w=[("xbD", bb_)])
                T.dma(SP, hmb_[bb_][:], hmT_v[:, :, sl_], w=[("hmD", bb_)])
                T.dma(SP, hdb_[bb_][:], hdT_v[:, :, sl_], w=[("hdD", bb_)])

            if blk == 0:
                load_blk(0)
                T.dma(SP, xt[0][:], x_in[0:128, :], w=[("xtD", 0)])
            for oc in range(8):
                st = oc % 2
                for which, (colbase, wbr, hsrc, hkey, wkey) in enumerate(((0, wbm, hmb_, "hmD", "wbm"), (1024, wbd, hdb_, "hdD", "wbd"))):
                    gb = 0 + which
                    pbk = 2 + which
                    for kc in range(8):
                        T.op(PE, lambda kc=kc, colbase=colbase, gb=gb: nc.tensor.matmul(
                            PS[gb][:, :], lhsT=wg[:, kc, colbase + oc * 128:colbase + (oc + 1) * 128], rhs=xb[xbb][:, kc, :],
                            start=(kc == 0), stop=(kc == 7)), r=[("wg",), ("xbD", xbb)], w=[("ps", gb)])
                    T.op(ACT, lambda gb=gb, which=which: nc.scalar.activation(out=sg[2 * st + which][:], in_=PS[gb][:, :], func=AF.Sigmoid),
                         r=[("ps", gb)], w=[("sg", 2 * st + which)])
                    for kc in range(4):
                        T.op(PE, lambda kc=kc, wbr=wbr, hsrc=hsrc, pbk=pbk: nc.tensor.matmul(
                            PS[pbk][:, :], lhsT=wbr[:, kc, oc * 128:(oc + 1) * 128], rhs=hsrc[xbb][:, kc, :],
                            start=(kc == 0), stop=(kc == 3)), r=[wkey, (hkey, xbb)], w=[("ps", pbk)])
                    T.op(DVE, lambda which=which, pbk=pbk: nc.vector.tensor_tensor(out=tt[2 * st + which][:], in0=PS[pbk][:, :], in1=sg[2 * st + which][:], op=ALU.mult),
                         r=[("ps", pbk), ("sg", 2 * st + which)], w=[("tt", 2 * st + which)])
                T.op(POOL, lambda st=st: nc.gpsimd.tensor_tensor(out=mT[:, oc, :], in0=tt[2 * st][:], in1=tt[2 * st + 1][:], op=ALU.add),
                     r=[("tt", 2 * st), ("tt", 2 * st + 1)], w=[("mT", oc)])
            for tl in range(4):
                t = blk * 4 + tl
                b = t % 2
                ts_ = slice(tl * 128, (tl + 1) * 128)
                if t + 1 < NT:
                    T.dma(SP, xt[(t + 1) % 2][:], x_in[(t + 1) * 128:(t + 2) * 128, :], w=[("xtD", (t + 1) % 2)])
                if tl == 3 and blk + 1 < NB:
                    load_blk(blk + 1)
                def emit_wout(tl_):
                    sl_ = slice(tl_ * 128, (tl_ + 1) * 128)
                    for half in range(2):
                        for oc in range(8):
                            T.op(PE, lambda oc=oc, half=half: nc.tensor.matmul(
                                PS[4 + half][:, :], lhsT=mT[:, oc, sl_], rhs=wo[:, oc, half * 512:(half + 1) * 512],
                                start=(oc == 0), stop=(oc == 7)), r=[("mT", oc), ("wo",)], w=[("ps", 4 + half)])

                if tl == 0:
                    emit_wout(0)
                for half in range(2):
                    T.op(DVE, lambda half=half, b=b: nc.vector.tensor_tensor(
                        out=x1t[b][:, half * 512:(half + 1) * 512], in0=PS[4 + half][:, :], in1=xt[b][:, half * 512:(half + 1) * 512], op=ALU.add),
                        r=[("ps", 4 + half), ("xtD", b)], w=[("x1t", b, half)])
                T.dma(SP, x1_d[t * 128:(t + 1) * 128, :], x1t[b][:], r=[("x1t", b)], w=[("x1d", t)])
                if tl < 3:
                    emit_wout(tl + 1)
                norm_tile(bufs, x1t[b][:], [("x1t", b), "epsc"], "n2")
                T.op(DVE, lambda b=b: nc.vector.tensor_scalar(out=xhf[:], in0=x1t[b][:], scalar1=bufs["rs"][:], scalar2=None, op0=ALU.mult),
                     r=[("x1t", b), "rs"], w=["xhf"])
                for kc in range(8):
                    T.op(PE, lambda kc=kc: nc.tensor.transpose(PS[6 + kc // 4][:, (kc % 4) * 128:(kc % 4 + 1) * 128], xhf[:, kc * 128:(kc + 1) * 128], ident_f),
                         r=["xhf", "cst"], w=[("ps", 6 + kc // 4, kc % 4)])
                for kc in range(8):
                    T.op(ACT, lambda kc=kc: nc.scalar.activation(
                        out=h2f[:, kc, :], in_=PS[6 + kc // 4][:, (kc % 4) * 128:(kc % 4 + 1) * 128], func=AF.Identity,
                        scale=a2[:, kc:kc + 1], bias=s2[:, kc:kc + 1]), r=[("ps", 6 + kc // 4, kc % 4), "a2", "modc"], w=[("h2f", kc)])
                T.op(DVE, lambda: nc.vector.tensor_tensor(out=xhf[:], in0=xhf[:], in1=a2b[:], op=ALU.mult), r=["xhf", ("a2b",)], w=["xhf"])
                T.op(POOL, lambda b=b: nc.gpsimd.tensor_tensor(out=x2t[b][:], in0=xhf[:], in1=s2b[:], op=ALU.add), r=["xhf", ("s2b",)], w=[("x2t", b)])
                T.dma(SP, X2_d[t * 128:(t + 1) * 128, :], x2t[b][:], r=[("x2t", b)], w=[("X2d", t)])
                for kc in range(8):
                    T.op(PE, lambda kc=kc: nc.tensor.matmul(PS[0][:, 0:36], lhsT=h2f[:, kc, :], rhs=wr[:, kc, :], start=(kc == 0), stop=(kc == 7)),
                         r=[("h2f", kc), "wrt"], w=[("ps", 0)])
                T.op(DVE, lambda: nc.vector.tensor_tensor(out=lg[:], in0=PS[0][:, 0:36], in1=brb[:], op=ALU.add), r=[("ps", 0), "brb"], w=["lg"])
                gl = lg[:, 0:4]
                el = lg[:, 4:36].rearrange("p (g j) -> p g j", g=4)
                R = lambda a_, b_: r8[:, a_:b_]
                T.op(DVE, lambda: nc.vector.tensor_reduce(out=R(0, 1), in_=gl, axis=AX.X, op=ALU.max), r=["lg"], w=[("r8", 0)])
                T.op(DVE, lambda: nc.vector.tensor_scalar(out=R(4, 8), in0=gl, scalar1=R(0, 1), scalar2=None, op0=ALU.is_ge), r=["lg", ("r8", 0)], w=[("r8", 1)])
                T.op(DVE, lambda: nc.vector.tensor_scalar(out=R(1, 2), in0=R(0, 1), scalar1=-1.0, scalar2=None, op0=ALU.mult), r=[("r8", 0)], w=[("r8", 2)])
                T.op(ACT, lambda: nc.scalar.activation(out=R(8, 12), in_=gl, func=AF.Exp, bias=R(1, 2), accum_out=R(2, 3)), r=["lg", ("r8", 2)], w=[("r8", 3)])
                T.op(DVE, lambda: nc.vector.reciprocal(out=R(3, 4), in_=R(2, 3)), r=[("r8", 3)], w=[("r8", 4)])
                T.op(DVE, lambda: nc.vector.tensor_tensor(out=R(16, 48).rearrange("p (g j) -> p g j", g=4), in0=el,
                                                          in1=R(4, 8).unsqueeze(2).to_broadcast([128, 4, 8]), op=ALU.mult),
                     r=["lg", ("r8", 1)], w=[("r8", 5)])
                T.op(DVE, lambda: nc.vector.tensor_reduce(out=R(48, 56), in_=R(16, 48).rearrange("p (g j) -> p j g", g=4), axis=AX.X, op=ALU.add),
                     r=[("r8", 5)], w=[("r8", 6)])
                T.op(DVE, lambda: nc.vector.max(out=R(56, 64), in_=R(48, 56)), r=[("r8", 6)], w=[("r8", 7)])
                T.op(DVE, lambda: nc.vector.tensor_scalar(out=R(64, 72), in0=R(48, 56), scalar1=R(56, 57), scalar2=None, op0=ALU.is_equal),
                     r=[("r8", 6), ("r8", 7)], w=[("r8", 9)])
                T.op(DVE, lambda: nc.vector.tensor_scalar(out=R(72, 80), in0=R(48, 56), scalar1=R(57, 58), scalar2=None, op0=ALU.is_equal),
                     r=[("r8", 6), ("r8", 7)], w=[("r8", 10)])
                T.op(DVE, lambda: nc.vector.tensor_tensor(out=R(1, 2), in0=R(57, 58), in1=R(56, 57), op=ALU.subtract), r=[("r8", 7), ("r8", 3)], w=[("r8", 2)])
                T.op(ACT, lambda: nc.scalar.activation(out=R(2, 3), in_=R(1, 2), func=AF.Exp), r=[("r8", 2), ("r8", 4)], w=[("r8", 3)])
                T.op(DVE, lambda: nc.vector.tensor_scalar(out=R(1, 2), in0=R(2, 3), scalar1=1.0, scalar2=None, op0=ALU.add), r=[("r8", 3)], w=[("r8", 2)])
                T.op(DVE, lambda: nc.vector.reciprocal(out=R(1, 2), in_=R(1, 2)), r=[("r8", 2)], w=[("r8", 2)])
                T.op(DVE, lambda t=t: nc.vector.tensor_tensor(out=w12[:, t, 0:1], in0=R(1, 2), in1=R(3, 4), op=ALU.mult), r=[("r8", 2), ("r8", 4)], w=[("w12", t, 0)])
                T.op(DVE, lambda t=t: nc.vector.tensor_tensor(out=w12[:, t, 1:2], in0=w12[:, t, 0:1], in1=R(2, 3), op=ALU.mult), r=[("w12", t, 0), ("r8", 3)], w=[("w12", t, 1)])
                for g in range(4):
                    T.op(DVE, lambda g=g, t=t: nc.vector.tensor_scalar(out=m1_all[:, t, g * 8:(g + 1) * 8], in0=R(64, 72), scalar1=R(4 + g, 5 + g), scalar2=None, op0=ALU.mult),
                         r=[("r8", 9), ("r8", 1)], w=[("m1", t, g)])
                    T.op(DVE, lambda g=g, t=t: nc.vector.tensor_scalar(out=m2_all[:, t, g * 8:(g + 1) * 8], in0=R(72, 80), scalar1=R(4 + g, 5 + g), scalar2=None, op0=ALU.mult),
                         r=[("r8", 10), ("r8", 1)], w=[("m2", t, g)])
                T.op(DVE, lambda t=t: nc.vector.tensor_tensor(out=ohb[:], in0=m1_all[:, t, :], in1=m2_all[:, t, :], op=ALU.add),
                     r=[("m1", t), ("m2", t)], w=["ohb"])
                T.op(PE, lambda: nc.tensor.matmul(PS[1][:, 0:32], lhsT=tri_b, rhs=ohb[:], start=True, stop=True), r=["ohb", "cstb"], w=[("ps", 1)])
                T.op(DVE, lambda t=t: nc.vector.tensor_tensor(out=rank_all[:, t, :], in0=PS[1][:, 0:32], in1=tot[:], op=ALU.add),
                     r=[("ps", 1), "tot"], w=[("rank", t)])
                T.op(DVE, lambda t=t: nc.vector.tensor_tensor(out=rank_all[:, t, :], in0=rank_all[:, t, :], in1=ohb[:], op=ALU.subtract),
                     r=[("rank", t), "ohb"], w=[("rank", t)])
                T.op(PE, lambda: nc.tensor.matmul(PS[1][:, 0:32], lhsT=ones_b, rhs=ohb[:], start=True, stop=True), r=["ohb", "cstb"], w=[("ps", 1)])
                T.op(DVE, lambda: nc.vector.tensor_tensor(out=tot[:], in0=PS[1][:, 0:32], in1=tot[:], op=ALU.add), r=[("ps", 1), "tot"], w=["tot"])
        T.barrier()
    if dbg and "x1" in dbg_out:
        dump_dram(dbg_out["x1"], x1_d, S, D, F32)
        T.dma(SP, dbg_out["tot"][:, :], tot[:], r=["tot"])
        T.dma(SP, dbg_out["rank"][:, :], rank_all[:].rearrange("p t e -> p (t e)"), r=[("rank",)])
        T.dma(SP, dbg_out["m1"][:, :], m1_all[:].rearrange("p t e -> p (t e)"), r=[("m1",)])
        T.dma(SP, dbg_out["m2"][:, :], m2_all[:].rearrange("p t e -> p (t e)"), r=[("m2",)])
        T.dma(SP, dbg_out["w12"][:, :], w12[:].rearrange("p t e -> p (t e)"), r=[("w12",)])
    if stage <= 5:
        return finish()

    scope_mark("phE")
    with ExitStack() as ph:
        def sb(name, shape, dt):
            return ph.enter_context(nc.sbuf_tensor(name, list(shape), dt))
        P12 = sb("P12", [128, 2, NT], I32)
        NSPL = int(os.environ.get("KSPLIT", 1))
        widx = sb("widx", [128, NSPL, NSLOT], I32)
        with ExitStack() as ph2:
            def sb2(name, shape, dt):
                return ph2.enter_context(nc.sbuf_tensor("s_" + name, list(shape), dt))
            ni = sb2("ni", [128, 32], I32)
            ntf = sb2("ntf", [128, 32], F32)
            onesr = sb2("ones32", [128, 32], F32)
            tend = sb2("tend", [128, 32], F32)
            off = sb2("off", [128, 32], F32)
            big = sb2("big", [128, NT, 32], F32)
            pf = sb2("pf", [128, 2, NT], F32)
            cmp3 = sb2("cmp3", [128, NSLOT, 32], F32)
            sidx = sb2("sidx", [128, NSLOT], F32)
            esl = sb2("esl", [128, NSLOT], F32)
            pio = sb2("pio", [128, 1], F32)
            pioi = sb2("pioi", [128, 1], I32)
            T.op(DVE, lambda: nc.vector.tensor_scalar(out=ntf[:], in0=tot[:], scalar1=127.0, scalar2=None, op0=ALU.add), r=["tot"], w=["ntf"])
            T.op(DVE, lambda: nc.vector.tensor_copy(out=ni[:], in_=ntf[:]), r=["ntf"], w=["ni"])
            T.op(DVE, lambda: nc.vector.tensor_scalar(out=ni[:], in0=ni[:], scalar1=7, scalar2=None, op0=ALU.arith_shift_right),
                 r=["ni"], w=["ni"])
            T.op(DVE, lambda: nc.vector.tensor_copy(out=ntf[:], in_=ni[:]), r=["ni"], w=["ntf"])
            T.op(DVE, lambda: nc.vector.memset(onesr[:], 1.0), w=["ones32"])
            T.op(DVE, lambda: nc.vector.tensor_tensor_scan(out=tend[:], data0=onesr[:], data1=ntf[:], initial=0.0, op0=ALU.mult, op1=ALU.add),
                 r=["ones32", "ntf"], w=["tend"])
            T.op(DVE, lambda: nc.vector.tensor_tensor(out=off[:], in0=tend[:], in1=ntf[:], op=ALU.subtract), r=["tend", "ntf"], w=["off"])
            T.op(DVE, lambda: nc.vector.tensor_scalar(out=off[:], in0=off[:], scalar1=128.0, scalar2=None, op0=ALU.mult), r=["off"], w=["off"])
            for k, mk in enumerate((m1_all, m2_all)):
                T.op(DVE, lambda: nc.vector.tensor_tensor(out=big[:], in0=rank_all[:], in1=off[:].unsqueeze(1).to_broadcast([128, NT, 32]), op=ALU.add),
                     r=[("rank",), "off"], w=["big"])
                T.op(DVE, lambda mk=mk: nc.vector.tensor_tensor(out=big[:], in0=big[:], in1=mk[:], op=ALU.mult), r=["big", ("m1",), ("m2",)], w=["big"])
                T.op(DVE, lambda k=k: nc.vector.tensor_reduce(out=pf[:, k, :], in_=big[:], axis=AX.X, op=ALU.add), r=["big"], w=[("pf", k)])
            T.op(DVE, lambda: nc.vector.tensor_scalar(out=pf[:], in0=pf[:], scalar1=0.0, scalar2=float(NSLOT * 128 - 1), op0=ALU.max, op1=ALU.min),
                 r=[("pf",)], w=[("pf",)])
            T.op(DVE, lambda: nc.vector.tensor_copy(out=P12[:], in_=pf[:]), r=[("pf",)], w=["P12"])
            T.op(POOL, lambda: nc.gpsimd.iota(out=sidx[:], pattern=[[1, NSLOT]], base=0, channel_multiplier=0, allow_small_or_imprecise_dtypes=True), w=["sidx"])
            T.op(POOL, lambda: nc.gpsimd.iota(out=pio[:], pattern=[[1, 1]], base=0, channel_multiplier=1, allow_small_or_imprecise_dtypes=True), w=["pio"])
            T.op(DVE, lambda: nc.vector.tensor_tensor(out=cmp3[:], in0=tend[:].unsqueeze(1).to_broadcast([128, NSLOT, 32]),
                                                      in1=sidx[:].unsqueeze(2).to_broadcast([128, NSLOT, 32]), op=ALU.is_le),
                 r=["tend", "sidx"], w=["cmp3"])
            T.op(DVE, lambda: nc.vector.tensor_reduce(out=esl[:], in_=cmp3[:], axis=AX.X, op=ALU.add), r=["cmp3"], w=["esl"])
            T.op(DVE, lambda: nc.vector.tensor_scalar(out=esl[:], in0=esl[:], scalar1=31.0, scalar2=128.0, op0=ALU.min, op1=ALU.mult), r=["esl"], w=["esl"])
            T.op(DVE, lambda: nc.vector.tensor_scalar(out=esl[:], in0=esl[:], scalar1=pio[:], scalar2=None, op0=ALU.add), r=["esl", "pio"], w=["esl"])
            for ci in range(NSPL):
                T.op(DVE, lambda ci=ci: nc.vector.tensor_scalar(out=sidx[:], in0=esl[:], scalar1=float(NSPL), scalar2=float(ci), op0=ALU.mult, op1=ALU.add),
                     r=["esl", "cmp3"], w=["sidx"])
                T.op(DVE, lambda ci=ci: nc.vector.tensor_copy(out=widx[:, ci, :], in_=sidx[:]), r=["sidx"], w=[("widx", ci)])
            T.barrier()
        if dbg and "P12" in dbg_out:
            T.dma(SP, dbg_out["P12"][:, :], P12[:].rearrange("p k t -> p (k t)"), r=["P12"])
            T.dma(SP, dbg_out["widx"][:, :], widx[:, 0, :], r=["widx"])

        KE = int(os.environ.get("KE", 9))
        if KE >= 2:
            x2l = [sb(f"x2l{i}", [128, D], BF16) for i in range(3)]
            for t in range(NT):
                b = t % 3
                T.dma(SP, x2l[b][:], X2_d[t * 128:(t + 1) * 128, :], w=[("x2l", b)])
                for k in range(2):
                    T.dmai(Xs_d[:, :], x2l[b][:], out_idx=P12[:, k, t:t + 1], r=[("x2l", b), "P12"], w=[("Xs", t, k)])
            T.barrier()

        if KE >= 3:
            xs = [sb(f"xs{i}", [128, D], BF16) for i in range(3)]
            wsl = [sb(f"wsl{i}", [128, 6144], BF16) for i in range(3)]
            xT = [sb(f"xTs{i}", [128, 8, 128], BF16) for i in range(2)]
            sS = [sb(f"sS{i}", [128, 256], F32) for i in range(2)]
            ac = [sb(f"ac{i}", [128, 256], BF16) for i in range(2)]
            aT = [sb(f"aTs{i}", [128, 2, 128], BF16) for i in range(2)]
            yo = [sb(f"yo{i}", [128, D], BF16) for i in range(2)]
            NSL = int(os.environ.get("KSLOTS", NSLOT))

            def load_slot(sl):
                b3 = sl % 3
                T.dma(SP, xs[b3][:], Xs_d[sl * 128:(sl + 1) * 128, :], r=[("Xs",)], w=[("xs", b3)])
                cw_ = 6144 // NSPL
                for ci in range(NSPL):
                    T.dmai(wsl[b3][:, ci * cw_:(ci + 1) * cw_], Wcat_d.rearrange("r (c w) -> (r c) w", c=NSPL), in_idx=widx[:, ci, sl:sl + 1],
                           r=[("Wcat",), "widx"], w=[("wsl", b3, ci)])

            def slot_s1(sl):
                b2, b3 = sl % 2, sl % 3
                base = 4 * b2
                xTp = psb(base).rearrange("p (k t) -> p k t", k=8)
                for kc in range(8):
                    T.op(PE, lambda kc=kc: nc.tensor.transpose(xTp[:, kc, :], xs[b3][:, kc * 128:(kc + 1) * 128], ident_b),
                         r=[("xs", b3), "cstb"], w=[("ps", base)])
                T.op(ACT, lambda: nc.scalar.copy(out=xT[b2][:, 0:4, :], in_=xTp[:, 0:4, :]), r=[("ps", base)], w=[("xTs", b2, 0)])
                T.op(DVE, lambda: nc.vector.tensor_copy(out=xT[b2][:, 4:8, :], in_=xTp[:, 4:8, :]), r=[("ps", base), ("xTs", b2, 0)], w=[("xTs", b2, 1)])
                for kc in range(8):
                    T.op(PE, lambda kc=kc: nc.tensor.matmul(PS[base + 1][:, :], lhsT=xT[b2][:, kc, :], rhs=wsl[b3][:, kc * 512:(kc + 1) * 512],
                                                            start=(kc == 0), stop=(kc == 7)), r=[("xTs", b2), ("wsl", b3)], w=[("ps", base + 1)])

            def slot_s2(sl):
                b2, b3 = sl % 2, sl % 3
                base = 4 * b2
                T.op(ACT, lambda: nc.scalar.activation(out=sS[b2][:], in_=PS[base + 1][:, 0:256], func=AF.Silu), r=[("ps", base + 1)], w=[("sS", b2)])
                T.op(DVE, lambda: nc.vector.tensor_tensor(out=ac[b2][:], in0=PS[base + 1][:, 256:512], in1=sS[b2][:], op=ALU.mult),
                     r=[("ps", base + 1), ("sS", b2)], w=[("ac", b2)])
                aTp = psb(base)[:, 0:256].rearrange("p (k t) -> p k t", k=2)
                for fc in range(2):
                    T.op(PE, lambda fc=fc: nc.tensor.transpose(aTp[:, fc, :], ac[b2][:, fc * 128:(fc + 1) * 128], ident_b),
                         r=[("ac", b2), "cstb"], w=[("ps", base)])
                T.op(ACT, lambda: nc.scalar.copy(out=aT[b2][:], in_=aTp), r=[("ps", base)], w=[("aTs", b2)])
                for cb_ in range(2):
                    for fc in range(2):
                        T.op(PE, lambda fc=fc, cb_=cb_: nc.tensor.matmul(
                            PS[base + 2 + cb_][:, :], lhsT=aT[b2][:, fc, :], rhs=wsl[b3][:, 4096 + fc * 1024 + cb_ * 512: 4096 + fc * 1024 + (cb_ + 1) * 512],
                            start=(fc == 0), stop=(fc == 1)), r=[("aTs", b2), ("wsl", b3)], w=[("ps", base + 2 + cb_)])
                T.op(ACT, lambda: nc.scalar.copy(out=yo[b2][:, 0:512], in_=PS[base + 2][:, :]), r=[("ps", base + 2)], w=[("yo", b2, 0)])
                T.op(DVE, lambda: nc.vector.tensor_copy(out=yo[b2][:, 512:1024], in_=PS[base + 3][:, :]), r=[("ps", base + 3)], w=[("yo", b2, 1)])
                T.dma(SP, Ys_d[sl * 128:(sl + 1) * 128, :], yo[b2][:], r=[("yo", b2)], w=[("Ys", sl)])

            for sl in range(min(2, NSL)):
                load_slot(sl)
            slot_s1(0)
            for sl in range(NSL):
                if sl + 2 < NSL:
                    load_slot(sl + 2)
                if sl + 1 < NSL:
                    slot_s1(sl + 1)
                slot_s2(sl)
            T.barrier()

        if KE >= 4:
            y1 = [sb(f"y1_{i}", [128, D], BF16) for i in range(3)]
            ya = [sb(f"ya_{i}", [128, D], F32) for i in range(3)]
            y2 = [sb(f"y2_{i}", [128, D], BF16) for i in range(3)]
            x1l = [sb(f"x1l{i}", [128, D], F32) for i in range(3)]
            def load_tok(t):
                b = t % 3
                T.dma(SP, x1l[b][:], x1_d[t * 128:(t + 1) * 128, :], w=[("x1l", b)])
                T.dmai(y1[b][:], Ys_d[:, :], in_idx=P12[:, 0, t:t + 1], r=[("Ys",), "P12"], w=[("y1", b)])
                T.dmai(y2[b][:], Ys_d[:, :], in_idx=P12[:, 1, t:t + 1], r=[("Ys",), "P12"], w=[("y2", b)])

            load_tok(0)
            load_tok(1)
            for t in range(NT):
                b = t % 3
                if t + 2 < NT:
                    load_tok(t + 2)
                T.op(ACT, lambda b=b, t=t: nc.scalar.activation(out=ya[b][:], in_=y1[b][:], func=AF.Copy, scale=w12[:, t, 0:1]),
                     r=[("y1", b), ("w12",)], w=[("ya", b)])
                T.op(DVE, lambda b=b, t=t: nc.vector.scalar_tensor_tensor(out=ya[b][:], in0=y2[b][:], scalar=w12[:, t, 1:2], in1=ya[b][:], op0=ALU.mult, op1=ALU.add),
                     r=[("ya", b), ("y2", b), ("w12",)], w=[("ya", b)])
                T.op(DVE, lambda b=b: nc.vector.tensor_tensor(out=ya[b][:], in0=ya[b][:], in1=g2b[:], op=ALU.mult), r=[("ya", b), ("g2b",)], w=[("ya", b)])
                T.op(DVE, lambda b=b: nc.vector.tensor_tensor(out=x1l[b][:], in0=x1l[b][:], in1=ya[b][:], op=ALU.add), r=[("ya", b), ("x1l", b)], w=[("x1l", b)])
                T.dma(SP, out_d[t * 128:(t + 1) * 128, :], x1l[b][:], r=[("x1l", b)], w=[("out", t)])
            T.barrier()
    return finish()


def _consts():
    c = np.zeros((128, NCST), np.float32)
    c[:, 0:128] = np.eye(128, dtype=np.float32)
    s = np.arange(128)[:, None]
    t = np.arange(128)[None, :]
    c[:, 128:256] = (s <= t).astype(np.float32)
    c[:, 256:384] = ((s // 64) == (t // 64)).astype(np.float32)
    R = np.zeros((128, 128), np.float32)
    for base in (0, 64):
        for d in range(8):
            R[base + d, base + d + 8] = -1.0
            R[base + d + 8, base + d] = 1.0
    c[:, 384:512] = R.T
    c[:, 512:640] = 1.0
    inv = 500000.0 ** (-np.arange(0, 16, 2, dtype=np.float32) / 16.0)
    for p in range(128):
        d = p % 64
        c[p, 640] = inv[d % 8] if d < 16 else 0.0
    return c


_NC_CACHE = {}


def make_in_maps(inputs):
    f = lambda k: np.ascontiguousarray(np.asarray(inputs[k], dtype=np.float32))
    x = f("x")
    c = f("c")
    pos = np.ascontiguousarray(np.asarray(inputs["positions"], dtype=np.int32))
    b_ada = f("b_ada")[0]
    shared = {
        "w_ada": f("w_ada")[0],
        "b_ada_col": np.ascontiguousarray(b_ada.reshape(48, 128).T),
        "b_ada_row": np.ascontiguousarray(b_ada.reshape(1, -1)),
        "n1w": np.ascontiguousarray(f("norm1_w")[0].reshape(8, 128).T),
        "n2w": np.ascontiguousarray(f("norm2_w")[0].reshape(8, 128).T),
        "n2row": np.ascontiguousarray(f("norm2_w")[0].reshape(1, D)),
        "w_in": f("w_in")[0],
        "bgate": np.ascontiguousarray(np.stack([f("b_igate")[0], f("b_fgate")[0]], axis=1)),
        "convw": np.ascontiguousarray(f("conv_w")[0].T.reshape(8, 128, 4).transpose(1, 0, 2)),
        "convb": np.ascontiguousarray(f("conv_b")[0].reshape(8, 128).T),
        "mnw": np.ascontiguousarray(f("mlstm_norm_w")[0].reshape(1, 512)),
        "qnw": np.ascontiguousarray(np.tile(f("q_norm_w")[0], 2).reshape(128, 1)),
        "knw": np.ascontiguousarray(np.tile(f("k_norm_w")[0], 2).reshape(128, 1)),
        "qkrow": np.ascontiguousarray(np.concatenate([f("q_norm_w")[0], f("k_norm_w")[0]]).reshape(1, 128)),
        "lamv": np.ascontiguousarray(np.concatenate([f("lam_q1")[0], f("lam_k1")[0], f("lam_q2")[0], f("lam_k2")[0]]).reshape(1, 256)),
        "subw": np.ascontiguousarray(f("subln_w")[0].reshape(128, 1)),
        "w_br_m": f("w_br_m")[0],
        "w_br_d": f("w_br_d")[0],
        "w_out": f("w_out")[0],
        "wr": np.ascontiguousarray(np.concatenate([f("w_rg")[0], f("w_re")[0]], axis=1)),
        "br": np.ascontiguousarray(np.concatenate([f("b_rg")[0], f("b_re")[0]]).reshape(1, 36)),
        "w_gate": f("w_gate")[0],
        "w_up": f("w_up")[0],
        "w_down": f("w_down")[0],
        "consts": _consts(),
    }
    maps = []
    for b in range(8):
        m = dict(shared)
        m["x"] = x[b]
        m["cT"] = np.ascontiguousarray(c[b].reshape(8, 128).T)
        m["pos"] = np.ascontiguousarray(pos[b].reshape(1, S))
        maps.append(m)
    return maps


def kernel(**inputs):
    if "nc" not in _NC_CACHE:
        _NC_CACHE["nc"] = build()
    nc = _NC_CACHE["nc"]
    maps = make_in_maps(inputs)
    res = run_bass_kernel_spmd(nc, maps, core_ids=list(range(8)))
    out = np.stack([np.asarray(r["out"], dtype=np.float32) for r in res.results], axis=0)
    return out
```
